# Optimizing a Trainium2 kernel written in Bass

```python
import math
import jax, jax.numpy as jnp
from jax import lax
import numpy as np

D_MODEL = 1024
BATCH = 32
SEQ = 2048
DEPTH = 1

RW_HEADS = 8
RW_HEAD = 64
RW_WIDTH = RW_HEADS * RW_HEAD
DECAY_LORA = 64
ICLR_LORA = 64
GATE_LORA = 128
DA_HEADS = 4
DA_HEAD = 64
DA_VDIM = 2 * DA_HEAD
DA_QK_WIDTH = DA_HEADS * 2 * DA_HEAD
DA_WIDTH = DA_HEADS * DA_VDIM
Q_BLOCK = 128
PK_HEADS = 8
PK_NKEYS = 128
PK_QDIM = 256
PK_TOPK = 16
PK_EXPERTS = PK_NKEYS * PK_NKEYS
PK_CHUNK = 128
NORM_EPS = 1e-6
GN_EPS = 64e-5
SUBLN_EPS = 1e-5
SHIFT_COLS = 3 * RW_WIDTH + DECAY_LORA + ICLR_LORA + GATE_LORA
GATE_COLS = 2 * D_MODEL
IN_COLS = SHIFT_COLS + 2 * DA_QK_WIDTH + DA_WIDTH + GATE_COLS

kernel_name = 'hybrid_rwkv7_diffattn_peer_block'


def rmsnorm(x, w, eps=NORM_EPS):
    xf = x.astype(jnp.float32)
    y = xf * lax.rsqrt(jnp.mean(xf * xf, axis=-1, keepdims=True) + eps)
    return (y * w.astype(jnp.float32)).astype(x.dtype)


def token_shift(z, mu):
    z_prev = jnp.pad(z, ((0, 0), (1, 0), (0, 0)))[:, :-1]
    return z + (z_prev - z) * mu


def rwkv7_time_mix(p, w0, w2, a0, a2, g2, k_k, k_a, r_k, lnx_w, lnx_b):
    B, S, _ = p.shape
    f32 = jnp.float32
    c = [RW_WIDTH, 2 * RW_WIDTH, 3 * RW_WIDTH, 3 * RW_WIDTH + DECAY_LORA, 3 * RW_WIDTH + DECAY_LORA + ICLR_LORA]
    r, k, v, wl, al, gl = jnp.split(p.astype(f32), c, axis=-1)
    w = -jax.nn.softplus(-(w0 + jnp.tanh(wl) @ w2)) - 0.5
    decay = jnp.exp(-jnp.exp(w))
    a = jax.nn.sigmoid(a0 + al @ a2)
    g = jax.nn.sigmoid(gl) @ g2
    heads = lambda t: t.reshape(B, S, RW_HEADS, RW_HEAD)
    kk = heads(k * k_k)
    kk = kk / jnp.maximum(jnp.sqrt(jnp.sum(kk * kk, axis=-1, keepdims=True)), 1e-12)
    k = k * (1.0 + (a - 1.0) * k_a)
    r, k, v, a, decay = heads(r), heads(k), heads(v), heads(a), heads(decay)

    def step(state, inp):
        r_t, w_t, k_t, v_t, kk_t, a_t = inp
        sa = jnp.einsum('bhvk,bhk->bhv', state, -kk_t)
        state = (state * w_t[:, :, None, :]
                 + sa[..., None] * (kk_t * a_t)[:, :, None, :]
                 + v_t[..., None] * k_t[:, :, None, :])
        return state, jnp.einsum('bhvk,bhk->bhv', state, r_t)

    tm = lambda t: jnp.moveaxis(t, 1, 0)
    state0 = jnp.zeros((B, RW_HEADS, RW_HEAD, RW_HEAD), f32)
    _, y = lax.scan(step, state0, (tm(r), tm(decay), tm(k), tm(v), tm(kk), tm(a)))
    y = jnp.moveaxis(y, 0, 1)
    mu = jnp.mean(y, axis=-1, keepdims=True)
    var = jnp.mean(jnp.square(y - mu), axis=-1, keepdims=True)
    y = ((y - mu) * lax.rsqrt(var + GN_EPS)).reshape(B, S, RW_WIDTH) * lnx_w + lnx_b
    bonus = jnp.sum(r * k * r_k, axis=-1, keepdims=True) * v
    return (y + bonus.reshape(B, S, RW_WIDTH)) * g


def diff_attention(q, k, v, lam_q1, lam_k1, lam_q2, lam_k2, subln_w, lam_init):
    B, S, _ = q.shape
    f32 = jnp.float32
    q = q.reshape(B, S, DA_HEADS, 2, DA_HEAD).astype(f32)
    k = k.reshape(B, S, DA_HEADS, 2, DA_HEAD).astype(f32)
    v = v.reshape(B, S, DA_HEADS, DA_VDIM).astype(f32)
    lam = (jnp.exp(jnp.sum(lam_q1.astype(f32) * lam_k1.astype(f32)))
           - jnp.exp(jnp.sum(lam_q2.astype(f32) * lam_k2.astype(f32))) + lam_init)
    slopes = jnp.asarray(2.0 ** (-8.0 * np.arange(1, DA_HEADS + 1) / DA_HEADS), dtype=f32)
    scale = 1.0 / math.sqrt(DA_HEAD)
    nb = S // Q_BLOCK
    qb = jnp.moveaxis(q.reshape(B, nb, Q_BLOCK, DA_HEADS, 2, DA_HEAD), 1, 0)
    kpos = jnp.arange(S)

    def block(args):
        qi, bi = args
        scores = jnp.einsum('bqhcd,bkhcd->bhcqk', qi, k) * scale
        qpos = bi * Q_BLOCK + jnp.arange(Q_BLOCK)
        dist = (qpos[:, None] - kpos[None, :]).astype(f32)
        scores = scores - (slopes[:, None, None] * dist)[None, :, None]
        scores = jnp.where((dist >= 0)[None, None, None], scores, -jnp.inf)
        pr = jax.nn.softmax(scores, axis=-1)
        attn = pr[:, :, 0] - lam * pr[:, :, 1]
        return jnp.einsum('bhqk,bkhe->bqhe', attn, v)

    o = lax.map(block, (qb, jnp.arange(nb)))
    o = jnp.moveaxis(o, 0, 1).reshape(B, S, DA_HEADS, DA_VDIM)
    o = rmsnorm(o, subln_w, SUBLN_EPS) * (1.0 - lam_init)
    return o.reshape(B, S, DA_WIDTH)


def peer_ffn(h, w_q, sub_keys, u_tab, v_tab):
    B, S, D = h.shape
    T = B * S
    hf = h.reshape(T, D)
    q = (hf @ w_q).reshape(T, PK_HEADS, 2, PK_QDIM // 2).astype(jnp.float32)
    s = jnp.einsum('thcd,hcnd->thcn', q, sub_keys.astype(jnp.float32))
    v1, i1 = lax.top_k(s[:, :, 0], PK_TOPK)
    v2, i2 = lax.top_k(s[:, :, 1], PK_TOPK)
    cand = (v1[..., :, None] + v2[..., None, :]).reshape(T, PK_HEADS, PK_TOPK * PK_TOPK)
    vals, ci = lax.top_k(cand, PK_TOPK)
    e1 = jnp.take_along_axis(i1, ci // PK_TOPK, axis=-1)
    e2 = jnp.take_along_axis(i2, ci % PK_TOPK, axis=-1)
    ids = (e1 * PK_NKEYS + e2).reshape(T, PK_HEADS * PK_TOPK)
    gates = jax.nn.softmax(vals, axis=-1).reshape(T, PK_HEADS * PK_TOPK).astype(h.dtype)
    nc = T // PK_CHUNK

    def chunk(args):
        xc, idc, gc = args
        act = jnp.einsum('ced,cd->ce', u_tab[idc], xc)
        act = jax.nn.gelu(act, approximate=False) * gc
        return jnp.einsum('ce,ced->cd', act, v_tab[idc])

    y = lax.map(chunk, (hf.reshape(nc, PK_CHUNK, D),
                        ids.reshape(nc, PK_CHUNK, PK_HEADS * PK_TOPK),
                        gates.reshape(nc, PK_CHUNK, PK_HEADS * PK_TOPK)))
    return y.reshape(B, S, D)


def setup_inputs(seed: int = 0) -> dict:
    key = jax.random.key(seed)
    ks = jax.random.split(key, 32)
    L = DEPTH
    f32 = jnp.float32
    nrm = lambda k, shape, sc: jax.random.normal(k, shape, f32) * sc
    return {
        'x': nrm(ks[0], (BATCH, SEQ, D_MODEL), 1.0),
        'norm_mix_w': 1.0 + nrm(ks[1], (L, D_MODEL), 0.1),
        'w_in': nrm(ks[2], (L, D_MODEL, IN_COLS), D_MODEL ** -0.5),
        'shift_mu': jax.random.uniform(ks[3], (L, SHIFT_COLS), f32, 0.1, 0.9),
        'w0': -1.0 + nrm(ks[4], (L, RW_WIDTH), 1.0),
        'w2': nrm(ks[5], (L, DECAY_LORA, RW_WIDTH), 0.1),
        'a0': nrm(ks[6], (L, RW_WIDTH), 0.1),
        'a2': nrm(ks[7], (L, ICLR_LORA, RW_WIDTH), 0.1),
        'g2': nrm(ks[8], (L, GATE_LORA, RW_WIDTH), GATE_LORA ** -0.5),
        'k_k': 0.85 + nrm(ks[9], (L, RW_WIDTH), 0.05),
        'k_a': 1.0 + nrm(ks[10], (L, RW_WIDTH), 0.05),
        'r_k': nrm(ks[11], (L, RW_HEADS, RW_HEAD), 0.1),
        'lnx_w': 1.0 + nrm(ks[12], (L, RW_WIDTH), 0.1),
        'lnx_b': nrm(ks[13], (L, RW_WIDTH), 0.02),
        'lam_q1': nrm(ks[14], (L, DA_HEAD), 0.1),
        'lam_k1': nrm(ks[15], (L, DA_HEAD), 0.1),
        'lam_q2': nrm(ks[16], (L, DA_HEAD), 0.1),
        'lam_k2': nrm(ks[17], (L, DA_HEAD), 0.1),
        'subln_w': 1.0 + nrm(ks[18], (L, DA_VDIM), 0.1),
        'proj_a': nrm(ks[19], (L, RW_WIDTH, D_MODEL), RW_WIDTH ** -0.5),
        'proj_b': nrm(ks[20], (L, DA_WIDTH, D_MODEL), DA_WIDTH ** -0.5),
        'w_out': nrm(ks[21], (L, D_MODEL, D_MODEL), D_MODEL ** -0.5),
        'norm_ffn_w': 1.0 + nrm(ks[22], (L, D_MODEL), 0.1),
        'peer_wq': nrm(ks[23], (L, D_MODEL, PK_HEADS * PK_QDIM), D_MODEL ** -0.5),
        'peer_keys': nrm(ks[24], (L, PK_HEADS, 2, PK_NKEYS, PK_QDIM // 2), (PK_QDIM // 2) ** -0.5),
        'peer_u': nrm(ks[25], (L, PK_EXPERTS, D_MODEL), D_MODEL ** -0.5),
        'peer_v': nrm(ks[26], (L, PK_EXPERTS, D_MODEL), 0.3),
        'final_norm_w': 1.0 + nrm(ks[27], (D_MODEL,), 0.1),
    }


def reference(x, norm_mix_w, w_in, shift_mu, w0, w2, a0, a2, g2, k_k, k_a, r_k, lnx_w, lnx_b,
              lam_q1, lam_k1, lam_q2, lam_k2, subln_w, proj_a, proj_b, w_out,
              norm_ffn_w, peer_wq, peer_keys, peer_u, peer_v, final_norm_w):
    c1 = SHIFT_COLS
    c2 = c1 + DA_QK_WIDTH
    c3 = c2 + DA_QK_WIDTH
    c4 = c3 + DA_WIDTH
    for l in range(DEPTH):
        lam_init = 0.8 - 0.6 * math.exp(-0.3 * l)
        h = rmsnorm(x, norm_mix_w[l])
        z = h @ w_in[l]
        zs, zq, zk, zv, zg = jnp.split(z, [c1, c2, c3, c4], axis=-1)
        zs = token_shift(zs, shift_mu[l])
        ya = rwkv7_time_mix(zs, w0[l], w2[l], a0[l], a2[l], g2[l], k_k[l], k_a[l], r_k[l],
                            lnx_w[l], lnx_b[l]).astype(x.dtype)
        yb = diff_attention(zq, zk, zv, lam_q1[l], lam_k1[l], lam_q2[l], lam_k2[l], subln_w[l],
                            lam_init).astype(x.dtype)
        ga, gb = jnp.split(zg, 2, axis=-1)
        merged = jax.nn.sigmoid(ga) * (ya @ proj_a[l]) + jax.nn.sigmoid(gb) * (yb @ proj_b[l])
        x = x + merged @ w_out[l]
        x = x + peer_ffn(rmsnorm(x, norm_ffn_w[l]), peer_wq[l], peer_keys[l], peer_u[l], peer_v[l])
    return rmsnorm(x, final_norm_w)
```

```python
import numpy as np
import ml_dtypes
from contextlib import ExitStack
import concourse.bass as bass
import concourse.mybir as mybir
from concourse.bass_utils import run_bass_kernel_spmd

F32 = mybir.dt.float32
BF16 = mybir.dt.bfloat16
I32 = mybir.dt.int32
U32 = mybir.dt.uint32
ALU = mybir.AluOpType
AF = mybir.ActivationFunctionType
AX = mybir.AxisListType

D = 1024
IN_COLS = 5376
SHIFT_COLS = 1792
NCORES = 8


class Dep:
    __slots__ = ("w", "r", "pw", "pr")

    def __init__(self):
        self.w = {}
        self.r = {}
        self.pw = {}
        self.pr = {}


class Stream:
    def __init__(self, K, name, is_pe=False, ndma=0):
        self.K = K
        self.name = name
        self.sem = K.new_sem("s_" + name)
        self.cnt = 0
        self.items = []
        self.waited = {}
        self.is_pe = is_pe
        self.dsems = [K.new_sem("d_%s%d" % (name, i)) for i in range(ndma)]
        self.duses = [0] * ndma
        self.dj = 0

    def wait_tok(self, tok):
        if tok is None:
            return
        sem, val = tok
        if sem is self.sem and self.is_pe:
            return
        key = id(sem)
        if self.waited.get(key, 0) >= val:
            return
        self.waited[key] = val
        self.items.append(("w", sem, val, self.K.phase))

    def _pre(self, reads, writes, adds):
        for d in reads:
            for t in list(d.w.values()):
                self.wait_tok(t)
        for d in writes:
            for t in list(d.w.values()):
                self.wait_tok(t)
            for t in list(d.r.values()):
                self.wait_tok(t)
        for d in adds:
            for t in list(d.r.values()) + list(d.pr.values()) + list(d.pw.values()):
                self.wait_tok(t)

    def _post(self, tok, reads, writes, adds):
        for d in reads:
            d.r[id(tok[0])] = tok
        for d in writes:
            d.pw = d.w
            d.pr = d.r
            d.w = {id(tok[0]): tok}
            d.r = {}
        for d in adds:
            d.w[id(tok[0])] = tok

    def op(self, fn, reads=(), writes=(), adds=()):
        self._pre(reads, writes, adds)
        self.cnt += 1
        tok = (self.sem, self.cnt)
        self.items.append(("o", fn, self.sem, 1, self.K.phase))
        self._post(tok, reads, writes, adds)
        return tok

    def dma(self, fn, reads=(), writes=(), adds=()):
        self._pre(reads, writes, adds)
        n = len(self.dsems)
        slot = self.dj % n
        self.dj += 1
        if self.duses[slot] > 0:
            self.wait_tok((self.dsems[slot], 16 * self.duses[slot]))
        self.duses[slot] += 1
        tok = (self.dsems[slot], 16 * self.duses[slot])
        self.items.append(("o", fn, self.dsems[slot], 16, self.K.phase))
        self._post(tok, reads, writes, adds)
        return tok

    def replay(self, eng):
        nc = self.K.nc
        cur = None
        ctx = None
        for it in self.items:
            ph = it[-1]
            if self.K.scopes and ph != cur:
                if ctx is not None:
                    ctx.__exit__(None, None, None)
                ctx = nc.named_scope(ph)
                ctx.__enter__()
                cur = ph
            if it[0] == "w":
                eng.wait_ge(it[1], it[2])
            else:
                ins = it[1](eng)
                ins.then_inc(it[2], it[3])
        if ctx is not None:
            ctx.__exit__(None, None, None)


class Kern:
    def __init__(self, nc, es, pool_slots=8):
        self.nc = nc
        self.es = es
        self.nsem = 0
        self.phase = "init"
        self.scopes = False
        self.pe = Stream(self, "pe", is_pe=True)
        self.act = Stream(self, "act", ndma=4)
        self.dve = Stream(self, "dve")
        self.pool = Stream(self, "pool", ndma=pool_slots)
        self.sp = Stream(self, "sp", ndma=8)
        self.uid = 0

    def new_sem(self, name):
        self.nsem += 1
        return self.es.enter_context(self.nc.semaphore(name))

    def sb(self, shape, dt, name=None, es=None):
        self.uid += 1
        nm = "%s_%d" % (name or "t", self.uid)
        return (es or self.es).enter_context(self.nc.sbuf_tensor(nm, list(shape), dt))

    def ps(self, shape, dt, name=None, es=None):
        self.uid += 1
        nm = "%s_%d" % (name or "p", self.uid)
        return (es or self.es).enter_context(self.nc.psum_tensor(nm, list(shape), dt))

    def dram(self, name, shape, dt, kind="Internal"):
        return self.nc.dram_tensor(name, list(shape), dt, kind=kind)

    def streams(self):
        return [self.pe, self.act, self.dve, self.pool, self.sp]

    def barrier(self):
        st = self.streams()
        toks = []
        for q in st:
            if q.cnt > 0:
                toks.append((q.sem, q.cnt))
            for i, sem in enumerate(q.dsems):
                if q.duses[i] > 0:
                    toks.append((sem, 16 * q.duses[i]))
        for s_ in st:
            for t in toks:
                s_.wait_tok(t)

    def finish(self):
        streams = [self.pe, self.act, self.dve, self.pool, self.sp]
        for s in streams:
            for q in streams:
                for i, sem in enumerate(q.dsems):
                    if q.duses[i] > 0:
                        s.wait_tok((sem, 16 * q.duses[i]))
        with self.nc.allow_non_contiguous_dma(reason="small strided param loads"), self.nc.Block() as block:
            @block.tensor
            def _(e):
                self.pe.replay(e)

            @block.scalar
            def _(e):
                self.act.replay(e)

            @block.vector
            def _(e):
                self.dve.replay(e)

            @block.gpsimd
            def _(e):
                self.pool.replay(e)

            @block.sync
            def _(e):
                self.sp.replay(e)


class Rot:
    def __init__(self, K, n, shape, dt, name, psum=False, es=None):
        self.t = [(K.ps if psum else K.sb)(shape, dt, name, es=es) for _ in range(n)]
        self.d = [Dep() for _ in range(n)]
        self.i = 0

    def next(self):
        j = self.i % len(self.t)
        self.i += 1
        return self.t[j], self.d[j]


def phase1(K, cfg, io, scr):
    nc = K.nc
    T = cfg["T"]
    NT = T // 512
    with ExitStack() as es:
        ident = K.sb([128, 128], BF16, "ident", es)
        d_ident = Dep()
        K.sp.dma(lambda e: e.dma_start(out=ident[:], in_=io["ident_bf"][:, :]), writes=[d_ident])
        nw = K.sb([128, 8], F32, "nw", es)
        d_nw = Dep()
        K.sp.dma(lambda e: e.dma_start(out=nw[:], in_=io["norm_mix_w"].rearrange("o (c p) -> p (o c)", p=128)),
                 writes=[d_nw])
        wt = K.sb([128, 8, IN_COLS], BF16, "wt", es)
        d_wt = Dep()
        wst = Rot(K, 2, [128, 1344], F32, "wst", es=es)
        q = 0
        for kc in range(8):
            for cp in range(4):
                st, dst = wst.next()
                eng = K.sp if q % 2 == 0 else K.pool
                q += 1
                eng.dma(lambda e, st=st, kc=kc, cp=cp: e.dma_start(
                    out=st[:], in_=io["w_in"][0, kc * 128:(kc + 1) * 128, cp * 1344:(cp + 1) * 1344]), writes=[dst])
                K.act.op(lambda e, st=st, kc=kc, cp=cp: e.activation(
                    out=wt[:, kc, cp * 1344:(cp + 1) * 1344], in_=st[:], func=AF.Copy, scale=nw[:, kc:kc + 1]),
                    reads=[dst, d_nw], writes=[d_wt])

        xs = Rot(K, 2, [128, D], F32, "xs", es=es)
        junk = K.sb([128, D], BF16, "junk", es)
        d_junk = Dep()
        xn = Rot(K, 2, [128, D], BF16, "xn", es=es)
        st4 = Rot(K, 4, [128, 4], F32, "st4", es=es)
        hT = Rot(K, 2, [128, 8, 512], BF16, "hT", es=es)
        ptr = Rot(K, 2, [128, 8, 128], BF16, "ptr", psum=True, es=es)
        pmm = Rot(K, 4, [128, 512], F32, "pmm", psum=True, es=es)
        o32 = Rot(K, 3, [128, 512], F32, "o32", es=es)
        o16 = Rot(K, 3, [128, 512], BF16, "o16", es=es)
        ev = [0]

        def evac(pt, pd, ncols, kind, dst_ap):
            if kind == "f32":
                ot, od = o32.next()
            else:
                ot, od = o16.next()
            use_act = (kind == "sig") or (ev[0] % 2 == 0)
            ev[0] += 1
            if kind == "sig":
                K.act.op(lambda e: e.activation(out=ot[:, :ncols], in_=pt[:, :ncols], func=AF.Sigmoid),
                         reads=[pd], writes=[od])
            elif use_act:
                K.act.op(lambda e: e.activation(out=ot[:, :ncols], in_=pt[:, :ncols], func=AF.Copy),
                         reads=[pd], writes=[od])
            else:
                K.dve.op(lambda e: e.tensor_copy(out=ot[:, :ncols], in_=pt[:, :ncols]), reads=[pd], writes=[od])
            K.sp.dma(lambda e: e.dma_start(out=dst_ap, in_=ot[:, :ncols]), reads=[od], adds=[scr["d_p1"]])

        for ti in range(NT):
            h_t, h_d = hT.next()
            for sub in range(4):
                t0 = ti * 512 + sub * 128
                x_t, x_d = xs.next()
                K.pool.dma(lambda e, x_t=x_t, t0=t0: e.dma_start(out=x_t[:], in_=io["x"][t0:t0 + 128, :]),
                           writes=[x_d])
                s_t, s_d = st4.next()
                K.dve.op(lambda e, s_t=s_t: e.memset(s_t[:], 0.0), writes=[s_d])
                K.act.op(lambda e, x_t=x_t, s_t=s_t: e.activation(out=junk[:], in_=x_t[:], func=AF.Square,
                                                                    accum_out=s_t[:, 0:1]),
                         reads=[x_d], writes=[d_junk, s_d])
                K.dve.op(lambda e, s_t=s_t: e.tensor_scalar(out=s_t[:, 1:2], in0=s_t[:, 0:1], scalar1=1.0 / D,
                                                            scalar2=1e-6, op0=ALU.mult, op1=ALU.add),
                         reads=[s_d], writes=[s_d])
                K.act.op(lambda e, s_t=s_t: e.activation(out=s_t[:, 3:4], in_=s_t[:, 1:2], func=AF.Sqrt),
                         reads=[s_d], writes=[s_d])
                K.dve.op(lambda e, s_t=s_t: e.reciprocal(out=s_t[:, 2:3], in_=s_t[:, 3:4]),
                         reads=[s_d], writes=[s_d])
                n_t, n_d = xn.next()
                K.dve.op(lambda e, x_t=x_t, s_t=s_t, n_t=n_t: e.tensor_scalar(
                    out=n_t[:], in0=x_t[:], scalar1=s_t[:, 2:3], scalar2=None, op0=ALU.mult),
                    reads=[x_d, s_d], writes=[n_d])
                p_t, p_d = ptr.next()
                for kc in range(8):
                    K.pe.op(lambda e, p_t=p_t, n_t=n_t, kc=kc: e.transpose(
                        out=p_t[:, kc, :], in_=n_t[:, kc * 128:(kc + 1) * 128], identity=ident[:]),
                        reads=[n_d, d_ident], writes=[p_d])
                K.act.op(lambda e, p_t=p_t, h_t=h_t, sub=sub: e.activation(
                    out=h_t[:, :, sub * 128:(sub + 1) * 128], in_=p_t[:], func=AF.Copy),
                    reads=[p_d], writes=[h_d])
            tsl = slice(ti * 512, (ti + 1) * 512)
            for sub in range(4):
                r0 = ti * 512 + sub * 128
                for (c0, ncols, kind, name, dc0) in [(0, 512, "f32", "zs_tm", 0), (512, 512, "f32", "zs_tm", 512),
                                                    (1024, 512, "f32", "zs_tm", 1024),
                                                    (1536, 256, "f32", "zs_tm", 1536),
                                                    (2816, 512, "bf16", "av_tm", 0)]:
                    pt, pd = pmm.next()
                    for kc in range(8):
                        K.pe.op(lambda e, pt=pt, kc=kc, sub=sub, c0=c0, ncols=ncols, h_t=h_t: e.matmul(
                            pt[:, :ncols], h_t[:, kc, sub * 128:(sub + 1) * 128], wt[:, kc, c0:c0 + ncols],
                            start=(kc == 0), stop=(kc == 7)), reads=[h_d, d_wt], writes=[pd])
                    evac(pt, pd, ncols, kind, scr[name][r0:r0 + 128, dc0:dc0 + ncols])
            fm = []
            for j in range(8):
                fm.append((1792 + j * 128, "bf16", "qk_fm", j * 128))
            for j in range(16):
                fm.append((3328 + j * 128, "sig", "sg_fm", j * 128))
            for (c0, kind, name, r0) in fm:
                pt, pd = pmm.next()
                for kc in range(8):
                    K.pe.op(lambda e, pt=pt, kc=kc, c0=c0, h_t=h_t: e.matmul(
                        pt[:, :], wt[:, kc, c0:c0 + 128], h_t[:, kc, :], start=(kc == 0), stop=(kc == 7)),
                        reads=[h_d, d_wt], writes=[pd])
                evac(pt, pd, 512, kind, scr[name][r0:r0 + 128, tsl])


def dap(apobj, offset, dims):
    return bass.AP(tensor=apobj.tensor, offset=offset, ap=[list(d) for d in dims])


def bcast_load(K, eng, dst, src_row_ap, n, dep):
    eng.dma(lambda e: e.dma_start(out=dst, in_=src_row_ap.broadcast_to([128, n])), writes=[dep])


def phase2_prep(K, cfg, io, scr):
    T, S, NB = cfg["T"], cfg["S"], cfg["NB"]
    NTT = T // 128
    with ExitStack() as es:
        identb = K.sb([128, 128], BF16, "identb", es)
        identf = K.sb([128, 128], F32, "identf", es)
        d_c = Dep()
        K.sp.dma(lambda e: e.dma_start(out=identb[:], in_=io["ident_bf"][:, :]), adds=[d_c])
        K.sp.dma(lambda e: e.dma_start(out=identf[:], in_=io["ident_f"][:, :]), adds=[d_c])
        MU = K.sb([128, SHIFT_COLS], F32, "MU", es)
        PR = K.sb([128, 5, 512], F32, "PR", es)
        K.sp.dma(lambda e: e.dma_start(out=MU[:], in_=io["shift_mu"][0:1, :].broadcast_to([128, SHIFT_COLS])), adds=[d_c])
        for j, nm in enumerate(["w0", "a0", "k_k", "k_a"]):
            K.pool.dma(lambda e, j=j, nm=nm: e.dma_start(out=PR[:, j, :], in_=io[nm][0:1, :].broadcast_to([128, 512])),
                       adds=[d_c])
        K.pool.dma(lambda e: e.dma_start(out=PR[:, 4, :], in_=io["r_k"].rearrange("o h k -> o (h k)").broadcast_to([128, 512])),
                   adds=[d_c])
        cst = K.sb([128, 2], F32, "cst", es)
        K.dve.op(lambda e: e.memset(cst[:, 0:1], 1.0), adds=[d_c])
        K.dve.op(lambda e: e.memset(cst[:, 1:2], -0.5), adds=[d_c])
        wst = K.sb([128, 3, 512], F32, "lwst", es)
        d_wst = Dep()
        K.dve.op(lambda e: e.memset(wst[:], 0.0), writes=[d_wst])
        K.sp.dma(lambda e: e.dma_start(out=wst[0:64, 0, :], in_=io["w2"][0, :, :]), reads=[d_wst], adds=[d_wst])
        K.sp.dma(lambda e: e.dma_start(out=wst[64:128, 1, :], in_=io["a2"][0, :, :]), reads=[d_wst], adds=[d_wst])
        K.sp.dma(lambda e: e.dma_start(out=wst[:, 2, :], in_=io["g2"][0, :, :]), reads=[d_wst], adds=[d_wst])
        LW = K.sb([128, 3, 512], BF16, "LW", es)
        K.dve.op(lambda e: e.tensor_copy(out=LW[:], in_=wst[:]), reads=[d_wst], adds=[d_c])

        Zr = Rot(K, 2, [128, SHIFT_COLS], F32, "Z", es=es)
        Zpr = Rot(K, 2, [128, SHIFT_COLS], F32, "Zp", es=es)
        ZSr = Rot(K, 2, [128, SHIFT_COLS], F32, "ZS", es=es)
        OUTr = Rot(K, 2, [128, 5, 512], F32, "OUT", es=es)
        Er = Rot(K, 2, [128, 192], F32, "E", es=es)
        Lr = Rot(K, 2, [128, 256], BF16, "L", es=es)
        LTr = Rot(K, 2, [128, 2, 128], BF16, "LT", es=es)
        Ur = Rot(K, 2, [128, 512], F32, "U", es=es)
        UAr = Rot(K, 2, [128, 512], F32, "UA", es=es)
        KKr = Rot(K, 2, [128, 512], F32, "KKt", es=es)
        SQr = Rot(K, 2, [128, 512], F32, "SQ", es=es)
        T1r = Rot(K, 2, [128, 512], F32, "T1", es=es)
        T2r = Rot(K, 2, [128, 512], F32, "T2", es=es)
        S8r = Rot(K, 2, [128, 4, 8], F32, "S8", es=es)
        VTr = Rot(K, 2, [128, 4, 128], F32, "VT", es=es)
        GTr = Rot(K, 2, [128, 4, 128], F32, "GT", es=es)
        CTr = Rot(K, 2, [8, 128], F32, "CT", es=es)
        PT = Rot(K, 1, [128, 2, 128], BF16, "PT", psum=True, es=es)
        PW = Rot(K, 1, [128, 512], F32, "PW", psum=True, es=es)
        PA = Rot(K, 1, [128, 512], F32, "PA", psum=True, es=es)
        PG = Rot(K, 1, [128, 4, 128], F32, "PG", psum=True, es=es)
        PV = Rot(K, 1, [128, 4, 128], F32, "PV", psum=True, es=es)
        PC = Rot(K, 1, [8, 128], F32, "PC", psum=True, es=es)
        chunked = cfg.get("chunked", True)
        if chunked:
            PL = Rot(K, 1, [128, 512], F32, "PL", psum=True, es=es)
            TRI = K.sb([128, 128], F32, "TRI", es)
            K.sp.dma(lambda e: e.dma_start(out=TRI[:], in_=io["tri"][:, :]), adds=[d_c])
            LWr = Rot(K, 2, [128, 512], F32, "LWt", es=es)
            ELr = Rot(K, 2, [128, 3, 512], F32, "EL", es=es)
            ABr = Rot(K, 2, [128, 5, 512], BF16, "AB", es=es)
        dq = [0]

        def ldq():
            dq[0] += 1
            return K.sp if dq[0] % 2 == 0 else K.pool

        def tile_gen(ti):
            t0 = ti * 128
            first = (t0 % S == 0)
            Z, dZ = Zr.next()
            Zp, dZp = Zpr.next()
            ZS, dZS = ZSr.next()
            OUT, dO = OUTr.next()
            ldq().dma(lambda e, Z=Z, t0=t0: e.dma_start(out=Z[:], in_=scr["zs_tm"][t0:t0 + 128, :]),
                      reads=[scr["d_p1"]], writes=[dZ])
            if first:
                K.pool.op(lambda e, Zp=Zp: e.memset(Zp[0:32, :], 0.0), writes=[dZp])
                ldq().dma(lambda e, Zp=Zp, t0=t0: e.dma_start(out=Zp[1:128, :], in_=scr["zs_tm"][t0:t0 + 127, :]),
                          reads=[scr["d_p1"], dZp], adds=[dZp])
            else:
                ldq().dma(lambda e, Zp=Zp, t0=t0: e.dma_start(out=Zp[:], in_=scr["zs_tm"][t0 - 1:t0 + 127, :]),
                          reads=[scr["d_p1"]], writes=[dZp])
            CS = 1216
            dZSa, dZSb = Dep(), Dep()
            K.dve.op(lambda e, Z=Z, Zp=Zp, ZS=ZS: e.tensor_tensor(out=ZS[:, :CS], in0=Zp[:, :CS], in1=Z[:, :CS], op=ALU.subtract),
                     reads=[dZ, dZp], writes=[dZS])
            K.pool.op(lambda e, Z=Z, Zp=Zp, ZS=ZS: e.tensor_tensor(out=ZS[:, CS:], in0=Zp[:, CS:], in1=Z[:, CS:], op=ALU.subtract),
                      reads=[dZ, dZp, dZS], writes=[dZSb])
            K.dve.op(lambda e, ZS=ZS: e.tensor_tensor(out=ZS[:, :CS], in0=ZS[:, :CS], in1=MU[:, :CS], op=ALU.mult),
                     reads=[d_c, dZS], writes=[dZSa])
            K.pool.op(lambda e, ZS=ZS: e.tensor_tensor(out=ZS[:, CS:], in0=ZS[:, CS:], in1=MU[:, CS:], op=ALU.mult),
                      reads=[d_c], writes=[dZSb])
            K.dve.op(lambda e, Z=Z, ZS=ZS: e.tensor_tensor(out=ZS[:, :CS], in0=ZS[:, :CS], in1=Z[:, :CS], op=ALU.add),
                     reads=[dZ], writes=[dZSa])
            K.pool.op(lambda e, Z=Z, ZS=ZS: e.tensor_tensor(out=ZS[:, CS:], in0=ZS[:, CS:], in1=Z[:, CS:], op=ALU.add),
                      reads=[dZ], writes=[dZSb])
            K.dve.op(lambda e, ZS=ZS: e.tensor_copy(out=ZS[:, 0:1], in_=ZS[:, 0:1]), reads=[dZSa, dZSb], writes=[dZS])
            r_ap = ZS[:, 0:512]
            k_ap = ZS[:, 512:1024]
            K.act.op(lambda e, OUT=OUT, ZS=ZS: e.activation(out=OUT[:, 4, :], in_=ZS[:, 0:512], func=AF.Copy),
                     reads=[dZS], writes=[dO])
            yield
            E, dE = Er.next()
            L, dL = Lr.next()
            K.act.op(lambda e, E=E, ZS=ZS: e.activation(out=E[:, 0:64], in_=ZS[:, 1536:1600], func=AF.Exp, scale=-2.0),
                     reads=[dZS], writes=[dE])
            K.act.op(lambda e, E=E, ZS=ZS: e.activation(out=E[:, 64:192], in_=ZS[:, 1664:1792], func=AF.Exp, scale=-1.0),
                     reads=[dZS], adds=[dE])
            K.act.op(lambda e, E=E: e.activation(out=E[:], in_=E[:], func=AF.Ln, bias=cst[:, 0:1]), reads=[d_c], writes=[dE])
            K.act.op(lambda e, E=E: e.activation(out=E[:], in_=E[:], func=AF.Exp, scale=-1.0), writes=[dE])
            K.dve.op(lambda e, E=E, L=L: e.tensor_scalar(out=L[:, 0:64], in0=E[:, 0:64], scalar1=2.0, scalar2=-1.0,
                                                        op0=ALU.mult, op1=ALU.add), reads=[dE], writes=[dL])
            K.act.op(lambda e, L=L, ZS=ZS: e.activation(out=L[:, 64:128], in_=ZS[:, 1600:1664], func=AF.Copy),
                     reads=[dZS, dL], adds=[dL])
            K.act.op(lambda e, L=L, E=E: e.activation(out=L[:, 128:256], in_=E[:, 64:192], func=AF.Copy),
                     reads=[dE, dL], adds=[dL])
            yield
            pt, dpt = PT.next()
            K.pe.op(lambda e, pt=pt, L=L: e.transpose(out=pt[:, 0, :], in_=L[:, 0:128], identity=identb[:]),
                    reads=[dL, d_c], writes=[dpt])
            K.pe.op(lambda e, pt=pt, L=L: e.transpose(out=pt[:, 1, :], in_=L[:, 128:256], identity=identb[:]),
                    reads=[dL, d_c], adds=[dpt])
            LT, dLT = LTr.next()
            K.act.op(lambda e, pt=pt, LT=LT: e.activation(out=LT[:], in_=pt[:], func=AF.Copy), reads=[dpt], writes=[dLT])
            yield
            pw, dpw = PW.next()
            pa, dpa = PA.next()
            pg, dpg = PG.next()
            K.pe.op(lambda e, pw=pw, LT=LT: e.matmul(pw[:], LT[:, 0, :], LW[:, 0, :], start=True, stop=True),
                    reads=[dLT, d_c], writes=[dpw])
            K.pe.op(lambda e, pa=pa, LT=LT: e.matmul(pa[:], LT[:, 0, :], LW[:, 1, :], start=True, stop=True),
                    reads=[dLT, d_c], writes=[dpa])
            for j in range(4):
                K.pe.op(lambda e, pg=pg, LT=LT, j=j: e.matmul(pg[:, j, :], LW[:, 2, j * 128:(j + 1) * 128], LT[:, 1, :],
                                                             start=True, stop=True),
                        reads=[dLT, d_c], writes=[dpg] if j == 0 else [], adds=[] if j == 0 else [dpg])
            GT, dGT = GTr.next()
            K.act.op(lambda e, pg=pg, GT=GT: e.activation(out=GT[:], in_=pg[:], func=AF.Copy), reads=[dpg], writes=[dGT])
            K.sp.dma(lambda e, GT=GT, t0=t0: e.dma_start(
                out=dap(scr["g_fm"], t0, [[T, 128], [128 * T, 4], [1, 128]]), in_=GT[:]),
                reads=[dGT], adds=[scr["d_p2a"]])
            yield
            U, dU = Ur.next()
            K.dve.op(lambda e, U=U, pw=pw: e.tensor_tensor(out=U[:], in0=pw[:], in1=PR[:, 0, :], op=ALU.add),
                     reads=[dpw, d_c], writes=[dU])
            K.act.op(lambda e, U=U: e.activation(out=U[:], in_=U[:], func=AF.Exp, scale=-1.0), writes=[dU])
            K.act.op(lambda e, U=U: e.activation(out=U[:], in_=U[:], func=AF.Ln, bias=cst[:, 0:1]), reads=[d_c], writes=[dU])
            K.act.op(lambda e, U=U: e.activation(out=U[:], in_=U[:], func=AF.Exp, scale=-1.0, bias=cst[:, 1:2]),
                     reads=[d_c], writes=[dU])
            K.act.op(lambda e, U=U, OUT=OUT: e.activation(out=OUT[:, 0, :], in_=U[:], func=AF.Exp, scale=-1.0),
                     reads=[dU], adds=[dO])
            yield
            UA, dUA = UAr.next()
            K.dve.op(lambda e, UA=UA, pa=pa: e.tensor_tensor(out=UA[:], in0=pa[:], in1=PR[:, 1, :], op=ALU.add),
                     reads=[dpa, d_c], writes=[dUA])
            K.act.op(lambda e, UA=UA: e.activation(out=UA[:], in_=UA[:], func=AF.Exp, scale=-1.0), writes=[dUA])
            K.act.op(lambda e, UA=UA: e.activation(out=UA[:], in_=UA[:], func=AF.Ln, bias=cst[:, 0:1]), reads=[d_c], writes=[dUA])
            K.act.op(lambda e, UA=UA: e.activation(out=UA[:], in_=UA[:], func=AF.Exp, scale=-1.0), writes=[dUA])
            yield
            KKt, dKK = KKr.next()
            SQ, dSQ = SQr.next()
            S8, dS8 = S8r.next()
            K.dve.op(lambda e, KKt=KKt, ZS=ZS: e.tensor_tensor(out=KKt[:], in0=ZS[:, 512:1024], in1=PR[:, 2, :], op=ALU.mult),
                     reads=[dZS, d_c], writes=[dKK])
            K.pool.op(lambda e, KKt=KKt, SQ=SQ: e.tensor_tensor(out=SQ[:], in0=KKt[:], in1=KKt[:], op=ALU.mult),
                      reads=[dKK], writes=[dSQ])
            K.dve.op(lambda e, SQ=SQ, S8=S8: e.tensor_reduce(out=S8[:, 0, :], in_=SQ[:].rearrange("p (h k) -> p h k", k=64),
                                                            axis=AX.X, op=ALU.add), reads=[dSQ], writes=[dS8])
            K.dve.op(lambda e, S8=S8: e.tensor_scalar(out=S8[:, 0, :], in0=S8[:, 0, :], scalar1=1e-24, scalar2=None,
                                                      op0=ALU.max), writes=[dS8])
            K.act.op(lambda e, S8=S8: e.activation(out=S8[:, 1, :], in_=S8[:, 0, :], func=AF.Ln), writes=[dS8])
            K.act.op(lambda e, S8=S8: e.activation(out=S8[:, 2, :], in_=S8[:, 1, :], func=AF.Exp, scale=-0.5), writes=[dS8])
            K.dve.op(lambda e, KKt=KKt, S8=S8, OUT=OUT: e.tensor_tensor(
                out=OUT[:, 1, :].rearrange("p (h k) -> p h k", k=64), in0=KKt[:].rearrange("p (h k) -> p h k", k=64),
                in1=S8[:, 2, :].unsqueeze(2).broadcast_to([128, 8, 64]), op=ALU.mult),
                reads=[dKK, dS8, dO], adds=[dO])
            K.dve.op(lambda e, OUT=OUT, UA=UA: e.scalar_tensor_tensor(
                out=OUT[:, 2, :], in0=OUT[:, 1, :], scalar=-1.0, in1=UA[:], op0=ALU.mult, op1=ALU.mult),
                reads=[dUA, dO], adds=[dO])
            yield
            T1, dT1 = T1r.next()
            K.dve.op(lambda e, T1=T1, UA=UA: e.scalar_tensor_tensor(
                out=T1[:], in0=UA[:], scalar=-1.0, in1=PR[:, 3, :], op0=ALU.add, op1=ALU.mult),
                reads=[dUA, d_c], writes=[dT1])
            K.dve.op(lambda e, T1=T1, OUT=OUT, ZS=ZS: e.scalar_tensor_tensor(
                out=OUT[:, 3, :], in0=T1[:], scalar=1.0, in1=ZS[:, 512:1024], op0=ALU.add, op1=ALU.mult),
                reads=[dT1, dZS, dO], adds=[dO])
            T2, dT2 = T2r.next()
            K.pool.op(lambda e, T2=T2, OUT=OUT, ZS=ZS: e.tensor_tensor(out=T2[:], in0=OUT[:, 3, :], in1=ZS[:, 0:512], op=ALU.mult),
                      reads=[dO, dZS], writes=[dT2])
            K.pool.op(lambda e, T2=T2: e.tensor_tensor(out=T2[:], in0=T2[:], in1=PR[:, 4, :], op=ALU.mult),
                      reads=[d_c], writes=[dT2])
            K.dve.op(lambda e, T2=T2, S8=S8: e.tensor_reduce(out=S8[:, 3, :], in_=T2[:].rearrange("p (h k) -> p h k", k=64),
                                                            axis=AX.X, op=ALU.add), reads=[dT2], writes=[dS8])
            yield
            pv, dpv = PV.next()
            pc, dpc = PC.next()
            for j in range(4):
                K.pe.op(lambda e, pv=pv, ZS=ZS, j=j: e.transpose(out=pv[:, j, :], in_=ZS[:, 1024 + j * 128:1024 + (j + 1) * 128],
                                                                identity=identf[:]),
                        reads=[dZS, d_c], writes=[dpv] if j == 0 else [], adds=[] if j == 0 else [dpv])
            K.pe.op(lambda e, pc=pc, S8=S8: e.transpose(out=pc[:, :], in_=S8[:, 3, :], identity=identf[:]),
                    reads=[dS8, d_c], writes=[dpc])
            VT, dVT = VTr.next()
            CT, dCT = CTr.next()
            K.act.op(lambda e, pv=pv, VT=VT: e.activation(out=VT[:], in_=pv[:], func=AF.Copy), reads=[dpv], writes=[dVT])
            K.act.op(lambda e, pc=pc, CT=CT: e.activation(out=CT[:], in_=pc[:], func=AF.Copy), reads=[dpc], writes=[dCT])
            K.sp.dma(lambda e, VT=VT, t0=t0: e.dma_start(
                out=dap(scr["v_fm"], t0, [[T, 128], [128 * T, 4], [1, 128]]), in_=VT[:]),
                reads=[dVT], adds=[scr["d_p2a"]])
            K.sp.dma(lambda e, CT=CT, t0=t0: e.dma_start(out=scr["coef_fm"][:, t0:t0 + 128], in_=CT[:]),
                     reads=[dCT], adds=[scr["d_p2a"]])
            yield
            if not chunked:
                K.pool.dma(lambda e, OUT=OUT, t0=t0: e.dma_start(out=scr["rw_tm"][t0:t0 + 128, :, :], in_=OUT[:]),
                           reads=[dO], adds=[scr["d_p2a"]])
            else:
                LWt, dLW = LWr.next()
                K.dve.op(lambda e, LWt=LWt, U=U: e.tensor_scalar(out=LWt[:], in0=U[:], scalar1=-1.0, scalar2=None, op0=ALU.mult),
                         reads=[dU], writes=[dLW])
                pl, dpl = PL.next()
                K.pe.op(lambda e, pl=pl, LWt=LWt: e.matmul(pl[:], TRI[:], LWt[:], start=True, stop=True),
                        reads=[dLW, d_c], writes=[dpl])
                EL, dEL = ELr.next()
                K.act.op(lambda e, EL=EL, pl=pl: e.activation(out=EL[:, 0, :], in_=pl[:], func=AF.Exp), reads=[dpl], writes=[dEL])
                K.act.op(lambda e, EL=EL, pl=pl: e.activation(out=EL[:, 1, :], in_=pl[:], func=AF.Exp, scale=-1.0),
                         reads=[dpl], adds=[dEL])
                K.dve.op(lambda e, EL=EL, pl=pl, U=U: e.tensor_tensor(out=EL[:, 2, :], in0=pl[:], in1=U[:], op=ALU.add),
                         reads=[dpl, dU, dEL], adds=[dEL])
                K.act.op(lambda e, EL=EL: e.activation(out=EL[:, 2, :], in_=EL[:, 2, :], func=AF.Exp), reads=[dEL], adds=[dEL])
                AB, dAB = ABr.next()
                K.dve.op(lambda e, AB=AB, OUT=OUT, EL=EL: e.tensor_tensor(out=AB[:, 0, :], in0=OUT[:, 1, :], in1=EL[:, 2, :], op=ALU.mult),
                         reads=[dO, dEL], writes=[dAB])
                K.dve.op(lambda e, AB=AB, OUT=OUT, EL=EL: e.scalar_tensor_tensor(
                    out=AB[:, 1, :], in0=OUT[:, 2, :], scalar=-1.0, in1=EL[:, 1, :], op0=ALU.mult, op1=ALU.mult),
                    reads=[dO, dEL, dAB], adds=[dAB])
                K.pool.op(lambda e, AB=AB, OUT=OUT, EL=EL: e.tensor_tensor(out=AB[:, 2, :], in0=OUT[:, 3, :], in1=EL[:, 1, :], op=ALU.mult),
                          reads=[dO, dEL, dAB], adds=[dAB])
                K.pool.op(lambda e, AB=AB, OUT=OUT, EL=EL: e.tensor_tensor(out=AB[:, 3, :], in0=OUT[:, 4, :], in1=EL[:, 0, :], op=ALU.mult),
                          reads=[dO, dEL, dAB], adds=[dAB])
                K.act.op(lambda e, AB=AB, ZS=ZS: e.activation(out=AB[:, 4, :], in_=ZS[:, 1024:1536], func=AF.Copy),
                         reads=[dZS, dAB], adds=[dAB])
                K.pool.dma(lambda e, AB=AB, t0=t0: e.dma_start(out=scr["ab_tm"][t0:t0 + 128, :, :], in_=AB[:]),
                           reads=[dAB], adds=[scr["d_p2a"]])
                K.sp.dma(lambda e, LWt=LWt, t0=t0: e.dma_start(out=scr["lw_tm"][t0:t0 + 128, :], in_=LWt[:]),
                         reads=[dLW], adds=[scr["d_p2a"]])

        LAG = cfg.get("prep_lag", 3)
        active = []
        nxt = 0
        while nxt < NTT or active:
            if len(active) < 2 and nxt < NTT and (not active or active[0][1] >= LAG):
                active.append([tile_gen(nxt), 0])
                nxt += 1
            for a in list(active):
                try:
                    next(a[0])
                    a[1] += 1
                except StopIteration:
                    active.remove(a)


def phase2_scan(K, cfg, io, scr):
    T, S, NB = cfg["T"], cfg["S"], cfg["NB"]
    NBH = 2 if NB >= 2 else 1
    NBL = NB // NBH
    NP = 64 * NBH
    TS = 2
    TC = 128
    RW = 2560
    with ExitStack() as es:
        St = K.sb([128, NBL, 8, 64], F32, "St", es)
        dS = Dep()
        TMP = K.sb([128, NBL, 8, 64], F32, "TMP", es)
        dT = Dep()
        SA = K.sb([128, NBL, 8], F32, "SA", es)
        dSA = Dep()
        T2r = Rot(K, 2, [128, NBL, 8, 64], F32, "TMP2", es=es)
        T3r = Rot(K, 2, [128, NBL, 8, 64], F32, "TMP3", es=es)
        BCr = Rot(K, 3, [128, TS, NBL, 5, 8, 64], F32, "BC", es=es)
        Vr = Rot(K, 2, [128, NBL, 8, TC], F32, "Vf", es=es)
        Yr = Rot(K, 2, [128, NBL, 8, TC], F32, "Yf", es=es)
        K.dve.op(lambda e: e.memset(St[:], 0.0), writes=[dS])
        qi = [0]

        def q():
            qi[0] += 1
            return K.sp if qi[0] % 2 == 0 else K.act

        def load_bc(ci):
            t = ci * TS
            BC, dBC = BCr.next()
            first = True
            for bhi in range(NBH):
                for blo in range(NBL):
                    src = dap(scr["rw_tm"], ((bhi * NBL + blo) * S + t) * RW, [[0, 64], [RW, TS], [1, RW]])
                    dst = BC[bhi * 64:(bhi + 1) * 64, :, blo].rearrange("p t j h k -> p t (j h k)")
                    q().dma(lambda e, src=src, dst=dst: e.dma_start(out=dst, in_=src), reads=[scr["d_p2a"]],
                            writes=[dBC] if first else [], adds=[] if first else [dBC])
                    first = False
            return BC, dBC

        def vy_ap(name, bhi, blo, t):
            return dap(scr[name], (bhi * NBL + blo) * S + t, [[T, 64], [64 * T, 8], [1, TC]])

        def load_v(ni):
            Vf, dV = Vr.next()
            first = True
            for bhi in range(NBH):
                for blo in range(NBL):
                    src = vy_ap("v_fm", bhi, blo, ni * TC)
                    dst = Vf[bhi * 64:(bhi + 1) * 64, blo]
                    q().dma(lambda e, src=src, dst=dst: e.dma_start(out=dst, in_=src), reads=[scr["d_p2a"]],
                            writes=[dV] if first else [], adds=[] if first else [dV])
                    first = False
            return Vf, dV

        nch = S // TS
        bcs = {}
        bcs[0] = load_bc(0)
        if nch > 1:
            bcs[1] = load_bc(1)
        vs = {0: load_v(0)}
        P = slice(0, NP)
        for t in range(S):
            ci, ts = divmod(t, TS)
            ni, tt = divmod(t, TC)
            if ts == 0 and ci + 2 < nch:
                bcs[ci + 2] = load_bc(ci + 2)
            if tt == 0:
                if (ni + 1) * TC < S:
                    vs[ni + 1] = load_v(ni + 1)
                Yf, dY = Yr.next()
            BC, dBC = bcs[ci]
            Vf, dV = vs[ni]
            W_ = BC[P, ts, :, 0]
            KN = BC[P, ts, :, 1]
            KA = BC[P, ts, :, 2]
            KP = BC[P, ts, :, 3]
            R_ = BC[P, ts, :, 4]
            shp = [NP, NBL, 8, 64]
            K.dve.op(lambda e, KN=KN: e.tensor_tensor(out=TMP[P], in0=St[P], in1=KN, op=ALU.mult),
                     reads=[dS, dBC], writes=[dT])
            K.dve.op(lambda e: e.tensor_reduce(out=SA[P], in_=TMP[P], axis=AX.X, op=ALU.add), reads=[dT], writes=[dSA])
            K.dve.op(lambda e, W_=W_: e.tensor_tensor(out=St[P], in0=St[P], in1=W_, op=ALU.mult),
                     reads=[dBC], writes=[dS])
            K.dve.op(lambda e, KA=KA: e.tensor_tensor(out=TMP[P], in0=KA, in1=SA[P].unsqueeze(3).broadcast_to(shp),
                                                     op=ALU.mult), reads=[dBC, dSA], writes=[dT])
            K.dve.op(lambda e: e.tensor_tensor(out=St[P], in0=St[P], in1=TMP[P], op=ALU.add), reads=[dT], writes=[dS])
            T2, dT2 = T2r.next()
            K.pool.op(lambda e, KP=KP, T2=T2, Vf=Vf, tt=tt: e.tensor_tensor(
                out=T2[P], in0=KP, in1=Vf[P, :, :, tt:tt + 1].broadcast_to(shp), op=ALU.mult),
                reads=[dBC, dV], writes=[dT2])
            K.dve.op(lambda e, T2=T2: e.tensor_tensor(out=St[P], in0=St[P], in1=T2[P], op=ALU.add),
                     reads=[dT2], writes=[dS])
            T3, dT3 = T3r.next()
            K.pool.op(lambda e, T3=T3, R_=R_: e.tensor_tensor(out=T3[P], in0=St[P], in1=R_, op=ALU.mult),
                      reads=[dS, dBC], writes=[dT3])
            K.dve.op(lambda e, T3=T3, Yf=Yf, tt=tt: e.tensor_reduce(out=Yf[P, :, :, tt], in_=T3[P], axis=AX.X, op=ALU.add),
                      reads=[dT3], writes=[dY] if tt == 0 else [], adds=[] if tt == 0 else [dY])
            if tt == TC - 1:
                for bhi in range(NBH):
                    for blo in range(NBL):
                        dst = vy_ap("y_fm", bhi, blo, ni * TC)
                        srcp = Yf[bhi * 64:(bhi + 1) * 64, blo]
                        K.sp.dma(lambda e, dst=dst, srcp=srcp: e.dma_start(out=dst, in_=srcp), reads=[dY],
                                 adds=[scr["d_p2b"]])


def phase2_chunk(K, cfg, io, scr):
    T, S, NB = cfg["T"], cfg["S"], cfg["NB"]
    C = 64
    NCH = S // C
    with ExitStack() as es:
        d_c = Dep()
        id64 = K.sb([64, 64], BF16, "c_id64", es)
        K.sp.dma(lambda e: e.dma_start(out=id64[:], in_=io["ident64"][:, :]), adds=[d_c])
        MK = K.sb([64, 3, 64], F32, "c_MK", es)
        K.sp.dma(lambda e: e.dma_start(out=MK[:], in_=io["masks"][:, :, :]), adds=[d_c])
        ONES = K.sb([64, 1], F32, "c_ones", es)
        K.sp.dma(lambda e: e.dma_start(out=ONES[:], in_=io["ones64"][:, :]), adds=[d_c])
        IDF = K.sb([64, 8, 64], F32, "c_IDF", es)
        K.sp.dma(lambda e: e.dma_start(out=IDF[:], in_=io["ident_f"][0:64, 0:64].unsqueeze(1).broadcast_to([64, 8, 64])), adds=[d_c])
        ST = [K.sb([64, 8, 64], F32, "c_S%d" % b, es) for b in range(NB)]
        STb = [K.sb([64, 8, 64], BF16, "c_Sb%d" % b, es) for b in range(NB)]
        dST = [Dep() for _ in range(NB)]
        dSTb = [Dep() for _ in range(NB)]
        for b in range(NB):
            K.dve.op(lambda e, b=b: e.memset(ST[b][:], 0.0), writes=[dST[b]])
            K.pool.op(lambda e, b=b: e.memset(STb[b][:], 0.0), writes=[dSTb[b]])
        TMr = Rot(K, 3, [64, 5, 512], BF16, "c_TM", es=es)
        LWr = Rot(K, 3, [64, 512], F32, "c_LW", es=es)
        FMr = Rot(K, 2, [64, 4, 8, 64], BF16, "c_FM", es=es)
        PCr = Rot(K, 2, [64, 8], F32, "c_PC", es=es)
        Nr = Rot(K, 3, [64, 8, 64], BF16, "c_N", es=es)
        NTr = Rot(K, 3, [64, 8, 64], BF16, "c_NT", es=es)
        MTr = Rot(K, 3, [64, 8, 64], BF16, "c_MT", es=es)
        MTfr = Rot(K, 2, [64, 8, 64], F32, "c_MTf", es=es)
        NAKr = Rot(K, 2, [64, 8, 64], BF16, "c_NAK", es=es)
        MRBr = Rot(K, 2, [64, 8, 64], BF16, "c_MRB", es=es)
        MRKr = Rot(K, 2, [64, 8, 64], BF16, "c_MRK", es=es)
        Xr = Rot(K, 2, [64, 8, 64], BF16, "c_X", es=es)
        NUr = Rot(K, 2, [64, 8, 64], BF16, "c_NU", es=es)
        Yr = Rot(K, 2, [64, 8, 64], F32, "c_Y", es=es)
        TSr = Rot(K, 2, [64, 8, 64], F32, "c_TS", es=es)
        PTf = Rot(K, 1, [64, 4, 8, 64], BF16, "c_PTf", psum=True, es=es)
        PA = Rot(K, 4, [64, 8, 64], F32, "c_PA", psum=True, es=es)
        PPC = Rot(K, 1, [64, 8], F32, "c_PPC", psum=True, es=es)
        ce = [0]

        def evac_copy(dst_ap, src_ap, reads, writes=(), adds=(), scale=None):
            ce[0] += 1
            if scale is not None or ce[0] % 2 == 0:
                if scale is None:
                    K.act.op(lambda e: e.activation(out=dst_ap, in_=src_ap, func=AF.Copy), reads=reads, writes=writes, adds=adds)
                else:
                    K.act.op(lambda e: e.activation(out=dst_ap, in_=src_ap, func=AF.Copy, scale=scale), reads=reads, writes=writes, adds=adds)
            else:
                K.dve.op(lambda e: e.tensor_copy(out=dst_ap, in_=src_ap), reads=reads, writes=writes, adds=adds)

        def mm8(pt, dpt, lhs_fn, rhs_fn, reads, first=True, last=True, wr=True):
            mmN(pt, dpt, [(lhs_fn, rhs_fn)], reads)

        def mmN(pt, dpt, terms, reads):
            n = len(terms)
            for h in range(8):
                for i, (lf, rf) in enumerate(terms):
                    K.pe.op(lambda e, h=h, lf=lf, rf=rf, i=i: e.matmul(pt[:, h, :], lf(h), rf(h), start=(i == 0), stop=(i == n - 1)),
                            reads=reads, writes=[dpt] if (h == 0 and i == 0) else [], adds=[] if (h == 0 and i == 0) else [dpt])

        q = [0]

        def dq():
            q[0] += 1
            return K.sp if q[0] % 2 == 0 else K.pool

        for ci in range(NCH):
            for b in range(NB):
                t0 = b * S + ci * C
                TM, dTM = TMr.next()
                LW, dLW = LWr.next()
                dq().dma(lambda e, TM=TM, t0=t0: e.dma_start(out=TM[:], in_=scr["ab_tm"][t0:t0 + C, :, :]),
                         reads=[scr["d_p2a"]], writes=[dTM])
                dq().dma(lambda e, LW=LW, t0=t0: e.dma_start(out=LW[:], in_=scr["lw_tm"][t0:t0 + C, :]),
                         reads=[scr["d_p2a"]], writes=[dLW])
                ptf, dptf = PTf.next()
                first = True
                for j in range(4):
                    for h in range(8):
                        K.pe.op(lambda e, ptf=ptf, TM=TM, j=j, h=h: e.transpose(
                            out=ptf[:, j, h, :], in_=TM[:, j, h * 64:(h + 1) * 64], identity=id64[:]),
                            reads=[dTM, d_c], writes=[dptf] if first else [], adds=[] if first else [dptf])
                        first = False
                FM, dFM = FMr.next()
                K.act.op(lambda e, FM=FM, ptf=ptf: e.activation(out=FM[:, 0:2], in_=ptf[:, 0:2], func=AF.Copy), reads=[dptf], writes=[dFM])
                K.dve.op(lambda e, FM=FM, ptf=ptf: e.tensor_copy(out=FM[:, 2:4], in_=ptf[:, 2:4]), reads=[dptf, dFM], adds=[dFM])
                Af = lambda h, FM=FM: FM[:, 0, h, :]
                Bf = lambda h, FM=FM: FM[:, 1, h, :]
                Kf = lambda h, FM=FM: FM[:, 2, h, :]
                Rf = lambda h, FM=FM: FM[:, 3, h, :]
                Vt = lambda h, TM=TM: TM[:, 4, h * 64:(h + 1) * 64]
                Bt = lambda h, TM=TM: TM[:, 1, h * 64:(h + 1) * 64]
                Kt = lambda h, TM=TM: TM[:, 2, h * 64:(h + 1) * 64]
                ppc, dppc = PPC.next()
                for h in range(8):
                    K.pe.op(lambda e, ppc=ppc, LW=LW, h=h: e.matmul(ppc[:, h:h + 1], LW[:, h * 64:(h + 1) * 64], ONES[:], start=True, stop=True),
                            reads=[dLW, d_c], writes=[dppc] if h == 0 else [], adds=[] if h == 0 else [dppc])
                PCt, dPC = PCr.next()
                K.act.op(lambda e, PCt=PCt, ppc=ppc: e.activation(out=PCt[:], in_=ppc[:], func=AF.Exp), reads=[dppc], writes=[dPC])
                mbc = lambda i: MK[:, i, :].unsqueeze(1).broadcast_to([64, 8, 64])
                pa, dpa = PA.next()
                mm8(pa, dpa, Af, Bf, [dFM])
                N0, dN0 = Nr.next()
                K.dve.op(lambda e, N0=N0, pa=pa: e.tensor_tensor(out=N0[:], in0=pa[:], in1=mbc(0), op=ALU.mult), reads=[dpa, d_c], writes=[dN0])
                pa, dpa = PA.next()
                mm8(pa, dpa, Bf, Af, [dFM])
                NT0, dNT0 = NTr.next()
                MTf, dMTf = MTfr.next()
                K.dve.op(lambda e, NT0=NT0, pa=pa: e.tensor_tensor(out=NT0[:], in0=pa[:], in1=mbc(1), op=ALU.mult), reads=[dpa, d_c], writes=[dNT0])
                K.pool.op(lambda e, MTf=MTf, NT0=NT0: e.tensor_tensor(out=MTf[:], in0=IDF[:], in1=NT0[:], op=ALU.subtract),
                          reads=[dNT0, d_c], writes=[dMTf])
                MT, dMT = MTr.next()
                K.act.op(lambda e, MT=MT, MTf=MTf: e.activation(out=MT[:], in_=MTf[:], func=AF.Copy), reads=[dMTf], writes=[dMT])
                pa, dpa = PA.next()
                mm8(pa, dpa, Kf, Af, [dFM])
                NAK, dNAK = NAKr.next()
                K.dve.op(lambda e, NAK=NAK, pa=pa: e.tensor_tensor(out=NAK[:], in0=pa[:], in1=mbc(1), op=ALU.mult), reads=[dpa, d_c], writes=[dNAK])
                pa, dpa = PA.next()
                mm8(pa, dpa, Bf, Rf, [dFM])
                MRB, dMRB = MRBr.next()
                K.dve.op(lambda e, MRB=MRB, pa=pa: e.tensor_tensor(out=MRB[:], in0=pa[:], in1=mbc(2), op=ALU.mult), reads=[dpa, d_c], writes=[dMRB])
                pa, dpa = PA.next()
                mm8(pa, dpa, Kf, Rf, [dFM])
                MRK, dMRK = MRKr.next()
                K.dve.op(lambda e, MRK=MRK, pa=pa: e.tensor_tensor(out=MRK[:], in0=pa[:], in1=mbc(2), op=ALU.mult), reads=[dpa, d_c], writes=[dMRK])
                Np, dNp, NTp, dNTp = N0, dN0, NT0, dNT0
                for lvl in range(1, 6):
                    pa, dpa = PA.next()
                    mm8(pa, dpa, lambda h, NTp=NTp: NTp[:, h, :], lambda h, Np=Np: Np[:, h, :], [dNp, dNTp])
                    Nn, dNn = Nr.next()
                    evac_copy(Nn[:], pa[:], [dpa], writes=[dNn])
                    if lvl < 5:
                        pa2, dpa2 = PA.next()
                        mm8(pa2, dpa2, lambda h, Np=Np: Np[:, h, :], lambda h, NTp=NTp: NTp[:, h, :], [dNp, dNTp])
                        NTn, dNTn = NTr.next()
                        evac_copy(NTn[:], pa2[:], [dpa2], writes=[dNTn])
                    pa3, dpa3 = PA.next()
                    mm8(pa3, dpa3, lambda h, Nn=Nn: Nn[:, h, :], lambda h, MT=MT: MT[:, h, :], [dNn, dMT])
                    K.dve.op(lambda e, MTf=MTf, pa3=pa3: e.tensor_tensor(out=MTf[:], in0=MTf[:], in1=pa3[:], op=ALU.add),
                             reads=[dpa3], writes=[dMTf])
                    MT, dMT = MTr.next()
                    K.act.op(lambda e, MT=MT, MTf=MTf: e.activation(out=MT[:], in_=MTf[:], func=AF.Copy), reads=[dMTf], writes=[dMT])
                    Np, dNp = Nn, dNn
                    if lvl < 5:
                        NTp, dNTp = NTn, dNTn
                Sb = STb[b]
                pa, dpa = PA.next()
                mmN(pa, dpa, [(Af, lambda h, Sb=Sb: Sb[:, h, :]), (lambda h, NAK=NAK: NAK[:, h, :], Vt)], [dFM, dSTb[b], dNAK, dTM])
                X, dX = Xr.next()
                K.act.op(lambda e, X=X, pa=pa: e.activation(out=X[:], in_=pa[:], func=AF.Copy), reads=[dpa], writes=[dX])
                pa, dpa = PA.next()
                mm8(pa, dpa, lambda h, MT=MT: MT[:, h, :], lambda h, X=X: X[:, h, :], [dMT, dX])
                NU, dNU = NUr.next()
                K.act.op(lambda e, NU=NU, pa=pa: e.activation(out=NU[:], in_=pa[:], func=AF.Copy, scale=-1.0), reads=[dpa], writes=[dNU])
                pa, dpa = PA.next()
                mmN(pa, dpa, [(lambda h, Sb=Sb: Sb[:, h, :], Rf), (lambda h, NU=NU: NU[:, h, :], lambda h, MRB=MRB: MRB[:, h, :]),
                              (Vt, lambda h, MRK=MRK: MRK[:, h, :])], [dFM, dSTb[b], dNU, dMRB, dTM, dMRK])
                Y, dY = Yr.next()
                K.dve.op(lambda e, Y=Y, pa=pa: e.tensor_copy(out=Y[:], in_=pa[:]), reads=[dpa], writes=[dY])
                K.sp.dma(lambda e, Y=Y, t0=t0: e.dma_start(out=dap(scr["y_fm"], t0, [[T, 64], [64 * T, 8], [1, 64]]), in_=Y[:]),
                         reads=[dY], adds=[scr["d_p2b"]])
                pa, dpa = PA.next()
                mmN(pa, dpa, [(Bt, lambda h, NU=NU: NU[:, h, :]), (Kt, Vt)], [dTM, dNU])
                TS_, dTS = TSr.next()
                K.dve.op(lambda e, TS_=TS_, pa=pa, b=b: e.tensor_tensor(out=TS_[:], in0=pa[:], in1=ST[b][:], op=ALU.add),
                         reads=[dpa, dST[b]], writes=[dTS])
                K.dve.op(lambda e, TS_=TS_, PCt=PCt, b=b: e.tensor_tensor(
                    out=ST[b][:], in0=TS_[:], in1=PCt[:].unsqueeze(2).broadcast_to([64, 8, 64]), op=ALU.mult),
                    reads=[dTS, dPC], writes=[dST[b]])
                K.act.op(lambda e, b=b: e.activation(out=STb[b][:], in_=ST[b][:], func=AF.Copy), reads=[dST[b]], writes=[dSTb[b]])


def phase2_post(K, cfg, io, scr):
    T, S, NB = cfg["T"], cfg["S"], cfg["NB"]
    NT = T // 512
    with ExitStack() as es:
        BO = K.sb([128, 128], F32, "BO", es)
        d_c = Dep()
        K.sp.dma(lambda e: e.dma_start(out=BO[:], in_=io["blockones"][:, :]), adds=[d_c])
        LN = K.sb([128, 2, 4], F32, "LN", es)
        K.sp.dma(lambda e: e.dma_start(out=LN[:, 0, :], in_=io["lnx_w"].rearrange("o (j p) -> p (o j)", p=128)), adds=[d_c])
        K.sp.dma(lambda e: e.dma_start(out=LN[:, 1, :], in_=io["lnx_b"].rearrange("o (j p) -> p (o j)", p=128)), adds=[d_c])
        Yr = Rot(K, 2, [128, 512], F32, "pY", es=es)
        Vr = Rot(K, 2, [128, 512], F32, "pV", es=es)
        Gr = Rot(K, 2, [128, 512], F32, "pG", es=es)
        Cr = Rot(K, 2, [128, 512], F32, "pC", es=es)
        YCr = Rot(K, 2, [128, 512], F32, "pYC", es=es)
        SQr = Rot(K, 2, [128, 512], F32, "pSQ", es=es)
        Rr = Rot(K, 2, [128, 512], F32, "pR", es=es)
        Or = Rot(K, 2, [128, 512], BF16, "pO", es=es)
        PM = Rot(K, 2, [128, 512], F32, "pPM", psum=True, es=es)
        PVr = Rot(K, 2, [128, 512], F32, "pPV", psum=True, es=es)
        for ti in range(NT):
            cs = slice(ti * 512, (ti + 1) * 512)
            for j in range(4):
                rs = slice(j * 128, (j + 1) * 128)
                Y, dY = Yr.next()
                V, dV = Vr.next()
                G, dG = Gr.next()
                C, dC = Cr.next()
                K.sp.dma(lambda e, Y=Y, rs=rs, cs=cs: e.dma_start(out=Y[:], in_=scr["y_fm"][rs, cs]),
                         reads=[scr["d_p2b"]], writes=[dY])
                K.pool.dma(lambda e, V=V, rs=rs, cs=cs: e.dma_start(out=V[:], in_=scr["v_fm"][rs, cs]),
                           reads=[scr["d_p2a"]], writes=[dV])
                K.sp.dma(lambda e, G=G, rs=rs, cs=cs: e.dma_start(out=G[:], in_=scr["g_fm"][rs, cs]),
                         reads=[scr["d_p2a"]], writes=[dG])
                K.pool.dma(lambda e, C=C, j=j, cs=cs: e.dma_start(
                    out=C[0:64, :], in_=scr["coef_fm"][2 * j:2 * j + 1, cs].broadcast_to([64, 512])),
                    reads=[scr["d_p2a"]], writes=[dC])
                K.pool.dma(lambda e, C=C, j=j, cs=cs: e.dma_start(
                    out=C[64:128, :], in_=scr["coef_fm"][2 * j + 1:2 * j + 2, cs].broadcast_to([64, 512])),
                    reads=[scr["d_p2a"]], adds=[dC])
                pm, dpm = PM.next()
                K.pe.op(lambda e, pm=pm, Y=Y: e.matmul(pm[:], BO[:], Y[:], start=True, stop=True),
                        reads=[dY, d_c], writes=[dpm])
                YC, dYC = YCr.next()
                K.dve.op(lambda e, YC=YC, Y=Y, pm=pm: e.tensor_tensor(out=YC[:], in0=Y[:], in1=pm[:], op=ALU.subtract),
                         reads=[dY, dpm], writes=[dYC])
                SQ, dSQ = SQr.next()
                K.act.op(lambda e, SQ=SQ, YC=YC: e.activation(out=SQ[:], in_=YC[:], func=AF.Square),
                         reads=[dYC], writes=[dSQ])
                pv, dpv = PVr.next()
                K.pe.op(lambda e, pv=pv, SQ=SQ: e.matmul(pv[:], BO[:], SQ[:], start=True, stop=True),
                        reads=[dSQ, d_c], writes=[dpv])
                R, dR = Rr.next()
                K.dve.op(lambda e, R=R, pv=pv: e.tensor_scalar(out=R[:], in0=pv[:], scalar1=64e-5, scalar2=None, op0=ALU.add),
                         reads=[dpv], writes=[dR])
                K.act.op(lambda e, R=R: e.activation(out=R[:], in_=R[:], func=AF.Ln), writes=[dR])
                K.act.op(lambda e, R=R: e.activation(out=R[:], in_=R[:], func=AF.Exp, scale=-0.5), writes=[dR])
                K.dve.op(lambda e, YC=YC, R=R: e.tensor_tensor(out=YC[:], in0=YC[:], in1=R[:], op=ALU.mult),
                         reads=[dR], writes=[dYC])
                K.dve.op(lambda e, YC=YC, j=j: e.tensor_scalar(out=YC[:], in0=YC[:], scalar1=LN[:, 0, j:j + 1],
                                                              scalar2=LN[:, 1, j:j + 1], op0=ALU.mult, op1=ALU.add),
                         reads=[d_c], writes=[dYC])
                K.pool.op(lambda e, C=C, V=V: e.tensor_tensor(out=C[:], in0=C[:], in1=V[:], op=ALU.mult),
                          reads=[dV], writes=[dC])
                K.dve.op(lambda e, YC=YC, C=C: e.tensor_tensor(out=YC[:], in0=YC[:], in1=C[:], op=ALU.add),
                         reads=[dC], writes=[dYC])
                O, dO = Or.next()
                K.dve.op(lambda e, O=O, YC=YC, G=G: e.tensor_tensor(out=O[:], in0=YC[:], in1=G[:], op=ALU.mult),
                         reads=[dYC, dG], writes=[dO])
                K.sp.dma(lambda e, O=O, rs=rs, cs=cs: e.dma_start(out=scr["ya_fm"][rs, cs], in_=O[:]),
                         reads=[dO], adds=[scr["d_p2c"]])


def phase3(K, cfg, io, scr):
    T, S, NB = cfg["T"], cfg["S"], cfg["NB"]
    NQ = S // 128
    lam_init = 0.2
    with ExitStack() as es:
        d_c = Dep()
        identb = K.sb([128, 128], BF16, "a_identb", es)
        K.sp.dma(lambda e: e.dma_start(out=identb[:], in_=io["ident_bf"][:, :]), adds=[d_c])
        TB = K.sb([128, 4, S], F32, "TB", es)
        for h in range(4):
            (K.sp if h % 2 == 0 else K.pool).dma(lambda e, h=h: e.dma_start(out=TB[:, h, :], in_=io["alibi"][h, :, :]), adds=[d_c])
        SW = K.sb([128, 128], F32, "SW", es)
        K.sp.dma(lambda e: e.dma_start(out=SW[:], in_=io["subln_w"][0:1, :].broadcast_to([128, 128])), adds=[d_c])
        LQ = K.sb([128, 4, 64], F32, "LQ", es)
        for j, nm in enumerate(["lam_q1", "lam_k1", "lam_q2", "lam_k2"]):
            K.pool.dma(lambda e, j=j, nm=nm: e.dma_start(out=LQ[:, j, :], in_=io[nm][0:1, :].broadcast_to([128, 64])), adds=[d_c])
        LM = K.sb([128, 8], F32, "LM", es)
        d_lm = Dep()
        LT_ = K.sb([128, 2, 64], F32, "LTt", es)
        K.dve.op(lambda e: e.tensor_tensor(out=LT_[:, 0, :], in0=LQ[:, 0, :], in1=LQ[:, 1, :], op=ALU.mult), reads=[d_c], writes=[d_lm])
        K.dve.op(lambda e: e.tensor_tensor(out=LT_[:, 1, :], in0=LQ[:, 2, :], in1=LQ[:, 3, :], op=ALU.mult), reads=[d_c], writes=[d_lm])
        K.dve.op(lambda e: e.tensor_reduce(out=LM[:, 0:2], in_=LT_[:], axis=AX.X, op=ALU.add), writes=[d_lm])
        K.act.op(lambda e: e.activation(out=LM[:, 2:4], in_=LM[:, 0:2], func=AF.Exp), writes=[d_lm])
        K.dve.op(lambda e: e.tensor_tensor(out=LM[:, 4:5], in0=LM[:, 3:4], in1=LM[:, 2:3], op=ALU.subtract), writes=[d_lm])
        K.dve.op(lambda e: e.tensor_scalar(out=LM[:, 4:5], in0=LM[:, 4:5], scalar1=-lam_init, scalar2=None, op0=ALU.add), writes=[d_lm])
        K.dve.op(lambda e: e.tensor_scalar(out=SW[:], in0=SW[:], scalar1=1.0 - lam_init, scalar2=None, op0=ALU.mult),
                 reads=[d_c], writes=[d_c])

        Vr = Rot(K, 2, [128, NQ, 512], BF16, "aV", es=es)
        QKr = Rot(K, 2, [64, 4, S], BF16, "aQK", es=es)
        SSr = Rot(K, 3, [128, 512], F32, "aSS", es=es)
        Pr = Rot(K, 3, [128, 512], BF16, "aP", es=es)
        PTsr = Rot(K, 4, [128, 4, 128], BF16, "aPTs", es=es)
        YB = K.sb([128, NQ, 512], BF16, "aYB", es)
        dYB = Dep()
        STr = Rot(K, 4, [128, 24], F32, "aST", es=es)
        O1r = Rot(K, 2, [128, 128], F32, "aO1", es=es)
        Or_ = Rot(K, 2, [128, 128], F32, "aO", es=es)
        junk = K.sb([128, 128], F32, "ajunk", es)
        d_junk = Dep()
        YTr = Rot(K, 2, [128, 4, 128], BF16, "aYT", es=es)
        PS = Rot(K, 3, [128, 512], F32, "aPS", psum=True, es=es)
        PTp = Rot(K, 2, [128, 4, 128], BF16, "aPTp", psum=True, es=es)
        PO = Rot(K, 2, [128, 2, 128], F32, "aPO", psum=True, es=es)
        cp = [0]

        def copy_eng():
            cp[0] += 1
            return cp[0] % 2

        SSQ = K.sb([128, NQ * 4], F32, "aSSQ", es)
        dSSQ = Dep()
        SWb = K.sb([128, 128], BF16, "aSWb", es)
        K.act.op(lambda e: e.activation(out=SWb[:], in_=SW[:], func=AF.Copy), reads=[d_c], adds=[d_c])
        pipe = []
        pidx = [0]

        def step_pipe():
            j = len(pipe) - 1
            pipe[j][0]()
            if j - 1 >= pidx[0]:
                pipe[j - 1][2]()
            if j - 2 >= pidx[0]:
                pipe[j - 2][3]()
                if pipe[j - 2][4] is not None:
                    pipe[j - 2][4]()
            pipe[j][1]()

        def flush_pipe():
            j = len(pipe) - 1
            if j - 0 >= pidx[0] and j >= 0:
                pipe[j][2]()
            for k in (j - 1, j):
                if k >= pidx[0] and k >= 0:
                    pipe[k][3]()
                    if pipe[k][4] is not None:
                        pipe[k][4]()
            pidx[0] = len(pipe)
        for b in range(NB):
            V, dV = Vr.next()
            K.sp.dma(lambda e, V=V, b=b: e.dma_start(
                out=V[:], in_=scr["av_tm"][b * S:(b + 1) * S, :].rearrange("(n p) c -> p n c", p=128)),
                reads=[scr["d_p1"]], writes=[dV])
            first_yb = True
            for h in range(4):
                QK, dQK = QKr.next()
                for j in range(4):
                    r0 = (0 if j < 2 else 512) + h * 128 + (j % 2) * 64
                    (K.sp if j % 2 == 0 else K.pool).dma(lambda e, QK=QK, j=j, r0=r0, b=b: e.dma_start(
                        out=QK[:, j, :], in_=scr["qk_fm"][r0:r0 + 64, b * S:(b + 1) * S]),
                        reads=[scr["d_p1"]], writes=[dQK] if j == 0 else [], adds=[] if j == 0 else [dQK])
                for qi in range(NQ):
                    nk = (qi + 1) * 128
                    off = (S - 128) - qi * 128
                    ST, dST = STr.next()
                    K.pool.op(lambda e, ST=ST: e.memset(ST[:], 0.0), writes=[dST])
                    po, dpo = PO.next()
                    items = []
                    for c in range(2):
                        nch = (nk + 511) // 512
                        for ch in range(nch):
                            items.append((c, ch))
                    for ii, (c, ch) in enumerate(items):
                        kb0 = ch * 512
                        n = min(512, nk - kb0)
                        nb = n // 128
                        ps, dps = PS.next()
                        SS, dSS = SSr.next()
                        Pt, dP = Pr.next()
                        hold = {}

                        def stA_pe(ps=ps, dps=dps, c=c, qi=qi, kb0=kb0, n=n, QK=QK, dQK=dQK):
                            K.pe.op(lambda e: e.matmul(
                                ps[:, :n], QK[:, c, qi * 128:(qi + 1) * 128], QK[:, 2 + c, kb0:kb0 + n],
                                start=True, stop=True), reads=[dQK], writes=[dps])

                        def stA_rest(ps=ps, dps=dps, SS=SS, dSS=dSS, Pt=Pt, dP=dP, ST=ST, dST=dST, n=n, off=off, h=h, kb0=kb0, c=c, ch=ch):
                            K.dve.op(lambda e: e.scalar_tensor_tensor(
                                out=SS[:, :n], in0=ps[:, :n], scalar=0.125, in1=TB[:, h, off + kb0:off + kb0 + n],
                                op0=ALU.mult, op1=ALU.add), reads=[dps, d_c], writes=[dSS])
                            K.act.op(lambda e: e.activation(
                                out=Pt[:, :n], in_=SS[:, :n], func=AF.Exp,
                                accum_out=ST[:, 4 * c + ch:4 * c + ch + 1]), reads=[dSS, dST], writes=[dP], adds=[dST])

                        def stB(Pt=Pt, dP=dP, nb=nb, hold=hold):
                            ptp, dptp = PTp.next()
                            for kk_ in range(nb):
                                K.pe.op(lambda e, kk_=kk_: e.transpose(
                                    out=ptp[:, kk_, :], in_=Pt[:, kk_ * 128:(kk_ + 1) * 128], identity=identb[:]),
                                    reads=[dP, d_c], writes=[dptp] if kk_ == 0 else [], adds=[] if kk_ == 0 else [dptp])
                            PTs, dPTs = PTsr.next()
                            hold["PTs"] = (PTs, dPTs)
                            if copy_eng():
                                K.act.op(lambda e: e.activation(
                                    out=PTs[:, :nb, :], in_=ptp[:, :nb, :], func=AF.Copy), reads=[dptp], writes=[dPTs])
                            else:
                                K.dve.op(lambda e: e.tensor_copy(
                                    out=PTs[:, :nb, :], in_=ptp[:, :nb, :]), reads=[dptp], writes=[dPTs])

                        def stC(nb=nb, kb0=kb0, c=c, h=h, qi=qi, po=po, dpo=dpo, V=V, dV=dV, hold=hold):
                            PTs, dPTs = hold["PTs"]
                            for kk_ in range(nb):
                                kb = kb0 // 128 + kk_
                                K.pe.op(lambda e, kb=kb, kk_=kk_: e.matmul(
                                    po[:, c, :], PTs[:, kk_, :], V[:, kb, h * 128:(h + 1) * 128],
                                    start=(kb == 0), stop=(kb == qi)), reads=[dPTs, dV],
                                    writes=[dpo] if (kb == 0 and c == 0) else [], adds=[] if (kb == 0 and c == 0) else [dpo])

                        def combine(ST=ST, dST=dST, po=po, dpo=dpo, qi=qi, h=h, fy=first_yb):
                            K.dve.op(lambda e: e.tensor_reduce(out=ST[:, 8:10], in_=ST[:, 0:8].rearrange("p (c k) -> p c k", k=4),
                                                               axis=AX.X, op=ALU.add), reads=[dST], writes=[dST])
                            K.dve.op(lambda e: e.reciprocal(out=ST[:, 10:12], in_=ST[:, 8:10]), writes=[dST])
                            K.dve.op(lambda e: e.tensor_tensor(out=ST[:, 12:13], in0=ST[:, 11:12], in1=LM[:, 4:5], op=ALU.mult),
                                     reads=[d_lm], writes=[dST])
                            O1, dO1 = O1r.next()
                            K.dve.op(lambda e: e.tensor_scalar(out=O1[:], in0=po[:, 1, :], scalar1=ST[:, 12:13],
                                                               scalar2=None, op0=ALU.mult), reads=[dpo, dST], writes=[dO1])
                            K.dve.op(lambda e: e.scalar_tensor_tensor(
                                out=YB[:, qi, h * 128:(h + 1) * 128], in0=po[:, 0, :], scalar=ST[:, 10:11], in1=O1[:], op0=ALU.mult, op1=ALU.add),
                                reads=[dpo, dST, dO1], writes=[dYB] if fy else [], adds=[] if fy else [dYB])
                            K.act.op(lambda e: e.activation(out=junk[:], in_=YB[:, qi, h * 128:(h + 1) * 128], func=AF.Square,
                                                            accum_out=SSQ[:, qi * 4 + h:qi * 4 + h + 1]),
                                     reads=[dYB], writes=[d_junk], adds=[dSSQ])

                        last = (ii == len(items) - 1)
                        pipe.append([stA_pe, stA_rest, stB, stC, combine if last else None])
                        step_pipe()
                    first_yb = False
            flush_pipe()
            K.dve.op(lambda e: e.tensor_scalar(out=SSQ[:], in0=SSQ[:], scalar1=1.0 / 128, scalar2=1e-5, op0=ALU.mult, op1=ALU.add),
                     reads=[dSSQ], writes=[dSSQ])
            K.act.op(lambda e: e.activation(out=SSQ[:], in_=SSQ[:], func=AF.Ln), writes=[dSSQ])
            K.act.op(lambda e: e.activation(out=SSQ[:], in_=SSQ[:], func=AF.Exp, scale=-0.5), writes=[dSSQ])
            YBv = YB[:].rearrange("p q (h e) -> p (q h) e", e=128)
            K.dve.op(lambda e: e.tensor_tensor(out=YBv, in0=YBv, in1=SSQ[:].unsqueeze(2).broadcast_to([128, NQ * 4, 128]), op=ALU.mult),
                     reads=[dSSQ], writes=[dYB])
            K.pool.op(lambda e: e.tensor_tensor(out=YBv, in0=YBv, in1=SWb[:].unsqueeze(1).broadcast_to([128, NQ * 4, 128]), op=ALU.mult),
                      reads=[d_c], writes=[dYB])
            for qi in range(NQ):
                ptp, dptp = PTp.next()
                for h in range(4):
                    K.pe.op(lambda e, ptp=ptp, qi=qi, h=h: e.transpose(out=ptp[:, h, :], in_=YB[:, qi, h * 128:(h + 1) * 128],
                                                                      identity=identb[:]),
                            reads=[dYB, d_c], writes=[dptp] if h == 0 else [], adds=[] if h == 0 else [dptp])
                YT, dYT = YTr.next()
                K.act.op(lambda e, ptp=ptp, YT=YT: e.activation(out=YT[:], in_=ptp[:, 0:4, :], func=AF.Copy),
                         reads=[dptp], writes=[dYT])
                t0 = b * S + qi * 128
                K.sp.dma(lambda e, YT=YT, t0=t0: e.dma_start(
                    out=dap(scr["yb_fm"], t0, [[T, 128], [128 * T, 4], [1, 128]]), in_=YT[:]),
                    reads=[dYT], adds=[scr["d_p3"]])


def load_w_bf16(K, es, src2d, rows, cols, name, dep, stage_rot, q, W=None):
    nk = rows // 128
    if W is None:
        W = K.sb([128, nk, cols], BF16, name, es)
    for kc in range(nk):
        for c0 in range(0, cols, 1024):
            n = min(1024, cols - c0)
            st, dst = stage_rot.next()
            q[0] += 1
            (K.sp if q[0] % 2 == 0 else K.pool).dma(lambda e, st=st, kc=kc, c0=c0, n=n: e.dma_start(
                out=st[:, :n], in_=src2d[kc * 128:(kc + 1) * 128, c0:c0 + n]), writes=[dst])
            if q[0] % 2 == 0:
                K.act.op(lambda e, st=st, kc=kc, c0=c0, n=n: e.activation(out=W[:, kc, c0:c0 + n], in_=st[:, :n], func=AF.Copy),
                         reads=[dst], adds=[dep])
            else:
                K.dve.op(lambda e, st=st, kc=kc, c0=c0, n=n: e.tensor_copy(out=W[:, kc, c0:c0 + n], in_=st[:, :n]),
                         reads=[dst], adds=[dep])
    return W


def phase4(K, cfg, io, scr):
    T, S, NB = cfg["T"], cfg["S"], cfg["NB"]
    NT = T // 512
    with ExitStack() as es:
        d_c = Dep()
        identb = K.sb([128, 128], BF16, "m_identb", es)
        K.sp.dma(lambda e: e.dma_start(out=identb[:], in_=io["ident_bf"][:, :]), adds=[d_c])
        NF = K.sb([128, D], F32, "NF", es)
        K.sp.dma(lambda e: e.dma_start(out=NF[:], in_=io["norm_ffn_w"][0:1, :].broadcast_to([128, D])), adds=[d_c])
        stg = Rot(K, 2, [128, 1024], F32, "m_stg", es=es)
        q = [0]
        PAw = load_w_bf16(K, es, io["proj_a"][0], 512, D, "PAw", d_c, stg, q)
        PBw = load_w_bf16(K, es, io["proj_b"][0], 512, D, "PBw", d_c, stg, q)
        WO = load_w_bf16(K, es, io["w_out"][0], D, D, "WO", d_c, stg, q)
        YAr = Rot(K, 2, [128, 4, 512], BF16, "mYA", es=es)
        YBr = Rot(K, 2, [128, 4, 512], BF16, "mYB", es=es)
        SGr = Rot(K, 2, [128, 16, 512], BF16, "mSG", es=es)
        MGr = Rot(K, 2, [128, 8, 512], BF16, "mMG", es=es)
        t1r = Rot(K, 2, [128, 512], F32, "mt1", es=es)
        t2r = Rot(K, 2, [128, 512], F32, "mt2", es=es)
        Xr = Rot(K, 2, [128, D], F32, "mX", es=es)
        X1r = Rot(K, 2, [128, D], F32, "mX1", es=es)
        XHr = Rot(K, 2, [128, D], F32, "mXH", es=es)
        XBr = Rot(K, 2, [128, D], BF16, "mXB", es=es)
        XTr = Rot(K, 2, [128, 8, 128], BF16, "mXT", es=es)
        junk = K.sb([128, D], BF16, "mjunk", es)
        d_junk = Dep()
        STr = Rot(K, 4, [128, 4], F32, "mST", es=es)
        PP = Rot(K, 2, [128, 2, 512], F32, "mPP", psum=True, es=es)
        PO2 = Rot(K, 1, [128, 2, 512], F32, "mPO", psum=True, es=es)
        PTp = Rot(K, 1, [128, 8, 128], BF16, "mPTp", psum=True, es=es)
        for ti in range(NT):
            cs = slice(ti * 512, (ti + 1) * 512)
            YA, dYA = YAr.next()
            YB, dYB = YBr.next()
            SG, dSG = SGr.next()
            K.sp.dma(lambda e, YA=YA, cs=cs: e.dma_start(out=YA[:], in_=scr["ya_fm"][:, cs].rearrange("(c p) t -> p c t", p=128)),
                     reads=[scr["d_p2c"]], writes=[dYA])
            K.pool.dma(lambda e, YB=YB, cs=cs: e.dma_start(out=YB[:], in_=scr["yb_fm"][:, cs].rearrange("(c p) t -> p c t", p=128)),
                       reads=[scr["d_p3"]], writes=[dYB])
            K.sp.dma(lambda e, SG=SG, cs=cs: e.dma_start(out=SG[:], in_=scr["sg_fm"][:, cs].rearrange("(c p) t -> p c t", p=128)),
                     reads=[scr["d_p1"]], writes=[dSG])
            MG, dMG = MGr.next()
            for m in range(8):
                pp, dpp = PP.next()
                for c in range(4):
                    K.pe.op(lambda e, pp=pp, YA=YA, c=c, m=m: e.matmul(pp[:, 0, :], PAw[:, c, m * 128:(m + 1) * 128], YA[:, c, :],
                                                                      start=(c == 0), stop=(c == 3)),
                            reads=[dYA, d_c], writes=[dpp] if c == 0 else [], adds=[] if c == 0 else [dpp])
                for c in range(4):
                    K.pe.op(lambda e, pp=pp, YB=YB, c=c, m=m: e.matmul(pp[:, 1, :], PBw[:, c, m * 128:(m + 1) * 128], YB[:, c, :],
                                                                      start=(c == 0), stop=(c == 3)),
                            reads=[dYB, d_c], adds=[dpp])
                t1, dt1 = t1r.next()
                t2, dt2 = t2r.next()
                K.dve.op(lambda e, t1=t1, pp=pp, SG=SG, m=m: e.tensor_tensor(out=t1[:], in0=pp[:, 0, :], in1=SG[:, m, :], op=ALU.mult),
                         reads=[dpp, dSG], writes=[dt1])
                K.dve.op(lambda e, t2=t2, pp=pp, SG=SG, m=m: e.tensor_tensor(out=t2[:], in0=pp[:, 1, :], in1=SG[:, 8 + m, :], op=ALU.mult),
                         reads=[dpp, dSG], writes=[dt2])
                K.pool.op(lambda e, t1=t1, t2=t2, MG=MG, m=m: e.tensor_tensor(out=MG[:, m, :], in0=t1[:], in1=t2[:], op=ALU.add),
                          reads=[dt1, dt2], writes=[dMG] if m == 0 else [], adds=[] if m == 0 else [dMG])
            for sub in range(4):
                t0 = ti * 512 + sub * 128
                X, dX = Xr.next()
                K.pool.dma(lambda e, X=X, t0=t0: e.dma_start(out=X[:], in_=io["x"][t0:t0 + 128, :]), writes=[dX])
                po, dpo = PO2.next()
                for n in range(2):
                    for m in range(8):
                        K.pe.op(lambda e, po=po, MG=MG, m=m, n=n, sub=sub: e.matmul(
                            po[:, n, :], MG[:, m, sub * 128:(sub + 1) * 128], WO[:, m, n * 512:(n + 1) * 512],
                            start=(m == 0), stop=(m == 7)), reads=[dMG, d_c],
                            writes=[dpo] if (m == 0 and n == 0) else [], adds=[] if (m == 0 and n == 0) else [dpo])
                X1, dX1 = X1r.next()
                K.dve.op(lambda e, X1=X1, X=X, po=po: e.tensor_tensor(out=X1[:], in0=X[:], in1=po[:].rearrange("p a b -> p (a b)"),
                                                                     op=ALU.add), reads=[dX, dpo], writes=[dX1])
                K.sp.dma(lambda e, X1=X1, t0=t0: e.dma_start(out=scr["x1_tm"][t0:t0 + 128, :], in_=X1[:]),
                         reads=[dX1], adds=[scr["d_p4"]])
                ST, dST = STr.next()
                K.act.op(lambda e, X1=X1, ST=ST: e.activation(out=junk[:], in_=X1[:], func=AF.Square, accum_out=ST[:, 0:1]),
                         reads=[dX1], writes=[d_junk, dST])
                K.dve.op(lambda e, ST=ST: e.tensor_scalar(out=ST[:, 1:2], in0=ST[:, 0:1], scalar1=1.0 / D, scalar2=1e-6,
                                                          op0=ALU.mult, op1=ALU.add), writes=[dST])
                K.act.op(lambda e, ST=ST: e.activation(out=ST[:, 2:3], in_=ST[:, 1:2], func=AF.Sqrt), writes=[dST])
                K.dve.op(lambda e, ST=ST: e.reciprocal(out=ST[:, 3:4], in_=ST[:, 2:3]), writes=[dST])
                XH, dXH = XHr.next()
                K.dve.op(lambda e, XH=XH, X1=X1, ST=ST: e.scalar_tensor_tensor(
                    out=XH[:], in0=X1[:], scalar=ST[:, 3:4], in1=NF[:], op0=ALU.mult, op1=ALU.mult),
                    reads=[dX1, dST, d_c], writes=[dXH])
                K.sp.dma(lambda e, XH=XH, t0=t0: e.dma_start(out=scr["xh_tm"][t0:t0 + 128, :], in_=XH[:]),
                         reads=[dXH], adds=[scr["d_p4"]])
                XB, dXB = XBr.next()
                K.pool.op(lambda e, XB=XB, XH=XH: e.tensor_copy(out=XB[:], in_=XH[:]), reads=[dXH], writes=[dXB])
                ptp, dptp = PTp.next()
                for kc in range(8):
                    K.pe.op(lambda e, ptp=ptp, XB=XB, kc=kc: e.transpose(out=ptp[:, kc, :], in_=XB[:, kc * 128:(kc + 1) * 128],
                                                                        identity=identb[:]),
                            reads=[dXB, d_c], writes=[dptp] if kc == 0 else [], adds=[] if kc == 0 else [dptp])
                XT, dXT = XTr.next()
                K.act.op(lambda e, ptp=ptp, XT=XT: e.activation(out=XT[:], in_=ptp[:], func=AF.Copy), reads=[dptp], writes=[dXT])
                K.sp.dma(lambda e, XT=XT, t0=t0: e.dma_start(
                    out=dap(scr["xhT_fm"], t0, [[T, 128], [128 * T, 8], [1, 128]]), in_=XT[:]),
                    reads=[dXT], adds=[scr["d_p4"]])


def phase5(K, cfg, io, scr):
    T, S, NB = cfg["T"], cfg["S"], cfg["NB"]
    NTT = T // 128
    with ExitStack() as es:
        d_c = Dep()
        identb = K.sb([128, 128], BF16, "f_identb", es)
        K.sp.dma(lambda e: e.dma_start(out=identb[:], in_=io["ident_bf"][:, :]), adds=[d_c])
        IOTA = K.sb([128, 16], F32, "IOTA", es)
        K.sp.dma(lambda e: e.dma_start(out=IOTA[:], in_=io["iota16"][:, :]), adds=[d_c])
        FNW = K.sb([128, D], F32, "FNW", es)
        K.sp.dma(lambda e: e.dma_start(out=FNW[:], in_=io["final_norm_w"][0:1, :].broadcast_to([128, D])), adds=[d_c])
        WQ = K.sb([128, 8, 2048], BF16, "WQ", es)
        KT = K.sb([128, 16, 128], BF16, "KT", es)
        PQ = Rot(K, 1, [128, 8, 128], F32, "fPQ", psum=True, es=es)
        PSc = Rot(K, 1, [128, 8, 128], F32, "fPSc", psum=True, es=es)
        PKT = Rot(K, 1, [128, 8, 128], BF16, "fPKT", psum=True, es=es)
        es_setup = ExitStack()
        stg = Rot(K, 2, [128, 1024], F32, "f_stg", es=es_setup)
        q = [0]
        load_w_bf16(K, es, io["peer_wq"][0], D, 2048, "WQ", d_c, stg, q, W=WQ)
        KF = K.sb([128, 16, 128], F32, "KF", es_setup)
        dKF = Dep()
        K.sp.dma(lambda e: e.dma_start(out=KF[:], in_=io["peer_keys"][0].rearrange("h c n d -> n (h c) d")), writes=[dKF])
        KB = K.sb([128, 16, 128], BF16, "KB", es_setup)
        K.dve.op(lambda e: e.tensor_copy(out=KB[:], in_=KF[:]), reads=[dKF], writes=[dKF])
        for half in range(2):
            pk, dpk = PKT.next()
            for i in range(8):
                K.pe.op(lambda e, pk=pk, i=i, half=half: e.transpose(out=pk[:, i, :], in_=KB[:, half * 8 + i, :], identity=identb[:]),
                        reads=[dKF, d_c], writes=[dpk] if i == 0 else [], adds=[] if i == 0 else [dpk])
            K.act.op(lambda e, pk=pk, half=half: e.activation(out=KT[:, half * 8:(half + 1) * 8, :], in_=pk[:], func=AF.Copy),
                     reads=[dpk], adds=[d_c])

        K.barrier()
        es_setup.close()
        XTr = Rot(K, 2, [128, 8, 128], BF16, "fXT", es=es)
        XHr = Rot(K, 2, [128, D], F32, "fXH", es=es)
        X1r = Rot(K, 2, [128, D], F32, "fX1", es=es)
        QTr = Rot(K, 1, [128, 16, 128], BF16, "fQT", es=es)
        SCr = Rot(K, 1, [128, 16, 128], F32, "fSC", es=es)
        SC2 = K.sb([128, 256], F32, "fSC2", es)
        dSC2 = Dep()
        M16r = Rot(K, 1, [128, 16, 16], F32, "fM16", es=es)
        I16r = Rot(K, 1, [128, 16, 16], U32, "fI16", es=es)
        I16fr = Rot(K, 1, [128, 16, 16], F32, "fI16f", es=es)
        CANDr = Rot(K, 1, [128, 8, 256], F32, "fCAND", es=es)
        VALr = Rot(K, 1, [128, 8, 16], F32, "fVAL", es=es)
        CIr = Rot(K, 1, [128, 3, 128], U32, "fCI", es=es)
        ABr = Rot(K, 1, [128, 2, 128], F32, "fAB", es=es)
        OHr = CANDr
        E12r = Rot(K, 1, [128, 3, 128], F32, "fE12", es=es)
        IDSr = Rot(K, 2, [128, 128], I32, "fIDS", es=es)
        GTr = Rot(K, 2, [128, 4, 128], F32, "fGT", es=es)
        S8r = Rot(K, 2, [128, 16], F32, "fS8", es=es)
        GRP = cfg.get("grp", 4)
        ACTDOT = tuple(cfg.get("actdot", (1, 3)))
        if isinstance(cfg.get("actdot_mask"), int):
            ACTDOT = tuple(i for i in range(GRP) if (cfg["actdot_mask"] >> i) & 1)
        junk3r = Rot(K, 2, [128, D], BF16, "fjunk3", es=es)
        junkr = Rot(K, 3, [128, D], BF16, "fjunkr", es=es)
        PRDr = Rot(K, 3, [128, D], BF16, "fPRD", es=es)
        GBr = Rot(K, cfg.get("ngbuf", 22), [128, 2 * D], BF16, "fGB", es=es)
        junk2 = K.sb([128, D], BF16, "fjunk2", es)
        d_junk2 = Dep()
        XHbr = Rot(K, 2, [128, D], BF16, "fXHb", es=es)
        DGr = Rot(K, 4, [128, 128], BF16, "fDG", es=es)
        PY = Rot(K, 1, [128, 2, 512], F32, "fPY", psum=True, es=es)

        RES = {}

        def routing(ti):
            t0 = ti * 128
            XT, dXT = XTr.next()
            XH, dXH = XHr.next()
            X1, dX1 = X1r.next()
            K.sp.dma(lambda e, XT=XT, t0=t0: e.dma_start(out=XT[:], in_=dap(scr["xhT_fm"], t0, [[T, 128], [128 * T, 8], [1, 128]])),
                     reads=[scr["d_p4"]], writes=[dXT])
            K.sp.dma(lambda e, XH=XH, t0=t0: e.dma_start(out=XH[:], in_=scr["xh_tm"][t0:t0 + 128, :]), reads=[scr["d_p4"]], writes=[dXH])
            K.sp.dma(lambda e, X1=X1, t0=t0: e.dma_start(out=X1[:], in_=scr["x1_tm"][t0:t0 + 128, :]), reads=[scr["d_p4"]], writes=[dX1])
            QT, dQT = QTr.next()
            for half in range(2):
                pq, dpq = PQ.next()
                for i in range(8):
                    hc = half * 8 + i
                    for kc in range(8):
                        K.pe.op(lambda e, pq=pq, i=i, hc=hc, kc=kc, XT=XT: e.matmul(
                            pq[:, i, :], WQ[:, kc, hc * 128:(hc + 1) * 128], XT[:, kc, :], start=(kc == 0), stop=(kc == 7)),
                            reads=[dXT, d_c], writes=[dpq] if (i == 0 and kc == 0) else [], adds=[] if (i == 0 and kc == 0) else [dpq])
                K.act.op(lambda e, pq=pq, QT=QT, half=half: e.activation(out=QT[:, half * 8:(half + 1) * 8, :], in_=pq[:], func=AF.Copy),
                         reads=[dpq], writes=[dQT] if half == 0 else [], adds=[] if half == 0 else [dQT])
            SC, dSC = SCr.next()
            for half in range(2):
                psc, dpsc = PSc.next()
                for i in range(8):
                    hc = half * 8 + i
                    K.pe.op(lambda e, psc=psc, i=i, hc=hc, QT=QT: e.matmul(psc[:, i, :], QT[:, hc, :], KT[:, hc, :], start=True, stop=True),
                            reads=[dQT, d_c], writes=[dpsc] if i == 0 else [], adds=[] if i == 0 else [dpsc])
                K.act.op(lambda e, psc=psc, SC=SC, half=half: e.activation(out=SC[:, half * 8:(half + 1) * 8, :], in_=psc[:], func=AF.Copy),
                         reads=[dpsc], writes=[dSC] if half == 0 else [], adds=[] if half == 0 else [dSC])
            yield
            M16, dM = M16r.next()
            I16, dI = I16r.next()
            for hc in range(16):
                if hc % 4 == 0 and hc > 0:
                    yield
                K.dve.op(lambda e, M16=M16, SC=SC, hc=hc: e.max(out=M16[:, hc, 0:8], in_=SC[:, hc, :]), reads=[dSC],
                         writes=[dM] if hc == 0 else [], adds=[] if hc == 0 else [dM])
                K.dve.op(lambda e, M16=M16, SC=SC, hc=hc: e.match_replace(out=SC2[:, 0:128], in_to_replace=M16[:, hc, 0:8],
                                                                         in_values=SC[:, hc, :], imm_value=-1e30),
                         reads=[dSC, dM], writes=[dSC2])
                K.dve.op(lambda e, M16=M16, hc=hc: e.max(out=M16[:, hc, 8:16], in_=SC2[:, 0:128]), reads=[dSC2], adds=[dM])
                K.dve.op(lambda e, M16=M16, I16=I16, SC=SC, hc=hc: e.max_index(out=I16[:, hc, 0:8], in_max=M16[:, hc, 0:8],
                                                                              in_values=SC[:, hc, :]),
                         reads=[dSC, dM], writes=[dI] if hc == 0 else [], adds=[] if hc == 0 else [dI])
                K.dve.op(lambda e, M16=M16, I16=I16, SC=SC, hc=hc: e.max_index(out=I16[:, hc, 8:16], in_max=M16[:, hc, 8:16],
                                                                              in_values=SC[:, hc, :]),
                         reads=[dSC, dM], adds=[dI])
            yield
            I16f, dIf = I16fr.next()
            K.dve.op(lambda e, I16f=I16f, I16=I16: e.tensor_copy(out=I16f[:], in_=I16[:]), reads=[dI], writes=[dIf])
            I16fv = I16f[:].rearrange("p (h c) k -> p h c k", c=2)
            K.dve.op(lambda e, I16fv=I16fv: e.tensor_scalar(out=I16fv[:, :, 0, :], in0=I16fv[:, :, 0, :], scalar1=128.0, scalar2=None,
                                                            op0=ALU.mult), writes=[dIf])
            CAND, dCA = CANDr.next()
            M16v = M16[:].rearrange("p (h c) k -> p h c k", c=2)
            K.dve.op(lambda e, CAND=CAND, M16v=M16v: e.tensor_tensor(
                out=CAND[:].rearrange("p h (a b) -> p h a b", b=16),
                in0=M16v[:, :, 0, :].unsqueeze(3).broadcast_to([128, 8, 16, 16]),
                in1=M16v[:, :, 1, :].unsqueeze(2).broadcast_to([128, 8, 16, 16]), op=ALU.add),
                reads=[dM], writes=[dCA])
            VAL, dVAL = VALr.next()
            CI, dCI = CIr.next()
            CIv = CI[:, 0, :].rearrange("p (h k) -> p h k", k=16)
            for h in range(8):
                if h % 4 == 0:
                    yield
                K.dve.op(lambda e, VAL=VAL, CAND=CAND, h=h: e.max(out=VAL[:, h, 0:8], in_=CAND[:, h, :]), reads=[dCA],
                         writes=[dVAL] if h == 0 else [], adds=[] if h == 0 else [dVAL])
                K.dve.op(lambda e, VAL=VAL, CAND=CAND, h=h: e.match_replace(out=SC2[:, :], in_to_replace=VAL[:, h, 0:8],
                                                                           in_values=CAND[:, h, :], imm_value=-1e30),
                         reads=[dCA, dVAL], writes=[dSC2])
                K.dve.op(lambda e, VAL=VAL, h=h: e.max(out=VAL[:, h, 8:16], in_=SC2[:, :]), reads=[dSC2], adds=[dVAL])
                K.dve.op(lambda e, VAL=VAL, CIv=CIv, CAND=CAND, h=h: e.max_index(out=CIv[:, h, 0:8], in_max=VAL[:, h, 0:8],
                                                                                in_values=CAND[:, h, :]),
                         reads=[dCA, dVAL], writes=[dCI] if h == 0 else [], adds=[] if h == 0 else [dCI])
                K.dve.op(lambda e, VAL=VAL, CIv=CIv, CAND=CAND, h=h: e.max_index(out=CIv[:, h, 8:16], in_max=VAL[:, h, 8:16],
                                                                                in_values=CAND[:, h, :]),
                         reads=[dCA, dVAL], adds=[dCI])
            yield
            GT, dGT = GTr.next()
            S8, dS8 = S8r.next()
            Ev = GT[:, 0, :].rearrange("p (h k) -> p h k", k=16)
            Gv = GT[:, 1, :].rearrange("p (h k) -> p h k", k=16)
            K.dve.op(lambda e, Ev=Ev, VAL=VAL: e.tensor_tensor(out=Ev, in0=VAL[:], in1=VAL[:, :, 0:1].broadcast_to([128, 8, 16]),
                                                              op=ALU.subtract), reads=[dVAL], writes=[dGT])
            K.act.op(lambda e, GT=GT: e.activation(out=GT[:, 0, :], in_=GT[:, 0, :], func=AF.Exp), writes=[dGT])
            K.dve.op(lambda e, Ev=Ev, S8=S8: e.tensor_reduce(out=S8[:, 0:8], in_=Ev, axis=AX.X, op=ALU.add), reads=[dGT], writes=[dS8])
            K.dve.op(lambda e, S8=S8: e.reciprocal(out=S8[:, 8:16], in_=S8[:, 0:8]), writes=[dS8])
            K.dve.op(lambda e, Ev=Ev, Gv=Gv, S8=S8: e.tensor_tensor(out=Gv, in0=Ev, in1=S8[:, 8:16].unsqueeze(2).broadcast_to([128, 8, 16]),
                                                                   op=ALU.mult), reads=[dS8], writes=[dGT])
            yield
            K.dve.op(lambda e, CI=CI: e.tensor_single_scalar(out=CI[:, 1, :], in_=CI[:, 0, :], scalar=4, op=ALU.logical_shift_right),
                     writes=[dCI])
            K.dve.op(lambda e, CI=CI: e.tensor_single_scalar(out=CI[:, 2, :], in_=CI[:, 0, :], scalar=15, op=ALU.bitwise_and),
                     writes=[dCI])
            AB, dAB = ABr.next()
            K.dve.op(lambda e, AB=AB, CI=CI: e.tensor_copy(out=AB[:], in_=CI[:, 1:3, :]), reads=[dCI], writes=[dAB])
            OH, dOH = OHr.next()
            E12, dE12 = E12r.next()
            OHv = OH[:].rearrange("p h (j a) -> p h j a", a=16)
            for c in range(2):
                ABv = AB[:, c, :].rearrange("p (h j) -> p h j", j=16)
                K.dve.op(lambda e, OHv=OHv, ABv=ABv: e.tensor_tensor(
                    out=OHv, in0=ABv.unsqueeze(3).broadcast_to([128, 8, 16, 16]),
                    in1=IOTA[:].unsqueeze(1).unsqueeze(1).broadcast_to([128, 8, 16, 16]), op=ALU.is_equal),
                    reads=[dAB, d_c], writes=[dOH])
                K.dve.op(lambda e, OHv=OHv, I16fv=I16fv, c=c: e.tensor_tensor(
                    out=OHv, in0=OHv, in1=I16fv[:, :, c, :].unsqueeze(2).broadcast_to([128, 8, 16, 16]), op=ALU.mult),
                    reads=[dIf], writes=[dOH])
                K.dve.op(lambda e, OHv=OHv, E12=E12, c=c: e.tensor_reduce(
                    out=E12[:, c, :].rearrange("p (h j) -> p h j", j=16), in_=OHv, axis=AX.X, op=ALU.add),
                    reads=[dOH], writes=[dE12] if c == 0 else [], adds=[] if c == 0 else [dE12])
            K.dve.op(lambda e, E12=E12: e.tensor_tensor(out=E12[:, 2, :], in0=E12[:, 0, :], in1=E12[:, 1, :], op=ALU.add), writes=[dE12])
            IDS, dIDS = IDSr.next()
            K.dve.op(lambda e, IDS=IDS, E12=E12: e.tensor_copy(out=IDS[:], in_=E12[:, 2, :]), reads=[dE12], writes=[dIDS])
            if "ids_dbg" in scr:
                K.sp.dma(lambda e, IDS=IDS, t0=t0: e.dma_start(out=scr["ids_dbg"][t0:t0 + 128, :], in_=IDS[:]), reads=[dIDS])
                K.sp.dma(lambda e, GT=GT, t0=t0: e.dma_start(out=scr["gate_dbg"][t0:t0 + 128, :], in_=GT[:, 1, :]), reads=[dGT])
            RES[ti] = dict(t0=t0, XH=XH, dXH=dXH, X1=X1, dX1=dX1, IDS=IDS, dIDS=dIDS, GT=GT, dGT=dGT, S8=S8, dS8=dS8)

        GDEPS = {}

        def expert(R):
            t0, XH, dXH, X1, dX1, IDS, dIDS, GT, dGT, S8, dS8 = (R[k] for k in
                ("t0", "XH", "dXH", "X1", "dX1", "IDS", "dIDS", "GT", "dGT", "S8", "dS8"))
            XHb, dXHb = XHbr.next()
            K.act.op(lambda e: e.activation(out=XHb[:], in_=XH[:], func=AF.Copy), reads=[dXH], writes=[dXHb])
            py, dpy = PY.next()
            NGRP = 128 // GRP
            bufs = {}
            gd = GDEPS.setdefault(id(GT), [(Dep(), Dep()) for _ in range(NGRP)])

            def stage_a(g):
                for jj in range(GRP):
                    j = g * GRP + jj
                    GB, dGB = GBr.next()
                    bufs[j] = (GB, dGB)
                    K.pool.dma(lambda e, GB=GB, j=j: e.indirect_dma_start(
                        out=GB[:], out_offset=None, in_=scr["uv_tab"][:, :],
                        in_offset=bass.IndirectOffsetOnAxis(ap=IDS[:, j:j + 1], axis=0)), reads=[dIDS, scr["d_uv"]], writes=[dGB])
                    if jj in ACTDOT:
                        PRD, dPRD = PRDr.next()
                        K.dve.op(lambda e, GB=GB, PRD=PRD: e.tensor_tensor(out=PRD[:], in0=GB[:, 0:D], in1=XHb[:], op=ALU.mult),
                                 reads=[dGB, dXHb], writes=[dPRD])
                        j3, dj3 = junk3r.next()
                        K.act.op(lambda e, PRD=PRD, j=j, j3=j3: e.activation(out=j3[:], in_=PRD[:], func=AF.Copy, accum_out=GT[:, 2, j:j + 1]),
                                 reads=[dPRD], writes=[dj3] + ([gd[g][0]] if jj == 0 else []), adds=[] if jj == 0 else [gd[g][0]])
                    else:
                        j1, dj1 = junkr.next()
                        K.dve.op(lambda e, GB=GB, j=j, j1=j1: e.scalar_tensor_tensor(
                            out=j1[:], in0=GB[:, 0:D], scalar=1.0, in1=XHb[:], op0=ALU.mult, op1=ALU.mult, accum_out=GT[:, 2, j:j + 1]),
                            reads=[dGB, dXHb], writes=[dj1] + ([gd[g][0]] if jj == 0 else []), adds=[] if jj == 0 else [gd[g][0]])
                gs = slice(g * GRP, (g + 1) * GRP)
                K.act.op(lambda e: e.activation(out=GT[:, 3, gs], in_=GT[:, 2, gs], func=AF.Gelu), reads=[gd[g][0]], writes=[gd[g][1]])

            def stage_b(g):
                gs = slice(g * GRP, (g + 1) * GRP)
                K.dve.op(lambda e: e.tensor_tensor(out=GT[:, 3, gs], in0=GT[:, 3, gs], in1=GT[:, 1, gs], op=ALU.mult), reads=[dGT], writes=[gd[g][1]])
                for jj in range(GRP):
                    j = g * GRP + jj
                    GB, dGB = bufs.pop(j)
                    DG, dDG = DGr.next()
                    K.act.op(lambda e, DG=DG, j=j: e.activation(out=DG[:], in_=identb[:], func=AF.Copy, scale=GT[:, 3, j:j + 1]),
                             reads=[gd[g][1], d_c], writes=[dDG])
                    for n in range(2):
                        K.pe.op(lambda e, DG=DG, GB=GB, n=n, j=j: e.matmul(
                            py[:, n, :], DG[:], GB[:, D + n * 512:D + (n + 1) * 512], start=(j == 0), stop=(j == 127)),
                            reads=[dDG, dGB], writes=[dpy] if (j == 0 and n == 0) else [], adds=[] if (j == 0 and n == 0) else [dpy])

            gen = routing(R["next"]) if R.get("next") is not None else None
            for g in range(NGRP):
                stage_a(g)
                if g >= 1:
                    stage_b(g - 1)
                if gen is not None and g >= 2 and g % 2 == 0:
                    try:
                        next(gen)
                    except StopIteration:
                        gen = None
            stage_b(NGRP - 1)
            if gen is not None:
                for _ in gen:
                    pass
            K.dve.op(lambda e: e.tensor_tensor(out=X1[:], in0=X1[:], in1=py[:].rearrange("p a b -> p (a b)"), op=ALU.add),
                     reads=[dpy], writes=[dX1])
            K.act.op(lambda e: e.activation(out=junk2[:], in_=X1[:], func=AF.Square, accum_out=S8[:, 0:1]),
                     reads=[dX1], writes=[d_junk2, dS8])
            K.dve.op(lambda e: e.tensor_scalar(out=S8[:, 1:2], in0=S8[:, 0:1], scalar1=1.0 / D, scalar2=1e-6,
                                               op0=ALU.mult, op1=ALU.add), writes=[dS8])
            K.act.op(lambda e: e.activation(out=S8[:, 2:3], in_=S8[:, 1:2], func=AF.Sqrt), writes=[dS8])
            K.dve.op(lambda e: e.reciprocal(out=S8[:, 3:4], in_=S8[:, 2:3]), writes=[dS8])
            K.dve.op(lambda e: e.scalar_tensor_tensor(
                out=XH[:], in0=X1[:], scalar=S8[:, 3:4], in1=FNW[:], op0=ALU.mult, op1=ALU.mult),
                reads=[dX1, dS8, d_c], writes=[dXH])
            K.sp.dma(lambda e: e.dma_start(out=io["out"][t0:t0 + 128, :], in_=XH[:]), reads=[dXH])

        for _ in routing(0):
            pass
        for ti in range(NTT):
            R = RES.pop(ti)
            R["next"] = ti + 1 if ti + 1 < NTT else None
            expert(R)


def make_consts():
    c = {}
    c["ident_bf"] = np.eye(128, dtype=np.float32).astype(ml_dtypes.bfloat16)
    c["ident_f"] = np.eye(128, dtype=np.float32)
    bo = np.zeros((128, 128), np.float32)
    bo[:64, :64] = 1.0 / 64
    bo[64:, 64:] = 1.0 / 64
    c["blockones"] = bo
    pp = np.arange(128)[:, None]
    ff = np.arange(128)[None, :]
    c["tri"] = ((pp <= ff) & (pp // 64 == ff // 64)).astype(np.float32)
    p6 = np.arange(64)[:, None]
    f6 = np.arange(64)[None, :]
    mk = np.zeros((64, 3, 64), np.float32)
    mk[:, 0, :] = (f6 < p6)
    mk[:, 1, :] = (f6 > p6)
    mk[:, 2, :] = (f6 >= p6)
    c["masks"] = mk
    c["ident64"] = np.eye(64, dtype=np.float32).astype(ml_dtypes.bfloat16)
    c["ones64"] = np.ones((64, 1), np.float32)
    c["iota16"] = np.tile(np.arange(16, dtype=np.float32)[None, :], (128, 1))
    return c


def make_alibi(S):
    al = np.zeros((4, 128, S), np.float32)
    ql = np.arange(128)[:, None]
    m = np.arange(S)[None, :]
    for h in range(4):
        slope = 2.0 ** (-8.0 * (h + 1) / 4)
        v = -slope * (ql - m + (S - 128)).astype(np.float32)
        al[h] = np.where(m <= ql + (S - 128), v, -30000.0)
    return al


def _unused():
    c = {}
    return c


def build(cfg):
    NB, S = cfg["NB"], cfg["S"]
    T = NB * S
    cfg["T"] = T
    dbg = set(cfg.get("debug", ()))
    phases = cfg.get("phases", (1,))
    nc = bass.Bass("TRN2", target_bir_lowering=False)
    io = {}

    def inp(name, shape, dt=F32):
        io[name] = nc.dram_tensor(name, list(shape), dt, kind="ExternalInput").ap()

    inp("x", [T, D])
    inp("norm_mix_w", [1, D])
    inp("w_in", [1, D, IN_COLS])
    inp("ident_bf", [128, 128], BF16)
    inp("ident_f", [128, 128])
    inp("blockones", [128, 128])
    inp("tri", [128, 128])
    inp("masks", [64, 3, 64])
    inp("ident64", [64, 64], BF16)
    inp("ones64", [64, 1])
    inp("alibi", [4, 128, S])
    for nm, shp in [("lam_q1", [1, 64]), ("lam_k1", [1, 64]), ("lam_q2", [1, 64]), ("lam_k2", [1, 64]),
                    ("subln_w", [1, 128])]:
        inp(nm, shp)
    scr = {}

    def scratch(name, shape, dt):
        kind = "ExternalOutput" if name in dbg else "Internal"
        scr[name] = nc.dram_tensor(name, list(shape), dt, kind=kind).ap()

    scratch("zs_tm", [T, SHIFT_COLS], F32)
    scratch("zv_fm", [512, T], F32)
    scratch("qk_fm", [1024, T], BF16)
    scratch("av_tm", [T, 512], BF16)
    scratch("sg_fm", [2048, T], BF16)
    if not cfg.get("chunked", True):
        scratch("rw_tm", [T, 5, 512], F32)
    scratch("ab_tm", [T, 5, 512], BF16)
    scratch("lw_tm", [T, 512], F32)
    scratch("v_fm", [512, T], F32)
    scratch("g_fm", [512, T], F32)
    scratch("coef_fm", [8, T], F32)
    scratch("y_fm", [512, T], F32)
    scratch("ya_fm", [512, T], BF16)
    scr["d_p1"] = Dep()
    scr["d_p2a"] = Dep()
    scr["d_p2b"] = Dep()
    scr["d_p2c"] = Dep()
    scr["d_p3"] = Dep()
    scr["d_p4"] = Dep()
    scr["d_uv"] = Dep()
    scratch("uv_tab", [16384, 2 * D], BF16)
    if "ids_dbg" in dbg:
        scratch("ids_dbg", [T, 128], I32)
        scratch("gate_dbg", [T, 128], F32)
    inp("peer_wq", [1, D, 2048])
    inp("peer_keys", [1, 8, 2, 128, 128])
    inp("peer_u", [1, 16384, D])
    inp("peer_v", [1, 16384, D])
    inp("final_norm_w", [1, D])
    inp("iota16", [128, 16])
    io["out"] = nc.dram_tensor("out", [T, D], F32, kind="ExternalOutput").ap()
    scratch("x1_tm", [T, D], F32)
    scratch("xh_tm", [T, D], F32)
    scratch("xhT_fm", [D, T], BF16)
    for nm, shp in [("proj_a", [1, 512, D]), ("proj_b", [1, 512, D]), ("w_out", [1, D, D]), ("norm_ffn_w", [1, D])]:
        inp(nm, shp)
    scratch("yb_fm", [512, T], BF16)
    for nm, shp in [("shift_mu", [1, SHIFT_COLS]), ("w0", [1, 512]), ("w2", [1, 64, 512]), ("a0", [1, 512]),
                    ("a2", [1, 64, 512]), ("g2", [1, 128, 512]), ("k_k", [1, 512]), ("k_a", [1, 512]),
                    ("r_k", [1, 8, 64]), ("lnx_w", [1, 512]), ("lnx_b", [1, 512])]:
        inp(nm, shp)
    with ExitStack() as es:
        K = Kern(nc, es, pool_slots=cfg.get("pool_slots", 8))
        K.scopes = bool(cfg.get("scopes", False))
        if 5 in phases:
            K.phase = "p0_uvtab"
            RB = 2048
            for r0 in range(0, 16384, RB):
                K.pool.dma(lambda e, r0=r0: e.dma_start(out=scr["uv_tab"][r0:r0 + RB, 0:D], in_=io["peer_u"][0, r0:r0 + RB, :]),
                           adds=[scr["d_uv"]])
                K.pool.dma(lambda e, r0=r0: e.dma_start(out=scr["uv_tab"][r0:r0 + RB, D:2 * D], in_=io["peer_v"][0, r0:r0 + RB, :]),
                           adds=[scr["d_uv"]])
        if 1 in phases:
            K.phase = "p1_inproj"
            phase1(K, cfg, io, scr)
            K.barrier()
        if 2 in phases:
            K.phase = "p2a_prep"
            phase2_prep(K, cfg, io, scr)
            K.barrier()
            K.phase = "p2b_scan"
            if cfg.get("chunked", True):
                phase2_chunk(K, cfg, io, scr)
            else:
                phase2_scan(K, cfg, io, scr)
            K.barrier()
            K.phase = "p2c_post"
            phase2_post(K, cfg, io, scr)
            K.barrier()
        if 3 in phases:
            K.phase = "p3_attn"
            phase3(K, cfg, io, scr)
            K.barrier()
        if 4 in phases:
            K.phase = "p4_merge"
            phase4(K, cfg, io, scr)
            K.barrier()
        if 5 in phases:
            K.phase = "p5_peer"
            phase5(K, cfg, io, scr)
            K.barrier()
        K.finish()
    return nc, io, scr


def kernel(**inputs):
    NB, S = 4, 2048
    cfg = dict(NB=NB, S=S, phases=(1, 2, 3, 4, 5))
    nc, io, scr = build(cfg)
    consts = make_consts()
    consts["alibi"] = make_alibi(S)
    x = np.ascontiguousarray(np.asarray(inputs["x"], dtype=np.float32))
    shared = {}
    for name in io:
        if name in ("x", "out"):
            continue
        if name in consts:
            shared[name] = consts[name]
        elif name == "final_norm_w":
            shared[name] = np.ascontiguousarray(np.asarray(inputs[name], dtype=np.float32).reshape(1, D))
        else:
            shared[name] = np.ascontiguousarray(np.asarray(inputs[name], dtype=np.float32))
    in_maps = []
    for c in range(NCORES):
        m = dict(shared)
        m["x"] = x[c * NB:(c + 1) * NB].reshape(NB * S, D)
        in_maps.append(m)
    res = run_bass_kernel_spmd(nc, in_maps, core_ids=list(range(NCORES)))
    out = np.concatenate([np.asarray(r["out"]).reshape(NB, S, D) for r in res.results], axis=0)
    return out.astype(np.float32)
```

```python
import numpy as np
import ml_dtypes
from contextlib import ExitStack
import concourse.bass as bass
import concourse.mybir as mybir
from concourse.bass_utils import run_bass_kernel_spmd

F32 = mybir.dt.float32
BF16 = mybir.dt.bfloat16
I32 = mybir.dt.int32
U32 = mybir.dt.uint32
ALU = mybir.AluOpType
AF = mybir.ActivationFunctionType
AX = mybir.AxisListType

D = 1024
IN_COLS = 5376
SHIFT_COLS = 1792
NCORES = 8


class Dep:
    __slots__ = ("w", "r", "pw", "pr")

    def __init__(self):
        self.w = {}
        self.r = {}
        self.pw = {}
        self.pr = {}


class Stream:
    def __init__(self, K, name, is_pe=False, ndma=0):
        self.K = K
        self.name = name
        self.sem = K.new_sem("s_" + name)
        self.cnt = 0
        self.items = []
        self.waited = {}
        self.is_pe = is_pe
        self.dsems = [K.new_sem("d_%s%d" % (name, i)) for i in range(ndma)]
        self.duses = [0] * ndma
        self.dj = 0

    def wait_tok(self, tok):
        if tok is None:
            return
        sem, val = tok
        if sem is self.sem and self.is_pe:
            return
        key = id(sem)
        if self.waited.get(key, 0) >= val:
            return
        self.waited[key] = val
        self.items.append(("w", sem, val, self.K.phase))

    def _pre(self, reads, writes, adds):
        for d in reads:
            for t in list(d.w.values()):
                self.wait_tok(t)
        for d in writes:
            for t in list(d.w.values()):
                self.wait_tok(t)
            for t in list(d.r.values()):
                self.wait_tok(t)
        for d in adds:
            for t in list(d.r.values()) + list(d.pr.values()) + list(d.pw.values()):
                self.wait_tok(t)

    def _post(self, tok, reads, writes, adds):
        for d in reads:
            d.r[id(tok[0])] = tok
        for d in writes:
            d.pw = d.w
            d.pr = d.r
            d.w = {id(tok[0]): tok}
            d.r = {}
        for d in adds:
            d.w[id(tok[0])] = tok

    def op(self, fn, reads=(), writes=(), adds=()):
        self._pre(reads, writes, adds)
        self.cnt += 1
        tok = (self.sem, self.cnt)
        self.items.append(("o", fn, self.sem, 1, self.K.phase))
        self._post(tok, reads, writes, adds)
        return tok

    def dma(self, fn, reads=(), writes=(), adds=()):
        self._pre(reads, writes, adds)
        n = len(self.dsems)
        slot = self.dj % n
        self.dj += 1
        if self.duses[slot] > 0:
            self.wait_tok((self.dsems[slot], 16 * self.duses[slot]))
        self.duses[slot] += 1
        tok = (self.dsems[slot], 16 * self.duses[slot])
        self.items.append(("o", fn, self.dsems[slot], 16, self.K.phase))
        self._post(tok, reads, writes, adds)
        return tok

    def replay(self, eng):
        nc = self.K.nc
        cur = None
        ctx = None
        for it in self.items:
            ph = it[-1]
            if self.K.scopes and ph != cur:
                if ctx is not None:
                    ctx.__exit__(None, None, None)
                ctx = nc.named_scope(ph)
                ctx.__enter__()
                cur = ph
            if it[0] == "w":
                eng.wait_ge(it[1], it[2])
            else:
                ins = it[1](eng)
                ins.then_inc(it[2], it[3])
        if ctx is not None:
            ctx.__exit__(None, None, None)


class Kern:
    def __init__(self, nc, es, pool_slots=8):
        self.nc = nc
        self.es = es
        self.nsem = 0
        self.phase = "init"
        self.scopes = False
        self.pe = Stream(self, "pe", is_pe=True)
        self.act = Stream(self, "act", ndma=4)
        self.dve = Stream(self, "dve")
        self.pool = Stream(self, "pool", ndma=pool_slots)
        self.sp = Stream(self, "sp", ndma=8)
        self.uid = 0

    def new_sem(self, name):
        self.nsem += 1
        return self.es.enter_context(self.nc.semaphore(name))

    def sb(self, shape, dt, name=None, es=None):
        self.uid += 1
        nm = "%s_%d" % (name or "t", self.uid)
        return (es or self.es).enter_context(self.nc.sbuf_tensor(nm, list(shape), dt))

    def ps(self, shape, dt, name=None, es=None):
        self.uid += 1
        nm = "%s_%d" % (name or "p", self.uid)
        return (es or self.es).enter_context(self.nc.psum_tensor(nm, list(shape), dt))

    def dram(self, name, shape, dt, kind="Internal"):
        return self.nc.dram_tensor(name, list(shape), dt, kind=kind)

    def streams(self):
        return [self.pe, self.act, self.dve, self.pool, self.sp]

    def barrier(self):
        st = self.streams()
        toks = []
        for q in st:
            if q.cnt > 0:
                toks.append((q.sem, q.cnt))
            for i, sem in enumerate(q.dsems):
                if q.duses[i] > 0:
                    toks.append((sem, 16 * q.duses[i]))
        for s_ in st:
            for t in toks:
                s_.wait_tok(t)

    def finish(self):
        streams = [self.pe, self.act, self.dve, self.pool, self.sp]
        for s in streams:
            for q in streams:
                for i, sem in enumerate(q.dsems):
                    if q.duses[i] > 0:
                        s.wait_tok((sem, 16 * q.duses[i]))
        with self.nc.allow_non_contiguous_dma(reason="small strided param loads"), self.nc.Block() as block:
            @block.tensor
            def _(e):
                self.pe.replay(e)

            @block.scalar
            def _(e):
                self.act.replay(e)

            @block.vector
            def _(e):
                self.dve.replay(e)

            @block.gpsimd
            def _(e):
                self.pool.replay(e)

            @block.sync
            def _(e):
                self.sp.replay(e)


class Rot:
    def __init__(self, K, n, shape, dt, name, psum=False, es=None):
        self.t = [(K.ps if psum else K.sb)(shape, dt, name, es=es) for _ in range(n)]
        self.d = [Dep() for _ in range(n)]
        self.i = 0

    def next(self):
        j = self.i % len(self.t)
        self.i += 1
        return self.t[j], self.d[j]


def phase1(K, cfg, io, scr):
    nc = K.nc
    T = cfg["T"]
    NT = T // 512
    with ExitStack() as es:
        ident = K.sb([128, 128], BF16, "ident", es)
        d_ident = Dep()
        K.sp.dma(lambda e: e.dma_start(out=ident[:], in_=io["ident_bf"][:, :]), writes=[d_ident])
        nw = K.sb([128, 8], F32, "nw", es)
        d_nw = Dep()
        K.sp.dma(lambda e: e.dma_start(out=nw[:], in_=io["norm_mix_w"].rearrange("o (c p) -> p (o c)", p=128)),
                 writes=[d_nw])
        wt = K.sb([128, 8, IN_COLS], BF16, "wt", es)
        d_wt = Dep()
        wst = Rot(K, 2, [128, 1344], F32, "wst", es=es)
        q = 0
        for kc in range(8):
            for cp in range(4):
                st, dst = wst.next()
                eng = K.sp if q % 2 == 0 else K.pool
                q += 1
                eng.dma(lambda e, st=st, kc=kc, cp=cp: e.dma_start(
                    out=st[:], in_=io["w_in"][0, kc * 128:(kc + 1) * 128, cp * 1344:(cp + 1) * 1344]), writes=[dst])
                K.act.op(lambda e, st=st, kc=kc, cp=cp: e.activation(
                    out=wt[:, kc, cp * 1344:(cp + 1) * 1344], in_=st[:], func=AF.Copy, scale=nw[:, kc:kc + 1]),
                    reads=[dst, d_nw], writes=[d_wt])

        xs = Rot(K, 2, [128, D], F32, "xs", es=es)
        junk = K.sb([128, D], BF16, "junk", es)
        d_junk = Dep()
        xn = Rot(K, 2, [128, D], BF16, "xn", es=es)
        st4 = Rot(K, 4, [128, 4], F32, "st4", es=es)
        hT = Rot(K, 2, [128, 8, 512], BF16, "hT", es=es)
        ptr = Rot(K, 2, [128, 8, 128], BF16, "ptr", psum=True, es=es)
        pmm = Rot(K, 4, [128, 512], F32, "pmm", psum=True, es=es)
        o32 = Rot(K, 3, [128, 512], F32, "o32", es=es)
        o16 = Rot(K, 3, [128, 512], BF16, "o16", es=es)
        ev = [0]

        def evac(pt, pd, ncols, kind, dst_ap):
            if kind == "f32":
                ot, od = o32.next()
            else:
                ot, od = o16.next()
            use_act = (kind == "sig") or (ev[0] % 2 == 0)
            ev[0] += 1
            if kind == "sig":
                K.act.op(lambda e: e.activation(out=ot[:, :ncols], in_=pt[:, :ncols], func=AF.Sigmoid),
                         reads=[pd], writes=[od])
            elif use_act:
                K.act.op(lambda e: e.activation(out=ot[:, :ncols], in_=pt[:, :ncols], func=AF.Copy),
                         reads=[pd], writes=[od])
            else:
                K.dve.op(lambda e: e.tensor_copy(out=ot[:, :ncols], in_=pt[:, :ncols]), reads=[pd], writes=[od])
            K.sp.dma(lambda e: e.dma_start(out=dst_ap, in_=ot[:, :ncols]), reads=[od], adds=[scr["d_p1"]])

        for ti in range(NT):
            h_t, h_d = hT.next()
            for sub in range(4):
                t0 = ti * 512 + sub * 128
                x_t, x_d = xs.next()
                K.pool.dma(lambda e, x_t=x_t, t0=t0: e.dma_start(out=x_t[:], in_=io["x"][t0:t0 + 128, :]),
                           writes=[x_d])
                s_t, s_d = st4.next()
                K.dve.op(lambda e, s_t=s_t: e.memset(s_t[:], 0.0), writes=[s_d])
                K.act.op(lambda e, x_t=x_t, s_t=s_t: e.activation(out=junk[:], in_=x_t[:], func=AF.Square,
                                                                    accum_out=s_t[:, 0:1]),
                         reads=[x_d], writes=[d_junk, s_d])
                K.dve.op(lambda e, s_t=s_t: e.tensor_scalar(out=s_t[:, 1:2], in0=s_t[:, 0:1], scalar1=1.0 / D,
                                                            scalar2=1e-6, op0=ALU.mult, op1=ALU.add),
                         reads=[s_d], writes=[s_d])
                K.act.op(lambda e, s_t=s_t: e.activation(out=s_t[:, 3:4], in_=s_t[:, 1:2], func=AF.Sqrt),
                         reads=[s_d], writes=[s_d])
                K.dve.op(lambda e, s_t=s_t: e.reciprocal(out=s_t[:, 2:3], in_=s_t[:, 3:4]),
                         reads=[s_d], writes=[s_d])
                n_t, n_d = xn.next()
                K.dve.op(lambda e, x_t=x_t, s_t=s_t, n_t=n_t: e.tensor_scalar(
                    out=n_t[:], in0=x_t[:], scalar1=s_t[:, 2:3], scalar2=None, op0=ALU.mult),
                    reads=[x_d, s_d], writes=[n_d])
                p_t, p_d = ptr.next()
                for kc in range(8):
                    K.pe.op(lambda e, p_t=p_t, n_t=n_t, kc=kc: e.transpose(
                        out=p_t[:, kc, :], in_=n_t[:, kc * 128:(kc + 1) * 128], identity=ident[:]),
                        reads=[n_d, d_ident], writes=[p_d])
                K.act.op(lambda e, p_t=p_t, h_t=h_t, sub=sub: e.activation(
                    out=h_t[:, :, sub * 128:(sub + 1) * 128], in_=p_t[:], func=AF.Copy),
                    reads=[p_d], writes=[h_d])
            tsl = slice(ti * 512, (ti + 1) * 512)
            for sub in range(4):
                r0 = ti * 512 + sub * 128
                for (c0, ncols, kind, name, dc0) in [(0, 512, "f32", "zs_tm", 0), (512, 512, "f32", "zs_tm", 512),
                                                    (1024, 512, "f32", "zs_tm", 1024),
                                                    (1536, 256, "f32", "zs_tm", 1536),
                                                    (2816, 512, "bf16", "av_tm", 0)]:
                    pt, pd = pmm.next()
                    for kc in range(8):
                        K.pe.op(lambda e, pt=pt, kc=kc, sub=sub, c0=c0, ncols=ncols, h_t=h_t: e.matmul(
                            pt[:, :ncols], h_t[:, kc, sub * 128:(sub + 1) * 128], wt[:, kc, c0:c0 + ncols],
                            start=(kc == 0), stop=(kc == 7)), reads=[h_d, d_wt], writes=[pd])
                    evac(pt, pd, ncols, kind, scr[name][r0:r0 + 128, dc0:dc0 + ncols])
            fm = []
            for j in range(8):
                fm.append((1792 + j * 128, "bf16", "qk_fm", j * 128))
            for j in range(16):
                fm.append((3328 + j * 128, "sig", "sg_fm", j * 128))
            for (c0, kind, name, r0) in fm:
                pt, pd = pmm.next()
                for kc in range(8):
                    K.pe.op(lambda e, pt=pt, kc=kc, c0=c0, h_t=h_t: e.matmul(
                        pt[:, :], wt[:, kc, c0:c0 + 128], h_t[:, kc, :], start=(kc == 0), stop=(kc == 7)),
                        reads=[h_d, d_wt], writes=[pd])
                evac(pt, pd, 512, kind, scr[name][r0:r0 + 128, tsl])


def dap(apobj, offset, dims):
    return bass.AP(tensor=apobj.tensor, offset=offset, ap=[list(d) for d in dims])


def bcast_load(K, eng, dst, src_row_ap, n, dep):
    eng.dma(lambda e: e.dma_start(out=dst, in_=src_row_ap.broadcast_to([128, n])), writes=[dep])


def phase2_prep(K, cfg, io, scr):
    T, S, NB = cfg["T"], cfg["S"], cfg["NB"]
    NTT = T // 128
    with ExitStack() as es:
        identb = K.sb([128, 128], BF16, "identb", es)
        identf = K.sb([128, 128], F32, "identf", es)
        d_c = Dep()
        K.sp.dma(lambda e: e.dma_start(out=identb[:], in_=io["ident_bf"][:, :]), adds=[d_c])
        K.sp.dma(lambda e: e.dma_start(out=identf[:], in_=io["ident_f"][:, :]), adds=[d_c])
        MU = K.sb([128, SHIFT_COLS], F32, "MU", es)
        PR = K.sb([128, 5, 512], F32, "PR", es)
        K.sp.dma(lambda e: e.dma_start(out=MU[:], in_=io["shift_mu"][0:1, :].broadcast_to([128, SHIFT_COLS])), adds=[d_c])
        for j, nm in enumerate(["w0", "a0", "k_k", "k_a"]):
            K.pool.dma(lambda e, j=j, nm=nm: e.dma_start(out=PR[:, j, :], in_=io[nm][0:1, :].broadcast_to([128, 512])),
                       adds=[d_c])
        K.pool.dma(lambda e: e.dma_start(out=PR[:, 4, :], in_=io["r_k"].rearrange("o h k -> o (h k)").broadcast_to([128, 512])),
                   adds=[d_c])
        cst = K.sb([128, 2], F32, "cst", es)
        K.dve.op(lambda e: e.memset(cst[:, 0:1], 1.0), adds=[d_c])
        K.dve.op(lambda e: e.memset(cst[:, 1:2], -0.5), adds=[d_c])
        wst = K.sb([128, 3, 512], F32, "lwst", es)
        d_wst = Dep()
        K.dve.op(lambda e: e.memset(wst[:], 0.0), writes=[d_wst])
        K.sp.dma(lambda e: e.dma_start(out=wst[0:64, 0, :], in_=io["w2"][0, :, :]), reads=[d_wst], adds=[d_wst])
        K.sp.dma(lambda e: e.dma_start(out=wst[64:128, 1, :], in_=io["a2"][0, :, :]), reads=[d_wst], adds=[d_wst])
        K.sp.dma(lambda e: e.dma_start(out=wst[:, 2, :], in_=io["g2"][0, :, :]), reads=[d_wst], adds=[d_wst])
        LW = K.sb([128, 3, 512], BF16, "LW", es)
        K.dve.op(lambda e: e.tensor_copy(out=LW[:], in_=wst[:]), reads=[d_wst], adds=[d_c])

        Zr = Rot(K, 2, [128, SHIFT_COLS], F32, "Z", es=es)
        Zpr = Rot(K, 2, [128, SHIFT_COLS], F32, "Zp", es=es)
        ZSr = Rot(K, 2, [128, SHIFT_COLS], F32, "ZS", es=es)
        OUTr = Rot(K, 2, [128, 5, 512], F32, "OUT", es=es)
        Er = Rot(K, 2, [128, 192], F32, "E", es=es)
        Lr = Rot(K, 2, [128, 256], BF16, "L", es=es)
        LTr = Rot(K, 2, [128, 2, 128], BF16, "LT", es=es)
        Ur = Rot(K, 2, [128, 512], F32, "U", es=es)
        UAr = Rot(K, 2, [128, 512], F32, "UA", es=es)
        KKr = Rot(K, 2, [128, 512], F32, "KKt", es=es)
        SQr = Rot(K, 2, [128, 512], F32, "SQ", es=es)
        T1r = Rot(K, 2, [128, 512], F32, "T1", es=es)
        T2r = Rot(K, 2, [128, 512], F32, "T2", es=es)
        S8r = Rot(K, 2, [128, 4, 8], F32, "S8", es=es)
        VTr = Rot(K, 2, [128, 4, 128], F32, "VT", es=es)
        GTr = Rot(K, 2, [128, 4, 128], F32, "GT", es=es)
        CTr = Rot(K, 2, [8, 128], F32, "CT", es=es)
        PT = Rot(K, 1, [128, 2, 128], BF16, "PT", psum=True, es=es)
        PW = Rot(K, 1, [128, 512], F32, "PW", psum=True, es=es)
        PA = Rot(K, 1, [128, 512], F32, "PA", psum=True, es=es)
        PG = Rot(K, 1, [128, 4, 128], F32, "PG", psum=True, es=es)
        PV = Rot(K, 1, [128, 4, 128], F32, "PV", psum=True, es=es)
        PC = Rot(K, 1, [8, 128], F32, "PC", psum=True, es=es)
        chunked = cfg.get("chunked", True)
        if chunked:
            PL = Rot(K, 1, [128, 512], F32, "PL", psum=True, es=es)
            TRI = K.sb([128, 128], F32, "TRI", es)
            K.sp.dma(lambda e: e.dma_start(out=TRI[:], in_=io["tri"][:, :]), adds=[d_c])
            LWr = Rot(K, 2, [128, 512], F32, "LWt", es=es)
            ELr = Rot(K, 2, [128, 3, 512], F32, "EL", es=es)
            ABr = Rot(K, 2, [128, 5, 512], BF16, "AB", es=es)
        dq = [0]

        def ldq():
            dq[0] += 1
            return K.sp if dq[0] % 2 == 0 else K.pool

        def tile_gen(ti):
            t0 = ti * 128
            first = (t0 % S == 0)
            Z, dZ = Zr.next()
            Zp, dZp = Zpr.next()
            ZS, dZS = ZSr.next()
            OUT, dO = OUTr.next()
            ldq().dma(lambda e, Z=Z, t0=t0: e.dma_start(out=Z[:], in_=scr["zs_tm"][t0:t0 + 128, :]),
                      reads=[scr["d_p1"]], writes=[dZ])
            if first:
                K.pool.op(lambda e, Zp=Zp: e.memset(Zp[0:32, :], 0.0), writes=[dZp])
                ldq().dma(lambda e, Zp=Zp, t0=t0: e.dma_start(out=Zp[1:128, :], in_=scr["zs_tm"][t0:t0 + 127, :]),
                          reads=[scr["d_p1"], dZp], adds=[dZp])
            else:
                ldq().dma(lambda e, Zp=Zp, t0=t0: e.dma_start(out=Zp[:], in_=scr["zs_tm"][t0 - 1:t0 + 127, :]),
                          reads=[scr["d_p1"]], writes=[dZp])
            CS = 1216
            dZSa, dZSb = Dep(), Dep()
            K.dve.op(lambda e, Z=Z, Zp=Zp, ZS=ZS: e.tensor_tensor(out=ZS[:, :CS], in0=Zp[:, :CS], in1=Z[:, :CS], op=ALU.subtract),
                     reads=[dZ, dZp], writes=[dZS])
            K.pool.op(lambda e, Z=Z, Zp=Zp, ZS=ZS: e.tensor_tensor(out=ZS[:, CS:], in0=Zp[:, CS:], in1=Z[:, CS:], op=ALU.subtract),
                      reads=[dZ, dZp, dZS], writes=[dZSb])
            K.dve.op(lambda e, ZS=ZS: e.tensor_tensor(out=ZS[:, :CS], in0=ZS[:, :CS], in1=MU[:, :CS], op=ALU.mult),
                     reads=[d_c, dZS], writes=[dZSa])
            K.pool.op(lambda e, ZS=ZS: e.tensor_tensor(out=ZS[:, CS:], in0=ZS[:, CS:], in1=MU[:, CS:], op=ALU.mult),
                      reads=[d_c], writes=[dZSb])
            K.dve.op(lambda e, Z=Z, ZS=ZS: e.tensor_tensor(out=ZS[:, :CS], in0=ZS[:, :CS], in1=Z[:, :CS], op=ALU.add),
                     reads=[dZ], writes=[dZSa])
            K.pool.op(lambda e, Z=Z, ZS=ZS: e.tensor_tensor(out=ZS[:, CS:], in0=ZS[:, CS:], in1=Z[:, CS:], op=ALU.add),
                      reads=[dZ], writes=[dZSb])
            K.dve.op(lambda e, ZS=ZS: e.tensor_copy(out=ZS[:, 0:1], in_=ZS[:, 0:1]), reads=[dZSa, dZSb], writes=[dZS])
            r_ap = ZS[:, 0:512]
            k_ap = ZS[:, 512:1024]
            K.act.op(lambda e, OUT=OUT, ZS=ZS: e.activation(out=OUT[:, 4, :], in_=ZS[:, 0:512], func=AF.Copy),
                     reads=[dZS], writes=[dO])
            yield
            E, dE = Er.next()
            L, dL = Lr.next()
            K.act.op(lambda e, E=E, ZS=ZS: e.activation(out=E[:, 0:64], in_=ZS[:, 1536:1600], func=AF.Exp, scale=-2.0),
                     reads=[dZS], writes=[dE])
            K.act.op(lambda e, E=E, ZS=ZS: e.activation(out=E[:, 64:192], in_=ZS[:, 1664:1792], func=AF.Exp, scale=-1.0),
                     reads=[dZS], adds=[dE])
            K.act.op(lambda e, E=E: e.activation(out=E[:], in_=E[:], func=AF.Ln, bias=cst[:, 0:1]), reads=[d_c], writes=[dE])
            K.act.op(lambda e, E=E: e.activation(out=E[:], in_=E[:], func=AF.Exp, scale=-1.0), writes=[dE])
            K.dve.op(lambda e, E=E, L=L: e.tensor_scalar(out=L[:, 0:64], in0=E[:, 0:64], scalar1=2.0, scalar2=-1.0,
                                                        op0=ALU.mult, op1=ALU.add), reads=[dE], writes=[dL])
            K.act.op(lambda e, L=L, ZS=ZS: e.activation(out=L[:, 64:128], in_=ZS[:, 1600:1664], func=AF.Copy),
                     reads=[dZS, dL], adds=[dL])
            K.act.op(lambda e, L=L, E=E: e.activation(out=L[:, 128:256], in_=E[:, 64:192], func=AF.Copy),
                     reads=[dE, dL], adds=[dL])
            yield
            pt, dpt = PT.next()
            K.pe.op(lambda e, pt=pt, L=L: e.transpose(out=pt[:, 0, :], in_=L[:, 0:128], identity=identb[:]),
                    reads=[dL, d_c], writes=[dpt])
            K.pe.op(lambda e, pt=pt, L=L: e.transpose(out=pt[:, 1, :], in_=L[:, 128:256], identity=identb[:]),
                    reads=[dL, d_c], adds=[dpt])
            LT, dLT = LTr.next()
            K.act.op(lambda e, pt=pt, LT=LT: e.activation(out=LT[:], in_=pt[:], func=AF.Copy), reads=[dpt], writes=[dLT])
            yield
            pw, dpw = PW.next()
            pa, dpa = PA.next()
            pg, dpg = PG.next()
            K.pe.op(lambda e, pw=pw, LT=LT: e.matmul(pw[:], LT[:, 0, :], LW[:, 0, :], start=True, stop=True),
                    reads=[dLT, d_c], writes=[dpw])
            K.pe.op(lambda e, pa=pa, LT=LT: e.matmul(pa[:], LT[:, 0, :], LW[:, 1, :], start=True, stop=True),
                    reads=[dLT, d_c], writes=[dpa])
            for j in range(4):
                K.pe.op(lambda e, pg=pg, LT=LT, j=j: e.matmul(pg[:, j, :], LW[:, 2, j * 128:(j + 1) * 128], LT[:, 1, :],
                                                             start=True, stop=True),
                        reads=[dLT, d_c], writes=[dpg] if j == 0 else [], adds=[] if j == 0 else [dpg])
            GT, dGT = GTr.next()
            K.act.op(lambda e, pg=pg, GT=GT: e.activation(out=GT[:], in_=pg[:], func=AF.Copy), reads=[dpg], writes=[dGT])
            K.sp.dma(lambda e, GT=GT, t0=t0: e.dma_start(
                out=dap(scr["g_fm"], t0, [[T, 128], [128 * T, 4], [1, 128]]), in_=GT[:]),
                reads=[dGT], adds=[scr["d_p2a"]])
            yield
            U, dU = Ur.next()
            K.dve.op(lambda e, U=U, pw=pw: e.tensor_tensor(out=U[:], in0=pw[:], in1=PR[:, 0, :], op=ALU.add),
                     reads=[dpw, d_c], writes=[dU])
            K.act.op(lambda e, U=U: e.activation(out=U[:], in_=U[:], func=AF.Exp, scale=-1.0), writes=[dU])
            K.act.op(lambda e, U=U: e.activation(out=U[:], in_=U[:], func=AF.Ln, bias=cst[:, 0:1]), reads=[d_c], writes=[dU])
            K.act.op(lambda e, U=U: e.activation(out=U[:], in_=U[:], func=AF.Exp, scale=-1.0, bias=cst[:, 1:2]),
                     reads=[d_c], writes=[dU])
            K.act.op(lambda e, U=U, OUT=OUT: e.activation(out=OUT[:, 0, :], in_=U[:], func=AF.Exp, scale=-1.0),
                     reads=[dU], adds=[dO])
            yield
            UA, dUA = UAr.next()
            K.dve.op(lambda e, UA=UA, pa=pa: e.tensor_tensor(out=UA[:], in0=pa[:], in1=PR[:, 1, :], op=ALU.add),
                     reads=[dpa, d_c], writes=[dUA])
            K.act.op(lambda e, UA=UA: e.activation(out=UA[:], in_=UA[:], func=AF.Exp, scale=-1.0), writes=[dUA])
            K.act.op(lambda e, UA=UA: e.activation(out=UA[:], in_=UA[:], func=AF.Ln, bias=cst[:, 0:1]), reads=[d_c], writes=[dUA])
            K.act.op(lambda e, UA=UA: e.activation(out=UA[:], in_=UA[:], func=AF.Exp, scale=-1.0), writes=[dUA])
            yield
            KKt, dKK = KKr.next()
            SQ, dSQ = SQr.next()
            S8, dS8 = S8r.next()
            K.dve.op(lambda e, KKt=KKt, ZS=ZS: e.tensor_tensor(out=KKt[:], in0=ZS[:, 512:1024], in1=PR[:, 2, :], op=ALU.mult),
                     reads=[dZS, d_c], writes=[dKK])
            K.pool.op(lambda e, KKt=KKt, SQ=SQ: e.tensor_tensor(out=SQ[:], in0=KKt[:], in1=KKt[:], op=ALU.mult),
                      reads=[dKK], writes=[dSQ])
            K.dve.op(lambda e, SQ=SQ, S8=S8: e.tensor_reduce(out=S8[:, 0, :], in_=SQ[:].rearrange("p (h k) -> p h k", k=64),
                                                            axis=AX.X, op=ALU.add), reads=[dSQ], writes=[dS8])
            K.dve.op(lambda e, S8=S8: e.tensor_scalar(out=S8[:, 0, :], in0=S8[:, 0, :], scalar1=1e-24, scalar2=None,
                                                      op0=ALU.max), writes=[dS8])
            K.act.op(lambda e, S8=S8: e.activation(out=S8[:, 1, :], in_=S8[:, 0, :], func=AF.Ln), writes=[dS8])
            K.act.op(lambda e, S8=S8: e.activation(out=S8[:, 2, :], in_=S8[:, 1, :], func=AF.Exp, scale=-0.5), writes=[dS8])
            K.dve.op(lambda e, KKt=KKt, S8=S8, OUT=OUT: e.tensor_tensor(
                out=OUT[:, 1, :].rearrange("p (h k) -> p h k", k=64), in0=KKt[:].rearrange("p (h k) -> p h k", k=64),
                in1=S8[:, 2, :].unsqueeze(2).broadcast_to([128, 8, 64]), op=ALU.mult),
                reads=[dKK, dS8, dO], adds=[dO])
            K.dve.op(lambda e, OUT=OUT, UA=UA: e.scalar_tensor_tensor(
                out=OUT[:, 2, :], in0=OUT[:, 1, :], scalar=-1.0, in1=UA[:], op0=ALU.mult, op1=ALU.mult),
                reads=[dUA, dO], adds=[dO])
            yield
            T1, dT1 = T1r.next()
            K.dve.op(lambda e, T1=T1, UA=UA: e.scalar_tensor_tensor(
                out=T1[:], in0=UA[:], scalar=-1.0, in1=PR[:, 3, :], op0=ALU.add, op1=ALU.mult),
                reads=[dUA, d_c], writes=[dT1])
            K.dve.op(lambda e, T1=T1, OUT=OUT, ZS=ZS: e.scalar_tensor_tensor(
                out=OUT[:, 3, :], in0=T1[:], scalar=1.0, in1=ZS[:, 512:1024], op0=ALU.add, op1=ALU.mult),
                reads=[dT1, dZS, dO], adds=[dO])
            T2, dT2 = T2r.next()
            K.pool.op(lambda e, T2=T2, OUT=OUT, ZS=ZS: e.tensor_tensor(out=T2[:], in0=OUT[:, 3, :], in1=ZS[:, 0:512], op=ALU.mult),
                      reads=[dO, dZS], writes=[dT2])
            K.pool.op(lambda e, T2=T2: e.tensor_tensor(out=T2[:], in0=T2[:], in1=PR[:, 4, :], op=ALU.mult),
                      reads=[d_c], writes=[dT2])
            K.dve.op(lambda e, T2=T2, S8=S8: e.tensor_reduce(out=S8[:, 3, :], in_=T2[:].rearrange("p (h k) -> p h k", k=64),
                                                            axis=AX.X, op=ALU.add), reads=[dT2], writes=[dS8])
            yield
            pv, dpv = PV.next()
            pc, dpc = PC.next()
            for j in range(4):
                K.pe.op(lambda e, pv=pv, ZS=ZS, j=j: e.transpose(out=pv[:, j, :], in_=ZS[:, 1024 + j * 128:1024 + (j + 1) * 128],
                                                                identity=identf[:]),
                        reads=[dZS, d_c], writes=[dpv] if j == 0 else [], adds=[] if j == 0 else [dpv])
            K.pe.op(lambda e, pc=pc, S8=S8: e.transpose(out=pc[:, :], in_=S8[:, 3, :], identity=identf[:]),
                    reads=[dS8, d_c], writes=[dpc])
            VT, dVT = VTr.next()
            CT, dCT = CTr.next()
            K.act.op(lambda e, pv=pv, VT=VT: e.activation(out=VT[:], in_=pv[:], func=AF.Copy), reads=[dpv], writes=[dVT])
            K.act.op(lambda e, pc=pc, CT=CT: e.activation(out=CT[:], in_=pc[:], func=AF.Copy), reads=[dpc], writes=[dCT])
            K.sp.dma(lambda e, VT=VT, t0=t0: e.dma_start(
                out=dap(scr["v_fm"], t0, [[T, 128], [128 * T, 4], [1, 128]]), in_=VT[:]),
                reads=[dVT], adds=[scr["d_p2a"]])
            K.sp.dma(lambda e, CT=CT, t0=t0: e.dma_start(out=scr["coef_fm"][:, t0:t0 + 128], in_=CT[:]),
                     reads=[dCT], adds=[scr["d_p2a"]])
            yield
            if not chunked:
                K.pool.dma(lambda e, OUT=OUT, t0=t0: e.dma_start(out=scr["rw_tm"][t0:t0 + 128, :, :], in_=OUT[:]),
                           reads=[dO], adds=[scr["d_p2a"]])
            else:
                LWt, dLW = LWr.next()
                K.dve.op(lambda e, LWt=LWt, U=U: e.tensor_scalar(out=LWt[:], in0=U[:], scalar1=-1.0, scalar2=None, op0=ALU.mult),
                         reads=[dU], writes=[dLW])
                pl, dpl = PL.next()
                K.pe.op(lambda e, pl=pl, LWt=LWt: e.matmul(pl[:], TRI[:], LWt[:], start=True, stop=True),
                        reads=[dLW, d_c], writes=[dpl])
                EL, dEL = ELr.next()
                K.act.op(lambda e, EL=EL, pl=pl: e.activation(out=EL[:, 0, :], in_=pl[:], func=AF.Exp), reads=[dpl], writes=[dEL])
                K.act.op(lambda e, EL=EL, pl=pl: e.activation(out=EL[:, 1, :], in_=pl[:], func=AF.Exp, scale=-1.0),
                         reads=[dpl], adds=[dEL])
                K.dve.op(lambda e, EL=EL, pl=pl, U=U: e.tensor_tensor(out=EL[:, 2, :], in0=pl[:], in1=U[:], op=ALU.add),
                         reads=[dpl, dU, dEL], adds=[dEL])
                K.act.op(lambda e, EL=EL: e.activation(out=EL[:, 2, :], in_=EL[:, 2, :], func=AF.Exp), reads=[dEL], adds=[dEL])
                AB, dAB = ABr.next()
                K.dve.op(lambda e, AB=AB, OUT=OUT, EL=EL: e.tensor_tensor(out=AB[:, 0, :], in0=OUT[:, 1, :], in1=EL[:, 2, :], op=ALU.mult),
                         reads=[dO, dEL], writes=[dAB])
                K.dve.op(lambda e, AB=AB, OUT=OUT, EL=EL: e.scalar_tensor_tensor(
                    out=AB[:, 1, :], in0=OUT[:, 2, :], scalar=-1.0, in1=EL[:, 1, :], op0=ALU.mult, op1=ALU.mult),
                    reads=[dO, dEL, dAB], adds=[dAB])
                K.pool.op(lambda e, AB=AB, OUT=OUT, EL=EL: e.tensor_tensor(out=AB[:, 2, :], in0=OUT[:, 3, :], in1=EL[:, 1, :], op=ALU.mult),
                          reads=[dO, dEL, dAB], adds=[dAB])
                K.pool.op(lambda e, AB=AB, OUT=OUT, EL=EL: e.tensor_tensor(out=AB[:, 3, :], in0=OUT[:, 4, :], in1=EL[:, 0, :], op=ALU.mult),
                          reads=[dO, dEL, dAB], adds=[dAB])
                K.act.op(lambda e, AB=AB, ZS=ZS: e.activation(out=AB[:, 4, :], in_=ZS[:, 1024:1536], func=AF.Copy),
                         reads=[dZS, dAB], adds=[dAB])
                K.pool.dma(lambda e, AB=AB, t0=t0: e.dma_start(out=scr["ab_tm"][t0:t0 + 128, :, :], in_=AB[:]),
                           reads=[dAB], adds=[scr["d_p2a"]])
                K.sp.dma(lambda e, LWt=LWt, t0=t0: e.dma_start(out=scr["lw_tm"][t0:t0 + 128, :], in_=LWt[:]),
                         reads=[dLW], adds=[scr["d_p2a"]])

        LAG = cfg.get("prep_lag", 3)
        active = []
        nxt = 0
        while nxt < NTT or active:
            if len(active) < 2 and nxt < NTT and (not active or active[0][1] >= LAG):
                active.append([tile_gen(nxt), 0])
                nxt += 1
            for a in list(active):
                try:
                    next(a[0])
                    a[1] += 1
                except StopIteration:
                    active.remove(a)


def phase2_scan(K, cfg, io, scr):
    T, S, NB = cfg["T"], cfg["S"], cfg["NB"]
    NBH = 2 if NB >= 2 else 1
    NBL = NB // NBH
    NP = 64 * NBH
    TS = 2
    TC = 128
    RW = 2560
    with ExitStack() as es:
        St = K.sb([128, NBL, 8, 64], F32, "St", es)
        dS = Dep()
        TMP = K.sb([128, NBL, 8, 64], F32, "TMP", es)
        dT = Dep()
        SA = K.sb([128, NBL, 8], F32, "SA", es)
        dSA = Dep()
        T2r = Rot(K, 2, [128, NBL, 8, 64], F32, "TMP2", es=es)
        T3r = Rot(K, 2, [128, NBL, 8, 64], F32, "TMP3", es=es)
        BCr = Rot(K, 3, [128, TS, NBL, 5, 8, 64], F32, "BC", es=es)
        Vr = Rot(K, 2, [128, NBL, 8, TC], F32, "Vf", es=es)
        Yr = Rot(K, 2, [128, NBL, 8, TC], F32, "Yf", es=es)
        K.dve.op(lambda e: e.memset(St[:], 0.0), writes=[dS])
        qi = [0]

        def q():
            qi[0] += 1
            return K.sp if qi[0] % 2 == 0 else K.act

        def load_bc(ci):
            t = ci * TS
            BC, dBC = BCr.next()
            first = True
            for bhi in range(NBH):
                for blo in range(NBL):
                    src = dap(scr["rw_tm"], ((bhi * NBL + blo) * S + t) * RW, [[0, 64], [RW, TS], [1, RW]])
                    dst = BC[bhi * 64:(bhi + 1) * 64, :, blo].rearrange("p t j h k -> p t (j h k)")
                    q().dma(lambda e, src=src, dst=dst: e.dma_start(out=dst, in_=src), reads=[scr["d_p2a"]],
                            writes=[dBC] if first else [], adds=[] if first else [dBC])
                    first = False
            return BC, dBC

        def vy_ap(name, bhi, blo, t):
            return dap(scr[name], (bhi * NBL + blo) * S + t, [[T, 64], [64 * T, 8], [1, TC]])

        def load_v(ni):
            Vf, dV = Vr.next()
            first = True
            for bhi in range(NBH):
                for blo in range(NBL):
                    src = vy_ap("v_fm", bhi, blo, ni * TC)
                    dst = Vf[bhi * 64:(bhi + 1) * 64, blo]
                    q().dma(lambda e, src=src, dst=dst: e.dma_start(out=dst, in_=src), reads=[scr["d_p2a"]],
                            writes=[dV] if first else [], adds=[] if first else [dV])
                    first = False
            return Vf, dV

        nch = S // TS
        bcs = {}
        bcs[0] = load_bc(0)
        if nch > 1:
            bcs[1] = load_bc(1)
        vs = {0: load_v(0)}
        P = slice(0, NP)
        for t in range(S):
            ci, ts = divmod(t, TS)
            ni, tt = divmod(t, TC)
            if ts == 0 and ci + 2 < nch:
                bcs[ci + 2] = load_bc(ci + 2)
            if tt == 0:
                if (ni + 1) * TC < S:
                    vs[ni + 1] = load_v(ni + 1)
                Yf, dY = Yr.next()
            BC, dBC = bcs[ci]
            Vf, dV = vs[ni]
            W_ = BC[P, ts, :, 0]
            KN = BC[P, ts, :, 1]
            KA = BC[P, ts, :, 2]
            KP = BC[P, ts, :, 3]
            R_ = BC[P, ts, :, 4]
            shp = [NP, NBL, 8, 64]
            K.dve.op(lambda e, KN=KN: e.tensor_tensor(out=TMP[P], in0=St[P], in1=KN, op=ALU.mult),
                     reads=[dS, dBC], writes=[dT])
            K.dve.op(lambda e: e.tensor_reduce(out=SA[P], in_=TMP[P], axis=AX.X, op=ALU.add), reads=[dT], writes=[dSA])
            K.dve.op(lambda e, W_=W_: e.tensor_tensor(out=St[P], in0=St[P], in1=W_, op=ALU.mult),
                     reads=[dBC], writes=[dS])
            K.dve.op(lambda e, KA=KA: e.tensor_tensor(out=TMP[P], in0=KA, in1=SA[P].unsqueeze(3).broadcast_to(shp),
                                                     op=ALU.mult), reads=[dBC, dSA], writes=[dT])
            K.dve.op(lambda e: e.tensor_tensor(out=St[P], in0=St[P], in1=TMP[P], op=ALU.add), reads=[dT], writes=[dS])
            T2, dT2 = T2r.next()
            K.pool.op(lambda e, KP=KP, T2=T2, Vf=Vf, tt=tt: e.tensor_tensor(
                out=T2[P], in0=KP, in1=Vf[P, :, :, tt:tt + 1].broadcast_to(shp), op=ALU.mult),
                reads=[dBC, dV], writes=[dT2])
            K.dve.op(lambda e, T2=T2: e.tensor_tensor(out=St[P], in0=St[P], in1=T2[P], op=ALU.add),
                     reads=[dT2], writes=[dS])
            T3, dT3 = T3r.next()
            K.pool.op(lambda e, T3=T3, R_=R_: e.tensor_tensor(out=T3[P], in0=St[P], in1=R_, op=ALU.mult),
                      reads=[dS, dBC], writes=[dT3])
            K.dve.op(lambda e, T3=T3, Yf=Yf, tt=tt: e.tensor_reduce(out=Yf[P, :, :, tt], in_=T3[P], axis=AX.X, op=ALU.add),
                      reads=[dT3], writes=[dY] if tt == 0 else [], adds=[] if tt == 0 else [dY])
            if tt == TC - 1:
                for bhi in range(NBH):
                    for blo in range(NBL):
                        dst = vy_ap("y_fm", bhi, blo, ni * TC)
                        srcp = Yf[bhi * 64:(bhi + 1) * 64, blo]
                        K.sp.dma(lambda e, dst=dst, srcp=srcp: e.dma_start(out=dst, in_=srcp), reads=[dY],
                                 adds=[scr["d_p2b"]])


def phase2_chunk(K, cfg, io, scr):
    T, S, NB = cfg["T"], cfg["S"], cfg["NB"]
    C = 64
    NCH = S // C
    with ExitStack() as es:
        d_c = Dep()
        id64 = K.sb([64, 64], BF16, "c_id64", es)
        K.sp.dma(lambda e: e.dma_start(out=id64[:], in_=io["ident64"][:, :]), adds=[d_c])
        MK = K.sb([64, 3, 64], F32, "c_MK", es)
        K.sp.dma(lambda e: e.dma_start(out=MK[:], in_=io["masks"][:, :, :]), adds=[d_c])
        ONES = K.sb([64, 1], F32, "c_ones", es)
        K.sp.dma(lambda e: e.dma_start(out=ONES[:], in_=io["ones64"][:, :]), adds=[d_c])
        IDF = K.sb([64, 8, 64], F32, "c_IDF", es)
        K.sp.dma(lambda e: e.dma_start(out=IDF[:], in_=io["ident_f"][0:64, 0:64].unsqueeze(1).broadcast_to([64, 8, 64])), adds=[d_c])
        ST = [K.sb([64, 8, 64], F32, "c_S%d" % b, es) for b in range(NB)]
        STb = [K.sb([64, 8, 64], BF16, "c_Sb%d" % b, es) for b in range(NB)]
        dST = [Dep() for _ in range(NB)]
        dSTb = [Dep() for _ in range(NB)]
        for b in range(NB):
            K.dve.op(lambda e, b=b: e.memset(ST[b][:], 0.0), writes=[dST[b]])
            K.pool.op(lambda e, b=b: e.memset(STb[b][:], 0.0), writes=[dSTb[b]])
        TMr = Rot(K, 3, [64, 5, 512], BF16, "c_TM", es=es)
        LWr = Rot(K, 3, [64, 512], F32, "c_LW", es=es)
        FMr = Rot(K, 2, [64, 4, 8, 64], BF16, "c_FM", es=es)
        PCr = Rot(K, 2, [64, 8], F32, "c_PC", es=es)
        Nr = Rot(K, 3, [64, 8, 64], BF16, "c_N", es=es)
        NTr = Rot(K, 3, [64, 8, 64], BF16, "c_NT", es=es)
        MTr = Rot(K, 3, [64, 8, 64], BF16, "c_MT", es=es)
        MTfr = Rot(K, 2, [64, 8, 64], F32, "c_MTf", es=es)
        NAKr = Rot(K, 2, [64, 8, 64], BF16, "c_NAK", es=es)
        MRBr = Rot(K, 2, [64, 8, 64], BF16, "c_MRB", es=es)
        MRKr = Rot(K, 2, [64, 8, 64], BF16, "c_MRK", es=es)
        Xr = Rot(K, 2, [64, 8, 64], BF16, "c_X", es=es)
        NUr = Rot(K, 2, [64, 8, 64], BF16, "c_NU", es=es)
        Yr = Rot(K, 2, [64, 8, 64], F32, "c_Y", es=es)
        TSr = Rot(K, 2, [64, 8, 64], F32, "c_TS", es=es)
        PTf = Rot(K, 1, [64, 4, 8, 64], BF16, "c_PTf", psum=True, es=es)
        PA = Rot(K, 4, [64, 8, 64], F32, "c_PA", psum=True, es=es)
        PPC = Rot(K, 1, [64, 8], F32, "c_PPC", psum=True, es=es)
        ce = [0]

        def evac_copy(dst_ap, src_ap, reads, writes=(), adds=(), scale=None):
            ce[0] += 1
            if scale is not None or ce[0] % 2 == 0:
                if scale is None:
                    K.act.op(lambda e: e.activation(out=dst_ap, in_=src_ap, func=AF.Copy), reads=reads, writes=writes, adds=adds)
                else:
                    K.act.op(lambda e: e.activation(out=dst_ap, in_=src_ap, func=AF.Copy, scale=scale), reads=reads, writes=writes, adds=adds)
            else:
                K.dve.op(lambda e: e.tensor_copy(out=dst_ap, in_=src_ap), reads=reads, writes=writes, adds=adds)

        def mm8(pt, dpt, lhs_fn, rhs_fn, reads, first=True, last=True, wr=True):
            mmN(pt, dpt, [(lhs_fn, rhs_fn)], reads)

        def mmN(pt, dpt, terms, reads):
            n = len(terms)
            for h in range(8):
                for i, (lf, rf) in enumerate(terms):
                    K.pe.op(lambda e, h=h, lf=lf, rf=rf, i=i: e.matmul(pt[:, h, :], lf(h), rf(h), start=(i == 0), stop=(i == n - 1)),
                            reads=reads, writes=[dpt] if (h == 0 and i == 0) else [], adds=[] if (h == 0 and i == 0) else [dpt])

        q = [0]

        def dq():
            q[0] += 1
            return K.sp if q[0] % 2 == 0 else K.pool

        for ci in range(NCH):
            for b in range(NB):
                t0 = b * S + ci * C
                TM, dTM = TMr.next()
                LW, dLW = LWr.next()
                dq().dma(lambda e, TM=TM, t0=t0: e.dma_start(out=TM[:], in_=scr["ab_tm"][t0:t0 + C, :, :]),
                         reads=[scr["d_p2a"]], writes=[dTM])
                dq().dma(lambda e, LW=LW, t0=t0: e.dma_start(out=LW[:], in_=scr["lw_tm"][t0:t0 + C, :]),
                         reads=[scr["d_p2a"]], writes=[dLW])
                ptf, dptf = PTf.next()
                first = True
                for j in range(4):
                    for h in range(8):
                        K.pe.op(lambda e, ptf=ptf, TM=TM, j=j, h=h: e.transpose(
                            out=ptf[:, j, h, :], in_=TM[:, j, h * 64:(h + 1) * 64], identity=id64[:]),
                            reads=[dTM, d_c], writes=[dptf] if first else [], adds=[] if first else [dptf])
                        first = False
                FM, dFM = FMr.next()
                K.act.op(lambda e, FM=FM, ptf=ptf: e.activation(out=FM[:, 0:2], in_=ptf[:, 0:2], func=AF.Copy), reads=[dptf], writes=[dFM])
                K.dve.op(lambda e, FM=FM, ptf=ptf: e.tensor_copy(out=FM[:, 2:4], in_=ptf[:, 2:4]), reads=[dptf, dFM], adds=[dFM])
                Af = lambda h, FM=FM: FM[:, 0, h, :]
                Bf = lambda h, FM=FM: FM[:, 1, h, :]
                Kf = lambda h, FM=FM: FM[:, 2, h, :]
                Rf = lambda h, FM=FM: FM[:, 3, h, :]
                Vt = lambda h, TM=TM: TM[:, 4, h * 64:(h + 1) * 64]
                Bt = lambda h, TM=TM: TM[:, 1, h * 64:(h + 1) * 64]
                Kt = lambda h, TM=TM: TM[:, 2, h * 64:(h + 1) * 64]
                ppc, dppc = PPC.next()
                for h in range(8):
                    K.pe.op(lambda e, ppc=ppc, LW=LW, h=h: e.matmul(ppc[:, h:h + 1], LW[:, h * 64:(h + 1) * 64], ONES[:], start=True, stop=True),
                            reads=[dLW, d_c], writes=[dppc] if h == 0 else [], adds=[] if h == 0 else [dppc])
                PCt, dPC = PCr.next()
                K.act.op(lambda e, PCt=PCt, ppc=ppc: e.activation(out=PCt[:], in_=ppc[:], func=AF.Exp), reads=[dppc], writes=[dPC])
                mbc = lambda i: MK[:, i, :].unsqueeze(1).broadcast_to([64, 8, 64])
                pa, dpa = PA.next()
                mm8(pa, dpa, Af, Bf, [dFM])
                N0, dN0 = Nr.next()
                K.dve.op(lambda e, N0=N0, pa=pa: e.tensor_tensor(out=N0[:], in0=pa[:], in1=mbc(0), op=ALU.mult), reads=[dpa, d_c], writes=[dN0])
                pa, dpa = PA.next()
                mm8(pa, dpa, Bf, Af, [dFM])
                NT0, dNT0 = NTr.next()
                MTf, dMTf = MTfr.next()
                K.dve.op(lambda e, NT0=NT0, pa=pa: e.tensor_tensor(out=NT0[:], in0=pa[:], in1=mbc(1), op=ALU.mult), reads=[dpa, d_c], writes=[dNT0])
                K.pool.op(lambda e, MTf=MTf, NT0=NT0: e.tensor_tensor(out=MTf[:], in0=IDF[:], in1=NT0[:], op=ALU.subtract),
                          reads=[dNT0, d_c], writes=[dMTf])
                MT, dMT = MTr.next()
                K.act.op(lambda e, MT=MT, MTf=MTf: e.activation(out=MT[:], in_=MTf[:], func=AF.Copy), reads=[dMTf], writes=[dMT])
                pa, dpa = PA.next()
                mm8(pa, dpa, Kf, Af, [dFM])
                NAK, dNAK = NAKr.next()
                K.dve.op(lambda e, NAK=NAK, pa=pa: e.tensor_tensor(out=NAK[:], in0=pa[:], in1=mbc(1), op=ALU.mult), reads=[dpa, d_c], writes=[dNAK])
                pa, dpa = PA.next()
                mm8(pa, dpa, Bf, Rf, [dFM])
                MRB, dMRB = MRBr.next()
                K.dve.op(lambda e, MRB=MRB, pa=pa: e.tensor_tensor(out=MRB[:], in0=pa[:], in1=mbc(2), op=ALU.mult), reads=[dpa, d_c], writes=[dMRB])
                pa, dpa = PA.next()
                mm8(pa, dpa, Kf, Rf, [dFM])
                MRK, dMRK = MRKr.next()
                K.dve.op(lambda e, MRK=MRK, pa=pa: e.tensor_tensor(out=MRK[:], in0=pa[:], in1=mbc(2), op=ALU.mult), reads=[dpa, d_c], writes=[dMRK])
                Np, dNp, NTp, dNTp = N0, dN0, NT0, dNT0
                for lvl in range(1, 6):
                    pa, dpa = PA.next()
                    mm8(pa, dpa, lambda h, NTp=NTp: NTp[:, h, :], lambda h, Np=Np: Np[:, h, :], [dNp, dNTp])
                    Nn, dNn = Nr.next()
                    evac_copy(Nn[:], pa[:], [dpa], writes=[dNn])
                    if lvl < 5:
                        pa2, dpa2 = PA.next()
                        mm8(pa2, dpa2, lambda h, Np=Np: Np[:, h, :], lambda h, NTp=NTp: NTp[:, h, :], [dNp, dNTp])
                        NTn, dNTn = NTr.next()
                        evac_copy(NTn[:], pa2[:], [dpa2], writes=[dNTn])
                    pa3, dpa3 = PA.next()
                    mm8(pa3, dpa3, lambda h, Nn=Nn: Nn[:, h, :], lambda h, MT=MT: MT[:, h, :], [dNn, dMT])
                    K.dve.op(lambda e, MTf=MTf, pa3=pa3: e.tensor_tensor(out=MTf[:], in0=MTf[:], in1=pa3[:], op=ALU.add),
                             reads=[dpa3], writes=[dMTf])
                    MT, dMT = MTr.next()
                    K.act.op(lambda e, MT=MT, MTf=MTf: e.activation(out=MT[:], in_=MTf[:], func=AF.Copy), reads=[dMTf], writes=[dMT])
                    Np, dNp = Nn, dNn
                    if lvl < 5:
                        NTp, dNTp = NTn, dNTn
                Sb = STb[b]
                pa, dpa = PA.next()
                mmN(pa, dpa, [(Af, lambda h, Sb=Sb: Sb[:, h, :]), (lambda h, NAK=NAK: NAK[:, h, :], Vt)], [dFM, dSTb[b], dNAK, dTM])
                X, dX = Xr.next()
                K.act.op(lambda e, X=X, pa=pa: e.activation(out=X[:], in_=pa[:], func=AF.Copy), reads=[dpa], writes=[dX])
                pa, dpa = PA.next()
                mm8(pa, dpa, lambda h, MT=MT: MT[:, h, :], lambda h, X=X: X[:, h, :], [dMT, dX])
                NU, dNU = NUr.next()
                K.act.op(lambda e, NU=NU, pa=pa: e.activation(out=NU[:], in_=pa[:], func=AF.Copy, scale=-1.0), reads=[dpa], writes=[dNU])
                pa, dpa = PA.next()
                mmN(pa, dpa, [(lambda h, Sb=Sb: Sb[:, h, :], Rf), (lambda h, NU=NU: NU[:, h, :], lambda h, MRB=MRB: MRB[:, h, :]),
                              (Vt, lambda h, MRK=MRK: MRK[:, h, :])], [dFM, dSTb[b], dNU, dMRB, dTM, dMRK])
                Y, dY = Yr.next()
                K.dve.op(lambda e, Y=Y, pa=pa: e.tensor_copy(out=Y[:], in_=pa[:]), reads=[dpa], writes=[dY])
                K.sp.dma(lambda e, Y=Y, t0=t0: e.dma_start(out=dap(scr["y_fm"], t0, [[T, 64], [64 * T, 8], [1, 64]]), in_=Y[:]),
                         reads=[dY], adds=[scr["d_p2b"]])
                pa, dpa = PA.next()
                mmN(pa, dpa, [(Bt, lambda h, NU=NU: NU[:, h, :]), (Kt, Vt)], [dTM, dNU])
                TS_, dTS = TSr.next()
                K.dve.op(lambda e, TS_=TS_, pa=pa, b=b: e.tensor_tensor(out=TS_[:], in0=pa[:], in1=ST[b][:], op=ALU.add),
                         reads=[dpa, dST[b]], writes=[dTS])
                K.dve.op(lambda e, TS_=TS_, PCt=PCt, b=b: e.tensor_tensor(
                    out=ST[b][:], in0=TS_[:], in1=PCt[:].unsqueeze(2).broadcast_to([64, 8, 64]), op=ALU.mult),
                    reads=[dTS, dPC], writes=[dST[b]])
                K.act.op(lambda e, b=b: e.activation(out=STb[b][:], in_=ST[b][:], func=AF.Copy), reads=[dST[b]], writes=[dSTb[b]])


def phase2_post(K, cfg, io, scr):
    T, S, NB = cfg["T"], cfg["S"], cfg["NB"]
    NT = T // 512
    with ExitStack() as es:
        BO = K.sb([128, 128], F32, "BO", es)
        d_c = Dep()
        K.sp.dma(lambda e: e.dma_start(out=BO[:], in_=io["blockones"][:, :]), adds=[d_c])
        LN = K.sb([128, 2, 4], F32, "LN", es)
        K.sp.dma(lambda e: e.dma_start(out=LN[:, 0, :], in_=io["lnx_w"].rearrange("o (j p) -> p (o j)", p=128)), adds=[d_c])
        K.sp.dma(lambda e: e.dma_start(out=LN[:, 1, :], in_=io["lnx_b"].rearrange("o (j p) -> p (o j)", p=128)), adds=[d_c])
        Yr = Rot(K, 2, [128, 512], F32, "pY", es=es)
        Vr = Rot(K, 2, [128, 512], F32, "pV", es=es)
        Gr = Rot(K, 2, [128, 512], F32, "pG", es=es)
        Cr = Rot(K, 2, [128, 512], F32, "pC", es=es)
        YCr = Rot(K, 2, [128, 512], F32, "pYC", es=es)
        SQr = Rot(K, 2, [128, 512], F32, "pSQ", es=es)
        Rr = Rot(K, 2, [128, 512], F32, "pR", es=es)
        Or = Rot(K, 2, [128, 512], BF16, "pO", es=es)
        PM = Rot(K, 2, [128, 512], F32, "pPM", psum=True, es=es)
        PVr = Rot(K, 2, [128, 512], F32, "pPV", psum=True, es=es)
        for ti in range(NT):
            cs = slice(ti * 512, (ti + 1) * 512)
            for j in range(4):
                rs = slice(j * 128, (j + 1) * 128)
                Y, dY = Yr.next()
                V, dV = Vr.next()
                G, dG = Gr.next()
                C, dC = Cr.next()
                K.sp.dma(lambda e, Y=Y, rs=rs, cs=cs: e.dma_start(out=Y[:], in_=scr["y_fm"][rs, cs]),
                         reads=[scr["d_p2b"]], writes=[dY])
                K.pool.dma(lambda e, V=V, rs=rs, cs=cs: e.dma_start(out=V[:], in_=scr["v_fm"][rs, cs]),
                           reads=[scr["d_p2a"]], writes=[dV])
                K.sp.dma(lambda e, G=G, rs=rs, cs=cs: e.dma_start(out=G[:], in_=scr["g_fm"][rs, cs]),
                         reads=[scr["d_p2a"]], writes=[dG])
                K.pool.dma(lambda e, C=C, j=j, cs=cs: e.dma_start(
                    out=C[0:64, :], in_=scr["coef_fm"][2 * j:2 * j + 1, cs].broadcast_to([64, 512])),
                    reads=[scr["d_p2a"]], writes=[dC])
                K.pool.dma(lambda e, C=C, j=j, cs=cs: e.dma_start(
                    out=C[64:128, :], in_=scr["coef_fm"][2 * j + 1:2 * j + 2, cs].broadcast_to([64, 512])),
                    reads=[scr["d_p2a"]], adds=[dC])
                pm, dpm = PM.next()
                K.pe.op(lambda e, pm=pm, Y=Y: e.matmul(pm[:], BO[:], Y[:], start=True, stop=True),
                        reads=[dY, d_c], writes=[dpm])
                YC, dYC = YCr.next()
                K.dve.op(lambda e, YC=YC, Y=Y, pm=pm: e.tensor_tensor(out=YC[:], in0=Y[:], in1=pm[:], op=ALU.subtract),
                         reads=[dY, dpm], writes=[dYC])
                SQ, dSQ = SQr.next()
                K.act.op(lambda e, SQ=SQ, YC=YC: e.activation(out=SQ[:], in_=YC[:], func=AF.Square),
                         reads=[dYC], writes=[dSQ])
                pv, dpv = PVr.next()
                K.pe.op(lambda e, pv=pv, SQ=SQ: e.matmul(pv[:], BO[:], SQ[:], start=True, stop=True),
                        reads=[dSQ, d_c], writes=[dpv])
                R, dR = Rr.next()
                K.dve.op(lambda e, R=R, pv=pv: e.tensor_scalar(out=R[:], in0=pv[:], scalar1=64e-5, scalar2=None, op0=ALU.add),
                         reads=[dpv], writes=[dR])
                K.act.op(lambda e, R=R: e.activation(out=R[:], in_=R[:], func=AF.Ln), writes=[dR])
                K.act.op(lambda e, R=R: e.activation(out=R[:], in_=R[:], func=AF.Exp, scale=-0.5), writes=[dR])
                K.dve.op(lambda e, YC=YC, R=R: e.tensor_tensor(out=YC[:], in0=YC[:], in1=R[:], op=ALU.mult),
                         reads=[dR], writes=[dYC])
                K.dve.op(lambda e, YC=YC, j=j: e.tensor_scalar(out=YC[:], in0=YC[:], scalar1=LN[:, 0, j:j + 1],
                                                              scalar2=LN[:, 1, j:j + 1], op0=ALU.mult, op1=ALU.add),
                         reads=[d_c], writes=[dYC])
                K.pool.op(lambda e, C=C, V=V: e.tensor_tensor(out=C[:], in0=C[:], in1=V[:], op=ALU.mult),
                          reads=[dV], writes=[dC])
                K.dve.op(lambda e, YC=YC, C=C: e.tensor_tensor(out=YC[:], in0=YC[:], in1=C[:], op=ALU.add),
                         reads=[dC], writes=[dYC])
                O, dO = Or.next()
                K.dve.op(lambda e, O=O, YC=YC, G=G: e.tensor_tensor(out=O[:], in0=YC[:], in1=G[:], op=ALU.mult),
                         reads=[dYC, dG], writes=[dO])
                K.sp.dma(lambda e, O=O, rs=rs, cs=cs: e.dma_start(out=scr["ya_fm"][rs, cs], in_=O[:]),
                         reads=[dO], adds=[scr["d_p2c"]])


def phase3(K, cfg, io, scr):
    T, S, NB = cfg["T"], cfg["S"], cfg["NB"]
    NQ = S // 128
    lam_init = 0.2
    with ExitStack() as es:
        d_c = Dep()
        identb = K.sb([128, 128], BF16, "a_identb", es)
        K.sp.dma(lambda e: e.dma_start(out=identb[:], in_=io["ident_bf"][:, :]), adds=[d_c])
        TB = K.sb([128, 4, S], F32, "TB", es)
        for h in range(4):
            (K.sp if h % 2 == 0 else K.pool).dma(lambda e, h=h: e.dma_start(out=TB[:, h, :], in_=io["alibi"][h, :, :]), adds=[d_c])
        SW = K.sb([128, 128], F32, "SW", es)
        K.sp.dma(lambda e: e.dma_start(out=SW[:], in_=io["subln_w"][0:1, :].broadcast_to([128, 128])), adds=[d_c])
        LQ = K.sb([128, 4, 64], F32, "LQ", es)
        for j, nm in enumerate(["lam_q1", "lam_k1", "lam_q2", "lam_k2"]):
            K.pool.dma(lambda e, j=j, nm=nm: e.dma_start(out=LQ[:, j, :], in_=io[nm][0:1, :].broadcast_to([128, 64])), adds=[d_c])
        LM = K.sb([128, 8], F32, "LM", es)
        d_lm = Dep()
        LT_ = K.sb([128, 2, 64], F32, "LTt", es)
        K.dve.op(lambda e: e.tensor_tensor(out=LT_[:, 0, :], in0=LQ[:, 0, :], in1=LQ[:, 1, :], op=ALU.mult), reads=[d_c], writes=[d_lm])
        K.dve.op(lambda e: e.tensor_tensor(out=LT_[:, 1, :], in0=LQ[:, 2, :], in1=LQ[:, 3, :], op=ALU.mult), reads=[d_c], writes=[d_lm])
        K.dve.op(lambda e: e.tensor_reduce(out=LM[:, 0:2], in_=LT_[:], axis=AX.X, op=ALU.add), writes=[d_lm])
        K.act.op(lambda e: e.activation(out=LM[:, 2:4], in_=LM[:, 0:2], func=AF.Exp), writes=[d_lm])
        K.dve.op(lambda e: e.tensor_tensor(out=LM[:, 4:5], in0=LM[:, 3:4], in1=LM[:, 2:3], op=ALU.subtract), writes=[d_lm])
        K.dve.op(lambda e: e.tensor_scalar(out=LM[:, 4:5], in0=LM[:, 4:5], scalar1=-lam_init, scalar2=None, op0=ALU.add), writes=[d_lm])
        K.dve.op(lambda e: e.tensor_scalar(out=SW[:], in0=SW[:], scalar1=1.0 - lam_init, scalar2=None, op0=ALU.mult),
                 reads=[d_c], writes=[d_c])

        Vr = Rot(K, 2, [128, NQ, 512], BF16, "aV", es=es)
        QKr = Rot(K, 2, [64, 4, S], BF16, "aQK", es=es)
        SSr = Rot(K, 3, [128, 512], F32, "aSS", es=es)
        Pr = Rot(K, 3, [128, 512], BF16, "aP", es=es)
        PTsr = Rot(K, 4, [128, 4, 128], BF16, "aPTs", es=es)
        YB = K.sb([128, NQ, 512], BF16, "aYB", es)
        dYB = Dep()
        STr = Rot(K, 4, [128, 24], F32, "aST", es=es)
        O1r = Rot(K, 2, [128, 128], F32, "aO1", es=es)
        Or_ = Rot(K, 2, [128, 128], F32, "aO", es=es)
        junk = K.sb([128, 128], F32, "ajunk", es)
        d_junk = Dep()
        YTr = Rot(K, 2, [128, 4, 128], BF16, "aYT", es=es)
        PS = Rot(K, 3, [128, 512], F32, "aPS", psum=True, es=es)
        PTp = Rot(K, 2, [128, 4, 128], BF16, "aPTp", psum=True, es=es)
        PO = Rot(K, 2, [128, 2, 128], F32, "aPO", psum=True, es=es)
        cp = [0]

        def copy_eng():
            cp[0] += 1
            return cp[0] % 2

        SSQ = K.sb([128, NQ * 4], F32, "aSSQ", es)
        dSSQ = Dep()
        SWb = K.sb([128, 128], BF16, "aSWb", es)
        K.act.op(lambda e: e.activation(out=SWb[:], in_=SW[:], func=AF.Copy), reads=[d_c], adds=[d_c])
        pipe = []
        pidx = [0]

        def step_pipe():
            j = len(pipe) - 1
            pipe[j][0]()
            if j - 1 >= pidx[0]:
                pipe[j - 1][2]()
            if j - 2 >= pidx[0]:
                pipe[j - 2][3]()
                if pipe[j - 2][4] is not None:
                    pipe[j - 2][4]()
            pipe[j][1]()

        def flush_pipe():
            j = len(pipe) - 1
            if j - 0 >= pidx[0] and j >= 0:
                pipe[j][2]()
            for k in (j - 1, j):
                if k >= pidx[0] and k >= 0:
                    pipe[k][3]()
                    if pipe[k][4] is not None:
                        pipe[k][4]()
            pidx[0] = len(pipe)
        for b in range(NB):
            V, dV = Vr.next()
            K.sp.dma(lambda e, V=V, b=b: e.dma_start(
                out=V[:], in_=scr["av_tm"][b * S:(b + 1) * S, :].rearrange("(n p) c -> p n c", p=128)),
                reads=[scr["d_p1"]], writes=[dV])
            first_yb = True
            for h in range(4):
                QK, dQK = QKr.next()
                for j in range(4):
                    r0 = (0 if j < 2 else 512) + h * 128 + (j % 2) * 64
                    (K.sp if j % 2 == 0 else K.pool).dma(lambda e, QK=QK, j=j, r0=r0, b=b: e.dma_start(
                        out=QK[:, j, :], in_=scr["qk_fm"][r0:r0 + 64, b * S:(b + 1) * S]),
                        reads=[scr["d_p1"]], writes=[dQK] if j == 0 else [], adds=[] if j == 0 else [dQK])
                for qi in range(NQ):
                    nk = (qi + 1) * 128
                    off = (S - 128) - qi * 128
                    ST, dST = STr.next()
                    K.pool.op(lambda e, ST=ST: e.memset(ST[:], 0.0), writes=[dST])
                    po, dpo = PO.next()
                    items = []
                    for c in range(2):
                        nch = (nk + 511) // 512
                        for ch in range(nch):
                            items.append((c, ch))
                    for ii, (c, ch) in enumerate(items):
                        kb0 = ch * 512
                        n = min(512, nk - kb0)
                        nb = n // 128
                        ps, dps = PS.next()
                        SS, dSS = SSr.next()
                        Pt, dP = Pr.next()
                        hold = {}

                        def stA_pe(ps=ps, dps=dps, c=c, qi=qi, kb0=kb0, n=n, QK=QK, dQK=dQK):
                            K.pe.op(lambda e: e.matmul(
                                ps[:, :n], QK[:, c, qi * 128:(qi + 1) * 128], QK[:, 2 + c, kb0:kb0 + n],
                                start=True, stop=True), reads=[dQK], writes=[dps])

                        def stA_rest(ps=ps, dps=dps, SS=SS, dSS=dSS, Pt=Pt, dP=dP, ST=ST, dST=dST, n=n, off=off, h=h, kb0=kb0, c=c, ch=ch):
                            K.dve.op(lambda e: e.scalar_tensor_tensor(
                                out=SS[:, :n], in0=ps[:, :n], scalar=0.125, in1=TB[:, h, off + kb0:off + kb0 + n],
                                op0=ALU.mult, op1=ALU.add), reads=[dps, d_c], writes=[dSS])
                            K.act.op(lambda e: e.activation(
                                out=Pt[:, :n], in_=SS[:, :n], func=AF.Exp,
                                accum_out=ST[:, 4 * c + ch:4 * c + ch + 1]), reads=[dSS, dST], writes=[dP], adds=[dST])

                        def stB(Pt=Pt, dP=dP, nb=nb, hold=hold):
                            ptp, dptp = PTp.next()
                            for kk_ in range(nb):
                                K.pe.op(lambda e, kk_=kk_: e.transpose(
                                    out=ptp[:, kk_, :], in_=Pt[:, kk_ * 128:(kk_ + 1) * 128], identity=identb[:]),
                                    reads=[dP, d_c], writes=[dptp] if kk_ == 0 else [], adds=[] if kk_ == 0 else [dptp])
                            PTs, dPTs = PTsr.next()
                            hold["PTs"] = (PTs, dPTs)
                            if copy_eng():
                                K.act.op(lambda e: e.activation(
                                    out=PTs[:, :nb, :], in_=ptp[:, :nb, :], func=AF.Copy), reads=[dptp], writes=[dPTs])
                            else:
                                K.dve.op(lambda e: e.tensor_copy(
                                    out=PTs[:, :nb, :], in_=ptp[:, :nb, :]), reads=[dptp], writes=[dPTs])

                        def stC(nb=nb, kb0=kb0, c=c, h=h, qi=qi, po=po, dpo=dpo, V=V, dV=dV, hold=hold):
                            PTs, dPTs = hold["PTs"]
                            for kk_ in range(nb):
                                kb = kb0 // 128 + kk_
                                K.pe.op(lambda e, kb=kb, kk_=kk_: e.matmul(
                                    po[:, c, :], PTs[:, kk_, :], V[:, kb, h * 128:(h + 1) * 128],
                                    start=(kb == 0), stop=(kb == qi)), reads=[dPTs, dV],
                                    writes=[dpo] if (kb == 0 and c == 0) else [], adds=[] if (kb == 0 and c == 0) else [dpo])

                        def combine(ST=ST, dST=dST, po=po, dpo=dpo, qi=qi, h=h, fy=first_yb):
                            K.dve.op(lambda e: e.tensor_reduce(out=ST[:, 8:10], in_=ST[:, 0:8].rearrange("p (c k) -> p c k", k=4),
                                                               axis=AX.X, op=ALU.add), reads=[dST], writes=[dST])
                            K.dve.op(lambda e: e.reciprocal(out=ST[:, 10:12], in_=ST[:, 8:10]), writes=[dST])
                            K.dve.op(lambda e: e.tensor_tensor(out=ST[:, 12:13], in0=ST[:, 11:12], in1=LM[:, 4:5], op=ALU.mult),
                                     reads=[d_lm], writes=[dST])
                            O1, dO1 = O1r.next()
                            K.dve.op(lambda e: e.tensor_scalar(out=O1[:], in0=po[:, 1, :], scalar1=ST[:, 12:13],
                                                               scalar2=None, op0=ALU.mult), reads=[dpo, dST], writes=[dO1])
                            K.dve.op(lambda e: e.scalar_tensor_tensor(
                                out=YB[:, qi, h * 128:(h + 1) * 128], in0=po[:, 0, :], scalar=ST[:, 10:11], in1=O1[:], op0=ALU.mult, op1=ALU.add),
                                reads=[dpo, dST, dO1], writes=[dYB] if fy else [], adds=[] if fy else [dYB])
                            K.act.op(lambda e: e.activation(out=junk[:], in_=YB[:, qi, h * 128:(h + 1) * 128], func=AF.Square,
                                                            accum_out=SSQ[:, qi * 4 + h:qi * 4 + h + 1]),
                                     reads=[dYB], writes=[d_junk], adds=[dSSQ])

                        last = (ii == len(items) - 1)
                        pipe.append([stA_pe, stA_rest, stB, stC, combine if last else None])
                        step_pipe()
                    first_yb = False
            flush_pipe()
            K.dve.op(lambda e: e.tensor_scalar(out=SSQ[:], in0=SSQ[:], scalar1=1.0 / 128, scalar2=1e-5, op0=ALU.mult, op1=ALU.add),
                     reads=[dSSQ], writes=[dSSQ])
            K.act.op(lambda e: e.activation(out=SSQ[:], in_=SSQ[:], func=AF.Ln), writes=[dSSQ])
            K.act.op(lambda e: e.activation(out=SSQ[:], in_=SSQ[:], func=AF.Exp, scale=-0.5), writes=[dSSQ])
            YBv = YB[:].rearrange("p q (h e) -> p (q h) e", e=128)
            K.dve.op(lambda e: e.tensor_tensor(out=YBv, in0=YBv, in1=SSQ[:].unsqueeze(2).broadcast_to([128, NQ * 4, 128]), op=ALU.mult),
                     reads=[dSSQ], writes=[dYB])
            K.pool.op(lambda e: e.tensor_tensor(out=YBv, in0=YBv, in1=SWb[:].unsqueeze(1).broadcast_to([128, NQ * 4, 128]), op=ALU.mult),
                      reads=[d_c], writes=[dYB])
            for qi in range(NQ):
                ptp, dptp = PTp.next()
                for h in range(4):
                    K.pe.op(lambda e, ptp=ptp, qi=qi, h=h: e.transpose(out=ptp[:, h, :], in_=YB[:, qi, h * 128:(h + 1) * 128],
                                                                      identity=identb[:]),
                            reads=[dYB, d_c], writes=[dptp] if h == 0 else [], adds=[] if h == 0 else [dptp])
                YT, dYT = YTr.next()
                K.act.op(lambda e, ptp=ptp, YT=YT: e.activation(out=YT[:], in_=ptp[:, 0:4, :], func=AF.Copy),
                         reads=[dptp], writes=[dYT])
                t0 = b * S + qi * 128
                K.sp.dma(lambda e, YT=YT, t0=t0: e.dma_start(
                    out=dap(scr["yb_fm"], t0, [[T, 128], [128 * T, 4], [1, 128]]), in_=YT[:]),
                    reads=[dYT], adds=[scr["d_p3"]])


def load_w_bf16(K, es, src2d, rows, cols, name, dep, stage_rot, q, W=None):
    nk = rows // 128
    if W is None:
        W = K.sb([128, nk, cols], BF16, name, es)
    for kc in range(nk):
        for c0 in range(0, cols, 1024):
            n = min(1024, cols - c0)
            st, dst = stage_rot.next()
            q[0] += 1
            (K.sp if q[0] % 2 == 0 else K.pool).dma(lambda e, st=st, kc=kc, c0=c0, n=n: e.dma_start(
                out=st[:, :n], in_=src2d[kc * 128:(kc + 1) * 128, c0:c0 + n]), writes=[dst])
            if q[0] % 2 == 0:
                K.act.op(lambda e, st=st, kc=kc, c0=c0, n=n: e.activation(out=W[:, kc, c0:c0 + n], in_=st[:, :n], func=AF.Copy),
                         reads=[dst], adds=[dep])
            else:
                K.dve.op(lambda e, st=st, kc=kc, c0=c0, n=n: e.tensor_copy(out=W[:, kc, c0:c0 + n], in_=st[:, :n]),
                         reads=[dst], adds=[dep])
    return W


def phase4(K, cfg, io, scr):
    T, S, NB = cfg["T"], cfg["S"], cfg["NB"]
    NT = T // 512
    with ExitStack() as es:
        d_c = Dep()
        identb = K.sb([128, 128], BF16, "m_identb", es)
        K.sp.dma(lambda e: e.dma_start(out=identb[:], in_=io["ident_bf"][:, :]), adds=[d_c])
        NF = K.sb([128, D], F32, "NF", es)
        K.sp.dma(lambda e: e.dma_start(out=NF[:], in_=io["norm_ffn_w"][0:1, :].broadcast_to([128, D])), adds=[d_c])
        stg = Rot(K, 2, [128, 1024], F32, "m_stg", es=es)
        q = [0]
        PAw = load_w_bf16(K, es, io["proj_a"][0], 512, D, "PAw", d_c, stg, q)
        PBw = load_w_bf16(K, es, io["proj_b"][0], 512, D, "PBw", d_c, stg, q)
        WO = load_w_bf16(K, es, io["w_out"][0], D, D, "WO", d_c, stg, q)
        YAr = Rot(K, 2, [128, 4, 512], BF16, "mYA", es=es)
        YBr = Rot(K, 2, [128, 4, 512], BF16, "mYB", es=es)
        SGr = Rot(K, 2, [128, 16, 512], BF16, "mSG", es=es)
        MGr = Rot(K, 2, [128, 8, 512], BF16, "mMG", es=es)
        t1r = Rot(K, 2, [128, 512], F32, "mt1", es=es)
        t2r = Rot(K, 2, [128, 512], F32, "mt2", es=es)
        Xr = Rot(K, 2, [128, D], F32, "mX", es=es)
        X1r = Rot(K, 2, [128, D], F32, "mX1", es=es)
        XHr = Rot(K, 2, [128, D], F32, "mXH", es=es)
        XBr = Rot(K, 2, [128, D], BF16, "mXB", es=es)
        XTr = Rot(K, 2, [128, 8, 128], BF16, "mXT", es=es)
        junk = K.sb([128, D], BF16, "mjunk", es)
        d_junk = Dep()
        STr = Rot(K, 4, [128, 4], F32, "mST", es=es)
        PP = Rot(K, 2, [128, 2, 512], F32, "mPP", psum=True, es=es)
        PO2 = Rot(K, 1, [128, 2, 512], F32, "mPO", psum=True, es=es)
        PTp = Rot(K, 1, [128, 8, 128], BF16, "mPTp", psum=True, es=es)
        for ti in range(NT):
            cs = slice(ti * 512, (ti + 1) * 512)
            YA, dYA = YAr.next()
            YB, dYB = YBr.next()
            SG, dSG = SGr.next()
            K.sp.dma(lambda e, YA=YA, cs=cs: e.dma_start(out=YA[:], in_=scr["ya_fm"][:, cs].rearrange("(c p) t -> p c t", p=128)),
                     reads=[scr["d_p2c"]], writes=[dYA])
            K.pool.dma(lambda e, YB=YB, cs=cs: e.dma_start(out=YB[:], in_=scr["yb_fm"][:, cs].rearrange("(c p) t -> p c t", p=128)),
                       reads=[scr["d_p3"]], writes=[dYB])
            K.sp.dma(lambda e, SG=SG, cs=cs: e.dma_start(out=SG[:], in_=scr["sg_fm"][:, cs].rearrange("(c p) t -> p c t", p=128)),
                     reads=[scr["d_p1"]], writes=[dSG])
            MG, dMG = MGr.next()
            for m in range(8):
                pp, dpp = PP.next()
                for c in range(4):
                    K.pe.op(lambda e, pp=pp, YA=YA, c=c, m=m: e.matmul(pp[:, 0, :], PAw[:, c, m * 128:(m + 1) * 128], YA[:, c, :],
                                                                      start=(c == 0), stop=(c == 3)),
                            reads=[dYA, d_c], writes=[dpp] if c == 0 else [], adds=[] if c == 0 else [dpp])
                for c in range(4):
                    K.pe.op(lambda e, pp=pp, YB=YB, c=c, m=m: e.matmul(pp[:, 1, :], PBw[:, c, m * 128:(m + 1) * 128], YB[:, c, :],
                                                                      start=(c == 0), stop=(c == 3)),
                            reads=[dYB, d_c], adds=[dpp])
                t1, dt1 = t1r.next()
                t2, dt2 = t2r.next()
                K.dve.op(lambda e, t1=t1, pp=pp, SG=SG, m=m: e.tensor_tensor(out=t1[:], in0=pp[:, 0, :], in1=SG[:, m, :], op=ALU.mult),
                         reads=[dpp, dSG], writes=[dt1])
                K.dve.op(lambda e, t2=t2, pp=pp, SG=SG, m=m: e.tensor_tensor(out=t2[:], in0=pp[:, 1, :], in1=SG[:, 8 + m, :], op=ALU.mult),
                         reads=[dpp, dSG], writes=[dt2])
                K.pool.op(lambda e, t1=t1, t2=t2, MG=MG, m=m: e.tensor_tensor(out=MG[:, m, :], in0=t1[:], in1=t2[:], op=ALU.add),
                          reads=[dt1, dt2], writes=[dMG] if m == 0 else [], adds=[] if m == 0 else [dMG])
            for sub in range(4):
                t0 = ti * 512 + sub * 128
                X, dX = Xr.next()
                K.pool.dma(lambda e, X=X, t0=t0: e.dma_start(out=X[:], in_=io["x"][t0:t0 + 128, :]), writes=[dX])
                po, dpo = PO2.next()
                for n in range(2):
                    for m in range(8):
                        K.pe.op(lambda e, po=po, MG=MG, m=m, n=n, sub=sub: e.matmul(
                            po[:, n, :], MG[:, m, sub * 128:(sub + 1) * 128], WO[:, m, n * 512:(n + 1) * 512],
                            start=(m == 0), stop=(m == 7)), reads=[dMG, d_c],
                            writes=[dpo] if (m == 0 and n == 0) else [], adds=[] if (m == 0 and n == 0) else [dpo])
                X1, dX1 = X1r.next()
                K.dve.op(lambda e, X1=X1, X=X, po=po: e.tensor_tensor(out=X1[:], in0=X[:], in1=po[:].rearrange("p a b -> p (a b)"),
                                                                     op=ALU.add), reads=[dX, dpo], writes=[dX1])
                K.sp.dma(lambda e, X1=X1, t0=t0: e.dma_start(out=scr["x1_tm"][t0:t0 + 128, :], in_=X1[:]),
                         reads=[dX1], adds=[scr["d_p4"]])
                ST, dST = STr.next()
                K.act.op(lambda e, X1=X1, ST=ST: e.activation(out=junk[:], in_=X1[:], func=AF.Square, accum_out=ST[:, 0:1]),
                         reads=[dX1], writes=[d_junk, dST])
                K.dve.op(lambda e, ST=ST: e.tensor_scalar(out=ST[:, 1:2], in0=ST[:, 0:1], scalar1=1.0 / D, scalar2=1e-6,
                                                          op0=ALU.mult, op1=ALU.add), writes=[dST])
                K.act.op(lambda e, ST=ST: e.activation(out=ST[:, 2:3], in_=ST[:, 1:2], func=AF.Sqrt), writes=[dST])
                K.dve.op(lambda e, ST=ST: e.reciprocal(out=ST[:, 3:4], in_=ST[:, 2:3]), writes=[dST])
                XH, dXH = XHr.next()
                K.dve.op(lambda e, XH=XH, X1=X1, ST=ST: e.scalar_tensor_tensor(
                    out=XH[:], in0=X1[:], scalar=ST[:, 3:4], in1=NF[:], op0=ALU.mult, op1=ALU.mult),
                    reads=[dX1, dST, d_c], writes=[dXH])
                K.sp.dma(lambda e, XH=XH, t0=t0: e.dma_start(out=scr["xh_tm"][t0:t0 + 128, :], in_=XH[:]),
                         reads=[dXH], adds=[scr["d_p4"]])
                XB, dXB = XBr.next()
                K.pool.op(lambda e, XB=XB, XH=XH: e.tensor_copy(out=XB[:], in_=XH[:]), reads=[dXH], writes=[dXB])
                ptp, dptp = PTp.next()
                for kc in range(8):
                    K.pe.op(lambda e, ptp=ptp, XB=XB, kc=kc: e.transpose(out=ptp[:, kc, :], in_=XB[:, kc * 128:(kc + 1) * 128],
                                                                        identity=identb[:]),
                            reads=[dXB, d_c], writes=[dptp] if kc == 0 else [], adds=[] if kc == 0 else [dptp])
                XT, dXT = XTr.next()
                K.act.op(lambda e, ptp=ptp, XT=XT: e.activation(out=XT[:], in_=ptp[:], func=AF.Copy), reads=[dptp], writes=[dXT])
                K.sp.dma(lambda e, XT=XT, t0=t0: e.dma_start(
                    out=dap(scr["xhT_fm"], t0, [[T, 128], [128 * T, 8], [1, 128]]), in_=XT[:]),
                    reads=[dXT], adds=[scr["d_p4"]])


def phase5(K, cfg, io, scr):
    T, S, NB = cfg["T"], cfg["S"], cfg["NB"]
    NTT = T // 128
    with ExitStack() as es:
        d_c = Dep()
        identb = K.sb([128, 128], BF16, "f_identb", es)
        K.sp.dma(lambda e: e.dma_start(out=identb[:], in_=io["ident_bf"][:, :]), adds=[d_c])
        IOTA = K.sb([128, 16], F32, "IOTA", es)
        K.sp.dma(lambda e: e.dma_start(out=IOTA[:], in_=io["iota16"][:, :]), adds=[d_c])
        FNW = K.sb([128, D], F32, "FNW", es)
        K.sp.dma(lambda e: e.dma_start(out=FNW[:], in_=io["final_norm_w"][0:1, :].broadcast_to([128, D])), adds=[d_c])
        WQ = K.sb([128, 8, 2048], BF16, "WQ", es)
        KT = K.sb([128, 16, 128], BF16, "KT", es)
        PQ = Rot(K, 1, [128, 8, 128], F32, "fPQ", psum=True, es=es)
        PSc = Rot(K, 1, [128, 8, 128], F32, "fPSc", psum=True, es=es)
        PKT = Rot(K, 1, [128, 8, 128], BF16, "fPKT", psum=True, es=es)
        es_setup = ExitStack()
        stg = Rot(K, 2, [128, 1024], F32, "f_stg", es=es_setup)
        q = [0]
        load_w_bf16(K, es, io["peer_wq"][0], D, 2048, "WQ", d_c, stg, q, W=WQ)
        KF = K.sb([128, 16, 128], F32, "KF", es_setup)
        dKF = Dep()
        K.sp.dma(lambda e: e.dma_start(out=KF[:], in_=io["peer_keys"][0].rearrange("h c n d -> n (h c) d")), writes=[dKF])
        KB = K.sb([128, 16, 128], BF16, "KB", es_setup)
        K.dve.op(lambda e: e.tensor_copy(out=KB[:], in_=KF[:]), reads=[dKF], writes=[dKF])
        for half in range(2):
            pk, dpk = PKT.next()
            for i in range(8):
                K.pe.op(lambda e, pk=pk, i=i, half=half: e.transpose(out=pk[:, i, :], in_=KB[:, half * 8 + i, :], identity=identb[:]),
                        reads=[dKF, d_c], writes=[dpk] if i == 0 else [], adds=[] if i == 0 else [dpk])
            K.act.op(lambda e, pk=pk, half=half: e.activation(out=KT[:, half * 8:(half + 1) * 8, :], in_=pk[:], func=AF.Copy),
                     reads=[dpk], adds=[d_c])

        K.barrier()
        es_setup.close()
        XTr = Rot(K, 2, [128, 8, 128], BF16, "fXT", es=es)
        XHr = Rot(K, 2, [128, D], F32, "fXH", es=es)
        X1r = Rot(K, 2, [128, D], F32, "fX1", es=es)
        QTr = Rot(K, 1, [128, 16, 128], BF16, "fQT", es=es)
        SCr = Rot(K, 1, [128, 16, 128], F32, "fSC", es=es)
        SC2 = K.sb([128, 256], F32, "fSC2", es)
        dSC2 = Dep()
        M16r = Rot(K, 1, [128, 16, 16], F32, "fM16", es=es)
        I16r = Rot(K, 1, [128, 16, 16], U32, "fI16", es=es)
        I16fr = Rot(K, 1, [128, 16, 16], F32, "fI16f", es=es)
        CANDr = Rot(K, 1, [128, 8, 256], F32, "fCAND", es=es)
        VALr = Rot(K, 1, [128, 8, 16], F32, "fVAL", es=es)
        CIr = Rot(K, 1, [128, 3, 128], U32, "fCI", es=es)
        ABr = Rot(K, 1, [128, 2, 128], F32, "fAB", es=es)
        OHr = CANDr
        E12r = Rot(K, 1, [128, 3, 128], F32, "fE12", es=es)
        IDSr = Rot(K, 2, [128, 128], I32, "fIDS", es=es)
        GTr = Rot(K, 2, [128, 4, 128], F32, "fGT", es=es)
        S8r = Rot(K, 2, [128, 16], F32, "fS8", es=es)
        GRP = cfg.get("grp", 4)
        ACTDOT = tuple(cfg.get("actdot", (0, 2)))
        if isinstance(cfg.get("actdot_mask"), int):
            ACTDOT = tuple(i for i in range(GRP) if (cfg["actdot_mask"] >> i) & 1)
        junk3r = Rot(K, 2, [128, D], BF16, "fjunk3", es=es)
        junkr = Rot(K, 3, [128, D], BF16, "fjunkr", es=es)
        PRDr = Rot(K, 3, [128, D], BF16, "fPRD", es=es)
        GBr = Rot(K, cfg.get("ngbuf", 22), [128, 2 * D], BF16, "fGB", es=es)
        junk2 = K.sb([128, D], BF16, "fjunk2", es)
        d_junk2 = Dep()
        XHbr = Rot(K, 2, [128, D], BF16, "fXHb", es=es)
        DGr = Rot(K, 4, [128, 128], BF16, "fDG", es=es)
        PY = Rot(K, 1, [128, 2, 512], F32, "fPY", psum=True, es=es)

        RES = {}

        def routing(ti):
            t0 = ti * 128
            XT, dXT = XTr.next()
            XH, dXH = XHr.next()
            X1, dX1 = X1r.next()
            K.sp.dma(lambda e, XT=XT, t0=t0: e.dma_start(out=XT[:], in_=dap(scr["xhT_fm"], t0, [[T, 128], [128 * T, 8], [1, 128]])),
                     reads=[scr["d_p4"]], writes=[dXT])
            K.sp.dma(lambda e, XH=XH, t0=t0: e.dma_start(out=XH[:], in_=scr["xh_tm"][t0:t0 + 128, :]), reads=[scr["d_p4"]], writes=[dXH])
            K.sp.dma(lambda e, X1=X1, t0=t0: e.dma_start(out=X1[:], in_=scr["x1_tm"][t0:t0 + 128, :]), reads=[scr["d_p4"]], writes=[dX1])
            QT, dQT = QTr.next()
            for half in range(2):
                pq, dpq = PQ.next()
                for i in range(8):
                    hc = half * 8 + i
                    for kc in range(8):
                        K.pe.op(lambda e, pq=pq, i=i, hc=hc, kc=kc, XT=XT: e.matmul(
                            pq[:, i, :], WQ[:, kc, hc * 128:(hc + 1) * 128], XT[:, kc, :], start=(kc == 0), stop=(kc == 7)),
                            reads=[dXT, d_c], writes=[dpq] if (i == 0 and kc == 0) else [], adds=[] if (i == 0 and kc == 0) else [dpq])
                K.act.op(lambda e, pq=pq, QT=QT, half=half: e.activation(out=QT[:, half * 8:(half + 1) * 8, :], in_=pq[:], func=AF.Copy),
                         reads=[dpq], writes=[dQT] if half == 0 else [], adds=[] if half == 0 else [dQT])
            SC, dSC = SCr.next()
            for half in range(2):
                psc, dpsc = PSc.next()
                for i in range(8):
                    hc = half * 8 + i
                    K.pe.op(lambda e, psc=psc, i=i, hc=hc, QT=QT: e.matmul(psc[:, i, :], QT[:, hc, :], KT[:, hc, :], start=True, stop=True),
                            reads=[dQT, d_c], writes=[dpsc] if i == 0 else [], adds=[] if i == 0 else [dpsc])
                K.act.op(lambda e, psc=psc, SC=SC, half=half: e.activation(out=SC[:, half * 8:(half + 1) * 8, :], in_=psc[:], func=AF.Copy),
                         reads=[dpsc], writes=[dSC] if half == 0 else [], adds=[] if half == 0 else [dSC])
            yield
            M16, dM = M16r.next()
            I16, dI = I16r.next()
            for hc in range(16):
                if hc % 4 == 0 and hc > 0:
                    yield
                K.dve.op(lambda e, M16=M16, SC=SC, hc=hc: e.max(out=M16[:, hc, 0:8], in_=SC[:, hc, :]), reads=[dSC],
                         writes=[dM] if hc == 0 else [], adds=[] if hc == 0 else [dM])
                K.dve.op(lambda e, M16=M16, SC=SC, hc=hc: e.match_replace(out=SC2[:, 0:128], in_to_replace=M16[:, hc, 0:8],
                                                                         in_values=SC[:, hc, :], imm_value=-1e30),
                         reads=[dSC, dM], writes=[dSC2])
                K.dve.op(lambda e, M16=M16, hc=hc: e.max(out=M16[:, hc, 8:16], in_=SC2[:, 0:128]), reads=[dSC2], adds=[dM])
                K.dve.op(lambda e, M16=M16, I16=I16, SC=SC, hc=hc: e.max_index(out=I16[:, hc, 0:8], in_max=M16[:, hc, 0:8],
                                                                              in_values=SC[:, hc, :]),
                         reads=[dSC, dM], writes=[dI] if hc == 0 else [], adds=[] if hc == 0 else [dI])
                K.dve.op(lambda e, M16=M16, I16=I16, SC=SC, hc=hc: e.max_index(out=I16[:, hc, 8:16], in_max=M16[:, hc, 8:16],
                                                                              in_values=SC[:, hc, :]),
                         reads=[dSC, dM], adds=[dI])
            yield
            I16f, dIf = I16fr.next()
            K.dve.op(lambda e, I16f=I16f, I16=I16: e.tensor_copy(out=I16f[:], in_=I16[:]), reads=[dI], writes=[dIf])
            I16fv = I16f[:].rearrange("p (h c) k -> p h c k", c=2)
            K.dve.op(lambda e, I16fv=I16fv: e.tensor_scalar(out=I16fv[:, :, 0, :], in0=I16fv[:, :, 0, :], scalar1=128.0, scalar2=None,
                                                            op0=ALU.mult), writes=[dIf])
            CAND, dCA = CANDr.next()
            M16v = M16[:].rearrange("p (h c) k -> p h c k", c=2)
            K.dve.op(lambda e, CAND=CAND, M16v=M16v: e.tensor_tensor(
                out=CAND[:].rearrange("p h (a b) -> p h a b", b=16),
                in0=M16v[:, :, 0, :].unsqueeze(3).broadcast_to([128, 8, 16, 16]),
                in1=M16v[:, :, 1, :].unsqueeze(2).broadcast_to([128, 8, 16, 16]), op=ALU.add),
                reads=[dM], writes=[dCA])
            VAL, dVAL = VALr.next()
            CI, dCI = CIr.next()
            CIv = CI[:, 0, :].rearrange("p (h k) -> p h k", k=16)
            for h in range(8):
                if h % 4 == 0:
                    yield
                K.dve.op(lambda e, VAL=VAL, CAND=CAND, h=h: e.max(out=VAL[:, h, 0:8], in_=CAND[:, h, :]), reads=[dCA],
                         writes=[dVAL] if h == 0 else [], adds=[] if h == 0 else [dVAL])
                K.dve.op(lambda e, VAL=VAL, CAND=CAND, h=h: e.match_replace(out=SC2[:, :], in_to_replace=VAL[:, h, 0:8],
                                                                           in_values=CAND[:, h, :], imm_value=-1e30),
                         reads=[dCA, dVAL], writes=[dSC2])
                K.dve.op(lambda e, VAL=VAL, h=h: e.max(out=VAL[:, h, 8:16], in_=SC2[:, :]), reads=[dSC2], adds=[dVAL])
                K.dve.op(lambda e, VAL=VAL, CIv=CIv, CAND=CAND, h=h: e.max_index(out=CIv[:, h, 0:8], in_max=VAL[:, h, 0:8],
                                                                                in_values=CAND[:, h, :]),
                         reads=[dCA, dVAL], writes=[dCI] if h == 0 else [], adds=[] if h == 0 else [dCI])
                K.dve.op(lambda e, VAL=VAL, CIv=CIv, CAND=CAND, h=h: e.max_index(out=CIv[:, h, 8:16], in_max=VAL[:, h, 8:16],
                                                                                in_values=CAND[:, h, :]),
                         reads=[dCA, dVAL], adds=[dCI])
            yield
            GT, dGT = GTr.next()
            S8, dS8 = S8r.next()
            Ev = GT[:, 0, :].rearrange("p (h k) -> p h k", k=16)
            Gv = GT[:, 1, :].rearrange("p (h k) -> p h k", k=16)
            K.dve.op(lambda e, Ev=Ev, VAL=VAL: e.tensor_tensor(out=Ev, in0=VAL[:], in1=VAL[:, :, 0:1].broadcast_to([128, 8, 16]),
                                                              op=ALU.subtract), reads=[dVAL], writes=[dGT])
            K.act.op(lambda e, GT=GT: e.activation(out=GT[:, 0, :], in_=GT[:, 0, :], func=AF.Exp), writes=[dGT])
            K.dve.op(lambda e, Ev=Ev, S8=S8: e.tensor_reduce(out=S8[:, 0:8], in_=Ev, axis=AX.X, op=ALU.add), reads=[dGT], writes=[dS8])
            K.dve.op(lambda e, S8=S8: e.reciprocal(out=S8[:, 8:16], in_=S8[:, 0:8]), writes=[dS8])
            K.dve.op(lambda e, Ev=Ev, Gv=Gv, S8=S8: e.tensor_tensor(out=Gv, in0=Ev, in1=S8[:, 8:16].unsqueeze(2).broadcast_to([128, 8, 16]),
                                                                   op=ALU.mult), reads=[dS8], writes=[dGT])
            yield
            K.dve.op(lambda e, CI=CI: e.tensor_single_scalar(out=CI[:, 1, :], in_=CI[:, 0, :], scalar=4, op=ALU.logical_shift_right),
                     writes=[dCI])
            K.dve.op(lambda e, CI=CI: e.tensor_single_scalar(out=CI[:, 2, :], in_=CI[:, 0, :], scalar=15, op=ALU.bitwise_and),
                     writes=[dCI])
            AB, dAB = ABr.next()
            K.dve.op(lambda e, AB=AB, CI=CI: e.tensor_copy(out=AB[:], in_=CI[:, 1:3, :]), reads=[dCI], writes=[dAB])
            OH, dOH = OHr.next()
            E12, dE12 = E12r.next()
            OHv = OH[:].rearrange("p h (j a) -> p h j a", a=16)
            for c in range(2):
                ABv = AB[:, c, :].rearrange("p (h j) -> p h j", j=16)
                K.dve.op(lambda e, OHv=OHv, ABv=ABv: e.tensor_tensor(
                    out=OHv, in0=ABv.unsqueeze(3).broadcast_to([128, 8, 16, 16]),
                    in1=IOTA[:].unsqueeze(1).unsqueeze(1).broadcast_to([128, 8, 16, 16]), op=ALU.is_equal),
                    reads=[dAB, d_c], writes=[dOH])
                K.dve.op(lambda e, OHv=OHv, I16fv=I16fv, c=c: e.tensor_tensor(
                    out=OHv, in0=OHv, in1=I16fv[:, :, c, :].unsqueeze(2).broadcast_to([128, 8, 16, 16]), op=ALU.mult),
                    reads=[dIf], writes=[dOH])
                K.dve.op(lambda e, OHv=OHv, E12=E12, c=c: e.tensor_reduce(
                    out=E12[:, c, :].rearrange("p (h j) -> p h j", j=16), in_=OHv, axis=AX.X, op=ALU.add),
                    reads=[dOH], writes=[dE12] if c == 0 else [], adds=[] if c == 0 else [dE12])
            K.dve.op(lambda e, E12=E12: e.tensor_tensor(out=E12[:, 2, :], in0=E12[:, 0, :], in1=E12[:, 1, :], op=ALU.add), writes=[dE12])
            IDS, dIDS = IDSr.next()
            K.dve.op(lambda e, IDS=IDS, E12=E12: e.tensor_copy(out=IDS[:], in_=E12[:, 2, :]), reads=[dE12], writes=[dIDS])
            if "ids_dbg" in scr:
                K.sp.dma(lambda e, IDS=IDS, t0=t0: e.dma_start(out=scr["ids_dbg"][t0:t0 + 128, :], in_=IDS[:]), reads=[dIDS])
                K.sp.dma(lambda e, GT=GT, t0=t0: e.dma_start(out=scr["gate_dbg"][t0:t0 + 128, :], in_=GT[:, 1, :]), reads=[dGT])
            RES[ti] = dict(t0=t0, XH=XH, dXH=dXH, X1=X1, dX1=dX1, IDS=IDS, dIDS=dIDS, GT=GT, dGT=dGT, S8=S8, dS8=dS8)

        GDEPS = {}

        def expert(R):
            t0, XH, dXH, X1, dX1, IDS, dIDS, GT, dGT, S8, dS8 = (R[k] for k in
                ("t0", "XH", "dXH", "X1", "dX1", "IDS", "dIDS", "GT", "dGT", "S8", "dS8"))
            XHb, dXHb = XHbr.next()
            K.act.op(lambda e: e.activation(out=XHb[:], in_=XH[:], func=AF.Copy), reads=[dXH], writes=[dXHb])
            py, dpy = PY.next()
            NGRP = 128 // GRP
            bufs = {}
            gd = GDEPS.setdefault(id(GT), [(Dep(), Dep()) for _ in range(NGRP)])

            def stage_a(g):
                for jj in range(GRP):
                    j = g * GRP + jj
                    GB, dGB = GBr.next()
                    bufs[j] = (GB, dGB)
                    K.pool.dma(lambda e, GB=GB, j=j: e.indirect_dma_start(
                        out=GB[:], out_offset=None, in_=scr["uv_tab"][:, :],
                        in_offset=bass.IndirectOffsetOnAxis(ap=IDS[:, j:j + 1], axis=0)), reads=[dIDS, scr["d_uv"]], writes=[dGB])
                    if jj in ACTDOT:
                        PRD, dPRD = PRDr.next()
                        K.dve.op(lambda e, GB=GB, PRD=PRD: e.tensor_tensor(out=PRD[:], in0=GB[:, 0:D], in1=XHb[:], op=ALU.mult),
                                 reads=[dGB, dXHb], writes=[dPRD])
                        j3, dj3 = junk3r.next()
                        K.act.op(lambda e, PRD=PRD, j=j, j3=j3: e.activation(out=j3[:], in_=PRD[:], func=AF.Copy, accum_out=GT[:, 2, j:j + 1]),
                                 reads=[dPRD], writes=[dj3] + ([gd[g][0]] if jj == 0 else []), adds=[] if jj == 0 else [gd[g][0]])
                    else:
                        j1, dj1 = junkr.next()
                        K.dve.op(lambda e, GB=GB, j=j, j1=j1: e.scalar_tensor_tensor(
                            out=j1[:], in0=GB[:, 0:D], scalar=1.0, in1=XHb[:], op0=ALU.mult, op1=ALU.mult, accum_out=GT[:, 2, j:j + 1]),
                            reads=[dGB, dXHb], writes=[dj1] + ([gd[g][0]] if jj == 0 else []), adds=[] if jj == 0 else [gd[g][0]])
                gs = slice(g * GRP, (g + 1) * GRP)
                K.act.op(lambda e: e.activation(out=GT[:, 3, gs], in_=GT[:, 2, gs], func=AF.Gelu), reads=[gd[g][0]], writes=[gd[g][1]])

            def stage_b(g):
                gs = slice(g * GRP, (g + 1) * GRP)
                K.dve.op(lambda e: e.tensor_tensor(out=GT[:, 3, gs], in0=GT[:, 3, gs], in1=GT[:, 1, gs], op=ALU.mult), reads=[dGT], writes=[gd[g][1]])
                for jj in range(GRP):
                    j = g * GRP + jj
                    GB, dGB = bufs.pop(j)
                    DG, dDG = DGr.next()
                    K.act.op(lambda e, DG=DG, j=j: e.activation(out=DG[:], in_=identb[:], func=AF.Copy, scale=GT[:, 3, j:j + 1]),
                             reads=[gd[g][1], d_c], writes=[dDG])
                    for n in range(2):
                        K.pe.op(lambda e, DG=DG, GB=GB, n=n, j=j: e.matmul(
                            py[:, n, :], DG[:], GB[:, D + n * 512:D + (n + 1) * 512], start=(j == 0), stop=(j == 127)),
                            reads=[dDG, dGB], writes=[dpy] if (j == 0 and n == 0) else [], adds=[] if (j == 0 and n == 0) else [dpy])

            gen = routing(R["next"]) if R.get("next") is not None else None
            for g in range(NGRP):
                stage_a(g)
                if g >= 1:
                    stage_b(g - 1)
                if gen is not None and g >= 2 and g % 2 == 0:
                    try:
                        next(gen)
                    except StopIteration:
                        gen = None
            stage_b(NGRP - 1)
            if gen is not None:
                for _ in gen:
                    pass
            K.dve.op(lambda e: e.tensor_tensor(out=X1[:], in0=X1[:], in1=py[:].rearrange("p a b -> p (a b)"), op=ALU.add),
                     reads=[dpy], writes=[dX1])
            K.act.op(lambda e: e.activation(out=junk2[:], in_=X1[:], func=AF.Square, accum_out=S8[:, 0:1]),
                     reads=[dX1], writes=[d_junk2, dS8])
            K.dve.op(lambda e: e.tensor_scalar(out=S8[:, 1:2], in0=S8[:, 0:1], scalar1=1.0 / D, scalar2=1e-6,
                                               op0=ALU.mult, op1=ALU.add), writes=[dS8])
            K.act.op(lambda e: e.activation(out=S8[:, 2:3], in_=S8[:, 1:2], func=AF.Sqrt), writes=[dS8])
            K.dve.op(lambda e: e.reciprocal(out=S8[:, 3:4], in_=S8[:, 2:3]), writes=[dS8])
            K.dve.op(lambda e: e.scalar_tensor_tensor(
                out=XH[:], in0=X1[:], scalar=S8[:, 3:4], in1=FNW[:], op0=ALU.mult, op1=ALU.mult),
                reads=[dX1, dS8, d_c], writes=[dXH])
            K.sp.dma(lambda e: e.dma_start(out=io["out"][t0:t0 + 128, :], in_=XH[:]), reads=[dXH])

        for _ in routing(0):
            pass
        for ti in range(NTT):
            R = RES.pop(ti)
            R["next"] = ti + 1 if ti + 1 < NTT else None
            expert(R)


def make_consts():
    c = {}
    c["ident_bf"] = np.eye(128, dtype=np.float32).astype(ml_dtypes.bfloat16)
    c["ident_f"] = np.eye(128, dtype=np.float32)
    bo = np.zeros((128, 128), np.float32)
    bo[:64, :64] = 1.0 / 64
    bo[64:, 64:] = 1.0 / 64
    c["blockones"] = bo
    pp = np.arange(128)[:, None]
    ff = np.arange(128)[None, :]
    c["tri"] = ((pp <= ff) & (pp // 64 == ff // 64)).astype(np.float32)
    p6 = np.arange(64)[:, None]
    f6 = np.arange(64)[None, :]
    mk = np.zeros((64, 3, 64), np.float32)
    mk[:, 0, :] = (f6 < p6)
    mk[:, 1, :] = (f6 > p6)
    mk[:, 2, :] = (f6 >= p6)
    c["masks"] = mk
    c["ident64"] = np.eye(64, dtype=np.float32).astype(ml_dtypes.bfloat16)
    c["ones64"] = np.ones((64, 1), np.float32)
    c["iota16"] = np.tile(np.arange(16, dtype=np.float32)[None, :], (128, 1))
    return c


def make_alibi(S):
    al = np.zeros((4, 128, S), np.float32)
    ql = np.arange(128)[:, None]
    m = np.arange(S)[None, :]
    for h in range(4):
        slope = 2.0 ** (-8.0 * (h + 1) / 4)
        v = -slope * (ql - m + (S - 128)).astype(np.float32)
        al[h] = np.where(m <= ql + (S - 128), v, -30000.0)
    return al


def _unused():
    c = {}
    return c


def build(cfg):
    NB, S = cfg["NB"], cfg["S"]
    T = NB * S
    cfg["T"] = T
    dbg = set(cfg.get("debug", ()))
    phases = cfg.get("phases", (1,))
    nc = bass.Bass("TRN2", target_bir_lowering=False)
    io = {}

    def inp(name, shape, dt=F32):
        io[name] = nc.dram_tensor(name, list(shape), dt, kind="ExternalInput").ap()

    inp("x", [T, D])
    inp("norm_mix_w", [1, D])
    inp("w_in", [1, D, IN_COLS])
    inp("ident_bf", [128, 128], BF16)
    inp("ident_f", [128, 128])
    inp("blockones", [128, 128])
    inp("tri", [128, 128])
    inp("masks", [64, 3, 64])
    inp("ident64", [64, 64], BF16)
    inp("ones64", [64, 1])
    inp("alibi", [4, 128, S])
    for nm, shp in [("lam_q1", [1, 64]), ("lam_k1", [1, 64]), ("lam_q2", [1, 64]), ("lam_k2", [1, 64]),
                    ("subln_w", [1, 128])]:
        inp(nm, shp)
    scr = {}

    def scratch(name, shape, dt):
        kind = "ExternalOutput" if name in dbg else "Internal"
        scr[name] = nc.dram_tensor(name, list(shape), dt, kind=kind).ap()

    scratch("zs_tm", [T, SHIFT_COLS], F32)
    scratch("zv_fm", [512, T], F32)
    scratch("qk_fm", [1024, T], BF16)
    scratch("av_tm", [T, 512], BF16)
    scratch("sg_fm", [2048, T], BF16)
    if not cfg.get("chunked", True):
        scratch("rw_tm", [T, 5, 512], F32)
    scratch("ab_tm", [T, 5, 512], BF16)
    scratch("lw_tm", [T, 512], F32)
    scratch("v_fm", [512, T], F32)
    scratch("g_fm", [512, T], F32)
    scratch("coef_fm", [8, T], F32)
    scratch("y_fm", [512, T], F32)
    scratch("ya_fm", [512, T], BF16)
    scr["d_p1"] = Dep()
    scr["d_p2a"] = Dep()
    scr["d_p2b"] = Dep()
    scr["d_p2c"] = Dep()
    scr["d_p3"] = Dep()
    scr["d_p4"] = Dep()
    scr["d_uv"] = Dep()
    scratch("uv_tab", [16384, 2 * D], BF16)
    if "ids_dbg" in dbg:
        scratch("ids_dbg", [T, 128], I32)
        scratch("gate_dbg", [T, 128], F32)
    inp("peer_wq", [1, D, 2048])
    inp("peer_keys", [1, 8, 2, 128, 128])
    inp("peer_u", [1, 16384, D])
    inp("peer_v", [1, 16384, D])
    inp("final_norm_w", [1, D])
    inp("iota16", [128, 16])
    io["out"] = nc.dram_tensor("out", [T, D], F32, kind="ExternalOutput").ap()
    scratch("x1_tm", [T, D], F32)
    scratch("xh_tm", [T, D], F32)
    scratch("xhT_fm", [D, T], BF16)
    for nm, shp in [("proj_a", [1, 512, D]), ("proj_b", [1, 512, D]), ("w_out", [1, D, D]), ("norm_ffn_w", [1, D])]:
        inp(nm, shp)
    scratch("yb_fm", [512, T], BF16)
    for nm, shp in [("shift_mu", [1, SHIFT_COLS]), ("w0", [1, 512]), ("w2", [1, 64, 512]), ("a0", [1, 512]),
                    ("a2", [1, 64, 512]), ("g2", [1, 128, 512]), ("k_k", [1, 512]), ("k_a", [1, 512]),
                    ("r_k", [1, 8, 64]), ("lnx_w", [1, 512]), ("lnx_b", [1, 512])]:
        inp(nm, shp)
    with ExitStack() as es:
        K = Kern(nc, es, pool_slots=cfg.get("pool_slots", 8))
        K.scopes = bool(cfg.get("scopes", False))
        if 5 in phases:
            K.phase = "p0_uvtab"
            RB = 2048
            for r0 in range(0, 16384, RB):
                K.pool.dma(lambda e, r0=r0: e.dma_start(out=scr["uv_tab"][r0:r0 + RB, 0:D], in_=io["peer_u"][0, r0:r0 + RB, :]),
                           adds=[scr["d_uv"]])
                K.pool.dma(lambda e, r0=r0: e.dma_start(out=scr["uv_tab"][r0:r0 + RB, D:2 * D], in_=io["peer_v"][0, r0:r0 + RB, :]),
                           adds=[scr["d_uv"]])
        if 1 in phases:
            K.phase = "p1_inproj"
            phase1(K, cfg, io, scr)
            K.barrier()
        if 2 in phases:
            K.phase = "p2a_prep"
            phase2_prep(K, cfg, io, scr)
            K.barrier()
            K.phase = "p2b_scan"
            if cfg.get("chunked", True):
                phase2_chunk(K, cfg, io, scr)
            else:
                phase2_scan(K, cfg, io, scr)
            K.barrier()
            K.phase = "p2c_post"
            phase2_post(K, cfg, io, scr)
            K.barrier()
        if 3 in phases:
            K.phase = "p3_attn"
            phase3(K, cfg, io, scr)
            K.barrier()
        if 4 in phases:
            K.phase = "p4_merge"
            phase4(K, cfg, io, scr)
            K.barrier()
        if 5 in phases:
            K.phase = "p5_peer"
            phase5(K, cfg, io, scr)
            K.barrier()
        K.finish()
    return nc, io, scr


def kernel(**inputs):
    NB, S = 4, 2048
    cfg = dict(NB=NB, S=S, phases=(1, 2, 3, 4, 5))
    nc, io, scr = build(cfg)
    consts = make_consts()
    consts["alibi"] = make_alibi(S)
    x = np.ascontiguousarray(np.asarray(inputs["x"], dtype=np.float32))
    shared = {}
    for name in io:
        if name in ("x", "out"):
            continue
        if name in consts:
            shared[name] = consts[name]
        elif name == "final_norm_w":
            shared[name] = np.ascontiguousarray(np.asarray(inputs[name], dtype=np.float32).reshape(1, D))
        else:
            shared[name] = np.ascontiguousarray(np.asarray(inputs[name], dtype=np.float32))
    in_maps = []
    for c in range(NCORES):
        m = dict(shared)
        m["x"] = x[c * NB:(c + 1) * NB].reshape(NB * S, D)
        in_maps.append(m)
    res = run_bass_kernel_spmd(nc, in_maps, core_ids=list(range(NCORES)))
    out = np.concatenate([np.asarray(r["out"]).reshape(NB, S, D) for r in res.results], axis=0)
    return out.astype(np.float32)
```

```python
import numpy as np
import ml_dtypes
from contextlib import ExitStack
import concourse.bass as bass
import concourse.mybir as mybir
from concourse.bass_utils import run_bass_kernel_spmd

F32 = mybir.dt.float32
BF16 = mybir.dt.bfloat16
I32 = mybir.dt.int32
U32 = mybir.dt.uint32
ALU = mybir.AluOpType
AF = mybir.ActivationFunctionType
AX = mybir.AxisListType

D = 1024
IN_COLS = 5376
SHIFT_COLS = 1792
NCORES = 8


class Dep:
    __slots__ = ("w", "r", "pw", "pr")

    def __init__(self):
        self.w = {}
        self.r = {}
        self.pw = {}
        self.pr = {}


class Stream:
    def __init__(self, K, name, is_pe=False, ndma=0):
        self.K = K
        self.name = name
        self.sem = K.new_sem("s_" + name)
        self.cnt = 0
        self.items = []
        self.waited = {}
        self.is_pe = is_pe
        self.dsems = [K.new_sem("d_%s%d" % (name, i)) for i in range(ndma)]
        self.duses = [0] * ndma
        self.dj = 0

    def wait_tok(self, tok):
        if tok is None:
            return
        sem, val = tok
        if sem is self.sem and self.is_pe:
            return
        key = id(sem)
        if self.waited.get(key, 0) >= val:
            return
        self.waited[key] = val
        self.items.append(("w", sem, val, self.K.phase))

    def _pre(self, reads, writes, adds):
        for d in reads:
            for t in list(d.w.values()):
                self.wait_tok(t)
        for d in writes:
            for t in list(d.w.values()):
                self.wait_tok(t)
            for t in list(d.r.values()):
                self.wait_tok(t)
        for d in adds:
            for t in list(d.r.values()) + list(d.pr.values()) + list(d.pw.values()):
                self.wait_tok(t)

    def _post(self, tok, reads, writes, adds):
        for d in reads:
            d.r[id(tok[0])] = tok
        for d in writes:
            d.pw = d.w
            d.pr = d.r
            d.w = {id(tok[0]): tok}
            d.r = {}
        for d in adds:
            d.w[id(tok[0])] = tok

    def op(self, fn, reads=(), writes=(), adds=()):
        self._pre(reads, writes, adds)
        self.cnt += 1
        tok = (self.sem, self.cnt)
        self.items.append(("o", fn, self.sem, 1, self.K.phase))
        self._post(tok, reads, writes, adds)
        return tok

    def dma(self, fn, reads=(), writes=(), adds=()):
        self._pre(reads, writes, adds)
        n = len(self.dsems)
        slot = self.dj % n
        self.dj += 1
        if self.duses[slot] > 0:
            self.wait_tok((self.dsems[slot], 16 * self.duses[slot]))
        self.duses[slot] += 1
        tok = (self.dsems[slot], 16 * self.duses[slot])
        self.items.append(("o", fn, self.dsems[slot], 16, self.K.phase))
        self._post(tok, reads, writes, adds)
        return tok

    def replay(self, eng):
        nc = self.K.nc
        cur = None
        ctx = None
        for it in self.items:
            ph = it[-1]
            if self.K.scopes and ph != cur:
                if ctx is not None:
                    ctx.__exit__(None, None, None)
                ctx = nc.named_scope(ph)
                ctx.__enter__()
                cur = ph
            if it[0] == "w":
                eng.wait_ge(it[1], it[2])
            else:
                ins = it[1](eng)
                ins.then_inc(it[2], it[3])
        if ctx is not None:
            ctx.__exit__(None, None, None)


class Kern:
    def __init__(self, nc, es, pool_slots=8):
        self.nc = nc
        self.es = es
        self.nsem = 0
        self.phase = "init"
        self.scopes = False
        self.pe = Stream(self, "pe", is_pe=True)
        self.act = Stream(self, "act", ndma=4)
        self.dve = Stream(self, "dve")
        self.pool = Stream(self, "pool", ndma=pool_slots)
        self.sp = Stream(self, "sp", ndma=8)
        self.uid = 0

    def new_sem(self, name):
        self.nsem += 1
        return self.es.enter_context(self.nc.semaphore(name))

    def sb(self, shape, dt, name=None, es=None):
        self.uid += 1
        nm = "%s_%d" % (name or "t", self.uid)
        return (es or self.es).enter_context(self.nc.sbuf_tensor(nm, list(shape), dt))

    def ps(self, shape, dt, name=None, es=None):
        self.uid += 1
        nm = "%s_%d" % (name or "p", self.uid)
        return (es or self.es).enter_context(self.nc.psum_tensor(nm, list(shape), dt))

    def dram(self, name, shape, dt, kind="Internal"):
        return self.nc.dram_tensor(name, list(shape), dt, kind=kind)

    def streams(self):
        return [self.pe, self.act, self.dve, self.pool, self.sp]

    def barrier(self):
        st = self.streams()
        toks = []
        for q in st:
            if q.cnt > 0:
                toks.append((q.sem, q.cnt))
            for i, sem in enumerate(q.dsems):
                if q.duses[i] > 0:
                    toks.append((sem, 16 * q.duses[i]))
        for s_ in st:
            for t in toks:
                s_.wait_tok(t)

    def finish(self):
        streams = [self.pe, self.act, self.dve, self.pool, self.sp]
        for s in streams:
            for q in streams:
                for i, sem in enumerate(q.dsems):
                    if q.duses[i] > 0:
                        s.wait_tok((sem, 16 * q.duses[i]))
        with self.nc.allow_non_contiguous_dma(reason="small strided param loads"), self.nc.Block() as block:
            @block.tensor
            def _(e):
                self.pe.replay(e)

            @block.scalar
            def _(e):
                self.act.replay(e)

            @block.vector
            def _(e):
                self.dve.replay(e)

            @block.gpsimd
            def _(e):
                self.pool.replay(e)

            @block.sync
            def _(e):
                self.sp.replay(e)


class Rot:
    def __init__(self, K, n, shape, dt, name, psum=False, es=None):
        self.t = [(K.ps if psum else K.sb)(shape, dt, name, es=es) for _ in range(n)]
        self.d = [Dep() for _ in range(n)]
        self.i = 0

    def next(self):
        j = self.i % len(self.t)
        self.i += 1
        return self.t[j], self.d[j]


def phase1(K, cfg, io, scr):
    nc = K.nc
    T = cfg["T"]
    NT = T // 512
    with ExitStack() as es:
        ident = K.sb([128, 128], BF16, "ident", es)
        d_ident = Dep()
        K.sp.dma(lambda e: e.dma_start(out=ident[:], in_=io["ident_bf"][:, :]), writes=[d_ident])
        nw = K.sb([128, 8], F32, "nw", es)
        d_nw = Dep()
        K.sp.dma(lambda e: e.dma_start(out=nw[:], in_=io["norm_mix_w"].rearrange("o (c p) -> p (o c)", p=128)),
                 writes=[d_nw])
        wt = K.sb([128, 8, IN_COLS], BF16, "wt", es)
        d_wt = Dep()
        wst = Rot(K, 2, [128, 1344], F32, "wst", es=es)
        q = 0
        for kc in range(8):
            for cp in range(4):
                st, dst = wst.next()
                eng = K.sp if q % 2 == 0 else K.pool
                q += 1
                eng.dma(lambda e, st=st, kc=kc, cp=cp: e.dma_start(
                    out=st[:], in_=io["w_in"][0, kc * 128:(kc + 1) * 128, cp * 1344:(cp + 1) * 1344]), writes=[dst])
                K.act.op(lambda e, st=st, kc=kc, cp=cp: e.activation(
                    out=wt[:, kc, cp * 1344:(cp + 1) * 1344], in_=st[:], func=AF.Copy, scale=nw[:, kc:kc + 1]),
                    reads=[dst, d_nw], writes=[d_wt])

        xs = Rot(K, 2, [128, D], F32, "xs", es=es)
        junk = K.sb([128, D], BF16, "junk", es)
        d_junk = Dep()
        xn = Rot(K, 2, [128, D], BF16, "xn", es=es)
        st4 = Rot(K, 4, [128, 4], F32, "st4", es=es)
        hT = Rot(K, 2, [128, 8, 512], BF16, "hT", es=es)
        ptr = Rot(K, 2, [128, 8, 128], BF16, "ptr", psum=True, es=es)
        pmm = Rot(K, 4, [128, 512], F32, "pmm", psum=True, es=es)
        o32 = Rot(K, 3, [128, 512], F32, "o32", es=es)
        o16 = Rot(K, 3, [128, 512], BF16, "o16", es=es)
        ev = [0]

        def evac(pt, pd, ncols, kind, dst_ap):
            if kind == "f32":
                ot, od = o32.next()
            else:
                ot, od = o16.next()
            use_act = (kind == "sig") or (ev[0] % 2 == 0)
            ev[0] += 1
            if kind == "sig":
                K.act.op(lambda e: e.activation(out=ot[:, :ncols], in_=pt[:, :ncols], func=AF.Sigmoid),
                         reads=[pd], writes=[od])
            elif use_act:
                K.act.op(lambda e: e.activation(out=ot[:, :ncols], in_=pt[:, :ncols], func=AF.Copy),
                         reads=[pd], writes=[od])
            else:
                K.dve.op(lambda e: e.tensor_copy(out=ot[:, :ncols], in_=pt[:, :ncols]), reads=[pd], writes=[od])
            K.sp.dma(lambda e: e.dma_start(out=dst_ap, in_=ot[:, :ncols]), reads=[od], adds=[scr["d_p1"]])

        for ti in range(NT):
            h_t, h_d = hT.next()
            for sub in range(4):
                t0 = ti * 512 + sub * 128
                x_t, x_d = xs.next()
                K.pool.dma(lambda e, x_t=x_t, t0=t0: e.dma_start(out=x_t[:], in_=io["x"][t0:t0 + 128, :]),
                           writes=[x_d])
                s_t, s_d = st4.next()
                K.dve.op(lambda e, s_t=s_t: e.memset(s_t[:], 0.0), writes=[s_d])
                K.act.op(lambda e, x_t=x_t, s_t=s_t: e.activation(out=junk[:], in_=x_t[:], func=AF.Square,
                                                                    accum_out=s_t[:, 0:1]),
                         reads=[x_d], writes=[d_junk, s_d])
                K.dve.op(lambda e, s_t=s_t: e.tensor_scalar(out=s_t[:, 1:2], in0=s_t[:, 0:1], scalar1=1.0 / D,
                                                            scalar2=1e-6, op0=ALU.mult, op1=ALU.add),
                         reads=[s_d], writes=[s_d])
                K.act.op(lambda e, s_t=s_t: e.activation(out=s_t[:, 3:4], in_=s_t[:, 1:2], func=AF.Sqrt),
                         reads=[s_d], writes=[s_d])
                K.dve.op(lambda e, s_t=s_t: e.reciprocal(out=s_t[:, 2:3], in_=s_t[:, 3:4]),
                         reads=[s_d], writes=[s_d])
                n_t, n_d = xn.next()
                K.dve.op(lambda e, x_t=x_t, s_t=s_t, n_t=n_t: e.tensor_scalar(
                    out=n_t[:], in0=x_t[:], scalar1=s_t[:, 2:3], scalar2=None, op0=ALU.mult),
                    reads=[x_d, s_d], writes=[n_d])
                p_t, p_d = ptr.next()
                for kc in range(8):
                    K.pe.op(lambda e, p_t=p_t, n_t=n_t, kc=kc: e.transpose(
                        out=p_t[:, kc, :], in_=n_t[:, kc * 128:(kc + 1) * 128], identity=ident[:]),
                        reads=[n_d, d_ident], writes=[p_d])
                K.act.op(lambda e, p_t=p_t, h_t=h_t, sub=sub: e.activation(
                    out=h_t[:, :, sub * 128:(sub + 1) * 128], in_=p_t[:], func=AF.Copy),
                    reads=[p_d], writes=[h_d])
            tsl = slice(ti * 512, (ti + 1) * 512)
            for sub in range(4):
                r0 = ti * 512 + sub * 128
                for (c0, ncols, kind, name, dc0) in [(0, 512, "f32", "zs_tm", 0), (512, 512, "f32", "zs_tm", 512),
                                                    (1024, 512, "f32", "zs_tm", 1024),
                                                    (1536, 256, "f32", "zs_tm", 1536),
                                                    (2816, 512, "bf16", "av_tm", 0)]:
                    pt, pd = pmm.next()
                    for kc in range(8):
                        K.pe.op(lambda e, pt=pt, kc=kc, sub=sub, c0=c0, ncols=ncols, h_t=h_t: e.matmul(
                            pt[:, :ncols], h_t[:, kc, sub * 128:(sub + 1) * 128], wt[:, kc, c0:c0 + ncols],
                            start=(kc == 0), stop=(kc == 7)), reads=[h_d, d_wt], writes=[pd])
                    evac(pt, pd, ncols, kind, scr[name][r0:r0 + 128, dc0:dc0 + ncols])
            fm = []
            for j in range(8):
                fm.append((1792 + j * 128, "bf16", "qk_fm", j * 128))
            for j in range(16):
                fm.append((3328 + j * 128, "sig", "sg_fm", j * 128))
            for (c0, kind, name, r0) in fm:
                pt, pd = pmm.next()
                for kc in range(8):
                    K.pe.op(lambda e, pt=pt, kc=kc, c0=c0, h_t=h_t: e.matmul(
                        pt[:, :], wt[:, kc, c0:c0 + 128], h_t[:, kc, :], start=(kc == 0), stop=(kc == 7)),
                        reads=[h_d, d_wt], writes=[pd])
                evac(pt, pd, 512, kind, scr[name][r0:r0 + 128, tsl])


def dap(apobj, offset, dims):
    return bass.AP(tensor=apobj.tensor, offset=offset, ap=[list(d) for d in dims])


def bcast_load(K, eng, dst, src_row_ap, n, dep):
    eng.dma(lambda e: e.dma_start(out=dst, in_=src_row_ap.broadcast_to([128, n])), writes=[dep])


def phase2_prep(K, cfg, io, scr):
    T, S, NB = cfg["T"], cfg["S"], cfg["NB"]
    NTT = T // 128
    with ExitStack() as es:
        identb = K.sb([128, 128], BF16, "identb", es)
        identf = K.sb([128, 128], F32, "identf", es)
        d_c = Dep()
        K.sp.dma(lambda e: e.dma_start(out=identb[:], in_=io["ident_bf"][:, :]), adds=[d_c])
        K.sp.dma(lambda e: e.dma_start(out=identf[:], in_=io["ident_f"][:, :]), adds=[d_c])
        MU = K.sb([128, SHIFT_COLS], F32, "MU", es)
        PR = K.sb([128, 5, 512], F32, "PR", es)
        K.sp.dma(lambda e: e.dma_start(out=MU[:], in_=io["shift_mu"][0:1, :].broadcast_to([128, SHIFT_COLS])), adds=[d_c])
        for j, nm in enumerate(["w0", "a0", "k_k", "k_a"]):
            K.pool.dma(lambda e, j=j, nm=nm: e.dma_start(out=PR[:, j, :], in_=io[nm][0:1, :].broadcast_to([128, 512])),
                       adds=[d_c])
        K.pool.dma(lambda e: e.dma_start(out=PR[:, 4, :], in_=io["r_k"].rearrange("o h k -> o (h k)").broadcast_to([128, 512])),
                   adds=[d_c])
        cst = K.sb([128, 2], F32, "cst", es)
        K.dve.op(lambda e: e.memset(cst[:, 0:1], 1.0), adds=[d_c])
        K.dve.op(lambda e: e.memset(cst[:, 1:2], -0.5), adds=[d_c])
        wst = K.sb([128, 3, 512], F32, "lwst", es)
        d_wst = Dep()
        K.dve.op(lambda e: e.memset(wst[:], 0.0), writes=[d_wst])
        K.sp.dma(lambda e: e.dma_start(out=wst[0:64, 0, :], in_=io["w2"][0, :, :]), reads=[d_wst], adds=[d_wst])
        K.sp.dma(lambda e: e.dma_start(out=wst[64:128, 1, :], in_=io["a2"][0, :, :]), reads=[d_wst], adds=[d_wst])
        K.sp.dma(lambda e: e.dma_start(out=wst[:, 2, :], in_=io["g2"][0, :, :]), reads=[d_wst], adds=[d_wst])
        LW = K.sb([128, 3, 512], BF16, "LW", es)
        K.dve.op(lambda e: e.tensor_copy(out=LW[:], in_=wst[:]), reads=[d_wst], adds=[d_c])

        Zr = Rot(K, 2, [128, SHIFT_COLS], F32, "Z", es=es)
        Zpr = Rot(K, 2, [128, SHIFT_COLS], F32, "Zp", es=es)
        ZSr = Rot(K, 2, [128, SHIFT_COLS], F32, "ZS", es=es)
        OUTr = Rot(K, 2, [128, 5, 512], F32, "OUT", es=es)
        Er = Rot(K, 2, [128, 192], F32, "E", es=es)
        Lr = Rot(K, 2, [128, 256], BF16, "L", es=es)
        LTr = Rot(K, 2, [128, 2, 128], BF16, "LT", es=es)
        Ur = Rot(K, 2, [128, 512], F32, "U", es=es)
        UAr = Rot(K, 2, [128, 512], F32, "UA", es=es)
        KKr = Rot(K, 2, [128, 512], F32, "KKt", es=es)
        SQr = Rot(K, 2, [128, 512], F32, "SQ", es=es)
        T1r = Rot(K, 2, [128, 512], F32, "T1", es=es)
        T2r = Rot(K, 2, [128, 512], F32, "T2", es=es)
        S8r = Rot(K, 2, [128, 4, 8], F32, "S8", es=es)
        VTr = Rot(K, 2, [128, 4, 128], F32, "VT", es=es)
        GTr = Rot(K, 2, [128, 4, 128], F32, "GT", es=es)
        CTr = Rot(K, 2, [8, 128], F32, "CT", es=es)
        PT = Rot(K, 1, [128, 2, 128], BF16, "PT", psum=True, es=es)
        PW = Rot(K, 1, [128, 512], F32, "PW", psum=True, es=es)
        PA = Rot(K, 1, [128, 512], F32, "PA", psum=True, es=es)
        PG = Rot(K, 1, [128, 4, 128], F32, "PG", psum=True, es=es)
        PV = Rot(K, 1, [128, 4, 128], F32, "PV", psum=True, es=es)
        PC = Rot(K, 1, [8, 128], F32, "PC", psum=True, es=es)
        chunked = cfg.get("chunked", True)
        if chunked:
            PL = Rot(K, 1, [128, 512], F32, "PL", psum=True, es=es)
            TRI = K.sb([128, 128], F32, "TRI", es)
            K.sp.dma(lambda e: e.dma_start(out=TRI[:], in_=io["tri"][:, :]), adds=[d_c])
            LWr = Rot(K, 2, [128, 512], F32, "LWt", es=es)
            ELr = Rot(K, 2, [128, 3, 512], F32, "EL", es=es)
            ABr = Rot(K, 2, [128, 5, 512], BF16, "AB", es=es)
        dq = [0]

        def ldq():
            dq[0] += 1
            return K.sp if dq[0] % 2 == 0 else K.pool

        def tile_gen(ti):
            t0 = ti * 128
            first = (t0 % S == 0)
            Z, dZ = Zr.next()
            Zp, dZp = Zpr.next()
            ZS, dZS = ZSr.next()
            OUT, dO = OUTr.next()
            ldq().dma(lambda e, Z=Z, t0=t0: e.dma_start(out=Z[:], in_=scr["zs_tm"][t0:t0 + 128, :]),
                      reads=[scr["d_p1"]], writes=[dZ])
            if first:
                K.pool.op(lambda e, Zp=Zp: e.memset(Zp[0:32, :], 0.0), writes=[dZp])
                ldq().dma(lambda e, Zp=Zp, t0=t0: e.dma_start(out=Zp[1:128, :], in_=scr["zs_tm"][t0:t0 + 127, :]),
                          reads=[scr["d_p1"], dZp], adds=[dZp])
            else:
                ldq().dma(lambda e, Zp=Zp, t0=t0: e.dma_start(out=Zp[:], in_=scr["zs_tm"][t0 - 1:t0 + 127, :]),
                          reads=[scr["d_p1"]], writes=[dZp])
            CS = 1216
            dZSa, dZSb = Dep(), Dep()
            K.dve.op(lambda e, Z=Z, Zp=Zp, ZS=ZS: e.tensor_tensor(out=ZS[:, :CS], in0=Zp[:, :CS], in1=Z[:, :CS], op=ALU.subtract),
                     reads=[dZ, dZp], writes=[dZS])
            K.pool.op(lambda e, Z=Z, Zp=Zp, ZS=ZS: e.tensor_tensor(out=ZS[:, CS:], in0=Zp[:, CS:], in1=Z[:, CS:], op=ALU.subtract),
                      reads=[dZ, dZp, dZS], writes=[dZSb])
            K.dve.op(lambda e, ZS=ZS: e.tensor_tensor(out=ZS[:, :CS], in0=ZS[:, :CS], in1=MU[:, :CS], op=ALU.mult),
                     reads=[d_c, dZS], writes=[dZSa])
            K.pool.op(lambda e, ZS=ZS: e.tensor_tensor(out=ZS[:, CS:], in0=ZS[:, CS:], in1=MU[:, CS:], op=ALU.mult),
                      reads=[d_c], writes=[dZSb])
            K.dve.op(lambda e, Z=Z, ZS=ZS: e.tensor_tensor(out=ZS[:, :CS], in0=ZS[:, :CS], in1=Z[:, :CS], op=ALU.add),
                     reads=[dZ], writes=[dZSa])
            K.pool.op(lambda e, Z=Z, ZS=ZS: e.tensor_tensor(out=ZS[:, CS:], in0=ZS[:, CS:], in1=Z[:, CS:], op=ALU.add),
                      reads=[dZ], writes=[dZSb])
            K.dve.op(lambda e, ZS=ZS: e.tensor_copy(out=ZS[:, 0:1], in_=ZS[:, 0:1]), reads=[dZSa, dZSb], writes=[dZS])
            r_ap = ZS[:, 0:512]
            k_ap = ZS[:, 512:1024]
            K.act.op(lambda e, OUT=OUT, ZS=ZS: e.activation(out=OUT[:, 4, :], in_=ZS[:, 0:512], func=AF.Copy),
                     reads=[dZS], writes=[dO])
            yield
            E, dE = Er.next()
            L, dL = Lr.next()
            K.act.op(lambda e, E=E, ZS=ZS: e.activation(out=E[:, 0:64], in_=ZS[:, 1536:1600], func=AF.Exp, scale=-2.0),
                     reads=[dZS], writes=[dE])
            K.act.op(lambda e, E=E, ZS=ZS: e.activation(out=E[:, 64:192], in_=ZS[:, 1664:1792], func=AF.Exp, scale=-1.0),
                     reads=[dZS], adds=[dE])
            K.act.op(lambda e, E=E: e.activation(out=E[:], in_=E[:], func=AF.Ln, bias=cst[:, 0:1]), reads=[d_c], writes=[dE])
            K.act.op(lambda e, E=E: e.activation(out=E[:], in_=E[:], func=AF.Exp, scale=-1.0), writes=[dE])
            K.dve.op(lambda e, E=E, L=L: e.tensor_scalar(out=L[:, 0:64], in0=E[:, 0:64], scalar1=2.0, scalar2=-1.0,
                                                        op0=ALU.mult, op1=ALU.add), reads=[dE], writes=[dL])
            K.act.op(lambda e, L=L, ZS=ZS: e.activation(out=L[:, 64:128], in_=ZS[:, 1600:1664], func=AF.Copy),
                     reads=[dZS, dL], adds=[dL])
            K.act.op(lambda e, L=L, E=E: e.activation(out=L[:, 128:256], in_=E[:, 64:192], func=AF.Copy),
                     reads=[dE, dL], adds=[dL])
            yield
            pt, dpt = PT.next()
            K.pe.op(lambda e, pt=pt, L=L: e.transpose(out=pt[:, 0, :], in_=L[:, 0:128], identity=identb[:]),
                    reads=[dL, d_c], writes=[dpt])
            K.pe.op(lambda e, pt=pt, L=L: e.transpose(out=pt[:, 1, :], in_=L[:, 128:256], identity=identb[:]),
                    reads=[dL, d_c], adds=[dpt])
            LT, dLT = LTr.next()
            K.act.op(lambda e, pt=pt, LT=LT: e.activation(out=LT[:], in_=pt[:], func=AF.Copy), reads=[dpt], writes=[dLT])
            yield
            pw, dpw = PW.next()
            pa, dpa = PA.next()
            pg, dpg = PG.next()
            K.pe.op(lambda e, pw=pw, LT=LT: e.matmul(pw[:], LT[:, 0, :], LW[:, 0, :], start=True, stop=True),
                    reads=[dLT, d_c], writes=[dpw])
            K.pe.op(lambda e, pa=pa, LT=LT: e.matmul(pa[:], LT[:, 0, :], LW[:, 1, :], start=True, stop=True),
                    reads=[dLT, d_c], writes=[dpa])
            for j in range(4):
                K.pe.op(lambda e, pg=pg, LT=LT, j=j: e.matmul(pg[:, j, :], LW[:, 2, j * 128:(j + 1) * 128], LT[:, 1, :],
                                                             start=True, stop=True),
                        reads=[dLT, d_c], writes=[dpg] if j == 0 else [], adds=[] if j == 0 else [dpg])
            GT, dGT = GTr.next()
            K.act.op(lambda e, pg=pg, GT=GT: e.activation(out=GT[:], in_=pg[:], func=AF.Copy), reads=[dpg], writes=[dGT])
            K.sp.dma(lambda e, GT=GT, t0=t0: e.dma_start(
                out=dap(scr["g_fm"], t0, [[T, 128], [128 * T, 4], [1, 128]]), in_=GT[:]),
                reads=[dGT], adds=[scr["d_p2a"]])
            yield
            U, dU = Ur.next()
            K.dve.op(lambda e, U=U, pw=pw: e.tensor_tensor(out=U[:], in0=pw[:], in1=PR[:, 0, :], op=ALU.add),
                     reads=[dpw, d_c], writes=[dU])
            K.act.op(lambda e, U=U: e.activation(out=U[:], in_=U[:], func=AF.Exp, scale=-1.0), writes=[dU])
            K.act.op(lambda e, U=U: e.activation(out=U[:], in_=U[:], func=AF.Ln, bias=cst[:, 0:1]), reads=[d_c], writes=[dU])
            K.act.op(lambda e, U=U: e.activation(out=U[:], in_=U[:], func=AF.Exp, scale=-1.0, bias=cst[:, 1:2]),
                     reads=[d_c], writes=[dU])
            K.act.op(lambda e, U=U, OUT=OUT: e.activation(out=OUT[:, 0, :], in_=U[:], func=AF.Exp, scale=-1.0),
                     reads=[dU], adds=[dO])
            yield
            UA, dUA = UAr.next()
            K.dve.op(lambda e, UA=UA, pa=pa: e.tensor_tensor(out=UA[:], in0=pa[:], in1=PR[:, 1, :], op=ALU.add),
                     reads=[dpa, d_c], writes=[dUA])
            K.act.op(lambda e, UA=UA: e.activation(out=UA[:], in_=UA[:], func=AF.Exp, scale=-1.0), writes=[dUA])
            K.act.op(lambda e, UA=UA: e.activation(out=UA[:], in_=UA[:], func=AF.Ln, bias=cst[:, 0:1]), reads=[d_c], writes=[dUA])
            K.act.op(lambda e, UA=UA: e.activation(out=UA[:], in_=UA[:], func=AF.Exp, scale=-1.0), writes=[dUA])
            yield
            KKt, dKK = KKr.next()
            SQ, dSQ = SQr.next()
            S8, dS8 = S8r.next()
            K.dve.op(lambda e, KKt=KKt, ZS=ZS: e.tensor_tensor(out=KKt[:], in0=ZS[:, 512:1024], in1=PR[:, 2, :], op=ALU.mult),
                     reads=[dZS, d_c], writes=[dKK])
            K.pool.op(lambda e, KKt=KKt, SQ=SQ: e.tensor_tensor(out=SQ[:], in0=KKt[:], in1=KKt[:], op=ALU.mult),
                      reads=[dKK], writes=[dSQ])
            K.dve.op(lambda e, SQ=SQ, S8=S8: e.tensor_reduce(out=S8[:, 0, :], in_=SQ[:].rearrange("p (h k) -> p h k", k=64),
                                                            axis=AX.X, op=ALU.add), reads=[dSQ], writes=[dS8])
            K.dve.op(lambda e, S8=S8: e.tensor_scalar(out=S8[:, 0, :], in0=S8[:, 0, :], scalar1=1e-24, scalar2=None,
                                                      op0=ALU.max), writes=[dS8])
            K.act.op(lambda e, S8=S8: e.activation(out=S8[:, 1, :], in_=S8[:, 0, :], func=AF.Ln), writes=[dS8])
            K.act.op(lambda e, S8=S8: e.activation(out=S8[:, 2, :], in_=S8[:, 1, :], func=AF.Exp, scale=-0.5), writes=[dS8])
            K.dve.op(lambda e, KKt=KKt, S8=S8, OUT=OUT: e.tensor_tensor(
                out=OUT[:, 1, :].rearrange("p (h k) -> p h k", k=64), in0=KKt[:].rearrange("p (h k) -> p h k", k=64),
                in1=S8[:, 2, :].unsqueeze(2).broadcast_to([128, 8, 64]), op=ALU.mult),
                reads=[dKK, dS8, dO], adds=[dO])
            K.dve.op(lambda e, OUT=OUT, UA=UA: e.scalar_tensor_tensor(
                out=OUT[:, 2, :], in0=OUT[:, 1, :], scalar=-1.0, in1=UA[:], op0=ALU.mult, op1=ALU.mult),
                reads=[dUA, dO], adds=[dO])
            yield
            T1, dT1 = T1r.next()
            K.dve.op(lambda e, T1=T1, UA=UA: e.scalar_tensor_tensor(
                out=T1[:], in0=UA[:], scalar=-1.0, in1=PR[:, 3, :], op0=ALU.add, op1=ALU.mult),
                reads=[dUA, d_c], writes=[dT1])
            K.dve.op(lambda e, T1=T1, OUT=OUT, ZS=ZS: e.scalar_tensor_tensor(
                out=OUT[:, 3, :], in0=T1[:], scalar=1.0, in1=ZS[:, 512:1024], op0=ALU.add, op1=ALU.mult),
                reads=[dT1, dZS, dO], adds=[dO])
            T2, dT2 = T2r.next()
            K.pool.op(lambda e, T2=T2, OUT=OUT, ZS=ZS: e.tensor_tensor(out=T2[:], in0=OUT[:, 3, :], in1=ZS[:, 0:512], op=ALU.mult),
                      reads=[dO, dZS], writes=[dT2])
            K.pool.op(lambda e, T2=T2: e.tensor_tensor(out=T2[:], in0=T2[:], in1=PR[:, 4, :], op=ALU.mult),
                      reads=[d_c], writes=[dT2])
            K.dve.op(lambda e, T2=T2, S8=S8: e.tensor_reduce(out=S8[:, 3, :], in_=T2[:].rearrange("p (h k) -> p h k", k=64),
                                                            axis=AX.X, op=ALU.add), reads=[dT2], writes=[dS8])
            yield
            pv, dpv = PV.next()
            pc, dpc = PC.next()
            for j in range(4):
                K.pe.op(lambda e, pv=pv, ZS=ZS, j=j: e.transpose(out=pv[:, j, :], in_=ZS[:, 1024 + j * 128:1024 + (j + 1) * 128],
                                                                identity=identf[:]),
                        reads=[dZS, d_c], writes=[dpv] if j == 0 else [], adds=[] if j == 0 else [dpv])
            K.pe.op(lambda e, pc=pc, S8=S8: e.transpose(out=pc[:, :], in_=S8[:, 3, :], identity=identf[:]),
                    reads=[dS8, d_c], writes=[dpc])
            VT, dVT = VTr.next()
            CT, dCT = CTr.next()
            K.act.op(lambda e, pv=pv, VT=VT: e.activation(out=VT[:], in_=pv[:], func=AF.Copy), reads=[dpv], writes=[dVT])
            K.act.op(lambda e, pc=pc, CT=CT: e.activation(out=CT[:], in_=pc[:], func=AF.Copy), reads=[dpc], writes=[dCT])
            K.sp.dma(lambda e, VT=VT, t0=t0: e.dma_start(
                out=dap(scr["v_fm"], t0, [[T, 128], [128 * T, 4], [1, 128]]), in_=VT[:]),
                reads=[dVT], adds=[scr["d_p2a"]])
            K.sp.dma(lambda e, CT=CT, t0=t0: e.dma_start(out=scr["coef_fm"][:, t0:t0 + 128], in_=CT[:]),
                     reads=[dCT], adds=[scr["d_p2a"]])
            yield
            if not chunked:
                K.pool.dma(lambda e, OUT=OUT, t0=t0: e.dma_start(out=scr["rw_tm"][t0:t0 + 128, :, :], in_=OUT[:]),
                           reads=[dO], adds=[scr["d_p2a"]])
            else:
                LWt, dLW = LWr.next()
                K.dve.op(lambda e, LWt=LWt, U=U: e.tensor_scalar(out=LWt[:], in0=U[:], scalar1=-1.0, scalar2=None, op0=ALU.mult),
                         reads=[dU], writes=[dLW])
                pl, dpl = PL.next()
                K.pe.op(lambda e, pl=pl, LWt=LWt: e.matmul(pl[:], TRI[:], LWt[:], start=True, stop=True),
                        reads=[dLW, d_c], writes=[dpl])
                EL, dEL = ELr.next()
                K.act.op(lambda e, EL=EL, pl=pl: e.activation(out=EL[:, 0, :], in_=pl[:], func=AF.Exp), reads=[dpl], writes=[dEL])
                K.act.op(lambda e, EL=EL, pl=pl: e.activation(out=EL[:, 1, :], in_=pl[:], func=AF.Exp, scale=-1.0),
                         reads=[dpl], adds=[dEL])
                K.dve.op(lambda e, EL=EL, pl=pl, U=U: e.tensor_tensor(out=EL[:, 2, :], in0=pl[:], in1=U[:], op=ALU.add),
                         reads=[dpl, dU, dEL], adds=[dEL])
                K.act.op(lambda e, EL=EL: e.activation(out=EL[:, 2, :], in_=EL[:, 2, :], func=AF.Exp), reads=[dEL], adds=[dEL])
                AB, dAB = ABr.next()
                K.dve.op(lambda e, AB=AB, OUT=OUT, EL=EL: e.tensor_tensor(out=AB[:, 0, :], in0=OUT[:, 1, :], in1=EL[:, 2, :], op=ALU.mult),
                         reads=[dO, dEL], writes=[dAB])
                K.dve.op(lambda e, AB=AB, OUT=OUT, EL=EL: e.scalar_tensor_tensor(
                    out=AB[:, 1, :], in0=OUT[:, 2, :], scalar=-1.0, in1=EL[:, 1, :], op0=ALU.mult, op1=ALU.mult),
                    reads=[dO, dEL, dAB], adds=[dAB])
                K.pool.op(lambda e, AB=AB, OUT=OUT, EL=EL: e.tensor_tensor(out=AB[:, 2, :], in0=OUT[:, 3, :], in1=EL[:, 1, :], op=ALU.mult),
                          reads=[dO, dEL, dAB], adds=[dAB])
                K.pool.op(lambda e, AB=AB, OUT=OUT, EL=EL: e.tensor_tensor(out=AB[:, 3, :], in0=OUT[:, 4, :], in1=EL[:, 0, :], op=ALU.mult),
                          reads=[dO, dEL, dAB], adds=[dAB])
                K.act.op(lambda e, AB=AB, ZS=ZS: e.activation(out=AB[:, 4, :], in_=ZS[:, 1024:1536], func=AF.Copy),
                         reads=[dZS, dAB], adds=[dAB])
                K.pool.dma(lambda e, AB=AB, t0=t0: e.dma_start(out=scr["ab_tm"][t0:t0 + 128, :, :], in_=AB[:]),
                           reads=[dAB], adds=[scr["d_p2a"]])
                K.sp.dma(lambda e, LWt=LWt, t0=t0: e.dma_start(out=scr["lw_tm"][t0:t0 + 128, :], in_=LWt[:]),
                         reads=[dLW], adds=[scr["d_p2a"]])

        LAG = cfg.get("prep_lag", 3)
        active = []
        nxt = 0
        while nxt < NTT or active:
            if len(active) < 2 and nxt < NTT and (not active or active[0][1] >= LAG):
                active.append([tile_gen(nxt), 0])
                nxt += 1
            for a in list(active):
                try:
                    next(a[0])
                    a[1] += 1
                except StopIteration:
                    active.remove(a)


def phase2_scan(K, cfg, io, scr):
    T, S, NB = cfg["T"], cfg["S"], cfg["NB"]
    NBH = 2 if NB >= 2 else 1
    NBL = NB // NBH
    NP = 64 * NBH
    TS = 2
    TC = 128
    RW = 2560
    with ExitStack() as es:
        St = K.sb([128, NBL, 8, 64], F32, "St", es)
        dS = Dep()
        TMP = K.sb([128, NBL, 8, 64], F32, "TMP", es)
        dT = Dep()
        SA = K.sb([128, NBL, 8], F32, "SA", es)
        dSA = Dep()
        T2r = Rot(K, 2, [128, NBL, 8, 64], F32, "TMP2", es=es)
        T3r = Rot(K, 2, [128, NBL, 8, 64], F32, "TMP3", es=es)
        BCr = Rot(K, 3, [128, TS, NBL, 5, 8, 64], F32, "BC", es=es)
        Vr = Rot(K, 2, [128, NBL, 8, TC], F32, "Vf", es=es)
        Yr = Rot(K, 2, [128, NBL, 8, TC], F32, "Yf", es=es)
        K.dve.op(lambda e: e.memset(St[:], 0.0), writes=[dS])
        qi = [0]

        def q():
            qi[0] += 1
            return K.sp if qi[0] % 2 == 0 else K.act

        def load_bc(ci):
            t = ci * TS
            BC, dBC = BCr.next()
            first = True
            for bhi in range(NBH):
                for blo in range(NBL):
                    src = dap(scr["rw_tm"], ((bhi * NBL + blo) * S + t) * RW, [[0, 64], [RW, TS], [1, RW]])
                    dst = BC[bhi * 64:(bhi + 1) * 64, :, blo].rearrange("p t j h k -> p t (j h k)")
                    q().dma(lambda e, src=src, dst=dst: e.dma_start(out=dst, in_=src), reads=[scr["d_p2a"]],
                            writes=[dBC] if first else [], adds=[] if first else [dBC])
                    first = False
            return BC, dBC

        def vy_ap(name, bhi, blo, t):
            return dap(scr[name], (bhi * NBL + blo) * S + t, [[T, 64], [64 * T, 8], [1, TC]])

        def load_v(ni):
            Vf, dV = Vr.next()
            first = True
            for bhi in range(NBH):
                for blo in range(NBL):
                    src = vy_ap("v_fm", bhi, blo, ni * TC)
                    dst = Vf[bhi * 64:(bhi + 1) * 64, blo]
                    q().dma(lambda e, src=src, dst=dst: e.dma_start(out=dst, in_=src), reads=[scr["d_p2a"]],
                            writes=[dV] if first else [], adds=[] if first else [dV])
                    first = False
            return Vf, dV

        nch = S // TS
        bcs = {}
        bcs[0] = load_bc(0)
        if nch > 1:
            bcs[1] = load_bc(1)
        vs = {0: load_v(0)}
        P = slice(0, NP)
        for t in range(S):
            ci, ts = divmod(t, TS)
            ni, tt = divmod(t, TC)
            if ts == 0 and ci + 2 < nch:
                bcs[ci + 2] = load_bc(ci + 2)
            if tt == 0:
                if (ni + 1) * TC < S:
                    vs[ni + 1] = load_v(ni + 1)
                Yf, dY = Yr.next()
            BC, dBC = bcs[ci]
            Vf, dV = vs[ni]
            W_ = BC[P, ts, :, 0]
            KN = BC[P, ts, :, 1]
            KA = BC[P, ts, :, 2]
            KP = BC[P, ts, :, 3]
            R_ = BC[P, ts, :, 4]
            shp = [NP, NBL, 8, 64]
            K.dve.op(lambda e, KN=KN: e.tensor_tensor(out=TMP[P], in0=St[P], in1=KN, op=ALU.mult),
                     reads=[dS, dBC], writes=[dT])
            K.dve.op(lambda e: e.tensor_reduce(out=SA[P], in_=TMP[P], axis=AX.X, op=ALU.add), reads=[dT], writes=[dSA])
            K.dve.op(lambda e, W_=W_: e.tensor_tensor(out=St[P], in0=St[P], in1=W_, op=ALU.mult),
                     reads=[dBC], writes=[dS])
            K.dve.op(lambda e, KA=KA: e.tensor_tensor(out=TMP[P], in0=KA, in1=SA[P].unsqueeze(3).broadcast_to(shp),
                                                     op=ALU.mult), reads=[dBC, dSA], writes=[dT])
            K.dve.op(lambda e: e.tensor_tensor(out=St[P], in0=St[P], in1=TMP[P], op=ALU.add), reads=[dT], writes=[dS])
            T2, dT2 = T2r.next()
            K.pool.op(lambda e, KP=KP, T2=T2, Vf=Vf, tt=tt: e.tensor_tensor(
                out=T2[P], in0=KP, in1=Vf[P, :, :, tt:tt + 1].broadcast_to(shp), op=ALU.mult),
                reads=[dBC, dV], writes=[dT2])
            K.dve.op(lambda e, T2=T2: e.tensor_tensor(out=St[P], in0=St[P], in1=T2[P], op=ALU.add),
                     reads=[dT2], writes=[dS])
            T3, dT3 = T3r.next()
            K.pool.op(lambda e, T3=T3, R_=R_: e.tensor_tensor(out=T3[P], in0=St[P], in1=R_, op=ALU.mult),
                      reads=[dS, dBC], writes=[dT3])
            K.dve.op(lambda e, T3=T3, Yf=Yf, tt=tt: e.tensor_reduce(out=Yf[P, :, :, tt], in_=T3[P], axis=AX.X, op=ALU.add),
                      reads=[dT3], writes=[dY] if tt == 0 else [], adds=[] if tt == 0 else [dY])
            if tt == TC - 1:
                for bhi in range(NBH):
                    for blo in range(NBL):
                        dst = vy_ap("y_fm", bhi, blo, ni * TC)
                        srcp = Yf[bhi * 64:(bhi + 1) * 64, blo]
                        K.sp.dma(lambda e, dst=dst, srcp=srcp: e.dma_start(out=dst, in_=srcp), reads=[dY],
                                 adds=[scr["d_p2b"]])


def phase2_chunk(K, cfg, io, scr):
    T, S, NB = cfg["T"], cfg["S"], cfg["NB"]
    C = 64
    NCH = S // C
    with ExitStack() as es:
        d_c = Dep()
        id64 = K.sb([64, 64], BF16, "c_id64", es)
        K.sp.dma(lambda e: e.dma_start(out=id64[:], in_=io["ident64"][:, :]), adds=[d_c])
        MK = K.sb([64, 3, 64], F32, "c_MK", es)
        K.sp.dma(lambda e: e.dma_start(out=MK[:], in_=io["masks"][:, :, :]), adds=[d_c])
        ONES = K.sb([64, 1], F32, "c_ones", es)
        K.sp.dma(lambda e: e.dma_start(out=ONES[:], in_=io["ones64"][:, :]), adds=[d_c])
        IDF = K.sb([64, 8, 64], F32, "c_IDF", es)
        K.sp.dma(lambda e: e.dma_start(out=IDF[:], in_=io["ident_f"][0:64, 0:64].unsqueeze(1).broadcast_to([64, 8, 64])), adds=[d_c])
        ST = [K.sb([64, 8, 64], F32, "c_S%d" % b, es) for b in range(NB)]
        STb = [K.sb([64, 8, 64], BF16, "c_Sb%d" % b, es) for b in range(NB)]
        dST = [Dep() for _ in range(NB)]
        dSTb = [Dep() for _ in range(NB)]
        for b in range(NB):
            K.dve.op(lambda e, b=b: e.memset(ST[b][:], 0.0), writes=[dST[b]])
            K.pool.op(lambda e, b=b: e.memset(STb[b][:], 0.0), writes=[dSTb[b]])
        TMr = Rot(K, 3, [64, 5, 512], BF16, "c_TM", es=es)
        LWr = Rot(K, 3, [64, 512], F32, "c_LW", es=es)
        FMr = Rot(K, 2, [64, 4, 8, 64], BF16, "c_FM", es=es)
        PCr = Rot(K, 2, [64, 8], F32, "c_PC", es=es)
        Nr = Rot(K, 3, [64, 8, 64], BF16, "c_N", es=es)
        NTr = Rot(K, 3, [64, 8, 64], BF16, "c_NT", es=es)
        MTr = Rot(K, 3, [64, 8, 64], BF16, "c_MT", es=es)
        MTfr = Rot(K, 2, [64, 8, 64], F32, "c_MTf", es=es)
        NAKr = Rot(K, 2, [64, 8, 64], BF16, "c_NAK", es=es)
        MRBr = Rot(K, 2, [64, 8, 64], BF16, "c_MRB", es=es)
        MRKr = Rot(K, 2, [64, 8, 64], BF16, "c_MRK", es=es)
        Xr = Rot(K, 2, [64, 8, 64], BF16, "c_X", es=es)
        NUr = Rot(K, 2, [64, 8, 64], BF16, "c_NU", es=es)
        Yr = Rot(K, 2, [64, 8, 64], F32, "c_Y", es=es)
        TSr = Rot(K, 2, [64, 8, 64], F32, "c_TS", es=es)
        PTf = Rot(K, 1, [64, 4, 8, 64], BF16, "c_PTf", psum=True, es=es)
        PA = Rot(K, 4, [64, 8, 64], F32, "c_PA", psum=True, es=es)
        PPC = Rot(K, 1, [64, 8], F32, "c_PPC", psum=True, es=es)
        ce = [0]

        def evac_copy(dst_ap, src_ap, reads, writes=(), adds=(), scale=None):
            ce[0] += 1
            if scale is not None or ce[0] % 2 == 0:
                if scale is None:
                    K.act.op(lambda e: e.activation(out=dst_ap, in_=src_ap, func=AF.Copy), reads=reads, writes=writes, adds=adds)
                else:
                    K.act.op(lambda e: e.activation(out=dst_ap, in_=src_ap, func=AF.Copy, scale=scale), reads=reads, writes=writes, adds=adds)
            else:
                K.dve.op(lambda e: e.tensor_copy(out=dst_ap, in_=src_ap), reads=reads, writes=writes, adds=adds)

        def mm8(pt, dpt, lhs_fn, rhs_fn, reads, first=True, last=True, wr=True):
            mmN(pt, dpt, [(lhs_fn, rhs_fn)], reads)

        def mmN(pt, dpt, terms, reads):
            n = len(terms)
            for h in range(8):
                for i, (lf, rf) in enumerate(terms):
                    K.pe.op(lambda e, h=h, lf=lf, rf=rf, i=i: e.matmul(pt[:, h, :], lf(h), rf(h), start=(i == 0), stop=(i == n - 1)),
                            reads=reads, writes=[dpt] if (h == 0 and i == 0) else [], adds=[] if (h == 0 and i == 0) else [dpt])

        q = [0]

        def dq():
            q[0] += 1
            return K.sp if q[0] % 2 == 0 else K.pool

        for ci in range(NCH):
            for b in range(NB):
                t0 = b * S + ci * C
                TM, dTM = TMr.next()
                LW, dLW = LWr.next()
                dq().dma(lambda e, TM=TM, t0=t0: e.dma_start(out=TM[:], in_=scr["ab_tm"][t0:t0 + C, :, :]),
                         reads=[scr["d_p2a"]], writes=[dTM])
                dq().dma(lambda e, LW=LW, t0=t0: e.dma_start(out=LW[:], in_=scr["lw_tm"][t0:t0 + C, :]),
                         reads=[scr["d_p2a"]], writes=[dLW])
                ptf, dptf = PTf.next()
                first = True
                for j in range(4):
                    for h in range(8):
                        K.pe.op(lambda e, ptf=ptf, TM=TM, j=j, h=h: e.transpose(
                            out=ptf[:, j, h, :], in_=TM[:, j, h * 64:(h + 1) * 64], identity=id64[:]),
                            reads=[dTM, d_c], writes=[dptf] if first else [], adds=[] if first else [dptf])
                        first = False
                FM, dFM = FMr.next()
                K.act.op(lambda e, FM=FM, ptf=ptf: e.activation(out=FM[:, 0:2], in_=ptf[:, 0:2], func=AF.Copy), reads=[dptf], writes=[dFM])
                K.dve.op(lambda e, FM=FM, ptf=ptf: e.tensor_copy(out=FM[:, 2:4], in_=ptf[:, 2:4]), reads=[dptf, dFM], adds=[dFM])
                Af = lambda h, FM=FM: FM[:, 0, h, :]
                Bf = lambda h, FM=FM: FM[:, 1, h, :]
                Kf = lambda h, FM=FM: FM[:, 2, h, :]
                Rf = lambda h, FM=FM: FM[:, 3, h, :]
                Vt = lambda h, TM=TM: TM[:, 4, h * 64:(h + 1) * 64]
                Bt = lambda h, TM=TM: TM[:, 1, h * 64:(h + 1) * 64]
                Kt = lambda h, TM=TM: TM[:, 2, h * 64:(h + 1) * 64]
                ppc, dppc = PPC.next()
                for h in range(8):
                    K.pe.op(lambda e, ppc=ppc, LW=LW, h=h: e.matmul(ppc[:, h:h + 1], LW[:, h * 64:(h + 1) * 64], ONES[:], start=True, stop=True),
                            reads=[dLW, d_c], writes=[dppc] if h == 0 else [], adds=[] if h == 0 else [dppc])
                PCt, dPC = PCr.next()
                K.act.op(lambda e, PCt=PCt, ppc=ppc: e.activation(out=PCt[:], in_=ppc[:], func=AF.Exp), reads=[dppc], writes=[dPC])
                mbc = lambda i: MK[:, i, :].unsqueeze(1).broadcast_to([64, 8, 64])
                pa, dpa = PA.next()
                mm8(pa, dpa, Af, Bf, [dFM])
                N0, dN0 = Nr.next()
                K.dve.op(lambda e, N0=N0, pa=pa: e.tensor_tensor(out=N0[:], in0=pa[:], in1=mbc(0), op=ALU.mult), reads=[dpa, d_c], writes=[dN0])
                pa, dpa = PA.next()
                mm8(pa, dpa, Bf, Af, [dFM])
                NT0, dNT0 = NTr.next()
                MTf, dMTf = MTfr.next()
                K.dve.op(lambda e, NT0=NT0, pa=pa: e.tensor_tensor(out=NT0[:], in0=pa[:], in1=mbc(1), op=ALU.mult), reads=[dpa, d_c], writes=[dNT0])
                K.pool.op(lambda e, MTf=MTf, NT0=NT0: e.tensor_tensor(out=MTf[:], in0=IDF[:], in1=NT0[:], op=ALU.subtract),
                          reads=[dNT0, d_c], writes=[dMTf])
                MT, dMT = MTr.next()
                K.act.op(lambda e, MT=MT, MTf=MTf: e.activation(out=MT[:], in_=MTf[:], func=AF.Copy), reads=[dMTf], writes=[dMT])
                pa, dpa = PA.next()
                mm8(pa, dpa, Kf, Af, [dFM])
                NAK, dNAK = NAKr.next()
                K.dve.op(lambda e, NAK=NAK, pa=pa: e.tensor_tensor(out=NAK[:], in0=pa[:], in1=mbc(1), op=ALU.mult), reads=[dpa, d_c], writes=[dNAK])
                pa, dpa = PA.next()
                mm8(pa, dpa, Bf, Rf, [dFM])
                MRB, dMRB = MRBr.next()
                K.dve.op(lambda e, MRB=MRB, pa=pa: e.tensor_tensor(out=MRB[:], in0=pa[:], in1=mbc(2), op=ALU.mult), reads=[dpa, d_c], writes=[dMRB])
                pa, dpa = PA.next()
                mm8(pa, dpa, Kf, Rf, [dFM])
                MRK, dMRK = MRKr.next()
                K.dve.op(lambda e, MRK=MRK, pa=pa: e.tensor_tensor(out=MRK[:], in0=pa[:], in1=mbc(2), op=ALU.mult), reads=[dpa, d_c], writes=[dMRK])
                Np, dNp, NTp, dNTp = N0, dN0, NT0, dNT0
                for lvl in range(1, 6):
                    pa, dpa = PA.next()
                    mm8(pa, dpa, lambda h, NTp=NTp: NTp[:, h, :], lambda h, Np=Np: Np[:, h, :], [dNp, dNTp])
                    Nn, dNn = Nr.next()
                    evac_copy(Nn[:], pa[:], [dpa], writes=[dNn])
                    if lvl < 5:
                        pa2, dpa2 = PA.next()
                        mm8(pa2, dpa2, lambda h, Np=Np: Np[:, h, :], lambda h, NTp=NTp: NTp[:, h, :], [dNp, dNTp])
                        NTn, dNTn = NTr.next()
                        evac_copy(NTn[:], pa2[:], [dpa2], writes=[dNTn])
                    pa3, dpa3 = PA.next()
                    mm8(pa3, dpa3, lambda h, Nn=Nn: Nn[:, h, :], lambda h, MT=MT: MT[:, h, :], [dNn, dMT])
                    K.dve.op(lambda e, MTf=MTf, pa3=pa3: e.tensor_tensor(out=MTf[:], in0=MTf[:], in1=pa3[:], op=ALU.add),
                             reads=[dpa3], writes=[dMTf])
                    MT, dMT = MTr.next()
                    K.act.op(lambda e, MT=MT, MTf=MTf: e.activation(out=MT[:], in_=MTf[:], func=AF.Copy), reads=[dMTf], writes=[dMT])
                    Np, dNp = Nn, dNn
                    if lvl < 5:
                        NTp, dNTp = NTn, dNTn
                Sb = STb[b]
                pa, dpa = PA.next()
                mmN(pa, dpa, [(Af, lambda h, Sb=Sb: Sb[:, h, :]), (lambda h, NAK=NAK: NAK[:, h, :], Vt)], [dFM, dSTb[b], dNAK, dTM])
                X, dX = Xr.next()
                K.act.op(lambda e, X=X, pa=pa: e.activation(out=X[:], in_=pa[:], func=AF.Copy), reads=[dpa], writes=[dX])
                pa, dpa = PA.next()
                mm8(pa, dpa, lambda h, MT=MT: MT[:, h, :], lambda h, X=X: X[:, h, :], [dMT, dX])
                NU, dNU = NUr.next()
                K.act.op(lambda e, NU=NU, pa=pa: e.activation(out=NU[:], in_=pa[:], func=AF.Copy, scale=-1.0), reads=[dpa], writes=[dNU])
                pa, dpa = PA.next()
                mmN(pa, dpa, [(lambda h, Sb=Sb: Sb[:, h, :], Rf), (lambda h, NU=NU: NU[:, h, :], lambda h, MRB=MRB: MRB[:, h, :]),
                              (Vt, lambda h, MRK=MRK: MRK[:, h, :])], [dFM, dSTb[b], dNU, dMRB, dTM, dMRK])
                Y, dY = Yr.next()
                K.dve.op(lambda e, Y=Y, pa=pa: e.tensor_copy(out=Y[:], in_=pa[:]), reads=[dpa], writes=[dY])
                K.sp.dma(lambda e, Y=Y, t0=t0: e.dma_start(out=dap(scr["y_fm"], t0, [[T, 64], [64 * T, 8], [1, 64]]), in_=Y[:]),
                         reads=[dY], adds=[scr["d_p2b"]])
                pa, dpa = PA.next()
                mmN(pa, dpa, [(Bt, lambda h, NU=NU: NU[:, h, :]), (Kt, Vt)], [dTM, dNU])
                TS_, dTS = TSr.next()
                K.dve.op(lambda e, TS_=TS_, pa=pa, b=b: e.tensor_tensor(out=TS_[:], in0=pa[:], in1=ST[b][:], op=ALU.add),
                         reads=[dpa, dST[b]], writes=[dTS])
                K.dve.op(lambda e, TS_=TS_, PCt=PCt, b=b: e.tensor_tensor(
                    out=ST[b][:], in0=TS_[:], in1=PCt[:].unsqueeze(2).broadcast_to([64, 8, 64]), op=ALU.mult),
                    reads=[dTS, dPC], writes=[dST[b]])
                K.act.op(lambda e, b=b: e.activation(out=STb[b][:], in_=ST[b][:], func=AF.Copy), reads=[dST[b]], writes=[dSTb[b]])


def phase2_post(K, cfg, io, scr):
    T, S, NB = cfg["T"], cfg["S"], cfg["NB"]
    NT = T // 512
    with ExitStack() as es:
        BO = K.sb([128, 128], F32, "BO", es)
        d_c = Dep()
        K.sp.dma(lambda e: e.dma_start(out=BO[:], in_=io["blockones"][:, :]), adds=[d_c])
        LN = K.sb([128, 2, 4], F32, "LN", es)
        K.sp.dma(lambda e: e.dma_start(out=LN[:, 0, :], in_=io["lnx_w"].rearrange("o (j p) -> p (o j)", p=128)), adds=[d_c])
        K.sp.dma(lambda e: e.dma_start(out=LN[:, 1, :], in_=io["lnx_b"].rearrange("o (j p) -> p (o j)", p=128)), adds=[d_c])
        Yr = Rot(K, 2, [128, 512], F32, "pY", es=es)
        Vr = Rot(K, 2, [128, 512], F32, "pV", es=es)
        Gr = Rot(K, 2, [128, 512], F32, "pG", es=es)
        Cr = Rot(K, 2, [128, 512], F32, "pC", es=es)
        YCr = Rot(K, 2, [128, 512], F32, "pYC", es=es)
        SQr = Rot(K, 2, [128, 512], F32, "pSQ", es=es)
        Rr = Rot(K, 2, [128, 512], F32, "pR", es=es)
        Or = Rot(K, 2, [128, 512], BF16, "pO", es=es)
        PM = Rot(K, 2, [128, 512], F32, "pPM", psum=True, es=es)
        PVr = Rot(K, 2, [128, 512], F32, "pPV", psum=True, es=es)
        def make_gen(ti, j):
            cs = slice(ti * 512, (ti + 1) * 512)
            if True:
                rs = slice(j * 128, (j + 1) * 128)
                Y, dY = Yr.next()
                V, dV = Vr.next()
                G, dG = Gr.next()
                C, dC = Cr.next()
                K.sp.dma(lambda e, Y=Y, rs=rs, cs=cs: e.dma_start(out=Y[:], in_=scr["y_fm"][rs, cs]),
                         reads=[scr["d_p2b"]], writes=[dY])
                K.pool.dma(lambda e, V=V, rs=rs, cs=cs: e.dma_start(out=V[:], in_=scr["v_fm"][rs, cs]),
                           reads=[scr["d_p2a"]], writes=[dV])
                K.sp.dma(lambda e, G=G, rs=rs, cs=cs: e.dma_start(out=G[:], in_=scr["g_fm"][rs, cs]),
                         reads=[scr["d_p2a"]], writes=[dG])
                K.pool.dma(lambda e, C=C, j=j, cs=cs: e.dma_start(
                    out=C[0:64, :], in_=scr["coef_fm"][2 * j:2 * j + 1, cs].broadcast_to([64, 512])),
                    reads=[scr["d_p2a"]], writes=[dC])
                K.pool.dma(lambda e, C=C, j=j, cs=cs: e.dma_start(
                    out=C[64:128, :], in_=scr["coef_fm"][2 * j + 1:2 * j + 2, cs].broadcast_to([64, 512])),
                    reads=[scr["d_p2a"]], adds=[dC])
                pm, dpm = PM.next()
                K.pe.op(lambda e, pm=pm, Y=Y: e.matmul(pm[:], BO[:], Y[:], start=True, stop=True),
                        reads=[dY, d_c], writes=[dpm])
                YC, dYC = YCr.next()
                K.dve.op(lambda e, YC=YC, Y=Y, pm=pm: e.tensor_tensor(out=YC[:], in0=Y[:], in1=pm[:], op=ALU.subtract),
                         reads=[dY, dpm], writes=[dYC])
                SQ, dSQ = SQr.next()
                K.act.op(lambda e, SQ=SQ, YC=YC: e.activation(out=SQ[:], in_=YC[:], func=AF.Square),
                         reads=[dYC], writes=[dSQ])
                yield
                pv, dpv = PVr.next()
                K.pe.op(lambda e, pv=pv, SQ=SQ: e.matmul(pv[:], BO[:], SQ[:], start=True, stop=True),
                        reads=[dSQ, d_c], writes=[dpv])
                yield
                R, dR = Rr.next()
                K.dve.op(lambda e, R=R, pv=pv: e.tensor_scalar(out=R[:], in0=pv[:], scalar1=64e-5, scalar2=None, op0=ALU.add),
                         reads=[dpv], writes=[dR])
                K.act.op(lambda e, R=R: e.activation(out=R[:], in_=R[:], func=AF.Ln), writes=[dR])
                K.act.op(lambda e, R=R: e.activation(out=R[:], in_=R[:], func=AF.Exp, scale=-0.5), writes=[dR])
                K.dve.op(lambda e, YC=YC, R=R: e.tensor_tensor(out=YC[:], in0=YC[:], in1=R[:], op=ALU.mult),
                         reads=[dR], writes=[dYC])
                K.dve.op(lambda e, YC=YC, j=j: e.tensor_scalar(out=YC[:], in0=YC[:], scalar1=LN[:, 0, j:j + 1],
                                                              scalar2=LN[:, 1, j:j + 1], op0=ALU.mult, op1=ALU.add),
                         reads=[d_c], writes=[dYC])
                yield
                K.pool.op(lambda e, C=C, V=V: e.tensor_tensor(out=C[:], in0=C[:], in1=V[:], op=ALU.mult),
                          reads=[dV], writes=[dC])
                K.dve.op(lambda e, YC=YC, C=C: e.tensor_tensor(out=YC[:], in0=YC[:], in1=C[:], op=ALU.add),
                         reads=[dC], writes=[dYC])
                O, dO = Or.next()
                K.dve.op(lambda e, O=O, YC=YC, G=G: e.tensor_tensor(out=O[:], in0=YC[:], in1=G[:], op=ALU.mult),
                         reads=[dYC, dG], writes=[dO])
                K.sp.dma(lambda e, O=O, rs=rs, cs=cs: e.dma_start(out=scr["ya_fm"][rs, cs], in_=O[:]),
                         reads=[dO], adds=[scr["d_p2c"]])

        work = [(ti, j) for ti in range(NT) for j in range(4)]

        active = []
        nxt = 0
        while nxt < len(work) or active:
            if len(active) < 2 and nxt < len(work):
                active.append(make_gen(*work[nxt]))
                nxt += 1
            for a in list(active):
                try:
                    next(a)
                except StopIteration:
                    active.remove(a)


def phase3(K, cfg, io, scr):
    T, S, NB = cfg["T"], cfg["S"], cfg["NB"]
    NQ = S // 128
    lam_init = 0.2
    with ExitStack() as es:
        d_c = Dep()
        identb = K.sb([128, 128], BF16, "a_identb", es)
        K.sp.dma(lambda e: e.dma_start(out=identb[:], in_=io["ident_bf"][:, :]), adds=[d_c])
        TB = K.sb([128, 4, S], F32, "TB", es)
        for h in range(4):
            (K.sp if h % 2 == 0 else K.pool).dma(lambda e, h=h: e.dma_start(out=TB[:, h, :], in_=io["alibi"][h, :, :]), adds=[d_c])
        SW = K.sb([128, 128], F32, "SW", es)
        K.sp.dma(lambda e: e.dma_start(out=SW[:], in_=io["subln_w"][0:1, :].broadcast_to([128, 128])), adds=[d_c])
        LQ = K.sb([128, 4, 64], F32, "LQ", es)
        for j, nm in enumerate(["lam_q1", "lam_k1", "lam_q2", "lam_k2"]):
            K.pool.dma(lambda e, j=j, nm=nm: e.dma_start(out=LQ[:, j, :], in_=io[nm][0:1, :].broadcast_to([128, 64])), adds=[d_c])
        LM = K.sb([128, 8], F32, "LM", es)
        d_lm = Dep()
        LT_ = K.sb([128, 2, 64], F32, "LTt", es)
        K.dve.op(lambda e: e.tensor_tensor(out=LT_[:, 0, :], in0=LQ[:, 0, :], in1=LQ[:, 1, :], op=ALU.mult), reads=[d_c], writes=[d_lm])
        K.dve.op(lambda e: e.tensor_tensor(out=LT_[:, 1, :], in0=LQ[:, 2, :], in1=LQ[:, 3, :], op=ALU.mult), reads=[d_c], writes=[d_lm])
        K.dve.op(lambda e: e.tensor_reduce(out=LM[:, 0:2], in_=LT_[:], axis=AX.X, op=ALU.add), writes=[d_lm])
        K.act.op(lambda e: e.activation(out=LM[:, 2:4], in_=LM[:, 0:2], func=AF.Exp), writes=[d_lm])
        K.dve.op(lambda e: e.tensor_tensor(out=LM[:, 4:5], in0=LM[:, 3:4], in1=LM[:, 2:3], op=ALU.subtract), writes=[d_lm])
        K.dve.op(lambda e: e.tensor_scalar(out=LM[:, 4:5], in0=LM[:, 4:5], scalar1=-lam_init, scalar2=None, op0=ALU.add), writes=[d_lm])
        K.dve.op(lambda e: e.tensor_scalar(out=SW[:], in0=SW[:], scalar1=1.0 - lam_init, scalar2=None, op0=ALU.mult),
                 reads=[d_c], writes=[d_c])

        Vr = Rot(K, 2, [128, NQ, 512], BF16, "aV", es=es)
        QKr = Rot(K, 2, [64, 4, S], BF16, "aQK", es=es)
        SSr = Rot(K, 3, [128, 512], F32, "aSS", es=es)
        Pr = Rot(K, 3, [128, 512], BF16, "aP", es=es)
        PTsr = Rot(K, 4, [128, 4, 128], BF16, "aPTs", es=es)
        YB = K.sb([128, NQ, 512], BF16, "aYB", es)
        dYB = Dep()
        STr = Rot(K, 4, [128, 24], F32, "aST", es=es)
        O1r = Rot(K, 2, [128, 128], F32, "aO1", es=es)
        Or_ = Rot(K, 2, [128, 128], F32, "aO", es=es)
        junk = K.sb([128, 128], F32, "ajunk", es)
        d_junk = Dep()
        YTr = Rot(K, 2, [128, 4, 128], BF16, "aYT", es=es)
        PS = Rot(K, 3, [128, 512], F32, "aPS", psum=True, es=es)
        PTp = Rot(K, 2, [128, 4, 128], BF16, "aPTp", psum=True, es=es)
        PO = Rot(K, 2, [128, 2, 128], F32, "aPO", psum=True, es=es)
        cp = [0]

        def copy_eng():
            cp[0] += 1
            return cp[0] % 2

        SSQ = K.sb([128, NQ * 4], F32, "aSSQ", es)
        dSSQ = Dep()
        SWb = K.sb([128, 128], BF16, "aSWb", es)
        K.act.op(lambda e: e.activation(out=SWb[:], in_=SW[:], func=AF.Copy), reads=[d_c], adds=[d_c])
        pipe = []
        pidx = [0]

        def step_pipe():
            j = len(pipe) - 1
            pipe[j][0]()
            if j - 1 >= pidx[0]:
                pipe[j - 1][2]()
            if j - 2 >= pidx[0]:
                pipe[j - 2][3]()
                if pipe[j - 2][4] is not None:
                    pipe[j - 2][4]()
            pipe[j][1]()

        def flush_pipe():
            j = len(pipe) - 1
            if j - 0 >= pidx[0] and j >= 0:
                pipe[j][2]()
            for k in (j - 1, j):
                if k >= pidx[0] and k >= 0:
                    pipe[k][3]()
                    if pipe[k][4] is not None:
                        pipe[k][4]()
            pidx[0] = len(pipe)
        for b in range(NB):
            V, dV = Vr.next()
            K.sp.dma(lambda e, V=V, b=b: e.dma_start(
                out=V[:], in_=scr["av_tm"][b * S:(b + 1) * S, :].rearrange("(n p) c -> p n c", p=128)),
                reads=[scr["d_p1"]], writes=[dV])
            first_yb = True
            for h in range(4):
                QK, dQK = QKr.next()
                for j in range(4):
                    r0 = (0 if j < 2 else 512) + h * 128 + (j % 2) * 64
                    (K.sp if j % 2 == 0 else K.pool).dma(lambda e, QK=QK, j=j, r0=r0, b=b: e.dma_start(
                        out=QK[:, j, :], in_=scr["qk_fm"][r0:r0 + 64, b * S:(b + 1) * S]),
                        reads=[scr["d_p1"]], writes=[dQK] if j == 0 else [], adds=[] if j == 0 else [dQK])
                for qi in range(NQ):
                    nk = (qi + 1) * 128
                    off = (S - 128) - qi * 128
                    ST, dST = STr.next()
                    K.pool.op(lambda e, ST=ST: e.memset(ST[:], 0.0), writes=[dST])
                    po, dpo = PO.next()
                    items = []
                    for c in range(2):
                        nch = (nk + 511) // 512
                        for ch in range(nch):
                            items.append((c, ch))
                    for ii, (c, ch) in enumerate(items):
                        kb0 = ch * 512
                        n = min(512, nk - kb0)
                        nb = n // 128
                        ps, dps = PS.next()
                        SS, dSS = SSr.next()
                        Pt, dP = Pr.next()
                        hold = {}

                        def stA_pe(ps=ps, dps=dps, c=c, qi=qi, kb0=kb0, n=n, QK=QK, dQK=dQK):
                            K.pe.op(lambda e: e.matmul(
                                ps[:, :n], QK[:, c, qi * 128:(qi + 1) * 128], QK[:, 2 + c, kb0:kb0 + n],
                                start=True, stop=True), reads=[dQK], writes=[dps])

                        def stA_rest(ps=ps, dps=dps, SS=SS, dSS=dSS, Pt=Pt, dP=dP, ST=ST, dST=dST, n=n, off=off, h=h, kb0=kb0, c=c, ch=ch):
                            K.dve.op(lambda e: e.scalar_tensor_tensor(
                                out=SS[:, :n], in0=ps[:, :n], scalar=0.125, in1=TB[:, h, off + kb0:off + kb0 + n],
                                op0=ALU.mult, op1=ALU.add), reads=[dps, d_c], writes=[dSS])
                            K.act.op(lambda e: e.activation(
                                out=Pt[:, :n], in_=SS[:, :n], func=AF.Exp,
                                accum_out=ST[:, 4 * c + ch:4 * c + ch + 1]), reads=[dSS, dST], writes=[dP], adds=[dST])

                        def stB(Pt=Pt, dP=dP, nb=nb, hold=hold):
                            ptp, dptp = PTp.next()
                            for kk_ in range(nb):
                                K.pe.op(lambda e, kk_=kk_: e.transpose(
                                    out=ptp[:, kk_, :], in_=Pt[:, kk_ * 128:(kk_ + 1) * 128], identity=identb[:]),
                                    reads=[dP, d_c], writes=[dptp] if kk_ == 0 else [], adds=[] if kk_ == 0 else [dptp])
                            PTs, dPTs = PTsr.next()
                            hold["PTs"] = (PTs, dPTs)
                            if copy_eng():
                                K.act.op(lambda e: e.activation(
                                    out=PTs[:, :nb, :], in_=ptp[:, :nb, :], func=AF.Copy), reads=[dptp], writes=[dPTs])
                            else:
                                K.dve.op(lambda e: e.tensor_copy(
                                    out=PTs[:, :nb, :], in_=ptp[:, :nb, :]), reads=[dptp], writes=[dPTs])

                        def stC(nb=nb, kb0=kb0, c=c, h=h, qi=qi, po=po, dpo=dpo, V=V, dV=dV, hold=hold):
                            PTs, dPTs = hold["PTs"]
                            for kk_ in range(nb):
                                kb = kb0 // 128 + kk_
                                K.pe.op(lambda e, kb=kb, kk_=kk_: e.matmul(
                                    po[:, c, :], PTs[:, kk_, :], V[:, kb, h * 128:(h + 1) * 128],
                                    start=(kb == 0), stop=(kb == qi)), reads=[dPTs, dV],
                                    writes=[dpo] if (kb == 0 and c == 0) else [], adds=[] if (kb == 0 and c == 0) else [dpo])

                        def combine(ST=ST, dST=dST, po=po, dpo=dpo, qi=qi, h=h, fy=first_yb):
                            K.dve.op(lambda e: e.tensor_reduce(out=ST[:, 8:10], in_=ST[:, 0:8].rearrange("p (c k) -> p c k", k=4),
                                                               axis=AX.X, op=ALU.add), reads=[dST], writes=[dST])
                            K.dve.op(lambda e: e.reciprocal(out=ST[:, 10:12], in_=ST[:, 8:10]), writes=[dST])
                            K.dve.op(lambda e: e.tensor_tensor(out=ST[:, 12:13], in0=ST[:, 11:12], in1=LM[:, 4:5], op=ALU.mult),
                                     reads=[d_lm], writes=[dST])
                            O1, dO1 = O1r.next()
                            K.dve.op(lambda e: e.tensor_scalar(out=O1[:], in0=po[:, 1, :], scalar1=ST[:, 12:13],
                                                               scalar2=None, op0=ALU.mult), reads=[dpo, dST], writes=[dO1])
                            K.dve.op(lambda e: e.scalar_tensor_tensor(
                                out=YB[:, qi, h * 128:(h + 1) * 128], in0=po[:, 0, :], scalar=ST[:, 10:11], in1=O1[:], op0=ALU.mult, op1=ALU.add),
                                reads=[dpo, dST, dO1], writes=[dYB] if fy else [], adds=[] if fy else [dYB])
                            K.act.op(lambda e: e.activation(out=junk[:], in_=YB[:, qi, h * 128:(h + 1) * 128], func=AF.Square,
                                                            accum_out=SSQ[:, qi * 4 + h:qi * 4 + h + 1]),
                                     reads=[dYB], writes=[d_junk], adds=[dSSQ])

                        last = (ii == len(items) - 1)
                        pipe.append([stA_pe, stA_rest, stB, stC, combine if last else None])
                        step_pipe()
                    first_yb = False
            flush_pipe()
            K.dve.op(lambda e: e.tensor_scalar(out=SSQ[:], in0=SSQ[:], scalar1=1.0 / 128, scalar2=1e-5, op0=ALU.mult, op1=ALU.add),
                     reads=[dSSQ], writes=[dSSQ])
            K.act.op(lambda e: e.activation(out=SSQ[:], in_=SSQ[:], func=AF.Ln), writes=[dSSQ])
            K.act.op(lambda e: e.activation(out=SSQ[:], in_=SSQ[:], func=AF.Exp, scale=-0.5), writes=[dSSQ])
            YBv = YB[:].rearrange("p q (h e) -> p (q h) e", e=128)
            K.dve.op(lambda e: e.tensor_tensor(out=YBv, in0=YBv, in1=SSQ[:].unsqueeze(2).broadcast_to([128, NQ * 4, 128]), op=ALU.mult),
                     reads=[dSSQ], writes=[dYB])
            K.pool.op(lambda e: e.tensor_tensor(out=YBv, in0=YBv, in1=SWb[:].unsqueeze(1).broadcast_to([128, NQ * 4, 128]), op=ALU.mult),
                      reads=[d_c], writes=[dYB])
            for qi in range(NQ):
                ptp, dptp = PTp.next()
                for h in range(4):
                    K.pe.op(lambda e, ptp=ptp, qi=qi, h=h: e.transpose(out=ptp[:, h, :], in_=YB[:, qi, h * 128:(h + 1) * 128],
                                                                      identity=identb[:]),
                            reads=[dYB, d_c], writes=[dptp] if h == 0 else [], adds=[] if h == 0 else [dptp])
                YT, dYT = YTr.next()
                K.act.op(lambda e, ptp=ptp, YT=YT: e.activation(out=YT[:], in_=ptp[:, 0:4, :], func=AF.Copy),
                         reads=[dptp], writes=[dYT])
                t0 = b * S + qi * 128
                K.sp.dma(lambda e, YT=YT, t0=t0: e.dma_start(
                    out=dap(scr["yb_fm"], t0, [[T, 128], [128 * T, 4], [1, 128]]), in_=YT[:]),
                    reads=[dYT], adds=[scr["d_p3"]])


def load_w_bf16(K, es, src2d, rows, cols, name, dep, stage_rot, q, W=None):
    nk = rows // 128
    if W is None:
        W = K.sb([128, nk, cols], BF16, name, es)
    for kc in range(nk):
        for c0 in range(0, cols, 1024):
            n = min(1024, cols - c0)
            st, dst = stage_rot.next()
            q[0] += 1
            (K.sp if q[0] % 2 == 0 else K.pool).dma(lambda e, st=st, kc=kc, c0=c0, n=n: e.dma_start(
                out=st[:, :n], in_=src2d[kc * 128:(kc + 1) * 128, c0:c0 + n]), writes=[dst])
            if q[0] % 2 == 0:
                K.act.op(lambda e, st=st, kc=kc, c0=c0, n=n: e.activation(out=W[:, kc, c0:c0 + n], in_=st[:, :n], func=AF.Copy),
                         reads=[dst], adds=[dep])
            else:
                K.dve.op(lambda e, st=st, kc=kc, c0=c0, n=n: e.tensor_copy(out=W[:, kc, c0:c0 + n], in_=st[:, :n]),
                         reads=[dst], adds=[dep])
    return W


def phase4(K, cfg, io, scr):
    T, S, NB = cfg["T"], cfg["S"], cfg["NB"]
    NT = T // 512
    with ExitStack() as es:
        d_c = Dep()
        identb = K.sb([128, 128], BF16, "m_identb", es)
        K.sp.dma(lambda e: e.dma_start(out=identb[:], in_=io["ident_bf"][:, :]), adds=[d_c])
        NF = K.sb([128, D], F32, "NF", es)
        K.sp.dma(lambda e: e.dma_start(out=NF[:], in_=io["norm_ffn_w"][0:1, :].broadcast_to([128, D])), adds=[d_c])
        stg = Rot(K, 2, [128, 1024], F32, "m_stg", es=es)
        q = [0]
        PAw = load_w_bf16(K, es, io["proj_a"][0], 512, D, "PAw", d_c, stg, q)
        PBw = load_w_bf16(K, es, io["proj_b"][0], 512, D, "PBw", d_c, stg, q)
        WO = load_w_bf16(K, es, io["w_out"][0], D, D, "WO", d_c, stg, q)
        YAr = Rot(K, 2, [128, 4, 512], BF16, "mYA", es=es)
        YBr = Rot(K, 2, [128, 4, 512], BF16, "mYB", es=es)
        SGr = Rot(K, 2, [128, 16, 512], BF16, "mSG", es=es)
        MGr = Rot(K, 2, [128, 8, 512], BF16, "mMG", es=es)
        t1r = Rot(K, 2, [128, 512], F32, "mt1", es=es)
        t2r = Rot(K, 2, [128, 512], F32, "mt2", es=es)
        Xr = Rot(K, 2, [128, D], F32, "mX", es=es)
        X1r = Rot(K, 2, [128, D], F32, "mX1", es=es)
        XHr = Rot(K, 2, [128, D], F32, "mXH", es=es)
        XBr = Rot(K, 2, [128, D], BF16, "mXB", es=es)
        XTr = Rot(K, 2, [128, 8, 128], BF16, "mXT", es=es)
        junk = K.sb([128, D], BF16, "mjunk", es)
        d_junk = Dep()
        STr = Rot(K, 4, [128, 4], F32, "mST", es=es)
        PP = Rot(K, 2, [128, 2, 512], F32, "mPP", psum=True, es=es)
        PO2 = Rot(K, 1, [128, 2, 512], F32, "mPO", psum=True, es=es)
        PTp = Rot(K, 1, [128, 8, 128], BF16, "mPTp", psum=True, es=es)
        def make_gen(ti):
            cs = slice(ti * 512, (ti + 1) * 512)
            YA, dYA = YAr.next()
            YB, dYB = YBr.next()
            SG, dSG = SGr.next()
            K.sp.dma(lambda e, YA=YA, cs=cs: e.dma_start(out=YA[:], in_=scr["ya_fm"][:, cs].rearrange("(c p) t -> p c t", p=128)),
                     reads=[scr["d_p2c"]], writes=[dYA])
            K.pool.dma(lambda e, YB=YB, cs=cs: e.dma_start(out=YB[:], in_=scr["yb_fm"][:, cs].rearrange("(c p) t -> p c t", p=128)),
                       reads=[scr["d_p3"]], writes=[dYB])
            K.sp.dma(lambda e, SG=SG, cs=cs: e.dma_start(out=SG[:], in_=scr["sg_fm"][:, cs].rearrange("(c p) t -> p c t", p=128)),
                     reads=[scr["d_p1"]], writes=[dSG])
            MG, dMG = MGr.next()
            for m in range(8):
                pp, dpp = PP.next()
                for c in range(4):
                    K.pe.op(lambda e, pp=pp, YA=YA, c=c, m=m: e.matmul(pp[:, 0, :], PAw[:, c, m * 128:(m + 1) * 128], YA[:, c, :],
                                                                      start=(c == 0), stop=(c == 3)),
                            reads=[dYA, d_c], writes=[dpp] if c == 0 else [], adds=[] if c == 0 else [dpp])
                for c in range(4):
                    K.pe.op(lambda e, pp=pp, YB=YB, c=c, m=m: e.matmul(pp[:, 1, :], PBw[:, c, m * 128:(m + 1) * 128], YB[:, c, :],
                                                                      start=(c == 0), stop=(c == 3)),
                            reads=[dYB, d_c], adds=[dpp])
                t1, dt1 = t1r.next()
                t2, dt2 = t2r.next()
                K.dve.op(lambda e, t1=t1, pp=pp, SG=SG, m=m: e.tensor_tensor(out=t1[:], in0=pp[:, 0, :], in1=SG[:, m, :], op=ALU.mult),
                         reads=[dpp, dSG], writes=[dt1])
                K.dve.op(lambda e, t2=t2, pp=pp, SG=SG, m=m: e.tensor_tensor(out=t2[:], in0=pp[:, 1, :], in1=SG[:, 8 + m, :], op=ALU.mult),
                         reads=[dpp, dSG], writes=[dt2])
                K.pool.op(lambda e, t1=t1, t2=t2, MG=MG, m=m: e.tensor_tensor(out=MG[:, m, :], in0=t1[:], in1=t2[:], op=ALU.add),
                          reads=[dt1, dt2], writes=[dMG] if m == 0 else [], adds=[] if m == 0 else [dMG])
                if m % 2 == 1:
                    yield
            for sub in range(4):
                t0 = ti * 512 + sub * 128
                X, dX = Xr.next()
                K.pool.dma(lambda e, X=X, t0=t0: e.dma_start(out=X[:], in_=io["x"][t0:t0 + 128, :]), writes=[dX])
                po, dpo = PO2.next()
                for n in range(2):
                    for m in range(8):
                        K.pe.op(lambda e, po=po, MG=MG, m=m, n=n, sub=sub: e.matmul(
                            po[:, n, :], MG[:, m, sub * 128:(sub + 1) * 128], WO[:, m, n * 512:(n + 1) * 512],
                            start=(m == 0), stop=(m == 7)), reads=[dMG, d_c],
                            writes=[dpo] if (m == 0 and n == 0) else [], adds=[] if (m == 0 and n == 0) else [dpo])
                X1, dX1 = X1r.next()
                K.dve.op(lambda e, X1=X1, X=X, po=po: e.tensor_tensor(out=X1[:], in0=X[:], in1=po[:].rearrange("p a b -> p (a b)"),
                                                                     op=ALU.add), reads=[dX, dpo], writes=[dX1])
                K.sp.dma(lambda e, X1=X1, t0=t0: e.dma_start(out=scr["x1_tm"][t0:t0 + 128, :], in_=X1[:]),
                         reads=[dX1], adds=[scr["d_p4"]])
                ST, dST = STr.next()
                K.act.op(lambda e, X1=X1, ST=ST: e.activation(out=junk[:], in_=X1[:], func=AF.Square, accum_out=ST[:, 0:1]),
                         reads=[dX1], writes=[d_junk, dST])
                K.dve.op(lambda e, ST=ST: e.tensor_scalar(out=ST[:, 1:2], in0=ST[:, 0:1], scalar1=1.0 / D, scalar2=1e-6,
                                                          op0=ALU.mult, op1=ALU.add), writes=[dST])
                K.act.op(lambda e, ST=ST: e.activation(out=ST[:, 2:3], in_=ST[:, 1:2], func=AF.Sqrt), writes=[dST])
                K.dve.op(lambda e, ST=ST: e.reciprocal(out=ST[:, 3:4], in_=ST[:, 2:3]), writes=[dST])
                XH, dXH = XHr.next()
                K.dve.op(lambda e, XH=XH, X1=X1, ST=ST: e.scalar_tensor_tensor(
                    out=XH[:], in0=X1[:], scalar=ST[:, 3:4], in1=NF[:], op0=ALU.mult, op1=ALU.mult),
                    reads=[dX1, dST, d_c], writes=[dXH])
                K.sp.dma(lambda e, XH=XH, t0=t0: e.dma_start(out=scr["xh_tm"][t0:t0 + 128, :], in_=XH[:]),
                         reads=[dXH], adds=[scr["d_p4"]])
                XB, dXB = XBr.next()
                K.pool.op(lambda e, XB=XB, XH=XH: e.tensor_copy(out=XB[:], in_=XH[:]), reads=[dXH], writes=[dXB])
                ptp, dptp = PTp.next()
                for kc in range(8):
                    K.pe.op(lambda e, ptp=ptp, XB=XB, kc=kc: e.transpose(out=ptp[:, kc, :], in_=XB[:, kc * 128:(kc + 1) * 128],
                                                                        identity=identb[:]),
                            reads=[dXB, d_c], writes=[dptp] if kc == 0 else [], adds=[] if kc == 0 else [dptp])
                XT, dXT = XTr.next()
                K.act.op(lambda e, ptp=ptp, XT=XT: e.activation(out=XT[:], in_=ptp[:], func=AF.Copy), reads=[dptp], writes=[dXT])
                K.sp.dma(lambda e, XT=XT, t0=t0: e.dma_start(
                    out=dap(scr["xhT_fm"], t0, [[T, 128], [128 * T, 8], [1, 128]]), in_=XT[:]),
                    reads=[dXT], adds=[scr["d_p4"]])
                yield

        work = [(ti,) for ti in range(NT)]

        active = []
        nxt = 0
        while nxt < len(work) or active:
            if len(active) < 2 and nxt < len(work):
                active.append(make_gen(*work[nxt]))
                nxt += 1
            for a in list(active):
                try:
                    next(a)
                except StopIteration:
                    active.remove(a)


def phase5(K, cfg, io, scr):
    T, S, NB = cfg["T"], cfg["S"], cfg["NB"]
    NTT = T // 128
    with ExitStack() as es:
        d_c = Dep()
        identb = K.sb([128, 128], BF16, "f_identb", es)
        K.sp.dma(lambda e: e.dma_start(out=identb[:], in_=io["ident_bf"][:, :]), adds=[d_c])
        IOTA = K.sb([128, 16], F32, "IOTA", es)
        K.sp.dma(lambda e: e.dma_start(out=IOTA[:], in_=io["iota16"][:, :]), adds=[d_c])
        FNW = K.sb([128, D], F32, "FNW", es)
        K.sp.dma(lambda e: e.dma_start(out=FNW[:], in_=io["final_norm_w"][0:1, :].broadcast_to([128, D])), adds=[d_c])
        WQ = K.sb([128, 8, 2048], BF16, "WQ", es)
        KT = K.sb([128, 16, 128], BF16, "KT", es)
        PQ = Rot(K, 1, [128, 8, 128], F32, "fPQ", psum=True, es=es)
        PSc = Rot(K, 1, [128, 8, 128], F32, "fPSc", psum=True, es=es)
        PKT = Rot(K, 1, [128, 8, 128], BF16, "fPKT", psum=True, es=es)
        es_setup = ExitStack()
        stg = Rot(K, 2, [128, 1024], F32, "f_stg", es=es_setup)
        q = [0]
        load_w_bf16(K, es, io["peer_wq"][0], D, 2048, "WQ", d_c, stg, q, W=WQ)
        KF = K.sb([128, 16, 128], F32, "KF", es_setup)
        dKF = Dep()
        K.sp.dma(lambda e: e.dma_start(out=KF[:], in_=io["peer_keys"][0].rearrange("h c n d -> n (h c) d")), writes=[dKF])
        KB = K.sb([128, 16, 128], BF16, "KB", es_setup)
        K.dve.op(lambda e: e.tensor_copy(out=KB[:], in_=KF[:]), reads=[dKF], writes=[dKF])
        for half in range(2):
            pk, dpk = PKT.next()
            for i in range(8):
                K.pe.op(lambda e, pk=pk, i=i, half=half: e.transpose(out=pk[:, i, :], in_=KB[:, half * 8 + i, :], identity=identb[:]),
                        reads=[dKF, d_c], writes=[dpk] if i == 0 else [], adds=[] if i == 0 else [dpk])
            K.act.op(lambda e, pk=pk, half=half: e.activation(out=KT[:, half * 8:(half + 1) * 8, :], in_=pk[:], func=AF.Copy),
                     reads=[dpk], adds=[d_c])

        K.barrier()
        es_setup.close()
        XTr = Rot(K, 2, [128, 8, 128], BF16, "fXT", es=es)
        XHr = Rot(K, 2, [128, D], F32, "fXH", es=es)
        X1r = Rot(K, 2, [128, D], F32, "fX1", es=es)
        QTr = Rot(K, 1, [128, 16, 128], BF16, "fQT", es=es)
        SCr = Rot(K, 1, [128, 16, 128], F32, "fSC", es=es)
        SC2 = K.sb([128, 256], F32, "fSC2", es)
        dSC2 = Dep()
        M16r = Rot(K, 1, [128, 16, 16], F32, "fM16", es=es)
        I16r = Rot(K, 1, [128, 16, 16], U32, "fI16", es=es)
        I16fr = Rot(K, 1, [128, 16, 16], F32, "fI16f", es=es)
        CANDr = Rot(K, 1, [128, 8, 256], F32, "fCAND", es=es)
        VALr = Rot(K, 1, [128, 8, 16], F32, "fVAL", es=es)
        CIr = Rot(K, 1, [128, 3, 128], U32, "fCI", es=es)
        ABr = Rot(K, 1, [128, 2, 128], F32, "fAB", es=es)
        OHr = CANDr
        E12r = Rot(K, 1, [128, 3, 128], F32, "fE12", es=es)
        IDSr = Rot(K, 2, [128, 128], I32, "fIDS", es=es)
        GTr = Rot(K, 2, [128, 4, 128], F32, "fGT", es=es)
        S8r = Rot(K, 2, [128, 16], F32, "fS8", es=es)
        GRP = cfg.get("grp", 4)
        ACTDOT = tuple(cfg.get("actdot", (0, 2)))
        if isinstance(cfg.get("actdot_mask"), int):
            ACTDOT = tuple(i for i in range(GRP) if (cfg["actdot_mask"] >> i) & 1)
        junk3r = Rot(K, 2, [128, D], BF16, "fjunk3", es=es)
        junkr = Rot(K, 3, [128, D], BF16, "fjunkr", es=es)
        PRDr = Rot(K, 3, [128, D], BF16, "fPRD", es=es)
        GBr = Rot(K, cfg.get("ngbuf", 22), [128, 2 * D], BF16, "fGB", es=es)
        junk2 = K.sb([128, D], BF16, "fjunk2", es)
        d_junk2 = Dep()
        XHbr = Rot(K, 2, [128, D], BF16, "fXHb", es=es)
        DGr = Rot(K, 4, [128, 128], BF16, "fDG", es=es)
        PY = Rot(K, 1, [128, 2, 512], F32, "fPY", psum=True, es=es)

        RES = {}

        def routing(ti):
            t0 = ti * 128
            XT, dXT = XTr.next()
            XH, dXH = XHr.next()
            X1, dX1 = X1r.next()
            K.sp.dma(lambda e, XT=XT, t0=t0: e.dma_start(out=XT[:], in_=dap(scr["xhT_fm"], t0, [[T, 128], [128 * T, 8], [1, 128]])),
                     reads=[scr["d_p4"]], writes=[dXT])
            K.sp.dma(lambda e, XH=XH, t0=t0: e.dma_start(out=XH[:], in_=scr["xh_tm"][t0:t0 + 128, :]), reads=[scr["d_p4"]], writes=[dXH])
            K.sp.dma(lambda e, X1=X1, t0=t0: e.dma_start(out=X1[:], in_=scr["x1_tm"][t0:t0 + 128, :]), reads=[scr["d_p4"]], writes=[dX1])
            QT, dQT = QTr.next()
            for half in range(2):
                pq, dpq = PQ.next()
                for i in range(8):
                    hc = half * 8 + i
                    for kc in range(8):
                        K.pe.op(lambda e, pq=pq, i=i, hc=hc, kc=kc, XT=XT: e.matmul(
                            pq[:, i, :], WQ[:, kc, hc * 128:(hc + 1) * 128], XT[:, kc, :], start=(kc == 0), stop=(kc == 7)),
                            reads=[dXT, d_c], writes=[dpq] if (i == 0 and kc == 0) else [], adds=[] if (i == 0 and kc == 0) else [dpq])
                K.act.op(lambda e, pq=pq, QT=QT, half=half: e.activation(out=QT[:, half * 8:(half + 1) * 8, :], in_=pq[:], func=AF.Copy),
                         reads=[dpq], writes=[dQT] if half == 0 else [], adds=[] if half == 0 else [dQT])
            SC, dSC = SCr.next()
            for half in range(2):
                psc, dpsc = PSc.next()
                for i in range(8):
                    hc = half * 8 + i
                    K.pe.op(lambda e, psc=psc, i=i, hc=hc, QT=QT: e.matmul(psc[:, i, :], QT[:, hc, :], KT[:, hc, :], start=True, stop=True),
                            reads=[dQT, d_c], writes=[dpsc] if i == 0 else [], adds=[] if i == 0 else [dpsc])
                K.act.op(lambda e, psc=psc, SC=SC, half=half: e.activation(out=SC[:, half * 8:(half + 1) * 8, :], in_=psc[:], func=AF.Copy),
                         reads=[dpsc], writes=[dSC] if half == 0 else [], adds=[] if half == 0 else [dSC])
            yield
            M16, dM = M16r.next()
            I16, dI = I16r.next()
            for hc in range(16):
                if hc % 4 == 0 and hc > 0:
                    yield
                K.dve.op(lambda e, M16=M16, SC=SC, hc=hc: e.max(out=M16[:, hc, 0:8], in_=SC[:, hc, :]), reads=[dSC],
                         writes=[dM] if hc == 0 else [], adds=[] if hc == 0 else [dM])
                K.dve.op(lambda e, M16=M16, SC=SC, hc=hc: e.match_replace(out=SC2[:, 0:128], in_to_replace=M16[:, hc, 0:8],
                                                                         in_values=SC[:, hc, :], imm_value=-1e30),
                         reads=[dSC, dM], writes=[dSC2])
                K.dve.op(lambda e, M16=M16, hc=hc: e.max(out=M16[:, hc, 8:16], in_=SC2[:, 0:128]), reads=[dSC2], adds=[dM])
                K.dve.op(lambda e, M16=M16, I16=I16, SC=SC, hc=hc: e.max_index(out=I16[:, hc, 0:8], in_max=M16[:, hc, 0:8],
                                                                              in_values=SC[:, hc, :]),
                         reads=[dSC, dM], writes=[dI] if hc == 0 else [], adds=[] if hc == 0 else [dI])
                K.dve.op(lambda e, M16=M16, I16=I16, SC=SC, hc=hc: e.max_index(out=I16[:, hc, 8:16], in_max=M16[:, hc, 8:16],
                                                                              in_values=SC[:, hc, :]),
                         reads=[dSC, dM], adds=[dI])
            yield
            I16f, dIf = I16fr.next()
            K.dve.op(lambda e, I16f=I16f, I16=I16: e.tensor_copy(out=I16f[:], in_=I16[:]), reads=[dI], writes=[dIf])
            I16fv = I16f[:].rearrange("p (h c) k -> p h c k", c=2)
            K.dve.op(lambda e, I16fv=I16fv: e.tensor_scalar(out=I16fv[:, :, 0, :], in0=I16fv[:, :, 0, :], scalar1=128.0, scalar2=None,
                                                            op0=ALU.mult), writes=[dIf])
            CAND, dCA = CANDr.next()
            M16v = M16[:].rearrange("p (h c) k -> p h c k", c=2)
            K.dve.op(lambda e, CAND=CAND, M16v=M16v: e.tensor_tensor(
                out=CAND[:].rearrange("p h (a b) -> p h a b", b=16),
                in0=M16v[:, :, 0, :].unsqueeze(3).broadcast_to([128, 8, 16, 16]),
                in1=M16v[:, :, 1, :].unsqueeze(2).broadcast_to([128, 8, 16, 16]), op=ALU.add),
                reads=[dM], writes=[dCA])
            VAL, dVAL = VALr.next()
            CI, dCI = CIr.next()
            CIv = CI[:, 0, :].rearrange("p (h k) -> p h k", k=16)
            for h in range(8):
                if h % 4 == 0:
                    yield
                K.dve.op(lambda e, VAL=VAL, CAND=CAND, h=h: e.max(out=VAL[:, h, 0:8], in_=CAND[:, h, :]), reads=[dCA],
                         writes=[dVAL] if h == 0 else [], adds=[] if h == 0 else [dVAL])
                K.dve.op(lambda e, VAL=VAL, CAND=CAND, h=h: e.match_replace(out=SC2[:, :], in_to_replace=VAL[:, h, 0:8],
                                                                           in_values=CAND[:, h, :], imm_value=-1e30),
                         reads=[dCA, dVAL], writes=[dSC2])
                K.dve.op(lambda e, VAL=VAL, h=h: e.max(out=VAL[:, h, 8:16], in_=SC2[:, :]), reads=[dSC2], adds=[dVAL])
                K.dve.op(lambda e, VAL=VAL, CIv=CIv, CAND=CAND, h=h: e.max_index(out=CIv[:, h, 0:8], in_max=VAL[:, h, 0:8],
                                                                                in_values=CAND[:, h, :]),
                         reads=[dCA, dVAL], writes=[dCI] if h == 0 else [], adds=[] if h == 0 else [dCI])
                K.dve.op(lambda e, VAL=VAL, CIv=CIv, CAND=CAND, h=h: e.max_index(out=CIv[:, h, 8:16], in_max=VAL[:, h, 8:16],
                                                                                in_values=CAND[:, h, :]),
                         reads=[dCA, dVAL], adds=[dCI])
            yield
            GT, dGT = GTr.next()
            S8, dS8 = S8r.next()
            Ev = GT[:, 0, :].rearrange("p (h k) -> p h k", k=16)
            Gv = GT[:, 1, :].rearrange("p (h k) -> p h k", k=16)
            K.dve.op(lambda e, Ev=Ev, VAL=VAL: e.tensor_tensor(out=Ev, in0=VAL[:], in1=VAL[:, :, 0:1].broadcast_to([128, 8, 16]),
                                                              op=ALU.subtract), reads=[dVAL], writes=[dGT])
            K.act.op(lambda e, GT=GT: e.activation(out=GT[:, 0, :], in_=GT[:, 0, :], func=AF.Exp), writes=[dGT])
            K.dve.op(lambda e, Ev=Ev, S8=S8: e.tensor_reduce(out=S8[:, 0:8], in_=Ev, axis=AX.X, op=ALU.add), reads=[dGT], writes=[dS8])
            K.dve.op(lambda e, S8=S8: e.reciprocal(out=S8[:, 8:16], in_=S8[:, 0:8]), writes=[dS8])
            K.dve.op(lambda e, Ev=Ev, Gv=Gv, S8=S8: e.tensor_tensor(out=Gv, in0=Ev, in1=S8[:, 8:16].unsqueeze(2).broadcast_to([128, 8, 16]),
                                                                   op=ALU.mult), reads=[dS8], writes=[dGT])
            yield
            K.dve.op(lambda e, CI=CI: e.tensor_single_scalar(out=CI[:, 1, :], in_=CI[:, 0, :], scalar=4, op=ALU.logical_shift_right),
                     writes=[dCI])
            K.dve.op(lambda e, CI=CI: e.tensor_single_scalar(out=CI[:, 2, :], in_=CI[:, 0, :], scalar=15, op=ALU.bitwise_and),
                     writes=[dCI])
            AB, dAB = ABr.next()
            K.dve.op(lambda e, AB=AB, CI=CI: e.tensor_copy(out=AB[:], in_=CI[:, 1:3, :]), reads=[dCI], writes=[dAB])
            OH, dOH = OHr.next()
            E12, dE12 = E12r.next()
            OHv = OH[:].rearrange("p h (j a) -> p h j a", a=16)
            for c in range(2):
                ABv = AB[:, c, :].rearrange("p (h j) -> p h j", j=16)
                K.dve.op(lambda e, OHv=OHv, ABv=ABv: e.tensor_tensor(
                    out=OHv, in0=ABv.unsqueeze(3).broadcast_to([128, 8, 16, 16]),
                    in1=IOTA[:].unsqueeze(1).unsqueeze(1).broadcast_to([128, 8, 16, 16]), op=ALU.is_equal),
                    reads=[dAB, d_c], writes=[dOH])
                K.dve.op(lambda e, OHv=OHv, I16fv=I16fv, c=c: e.tensor_tensor(
                    out=OHv, in0=OHv, in1=I16fv[:, :, c, :].unsqueeze(2).broadcast_to([128, 8, 16, 16]), op=ALU.mult),
                    reads=[dIf], writes=[dOH])
                K.dve.op(lambda e, OHv=OHv, E12=E12, c=c: e.tensor_reduce(
                    out=E12[:, c, :].rearrange("p (h j) -> p h j", j=16), in_=OHv, axis=AX.X, op=ALU.add),
                    reads=[dOH], writes=[dE12] if c == 0 else [], adds=[] if c == 0 else [dE12])
            K.dve.op(lambda e, E12=E12: e.tensor_tensor(out=E12[:, 2, :], in0=E12[:, 0, :], in1=E12[:, 1, :], op=ALU.add), writes=[dE12])
            IDS, dIDS = IDSr.next()
            K.dve.op(lambda e, IDS=IDS, E12=E12: e.tensor_copy(out=IDS[:], in_=E12[:, 2, :]), reads=[dE12], writes=[dIDS])
            if "ids_dbg" in scr:
                K.sp.dma(lambda e, IDS=IDS, t0=t0: e.dma_start(out=scr["ids_dbg"][t0:t0 + 128, :], in_=IDS[:]), reads=[dIDS])
                K.sp.dma(lambda e, GT=GT, t0=t0: e.dma_start(out=scr["gate_dbg"][t0:t0 + 128, :], in_=GT[:, 1, :]), reads=[dGT])
            RES[ti] = dict(t0=t0, XH=XH, dXH=dXH, X1=X1, dX1=dX1, IDS=IDS, dIDS=dIDS, GT=GT, dGT=dGT, S8=S8, dS8=dS8)

        GDEPS = {}

        def expert(R):
            t0, XH, dXH, X1, dX1, IDS, dIDS, GT, dGT, S8, dS8 = (R[k] for k in
                ("t0", "XH", "dXH", "X1", "dX1", "IDS", "dIDS", "GT", "dGT", "S8", "dS8"))
            XHb, dXHb = XHbr.next()
            K.act.op(lambda e: e.activation(out=XHb[:], in_=XH[:], func=AF.Copy), reads=[dXH], writes=[dXHb])
            py, dpy = PY.next()
            NGRP = 128 // GRP
            bufs = {}
            gd = GDEPS.setdefault(id(GT), [(Dep(), Dep()) for _ in range(NGRP)])

            def stage_a(g):
                for jj in range(GRP):
                    j = g * GRP + jj
                    GB, dGB = GBr.next()
                    bufs[j] = (GB, dGB)
                    K.pool.dma(lambda e, GB=GB, j=j: e.indirect_dma_start(
                        out=GB[:], out_offset=None, in_=scr["uv_tab"][:, :],
                        in_offset=bass.IndirectOffsetOnAxis(ap=IDS[:, j:j + 1], axis=0)), reads=[dIDS, scr["d_uv"]], writes=[dGB])
                    if jj in ACTDOT:
                        PRD, dPRD = PRDr.next()
                        K.dve.op(lambda e, GB=GB, PRD=PRD: e.tensor_tensor(out=PRD[:], in0=GB[:, 0:D], in1=XHb[:], op=ALU.mult),
                                 reads=[dGB, dXHb], writes=[dPRD])
                        j3, dj3 = junk3r.next()
                        K.act.op(lambda e, PRD=PRD, j=j, j3=j3: e.activation(out=j3[:], in_=PRD[:], func=AF.Copy, accum_out=GT[:, 2, j:j + 1]),
                                 reads=[dPRD], writes=[dj3] + ([gd[g][0]] if jj == 0 else []), adds=[] if jj == 0 else [gd[g][0]])
                    else:
                        j1, dj1 = junkr.next()
                        K.dve.op(lambda e, GB=GB, j=j, j1=j1: e.scalar_tensor_tensor(
                            out=j1[:], in0=GB[:, 0:D], scalar=1.0, in1=XHb[:], op0=ALU.mult, op1=ALU.mult, accum_out=GT[:, 2, j:j + 1]),
                            reads=[dGB, dXHb], writes=[dj1] + ([gd[g][0]] if jj == 0 else []), adds=[] if jj == 0 else [gd[g][0]])
                gs = slice(g * GRP, (g + 1) * GRP)
                K.act.op(lambda e: e.activation(out=GT[:, 3, gs], in_=GT[:, 2, gs], func=AF.Gelu), reads=[gd[g][0]], writes=[gd[g][1]])

            def stage_b(g):
                gs = slice(g * GRP, (g + 1) * GRP)
                K.dve.op(lambda e: e.tensor_tensor(out=GT[:, 3, gs], in0=GT[:, 3, gs], in1=GT[:, 1, gs], op=ALU.mult), reads=[dGT], writes=[gd[g][1]])
                for jj in range(GRP):
                    j = g * GRP + jj
                    GB, dGB = bufs.pop(j)
                    DG, dDG = DGr.next()
                    K.act.op(lambda e, DG=DG, j=j: e.activation(out=DG[:], in_=identb[:], func=AF.Copy, scale=GT[:, 3, j:j + 1]),
                             reads=[gd[g][1], d_c], writes=[dDG])
                    for n in range(2):
                        K.pe.op(lambda e, DG=DG, GB=GB, n=n, j=j: e.matmul(
                            py[:, n, :], DG[:], GB[:, D + n * 512:D + (n + 1) * 512], start=(j == 0), stop=(j == 127)),
                            reads=[dDG, dGB], writes=[dpy] if (j == 0 and n == 0) else [], adds=[] if (j == 0 and n == 0) else [dpy])

            gen = routing(R["next"]) if R.get("next") is not None else None
            for g in range(NGRP):
                stage_a(g)
                if g >= 1:
                    stage_b(g - 1)
                if gen is not None and g >= 2 and g % 2 == 0:
                    try:
                        next(gen)
                    except StopIteration:
                        gen = None
            stage_b(NGRP - 1)
            if gen is not None:
                for _ in gen:
                    pass
            K.dve.op(lambda e: e.tensor_tensor(out=X1[:], in0=X1[:], in1=py[:].rearrange("p a b -> p (a b)"), op=ALU.add),
                     reads=[dpy], writes=[dX1])
            K.act.op(lambda e: e.activation(out=junk2[:], in_=X1[:], func=AF.Square, accum_out=S8[:, 0:1]),
                     reads=[dX1], writes=[d_junk2, dS8])
            K.dve.op(lambda e: e.tensor_scalar(out=S8[:, 1:2], in0=S8[:, 0:1], scalar1=1.0 / D, scalar2=1e-6,
                                               op0=ALU.mult, op1=ALU.add), writes=[dS8])
            K.act.op(lambda e: e.activation(out=S8[:, 2:3], in_=S8[:, 1:2], func=AF.Sqrt), writes=[dS8])
            K.dve.op(lambda e: e.reciprocal(out=S8[:, 3:4], in_=S8[:, 2:3]), writes=[dS8])
            K.dve.op(lambda e: e.scalar_tensor_tensor(
                out=XH[:], in0=X1[:], scalar=S8[:, 3:4], in1=FNW[:], op0=ALU.mult, op1=ALU.mult),
                reads=[dX1, dS8, d_c], writes=[dXH])
            K.sp.dma(lambda e: e.dma_start(out=io["out"][t0:t0 + 128, :], in_=XH[:]), reads=[dXH])

        for _ in routing(0):
            pass
        for ti in range(NTT):
            R = RES.pop(ti)
            R["next"] = ti + 1 if ti + 1 < NTT else None
            expert(R)


def make_consts():
    c = {}
    c["ident_bf"] = np.eye(128, dtype=np.float32).astype(ml_dtypes.bfloat16)
    c["ident_f"] = np.eye(128, dtype=np.float32)
    bo = np.zeros((128, 128), np.float32)
    bo[:64, :64] = 1.0 / 64
    bo[64:, 64:] = 1.0 / 64
    c["blockones"] = bo
    pp = np.arange(128)[:, None]
    ff = np.arange(128)[None, :]
    c["tri"] = ((pp <= ff) & (pp // 64 == ff // 64)).astype(np.float32)
    p6 = np.arange(64)[:, None]
    f6 = np.arange(64)[None, :]
    mk = np.zeros((64, 3, 64), np.float32)
    mk[:, 0, :] = (f6 < p6)
    mk[:, 1, :] = (f6 > p6)
    mk[:, 2, :] = (f6 >= p6)
    c["masks"] = mk
    c["ident64"] = np.eye(64, dtype=np.float32).astype(ml_dtypes.bfloat16)
    c["ones64"] = np.ones((64, 1), np.float32)
    c["iota16"] = np.tile(np.arange(16, dtype=np.float32)[None, :], (128, 1))
    return c


def make_alibi(S):
    al = np.zeros((4, 128, S), np.float32)
    ql = np.arange(128)[:, None]
    m = np.arange(S)[None, :]
    for h in range(4):
        slope = 2.0 ** (-8.0 * (h + 1) / 4)
        v = -slope * (ql - m + (S - 128)).astype(np.float32)
        al[h] = np.where(m <= ql + (S - 128), v, -30000.0)
    return al


def _unused():
    c = {}
    return c


def build(cfg):
    NB, S = cfg["NB"], cfg["S"]
    T = NB * S
    cfg["T"] = T
    dbg = set(cfg.get("debug", ()))
    phases = cfg.get("phases", (1,))
    nc = bass.Bass("TRN2", target_bir_lowering=False)
    io = {}

    def inp(name, shape, dt=F32):
        io[name] = nc.dram_tensor(name, list(shape), dt, kind="ExternalInput").ap()

    inp("x", [T, D])
    inp("norm_mix_w", [1, D])
    inp("w_in", [1, D, IN_COLS])
    inp("ident_bf", [128, 128], BF16)
    inp("ident_f", [128, 128])
    inp("blockones", [128, 128])
    inp("tri", [128, 128])
    inp("masks", [64, 3, 64])
    inp("ident64", [64, 64], BF16)
    inp("ones64", [64, 1])
    inp("alibi", [4, 128, S])
    for nm, shp in [("lam_q1", [1, 64]), ("lam_k1", [1, 64]), ("lam_q2", [1, 64]), ("lam_k2", [1, 64]),
                    ("subln_w", [1, 128])]:
        inp(nm, shp)
    scr = {}

    def scratch(name, shape, dt):
        kind = "ExternalOutput" if name in dbg else "Internal"
        scr[name] = nc.dram_tensor(name, list(shape), dt, kind=kind).ap()

    scratch("zs_tm", [T, SHIFT_COLS], F32)
    scratch("zv_fm", [512, T], F32)
    scratch("qk_fm", [1024, T], BF16)
    scratch("av_tm", [T, 512], BF16)
    scratch("sg_fm", [2048, T], BF16)
    if not cfg.get("chunked", True):
        scratch("rw_tm", [T, 5, 512], F32)
    scratch("ab_tm", [T, 5, 512], BF16)
    scratch("lw_tm", [T, 512], F32)
    scratch("v_fm", [512, T], F32)
    scratch("g_fm", [512, T], F32)
    scratch("coef_fm", [8, T], F32)
    scratch("y_fm", [512, T], F32)
    scratch("ya_fm", [512, T], BF16)
    scr["d_p1"] = Dep()
    scr["d_p2a"] = Dep()
    scr["d_p2b"] = Dep()
    scr["d_p2c"] = Dep()
    scr["d_p3"] = Dep()
    scr["d_p4"] = Dep()
    scr["d_uv"] = Dep()
    scratch("uv_tab", [16384, 2 * D], BF16)
    if "ids_dbg" in dbg:
        scratch("ids_dbg", [T, 128], I32)
        scratch("gate_dbg", [T, 128], F32)
    inp("peer_wq", [1, D, 2048])
    inp("peer_keys", [1, 8, 2, 128, 128])
    inp("peer_u", [1, 16384, D])
    inp("peer_v", [1, 16384, D])
    inp("final_norm_w", [1, D])
    inp("iota16", [128, 16])
    io["out"] = nc.dram_tensor("out", [T, D], F32, kind="ExternalOutput").ap()
    scratch("x1_tm", [T, D], F32)
    scratch("xh_tm", [T, D], F32)
    scratch("xhT_fm", [D, T], BF16)
    for nm, shp in [("proj_a", [1, 512, D]), ("proj_b", [1, 512, D]), ("w_out", [1, D, D]), ("norm_ffn_w", [1, D])]:
        inp(nm, shp)
    scratch("yb_fm", [512, T], BF16)
    for nm, shp in [("shift_mu", [1, SHIFT_COLS]), ("w0", [1, 512]), ("w2", [1, 64, 512]), ("a0", [1, 512]),
                    ("a2", [1, 64, 512]), ("g2", [1, 128, 512]), ("k_k", [1, 512]), ("k_a", [1, 512]),
                    ("r_k", [1, 8, 64]), ("lnx_w", [1, 512]), ("lnx_b", [1, 512])]:
        inp(nm, shp)
    with ExitStack() as es:
        K = Kern(nc, es, pool_slots=cfg.get("pool_slots", 8))
        K.scopes = bool(cfg.get("scopes", False))
        if 5 in phases:
            K.phase = "p0_uvtab"
            RB = 2048
            for r0 in range(0, 16384, RB):
                K.pool.dma(lambda e, r0=r0: e.dma_start(out=scr["uv_tab"][r0:r0 + RB, 0:D], in_=io["peer_u"][0, r0:r0 + RB, :]),
                           adds=[scr["d_uv"]])
                K.pool.dma(lambda e, r0=r0: e.dma_start(out=scr["uv_tab"][r0:r0 + RB, D:2 * D], in_=io["peer_v"][0, r0:r0 + RB, :]),
                           adds=[scr["d_uv"]])
        if 1 in phases:
            K.phase = "p1_inproj"
            phase1(K, cfg, io, scr)
            K.barrier()
        if 2 in phases:
            K.phase = "p2a_prep"
            phase2_prep(K, cfg, io, scr)
            K.barrier()
            K.phase = "p2b_scan"
            if cfg.get("chunked", True):
                phase2_chunk(K, cfg, io, scr)
            else:
                phase2_scan(K, cfg, io, scr)
            K.barrier()
            K.phase = "p2c_post"
            phase2_post(K, cfg, io, scr)
            K.barrier()
        if 3 in phases:
            K.phase = "p3_attn"
            phase3(K, cfg, io, scr)
            K.barrier()
        if 4 in phases:
            K.phase = "p4_merge"
            phase4(K, cfg, io, scr)
            K.barrier()
        if 5 in phases:
            K.phase = "p5_peer"
            phase5(K, cfg, io, scr)
            K.barrier()
        K.finish()
    return nc, io, scr


def kernel(**inputs):
    NB, S = 4, 2048
    cfg = dict(NB=NB, S=S, phases=(1, 2, 3, 4, 5))
    nc, io, scr = build(cfg)
    consts = make_consts()
    consts["alibi"] = make_alibi(S)
    x = np.ascontiguousarray(np.asarray(inputs["x"], dtype=np.float32))
    shared = {}
    for name in io:
        if name in ("x", "out"):
            continue
        if name in consts:
            shared[name] = consts[name]
        elif name == "final_norm_w":
            shared[name] = np.ascontiguousarray(np.asarray(inputs[name], dtype=np.float32).reshape(1, D))
        else:
            shared[name] = np.ascontiguousarray(np.asarray(inputs[name], dtype=np.float32))
    in_maps = []
    for c in range(NCORES):
        m = dict(shared)
        m["x"] = x[c * NB:(c + 1) * NB].reshape(NB * S, D)
        in_maps.append(m)
    res = run_bass_kernel_spmd(nc, in_maps, core_ids=list(range(NCORES)))
    out = np.concatenate([np.asarray(r["out"]).reshape(NB, S, D) for r in res.results], axis=0)
    return out.astype(np.float32)
```

```python
import numpy as np
import ml_dtypes
from contextlib import ExitStack
import concourse.bass as bass
import concourse.mybir as mybir
from concourse.bass_utils import run_bass_kernel_spmd

F32 = mybir.dt.float32
BF16 = mybir.dt.bfloat16
I32 = mybir.dt.int32
U32 = mybir.dt.uint32
ALU = mybir.AluOpType
AF = mybir.ActivationFunctionType
AX = mybir.AxisListType

D = 1024
IN_COLS = 5376
SHIFT_COLS = 1792
NCORES = 8


class Dep:
    __slots__ = ("w", "r", "pw", "pr")

    def __init__(self):
        self.w = {}
        self.r = {}
        self.pw = {}
        self.pr = {}


class Stream:
    def __init__(self, K, name, is_pe=False, ndma=0):
        self.K = K
        self.name = name
        self.sem = K.new_sem("s_" + name)
        self.cnt = 0
        self.items = []
        self.waited = {}
        self.is_pe = is_pe
        self.dsems = [K.new_sem("d_%s%d" % (name, i)) for i in range(ndma)]
        self.duses = [0] * ndma
        self.dj = 0

    def wait_tok(self, tok):
        if tok is None:
            return
        sem, val = tok
        if sem is self.sem and self.is_pe:
            return
        key = id(sem)
        if self.waited.get(key, 0) >= val:
            return
        self.waited[key] = val
        self.items.append(("w", sem, val, self.K.phase))

    def _pre(self, reads, writes, adds):
        for d in reads:
            for t in list(d.w.values()):
                self.wait_tok(t)
        for d in writes:
            for t in list(d.w.values()):
                self.wait_tok(t)
            for t in list(d.r.values()):
                self.wait_tok(t)
        for d in adds:
            for t in list(d.r.values()) + list(d.pr.values()) + list(d.pw.values()):
                self.wait_tok(t)

    def _post(self, tok, reads, writes, adds):
        for d in reads:
            d.r[id(tok[0])] = tok
        for d in writes:
            d.pw = d.w
            d.pr = d.r
            d.w = {id(tok[0]): tok}
            d.r = {}
        for d in adds:
            d.w[id(tok[0])] = tok

    def op(self, fn, reads=(), writes=(), adds=()):
        self._pre(reads, writes, adds)
        self.cnt += 1
        tok = (self.sem, self.cnt)
        self.items.append(("o", fn, self.sem, 1, self.K.phase))
        self._post(tok, reads, writes, adds)
        return tok

    def dma(self, fn, reads=(), writes=(), adds=()):
        self._pre(reads, writes, adds)
        n = len(self.dsems)
        slot = self.dj % n
        self.dj += 1
        if self.duses[slot] > 0:
            self.wait_tok((self.dsems[slot], 16 * self.duses[slot]))
        self.duses[slot] += 1
        tok = (self.dsems[slot], 16 * self.duses[slot])
        self.items.append(("o", fn, self.dsems[slot], 16, self.K.phase))
        self._post(tok, reads, writes, adds)
        return tok

    def replay(self, eng):
        nc = self.K.nc
        cur = None
        ctx = None
        for it in self.items:
            ph = it[-1]
            if self.K.scopes and ph != cur:
                if ctx is not None:
                    ctx.__exit__(None, None, None)
                ctx = nc.named_scope(ph)
                ctx.__enter__()
                cur = ph
            if it[0] == "w":
                eng.wait_ge(it[1], it[2])
            else:
                ins = it[1](eng)
                ins.then_inc(it[2], it[3])
        if ctx is not None:
            ctx.__exit__(None, None, None)


class Kern:
    def __init__(self, nc, es, pool_slots=8):
        self.nc = nc
        self.es = es
        self.nsem = 0
        self.phase = "init"
        self.scopes = False
        self.pe = Stream(self, "pe", is_pe=True)
        self.act = Stream(self, "act", ndma=4)
        self.dve = Stream(self, "dve")
        self.pool = Stream(self, "pool", ndma=pool_slots)
        self.sp = Stream(self, "sp", ndma=8)
        self.uid = 0

    def new_sem(self, name):
        self.nsem += 1
        return self.es.enter_context(self.nc.semaphore(name))

    def sb(self, shape, dt, name=None, es=None):
        self.uid += 1
        nm = "%s_%d" % (name or "t", self.uid)
        return (es or self.es).enter_context(self.nc.sbuf_tensor(nm, list(shape), dt))

    def ps(self, shape, dt, name=None, es=None):
        self.uid += 1
        nm = "%s_%d" % (name or "p", self.uid)
        return (es or self.es).enter_context(self.nc.psum_tensor(nm, list(shape), dt))

    def dram(self, name, shape, dt, kind="Internal"):
        return self.nc.dram_tensor(name, list(shape), dt, kind=kind)

    def streams(self):
        return [self.pe, self.act, self.dve, self.pool, self.sp]

    def barrier(self):
        st = self.streams()
        toks = []
        for q in st:
            if q.cnt > 0:
                toks.append((q.sem, q.cnt))
            for i, sem in enumerate(q.dsems):
                if q.duses[i] > 0:
                    toks.append((sem, 16 * q.duses[i]))
        for s_ in st:
            for t in toks:
                s_.wait_tok(t)

    def finish(self):
        streams = [self.pe, self.act, self.dve, self.pool, self.sp]
        for s in streams:
            for q in streams:
                for i, sem in enumerate(q.dsems):
                    if q.duses[i] > 0:
                        s.wait_tok((sem, 16 * q.duses[i]))
        with self.nc.allow_non_contiguous_dma(reason="small strided param loads"), self.nc.Block() as block:
            @block.tensor
            def _(e):
                self.pe.replay(e)

            @block.scalar
            def _(e):
                self.act.replay(e)

            @block.vector
            def _(e):
                self.dve.replay(e)

            @block.gpsimd
            def _(e):
                self.pool.replay(e)

            @block.sync
            def _(e):
                self.sp.replay(e)


class Rot:
    def __init__(self, K, n, shape, dt, name, psum=False, es=None):
        self.t = [(K.ps if psum else K.sb)(shape, dt, name, es=es) for _ in range(n)]
        self.d = [Dep() for _ in range(n)]
        self.i = 0

    def next(self):
        j = self.i % len(self.t)
        self.i += 1
        return self.t[j], self.d[j]


def phase1(K, cfg, io, scr):
    nc = K.nc
    T = cfg["T"]
    NT = T // 512
    with ExitStack() as es:
        ident = K.sb([128, 128], BF16, "ident", es)
        d_ident = Dep()
        K.sp.dma(lambda e: e.dma_start(out=ident[:], in_=io["ident_bf"][:, :]), writes=[d_ident])
        nw = K.sb([128, 8], F32, "nw", es)
        d_nw = Dep()
        K.sp.dma(lambda e: e.dma_start(out=nw[:], in_=io["norm_mix_w"].rearrange("o (c p) -> p (o c)", p=128)),
                 writes=[d_nw])
        wt = K.sb([128, 8, IN_COLS], BF16, "wt", es)
        d_wt = Dep()
        wst = Rot(K, 2, [128, 1344], F32, "wst", es=es)
        q = 0
        for kc in range(8):
            for cp in range(4):
                st, dst = wst.next()
                eng = K.sp if q % 2 == 0 else K.pool
                q += 1
                eng.dma(lambda e, st=st, kc=kc, cp=cp: e.dma_start(
                    out=st[:], in_=io["w_in"][0, kc * 128:(kc + 1) * 128, cp * 1344:(cp + 1) * 1344]), writes=[dst])
                K.act.op(lambda e, st=st, kc=kc, cp=cp: e.activation(
                    out=wt[:, kc, cp * 1344:(cp + 1) * 1344], in_=st[:], func=AF.Copy, scale=nw[:, kc:kc + 1]),
                    reads=[dst, d_nw], writes=[d_wt])

        xs = Rot(K, 2, [128, D], F32, "xs", es=es)
        junk = K.sb([128, D], BF16, "junk", es)
        d_junk = Dep()
        xn = Rot(K, 2, [128, D], BF16, "xn", es=es)
        st4 = Rot(K, 4, [128, 4], F32, "st4", es=es)
        hT = Rot(K, 2, [128, 8, 512], BF16, "hT", es=es)
        ptr = Rot(K, 2, [128, 8, 128], BF16, "ptr", psum=True, es=es)
        pmm = Rot(K, 4, [128, 512], F32, "pmm", psum=True, es=es)
        o32 = Rot(K, 3, [128, 512], F32, "o32", es=es)
        o16 = Rot(K, 3, [128, 512], BF16, "o16", es=es)
        ev = [0]

        def evac(pt, pd, ncols, kind, dst_ap):
            if kind == "f32":
                ot, od = o32.next()
            else:
                ot, od = o16.next()
            use_act = (kind == "sig") or (ev[0] % 2 == 0)
            ev[0] += 1
            if kind == "sig":
                K.act.op(lambda e: e.activation(out=ot[:, :ncols], in_=pt[:, :ncols], func=AF.Sigmoid),
                         reads=[pd], writes=[od])
            elif use_act:
                K.act.op(lambda e: e.activation(out=ot[:, :ncols], in_=pt[:, :ncols], func=AF.Copy),
                         reads=[pd], writes=[od])
            else:
                K.dve.op(lambda e: e.tensor_copy(out=ot[:, :ncols], in_=pt[:, :ncols]), reads=[pd], writes=[od])
            K.sp.dma(lambda e: e.dma_start(out=dst_ap, in_=ot[:, :ncols]), reads=[od], adds=[scr["d_p1"]])

        for ti in range(NT):
            h_t, h_d = hT.next()
            for sub in range(4):
                t0 = ti * 512 + sub * 128
                x_t, x_d = xs.next()
                K.pool.dma(lambda e, x_t=x_t, t0=t0: e.dma_start(out=x_t[:], in_=io["x"][t0:t0 + 128, :]),
                           writes=[x_d])
                s_t, s_d = st4.next()
                K.dve.op(lambda e, s_t=s_t: e.memset(s_t[:], 0.0), writes=[s_d])
                K.act.op(lambda e, x_t=x_t, s_t=s_t: e.activation(out=junk[:], in_=x_t[:], func=AF.Square,
                                                                    accum_out=s_t[:, 0:1]),
                         reads=[x_d], writes=[d_junk, s_d])
                K.dve.op(lambda e, s_t=s_t: e.tensor_scalar(out=s_t[:, 1:2], in0=s_t[:, 0:1], scalar1=1.0 / D,
                                                            scalar2=1e-6, op0=ALU.mult, op1=ALU.add),
                         reads=[s_d], writes=[s_d])
                K.act.op(lambda e, s_t=s_t: e.activation(out=s_t[:, 3:4], in_=s_t[:, 1:2], func=AF.Sqrt),
                         reads=[s_d], writes=[s_d])
                K.dve.op(lambda e, s_t=s_t: e.reciprocal(out=s_t[:, 2:3], in_=s_t[:, 3:4]),
                         reads=[s_d], writes=[s_d])
                n_t, n_d = xn.next()
                K.dve.op(lambda e, x_t=x_t, s_t=s_t, n_t=n_t: e.tensor_scalar(
                    out=n_t[:], in0=x_t[:], scalar1=s_t[:, 2:3], scalar2=None, op0=ALU.mult),
                    reads=[x_d, s_d], writes=[n_d])
                p_t, p_d = ptr.next()
                for kc in range(8):
                    K.pe.op(lambda e, p_t=p_t, n_t=n_t, kc=kc: e.transpose(
                        out=p_t[:, kc, :], in_=n_t[:, kc * 128:(kc + 1) * 128], identity=ident[:]),
                        reads=[n_d, d_ident], writes=[p_d])
                K.act.op(lambda e, p_t=p_t, h_t=h_t, sub=sub: e.activation(
                    out=h_t[:, :, sub * 128:(sub + 1) * 128], in_=p_t[:], func=AF.Copy),
                    reads=[p_d], writes=[h_d])
            tsl = slice(ti * 512, (ti + 1) * 512)
            for sub in range(4):
                r0 = ti * 512 + sub * 128
                for (c0, ncols, kind, name, dc0) in [(0, 512, "f32", "zs_tm", 0), (512, 512, "f32", "zs_tm", 512),
                                                    (1024, 512, "f32", "zs_tm", 1024),
                                                    (1536, 256, "f32", "zs_tm", 1536),
                                                    (2816, 512, "bf16", "av_tm", 0)]:
                    pt, pd = pmm.next()
                    for kc in range(8):
                        K.pe.op(lambda e, pt=pt, kc=kc, sub=sub, c0=c0, ncols=ncols, h_t=h_t: e.matmul(
                            pt[:, :ncols], h_t[:, kc, sub * 128:(sub + 1) * 128], wt[:, kc, c0:c0 + ncols],
                            start=(kc == 0), stop=(kc == 7)), reads=[h_d, d_wt], writes=[pd])
                    evac(pt, pd, ncols, kind, scr[name][r0:r0 + 128, dc0:dc0 + ncols])
            fm = []
            for j in range(8):
                fm.append((1792 + j * 128, "bf16", "qk_fm", j * 128))
            for j in range(16):
                fm.append((3328 + j * 128, "sig", "sg_fm", j * 128))
            for (c0, kind, name, r0) in fm:
                pt, pd = pmm.next()
                for kc in range(8):
                    K.pe.op(lambda e, pt=pt, kc=kc, c0=c0, h_t=h_t: e.matmul(
                        pt[:, :], wt[:, kc, c0:c0 + 128], h_t[:, kc, :], start=(kc == 0), stop=(kc == 7)),
                        reads=[h_d, d_wt], writes=[pd])
                evac(pt, pd, 512, kind, scr[name][r0:r0 + 128, tsl])


def dap(apobj, offset, dims):
    return bass.AP(tensor=apobj.tensor, offset=offset, ap=[list(d) for d in dims])


def bcast_load(K, eng, dst, src_row_ap, n, dep):
    eng.dma(lambda e: e.dma_start(out=dst, in_=src_row_ap.broadcast_to([128, n])), writes=[dep])


def phase2_prep(K, cfg, io, scr):
    T, S, NB = cfg["T"], cfg["S"], cfg["NB"]
    NTT = T // 128
    with ExitStack() as es:
        identb = K.sb([128, 128], BF16, "identb", es)
        identf = K.sb([128, 128], F32, "identf", es)
        d_c = Dep()
        K.sp.dma(lambda e: e.dma_start(out=identb[:], in_=io["ident_bf"][:, :]), adds=[d_c])
        K.sp.dma(lambda e: e.dma_start(out=identf[:], in_=io["ident_f"][:, :]), adds=[d_c])
        MU = K.sb([128, SHIFT_COLS], F32, "MU", es)
        PR = K.sb([128, 5, 512], F32, "PR", es)
        K.sp.dma(lambda e: e.dma_start(out=MU[:], in_=io["shift_mu"][0:1, :].broadcast_to([128, SHIFT_COLS])), adds=[d_c])
        for j, nm in enumerate(["w0", "a0", "k_k", "k_a"]):
            K.pool.dma(lambda e, j=j, nm=nm: e.dma_start(out=PR[:, j, :], in_=io[nm][0:1, :].broadcast_to([128, 512])),
                       adds=[d_c])
        K.pool.dma(lambda e: e.dma_start(out=PR[:, 4, :], in_=io["r_k"].rearrange("o h k -> o (h k)").broadcast_to([128, 512])),
                   adds=[d_c])
        cst = K.sb([128, 2], F32, "cst", es)
        K.dve.op(lambda e: e.memset(cst[:, 0:1], 1.0), adds=[d_c])
        K.dve.op(lambda e: e.memset(cst[:, 1:2], -0.5), adds=[d_c])
        wst = K.sb([128, 3, 512], F32, "lwst", es)
        d_wst = Dep()
        K.dve.op(lambda e: e.memset(wst[:], 0.0), writes=[d_wst])
        K.sp.dma(lambda e: e.dma_start(out=wst[0:64, 0, :], in_=io["w2"][0, :, :]), reads=[d_wst], adds=[d_wst])
        K.sp.dma(lambda e: e.dma_start(out=wst[64:128, 1, :], in_=io["a2"][0, :, :]), reads=[d_wst], adds=[d_wst])
        K.sp.dma(lambda e: e.dma_start(out=wst[:, 2, :], in_=io["g2"][0, :, :]), reads=[d_wst], adds=[d_wst])
        LW = K.sb([128, 3, 512], BF16, "LW", es)
        K.dve.op(lambda e: e.tensor_copy(out=LW[:], in_=wst[:]), reads=[d_wst], adds=[d_c])

        Zr = Rot(K, 2, [128, SHIFT_COLS], F32, "Z", es=es)
        Zpr = Rot(K, 2, [128, SHIFT_COLS], F32, "Zp", es=es)
        ZSr = Rot(K, 2, [128, SHIFT_COLS], F32, "ZS", es=es)
        OUTr = Rot(K, 2, [128, 5, 512], F32, "OUT", es=es)
        Er = Rot(K, 2, [128, 192], F32, "E", es=es)
        Lr = Rot(K, 2, [128, 256], BF16, "L", es=es)
        LTr = Rot(K, 2, [128, 2, 128], BF16, "LT", es=es)
        Ur = Rot(K, 2, [128, 512], F32, "U", es=es)
        UAr = Rot(K, 2, [128, 512], F32, "UA", es=es)
        KKr = Rot(K, 2, [128, 512], F32, "KKt", es=es)
        SQr = Rot(K, 2, [128, 512], F32, "SQ", es=es)
        T1r = Rot(K, 2, [128, 512], F32, "T1", es=es)
        T2r = Rot(K, 2, [128, 512], F32, "T2", es=es)
        S8r = Rot(K, 2, [128, 4, 8], F32, "S8", es=es)
        VTr = Rot(K, 2, [128, 4, 128], F32, "VT", es=es)
        GTr = Rot(K, 2, [128, 4, 128], F32, "GT", es=es)
        CTr = Rot(K, 2, [8, 128], F32, "CT", es=es)
        PT = Rot(K, 1, [128, 2, 128], BF16, "PT", psum=True, es=es)
        PW = Rot(K, 1, [128, 512], F32, "PW", psum=True, es=es)
        PA = Rot(K, 1, [128, 512], F32, "PA", psum=True, es=es)
        PG = Rot(K, 1, [128, 4, 128], F32, "PG", psum=True, es=es)
        PV = Rot(K, 1, [128, 4, 128], F32, "PV", psum=True, es=es)
        PC = Rot(K, 1, [8, 128], F32, "PC", psum=True, es=es)
        chunked = cfg.get("chunked", True)
        if chunked:
            PL = Rot(K, 1, [128, 512], F32, "PL", psum=True, es=es)
            TRI = K.sb([128, 128], F32, "TRI", es)
            K.sp.dma(lambda e: e.dma_start(out=TRI[:], in_=io["tri"][:, :]), adds=[d_c])
            LWr = Rot(K, 2, [128, 512], F32, "LWt", es=es)
            ELr = Rot(K, 2, [128, 3, 512], F32, "EL", es=es)
            ABr = Rot(K, 2, [128, 5, 512], BF16, "AB", es=es)
        dq = [0]

        def ldq():
            dq[0] += 1
            return K.sp if dq[0] % 2 == 0 else K.pool

        def tile_gen(ti):
            t0 = ti * 128
            first = (t0 % S == 0)
            Z, dZ = Zr.next()
            Zp, dZp = Zpr.next()
            ZS, dZS = ZSr.next()
            OUT, dO = OUTr.next()
            ldq().dma(lambda e, Z=Z, t0=t0: e.dma_start(out=Z[:], in_=scr["zs_tm"][t0:t0 + 128, :]),
                      reads=[scr["d_p1"]], writes=[dZ])
            if first:
                K.pool.op(lambda e, Zp=Zp: e.memset(Zp[0:32, :], 0.0), writes=[dZp])
                ldq().dma(lambda e, Zp=Zp, t0=t0: e.dma_start(out=Zp[1:128, :], in_=scr["zs_tm"][t0:t0 + 127, :]),
                          reads=[scr["d_p1"], dZp], adds=[dZp])
            else:
                ldq().dma(lambda e, Zp=Zp, t0=t0: e.dma_start(out=Zp[:], in_=scr["zs_tm"][t0 - 1:t0 + 127, :]),
                          reads=[scr["d_p1"]], writes=[dZp])
            CS = 1216
            dZSa, dZSb = Dep(), Dep()
            K.dve.op(lambda e, Z=Z, Zp=Zp, ZS=ZS: e.tensor_tensor(out=ZS[:, :CS], in0=Zp[:, :CS], in1=Z[:, :CS], op=ALU.subtract),
                     reads=[dZ, dZp], writes=[dZS])
            K.pool.op(lambda e, Z=Z, Zp=Zp, ZS=ZS: e.tensor_tensor(out=ZS[:, CS:], in0=Zp[:, CS:], in1=Z[:, CS:], op=ALU.subtract),
                      reads=[dZ, dZp, dZS], writes=[dZSb])
            K.dve.op(lambda e, ZS=ZS: e.tensor_tensor(out=ZS[:, :CS], in0=ZS[:, :CS], in1=MU[:, :CS], op=ALU.mult),
                     reads=[d_c, dZS], writes=[dZSa])
            K.pool.op(lambda e, ZS=ZS: e.tensor_tensor(out=ZS[:, CS:], in0=ZS[:, CS:], in1=MU[:, CS:], op=ALU.mult),
                      reads=[d_c], writes=[dZSb])
            K.dve.op(lambda e, Z=Z, ZS=ZS: e.tensor_tensor(out=ZS[:, :CS], in0=ZS[:, :CS], in1=Z[:, :CS], op=ALU.add),
                     reads=[dZ], writes=[dZSa])
            K.pool.op(lambda e, Z=Z, ZS=ZS: e.tensor_tensor(out=ZS[:, CS:], in0=ZS[:, CS:], in1=Z[:, CS:], op=ALU.add),
                      reads=[dZ], writes=[dZSb])
            K.dve.op(lambda e, ZS=ZS: e.tensor_copy(out=ZS[:, 0:1], in_=ZS[:, 0:1]), reads=[dZSa, dZSb], writes=[dZS])
            r_ap = ZS[:, 0:512]
            k_ap = ZS[:, 512:1024]
            K.act.op(lambda e, OUT=OUT, ZS=ZS: e.activation(out=OUT[:, 4, :], in_=ZS[:, 0:512], func=AF.Copy),
                     reads=[dZS], writes=[dO])
            yield
            E, dE = Er.next()
            L, dL = Lr.next()
            K.act.op(lambda e, E=E, ZS=ZS: e.activation(out=E[:, 0:64], in_=ZS[:, 1536:1600], func=AF.Exp, scale=-2.0),
                     reads=[dZS], writes=[dE])
            K.act.op(lambda e, E=E, ZS=ZS: e.activation(out=E[:, 64:192], in_=ZS[:, 1664:1792], func=AF.Exp, scale=-1.0),
                     reads=[dZS], adds=[dE])
            K.act.op(lambda e, E=E: e.activation(out=E[:], in_=E[:], func=AF.Ln, bias=cst[:, 0:1]), reads=[d_c], writes=[dE])
            K.act.op(lambda e, E=E: e.activation(out=E[:], in_=E[:], func=AF.Exp, scale=-1.0), writes=[dE])
            K.dve.op(lambda e, E=E, L=L: e.tensor_scalar(out=L[:, 0:64], in0=E[:, 0:64], scalar1=2.0, scalar2=-1.0,
                                                        op0=ALU.mult, op1=ALU.add), reads=[dE], writes=[dL])
            K.act.op(lambda e, L=L, ZS=ZS: e.activation(out=L[:, 64:128], in_=ZS[:, 1600:1664], func=AF.Copy),
                     reads=[dZS, dL], adds=[dL])
            K.act.op(lambda e, L=L, E=E: e.activation(out=L[:, 128:256], in_=E[:, 64:192], func=AF.Copy),
                     reads=[dE, dL], adds=[dL])
            yield
            pt, dpt = PT.next()
            K.pe.op(lambda e, pt=pt, L=L: e.transpose(out=pt[:, 0, :], in_=L[:, 0:128], identity=identb[:]),
                    reads=[dL, d_c], writes=[dpt])
            K.pe.op(lambda e, pt=pt, L=L: e.transpose(out=pt[:, 1, :], in_=L[:, 128:256], identity=identb[:]),
                    reads=[dL, d_c], adds=[dpt])
            LT, dLT = LTr.next()
            K.act.op(lambda e, pt=pt, LT=LT: e.activation(out=LT[:], in_=pt[:], func=AF.Copy), reads=[dpt], writes=[dLT])
            yield
            pw, dpw = PW.next()
            pa, dpa = PA.next()
            pg, dpg = PG.next()
            K.pe.op(lambda e, pw=pw, LT=LT: e.matmul(pw[:], LT[:, 0, :], LW[:, 0, :], start=True, stop=True),
                    reads=[dLT, d_c], writes=[dpw])
            K.pe.op(lambda e, pa=pa, LT=LT: e.matmul(pa[:], LT[:, 0, :], LW[:, 1, :], start=True, stop=True),
                    reads=[dLT, d_c], writes=[dpa])
            for j in range(4):
                K.pe.op(lambda e, pg=pg, LT=LT, j=j: e.matmul(pg[:, j, :], LW[:, 2, j * 128:(j + 1) * 128], LT[:, 1, :],
                                                             start=True, stop=True),
                        reads=[dLT, d_c], writes=[dpg] if j == 0 else [], adds=[] if j == 0 else [dpg])
            GT, dGT = GTr.next()
            K.act.op(lambda e, pg=pg, GT=GT: e.activation(out=GT[:], in_=pg[:], func=AF.Copy), reads=[dpg], writes=[dGT])
            K.sp.dma(lambda e, GT=GT, t0=t0: e.dma_start(
                out=dap(scr["g_fm"], t0, [[T, 128], [128 * T, 4], [1, 128]]), in_=GT[:]),
                reads=[dGT], adds=[scr["d_p2a"]])
            yield
            U, dU = Ur.next()
            K.dve.op(lambda e, U=U, pw=pw: e.tensor_tensor(out=U[:], in0=pw[:], in1=PR[:, 0, :], op=ALU.add),
                     reads=[dpw, d_c], writes=[dU])
            K.act.op(lambda e, U=U: e.activation(out=U[:], in_=U[:], func=AF.Exp, scale=-1.0), writes=[dU])
            K.act.op(lambda e, U=U: e.activation(out=U[:], in_=U[:], func=AF.Ln, bias=cst[:, 0:1]), reads=[d_c], writes=[dU])
            K.act.op(lambda e, U=U: e.activation(out=U[:], in_=U[:], func=AF.Exp, scale=-1.0, bias=cst[:, 1:2]),
                     reads=[d_c], writes=[dU])
            K.act.op(lambda e, U=U, OUT=OUT: e.activation(out=OUT[:, 0, :], in_=U[:], func=AF.Exp, scale=-1.0),
                     reads=[dU], adds=[dO])
            yield
            UA, dUA = UAr.next()
            K.dve.op(lambda e, UA=UA, pa=pa: e.tensor_tensor(out=UA[:], in0=pa[:], in1=PR[:, 1, :], op=ALU.add),
                     reads=[dpa, d_c], writes=[dUA])
            K.act.op(lambda e, UA=UA: e.activation(out=UA[:], in_=UA[:], func=AF.Exp, scale=-1.0), writes=[dUA])
            K.act.op(lambda e, UA=UA: e.activation(out=UA[:], in_=UA[:], func=AF.Ln, bias=cst[:, 0:1]), reads=[d_c], writes=[dUA])
            K.act.op(lambda e, UA=UA: e.activation(out=UA[:], in_=UA[:], func=AF.Exp, scale=-1.0), writes=[dUA])
            yield
            KKt, dKK = KKr.next()
            SQ, dSQ = SQr.next()
            S8, dS8 = S8r.next()
            K.dve.op(lambda e, KKt=KKt, ZS=ZS: e.tensor_tensor(out=KKt[:], in0=ZS[:, 512:1024], in1=PR[:, 2, :], op=ALU.mult),
                     reads=[dZS, d_c], writes=[dKK])
            K.pool.op(lambda e, KKt=KKt, SQ=SQ: e.tensor_tensor(out=SQ[:], in0=KKt[:], in1=KKt[:], op=ALU.mult),
                      reads=[dKK], writes=[dSQ])
            K.dve.op(lambda e, SQ=SQ, S8=S8: e.tensor_reduce(out=S8[:, 0, :], in_=SQ[:].rearrange("p (h k) -> p h k", k=64),
                                                            axis=AX.X, op=ALU.add), reads=[dSQ], writes=[dS8])
            K.dve.op(lambda e, S8=S8: e.tensor_scalar(out=S8[:, 0, :], in0=S8[:, 0, :], scalar1=1e-24, scalar2=None,
                                                      op0=ALU.max), writes=[dS8])
            K.act.op(lambda e, S8=S8: e.activation(out=S8[:, 1, :], in_=S8[:, 0, :], func=AF.Ln), writes=[dS8])
            K.act.op(lambda e, S8=S8: e.activation(out=S8[:, 2, :], in_=S8[:, 1, :], func=AF.Exp, scale=-0.5), writes=[dS8])
            K.dve.op(lambda e, KKt=KKt, S8=S8, OUT=OUT: e.tensor_tensor(
                out=OUT[:, 1, :].rearrange("p (h k) -> p h k", k=64), in0=KKt[:].rearrange("p (h k) -> p h k", k=64),
                in1=S8[:, 2, :].unsqueeze(2).broadcast_to([128, 8, 64]), op=ALU.mult),
                reads=[dKK, dS8, dO], adds=[dO])
            K.dve.op(lambda e, OUT=OUT, UA=UA: e.scalar_tensor_tensor(
                out=OUT[:, 2, :], in0=OUT[:, 1, :], scalar=-1.0, in1=UA[:], op0=ALU.mult, op1=ALU.mult),
                reads=[dUA, dO], adds=[dO])
            yield
            T1, dT1 = T1r.next()
            K.dve.op(lambda e, T1=T1, UA=UA: e.scalar_tensor_tensor(
                out=T1[:], in0=UA[:], scalar=-1.0, in1=PR[:, 3, :], op0=ALU.add, op1=ALU.mult),
                reads=[dUA, d_c], writes=[dT1])
            K.dve.op(lambda e, T1=T1, OUT=OUT, ZS=ZS: e.scalar_tensor_tensor(
                out=OUT[:, 3, :], in0=T1[:], scalar=1.0, in1=ZS[:, 512:1024], op0=ALU.add, op1=ALU.mult),
                reads=[dT1, dZS, dO], adds=[dO])
            T2, dT2 = T2r.next()
            K.pool.op(lambda e, T2=T2, OUT=OUT, ZS=ZS: e.tensor_tensor(out=T2[:], in0=OUT[:, 3, :], in1=ZS[:, 0:512], op=ALU.mult),
                      reads=[dO, dZS], writes=[dT2])
            K.pool.op(lambda e, T2=T2: e.tensor_tensor(out=T2[:], in0=T2[:], in1=PR[:, 4, :], op=ALU.mult),
                      reads=[d_c], writes=[dT2])
            K.dve.op(lambda e, T2=T2, S8=S8: e.tensor_reduce(out=S8[:, 3, :], in_=T2[:].rearrange("p (h k) -> p h k", k=64),
                                                            axis=AX.X, op=ALU.add), reads=[dT2], writes=[dS8])
            yield
            pv, dpv = PV.next()
            pc, dpc = PC.next()
            for j in range(4):
                K.pe.op(lambda e, pv=pv, ZS=ZS, j=j: e.transpose(out=pv[:, j, :], in_=ZS[:, 1024 + j * 128:1024 + (j + 1) * 128],
                                                                identity=identf[:]),
                        reads=[dZS, d_c], writes=[dpv] if j == 0 else [], adds=[] if j == 0 else [dpv])
            K.pe.op(lambda e, pc=pc, S8=S8: e.transpose(out=pc[:, :], in_=S8[:, 3, :], identity=identf[:]),
                    reads=[dS8, d_c], writes=[dpc])
            VT, dVT = VTr.next()
            CT, dCT = CTr.next()
            K.act.op(lambda e, pv=pv, VT=VT: e.activation(out=VT[:], in_=pv[:], func=AF.Copy), reads=[dpv], writes=[dVT])
            K.act.op(lambda e, pc=pc, CT=CT: e.activation(out=CT[:], in_=pc[:], func=AF.Copy), reads=[dpc], writes=[dCT])
            K.sp.dma(lambda e, VT=VT, t0=t0: e.dma_start(
                out=dap(scr["v_fm"], t0, [[T, 128], [128 * T, 4], [1, 128]]), in_=VT[:]),
                reads=[dVT], adds=[scr["d_p2a"]])
            K.sp.dma(lambda e, CT=CT, t0=t0: e.dma_start(out=scr["coef_fm"][:, t0:t0 + 128], in_=CT[:]),
                     reads=[dCT], adds=[scr["d_p2a"]])
            yield
            if not chunked:
                K.pool.dma(lambda e, OUT=OUT, t0=t0: e.dma_start(out=scr["rw_tm"][t0:t0 + 128, :, :], in_=OUT[:]),
                           reads=[dO], adds=[scr["d_p2a"]])
            else:
                LWt, dLW = LWr.next()
                K.dve.op(lambda e, LWt=LWt, U=U: e.tensor_scalar(out=LWt[:], in0=U[:], scalar1=-1.0, scalar2=None, op0=ALU.mult),
                         reads=[dU], writes=[dLW])
                pl, dpl = PL.next()
                K.pe.op(lambda e, pl=pl, LWt=LWt: e.matmul(pl[:], TRI[:], LWt[:], start=True, stop=True),
                        reads=[dLW, d_c], writes=[dpl])
                EL, dEL = ELr.next()
                K.act.op(lambda e, EL=EL, pl=pl: e.activation(out=EL[:, 0, :], in_=pl[:], func=AF.Exp), reads=[dpl], writes=[dEL])
                K.act.op(lambda e, EL=EL, pl=pl: e.activation(out=EL[:, 1, :], in_=pl[:], func=AF.Exp, scale=-1.0),
                         reads=[dpl], adds=[dEL])
                K.dve.op(lambda e, EL=EL, pl=pl, U=U: e.tensor_tensor(out=EL[:, 2, :], in0=pl[:], in1=U[:], op=ALU.add),
                         reads=[dpl, dU, dEL], adds=[dEL])
                K.act.op(lambda e, EL=EL: e.activation(out=EL[:, 2, :], in_=EL[:, 2, :], func=AF.Exp), reads=[dEL], adds=[dEL])
                AB, dAB = ABr.next()
                K.dve.op(lambda e, AB=AB, OUT=OUT, EL=EL: e.tensor_tensor(out=AB[:, 0, :], in0=OUT[:, 1, :], in1=EL[:, 2, :], op=ALU.mult),
                         reads=[dO, dEL], writes=[dAB])
                K.dve.op(lambda e, AB=AB, OUT=OUT, EL=EL: e.scalar_tensor_tensor(
                    out=AB[:, 1, :], in0=OUT[:, 2, :], scalar=-1.0, in1=EL[:, 1, :], op0=ALU.mult, op1=ALU.mult),
                    reads=[dO, dEL, dAB], adds=[dAB])
                K.pool.op(lambda e, AB=AB, OUT=OUT, EL=EL: e.tensor_tensor(out=AB[:, 2, :], in0=OUT[:, 3, :], in1=EL[:, 1, :], op=ALU.mult),
                          reads=[dO, dEL, dAB], adds=[dAB])
                K.pool.op(lambda e, AB=AB, OUT=OUT, EL=EL: e.tensor_tensor(out=AB[:, 3, :], in0=OUT[:, 4, :], in1=EL[:, 0, :], op=ALU.mult),
                          reads=[dO, dEL, dAB], adds=[dAB])
                K.act.op(lambda e, AB=AB, ZS=ZS: e.activation(out=AB[:, 4, :], in_=ZS[:, 1024:1536], func=AF.Copy),
                         reads=[dZS, dAB], adds=[dAB])
                K.pool.dma(lambda e, AB=AB, t0=t0: e.dma_start(out=scr["ab_tm"][t0:t0 + 128, :, :], in_=AB[:]),
                           reads=[dAB], adds=[scr["d_p2a"]])
                K.sp.dma(lambda e, LWt=LWt, t0=t0: e.dma_start(out=scr["lw_tm"][t0:t0 + 128, :], in_=LWt[:]),
                         reads=[dLW], adds=[scr["d_p2a"]])

        LAG = cfg.get("prep_lag", 3)
        active = []
        nxt = 0
        while nxt < NTT or active:
            if len(active) < 2 and nxt < NTT and (not active or active[0][1] >= LAG):
                active.append([tile_gen(nxt), 0])
                nxt += 1
            for a in list(active):
                try:
                    next(a[0])
                    a[1] += 1
                except StopIteration:
                    active.remove(a)


def phase2_scan(K, cfg, io, scr):
    T, S, NB = cfg["T"], cfg["S"], cfg["NB"]
    NBH = 2 if NB >= 2 else 1
    NBL = NB // NBH
    NP = 64 * NBH
    TS = 2
    TC = 128
    RW = 2560
    with ExitStack() as es:
        St = K.sb([128, NBL, 8, 64], F32, "St", es)
        dS = Dep()
        TMP = K.sb([128, NBL, 8, 64], F32, "TMP", es)
        dT = Dep()
        SA = K.sb([128, NBL, 8], F32, "SA", es)
        dSA = Dep()
        T2r = Rot(K, 2, [128, NBL, 8, 64], F32, "TMP2", es=es)
        T3r = Rot(K, 2, [128, NBL, 8, 64], F32, "TMP3", es=es)
        BCr = Rot(K, 3, [128, TS, NBL, 5, 8, 64], F32, "BC", es=es)
        Vr = Rot(K, 2, [128, NBL, 8, TC], F32, "Vf", es=es)
        Yr = Rot(K, 2, [128, NBL, 8, TC], F32, "Yf", es=es)
        K.dve.op(lambda e: e.memset(St[:], 0.0), writes=[dS])
        qi = [0]

        def q():
            qi[0] += 1
            return K.sp if qi[0] % 2 == 0 else K.act

        def load_bc(ci):
            t = ci * TS
            BC, dBC = BCr.next()
            first = True
            for bhi in range(NBH):
                for blo in range(NBL):
                    src = dap(scr["rw_tm"], ((bhi * NBL + blo) * S + t) * RW, [[0, 64], [RW, TS], [1, RW]])
                    dst = BC[bhi * 64:(bhi + 1) * 64, :, blo].rearrange("p t j h k -> p t (j h k)")
                    q().dma(lambda e, src=src, dst=dst: e.dma_start(out=dst, in_=src), reads=[scr["d_p2a"]],
                            writes=[dBC] if first else [], adds=[] if first else [dBC])
                    first = False
            return BC, dBC

        def vy_ap(name, bhi, blo, t):
            return dap(scr[name], (bhi * NBL + blo) * S + t, [[T, 64], [64 * T, 8], [1, TC]])

        def load_v(ni):
            Vf, dV = Vr.next()
            first = True
            for bhi in range(NBH):
                for blo in range(NBL):
                    src = vy_ap("v_fm", bhi, blo, ni * TC)
                    dst = Vf[bhi * 64:(bhi + 1) * 64, blo]
                    q().dma(lambda e, src=src, dst=dst: e.dma_start(out=dst, in_=src), reads=[scr["d_p2a"]],
                            writes=[dV] if first else [], adds=[] if first else [dV])
                    first = False
            return Vf, dV

        nch = S // TS
        bcs = {}
        bcs[0] = load_bc(0)
        if nch > 1:
            bcs[1] = load_bc(1)
        vs = {0: load_v(0)}
        P = slice(0, NP)
        for t in range(S):
            ci, ts = divmod(t, TS)
            ni, tt = divmod(t, TC)
            if ts == 0 and ci + 2 < nch:
                bcs[ci + 2] = load_bc(ci + 2)
            if tt == 0:
                if (ni + 1) * TC < S:
                    vs[ni + 1] = load_v(ni + 1)
                Yf, dY = Yr.next()
            BC, dBC = bcs[ci]
            Vf, dV = vs[ni]
            W_ = BC[P, ts, :, 0]
            KN = BC[P, ts, :, 1]
            KA = BC[P, ts, :, 2]
            KP = BC[P, ts, :, 3]
            R_ = BC[P, ts, :, 4]
            shp = [NP, NBL, 8, 64]
            K.dve.op(lambda e, KN=KN: e.tensor_tensor(out=TMP[P], in0=St[P], in1=KN, op=ALU.mult),
                     reads=[dS, dBC], writes=[dT])
            K.dve.op(lambda e: e.tensor_reduce(out=SA[P], in_=TMP[P], axis=AX.X, op=ALU.add), reads=[dT], writes=[dSA])
            K.dve.op(lambda e, W_=W_: e.tensor_tensor(out=St[P], in0=St[P], in1=W_, op=ALU.mult),
                     reads=[dBC], writes=[dS])
            K.dve.op(lambda e, KA=KA: e.tensor_tensor(out=TMP[P], in0=KA, in1=SA[P].unsqueeze(3).broadcast_to(shp),
                                                     op=ALU.mult), reads=[dBC, dSA], writes=[dT])
            K.dve.op(lambda e: e.tensor_tensor(out=St[P], in0=St[P], in1=TMP[P], op=ALU.add), reads=[dT], writes=[dS])
            T2, dT2 = T2r.next()
            K.pool.op(lambda e, KP=KP, T2=T2, Vf=Vf, tt=tt: e.tensor_tensor(
                out=T2[P], in0=KP, in1=Vf[P, :, :, tt:tt + 1].broadcast_to(shp), op=ALU.mult),
                reads=[dBC, dV], writes=[dT2])
            K.dve.op(lambda e, T2=T2: e.tensor_tensor(out=St[P], in0=St[P], in1=T2[P], op=ALU.add),
                     reads=[dT2], writes=[dS])
            T3, dT3 = T3r.next()
            K.pool.op(lambda e, T3=T3, R_=R_: e.tensor_tensor(out=T3[P], in0=St[P], in1=R_, op=ALU.mult),
                      reads=[dS, dBC], writes=[dT3])
            K.dve.op(lambda e, T3=T3, Yf=Yf, tt=tt: e.tensor_reduce(out=Yf[P, :, :, tt], in_=T3[P], axis=AX.X, op=ALU.add),
                      reads=[dT3], writes=[dY] if tt == 0 else [], adds=[] if tt == 0 else [dY])
            if tt == TC - 1:
                for bhi in range(NBH):
                    for blo in range(NBL):
                        dst = vy_ap("y_fm", bhi, blo, ni * TC)
                        srcp = Yf[bhi * 64:(bhi + 1) * 64, blo]
                        K.sp.dma(lambda e, dst=dst, srcp=srcp: e.dma_start(out=dst, in_=srcp), reads=[dY],
                                 adds=[scr["d_p2b"]])


def phase2_chunk(K, cfg, io, scr):
    T, S, NB = cfg["T"], cfg["S"], cfg["NB"]
    C = 64
    NCH = S // C
    with ExitStack() as es:
        d_c = Dep()
        id64 = K.sb([64, 64], BF16, "c_id64", es)
        K.sp.dma(lambda e: e.dma_start(out=id64[:], in_=io["ident64"][:, :]), adds=[d_c])
        MK = K.sb([64, 3, 64], F32, "c_MK", es)
        K.sp.dma(lambda e: e.dma_start(out=MK[:], in_=io["masks"][:, :, :]), adds=[d_c])
        ONES = K.sb([64, 1], F32, "c_ones", es)
        K.sp.dma(lambda e: e.dma_start(out=ONES[:], in_=io["ones64"][:, :]), adds=[d_c])
        IDF = K.sb([64, 8, 64], F32, "c_IDF", es)
        K.sp.dma(lambda e: e.dma_start(out=IDF[:], in_=io["ident_f"][0:64, 0:64].unsqueeze(1).broadcast_to([64, 8, 64])), adds=[d_c])
        ST = [K.sb([64, 8, 64], F32, "c_S%d" % b, es) for b in range(NB)]
        STb = [K.sb([64, 8, 64], BF16, "c_Sb%d" % b, es) for b in range(NB)]
        dST = [Dep() for _ in range(NB)]
        dSTb = [Dep() for _ in range(NB)]
        for b in range(NB):
            K.dve.op(lambda e, b=b: e.memset(ST[b][:], 0.0), writes=[dST[b]])
            K.pool.op(lambda e, b=b: e.memset(STb[b][:], 0.0), writes=[dSTb[b]])
        TMr = Rot(K, 3, [64, 5, 512], BF16, "c_TM", es=es)
        LWr = Rot(K, 3, [64, 512], F32, "c_LW", es=es)
        FMr = Rot(K, 2, [64, 4, 8, 64], BF16, "c_FM", es=es)
        PCr = Rot(K, 2, [64, 8], F32, "c_PC", es=es)
        Nr = Rot(K, 3, [64, 8, 64], BF16, "c_N", es=es)
        NTr = Rot(K, 3, [64, 8, 64], BF16, "c_NT", es=es)
        MTr = Rot(K, 3, [64, 8, 64], BF16, "c_MT", es=es)
        MTfr = Rot(K, 2, [64, 8, 64], F32, "c_MTf", es=es)
        NAKr = Rot(K, 2, [64, 8, 64], BF16, "c_NAK", es=es)
        MRBr = Rot(K, 2, [64, 8, 64], BF16, "c_MRB", es=es)
        MRKr = Rot(K, 2, [64, 8, 64], BF16, "c_MRK", es=es)
        Xr = Rot(K, 2, [64, 8, 64], BF16, "c_X", es=es)
        NUr = Rot(K, 2, [64, 8, 64], BF16, "c_NU", es=es)
        Yr = Rot(K, 2, [64, 8, 64], F32, "c_Y", es=es)
        TSr = Rot(K, 2, [64, 8, 64], F32, "c_TS", es=es)
        PTf = Rot(K, 1, [64, 4, 8, 64], BF16, "c_PTf", psum=True, es=es)
        PA = Rot(K, 4, [64, 8, 64], F32, "c_PA", psum=True, es=es)
        PPC = Rot(K, 1, [64, 8], F32, "c_PPC", psum=True, es=es)
        ce = [0]

        def evac_copy(dst_ap, src_ap, reads, writes=(), adds=(), scale=None):
            ce[0] += 1
            if scale is not None or ce[0] % 2 == 0:
                if scale is None:
                    K.act.op(lambda e: e.activation(out=dst_ap, in_=src_ap, func=AF.Copy), reads=reads, writes=writes, adds=adds)
                else:
                    K.act.op(lambda e: e.activation(out=dst_ap, in_=src_ap, func=AF.Copy, scale=scale), reads=reads, writes=writes, adds=adds)
            else:
                K.dve.op(lambda e: e.tensor_copy(out=dst_ap, in_=src_ap), reads=reads, writes=writes, adds=adds)

        def mm8(pt, dpt, lhs_fn, rhs_fn, reads, first=True, last=True, wr=True):
            mmN(pt, dpt, [(lhs_fn, rhs_fn)], reads)

        def mmN(pt, dpt, terms, reads):
            n = len(terms)
            for h in range(8):
                for i, (lf, rf) in enumerate(terms):
                    K.pe.op(lambda e, h=h, lf=lf, rf=rf, i=i: e.matmul(pt[:, h, :], lf(h), rf(h), start=(i == 0), stop=(i == n - 1)),
                            reads=reads, writes=[dpt] if (h == 0 and i == 0) else [], adds=[] if (h == 0 and i == 0) else [dpt])

        q = [0]

        def dq():
            q[0] += 1
            return K.sp if q[0] % 2 == 0 else K.pool

        for ci in range(NCH):
            for b in range(NB):
                t0 = b * S + ci * C
                TM, dTM = TMr.next()
                LW, dLW = LWr.next()
                dq().dma(lambda e, TM=TM, t0=t0: e.dma_start(out=TM[:], in_=scr["ab_tm"][t0:t0 + C, :, :]),
                         reads=[scr["d_p2a"]], writes=[dTM])
                dq().dma(lambda e, LW=LW, t0=t0: e.dma_start(out=LW[:], in_=scr["lw_tm"][t0:t0 + C, :]),
                         reads=[scr["d_p2a"]], writes=[dLW])
                ptf, dptf = PTf.next()
                first = True
                for j in range(4):
                    for h in range(8):
                        K.pe.op(lambda e, ptf=ptf, TM=TM, j=j, h=h: e.transpose(
                            out=ptf[:, j, h, :], in_=TM[:, j, h * 64:(h + 1) * 64], identity=id64[:]),
                            reads=[dTM, d_c], writes=[dptf] if first else [], adds=[] if first else [dptf])
                        first = False
                FM, dFM = FMr.next()
                K.act.op(lambda e, FM=FM, ptf=ptf: e.activation(out=FM[:, 0:2], in_=ptf[:, 0:2], func=AF.Copy), reads=[dptf], writes=[dFM])
                K.dve.op(lambda e, FM=FM, ptf=ptf: e.tensor_copy(out=FM[:, 2:4], in_=ptf[:, 2:4]), reads=[dptf, dFM], adds=[dFM])
                Af = lambda h, FM=FM: FM[:, 0, h, :]
                Bf = lambda h, FM=FM: FM[:, 1, h, :]
                Kf = lambda h, FM=FM: FM[:, 2, h, :]
                Rf = lambda h, FM=FM: FM[:, 3, h, :]
                Vt = lambda h, TM=TM: TM[:, 4, h * 64:(h + 1) * 64]
                Bt = lambda h, TM=TM: TM[:, 1, h * 64:(h + 1) * 64]
                Kt = lambda h, TM=TM: TM[:, 2, h * 64:(h + 1) * 64]
                ppc, dppc = PPC.next()
                for h in range(8):
                    K.pe.op(lambda e, ppc=ppc, LW=LW, h=h: e.matmul(ppc[:, h:h + 1], LW[:, h * 64:(h + 1) * 64], ONES[:], start=True, stop=True),
                            reads=[dLW, d_c], writes=[dppc] if h == 0 else [], adds=[] if h == 0 else [dppc])
                PCt, dPC = PCr.next()
                K.act.op(lambda e, PCt=PCt, ppc=ppc: e.activation(out=PCt[:], in_=ppc[:], func=AF.Exp), reads=[dppc], writes=[dPC])
                mbc = lambda i: MK[:, i, :].unsqueeze(1).broadcast_to([64, 8, 64])
                pa, dpa = PA.next()
                mm8(pa, dpa, Af, Bf, [dFM])
                N0, dN0 = Nr.next()
                K.dve.op(lambda e, N0=N0, pa=pa: e.tensor_tensor(out=N0[:], in0=pa[:], in1=mbc(0), op=ALU.mult), reads=[dpa, d_c], writes=[dN0])
                pa, dpa = PA.next()
                mm8(pa, dpa, Bf, Af, [dFM])
                NT0, dNT0 = NTr.next()
                MTf, dMTf = MTfr.next()
                K.dve.op(lambda e, NT0=NT0, pa=pa: e.tensor_tensor(out=NT0[:], in0=pa[:], in1=mbc(1), op=ALU.mult), reads=[dpa, d_c], writes=[dNT0])
                K.pool.op(lambda e, MTf=MTf, NT0=NT0: e.tensor_tensor(out=MTf[:], in0=IDF[:], in1=NT0[:], op=ALU.subtract),
                          reads=[dNT0, d_c], writes=[dMTf])
                MT, dMT = MTr.next()
                K.act.op(lambda e, MT=MT, MTf=MTf: e.activation(out=MT[:], in_=MTf[:], func=AF.Copy), reads=[dMTf], writes=[dMT])
                pa, dpa = PA.next()
                mm8(pa, dpa, Kf, Af, [dFM])
                NAK, dNAK = NAKr.next()
                K.dve.op(lambda e, NAK=NAK, pa=pa: e.tensor_tensor(out=NAK[:], in0=pa[:], in1=mbc(1), op=ALU.mult), reads=[dpa, d_c], writes=[dNAK])
                pa, dpa = PA.next()
                mm8(pa, dpa, Bf, Rf, [dFM])
                MRB, dMRB = MRBr.next()
                K.dve.op(lambda e, MRB=MRB, pa=pa: e.tensor_tensor(out=MRB[:], in0=pa[:], in1=mbc(2), op=ALU.mult), reads=[dpa, d_c], writes=[dMRB])
                pa, dpa = PA.next()
                mm8(pa, dpa, Kf, Rf, [dFM])
                MRK, dMRK = MRKr.next()
                K.dve.op(lambda e, MRK=MRK, pa=pa: e.tensor_tensor(out=MRK[:], in0=pa[:], in1=mbc(2), op=ALU.mult), reads=[dpa, d_c], writes=[dMRK])
                Np, dNp, NTp, dNTp = N0, dN0, NT0, dNT0
                for lvl in range(1, 6):
                    pa, dpa = PA.next()
                    mm8(pa, dpa, lambda h, NTp=NTp: NTp[:, h, :], lambda h, Np=Np: Np[:, h, :], [dNp, dNTp])
                    Nn, dNn = Nr.next()
                    evac_copy(Nn[:], pa[:], [dpa], writes=[dNn])
                    if lvl < 5:
                        pa2, dpa2 = PA.next()
                        mm8(pa2, dpa2, lambda h, Np=Np: Np[:, h, :], lambda h, NTp=NTp: NTp[:, h, :], [dNp, dNTp])
                        NTn, dNTn = NTr.next()
                        evac_copy(NTn[:], pa2[:], [dpa2], writes=[dNTn])
                    pa3, dpa3 = PA.next()
                    mm8(pa3, dpa3, lambda h, Nn=Nn: Nn[:, h, :], lambda h, MT=MT: MT[:, h, :], [dNn, dMT])
                    K.dve.op(lambda e, MTf=MTf, pa3=pa3: e.tensor_tensor(out=MTf[:], in0=MTf[:], in1=pa3[:], op=ALU.add),
                             reads=[dpa3], writes=[dMTf])
                    MT, dMT = MTr.next()
                    K.act.op(lambda e, MT=MT, MTf=MTf: e.activation(out=MT[:], in_=MTf[:], func=AF.Copy), reads=[dMTf], writes=[dMT])
                    Np, dNp = Nn, dNn
                    if lvl < 5:
                        NTp, dNTp = NTn, dNTn
                Sb = STb[b]
                pa, dpa = PA.next()
                mmN(pa, dpa, [(Af, lambda h, Sb=Sb: Sb[:, h, :]), (lambda h, NAK=NAK: NAK[:, h, :], Vt)], [dFM, dSTb[b], dNAK, dTM])
                X, dX = Xr.next()
                K.act.op(lambda e, X=X, pa=pa: e.activation(out=X[:], in_=pa[:], func=AF.Copy), reads=[dpa], writes=[dX])
                pa, dpa = PA.next()
                mm8(pa, dpa, lambda h, MT=MT: MT[:, h, :], lambda h, X=X: X[:, h, :], [dMT, dX])
                NU, dNU = NUr.next()
                K.act.op(lambda e, NU=NU, pa=pa: e.activation(out=NU[:], in_=pa[:], func=AF.Copy, scale=-1.0), reads=[dpa], writes=[dNU])
                pa, dpa = PA.next()
                mmN(pa, dpa, [(lambda h, Sb=Sb: Sb[:, h, :], Rf), (lambda h, NU=NU: NU[:, h, :], lambda h, MRB=MRB: MRB[:, h, :]),
                              (Vt, lambda h, MRK=MRK: MRK[:, h, :])], [dFM, dSTb[b], dNU, dMRB, dTM, dMRK])
                Y, dY = Yr.next()
                K.dve.op(lambda e, Y=Y, pa=pa: e.tensor_copy(out=Y[:], in_=pa[:]), reads=[dpa], writes=[dY])
                K.sp.dma(lambda e, Y=Y, t0=t0: e.dma_start(out=dap(scr["y_fm"], t0, [[T, 64], [64 * T, 8], [1, 64]]), in_=Y[:]),
                         reads=[dY], adds=[scr["d_p2b"]])
                pa, dpa = PA.next()
                mmN(pa, dpa, [(Bt, lambda h, NU=NU: NU[:, h, :]), (Kt, Vt)], [dTM, dNU])
                TS_, dTS = TSr.next()
                K.dve.op(lambda e, TS_=TS_, pa=pa, b=b: e.tensor_tensor(out=TS_[:], in0=pa[:], in1=ST[b][:], op=ALU.add),
                         reads=[dpa, dST[b]], writes=[dTS])
                K.dve.op(lambda e, TS_=TS_, PCt=PCt, b=b: e.tensor_tensor(
                    out=ST[b][:], in0=TS_[:], in1=PCt[:].unsqueeze(2).broadcast_to([64, 8, 64]), op=ALU.mult),
                    reads=[dTS, dPC], writes=[dST[b]])
                K.act.op(lambda e, b=b: e.activation(out=STb[b][:], in_=ST[b][:], func=AF.Copy), reads=[dST[b]], writes=[dSTb[b]])


def phase2_post(K, cfg, io, scr):
    T, S, NB = cfg["T"], cfg["S"], cfg["NB"]
    NT = T // 512
    with ExitStack() as es:
        BO = K.sb([128, 128], F32, "BO", es)
        d_c = Dep()
        K.sp.dma(lambda e: e.dma_start(out=BO[:], in_=io["blockones"][:, :]), adds=[d_c])
        LN = K.sb([128, 2, 4], F32, "LN", es)
        K.sp.dma(lambda e: e.dma_start(out=LN[:, 0, :], in_=io["lnx_w"].rearrange("o (j p) -> p (o j)", p=128)), adds=[d_c])
        K.sp.dma(lambda e: e.dma_start(out=LN[:, 1, :], in_=io["lnx_b"].rearrange("o (j p) -> p (o j)", p=128)), adds=[d_c])
        Yr = Rot(K, 2, [128, 512], F32, "pY", es=es)
        Vr = Rot(K, 2, [128, 512], F32, "pV", es=es)
        Gr = Rot(K, 2, [128, 512], F32, "pG", es=es)
        Cr = Rot(K, 2, [128, 512], F32, "pC", es=es)
        YCr = Rot(K, 2, [128, 512], F32, "pYC", es=es)
        SQr = Rot(K, 2, [128, 512], F32, "pSQ", es=es)
        Rr = Rot(K, 2, [128, 512], F32, "pR", es=es)
        Or = Rot(K, 2, [128, 512], BF16, "pO", es=es)
        PM = Rot(K, 2, [128, 512], F32, "pPM", psum=True, es=es)
        PVr = Rot(K, 2, [128, 512], F32, "pPV", psum=True, es=es)
        def make_gen(ti, j):
            cs = slice(ti * 512, (ti + 1) * 512)
            if True:
                rs = slice(j * 128, (j + 1) * 128)
                Y, dY = Yr.next()
                V, dV = Vr.next()
                G, dG = Gr.next()
                C, dC = Cr.next()
                K.sp.dma(lambda e, Y=Y, rs=rs, cs=cs: e.dma_start(out=Y[:], in_=scr["y_fm"][rs, cs]),
                         reads=[scr["d_p2b"]], writes=[dY])
                K.pool.dma(lambda e, V=V, rs=rs, cs=cs: e.dma_start(out=V[:], in_=scr["v_fm"][rs, cs]),
                           reads=[scr["d_p2a"]], writes=[dV])
                K.sp.dma(lambda e, G=G, rs=rs, cs=cs: e.dma_start(out=G[:], in_=scr["g_fm"][rs, cs]),
                         reads=[scr["d_p2a"]], writes=[dG])
                K.pool.dma(lambda e, C=C, j=j, cs=cs: e.dma_start(
                    out=C[0:64, :], in_=scr["coef_fm"][2 * j:2 * j + 1, cs].broadcast_to([64, 512])),
                    reads=[scr["d_p2a"]], writes=[dC])
                K.pool.dma(lambda e, C=C, j=j, cs=cs: e.dma_start(
                    out=C[64:128, :], in_=scr["coef_fm"][2 * j + 1:2 * j + 2, cs].broadcast_to([64, 512])),
                    reads=[scr["d_p2a"]], adds=[dC])
                pm, dpm = PM.next()
                K.pe.op(lambda e, pm=pm, Y=Y: e.matmul(pm[:], BO[:], Y[:], start=True, stop=True),
                        reads=[dY, d_c], writes=[dpm])
                YC, dYC = YCr.next()
                K.dve.op(lambda e, YC=YC, Y=Y, pm=pm: e.tensor_tensor(out=YC[:], in0=Y[:], in1=pm[:], op=ALU.subtract),
                         reads=[dY, dpm], writes=[dYC])
                SQ, dSQ = SQr.next()
                K.act.op(lambda e, SQ=SQ, YC=YC: e.activation(out=SQ[:], in_=YC[:], func=AF.Square),
                         reads=[dYC], writes=[dSQ])
                yield
                pv, dpv = PVr.next()
                K.pe.op(lambda e, pv=pv, SQ=SQ: e.matmul(pv[:], BO[:], SQ[:], start=True, stop=True),
                        reads=[dSQ, d_c], writes=[dpv])
                yield
                R, dR = Rr.next()
                K.dve.op(lambda e, R=R, pv=pv: e.tensor_scalar(out=R[:], in0=pv[:], scalar1=64e-5, scalar2=None, op0=ALU.add),
                         reads=[dpv], writes=[dR])
                K.act.op(lambda e, R=R: e.activation(out=R[:], in_=R[:], func=AF.Ln), writes=[dR])
                K.act.op(lambda e, R=R: e.activation(out=R[:], in_=R[:], func=AF.Exp, scale=-0.5), writes=[dR])
                K.dve.op(lambda e, YC=YC, R=R: e.tensor_tensor(out=YC[:], in0=YC[:], in1=R[:], op=ALU.mult),
                         reads=[dR], writes=[dYC])
                K.dve.op(lambda e, YC=YC, j=j: e.tensor_scalar(out=YC[:], in0=YC[:], scalar1=LN[:, 0, j:j + 1],
                                                              scalar2=LN[:, 1, j:j + 1], op0=ALU.mult, op1=ALU.add),
                         reads=[d_c], writes=[dYC])
                yield
                K.pool.op(lambda e, C=C, V=V: e.tensor_tensor(out=C[:], in0=C[:], in1=V[:], op=ALU.mult),
                          reads=[dV], writes=[dC])
                K.dve.op(lambda e, YC=YC, C=C: e.tensor_tensor(out=YC[:], in0=YC[:], in1=C[:], op=ALU.add),
                         reads=[dC], writes=[dYC])
                O, dO = Or.next()
                K.dve.op(lambda e, O=O, YC=YC, G=G: e.tensor_tensor(out=O[:], in0=YC[:], in1=G[:], op=ALU.mult),
                         reads=[dYC, dG], writes=[dO])
                K.sp.dma(lambda e, O=O, rs=rs, cs=cs: e.dma_start(out=scr["ya_fm"][rs, cs], in_=O[:]),
                         reads=[dO], adds=[scr["d_p2c"]])

        work = [(ti, j) for ti in range(NT) for j in range(4)]

        active = []
        nxt = 0
        while nxt < len(work) or active:
            if len(active) < 2 and nxt < len(work):
                active.append(make_gen(*work[nxt]))
                nxt += 1
            for a in list(active):
                try:
                    next(a)
                except StopIteration:
                    active.remove(a)


def phase3(K, cfg, io, scr):
    T, S, NB = cfg["T"], cfg["S"], cfg["NB"]
    NQ = S // 128
    lam_init = 0.2
    with ExitStack() as es:
        d_c = Dep()
        identb = K.sb([128, 128], BF16, "a_identb", es)
        K.sp.dma(lambda e: e.dma_start(out=identb[:], in_=io["ident_bf"][:, :]), adds=[d_c])
        TB = K.sb([128, 4, S], F32, "TB", es)
        for h in range(4):
            (K.sp if h % 2 == 0 else K.pool).dma(lambda e, h=h: e.dma_start(out=TB[:, h, :], in_=io["alibi"][h, :, :]), adds=[d_c])
        SW = K.sb([128, 128], F32, "SW", es)
        K.sp.dma(lambda e: e.dma_start(out=SW[:], in_=io["subln_w"][0:1, :].broadcast_to([128, 128])), adds=[d_c])
        LQ = K.sb([128, 4, 64], F32, "LQ", es)
        for j, nm in enumerate(["lam_q1", "lam_k1", "lam_q2", "lam_k2"]):
            K.pool.dma(lambda e, j=j, nm=nm: e.dma_start(out=LQ[:, j, :], in_=io[nm][0:1, :].broadcast_to([128, 64])), adds=[d_c])
        LM = K.sb([128, 8], F32, "LM", es)
        d_lm = Dep()
        LT_ = K.sb([128, 2, 64], F32, "LTt", es)
        K.dve.op(lambda e: e.tensor_tensor(out=LT_[:, 0, :], in0=LQ[:, 0, :], in1=LQ[:, 1, :], op=ALU.mult), reads=[d_c], writes=[d_lm])
        K.dve.op(lambda e: e.tensor_tensor(out=LT_[:, 1, :], in0=LQ[:, 2, :], in1=LQ[:, 3, :], op=ALU.mult), reads=[d_c], writes=[d_lm])
        K.dve.op(lambda e: e.tensor_reduce(out=LM[:, 0:2], in_=LT_[:], axis=AX.X, op=ALU.add), writes=[d_lm])
        K.act.op(lambda e: e.activation(out=LM[:, 2:4], in_=LM[:, 0:2], func=AF.Exp), writes=[d_lm])
        K.dve.op(lambda e: e.tensor_tensor(out=LM[:, 4:5], in0=LM[:, 3:4], in1=LM[:, 2:3], op=ALU.subtract), writes=[d_lm])
        K.dve.op(lambda e: e.tensor_scalar(out=LM[:, 4:5], in0=LM[:, 4:5], scalar1=-lam_init, scalar2=None, op0=ALU.add), writes=[d_lm])
        K.dve.op(lambda e: e.tensor_scalar(out=SW[:], in0=SW[:], scalar1=1.0 - lam_init, scalar2=None, op0=ALU.mult),
                 reads=[d_c], writes=[d_c])

        Vr = Rot(K, 2, [128, NQ, 512], BF16, "aV", es=es)
        QKr = Rot(K, 2, [64, 4, S], BF16, "aQK", es=es)
        SSr = Rot(K, 3, [128, 512], F32, "aSS", es=es)
        Pr = Rot(K, 3, [128, 512], BF16, "aP", es=es)
        PTsr = Rot(K, 4, [128, 4, 128], BF16, "aPTs", es=es)
        YB = K.sb([128, NQ, 512], BF16, "aYB", es)
        dYB = Dep()
        STr = Rot(K, 4, [128, 24], F32, "aST", es=es)
        O1r = Rot(K, 2, [128, 128], F32, "aO1", es=es)
        Or_ = Rot(K, 2, [128, 128], F32, "aO", es=es)
        junk = K.sb([128, 128], F32, "ajunk", es)
        d_junk = Dep()
        YTr = Rot(K, 2, [128, 4, 128], BF16, "aYT", es=es)
        PS = Rot(K, 3, [128, 512], F32, "aPS", psum=True, es=es)
        PTp = Rot(K, 2, [128, 4, 128], BF16, "aPTp", psum=True, es=es)
        PO = Rot(K, 2, [128, 2, 128], F32, "aPO", psum=True, es=es)
        cp = [0]

        def copy_eng():
            cp[0] += 1
            return cp[0] % 2

        SSQ = K.sb([128, NQ * 4], F32, "aSSQ", es)
        dSSQ = Dep()
        SWb = K.sb([128, 128], BF16, "aSWb", es)
        K.act.op(lambda e: e.activation(out=SWb[:], in_=SW[:], func=AF.Copy), reads=[d_c], adds=[d_c])
        pipe = []
        pidx = [0]

        def step_pipe():
            j = len(pipe) - 1
            pipe[j][0]()
            if j - 1 >= pidx[0]:
                pipe[j - 1][2]()
            if j - 2 >= pidx[0]:
                pipe[j - 2][3]()
                if pipe[j - 2][4] is not None:
                    pipe[j - 2][4]()
            pipe[j][1]()

        def flush_pipe():
            j = len(pipe) - 1
            if j - 0 >= pidx[0] and j >= 0:
                pipe[j][2]()
            for k in (j - 1, j):
                if k >= pidx[0] and k >= 0:
                    pipe[k][3]()
                    if pipe[k][4] is not None:
                        pipe[k][4]()
            pidx[0] = len(pipe)
        for b in range(NB):
            V, dV = Vr.next()
            K.sp.dma(lambda e, V=V, b=b: e.dma_start(
                out=V[:], in_=scr["av_tm"][b * S:(b + 1) * S, :].rearrange("(n p) c -> p n c", p=128)),
                reads=[scr["d_p1"]], writes=[dV])
            first_yb = True
            for h in range(4):
                QK, dQK = QKr.next()
                for j in range(4):
                    r0 = (0 if j < 2 else 512) + h * 128 + (j % 2) * 64
                    (K.sp if j % 2 == 0 else K.pool).dma(lambda e, QK=QK, j=j, r0=r0, b=b: e.dma_start(
                        out=QK[:, j, :], in_=scr["qk_fm"][r0:r0 + 64, b * S:(b + 1) * S]),
                        reads=[scr["d_p1"]], writes=[dQK] if j == 0 else [], adds=[] if j == 0 else [dQK])
                for qi in range(NQ):
                    nk = (qi + 1) * 128
                    off = (S - 128) - qi * 128
                    ST, dST = STr.next()
                    K.pool.op(lambda e, ST=ST: e.memset(ST[:], 0.0), writes=[dST])
                    po, dpo = PO.next()
                    items = []
                    for c in range(2):
                        nch = (nk + 511) // 512
                        for ch in range(nch):
                            items.append((c, ch))
                    for ii, (c, ch) in enumerate(items):
                        kb0 = ch * 512
                        n = min(512, nk - kb0)
                        nb = n // 128
                        ps, dps = PS.next()
                        SS, dSS = SSr.next()
                        Pt, dP = Pr.next()
                        hold = {}

                        def stA_pe(ps=ps, dps=dps, c=c, qi=qi, kb0=kb0, n=n, QK=QK, dQK=dQK):
                            K.pe.op(lambda e: e.matmul(
                                ps[:, :n], QK[:, c, qi * 128:(qi + 1) * 128], QK[:, 2 + c, kb0:kb0 + n],
                                start=True, stop=True), reads=[dQK], writes=[dps])

                        def stA_rest(ps=ps, dps=dps, SS=SS, dSS=dSS, Pt=Pt, dP=dP, ST=ST, dST=dST, n=n, off=off, h=h, kb0=kb0, c=c, ch=ch):
                            K.dve.op(lambda e: e.scalar_tensor_tensor(
                                out=SS[:, :n], in0=ps[:, :n], scalar=0.125, in1=TB[:, h, off + kb0:off + kb0 + n],
                                op0=ALU.mult, op1=ALU.add), reads=[dps, d_c], writes=[dSS])
                            K.act.op(lambda e: e.activation(
                                out=Pt[:, :n], in_=SS[:, :n], func=AF.Exp,
                                accum_out=ST[:, 4 * c + ch:4 * c + ch + 1]), reads=[dSS, dST], writes=[dP], adds=[dST])

                        def stB(Pt=Pt, dP=dP, nb=nb, hold=hold):
                            ptp, dptp = PTp.next()
                            for kk_ in range(nb):
                                K.pe.op(lambda e, kk_=kk_: e.transpose(
                                    out=ptp[:, kk_, :], in_=Pt[:, kk_ * 128:(kk_ + 1) * 128], identity=identb[:]),
                                    reads=[dP, d_c], writes=[dptp] if kk_ == 0 else [], adds=[] if kk_ == 0 else [dptp])
                            PTs, dPTs = PTsr.next()
                            hold["PTs"] = (PTs, dPTs)
                            if copy_eng():
                                K.act.op(lambda e: e.activation(
                                    out=PTs[:, :nb, :], in_=ptp[:, :nb, :], func=AF.Copy), reads=[dptp], writes=[dPTs])
                            else:
                                K.dve.op(lambda e: e.tensor_copy(
                                    out=PTs[:, :nb, :], in_=ptp[:, :nb, :]), reads=[dptp], writes=[dPTs])

                        def stC(nb=nb, kb0=kb0, c=c, h=h, qi=qi, po=po, dpo=dpo, V=V, dV=dV, hold=hold):
                            PTs, dPTs = hold["PTs"]
                            for kk_ in range(nb):
                                kb = kb0 // 128 + kk_
                                K.pe.op(lambda e, kb=kb, kk_=kk_: e.matmul(
                                    po[:, c, :], PTs[:, kk_, :], V[:, kb, h * 128:(h + 1) * 128],
                                    start=(kb == 0), stop=(kb == qi)), reads=[dPTs, dV],
                                    writes=[dpo] if (kb == 0 and c == 0) else [], adds=[] if (kb == 0 and c == 0) else [dpo])

                        def combine(ST=ST, dST=dST, po=po, dpo=dpo, qi=qi, h=h, fy=first_yb):
                            K.dve.op(lambda e: e.tensor_reduce(out=ST[:, 8:10], in_=ST[:, 0:8].rearrange("p (c k) -> p c k", k=4),
                                                               axis=AX.X, op=ALU.add), reads=[dST], writes=[dST])
                            K.dve.op(lambda e: e.reciprocal(out=ST[:, 10:12], in_=ST[:, 8:10]), writes=[dST])
                            K.dve.op(lambda e: e.tensor_tensor(out=ST[:, 12:13], in0=ST[:, 11:12], in1=LM[:, 4:5], op=ALU.mult),
                                     reads=[d_lm], writes=[dST])
                            O1, dO1 = O1r.next()
                            K.dve.op(lambda e: e.tensor_scalar(out=O1[:], in0=po[:, 1, :], scalar1=ST[:, 12:13],
                                                               scalar2=None, op0=ALU.mult), reads=[dpo, dST], writes=[dO1])
                            K.dve.op(lambda e: e.scalar_tensor_tensor(
                                out=YB[:, qi, h * 128:(h + 1) * 128], in0=po[:, 0, :], scalar=ST[:, 10:11], in1=O1[:], op0=ALU.mult, op1=ALU.add),
                                reads=[dpo, dST, dO1], writes=[dYB] if fy else [], adds=[] if fy else [dYB])
                            K.act.op(lambda e: e.activation(out=junk[:], in_=YB[:, qi, h * 128:(h + 1) * 128], func=AF.Square,
                                                            accum_out=SSQ[:, qi * 4 + h:qi * 4 + h + 1]),
                                     reads=[dYB], writes=[d_junk], adds=[dSSQ])

                        last = (ii == len(items) - 1)
                        pipe.append([stA_pe, stA_rest, stB, stC, combine if last else None])
                        step_pipe()
                    first_yb = False
            flush_pipe()
            K.dve.op(lambda e: e.tensor_scalar(out=SSQ[:], in0=SSQ[:], scalar1=1.0 / 128, scalar2=1e-5, op0=ALU.mult, op1=ALU.add),
                     reads=[dSSQ], writes=[dSSQ])
            K.act.op(lambda e: e.activation(out=SSQ[:], in_=SSQ[:], func=AF.Ln), writes=[dSSQ])
            K.act.op(lambda e: e.activation(out=SSQ[:], in_=SSQ[:], func=AF.Exp, scale=-0.5), writes=[dSSQ])
            YBv = YB[:].rearrange("p q (h e) -> p (q h) e", e=128)
            K.dve.op(lambda e: e.tensor_tensor(out=YBv, in0=YBv, in1=SSQ[:].unsqueeze(2).broadcast_to([128, NQ * 4, 128]), op=ALU.mult),
                     reads=[dSSQ], writes=[dYB])
            K.pool.op(lambda e: e.tensor_tensor(out=YBv, in0=YBv, in1=SWb[:].unsqueeze(1).broadcast_to([128, NQ * 4, 128]), op=ALU.mult),
                      reads=[d_c], writes=[dYB])
            for qi in range(NQ):
                ptp, dptp = PTp.next()
                for h in range(4):
                    K.pe.op(lambda e, ptp=ptp, qi=qi, h=h: e.transpose(out=ptp[:, h, :], in_=YB[:, qi, h * 128:(h + 1) * 128],
                                                                      identity=identb[:]),
                            reads=[dYB, d_c], writes=[dptp] if h == 0 else [], adds=[] if h == 0 else [dptp])
                YT, dYT = YTr.next()
                K.act.op(lambda e, ptp=ptp, YT=YT: e.activation(out=YT[:], in_=ptp[:, 0:4, :], func=AF.Copy),
                         reads=[dptp], writes=[dYT])
                t0 = b * S + qi * 128
                K.sp.dma(lambda e, YT=YT, t0=t0: e.dma_start(
                    out=dap(scr["yb_fm"], t0, [[T, 128], [128 * T, 4], [1, 128]]), in_=YT[:]),
                    reads=[dYT], adds=[scr["d_p3"]])


def load_w_bf16(K, es, src2d, rows, cols, name, dep, stage_rot, q, W=None):
    nk = rows // 128
    if W is None:
        W = K.sb([128, nk, cols], BF16, name, es)
    for kc in range(nk):
        for c0 in range(0, cols, 1024):
            n = min(1024, cols - c0)
            st, dst = stage_rot.next()
            q[0] += 1
            (K.sp if q[0] % 2 == 0 else K.pool).dma(lambda e, st=st, kc=kc, c0=c0, n=n: e.dma_start(
                out=st[:, :n], in_=src2d[kc * 128:(kc + 1) * 128, c0:c0 + n]), writes=[dst])
            if q[0] % 2 == 0:
                K.act.op(lambda e, st=st, kc=kc, c0=c0, n=n: e.activation(out=W[:, kc, c0:c0 + n], in_=st[:, :n], func=AF.Copy),
                         reads=[dst], adds=[dep])
            else:
                K.dve.op(lambda e, st=st, kc=kc, c0=c0, n=n: e.tensor_copy(out=W[:, kc, c0:c0 + n], in_=st[:, :n]),
                         reads=[dst], adds=[dep])
    return W


def phase4(K, cfg, io, scr):
    T, S, NB = cfg["T"], cfg["S"], cfg["NB"]
    NT = T // 512
    with ExitStack() as es:
        d_c = Dep()
        identb = K.sb([128, 128], BF16, "m_identb", es)
        K.sp.dma(lambda e: e.dma_start(out=identb[:], in_=io["ident_bf"][:, :]), adds=[d_c])
        NF = K.sb([128, D], F32, "NF", es)
        K.sp.dma(lambda e: e.dma_start(out=NF[:], in_=io["norm_ffn_w"][0:1, :].broadcast_to([128, D])), adds=[d_c])
        stg = Rot(K, 2, [128, 1024], F32, "m_stg", es=es)
        q = [0]
        PAw = load_w_bf16(K, es, io["proj_a"][0], 512, D, "PAw", d_c, stg, q)
        PBw = load_w_bf16(K, es, io["proj_b"][0], 512, D, "PBw", d_c, stg, q)
        WO = load_w_bf16(K, es, io["w_out"][0], D, D, "WO", d_c, stg, q)
        YAr = Rot(K, 2, [128, 4, 512], BF16, "mYA", es=es)
        YBr = Rot(K, 2, [128, 4, 512], BF16, "mYB", es=es)
        SGr = Rot(K, 2, [128, 16, 512], BF16, "mSG", es=es)
        MGr = Rot(K, 2, [128, 8, 512], BF16, "mMG", es=es)
        t1r = Rot(K, 2, [128, 512], F32, "mt1", es=es)
        t2r = Rot(K, 2, [128, 512], F32, "mt2", es=es)
        Xr = Rot(K, 2, [128, D], F32, "mX", es=es)
        X1r = Rot(K, 2, [128, D], F32, "mX1", es=es)
        XHr = Rot(K, 2, [128, D], F32, "mXH", es=es)
        XBr = Rot(K, 2, [128, D], BF16, "mXB", es=es)
        XTr = Rot(K, 2, [128, 8, 128], BF16, "mXT", es=es)
        junk = K.sb([128, D], BF16, "mjunk", es)
        d_junk = Dep()
        STr = Rot(K, 4, [128, 4], F32, "mST", es=es)
        PP = Rot(K, 2, [128, 2, 512], F32, "mPP", psum=True, es=es)
        PO2 = Rot(K, 1, [128, 2, 512], F32, "mPO", psum=True, es=es)
        PTp = Rot(K, 1, [128, 8, 128], BF16, "mPTp", psum=True, es=es)
        def make_gen(ti):
            cs = slice(ti * 512, (ti + 1) * 512)
            YA, dYA = YAr.next()
            YB, dYB = YBr.next()
            SG, dSG = SGr.next()
            K.sp.dma(lambda e, YA=YA, cs=cs: e.dma_start(out=YA[:], in_=scr["ya_fm"][:, cs].rearrange("(c p) t -> p c t", p=128)),
                     reads=[scr["d_p2c"]], writes=[dYA])
            K.pool.dma(lambda e, YB=YB, cs=cs: e.dma_start(out=YB[:], in_=scr["yb_fm"][:, cs].rearrange("(c p) t -> p c t", p=128)),
                       reads=[scr["d_p3"]], writes=[dYB])
            K.sp.dma(lambda e, SG=SG, cs=cs: e.dma_start(out=SG[:], in_=scr["sg_fm"][:, cs].rearrange("(c p) t -> p c t", p=128)),
                     reads=[scr["d_p1"]], writes=[dSG])
            MG, dMG = MGr.next()
            for m in range(8):
                pp, dpp = PP.next()
                for c in range(4):
                    K.pe.op(lambda e, pp=pp, YA=YA, c=c, m=m: e.matmul(pp[:, 0, :], PAw[:, c, m * 128:(m + 1) * 128], YA[:, c, :],
                                                                      start=(c == 0), stop=(c == 3)),
                            reads=[dYA, d_c], writes=[dpp] if c == 0 else [], adds=[] if c == 0 else [dpp])
                for c in range(4):
                    K.pe.op(lambda e, pp=pp, YB=YB, c=c, m=m: e.matmul(pp[:, 1, :], PBw[:, c, m * 128:(m + 1) * 128], YB[:, c, :],
                                                                      start=(c == 0), stop=(c == 3)),
                            reads=[dYB, d_c], adds=[dpp])
                t1, dt1 = t1r.next()
                t2, dt2 = t2r.next()
                K.dve.op(lambda e, t1=t1, pp=pp, SG=SG, m=m: e.tensor_tensor(out=t1[:], in0=pp[:, 0, :], in1=SG[:, m, :], op=ALU.mult),
                         reads=[dpp, dSG], writes=[dt1])
                K.dve.op(lambda e, t2=t2, pp=pp, SG=SG, m=m: e.tensor_tensor(out=t2[:], in0=pp[:, 1, :], in1=SG[:, 8 + m, :], op=ALU.mult),
                         reads=[dpp, dSG], writes=[dt2])
                K.pool.op(lambda e, t1=t1, t2=t2, MG=MG, m=m: e.tensor_tensor(out=MG[:, m, :], in0=t1[:], in1=t2[:], op=ALU.add),
                          reads=[dt1, dt2], writes=[dMG] if m == 0 else [], adds=[] if m == 0 else [dMG])
                if m % 2 == 1:
                    yield
            for sub in range(4):
                t0 = ti * 512 + sub * 128
                X, dX = Xr.next()
                K.pool.dma(lambda e, X=X, t0=t0: e.dma_start(out=X[:], in_=io["x"][t0:t0 + 128, :]), writes=[dX])
                po, dpo = PO2.next()
                for n in range(2):
                    for m in range(8):
                        K.pe.op(lambda e, po=po, MG=MG, m=m, n=n, sub=sub: e.matmul(
                            po[:, n, :], MG[:, m, sub * 128:(sub + 1) * 128], WO[:, m, n * 512:(n + 1) * 512],
                            start=(m == 0), stop=(m == 7)), reads=[dMG, d_c],
                            writes=[dpo] if (m == 0 and n == 0) else [], adds=[] if (m == 0 and n == 0) else [dpo])
                X1, dX1 = X1r.next()
                K.dve.op(lambda e, X1=X1, X=X, po=po: e.tensor_tensor(out=X1[:], in0=X[:], in1=po[:].rearrange("p a b -> p (a b)"),
                                                                     op=ALU.add), reads=[dX, dpo], writes=[dX1])
                K.sp.dma(lambda e, X1=X1, t0=t0: e.dma_start(out=scr["x1_tm"][t0:t0 + 128, :], in_=X1[:]),
                         reads=[dX1], adds=[scr["d_p4"]])
                ST, dST = STr.next()
                K.act.op(lambda e, X1=X1, ST=ST: e.activation(out=junk[:], in_=X1[:], func=AF.Square, accum_out=ST[:, 0:1]),
                         reads=[dX1], writes=[d_junk, dST])
                K.dve.op(lambda e, ST=ST: e.tensor_scalar(out=ST[:, 1:2], in0=ST[:, 0:1], scalar1=1.0 / D, scalar2=1e-6,
                                                          op0=ALU.mult, op1=ALU.add), writes=[dST])
                K.act.op(lambda e, ST=ST: e.activation(out=ST[:, 2:3], in_=ST[:, 1:2], func=AF.Ln), writes=[dST])
                K.act.op(lambda e, ST=ST: e.activation(out=ST[:, 3:4], in_=ST[:, 2:3], func=AF.Exp, scale=-0.5), writes=[dST])
                XH, dXH = XHr.next()
                K.dve.op(lambda e, XH=XH, X1=X1, ST=ST: e.scalar_tensor_tensor(
                    out=XH[:], in0=X1[:], scalar=ST[:, 3:4], in1=NF[:], op0=ALU.mult, op1=ALU.mult),
                    reads=[dX1, dST, d_c], writes=[dXH])
                K.sp.dma(lambda e, XH=XH, t0=t0: e.dma_start(out=scr["xh_tm"][t0:t0 + 128, :], in_=XH[:]),
                         reads=[dXH], adds=[scr["d_p4"]])
                XB, dXB = XBr.next()
                K.act.op(lambda e, XB=XB, XH=XH: e.activation(out=XB[:], in_=XH[:], func=AF.Copy), reads=[dXH], writes=[dXB])
                yield
                ptp, dptp = PTp.next()
                for kc in range(8):
                    K.pe.op(lambda e, ptp=ptp, XB=XB, kc=kc: e.transpose(out=ptp[:, kc, :], in_=XB[:, kc * 128:(kc + 1) * 128],
                                                                        identity=identb[:]),
                            reads=[dXB, d_c], writes=[dptp] if kc == 0 else [], adds=[] if kc == 0 else [dptp])
                XT, dXT = XTr.next()
                K.act.op(lambda e, ptp=ptp, XT=XT: e.activation(out=XT[:], in_=ptp[:], func=AF.Copy), reads=[dptp], writes=[dXT])
                K.sp.dma(lambda e, XT=XT, t0=t0: e.dma_start(
                    out=dap(scr["xhT_fm"], t0, [[T, 128], [128 * T, 8], [1, 128]]), in_=XT[:]),
                    reads=[dXT], adds=[scr["d_p4"]])
                yield

        work = [(ti,) for ti in range(NT)]

        active = []
        nxt = 0
        while nxt < len(work) or active:
            if len(active) < 2 and nxt < len(work):
                active.append(make_gen(*work[nxt]))
                nxt += 1
            for a in list(active):
                try:
                    next(a)
                except StopIteration:
                    active.remove(a)


def phase5(K, cfg, io, scr):
    T, S, NB = cfg["T"], cfg["S"], cfg["NB"]
    NTT = T // 128
    with ExitStack() as es:
        d_c = Dep()
        identb = K.sb([128, 128], BF16, "f_identb", es)
        K.sp.dma(lambda e: e.dma_start(out=identb[:], in_=io["ident_bf"][:, :]), adds=[d_c])
        IOTA = K.sb([128, 16], F32, "IOTA", es)
        K.sp.dma(lambda e: e.dma_start(out=IOTA[:], in_=io["iota16"][:, :]), adds=[d_c])
        FNW = K.sb([128, D], F32, "FNW", es)
        K.sp.dma(lambda e: e.dma_start(out=FNW[:], in_=io["final_norm_w"][0:1, :].broadcast_to([128, D])), adds=[d_c])
        WQ = K.sb([128, 8, 2048], BF16, "WQ", es)
        KT = K.sb([128, 16, 128], BF16, "KT", es)
        PQ = Rot(K, 1, [128, 8, 128], F32, "fPQ", psum=True, es=es)
        PSc = Rot(K, 1, [128, 8, 128], F32, "fPSc", psum=True, es=es)
        PKT = Rot(K, 1, [128, 8, 128], BF16, "fPKT", psum=True, es=es)
        es_setup = ExitStack()
        stg = Rot(K, 2, [128, 1024], F32, "f_stg", es=es_setup)
        q = [0]
        load_w_bf16(K, es, io["peer_wq"][0], D, 2048, "WQ", d_c, stg, q, W=WQ)
        KF = K.sb([128, 16, 128], F32, "KF", es_setup)
        dKF = Dep()
        K.sp.dma(lambda e: e.dma_start(out=KF[:], in_=io["peer_keys"][0].rearrange("h c n d -> n (h c) d")), writes=[dKF])
        KB = K.sb([128, 16, 128], BF16, "KB", es_setup)
        K.dve.op(lambda e: e.tensor_copy(out=KB[:], in_=KF[:]), reads=[dKF], writes=[dKF])
        for half in range(2):
            pk, dpk = PKT.next()
            for i in range(8):
                K.pe.op(lambda e, pk=pk, i=i, half=half: e.transpose(out=pk[:, i, :], in_=KB[:, half * 8 + i, :], identity=identb[:]),
                        reads=[dKF, d_c], writes=[dpk] if i == 0 else [], adds=[] if i == 0 else [dpk])
            K.act.op(lambda e, pk=pk, half=half: e.activation(out=KT[:, half * 8:(half + 1) * 8, :], in_=pk[:], func=AF.Copy),
                     reads=[dpk], adds=[d_c])

        K.barrier()
        es_setup.close()
        XTr = Rot(K, 2, [128, 8, 128], BF16, "fXT", es=es)
        XHr = Rot(K, 2, [128, D], F32, "fXH", es=es)
        X1r = Rot(K, 2, [128, D], F32, "fX1", es=es)
        QTr = Rot(K, 1, [128, 16, 128], BF16, "fQT", es=es)
        SCr = Rot(K, 1, [128, 16, 128], F32, "fSC", es=es)
        SC2 = K.sb([128, 256], F32, "fSC2", es)
        dSC2 = Dep()
        M16r = Rot(K, 1, [128, 16, 16], F32, "fM16", es=es)
        I16r = Rot(K, 1, [128, 16, 16], U32, "fI16", es=es)
        I16fr = Rot(K, 1, [128, 16, 16], F32, "fI16f", es=es)
        CANDr = Rot(K, 1, [128, 8, 256], F32, "fCAND", es=es)
        VALr = Rot(K, 1, [128, 8, 16], F32, "fVAL", es=es)
        CIr = Rot(K, 1, [128, 3, 128], U32, "fCI", es=es)
        ABr = Rot(K, 1, [128, 2, 128], F32, "fAB", es=es)
        OHr = CANDr
        E12r = Rot(K, 1, [128, 3, 128], F32, "fE12", es=es)
        IDSr = Rot(K, 2, [128, 128], I32, "fIDS", es=es)
        GTr = Rot(K, 2, [128, 4, 128], F32, "fGT", es=es)
        S8r = Rot(K, 2, [128, 16], F32, "fS8", es=es)
        GRP = cfg.get("grp", 4)
        ACTDOT = tuple(cfg.get("actdot", (0, 2)))
        if isinstance(cfg.get("actdot_mask"), int):
            ACTDOT = tuple(i for i in range(GRP) if (cfg["actdot_mask"] >> i) & 1)
        junk3r = Rot(K, 2, [128, D], BF16, "fjunk3", es=es)
        junkr = Rot(K, 3, [128, D], BF16, "fjunkr", es=es)
        PRDr = Rot(K, 3, [128, D], BF16, "fPRD", es=es)
        GBr = Rot(K, cfg.get("ngbuf", 22), [128, 2 * D], BF16, "fGB", es=es)
        junk2 = K.sb([128, D], BF16, "fjunk2", es)
        d_junk2 = Dep()
        XHbr = Rot(K, 2, [128, D], BF16, "fXHb", es=es)
        DGr = Rot(K, 4, [128, 128], BF16, "fDG", es=es)
        PY = Rot(K, 1, [128, 2, 512], F32, "fPY", psum=True, es=es)

        RES = {}

        def routing(ti):
            t0 = ti * 128
            XT, dXT = XTr.next()
            XH, dXH = XHr.next()
            X1, dX1 = X1r.next()
            K.sp.dma(lambda e, XT=XT, t0=t0: e.dma_start(out=XT[:], in_=dap(scr["xhT_fm"], t0, [[T, 128], [128 * T, 8], [1, 128]])),
                     reads=[scr["d_p4"]], writes=[dXT])
            K.sp.dma(lambda e, XH=XH, t0=t0: e.dma_start(out=XH[:], in_=scr["xh_tm"][t0:t0 + 128, :]), reads=[scr["d_p4"]], writes=[dXH])
            K.sp.dma(lambda e, X1=X1, t0=t0: e.dma_start(out=X1[:], in_=scr["x1_tm"][t0:t0 + 128, :]), reads=[scr["d_p4"]], writes=[dX1])
            QT, dQT = QTr.next()
            for half in range(2):
                pq, dpq = PQ.next()
                for i in range(8):
                    hc = half * 8 + i
                    for kc in range(8):
                        K.pe.op(lambda e, pq=pq, i=i, hc=hc, kc=kc, XT=XT: e.matmul(
                            pq[:, i, :], WQ[:, kc, hc * 128:(hc + 1) * 128], XT[:, kc, :], start=(kc == 0), stop=(kc == 7)),
                            reads=[dXT, d_c], writes=[dpq] if (i == 0 and kc == 0) else [], adds=[] if (i == 0 and kc == 0) else [dpq])
                K.act.op(lambda e, pq=pq, QT=QT, half=half: e.activation(out=QT[:, half * 8:(half + 1) * 8, :], in_=pq[:], func=AF.Copy),
                         reads=[dpq], writes=[dQT] if half == 0 else [], adds=[] if half == 0 else [dQT])
            SC, dSC = SCr.next()
            for half in range(2):
                psc, dpsc = PSc.next()
                for i in range(8):
                    hc = half * 8 + i
                    K.pe.op(lambda e, psc=psc, i=i, hc=hc, QT=QT: e.matmul(psc[:, i, :], QT[:, hc, :], KT[:, hc, :], start=True, stop=True),
                            reads=[dQT, d_c], writes=[dpsc] if i == 0 else [], adds=[] if i == 0 else [dpsc])
                K.act.op(lambda e, psc=psc, SC=SC, half=half: e.activation(out=SC[:, half * 8:(half + 1) * 8, :], in_=psc[:], func=AF.Copy),
                         reads=[dpsc], writes=[dSC] if half == 0 else [], adds=[] if half == 0 else [dSC])
            yield
            M16, dM = M16r.next()
            I16, dI = I16r.next()
            for hc in range(16):
                if hc % 4 == 0 and hc > 0:
                    yield
                K.dve.op(lambda e, M16=M16, SC=SC, hc=hc: e.max(out=M16[:, hc, 0:8], in_=SC[:, hc, :]), reads=[dSC],
                         writes=[dM] if hc == 0 else [], adds=[] if hc == 0 else [dM])
                K.dve.op(lambda e, M16=M16, SC=SC, hc=hc: e.match_replace(out=SC2[:, 0:128], in_to_replace=M16[:, hc, 0:8],
                                                                         in_values=SC[:, hc, :], imm_value=-1e30),
                         reads=[dSC, dM], writes=[dSC2])
                K.dve.op(lambda e, M16=M16, hc=hc: e.max(out=M16[:, hc, 8:16], in_=SC2[:, 0:128]), reads=[dSC2], adds=[dM])
                K.dve.op(lambda e, M16=M16, I16=I16, SC=SC, hc=hc: e.max_index(out=I16[:, hc, 0:8], in_max=M16[:, hc, 0:8],
                                                                              in_values=SC[:, hc, :]),
                         reads=[dSC, dM], writes=[dI] if hc == 0 else [], adds=[] if hc == 0 else [dI])
                K.dve.op(lambda e, M16=M16, I16=I16, SC=SC, hc=hc: e.max_index(out=I16[:, hc, 8:16], in_max=M16[:, hc, 8:16],
                                                                              in_values=SC[:, hc, :]),
                         reads=[dSC, dM], adds=[dI])
            yield
            I16f, dIf = I16fr.next()
            K.dve.op(lambda e, I16f=I16f, I16=I16: e.tensor_copy(out=I16f[:], in_=I16[:]), reads=[dI], writes=[dIf])
            I16fv = I16f[:].rearrange("p (h c) k -> p h c k", c=2)
            K.dve.op(lambda e, I16fv=I16fv: e.tensor_scalar(out=I16fv[:, :, 0, :], in0=I16fv[:, :, 0, :], scalar1=128.0, scalar2=None,
                                                            op0=ALU.mult), writes=[dIf])
            CAND, dCA = CANDr.next()
            M16v = M16[:].rearrange("p (h c) k -> p h c k", c=2)
            K.dve.op(lambda e, CAND=CAND, M16v=M16v: e.tensor_tensor(
                out=CAND[:].rearrange("p h (a b) -> p h a b", b=16),
                in0=M16v[:, :, 0, :].unsqueeze(3).broadcast_to([128, 8, 16, 16]),
                in1=M16v[:, :, 1, :].unsqueeze(2).broadcast_to([128, 8, 16, 16]), op=ALU.add),
                reads=[dM], writes=[dCA])
            VAL, dVAL = VALr.next()
            CI, dCI = CIr.next()
            CIv = CI[:, 0, :].rearrange("p (h k) -> p h k", k=16)
            for h in range(8):
                if h % 4 == 0:
                    yield
                K.dve.op(lambda e, VAL=VAL, CAND=CAND, h=h: e.max(out=VAL[:, h, 0:8], in_=CAND[:, h, :]), reads=[dCA],
                         writes=[dVAL] if h == 0 else [], adds=[] if h == 0 else [dVAL])
                K.dve.op(lambda e, VAL=VAL, CAND=CAND, h=h: e.match_replace(out=SC2[:, :], in_to_replace=VAL[:, h, 0:8],
                                                                           in_values=CAND[:, h, :], imm_value=-1e30),
                         reads=[dCA, dVAL], writes=[dSC2])
                K.dve.op(lambda e, VAL=VAL, h=h: e.max(out=VAL[:, h, 8:16], in_=SC2[:, :]), reads=[dSC2], adds=[dVAL])
                K.dve.op(lambda e, VAL=VAL, CIv=CIv, CAND=CAND, h=h: e.max_index(out=CIv[:, h, 0:8], in_max=VAL[:, h, 0:8],
                                                                                in_values=CAND[:, h, :]),
                         reads=[dCA, dVAL], writes=[dCI] if h == 0 else [], adds=[] if h == 0 else [dCI])
                K.dve.op(lambda e, VAL=VAL, CIv=CIv, CAND=CAND, h=h: e.max_index(out=CIv[:, h, 8:16], in_max=VAL[:, h, 8:16],
                                                                                in_values=CAND[:, h, :]),
                         reads=[dCA, dVAL], adds=[dCI])
            yield
            GT, dGT = GTr.next()
            S8, dS8 = S8r.next()
            Ev = GT[:, 0, :].rearrange("p (h k) -> p h k", k=16)
            Gv = GT[:, 1, :].rearrange("p (h k) -> p h k", k=16)
            K.dve.op(lambda e, Ev=Ev, VAL=VAL: e.tensor_tensor(out=Ev, in0=VAL[:], in1=VAL[:, :, 0:1].broadcast_to([128, 8, 16]),
                                                              op=ALU.subtract), reads=[dVAL], writes=[dGT])
            K.act.op(lambda e, GT=GT: e.activation(out=GT[:, 0, :], in_=GT[:, 0, :], func=AF.Exp), writes=[dGT])
            K.dve.op(lambda e, Ev=Ev, S8=S8: e.tensor_reduce(out=S8[:, 0:8], in_=Ev, axis=AX.X, op=ALU.add), reads=[dGT], writes=[dS8])
            K.dve.op(lambda e, S8=S8: e.reciprocal(out=S8[:, 8:16], in_=S8[:, 0:8]), writes=[dS8])
            K.dve.op(lambda e, Ev=Ev, Gv=Gv, S8=S8: e.tensor_tensor(out=Gv, in0=Ev, in1=S8[:, 8:16].unsqueeze(2).broadcast_to([128, 8, 16]),
                                                                   op=ALU.mult), reads=[dS8], writes=[dGT])
            yield
            K.dve.op(lambda e, CI=CI: e.tensor_single_scalar(out=CI[:, 1, :], in_=CI[:, 0, :], scalar=4, op=ALU.logical_shift_right),
                     writes=[dCI])
            K.dve.op(lambda e, CI=CI: e.tensor_single_scalar(out=CI[:, 2, :], in_=CI[:, 0, :], scalar=15, op=ALU.bitwise_and),
                     writes=[dCI])
            AB, dAB = ABr.next()
            K.dve.op(lambda e, AB=AB, CI=CI: e.tensor_copy(out=AB[:], in_=CI[:, 1:3, :]), reads=[dCI], writes=[dAB])
            OH, dOH = OHr.next()
            E12, dE12 = E12r.next()
            OHv = OH[:].rearrange("p h (j a) -> p h j a", a=16)
            for c in range(2):
                ABv = AB[:, c, :].rearrange("p (h j) -> p h j", j=16)
                K.dve.op(lambda e, OHv=OHv, ABv=ABv: e.tensor_tensor(
                    out=OHv, in0=ABv.unsqueeze(3).broadcast_to([128, 8, 16, 16]),
                    in1=IOTA[:].unsqueeze(1).unsqueeze(1).broadcast_to([128, 8, 16, 16]), op=ALU.is_equal),
                    reads=[dAB, d_c], writes=[dOH])
                K.dve.op(lambda e, OHv=OHv, I16fv=I16fv, c=c: e.tensor_tensor(
                    out=OHv, in0=OHv, in1=I16fv[:, :, c, :].unsqueeze(2).broadcast_to([128, 8, 16, 16]), op=ALU.mult),
                    reads=[dIf], writes=[dOH])
                K.dve.op(lambda e, OHv=OHv, E12=E12, c=c: e.tensor_reduce(
                    out=E12[:, c, :].rearrange("p (h j) -> p h j", j=16), in_=OHv, axis=AX.X, op=ALU.add),
                    reads=[dOH], writes=[dE12] if c == 0 else [], adds=[] if c == 0 else [dE12])
            K.dve.op(lambda e, E12=E12: e.tensor_tensor(out=E12[:, 2, :], in0=E12[:, 0, :], in1=E12[:, 1, :], op=ALU.add), writes=[dE12])
            IDS, dIDS = IDSr.next()
            K.dve.op(lambda e, IDS=IDS, E12=E12: e.tensor_copy(out=IDS[:], in_=E12[:, 2, :]), reads=[dE12], writes=[dIDS])
            if "ids_dbg" in scr:
                K.sp.dma(lambda e, IDS=IDS, t0=t0: e.dma_start(out=scr["ids_dbg"][t0:t0 + 128, :], in_=IDS[:]), reads=[dIDS])
                K.sp.dma(lambda e, GT=GT, t0=t0: e.dma_start(out=scr["gate_dbg"][t0:t0 + 128, :], in_=GT[:, 1, :]), reads=[dGT])
            RES[ti] = dict(t0=t0, XH=XH, dXH=dXH, X1=X1, dX1=dX1, IDS=IDS, dIDS=dIDS, GT=GT, dGT=dGT, S8=S8, dS8=dS8)

        GDEPS = {}

        def expert(R):
            t0, XH, dXH, X1, dX1, IDS, dIDS, GT, dGT, S8, dS8 = (R[k] for k in
                ("t0", "XH", "dXH", "X1", "dX1", "IDS", "dIDS", "GT", "dGT", "S8", "dS8"))
            XHb, dXHb = XHbr.next()
            K.act.op(lambda e: e.activation(out=XHb[:], in_=XH[:], func=AF.Copy), reads=[dXH], writes=[dXHb])
            py, dpy = PY.next()
            NGRP = 128 // GRP
            bufs = {}
            gd = GDEPS.setdefault(id(GT), [(Dep(), Dep()) for _ in range(NGRP)])

            def stage_a(g):
                for jj in range(GRP):
                    j = g * GRP + jj
                    GB, dGB = GBr.next()
                    bufs[j] = (GB, dGB)
                    K.pool.dma(lambda e, GB=GB, j=j: e.indirect_dma_start(
                        out=GB[:], out_offset=None, in_=scr["uv_tab"][:, :],
                        in_offset=bass.IndirectOffsetOnAxis(ap=IDS[:, j:j + 1], axis=0)), reads=[dIDS, scr["d_uv"]], writes=[dGB])
                    if jj in ACTDOT:
                        PRD, dPRD = PRDr.next()
                        K.dve.op(lambda e, GB=GB, PRD=PRD: e.tensor_tensor(out=PRD[:], in0=GB[:, 0:D], in1=XHb[:], op=ALU.mult),
                                 reads=[dGB, dXHb], writes=[dPRD])
                        j3, dj3 = junk3r.next()
                        K.act.op(lambda e, PRD=PRD, j=j, j3=j3: e.activation(out=j3[:], in_=PRD[:], func=AF.Copy, accum_out=GT[:, 2, j:j + 1]),
                                 reads=[dPRD], writes=[dj3] + ([gd[g][0]] if jj == 0 else []), adds=[] if jj == 0 else [gd[g][0]])
                    else:
                        j1, dj1 = junkr.next()
                        K.dve.op(lambda e, GB=GB, j=j, j1=j1: e.scalar_tensor_tensor(
                            out=j1[:], in0=GB[:, 0:D], scalar=1.0, in1=XHb[:], op0=ALU.mult, op1=ALU.mult, accum_out=GT[:, 2, j:j + 1]),
                            reads=[dGB, dXHb], writes=[dj1] + ([gd[g][0]] if jj == 0 else []), adds=[] if jj == 0 else [gd[g][0]])
                gs = slice(g * GRP, (g + 1) * GRP)
                K.act.op(lambda e: e.activation(out=GT[:, 3, gs], in_=GT[:, 2, gs], func=AF.Gelu), reads=[gd[g][0]], writes=[gd[g][1]])

            def stage_b(g):
                gs = slice(g * GRP, (g + 1) * GRP)
                K.dve.op(lambda e: e.tensor_tensor(out=GT[:, 3, gs], in0=GT[:, 3, gs], in1=GT[:, 1, gs], op=ALU.mult), reads=[dGT], writes=[gd[g][1]])
                for jj in range(GRP):
                    j = g * GRP + jj
                    GB, dGB = bufs.pop(j)
                    DG, dDG = DGr.next()
                    K.act.op(lambda e, DG=DG, j=j: e.activation(out=DG[:], in_=identb[:], func=AF.Copy, scale=GT[:, 3, j:j + 1]),
                             reads=[gd[g][1], d_c], writes=[dDG])
                    for n in range(2):
                        K.pe.op(lambda e, DG=DG, GB=GB, n=n, j=j: e.matmul(
                            py[:, n, :], DG[:], GB[:, D + n * 512:D + (n + 1) * 512], start=(j == 0), stop=(j == 127)),
                            reads=[dDG, dGB], writes=[dpy] if (j == 0 and n == 0) else [], adds=[] if (j == 0 and n == 0) else [dpy])

            gen = routing(R["next"]) if R.get("next") is not None else None
            for g in range(NGRP):
                stage_a(g)
                if g >= 1:
                    stage_b(g - 1)
                if gen is not None and g >= 2 and g % 2 == 0:
                    try:
                        next(gen)
                    except StopIteration:
                        gen = None
            stage_b(NGRP - 1)
            if gen is not None:
                for _ in gen:
                    pass
            K.dve.op(lambda e: e.tensor_tensor(out=X1[:], in0=X1[:], in1=py[:].rearrange("p a b -> p (a b)"), op=ALU.add),
                     reads=[dpy], writes=[dX1])
            K.act.op(lambda e: e.activation(out=junk2[:], in_=X1[:], func=AF.Square, accum_out=S8[:, 0:1]),
                     reads=[dX1], writes=[d_junk2, dS8])
            K.dve.op(lambda e: e.tensor_scalar(out=S8[:, 1:2], in0=S8[:, 0:1], scalar1=1.0 / D, scalar2=1e-6,
                                               op0=ALU.mult, op1=ALU.add), writes=[dS8])
            K.act.op(lambda e: e.activation(out=S8[:, 2:3], in_=S8[:, 1:2], func=AF.Sqrt), writes=[dS8])
            K.dve.op(lambda e: e.reciprocal(out=S8[:, 3:4], in_=S8[:, 2:3]), writes=[dS8])
            K.dve.op(lambda e: e.scalar_tensor_tensor(
                out=XH[:], in0=X1[:], scalar=S8[:, 3:4], in1=FNW[:], op0=ALU.mult, op1=ALU.mult),
                reads=[dX1, dS8, d_c], writes=[dXH])
            K.sp.dma(lambda e: e.dma_start(out=io["out"][t0:t0 + 128, :], in_=XH[:]), reads=[dXH])

        for _ in routing(0):
            pass
        for ti in range(NTT):
            R = RES.pop(ti)
            R["next"] = ti + 1 if ti + 1 < NTT else None
            expert(R)


def make_consts():
    c = {}
    c["ident_bf"] = np.eye(128, dtype=np.float32).astype(ml_dtypes.bfloat16)
    c["ident_f"] = np.eye(128, dtype=np.float32)
    bo = np.zeros((128, 128), np.float32)
    bo[:64, :64] = 1.0 / 64
    bo[64:, 64:] = 1.0 / 64
    c["blockones"] = bo
    pp = np.arange(128)[:, None]
    ff = np.arange(128)[None, :]
    c["tri"] = ((pp <= ff) & (pp // 64 == ff // 64)).astype(np.float32)
    p6 = np.arange(64)[:, None]
    f6 = np.arange(64)[None, :]
    mk = np.zeros((64, 3, 64), np.float32)
    mk[:, 0, :] = (f6 < p6)
    mk[:, 1, :] = (f6 > p6)
    mk[:, 2, :] = (f6 >= p6)
    c["masks"] = mk
    c["ident64"] = np.eye(64, dtype=np.float32).astype(ml_dtypes.bfloat16)
    c["ones64"] = np.ones((64, 1), np.float32)
    c["iota16"] = np.tile(np.arange(16, dtype=np.float32)[None, :], (128, 1))
    return c


def make_alibi(S):
    al = np.zeros((4, 128, S), np.float32)
    ql = np.arange(128)[:, None]
    m = np.arange(S)[None, :]
    for h in range(4):
        slope = 2.0 ** (-8.0 * (h + 1) / 4)
        v = -slope * (ql - m + (S - 128)).astype(np.float32)
        al[h] = np.where(m <= ql + (S - 128), v, -30000.0)
    return al


def _unused():
    c = {}
    return c


def build(cfg):
    NB, S = cfg["NB"], cfg["S"]
    T = NB * S
    cfg["T"] = T
    dbg = set(cfg.get("debug", ()))
    phases = cfg.get("phases", (1,))
    nc = bass.Bass("TRN2", target_bir_lowering=False)
    io = {}

    def inp(name, shape, dt=F32):
        io[name] = nc.dram_tensor(name, list(shape), dt, kind="ExternalInput").ap()

    inp("x", [T, D])
    inp("norm_mix_w", [1, D])
    inp("w_in", [1, D, IN_COLS])
    inp("ident_bf", [128, 128], BF16)
    inp("ident_f", [128, 128])
    inp("blockones", [128, 128])
    inp("tri", [128, 128])
    inp("masks", [64, 3, 64])
    inp("ident64", [64, 64], BF16)
    inp("ones64", [64, 1])
    inp("alibi", [4, 128, S])
    for nm, shp in [("lam_q1", [1, 64]), ("lam_k1", [1, 64]), ("lam_q2", [1, 64]), ("lam_k2", [1, 64]),
                    ("subln_w", [1, 128])]:
        inp(nm, shp)
    scr = {}

    def scratch(name, shape, dt):
        kind = "ExternalOutput" if name in dbg else "Internal"
        scr[name] = nc.dram_tensor(name, list(shape), dt, kind=kind).ap()

    scratch("zs_tm", [T, SHIFT_COLS], F32)
    scratch("zv_fm", [512, T], F32)
    scratch("qk_fm", [1024, T], BF16)
    scratch("av_tm", [T, 512], BF16)
    scratch("sg_fm", [2048, T], BF16)
    if not cfg.get("chunked", True):
        scratch("rw_tm", [T, 5, 512], F32)
    scratch("ab_tm", [T, 5, 512], BF16)
    scratch("lw_tm", [T, 512], F32)
    scratch("v_fm", [512, T], F32)
    scratch("g_fm", [512, T], F32)
    scratch("coef_fm", [8, T], F32)
    scratch("y_fm", [512, T], F32)
    scratch("ya_fm", [512, T], BF16)
    scr["d_p1"] = Dep()
    scr["d_p2a"] = Dep()
    scr["d_p2b"] = Dep()
    scr["d_p2c"] = Dep()
    scr["d_p3"] = Dep()
    scr["d_p4"] = Dep()
    scr["d_uv"] = Dep()
    scratch("uv_tab", [16384, 2 * D], BF16)
    if "ids_dbg" in dbg:
        scratch("ids_dbg", [T, 128], I32)
        scratch("gate_dbg", [T, 128], F32)
    inp("peer_wq", [1, D, 2048])
    inp("peer_keys", [1, 8, 2, 128, 128])
    inp("peer_u", [1, 16384, D])
    inp("peer_v", [1, 16384, D])
    inp("final_norm_w", [1, D])
    inp("iota16", [128, 16])
    io["out"] = nc.dram_tensor("out", [T, D], F32, kind="ExternalOutput").ap()
    scratch("x1_tm", [T, D], F32)
    scratch("xh_tm", [T, D], F32)
    scratch("xhT_fm", [D, T], BF16)
    for nm, shp in [("proj_a", [1, 512, D]), ("proj_b", [1, 512, D]), ("w_out", [1, D, D]), ("norm_ffn_w", [1, D])]:
        inp(nm, shp)
    scratch("yb_fm", [512, T], BF16)
    for nm, shp in [("shift_mu", [1, SHIFT_COLS]), ("w0", [1, 512]), ("w2", [1, 64, 512]), ("a0", [1, 512]),
                    ("a2", [1, 64, 512]), ("g2", [1, 128, 512]), ("k_k", [1, 512]), ("k_a", [1, 512]),
                    ("r_k", [1, 8, 64]), ("lnx_w", [1, 512]), ("lnx_b", [1, 512])]:
        inp(nm, shp)
    with ExitStack() as es:
        K = Kern(nc, es, pool_slots=cfg.get("pool_slots", 8))
        K.scopes = bool(cfg.get("scopes", False))
        if 5 in phases:
            K.phase = "p0_uvtab"
            RB = 2048
            for r0 in range(0, 16384, RB):
                K.pool.dma(lambda e, r0=r0: e.dma_start(out=scr["uv_tab"][r0:r0 + RB, 0:D], in_=io["peer_u"][0, r0:r0 + RB, :]),
                           adds=[scr["d_uv"]])
                K.pool.dma(lambda e, r0=r0: e.dma_start(out=scr["uv_tab"][r0:r0 + RB, D:2 * D], in_=io["peer_v"][0, r0:r0 + RB, :]),
                           adds=[scr["d_uv"]])
        if 1 in phases:
            K.phase = "p1_inproj"
            phase1(K, cfg, io, scr)
            K.barrier()
        if 2 in phases:
            K.phase = "p2a_prep"
            phase2_prep(K, cfg, io, scr)
            K.barrier()
            K.phase = "p2b_scan"
            if cfg.get("chunked", True):
                phase2_chunk(K, cfg, io, scr)
            else:
                phase2_scan(K, cfg, io, scr)
            K.barrier()
            K.phase = "p2c_post"
            phase2_post(K, cfg, io, scr)
            K.barrier()
        if 3 in phases:
            K.phase = "p3_attn"
            phase3(K, cfg, io, scr)
            K.barrier()
        if 4 in phases:
            K.phase = "p4_merge"
            phase4(K, cfg, io, scr)
            K.barrier()
        if 5 in phases:
            K.phase = "p5_peer"
            phase5(K, cfg, io, scr)
            K.barrier()
        K.finish()
    return nc, io, scr


def kernel(**inputs):
    NB, S = 4, 2048
    cfg = dict(NB=NB, S=S, phases=(1, 2, 3, 4, 5))
    nc, io, scr = build(cfg)
    consts = make_consts()
    consts["alibi"] = make_alibi(S)
    x = np.ascontiguousarray(np.asarray(inputs["x"], dtype=np.float32))
    shared = {}
    for name in io:
        if name in ("x", "out"):
            continue
        if name in consts:
            shared[name] = consts[name]
        elif name == "final_norm_w":
            shared[name] = np.ascontiguousarray(np.asarray(inputs[name], dtype=np.float32).reshape(1, D))
        else:
            shared[name] = np.ascontiguousarray(np.asarray(inputs[name], dtype=np.float32))
    in_maps = []
    for c in range(NCORES):
        m = dict(shared)
        m["x"] = x[c * NB:(c + 1) * NB].reshape(NB * S, D)
        in_maps.append(m)
    res = run_bass_kernel_spmd(nc, in_maps, core_ids=list(range(NCORES)))
    out = np.concatenate([np.asarray(r["out"]).reshape(NB, S, D) for r in res.results], axis=0)
    return out.astype(np.float32)
```

```python
import numpy as np
import ml_dtypes
from contextlib import ExitStack
import concourse.bass as bass
import concourse.mybir as mybir
from concourse.bass_utils import run_bass_kernel_spmd

F32 = mybir.dt.float32
BF16 = mybir.dt.bfloat16
I32 = mybir.dt.int32
U32 = mybir.dt.uint32
ALU = mybir.AluOpType
AF = mybir.ActivationFunctionType
AX = mybir.AxisListType

D = 1024
IN_COLS = 5376
SHIFT_COLS = 1792
NCORES = 8


class Dep:
    __slots__ = ("w", "r", "pw", "pr")

    def __init__(self):
        self.w = {}
        self.r = {}
        self.pw = {}
        self.pr = {}


class Stream:
    def __init__(self, K, name, is_pe=False, ndma=0):
        self.K = K
        self.name = name
        self.sem = K.new_sem("s_" + name)
        self.cnt = 0
        self.items = []
        self.waited = {}
        self.is_pe = is_pe
        self.dsems = [K.new_sem("d_%s%d" % (name, i)) for i in range(ndma)]
        self.duses = [0] * ndma
        self.dj = 0

    def wait_tok(self, tok):
        if tok is None:
            return
        sem, val = tok
        if sem is self.sem and self.is_pe:
            return
        key = id(sem)
        if self.waited.get(key, 0) >= val:
            return
        self.waited[key] = val
        self.items.append(("w", sem, val, self.K.phase))

    def _pre(self, reads, writes, adds):
        for d in reads:
            for t in list(d.w.values()):
                self.wait_tok(t)
        for d in writes:
            for t in list(d.w.values()):
                self.wait_tok(t)
            for t in list(d.r.values()):
                self.wait_tok(t)
        for d in adds:
            for t in list(d.r.values()) + list(d.pr.values()) + list(d.pw.values()):
                self.wait_tok(t)

    def _post(self, tok, reads, writes, adds):
        for d in reads:
            d.r[id(tok[0])] = tok
        for d in writes:
            d.pw = d.w
            d.pr = d.r
            d.w = {id(tok[0]): tok}
            d.r = {}
        for d in adds:
            d.w[id(tok[0])] = tok

    def op(self, fn, reads=(), writes=(), adds=()):
        self._pre(reads, writes, adds)
        self.cnt += 1
        tok = (self.sem, self.cnt)
        self.items.append(("o", fn, self.sem, 1, self.K.phase))
        self._post(tok, reads, writes, adds)
        return tok

    def dma(self, fn, reads=(), writes=(), adds=()):
        self._pre(reads, writes, adds)
        n = len(self.dsems)
        slot = self.dj % n
        self.dj += 1
        if self.duses[slot] > 0:
            self.wait_tok((self.dsems[slot], 16 * self.duses[slot]))
        self.duses[slot] += 1
        tok = (self.dsems[slot], 16 * self.duses[slot])
        self.items.append(("o", fn, self.dsems[slot], 16, self.K.phase))
        self._post(tok, reads, writes, adds)
        return tok

    def replay(self, eng):
        nc = self.K.nc
        cur = None
        ctx = None
        for it in self.items:
            ph = it[-1]
            if self.K.scopes and ph != cur:
                if ctx is not None:
                    ctx.__exit__(None, None, None)
                ctx = nc.named_scope(ph)
                ctx.__enter__()
                cur = ph
            if it[0] == "w":
                eng.wait_ge(it[1], it[2])
            else:
                ins = it[1](eng)
                ins.then_inc(it[2], it[3])
        if ctx is not None:
            ctx.__exit__(None, None, None)


class Kern:
    def __init__(self, nc, es, pool_slots=8):
        self.nc = nc
        self.es = es
        self.nsem = 0
        self.phase = "init"
        self.scopes = False
        self.pe = Stream(self, "pe", is_pe=True)
        self.act = Stream(self, "act", ndma=4)
        self.dve = Stream(self, "dve")
        self.pool = Stream(self, "pool", ndma=pool_slots)
        self.sp = Stream(self, "sp", ndma=8)
        self.uid = 0

    def new_sem(self, name):
        self.nsem += 1
        return self.es.enter_context(self.nc.semaphore(name))

    def sb(self, shape, dt, name=None, es=None):
        self.uid += 1
        nm = "%s_%d" % (name or "t", self.uid)
        return (es or self.es).enter_context(self.nc.sbuf_tensor(nm, list(shape), dt))

    def ps(self, shape, dt, name=None, es=None):
        self.uid += 1
        nm = "%s_%d" % (name or "p", self.uid)
        return (es or self.es).enter_context(self.nc.psum_tensor(nm, list(shape), dt))

    def dram(self, name, shape, dt, kind="Internal"):
        return self.nc.dram_tensor(name, list(shape), dt, kind=kind)

    def streams(self):
        return [self.pe, self.act, self.dve, self.pool, self.sp]

    def barrier(self):
        st = self.streams()
        toks = []
        for q in st:
            if q.cnt > 0:
                toks.append((q.sem, q.cnt))
            for i, sem in enumerate(q.dsems):
                if q.duses[i] > 0:
                    toks.append((sem, 16 * q.duses[i]))
        for s_ in st:
            for t in toks:
                s_.wait_tok(t)

    def finish(self):
        streams = [self.pe, self.act, self.dve, self.pool, self.sp]
        for s in streams:
            for q in streams:
                for i, sem in enumerate(q.dsems):
                    if q.duses[i] > 0:
                        s.wait_tok((sem, 16 * q.duses[i]))
        with self.nc.allow_non_contiguous_dma(reason="small strided param loads"), self.nc.Block() as block:
            @block.tensor
            def _(e):
                self.pe.replay(e)

            @block.scalar
            def _(e):
                self.act.replay(e)

            @block.vector
            def _(e):
                self.dve.replay(e)

            @block.gpsimd
            def _(e):
                self.pool.replay(e)

            @block.sync
            def _(e):
                self.sp.replay(e)


class Rot:
    def __init__(self, K, n, shape, dt, name, psum=False, es=None):
        self.t = [(K.ps if psum else K.sb)(shape, dt, name, es=es) for _ in range(n)]
        self.d = [Dep() for _ in range(n)]
        self.i = 0

    def next(self):
        j = self.i % len(self.t)
        self.i += 1
        return self.t[j], self.d[j]


def phase1(K, cfg, io, scr):
    nc = K.nc
    T = cfg["T"]
    NT = T // 512
    with ExitStack() as es:
        ident = K.sb([128, 128], BF16, "ident", es)
        d_ident = Dep()
        K.sp.dma(lambda e: e.dma_start(out=ident[:], in_=io["ident_bf"][:, :]), writes=[d_ident])
        nw = K.sb([128, 8], F32, "nw", es)
        d_nw = Dep()
        K.sp.dma(lambda e: e.dma_start(out=nw[:], in_=io["norm_mix_w"].rearrange("o (c p) -> p (o c)", p=128)),
                 writes=[d_nw])
        wt = K.sb([128, 8, IN_COLS], BF16, "wt", es)
        d_wt = Dep()
        wst = Rot(K, 2, [128, 1344], F32, "wst", es=es)
        q = 0
        for kc in range(8):
            for cp in range(4):
                st, dst = wst.next()
                eng = K.sp if q % 2 == 0 else K.pool
                q += 1
                eng.dma(lambda e, st=st, kc=kc, cp=cp: e.dma_start(
                    out=st[:], in_=io["w_in"][0, kc * 128:(kc + 1) * 128, cp * 1344:(cp + 1) * 1344]), writes=[dst])
                K.act.op(lambda e, st=st, kc=kc, cp=cp: e.activation(
                    out=wt[:, kc, cp * 1344:(cp + 1) * 1344], in_=st[:], func=AF.Copy, scale=nw[:, kc:kc + 1]),
                    reads=[dst, d_nw], writes=[d_wt])

        xs = Rot(K, 2, [128, D], F32, "xs", es=es)
        junk = K.sb([128, D], BF16, "junk", es)
        d_junk = Dep()
        xn = Rot(K, 2, [128, D], BF16, "xn", es=es)
        st4 = Rot(K, 4, [128, 4], F32, "st4", es=es)
        hT = Rot(K, 2, [128, 8, 512], BF16, "hT", es=es)
        ptr = Rot(K, 2, [128, 8, 128], BF16, "ptr", psum=True, es=es)
        pmm = Rot(K, 4, [128, 512], F32, "pmm", psum=True, es=es)
        o32 = Rot(K, 3, [128, 512], F32, "o32", es=es)
        o16 = Rot(K, 3, [128, 512], BF16, "o16", es=es)
        ev = [0]

        def evac(pt, pd, ncols, kind, dst_ap):
            if kind == "f32":
                ot, od = o32.next()
            else:
                ot, od = o16.next()
            use_act = (kind == "sig") or (ev[0] % 2 == 0)
            ev[0] += 1
            if kind == "sig":
                K.act.op(lambda e: e.activation(out=ot[:, :ncols], in_=pt[:, :ncols], func=AF.Sigmoid),
                         reads=[pd], writes=[od])
            elif use_act:
                K.act.op(lambda e: e.activation(out=ot[:, :ncols], in_=pt[:, :ncols], func=AF.Copy),
                         reads=[pd], writes=[od])
            else:
                K.dve.op(lambda e: e.tensor_copy(out=ot[:, :ncols], in_=pt[:, :ncols]), reads=[pd], writes=[od])
            K.sp.dma(lambda e: e.dma_start(out=dst_ap, in_=ot[:, :ncols]), reads=[od], adds=[scr["d_p1"]])

        for ti in range(NT):
            h_t, h_d = hT.next()
            for sub in range(4):
                t0 = ti * 512 + sub * 128
                x_t, x_d = xs.next()
                K.pool.dma(lambda e, x_t=x_t, t0=t0: e.dma_start(out=x_t[:], in_=io["x"][t0:t0 + 128, :]),
                           writes=[x_d])
                s_t, s_d = st4.next()
                K.dve.op(lambda e, s_t=s_t: e.memset(s_t[:], 0.0), writes=[s_d])
                K.act.op(lambda e, x_t=x_t, s_t=s_t: e.activation(out=junk[:], in_=x_t[:], func=AF.Square,
                                                                    accum_out=s_t[:, 0:1]),
                         reads=[x_d], writes=[d_junk, s_d])
                K.dve.op(lambda e, s_t=s_t: e.tensor_scalar(out=s_t[:, 1:2], in0=s_t[:, 0:1], scalar1=1.0 / D,
                                                            scalar2=1e-6, op0=ALU.mult, op1=ALU.add),
                         reads=[s_d], writes=[s_d])
                K.act.op(lambda e, s_t=s_t: e.activation(out=s_t[:, 3:4], in_=s_t[:, 1:2], func=AF.Sqrt),
                         reads=[s_d], writes=[s_d])
                K.dve.op(lambda e, s_t=s_t: e.reciprocal(out=s_t[:, 2:3], in_=s_t[:, 3:4]),
                         reads=[s_d], writes=[s_d])
                n_t, n_d = xn.next()
                K.dve.op(lambda e, x_t=x_t, s_t=s_t, n_t=n_t: e.tensor_scalar(
                    out=n_t[:], in0=x_t[:], scalar1=s_t[:, 2:3], scalar2=None, op0=ALU.mult),
                    reads=[x_d, s_d], writes=[n_d])
                p_t, p_d = ptr.next()
                for kc in range(8):
                    K.pe.op(lambda e, p_t=p_t, n_t=n_t, kc=kc: e.transpose(
                        out=p_t[:, kc, :], in_=n_t[:, kc * 128:(kc + 1) * 128], identity=ident[:]),
                        reads=[n_d, d_ident], writes=[p_d])
                K.act.op(lambda e, p_t=p_t, h_t=h_t, sub=sub: e.activation(
                    out=h_t[:, :, sub * 128:(sub + 1) * 128], in_=p_t[:], func=AF.Copy),
                    reads=[p_d], writes=[h_d])
            tsl = slice(ti * 512, (ti + 1) * 512)
            for sub in range(4):
                r0 = ti * 512 + sub * 128
                for (c0, ncols, kind, name, dc0) in [(0, 512, "f32", "zs_tm", 0), (512, 512, "f32", "zs_tm", 512),
                                                    (1024, 512, "f32", "zs_tm", 1024),
                                                    (1536, 256, "f32", "zs_tm", 1536),
                                                    (2816, 512, "bf16", "av_tm", 0)]:
                    pt, pd = pmm.next()
                    for kc in range(8):
                        K.pe.op(lambda e, pt=pt, kc=kc, sub=sub, c0=c0, ncols=ncols, h_t=h_t: e.matmul(
                            pt[:, :ncols], h_t[:, kc, sub * 128:(sub + 1) * 128], wt[:, kc, c0:c0 + ncols],
                            start=(kc == 0), stop=(kc == 7)), reads=[h_d, d_wt], writes=[pd])
                    evac(pt, pd, ncols, kind, scr[name][r0:r0 + 128, dc0:dc0 + ncols])
            fm = []
            for j in range(8):
                fm.append((1792 + j * 128, "bf16", "qk_fm", j * 128))
            for j in range(16):
                fm.append((3328 + j * 128, "sig", "sg_fm", j * 128))
            for (c0, kind, name, r0) in fm:
                pt, pd = pmm.next()
                for kc in range(8):
                    K.pe.op(lambda e, pt=pt, kc=kc, c0=c0, h_t=h_t: e.matmul(
                        pt[:, :], wt[:, kc, c0:c0 + 128], h_t[:, kc, :], start=(kc == 0), stop=(kc == 7)),
                        reads=[h_d, d_wt], writes=[pd])
                evac(pt, pd, 512, kind, scr[name][r0:r0 + 128, tsl])


def dap(apobj, offset, dims):
    return bass.AP(tensor=apobj.tensor, offset=offset, ap=[list(d) for d in dims])


def bcast_load(K, eng, dst, src_row_ap, n, dep):
    eng.dma(lambda e: e.dma_start(out=dst, in_=src_row_ap.broadcast_to([128, n])), writes=[dep])


def phase2_prep(K, cfg, io, scr):
    T, S, NB = cfg["T"], cfg["S"], cfg["NB"]
    NTT = T // 128
    with ExitStack() as es:
        identb = K.sb([128, 128], BF16, "identb", es)
        identf = K.sb([128, 128], F32, "identf", es)
        d_c = Dep()
        K.sp.dma(lambda e: e.dma_start(out=identb[:], in_=io["ident_bf"][:, :]), adds=[d_c])
        K.sp.dma(lambda e: e.dma_start(out=identf[:], in_=io["ident_f"][:, :]), adds=[d_c])
        MU = K.sb([128, SHIFT_COLS], F32, "MU", es)
        PR = K.sb([128, 5, 512], F32, "PR", es)
        K.sp.dma(lambda e: e.dma_start(out=MU[:], in_=io["shift_mu"][0:1, :].broadcast_to([128, SHIFT_COLS])), adds=[d_c])
        for j, nm in enumerate(["w0", "a0", "k_k", "k_a"]):
            K.pool.dma(lambda e, j=j, nm=nm: e.dma_start(out=PR[:, j, :], in_=io[nm][0:1, :].broadcast_to([128, 512])),
                       adds=[d_c])
        K.pool.dma(lambda e: e.dma_start(out=PR[:, 4, :], in_=io["r_k"].rearrange("o h k -> o (h k)").broadcast_to([128, 512])),
                   adds=[d_c])
        cst = K.sb([128, 2], F32, "cst", es)
        K.dve.op(lambda e: e.memset(cst[:, 0:1], 1.0), adds=[d_c])
        K.dve.op(lambda e: e.memset(cst[:, 1:2], -0.5), adds=[d_c])
        wst = K.sb([128, 3, 512], F32, "lwst", es)
        d_wst = Dep()
        K.dve.op(lambda e: e.memset(wst[:], 0.0), writes=[d_wst])
        K.sp.dma(lambda e: e.dma_start(out=wst[0:64, 0, :], in_=io["w2"][0, :, :]), reads=[d_wst], adds=[d_wst])
        K.sp.dma(lambda e: e.dma_start(out=wst[64:128, 1, :], in_=io["a2"][0, :, :]), reads=[d_wst], adds=[d_wst])
        K.sp.dma(lambda e: e.dma_start(out=wst[:, 2, :], in_=io["g2"][0, :, :]), reads=[d_wst], adds=[d_wst])
        LW = K.sb([128, 3, 512], BF16, "LW", es)
        K.dve.op(lambda e: e.tensor_copy(out=LW[:], in_=wst[:]), reads=[d_wst], adds=[d_c])

        Zr = Rot(K, 2, [128, SHIFT_COLS], F32, "Z", es=es)
        Zpr = Rot(K, 2, [128, SHIFT_COLS], F32, "Zp", es=es)
        ZSr = Rot(K, 2, [128, SHIFT_COLS], F32, "ZS", es=es)
        OUTr = Rot(K, 2, [128, 5, 512], F32, "OUT", es=es)
        Er = Rot(K, 2, [128, 192], F32, "E", es=es)
        Lr = Rot(K, 2, [128, 256], BF16, "L", es=es)
        LTr = Rot(K, 2, [128, 2, 128], BF16, "LT", es=es)
        Ur = Rot(K, 2, [128, 512], F32, "U", es=es)
        UAr = Rot(K, 2, [128, 512], F32, "UA", es=es)
        KKr = Rot(K, 2, [128, 512], F32, "KKt", es=es)
        SQr = Rot(K, 2, [128, 512], F32, "SQ", es=es)
        T1r = Rot(K, 2, [128, 512], F32, "T1", es=es)
        T2r = Rot(K, 2, [128, 512], F32, "T2", es=es)
        S8r = Rot(K, 2, [128, 4, 8], F32, "S8", es=es)
        VTr = Rot(K, 2, [128, 4, 128], F32, "VT", es=es)
        GTr = Rot(K, 2, [128, 4, 128], F32, "GT", es=es)
        CTr = Rot(K, 2, [8, 128], F32, "CT", es=es)
        PT = Rot(K, 1, [128, 2, 128], BF16, "PT", psum=True, es=es)
        PW = Rot(K, 1, [128, 512], F32, "PW", psum=True, es=es)
        PA = Rot(K, 1, [128, 512], F32, "PA", psum=True, es=es)
        PG = Rot(K, 1, [128, 4, 128], F32, "PG", psum=True, es=es)
        PV = Rot(K, 1, [128, 4, 128], F32, "PV", psum=True, es=es)
        PC = Rot(K, 1, [8, 128], F32, "PC", psum=True, es=es)
        chunked = cfg.get("chunked", True)
        if chunked:
            PL = Rot(K, 1, [128, 512], F32, "PL", psum=True, es=es)
            TRI = K.sb([128, 128], F32, "TRI", es)
            K.sp.dma(lambda e: e.dma_start(out=TRI[:], in_=io["tri"][:, :]), adds=[d_c])
            LWr = Rot(K, 2, [128, 512], F32, "LWt", es=es)
            ELr = Rot(K, 2, [128, 3, 512], F32, "EL", es=es)
            ABr = Rot(K, 2, [128, 5, 512], BF16, "AB", es=es)
        dq = [0]

        def ldq():
            dq[0] += 1
            return K.sp if dq[0] % 2 == 0 else K.pool

        def tile_gen(ti):
            t0 = ti * 128
            first = (t0 % S == 0)
            Z, dZ = Zr.next()
            Zp, dZp = Zpr.next()
            ZS, dZS = ZSr.next()
            OUT, dO = OUTr.next()
            ldq().dma(lambda e, Z=Z, t0=t0: e.dma_start(out=Z[:], in_=scr["zs_tm"][t0:t0 + 128, :]),
                      reads=[scr["d_p1"]], writes=[dZ])
            if first:
                K.pool.op(lambda e, Zp=Zp: e.memset(Zp[0:32, :], 0.0), writes=[dZp])
                ldq().dma(lambda e, Zp=Zp, t0=t0: e.dma_start(out=Zp[1:128, :], in_=scr["zs_tm"][t0:t0 + 127, :]),
                          reads=[scr["d_p1"], dZp], adds=[dZp])
            else:
                ldq().dma(lambda e, Zp=Zp, t0=t0: e.dma_start(out=Zp[:], in_=scr["zs_tm"][t0 - 1:t0 + 127, :]),
                          reads=[scr["d_p1"]], writes=[dZp])
            CS = 1216
            dZSa, dZSb = Dep(), Dep()
            K.dve.op(lambda e, Z=Z, Zp=Zp, ZS=ZS: e.tensor_tensor(out=ZS[:, :CS], in0=Zp[:, :CS], in1=Z[:, :CS], op=ALU.subtract),
                     reads=[dZ, dZp], writes=[dZS])
            K.pool.op(lambda e, Z=Z, Zp=Zp, ZS=ZS: e.tensor_tensor(out=ZS[:, CS:], in0=Zp[:, CS:], in1=Z[:, CS:], op=ALU.subtract),
                      reads=[dZ, dZp, dZS], writes=[dZSb])
            K.dve.op(lambda e, ZS=ZS: e.tensor_tensor(out=ZS[:, :CS], in0=ZS[:, :CS], in1=MU[:, :CS], op=ALU.mult),
                     reads=[d_c, dZS], writes=[dZSa])
            K.pool.op(lambda e, ZS=ZS: e.tensor_tensor(out=ZS[:, CS:], in0=ZS[:, CS:], in1=MU[:, CS:], op=ALU.mult),
                      reads=[d_c], writes=[dZSb])
            K.dve.op(lambda e, Z=Z, ZS=ZS: e.tensor_tensor(out=ZS[:, :CS], in0=ZS[:, :CS], in1=Z[:, :CS], op=ALU.add),
                     reads=[dZ], writes=[dZSa])
            K.pool.op(lambda e, Z=Z, ZS=ZS: e.tensor_tensor(out=ZS[:, CS:], in0=ZS[:, CS:], in1=Z[:, CS:], op=ALU.add),
                      reads=[dZ], writes=[dZSb])
            K.dve.op(lambda e, ZS=ZS: e.tensor_copy(out=ZS[:, 0:1], in_=ZS[:, 0:1]), reads=[dZSa, dZSb], writes=[dZS])
            yield
            r_ap = ZS[:, 0:512]
            k_ap = ZS[:, 512:1024]
            K.act.op(lambda e, OUT=OUT, ZS=ZS: e.activation(out=OUT[:, 4, :], in_=ZS[:, 0:512], func=AF.Copy),
                     reads=[dZS], writes=[dO])
            yield
            E, dE = Er.next()
            L, dL = Lr.next()
            K.act.op(lambda e, E=E, ZS=ZS: e.activation(out=E[:, 0:64], in_=ZS[:, 1536:1600], func=AF.Exp, scale=-2.0),
                     reads=[dZS], writes=[dE])
            K.act.op(lambda e, E=E, ZS=ZS: e.activation(out=E[:, 64:192], in_=ZS[:, 1664:1792], func=AF.Exp, scale=-1.0),
                     reads=[dZS], adds=[dE])
            K.act.op(lambda e, E=E: e.activation(out=E[:], in_=E[:], func=AF.Ln, bias=cst[:, 0:1]), reads=[d_c], writes=[dE])
            K.act.op(lambda e, E=E: e.activation(out=E[:], in_=E[:], func=AF.Exp, scale=-1.0), writes=[dE])
            yield
            K.dve.op(lambda e, E=E, L=L: e.tensor_scalar(out=L[:, 0:64], in0=E[:, 0:64], scalar1=2.0, scalar2=-1.0,
                                                        op0=ALU.mult, op1=ALU.add), reads=[dE], writes=[dL])
            K.act.op(lambda e, L=L, ZS=ZS: e.activation(out=L[:, 64:128], in_=ZS[:, 1600:1664], func=AF.Copy),
                     reads=[dZS, dL], adds=[dL])
            K.act.op(lambda e, L=L, E=E: e.activation(out=L[:, 128:256], in_=E[:, 64:192], func=AF.Copy),
                     reads=[dE, dL], adds=[dL])
            yield
            pt, dpt = PT.next()
            K.pe.op(lambda e, pt=pt, L=L: e.transpose(out=pt[:, 0, :], in_=L[:, 0:128], identity=identb[:]),
                    reads=[dL, d_c], writes=[dpt])
            K.pe.op(lambda e, pt=pt, L=L: e.transpose(out=pt[:, 1, :], in_=L[:, 128:256], identity=identb[:]),
                    reads=[dL, d_c], adds=[dpt])
            LT, dLT = LTr.next()
            K.act.op(lambda e, pt=pt, LT=LT: e.activation(out=LT[:], in_=pt[:], func=AF.Copy), reads=[dpt], writes=[dLT])
            yield "pre_pw"
            pw, dpw = PW.next()
            pa, dpa = PA.next()
            pg, dpg = PG.next()
            K.pe.op(lambda e, pw=pw, LT=LT: e.matmul(pw[:], LT[:, 0, :], LW[:, 0, :], start=True, stop=True),
                    reads=[dLT, d_c], writes=[dpw])
            K.pe.op(lambda e, pa=pa, LT=LT: e.matmul(pa[:], LT[:, 0, :], LW[:, 1, :], start=True, stop=True),
                    reads=[dLT, d_c], writes=[dpa])
            for j in range(4):
                K.pe.op(lambda e, pg=pg, LT=LT, j=j: e.matmul(pg[:, j, :], LW[:, 2, j * 128:(j + 1) * 128], LT[:, 1, :],
                                                             start=True, stop=True),
                        reads=[dLT, d_c], writes=[dpg] if j == 0 else [], adds=[] if j == 0 else [dpg])
            GT, dGT = GTr.next()
            K.act.op(lambda e, pg=pg, GT=GT: e.activation(out=GT[:], in_=pg[:], func=AF.Copy), reads=[dpg], writes=[dGT])
            K.sp.dma(lambda e, GT=GT, t0=t0: e.dma_start(
                out=dap(scr["g_fm"], t0, [[T, 128], [128 * T, 4], [1, 128]]), in_=GT[:]),
                reads=[dGT], adds=[scr["d_p2a"]])
            yield
            U, dU = Ur.next()
            K.dve.op(lambda e, U=U, pw=pw: e.tensor_tensor(out=U[:], in0=pw[:], in1=PR[:, 0, :], op=ALU.add),
                     reads=[dpw, d_c], writes=[dU])
            yield
            K.act.op(lambda e, U=U: e.activation(out=U[:], in_=U[:], func=AF.Exp, scale=-1.0), writes=[dU])
            K.act.op(lambda e, U=U: e.activation(out=U[:], in_=U[:], func=AF.Ln, bias=cst[:, 0:1]), reads=[d_c], writes=[dU])
            yield
            K.act.op(lambda e, U=U: e.activation(out=U[:], in_=U[:], func=AF.Exp, scale=-1.0, bias=cst[:, 1:2]),
                     reads=[d_c], writes=[dU])
            K.act.op(lambda e, U=U, OUT=OUT: e.activation(out=OUT[:, 0, :], in_=U[:], func=AF.Exp, scale=-1.0),
                     reads=[dU], adds=[dO])
            yield
            UA, dUA = UAr.next()
            K.dve.op(lambda e, UA=UA, pa=pa: e.tensor_tensor(out=UA[:], in0=pa[:], in1=PR[:, 1, :], op=ALU.add),
                     reads=[dpa, d_c], writes=[dUA])
            yield
            K.act.op(lambda e, UA=UA: e.activation(out=UA[:], in_=UA[:], func=AF.Exp, scale=-1.0), writes=[dUA])
            K.act.op(lambda e, UA=UA: e.activation(out=UA[:], in_=UA[:], func=AF.Ln, bias=cst[:, 0:1]), reads=[d_c], writes=[dUA])
            K.act.op(lambda e, UA=UA: e.activation(out=UA[:], in_=UA[:], func=AF.Exp, scale=-1.0), writes=[dUA])
            yield "post_a"
            KKt, dKK = KKr.next()
            SQ, dSQ = SQr.next()
            S8, dS8 = S8r.next()
            K.dve.op(lambda e, KKt=KKt, ZS=ZS: e.tensor_tensor(out=KKt[:], in0=ZS[:, 512:1024], in1=PR[:, 2, :], op=ALU.mult),
                     reads=[dZS, d_c], writes=[dKK])
            K.pool.op(lambda e, KKt=KKt, SQ=SQ: e.tensor_tensor(out=SQ[:], in0=KKt[:], in1=KKt[:], op=ALU.mult),
                      reads=[dKK], writes=[dSQ])
            yield
            K.dve.op(lambda e, SQ=SQ, S8=S8: e.tensor_reduce(out=S8[:, 0, :], in_=SQ[:].rearrange("p (h k) -> p h k", k=64),
                                                            axis=AX.X, op=ALU.add), reads=[dSQ], writes=[dS8])
            K.dve.op(lambda e, S8=S8: e.tensor_scalar(out=S8[:, 0, :], in0=S8[:, 0, :], scalar1=1e-24, scalar2=None,
                                                      op0=ALU.max), writes=[dS8])
            K.act.op(lambda e, S8=S8: e.activation(out=S8[:, 1, :], in_=S8[:, 0, :], func=AF.Ln), writes=[dS8])
            K.act.op(lambda e, S8=S8: e.activation(out=S8[:, 2, :], in_=S8[:, 1, :], func=AF.Exp, scale=-0.5), writes=[dS8])
            yield
            K.dve.op(lambda e, KKt=KKt, S8=S8, OUT=OUT: e.tensor_tensor(
                out=OUT[:, 1, :].rearrange("p (h k) -> p h k", k=64), in0=KKt[:].rearrange("p (h k) -> p h k", k=64),
                in1=S8[:, 2, :].unsqueeze(2).broadcast_to([128, 8, 64]), op=ALU.mult),
                reads=[dKK, dS8, dO], adds=[dO])
            K.dve.op(lambda e, OUT=OUT, UA=UA: e.scalar_tensor_tensor(
                out=OUT[:, 2, :], in0=OUT[:, 1, :], scalar=-1.0, in1=UA[:], op0=ALU.mult, op1=ALU.mult),
                reads=[dUA, dO], adds=[dO])
            yield
            T1, dT1 = T1r.next()
            K.dve.op(lambda e, T1=T1, UA=UA: e.scalar_tensor_tensor(
                out=T1[:], in0=UA[:], scalar=-1.0, in1=PR[:, 3, :], op0=ALU.add, op1=ALU.mult),
                reads=[dUA, d_c], writes=[dT1])
            K.dve.op(lambda e, T1=T1, OUT=OUT, ZS=ZS: e.scalar_tensor_tensor(
                out=OUT[:, 3, :], in0=T1[:], scalar=1.0, in1=ZS[:, 512:1024], op0=ALU.add, op1=ALU.mult),
                reads=[dT1, dZS, dO], adds=[dO])
            T2, dT2 = T2r.next()
            K.pool.op(lambda e, T2=T2, OUT=OUT, ZS=ZS: e.tensor_tensor(out=T2[:], in0=OUT[:, 3, :], in1=ZS[:, 0:512], op=ALU.mult),
                      reads=[dO, dZS], writes=[dT2])
            K.pool.op(lambda e, T2=T2: e.tensor_tensor(out=T2[:], in0=T2[:], in1=PR[:, 4, :], op=ALU.mult),
                      reads=[d_c], writes=[dT2])
            yield
            K.dve.op(lambda e, T2=T2, S8=S8: e.tensor_reduce(out=S8[:, 3, :], in_=T2[:].rearrange("p (h k) -> p h k", k=64),
                                                            axis=AX.X, op=ALU.add), reads=[dT2], writes=[dS8])
            yield
            pv, dpv = PV.next()
            pc, dpc = PC.next()
            for j in range(4):
                K.pe.op(lambda e, pv=pv, ZS=ZS, j=j: e.transpose(out=pv[:, j, :], in_=ZS[:, 1024 + j * 128:1024 + (j + 1) * 128],
                                                                identity=identf[:]),
                        reads=[dZS, d_c], writes=[dpv] if j == 0 else [], adds=[] if j == 0 else [dpv])
            K.pe.op(lambda e, pc=pc, S8=S8: e.transpose(out=pc[:, :], in_=S8[:, 3, :], identity=identf[:]),
                    reads=[dS8, d_c], writes=[dpc])
            VT, dVT = VTr.next()
            CT, dCT = CTr.next()
            K.act.op(lambda e, pv=pv, VT=VT: e.activation(out=VT[:], in_=pv[:], func=AF.Copy), reads=[dpv], writes=[dVT])
            K.act.op(lambda e, pc=pc, CT=CT: e.activation(out=CT[:], in_=pc[:], func=AF.Copy), reads=[dpc], writes=[dCT])
            K.sp.dma(lambda e, VT=VT, t0=t0: e.dma_start(
                out=dap(scr["v_fm"], t0, [[T, 128], [128 * T, 4], [1, 128]]), in_=VT[:]),
                reads=[dVT], adds=[scr["d_p2a"]])
            K.sp.dma(lambda e, CT=CT, t0=t0: e.dma_start(out=scr["coef_fm"][:, t0:t0 + 128], in_=CT[:]),
                     reads=[dCT], adds=[scr["d_p2a"]])
            yield
            if not chunked:
                K.pool.dma(lambda e, OUT=OUT, t0=t0: e.dma_start(out=scr["rw_tm"][t0:t0 + 128, :, :], in_=OUT[:]),
                           reads=[dO], adds=[scr["d_p2a"]])
            else:
                LWt, dLW = LWr.next()
                K.dve.op(lambda e, LWt=LWt, U=U: e.tensor_scalar(out=LWt[:], in0=U[:], scalar1=-1.0, scalar2=None, op0=ALU.mult),
                         reads=[dU], writes=[dLW])
                pl, dpl = PL.next()
                K.pe.op(lambda e, pl=pl, LWt=LWt: e.matmul(pl[:], TRI[:], LWt[:], start=True, stop=True),
                        reads=[dLW, d_c], writes=[dpl])
                EL, dEL = ELr.next()
                K.act.op(lambda e, EL=EL, pl=pl: e.activation(out=EL[:, 0, :], in_=pl[:], func=AF.Exp), reads=[dpl], writes=[dEL])
                K.act.op(lambda e, EL=EL, pl=pl: e.activation(out=EL[:, 1, :], in_=pl[:], func=AF.Exp, scale=-1.0),
                         reads=[dpl], adds=[dEL])
                K.dve.op(lambda e, EL=EL, pl=pl, U=U: e.tensor_tensor(out=EL[:, 2, :], in0=pl[:], in1=U[:], op=ALU.add),
                         reads=[dpl, dU, dEL], adds=[dEL])
                K.act.op(lambda e, EL=EL: e.activation(out=EL[:, 2, :], in_=EL[:, 2, :], func=AF.Exp), reads=[dEL], adds=[dEL])
                AB, dAB = ABr.next()
                K.dve.op(lambda e, AB=AB, OUT=OUT, EL=EL: e.tensor_tensor(out=AB[:, 0, :], in0=OUT[:, 1, :], in1=EL[:, 2, :], op=ALU.mult),
                         reads=[dO, dEL], writes=[dAB])
                K.dve.op(lambda e, AB=AB, OUT=OUT, EL=EL: e.scalar_tensor_tensor(
                    out=AB[:, 1, :], in0=OUT[:, 2, :], scalar=-1.0, in1=EL[:, 1, :], op0=ALU.mult, op1=ALU.mult),
                    reads=[dO, dEL, dAB], adds=[dAB])
                K.pool.op(lambda e, AB=AB, OUT=OUT, EL=EL: e.tensor_tensor(out=AB[:, 2, :], in0=OUT[:, 3, :], in1=EL[:, 1, :], op=ALU.mult),
                          reads=[dO, dEL, dAB], adds=[dAB])
                K.pool.op(lambda e, AB=AB, OUT=OUT, EL=EL: e.tensor_tensor(out=AB[:, 3, :], in0=OUT[:, 4, :], in1=EL[:, 0, :], op=ALU.mult),
                          reads=[dO, dEL, dAB], adds=[dAB])
                K.act.op(lambda e, AB=AB, ZS=ZS: e.activation(out=AB[:, 4, :], in_=ZS[:, 1024:1536], func=AF.Copy),
                         reads=[dZS, dAB], adds=[dAB])
                K.pool.dma(lambda e, AB=AB, t0=t0: e.dma_start(out=scr["ab_tm"][t0:t0 + 128, :, :], in_=AB[:]),
                           reads=[dAB], adds=[scr["d_p2a"]])
                K.sp.dma(lambda e, LWt=LWt, t0=t0: e.dma_start(out=scr["lw_tm"][t0:t0 + 128, :], in_=LWt[:]),
                         reads=[dLW], adds=[scr["d_p2a"]])

        active = []
        nxt = 0
        while nxt < NTT or active:
            if len(active) < 2 and nxt < NTT and (not active or active[0][2] or active[0][3] >= 2):
                active.append([tile_gen(nxt), None, False, 0])
                nxt += 1
            for idx, a_ in enumerate(list(active)):
                if a_[1] == "pre_pw" and idx > 0 and not active[0][2]:
                    continue
                try:
                    tok = next(a_[0])
                    a_[1] = tok
                    a_[3] += 1
                    if tok == "post_a":
                        a_[2] = True
                except StopIteration:
                    active.remove(a_)


def phase2_scan(K, cfg, io, scr):
    T, S, NB = cfg["T"], cfg["S"], cfg["NB"]
    NBH = 2 if NB >= 2 else 1
    NBL = NB // NBH
    NP = 64 * NBH
    TS = 2
    TC = 128
    RW = 2560
    with ExitStack() as es:
        St = K.sb([128, NBL, 8, 64], F32, "St", es)
        dS = Dep()
        TMP = K.sb([128, NBL, 8, 64], F32, "TMP", es)
        dT = Dep()
        SA = K.sb([128, NBL, 8], F32, "SA", es)
        dSA = Dep()
        T2r = Rot(K, 2, [128, NBL, 8, 64], F32, "TMP2", es=es)
        T3r = Rot(K, 2, [128, NBL, 8, 64], F32, "TMP3", es=es)
        BCr = Rot(K, 3, [128, TS, NBL, 5, 8, 64], F32, "BC", es=es)
        Vr = Rot(K, 2, [128, NBL, 8, TC], F32, "Vf", es=es)
        Yr = Rot(K, 2, [128, NBL, 8, TC], F32, "Yf", es=es)
        K.dve.op(lambda e: e.memset(St[:], 0.0), writes=[dS])
        qi = [0]

        def q():
            qi[0] += 1
            return K.sp if qi[0] % 2 == 0 else K.act

        def load_bc(ci):
            t = ci * TS
            BC, dBC = BCr.next()
            first = True
            for bhi in range(NBH):
                for blo in range(NBL):
                    src = dap(scr["rw_tm"], ((bhi * NBL + blo) * S + t) * RW, [[0, 64], [RW, TS], [1, RW]])
                    dst = BC[bhi * 64:(bhi + 1) * 64, :, blo].rearrange("p t j h k -> p t (j h k)")
                    q().dma(lambda e, src=src, dst=dst: e.dma_start(out=dst, in_=src), reads=[scr["d_p2a"]],
                            writes=[dBC] if first else [], adds=[] if first else [dBC])
                    first = False
            return BC, dBC

        def vy_ap(name, bhi, blo, t):
            return dap(scr[name], (bhi * NBL + blo) * S + t, [[T, 64], [64 * T, 8], [1, TC]])

        def load_v(ni):
            Vf, dV = Vr.next()
            first = True
            for bhi in range(NBH):
                for blo in range(NBL):
                    src = vy_ap("v_fm", bhi, blo, ni * TC)
                    dst = Vf[bhi * 64:(bhi + 1) * 64, blo]
                    q().dma(lambda e, src=src, dst=dst: e.dma_start(out=dst, in_=src), reads=[scr["d_p2a"]],
                            writes=[dV] if first else [], adds=[] if first else [dV])
                    first = False
            return Vf, dV

        nch = S // TS
        bcs = {}
        bcs[0] = load_bc(0)
        if nch > 1:
            bcs[1] = load_bc(1)
        vs = {0: load_v(0)}
        P = slice(0, NP)
        for t in range(S):
            ci, ts = divmod(t, TS)
            ni, tt = divmod(t, TC)
            if ts == 0 and ci + 2 < nch:
                bcs[ci + 2] = load_bc(ci + 2)
            if tt == 0:
                if (ni + 1) * TC < S:
                    vs[ni + 1] = load_v(ni + 1)
                Yf, dY = Yr.next()
            BC, dBC = bcs[ci]
            Vf, dV = vs[ni]
            W_ = BC[P, ts, :, 0]
            KN = BC[P, ts, :, 1]
            KA = BC[P, ts, :, 2]
            KP = BC[P, ts, :, 3]
            R_ = BC[P, ts, :, 4]
            shp = [NP, NBL, 8, 64]
            K.dve.op(lambda e, KN=KN: e.tensor_tensor(out=TMP[P], in0=St[P], in1=KN, op=ALU.mult),
                     reads=[dS, dBC], writes=[dT])
            K.dve.op(lambda e: e.tensor_reduce(out=SA[P], in_=TMP[P], axis=AX.X, op=ALU.add), reads=[dT], writes=[dSA])
            K.dve.op(lambda e, W_=W_: e.tensor_tensor(out=St[P], in0=St[P], in1=W_, op=ALU.mult),
                     reads=[dBC], writes=[dS])
            K.dve.op(lambda e, KA=KA: e.tensor_tensor(out=TMP[P], in0=KA, in1=SA[P].unsqueeze(3).broadcast_to(shp),
                                                     op=ALU.mult), reads=[dBC, dSA], writes=[dT])
            K.dve.op(lambda e: e.tensor_tensor(out=St[P], in0=St[P], in1=TMP[P], op=ALU.add), reads=[dT], writes=[dS])
            T2, dT2 = T2r.next()
            K.pool.op(lambda e, KP=KP, T2=T2, Vf=Vf, tt=tt: e.tensor_tensor(
                out=T2[P], in0=KP, in1=Vf[P, :, :, tt:tt + 1].broadcast_to(shp), op=ALU.mult),
                reads=[dBC, dV], writes=[dT2])
            K.dve.op(lambda e, T2=T2: e.tensor_tensor(out=St[P], in0=St[P], in1=T2[P], op=ALU.add),
                     reads=[dT2], writes=[dS])
            T3, dT3 = T3r.next()
            K.pool.op(lambda e, T3=T3, R_=R_: e.tensor_tensor(out=T3[P], in0=St[P], in1=R_, op=ALU.mult),
                      reads=[dS, dBC], writes=[dT3])
            K.dve.op(lambda e, T3=T3, Yf=Yf, tt=tt: e.tensor_reduce(out=Yf[P, :, :, tt], in_=T3[P], axis=AX.X, op=ALU.add),
                      reads=[dT3], writes=[dY] if tt == 0 else [], adds=[] if tt == 0 else [dY])
            if tt == TC - 1:
                for bhi in range(NBH):
                    for blo in range(NBL):
                        dst = vy_ap("y_fm", bhi, blo, ni * TC)
                        srcp = Yf[bhi * 64:(bhi + 1) * 64, blo]
                        K.sp.dma(lambda e, dst=dst, srcp=srcp: e.dma_start(out=dst, in_=srcp), reads=[dY],
                                 adds=[scr["d_p2b"]])


def phase2_chunk(K, cfg, io, scr):
    T, S, NB = cfg["T"], cfg["S"], cfg["NB"]
    C = 64
    NCH = S // C
    with ExitStack() as es:
        d_c = Dep()
        id64 = K.sb([64, 64], BF16, "c_id64", es)
        K.sp.dma(lambda e: e.dma_start(out=id64[:], in_=io["ident64"][:, :]), adds=[d_c])
        MK = K.sb([64, 3, 64], F32, "c_MK", es)
        K.sp.dma(lambda e: e.dma_start(out=MK[:], in_=io["masks"][:, :, :]), adds=[d_c])
        ONES = K.sb([64, 1], F32, "c_ones", es)
        K.sp.dma(lambda e: e.dma_start(out=ONES[:], in_=io["ones64"][:, :]), adds=[d_c])
        IDF = K.sb([64, 8, 64], F32, "c_IDF", es)
        K.sp.dma(lambda e: e.dma_start(out=IDF[:], in_=io["ident_f"][0:64, 0:64].unsqueeze(1).broadcast_to([64, 8, 64])), adds=[d_c])
        ST = [K.sb([64, 8, 64], F32, "c_S%d" % b, es) for b in range(NB)]
        STb = [K.sb([64, 8, 64], BF16, "c_Sb%d" % b, es) for b in range(NB)]
        dST = [Dep() for _ in range(NB)]
        dSTb = [Dep() for _ in range(NB)]
        for b in range(NB):
            K.dve.op(lambda e, b=b: e.memset(ST[b][:], 0.0), writes=[dST[b]])
            K.pool.op(lambda e, b=b: e.memset(STb[b][:], 0.0), writes=[dSTb[b]])
        TMr = Rot(K, 3, [64, 5, 512], BF16, "c_TM", es=es)
        LWr = Rot(K, 3, [64, 512], F32, "c_LW", es=es)
        FMr = Rot(K, 2, [64, 4, 8, 64], BF16, "c_FM", es=es)
        PCr = Rot(K, 2, [64, 8], F32, "c_PC", es=es)
        Nr = Rot(K, 3, [64, 8, 64], BF16, "c_N", es=es)
        NTr = Rot(K, 3, [64, 8, 64], BF16, "c_NT", es=es)
        MTr = Rot(K, 3, [64, 8, 64], BF16, "c_MT", es=es)
        MTfr = Rot(K, 2, [64, 8, 64], F32, "c_MTf", es=es)
        NAKr = Rot(K, 2, [64, 8, 64], BF16, "c_NAK", es=es)
        MRBr = Rot(K, 2, [64, 8, 64], BF16, "c_MRB", es=es)
        MRKr = Rot(K, 2, [64, 8, 64], BF16, "c_MRK", es=es)
        Xr = Rot(K, 2, [64, 8, 64], BF16, "c_X", es=es)
        NUr = Rot(K, 2, [64, 8, 64], BF16, "c_NU", es=es)
        Yr = Rot(K, 2, [64, 8, 64], F32, "c_Y", es=es)
        TSr = Rot(K, 2, [64, 8, 64], F32, "c_TS", es=es)
        PTf = Rot(K, 1, [64, 4, 8, 64], BF16, "c_PTf", psum=True, es=es)
        PA = Rot(K, 4, [64, 8, 64], F32, "c_PA", psum=True, es=es)
        PPC = Rot(K, 1, [64, 8], F32, "c_PPC", psum=True, es=es)
        ce = [0]

        def evac_copy(dst_ap, src_ap, reads, writes=(), adds=(), scale=None):
            ce[0] += 1
            if scale is not None or ce[0] % 2 == 0:
                if scale is None:
                    K.act.op(lambda e: e.activation(out=dst_ap, in_=src_ap, func=AF.Copy), reads=reads, writes=writes, adds=adds)
                else:
                    K.act.op(lambda e: e.activation(out=dst_ap, in_=src_ap, func=AF.Copy, scale=scale), reads=reads, writes=writes, adds=adds)
            else:
                K.dve.op(lambda e: e.tensor_copy(out=dst_ap, in_=src_ap), reads=reads, writes=writes, adds=adds)

        def mm8(pt, dpt, lhs_fn, rhs_fn, reads, first=True, last=True, wr=True):
            mmN(pt, dpt, [(lhs_fn, rhs_fn)], reads)

        def mmN(pt, dpt, terms, reads):
            n = len(terms)
            for h in range(8):
                for i, (lf, rf) in enumerate(terms):
                    K.pe.op(lambda e, h=h, lf=lf, rf=rf, i=i: e.matmul(pt[:, h, :], lf(h), rf(h), start=(i == 0), stop=(i == n - 1)),
                            reads=reads, writes=[dpt] if (h == 0 and i == 0) else [], adds=[] if (h == 0 and i == 0) else [dpt])

        q = [0]

        def dq():
            q[0] += 1
            return K.sp if q[0] % 2 == 0 else K.pool

        for ci in range(NCH):
            for b in range(NB):
                t0 = b * S + ci * C
                TM, dTM = TMr.next()
                LW, dLW = LWr.next()
                dq().dma(lambda e, TM=TM, t0=t0: e.dma_start(out=TM[:], in_=scr["ab_tm"][t0:t0 + C, :, :]),
                         reads=[scr["d_p2a"]], writes=[dTM])
                dq().dma(lambda e, LW=LW, t0=t0: e.dma_start(out=LW[:], in_=scr["lw_tm"][t0:t0 + C, :]),
                         reads=[scr["d_p2a"]], writes=[dLW])
                ptf, dptf = PTf.next()
                first = True
                for j in range(4):
                    for h in range(8):
                        K.pe.op(lambda e, ptf=ptf, TM=TM, j=j, h=h: e.transpose(
                            out=ptf[:, j, h, :], in_=TM[:, j, h * 64:(h + 1) * 64], identity=id64[:]),
                            reads=[dTM, d_c], writes=[dptf] if first else [], adds=[] if first else [dptf])
                        first = False
                FM, dFM = FMr.next()
                K.act.op(lambda e, FM=FM, ptf=ptf: e.activation(out=FM[:, 0:2], in_=ptf[:, 0:2], func=AF.Copy), reads=[dptf], writes=[dFM])
                K.dve.op(lambda e, FM=FM, ptf=ptf: e.tensor_copy(out=FM[:, 2:4], in_=ptf[:, 2:4]), reads=[dptf, dFM], adds=[dFM])
                Af = lambda h, FM=FM: FM[:, 0, h, :]
                Bf = lambda h, FM=FM: FM[:, 1, h, :]
                Kf = lambda h, FM=FM: FM[:, 2, h, :]
                Rf = lambda h, FM=FM: FM[:, 3, h, :]
                Vt = lambda h, TM=TM: TM[:, 4, h * 64:(h + 1) * 64]
                Bt = lambda h, TM=TM: TM[:, 1, h * 64:(h + 1) * 64]
                Kt = lambda h, TM=TM: TM[:, 2, h * 64:(h + 1) * 64]
                ppc, dppc = PPC.next()
                for h in range(8):
                    K.pe.op(lambda e, ppc=ppc, LW=LW, h=h: e.matmul(ppc[:, h:h + 1], LW[:, h * 64:(h + 1) * 64], ONES[:], start=True, stop=True),
                            reads=[dLW, d_c], writes=[dppc] if h == 0 else [], adds=[] if h == 0 else [dppc])
                PCt, dPC = PCr.next()
                K.act.op(lambda e, PCt=PCt, ppc=ppc: e.activation(out=PCt[:], in_=ppc[:], func=AF.Exp), reads=[dppc], writes=[dPC])
                mbc = lambda i: MK[:, i, :].unsqueeze(1).broadcast_to([64, 8, 64])
                pa, dpa = PA.next()
                mm8(pa, dpa, Af, Bf, [dFM])
                N0, dN0 = Nr.next()
                K.dve.op(lambda e, N0=N0, pa=pa: e.tensor_tensor(out=N0[:], in0=pa[:], in1=mbc(0), op=ALU.mult), reads=[dpa, d_c], writes=[dN0])
                pa, dpa = PA.next()
                mm8(pa, dpa, Bf, Af, [dFM])
                NT0, dNT0 = NTr.next()
                MTf, dMTf = MTfr.next()
                K.dve.op(lambda e, NT0=NT0, pa=pa: e.tensor_tensor(out=NT0[:], in0=pa[:], in1=mbc(1), op=ALU.mult), reads=[dpa, d_c], writes=[dNT0])
                K.pool.op(lambda e, MTf=MTf, NT0=NT0: e.tensor_tensor(out=MTf[:], in0=IDF[:], in1=NT0[:], op=ALU.subtract),
                          reads=[dNT0, d_c], writes=[dMTf])
                MT, dMT = MTr.next()
                K.act.op(lambda e, MT=MT, MTf=MTf: e.activation(out=MT[:], in_=MTf[:], func=AF.Copy), reads=[dMTf], writes=[dMT])
                pa, dpa = PA.next()
                mm8(pa, dpa, Kf, Af, [dFM])
                NAK, dNAK = NAKr.next()
                K.dve.op(lambda e, NAK=NAK, pa=pa: e.tensor_tensor(out=NAK[:], in0=pa[:], in1=mbc(1), op=ALU.mult), reads=[dpa, d_c], writes=[dNAK])
                pa, dpa = PA.next()
                mm8(pa, dpa, Bf, Rf, [dFM])
                MRB, dMRB = MRBr.next()
                K.dve.op(lambda e, MRB=MRB, pa=pa: e.tensor_tensor(out=MRB[:], in0=pa[:], in1=mbc(2), op=ALU.mult), reads=[dpa, d_c], writes=[dMRB])
                pa, dpa = PA.next()
                mm8(pa, dpa, Kf, Rf, [dFM])
                MRK, dMRK = MRKr.next()
                K.dve.op(lambda e, MRK=MRK, pa=pa: e.tensor_tensor(out=MRK[:], in0=pa[:], in1=mbc(2), op=ALU.mult), reads=[dpa, d_c], writes=[dMRK])
                Np, dNp, NTp, dNTp = N0, dN0, NT0, dNT0
                for lvl in range(1, 6):
                    pa, dpa = PA.next()
                    mm8(pa, dpa, lambda h, NTp=NTp: NTp[:, h, :], lambda h, Np=Np: Np[:, h, :], [dNp, dNTp])
                    Nn, dNn = Nr.next()
                    evac_copy(Nn[:], pa[:], [dpa], writes=[dNn])
                    if lvl < 5:
                        pa2, dpa2 = PA.next()
                        mm8(pa2, dpa2, lambda h, Np=Np: Np[:, h, :], lambda h, NTp=NTp: NTp[:, h, :], [dNp, dNTp])
                        NTn, dNTn = NTr.next()
                        evac_copy(NTn[:], pa2[:], [dpa2], writes=[dNTn])
                    pa3, dpa3 = PA.next()
                    mm8(pa3, dpa3, lambda h, Nn=Nn: Nn[:, h, :], lambda h, MT=MT: MT[:, h, :], [dNn, dMT])
                    K.dve.op(lambda e, MTf=MTf, pa3=pa3: e.tensor_tensor(out=MTf[:], in0=MTf[:], in1=pa3[:], op=ALU.add),
                             reads=[dpa3], writes=[dMTf])
                    MT, dMT = MTr.next()
                    K.act.op(lambda e, MT=MT, MTf=MTf: e.activation(out=MT[:], in_=MTf[:], func=AF.Copy), reads=[dMTf], writes=[dMT])
                    Np, dNp = Nn, dNn
                    if lvl < 5:
                        NTp, dNTp = NTn, dNTn
                Sb = STb[b]
                pa, dpa = PA.next()
                mmN(pa, dpa, [(Af, lambda h, Sb=Sb: Sb[:, h, :]), (lambda h, NAK=NAK: NAK[:, h, :], Vt)], [dFM, dSTb[b], dNAK, dTM])
                X, dX = Xr.next()
                K.act.op(lambda e, X=X, pa=pa: e.activation(out=X[:], in_=pa[:], func=AF.Copy), reads=[dpa], writes=[dX])
                pa, dpa = PA.next()
                mm8(pa, dpa, lambda h, MT=MT: MT[:, h, :], lambda h, X=X: X[:, h, :], [dMT, dX])
                NU, dNU = NUr.next()
                K.act.op(lambda e, NU=NU, pa=pa: e.activation(out=NU[:], in_=pa[:], func=AF.Copy, scale=-1.0), reads=[dpa], writes=[dNU])
                pa, dpa = PA.next()
                mmN(pa, dpa, [(lambda h, Sb=Sb: Sb[:, h, :], Rf), (lambda h, NU=NU: NU[:, h, :], lambda h, MRB=MRB: MRB[:, h, :]),
                              (Vt, lambda h, MRK=MRK: MRK[:, h, :])], [dFM, dSTb[b], dNU, dMRB, dTM, dMRK])
                Y, dY = Yr.next()
                K.dve.op(lambda e, Y=Y, pa=pa: e.tensor_copy(out=Y[:], in_=pa[:]), reads=[dpa], writes=[dY])
                K.sp.dma(lambda e, Y=Y, t0=t0: e.dma_start(out=dap(scr["y_fm"], t0, [[T, 64], [64 * T, 8], [1, 64]]), in_=Y[:]),
                         reads=[dY], adds=[scr["d_p2b"]])
                pa, dpa = PA.next()
                mmN(pa, dpa, [(Bt, lambda h, NU=NU: NU[:, h, :]), (Kt, Vt)], [dTM, dNU])
                TS_, dTS = TSr.next()
                K.dve.op(lambda e, TS_=TS_, pa=pa, b=b: e.tensor_tensor(out=TS_[:], in0=pa[:], in1=ST[b][:], op=ALU.add),
                         reads=[dpa, dST[b]], writes=[dTS])
                K.dve.op(lambda e, TS_=TS_, PCt=PCt, b=b: e.tensor_tensor(
                    out=ST[b][:], in0=TS_[:], in1=PCt[:].unsqueeze(2).broadcast_to([64, 8, 64]), op=ALU.mult),
                    reads=[dTS, dPC], writes=[dST[b]])
                K.act.op(lambda e, b=b: e.activation(out=STb[b][:], in_=ST[b][:], func=AF.Copy), reads=[dST[b]], writes=[dSTb[b]])


def phase2_post(K, cfg, io, scr):
    T, S, NB = cfg["T"], cfg["S"], cfg["NB"]
    NT = T // 512
    with ExitStack() as es:
        BO = K.sb([128, 128], F32, "BO", es)
        d_c = Dep()
        K.sp.dma(lambda e: e.dma_start(out=BO[:], in_=io["blockones"][:, :]), adds=[d_c])
        LN = K.sb([128, 2, 4], F32, "LN", es)
        K.sp.dma(lambda e: e.dma_start(out=LN[:, 0, :], in_=io["lnx_w"].rearrange("o (j p) -> p (o j)", p=128)), adds=[d_c])
        K.sp.dma(lambda e: e.dma_start(out=LN[:, 1, :], in_=io["lnx_b"].rearrange("o (j p) -> p (o j)", p=128)), adds=[d_c])
        Yr = Rot(K, 2, [128, 512], F32, "pY", es=es)
        Vr = Rot(K, 2, [128, 512], F32, "pV", es=es)
        Gr = Rot(K, 2, [128, 512], F32, "pG", es=es)
        Cr = Rot(K, 2, [128, 512], F32, "pC", es=es)
        YCr = Rot(K, 2, [128, 512], F32, "pYC", es=es)
        SQr = Rot(K, 2, [128, 512], F32, "pSQ", es=es)
        Rr = Rot(K, 2, [128, 512], F32, "pR", es=es)
        Or = Rot(K, 2, [128, 512], BF16, "pO", es=es)
        PM = Rot(K, 2, [128, 512], F32, "pPM", psum=True, es=es)
        PVr = Rot(K, 2, [128, 512], F32, "pPV", psum=True, es=es)
        def make_gen(ti, j):
            cs = slice(ti * 512, (ti + 1) * 512)
            if True:
                rs = slice(j * 128, (j + 1) * 128)
                Y, dY = Yr.next()
                V, dV = Vr.next()
                G, dG = Gr.next()
                C, dC = Cr.next()
                K.sp.dma(lambda e, Y=Y, rs=rs, cs=cs: e.dma_start(out=Y[:], in_=scr["y_fm"][rs, cs]),
                         reads=[scr["d_p2b"]], writes=[dY])
                K.pool.dma(lambda e, V=V, rs=rs, cs=cs: e.dma_start(out=V[:], in_=scr["v_fm"][rs, cs]),
                           reads=[scr["d_p2a"]], writes=[dV])
                K.sp.dma(lambda e, G=G, rs=rs, cs=cs: e.dma_start(out=G[:], in_=scr["g_fm"][rs, cs]),
                         reads=[scr["d_p2a"]], writes=[dG])
                K.pool.dma(lambda e, C=C, j=j, cs=cs: e.dma_start(
                    out=C[0:64, :], in_=scr["coef_fm"][2 * j:2 * j + 1, cs].broadcast_to([64, 512])),
                    reads=[scr["d_p2a"]], writes=[dC])
                K.pool.dma(lambda e, C=C, j=j, cs=cs: e.dma_start(
                    out=C[64:128, :], in_=scr["coef_fm"][2 * j + 1:2 * j + 2, cs].broadcast_to([64, 512])),
                    reads=[scr["d_p2a"]], adds=[dC])
                pm, dpm = PM.next()
                K.pe.op(lambda e, pm=pm, Y=Y: e.matmul(pm[:], BO[:], Y[:], start=True, stop=True),
                        reads=[dY, d_c], writes=[dpm])
                YC, dYC = YCr.next()
                K.dve.op(lambda e, YC=YC, Y=Y, pm=pm: e.tensor_tensor(out=YC[:], in0=Y[:], in1=pm[:], op=ALU.subtract),
                         reads=[dY, dpm], writes=[dYC])
                SQ, dSQ = SQr.next()
                K.act.op(lambda e, SQ=SQ, YC=YC: e.activation(out=SQ[:], in_=YC[:], func=AF.Square),
                         reads=[dYC], writes=[dSQ])
                yield
                pv, dpv = PVr.next()
                K.pe.op(lambda e, pv=pv, SQ=SQ: e.matmul(pv[:], BO[:], SQ[:], start=True, stop=True),
                        reads=[dSQ, d_c], writes=[dpv])
                yield
                R, dR = Rr.next()
                K.dve.op(lambda e, R=R, pv=pv: e.tensor_scalar(out=R[:], in0=pv[:], scalar1=64e-5, scalar2=None, op0=ALU.add),
                         reads=[dpv], writes=[dR])
                K.act.op(lambda e, R=R: e.activation(out=R[:], in_=R[:], func=AF.Ln), writes=[dR])
                K.act.op(lambda e, R=R: e.activation(out=R[:], in_=R[:], func=AF.Exp, scale=-0.5), writes=[dR])
                K.dve.op(lambda e, YC=YC, R=R: e.tensor_tensor(out=YC[:], in0=YC[:], in1=R[:], op=ALU.mult),
                         reads=[dR], writes=[dYC])
                K.dve.op(lambda e, YC=YC, j=j: e.tensor_scalar(out=YC[:], in0=YC[:], scalar1=LN[:, 0, j:j + 1],
                                                              scalar2=LN[:, 1, j:j + 1], op0=ALU.mult, op1=ALU.add),
                         reads=[d_c], writes=[dYC])
                yield
                K.pool.op(lambda e, C=C, V=V: e.tensor_tensor(out=C[:], in0=C[:], in1=V[:], op=ALU.mult),
                          reads=[dV], writes=[dC])
                K.dve.op(lambda e, YC=YC, C=C: e.tensor_tensor(out=YC[:], in0=YC[:], in1=C[:], op=ALU.add),
                         reads=[dC], writes=[dYC])
                O, dO = Or.next()
                K.dve.op(lambda e, O=O, YC=YC, G=G: e.tensor_tensor(out=O[:], in0=YC[:], in1=G[:], op=ALU.mult),
                         reads=[dYC, dG], writes=[dO])
                K.sp.dma(lambda e, O=O, rs=rs, cs=cs: e.dma_start(out=scr["ya_fm"][rs, cs], in_=O[:]),
                         reads=[dO], adds=[scr["d_p2c"]])

        work = [(ti, j) for ti in range(NT) for j in range(4)]

        active = []
        nxt = 0
        while nxt < len(work) or active:
            if len(active) < 2 and nxt < len(work):
                active.append(make_gen(*work[nxt]))
                nxt += 1
            for a in list(active):
                try:
                    next(a)
                except StopIteration:
                    active.remove(a)


def phase3(K, cfg, io, scr):
    T, S, NB = cfg["T"], cfg["S"], cfg["NB"]
    NQ = S // 128
    lam_init = 0.2
    with ExitStack() as es:
        d_c = Dep()
        identb = K.sb([128, 128], BF16, "a_identb", es)
        K.sp.dma(lambda e: e.dma_start(out=identb[:], in_=io["ident_bf"][:, :]), adds=[d_c])
        TB = K.sb([128, 4, S], F32, "TB", es)
        for h in range(4):
            (K.sp if h % 2 == 0 else K.pool).dma(lambda e, h=h: e.dma_start(out=TB[:, h, :], in_=io["alibi"][h, :, :]), adds=[d_c])
        SW = K.sb([128, 128], F32, "SW", es)
        K.sp.dma(lambda e: e.dma_start(out=SW[:], in_=io["subln_w"][0:1, :].broadcast_to([128, 128])), adds=[d_c])
        LQ = K.sb([128, 4, 64], F32, "LQ", es)
        for j, nm in enumerate(["lam_q1", "lam_k1", "lam_q2", "lam_k2"]):
            K.pool.dma(lambda e, j=j, nm=nm: e.dma_start(out=LQ[:, j, :], in_=io[nm][0:1, :].broadcast_to([128, 64])), adds=[d_c])
        LM = K.sb([128, 8], F32, "LM", es)
        d_lm = Dep()
        LT_ = K.sb([128, 2, 64], F32, "LTt", es)
        K.dve.op(lambda e: e.tensor_tensor(out=LT_[:, 0, :], in0=LQ[:, 0, :], in1=LQ[:, 1, :], op=ALU.mult), reads=[d_c], writes=[d_lm])
        K.dve.op(lambda e: e.tensor_tensor(out=LT_[:, 1, :], in0=LQ[:, 2, :], in1=LQ[:, 3, :], op=ALU.mult), reads=[d_c], writes=[d_lm])
        K.dve.op(lambda e: e.tensor_reduce(out=LM[:, 0:2], in_=LT_[:], axis=AX.X, op=ALU.add), writes=[d_lm])
        K.act.op(lambda e: e.activation(out=LM[:, 2:4], in_=LM[:, 0:2], func=AF.Exp), writes=[d_lm])
        K.dve.op(lambda e: e.tensor_tensor(out=LM[:, 4:5], in0=LM[:, 3:4], in1=LM[:, 2:3], op=ALU.subtract), writes=[d_lm])
        K.dve.op(lambda e: e.tensor_scalar(out=LM[:, 4:5], in0=LM[:, 4:5], scalar1=-lam_init, scalar2=None, op0=ALU.add), writes=[d_lm])
        K.dve.op(lambda e: e.tensor_scalar(out=SW[:], in0=SW[:], scalar1=1.0 - lam_init, scalar2=None, op0=ALU.mult),
                 reads=[d_c], writes=[d_c])

        Vr = Rot(K, 2, [128, NQ, 512], BF16, "aV", es=es)
        QKr = Rot(K, 2, [64, 4, S], BF16, "aQK", es=es)
        SSr = Rot(K, 3, [128, 512], F32, "aSS", es=es)
        Pr = Rot(K, 3, [128, 512], BF16, "aP", es=es)
        PTsr = Rot(K, 4, [128, 4, 128], BF16, "aPTs", es=es)
        YB = K.sb([128, NQ, 512], BF16, "aYB", es)
        dYB = Dep()
        STr = Rot(K, 4, [128, 24], F32, "aST", es=es)
        O1r = Rot(K, 2, [128, 128], F32, "aO1", es=es)
        Or_ = Rot(K, 2, [128, 128], F32, "aO", es=es)
        junk = K.sb([128, 128], F32, "ajunk", es)
        d_junk = Dep()
        YTr = Rot(K, 2, [128, 4, 128], BF16, "aYT", es=es)
        PS = Rot(K, 3, [128, 512], F32, "aPS", psum=True, es=es)
        PTp = Rot(K, 2, [128, 4, 128], BF16, "aPTp", psum=True, es=es)
        PO = Rot(K, 2, [128, 2, 128], F32, "aPO", psum=True, es=es)
        cp = [0]

        def copy_eng():
            cp[0] += 1
            return cp[0] % 2

        SSQ = K.sb([128, NQ * 4], F32, "aSSQ", es)
        dSSQ = Dep()
        SWb = K.sb([128, 128], BF16, "aSWb", es)
        K.act.op(lambda e: e.activation(out=SWb[:], in_=SW[:], func=AF.Copy), reads=[d_c], adds=[d_c])
        pipe = []
        pidx = [0]

        def step_pipe():
            j = len(pipe) - 1
            pipe[j][0]()
            if j - 1 >= pidx[0]:
                pipe[j - 1][2]()
            if j - 2 >= pidx[0]:
                pipe[j - 2][3]()
                if pipe[j - 2][4] is not None:
                    pipe[j - 2][4]()
            pipe[j][1]()

        def flush_pipe():
            j = len(pipe) - 1
            if j - 0 >= pidx[0] and j >= 0:
                pipe[j][2]()
            for k in (j - 1, j):
                if k >= pidx[0] and k >= 0:
                    pipe[k][3]()
                    if pipe[k][4] is not None:
                        pipe[k][4]()
            pidx[0] = len(pipe)
        for b in range(NB):
            V, dV = Vr.next()
            K.sp.dma(lambda e, V=V, b=b: e.dma_start(
                out=V[:], in_=scr["av_tm"][b * S:(b + 1) * S, :].rearrange("(n p) c -> p n c", p=128)),
                reads=[scr["d_p1"]], writes=[dV])
            first_yb = True
            for h in range(4):
                QK, dQK = QKr.next()
                for j in range(4):
                    r0 = (0 if j < 2 else 512) + h * 128 + (j % 2) * 64
                    (K.sp if j % 2 == 0 else K.pool).dma(lambda e, QK=QK, j=j, r0=r0, b=b: e.dma_start(
                        out=QK[:, j, :], in_=scr["qk_fm"][r0:r0 + 64, b * S:(b + 1) * S]),
                        reads=[scr["d_p1"]], writes=[dQK] if j == 0 else [], adds=[] if j == 0 else [dQK])
                for qi in range(NQ):
                    nk = (qi + 1) * 128
                    off = (S - 128) - qi * 128
                    ST, dST = STr.next()
                    K.pool.op(lambda e, ST=ST: e.memset(ST[:], 0.0), writes=[dST])
                    po, dpo = PO.next()
                    items = []
                    for c in range(2):
                        nch = (nk + 511) // 512
                        for ch in range(nch):
                            items.append((c, ch))
                    for ii, (c, ch) in enumerate(items):
                        kb0 = ch * 512
                        n = min(512, nk - kb0)
                        nb = n // 128
                        ps, dps = PS.next()
                        SS, dSS = SSr.next()
                        Pt, dP = Pr.next()
                        hold = {}

                        def stA_pe(ps=ps, dps=dps, c=c, qi=qi, kb0=kb0, n=n, QK=QK, dQK=dQK):
                            K.pe.op(lambda e: e.matmul(
                                ps[:, :n], QK[:, c, qi * 128:(qi + 1) * 128], QK[:, 2 + c, kb0:kb0 + n],
                                start=True, stop=True), reads=[dQK], writes=[dps])

                        def stA_rest(ps=ps, dps=dps, SS=SS, dSS=dSS, Pt=Pt, dP=dP, ST=ST, dST=dST, n=n, off=off, h=h, kb0=kb0, c=c, ch=ch):
                            K.dve.op(lambda e: e.scalar_tensor_tensor(
                                out=SS[:, :n], in0=ps[:, :n], scalar=0.125, in1=TB[:, h, off + kb0:off + kb0 + n],
                                op0=ALU.mult, op1=ALU.add), reads=[dps, d_c], writes=[dSS])
                            K.act.op(lambda e: e.activation(
                                out=Pt[:, :n], in_=SS[:, :n], func=AF.Exp,
                                accum_out=ST[:, 4 * c + ch:4 * c + ch + 1]), reads=[dSS, dST], writes=[dP], adds=[dST])

                        def stB(Pt=Pt, dP=dP, nb=nb, hold=hold):
                            ptp, dptp = PTp.next()
                            for kk_ in range(nb):
                                K.pe.op(lambda e, kk_=kk_: e.transpose(
                                    out=ptp[:, kk_, :], in_=Pt[:, kk_ * 128:(kk_ + 1) * 128], identity=identb[:]),
                                    reads=[dP, d_c], writes=[dptp] if kk_ == 0 else [], adds=[] if kk_ == 0 else [dptp])
                            PTs, dPTs = PTsr.next()
                            hold["PTs"] = (PTs, dPTs)
                            if copy_eng():
                                K.act.op(lambda e: e.activation(
                                    out=PTs[:, :nb, :], in_=ptp[:, :nb, :], func=AF.Copy), reads=[dptp], writes=[dPTs])
                            else:
                                K.dve.op(lambda e: e.tensor_copy(
                                    out=PTs[:, :nb, :], in_=ptp[:, :nb, :]), reads=[dptp], writes=[dPTs])

                        def stC(nb=nb, kb0=kb0, c=c, h=h, qi=qi, po=po, dpo=dpo, V=V, dV=dV, hold=hold):
                            PTs, dPTs = hold["PTs"]
                            for kk_ in range(nb):
                                kb = kb0 // 128 + kk_
                                K.pe.op(lambda e, kb=kb, kk_=kk_: e.matmul(
                                    po[:, c, :], PTs[:, kk_, :], V[:, kb, h * 128:(h + 1) * 128],
                                    start=(kb == 0), stop=(kb == qi)), reads=[dPTs, dV],
                                    writes=[dpo] if (kb == 0 and c == 0) else [], adds=[] if (kb == 0 and c == 0) else [dpo])

                        def combine(ST=ST, dST=dST, po=po, dpo=dpo, qi=qi, h=h, fy=first_yb):
                            K.dve.op(lambda e: e.tensor_reduce(out=ST[:, 8:10], in_=ST[:, 0:8].rearrange("p (c k) -> p c k", k=4),
                                                               axis=AX.X, op=ALU.add), reads=[dST], writes=[dST])
                            K.dve.op(lambda e: e.reciprocal(out=ST[:, 10:12], in_=ST[:, 8:10]), writes=[dST])
                            K.dve.op(lambda e: e.tensor_tensor(out=ST[:, 12:13], in0=ST[:, 11:12], in1=LM[:, 4:5], op=ALU.mult),
                                     reads=[d_lm], writes=[dST])
                            O1, dO1 = O1r.next()
                            K.dve.op(lambda e: e.tensor_scalar(out=O1[:], in0=po[:, 1, :], scalar1=ST[:, 12:13],
                                                               scalar2=None, op0=ALU.mult), reads=[dpo, dST], writes=[dO1])
                            K.dve.op(lambda e: e.scalar_tensor_tensor(
                                out=YB[:, qi, h * 128:(h + 1) * 128], in0=po[:, 0, :], scalar=ST[:, 10:11], in1=O1[:], op0=ALU.mult, op1=ALU.add),
                                reads=[dpo, dST, dO1], writes=[dYB] if fy else [], adds=[] if fy else [dYB])
                            K.act.op(lambda e: e.activation(out=junk[:], in_=YB[:, qi, h * 128:(h + 1) * 128], func=AF.Square,
                                                            accum_out=SSQ[:, qi * 4 + h:qi * 4 + h + 1]),
                                     reads=[dYB], writes=[d_junk], adds=[dSSQ])

                        last = (ii == len(items) - 1)
                        pipe.append([stA_pe, stA_rest, stB, stC, combine if last else None])
                        step_pipe()
                    first_yb = False
            flush_pipe()
            K.dve.op(lambda e: e.tensor_scalar(out=SSQ[:], in0=SSQ[:], scalar1=1.0 / 128, scalar2=1e-5, op0=ALU.mult, op1=ALU.add),
                     reads=[dSSQ], writes=[dSSQ])
            K.act.op(lambda e: e.activation(out=SSQ[:], in_=SSQ[:], func=AF.Ln), writes=[dSSQ])
            K.act.op(lambda e: e.activation(out=SSQ[:], in_=SSQ[:], func=AF.Exp, scale=-0.5), writes=[dSSQ])
            YBv = YB[:].rearrange("p q (h e) -> p (q h) e", e=128)
            K.dve.op(lambda e: e.tensor_tensor(out=YBv, in0=YBv, in1=SSQ[:].unsqueeze(2).broadcast_to([128, NQ * 4, 128]), op=ALU.mult),
                     reads=[dSSQ], writes=[dYB])
            K.pool.op(lambda e: e.tensor_tensor(out=YBv, in0=YBv, in1=SWb[:].unsqueeze(1).broadcast_to([128, NQ * 4, 128]), op=ALU.mult),
                      reads=[d_c], writes=[dYB])
            for qi in range(NQ):
                ptp, dptp = PTp.next()
                for h in range(4):
                    K.pe.op(lambda e, ptp=ptp, qi=qi, h=h: e.transpose(out=ptp[:, h, :], in_=YB[:, qi, h * 128:(h + 1) * 128],
                                                                      identity=identb[:]),
                            reads=[dYB, d_c], writes=[dptp] if h == 0 else [], adds=[] if h == 0 else [dptp])
                YT, dYT = YTr.next()
                K.act.op(lambda e, ptp=ptp, YT=YT: e.activation(out=YT[:], in_=ptp[:, 0:4, :], func=AF.Copy),
                         reads=[dptp], writes=[dYT])
                t0 = b * S + qi * 128
                K.sp.dma(lambda e, YT=YT, t0=t0: e.dma_start(
                    out=dap(scr["yb_fm"], t0, [[T, 128], [128 * T, 4], [1, 128]]), in_=YT[:]),
                    reads=[dYT], adds=[scr["d_p3"]])


def load_w_bf16(K, es, src2d, rows, cols, name, dep, stage_rot, q, W=None):
    nk = rows // 128
    if W is None:
        W = K.sb([128, nk, cols], BF16, name, es)
    for kc in range(nk):
        for c0 in range(0, cols, 1024):
            n = min(1024, cols - c0)
            st, dst = stage_rot.next()
            q[0] += 1
            (K.sp if q[0] % 2 == 0 else K.pool).dma(lambda e, st=st, kc=kc, c0=c0, n=n: e.dma_start(
                out=st[:, :n], in_=src2d[kc * 128:(kc + 1) * 128, c0:c0 + n]), writes=[dst])
            if q[0] % 2 == 0:
                K.act.op(lambda e, st=st, kc=kc, c0=c0, n=n: e.activation(out=W[:, kc, c0:c0 + n], in_=st[:, :n], func=AF.Copy),
                         reads=[dst], adds=[dep])
            else:
                K.dve.op(lambda e, st=st, kc=kc, c0=c0, n=n: e.tensor_copy(out=W[:, kc, c0:c0 + n], in_=st[:, :n]),
                         reads=[dst], adds=[dep])
    return W


def phase4(K, cfg, io, scr):
    T, S, NB = cfg["T"], cfg["S"], cfg["NB"]
    NT = T // 512
    with ExitStack() as es:
        d_c = Dep()
        identb = K.sb([128, 128], BF16, "m_identb", es)
        K.sp.dma(lambda e: e.dma_start(out=identb[:], in_=io["ident_bf"][:, :]), adds=[d_c])
        NF = K.sb([128, D], F32, "NF", es)
        K.sp.dma(lambda e: e.dma_start(out=NF[:], in_=io["norm_ffn_w"][0:1, :].broadcast_to([128, D])), adds=[d_c])
        stg = Rot(K, 2, [128, 1024], F32, "m_stg", es=es)
        q = [0]
        PAw = load_w_bf16(K, es, io["proj_a"][0], 512, D, "PAw", d_c, stg, q)
        PBw = load_w_bf16(K, es, io["proj_b"][0], 512, D, "PBw", d_c, stg, q)
        WO = load_w_bf16(K, es, io["w_out"][0], D, D, "WO", d_c, stg, q)
        YAr = Rot(K, 2, [128, 4, 512], BF16, "mYA", es=es)
        YBr = Rot(K, 2, [128, 4, 512], BF16, "mYB", es=es)
        SGr = Rot(K, 2, [128, 16, 512], BF16, "mSG", es=es)
        MGr = Rot(K, 2, [128, 8, 512], BF16, "mMG", es=es)
        t1r = Rot(K, 2, [128, 512], F32, "mt1", es=es)
        t2r = Rot(K, 2, [128, 512], F32, "mt2", es=es)
        Xr = Rot(K, 2, [128, D], F32, "mX", es=es)
        X1r = Rot(K, 2, [128, D], F32, "mX1", es=es)
        XHr = Rot(K, 2, [128, D], F32, "mXH", es=es)
        XBr = Rot(K, 2, [128, D], BF16, "mXB", es=es)
        XTr = Rot(K, 2, [128, 8, 128], BF16, "mXT", es=es)
        junk = K.sb([128, D], BF16, "mjunk", es)
        d_junk = Dep()
        STr = Rot(K, 4, [128, 4], F32, "mST", es=es)
        PP = Rot(K, 2, [128, 2, 512], F32, "mPP", psum=True, es=es)
        PO2 = Rot(K, 1, [128, 2, 512], F32, "mPO", psum=True, es=es)
        PTp = Rot(K, 1, [128, 8, 128], BF16, "mPTp", psum=True, es=es)
        def make_gen(ti):
            cs = slice(ti * 512, (ti + 1) * 512)
            YA, dYA = YAr.next()
            YB, dYB = YBr.next()
            SG, dSG = SGr.next()
            K.sp.dma(lambda e, YA=YA, cs=cs: e.dma_start(out=YA[:], in_=scr["ya_fm"][:, cs].rearrange("(c p) t -> p c t", p=128)),
                     reads=[scr["d_p2c"]], writes=[dYA])
            K.pool.dma(lambda e, YB=YB, cs=cs: e.dma_start(out=YB[:], in_=scr["yb_fm"][:, cs].rearrange("(c p) t -> p c t", p=128)),
                       reads=[scr["d_p3"]], writes=[dYB])
            K.sp.dma(lambda e, SG=SG, cs=cs: e.dma_start(out=SG[:], in_=scr["sg_fm"][:, cs].rearrange("(c p) t -> p c t", p=128)),
                     reads=[scr["d_p1"]], writes=[dSG])
            MG, dMG = MGr.next()
            for m in range(8):
                pp, dpp = PP.next()
                for c in range(4):
                    K.pe.op(lambda e, pp=pp, YA=YA, c=c, m=m: e.matmul(pp[:, 0, :], PAw[:, c, m * 128:(m + 1) * 128], YA[:, c, :],
                                                                      start=(c == 0), stop=(c == 3)),
                            reads=[dYA, d_c], writes=[dpp] if c == 0 else [], adds=[] if c == 0 else [dpp])
                for c in range(4):
                    K.pe.op(lambda e, pp=pp, YB=YB, c=c, m=m: e.matmul(pp[:, 1, :], PBw[:, c, m * 128:(m + 1) * 128], YB[:, c, :],
                                                                      start=(c == 0), stop=(c == 3)),
                            reads=[dYB, d_c], adds=[dpp])
                t1, dt1 = t1r.next()
                t2, dt2 = t2r.next()
                K.dve.op(lambda e, t1=t1, pp=pp, SG=SG, m=m: e.tensor_tensor(out=t1[:], in0=pp[:, 0, :], in1=SG[:, m, :], op=ALU.mult),
                         reads=[dpp, dSG], writes=[dt1])
                K.dve.op(lambda e, t2=t2, pp=pp, SG=SG, m=m: e.tensor_tensor(out=t2[:], in0=pp[:, 1, :], in1=SG[:, 8 + m, :], op=ALU.mult),
                         reads=[dpp, dSG], writes=[dt2])
                K.pool.op(lambda e, t1=t1, t2=t2, MG=MG, m=m: e.tensor_tensor(out=MG[:, m, :], in0=t1[:], in1=t2[:], op=ALU.add),
                          reads=[dt1, dt2], writes=[dMG] if m == 0 else [], adds=[] if m == 0 else [dMG])
                if m % 2 == 1:
                    yield
            for sub in range(4):
                t0 = ti * 512 + sub * 128
                X, dX = Xr.next()
                K.pool.dma(lambda e, X=X, t0=t0: e.dma_start(out=X[:], in_=io["x"][t0:t0 + 128, :]), writes=[dX])
                po, dpo = PO2.next()
                for n in range(2):
                    for m in range(8):
                        K.pe.op(lambda e, po=po, MG=MG, m=m, n=n, sub=sub: e.matmul(
                            po[:, n, :], MG[:, m, sub * 128:(sub + 1) * 128], WO[:, m, n * 512:(n + 1) * 512],
                            start=(m == 0), stop=(m == 7)), reads=[dMG, d_c],
                            writes=[dpo] if (m == 0 and n == 0) else [], adds=[] if (m == 0 and n == 0) else [dpo])
                X1, dX1 = X1r.next()
                K.dve.op(lambda e, X1=X1, X=X, po=po: e.tensor_tensor(out=X1[:], in0=X[:], in1=po[:].rearrange("p a b -> p (a b)"),
                                                                     op=ALU.add), reads=[dX, dpo], writes=[dX1])
                K.sp.dma(lambda e, X1=X1, t0=t0: e.dma_start(out=scr["x1_tm"][t0:t0 + 128, :], in_=X1[:]),
                         reads=[dX1], adds=[scr["d_p4"]])
                ST, dST = STr.next()
                K.act.op(lambda e, X1=X1, ST=ST: e.activation(out=junk[:], in_=X1[:], func=AF.Square, accum_out=ST[:, 0:1]),
                         reads=[dX1], writes=[d_junk, dST])
                K.dve.op(lambda e, ST=ST: e.tensor_scalar(out=ST[:, 1:2], in0=ST[:, 0:1], scalar1=1.0 / D, scalar2=1e-6,
                                                          op0=ALU.mult, op1=ALU.add), writes=[dST])
                K.act.op(lambda e, ST=ST: e.activation(out=ST[:, 2:3], in_=ST[:, 1:2], func=AF.Ln), writes=[dST])
                K.act.op(lambda e, ST=ST: e.activation(out=ST[:, 3:4], in_=ST[:, 2:3], func=AF.Exp, scale=-0.5), writes=[dST])
                XH, dXH = XHr.next()
                K.dve.op(lambda e, XH=XH, X1=X1, ST=ST: e.scalar_tensor_tensor(
                    out=XH[:], in0=X1[:], scalar=ST[:, 3:4], in1=NF[:], op0=ALU.mult, op1=ALU.mult),
                    reads=[dX1, dST, d_c], writes=[dXH])
                K.sp.dma(lambda e, XH=XH, t0=t0: e.dma_start(out=scr["xh_tm"][t0:t0 + 128, :], in_=XH[:]),
                         reads=[dXH], adds=[scr["d_p4"]])
                XB, dXB = XBr.next()
                K.act.op(lambda e, XB=XB, XH=XH: e.activation(out=XB[:], in_=XH[:], func=AF.Copy), reads=[dXH], writes=[dXB])
                yield
                ptp, dptp = PTp.next()
                for kc in range(8):
                    K.pe.op(lambda e, ptp=ptp, XB=XB, kc=kc: e.transpose(out=ptp[:, kc, :], in_=XB[:, kc * 128:(kc + 1) * 128],
                                                                        identity=identb[:]),
                            reads=[dXB, d_c], writes=[dptp] if kc == 0 else [], adds=[] if kc == 0 else [dptp])
                XT, dXT = XTr.next()
                K.act.op(lambda e, ptp=ptp, XT=XT: e.activation(out=XT[:], in_=ptp[:], func=AF.Copy), reads=[dptp], writes=[dXT])
                K.sp.dma(lambda e, XT=XT, t0=t0: e.dma_start(
                    out=dap(scr["xhT_fm"], t0, [[T, 128], [128 * T, 8], [1, 128]]), in_=XT[:]),
                    reads=[dXT], adds=[scr["d_p4"]])
                yield

        work = [(ti,) for ti in range(NT)]

        active = []
        nxt = 0
        while nxt < len(work) or active:
            if len(active) < 2 and nxt < len(work):
                active.append(make_gen(*work[nxt]))
                nxt += 1
            for a in list(active):
                try:
                    next(a)
                except StopIteration:
                    active.remove(a)


def phase5(K, cfg, io, scr):
    T, S, NB = cfg["T"], cfg["S"], cfg["NB"]
    NTT = T // 128
    with ExitStack() as es:
        d_c = Dep()
        identb = K.sb([128, 128], BF16, "f_identb", es)
        K.sp.dma(lambda e: e.dma_start(out=identb[:], in_=io["ident_bf"][:, :]), adds=[d_c])
        IOTA = K.sb([128, 16], F32, "IOTA", es)
        K.sp.dma(lambda e: e.dma_start(out=IOTA[:], in_=io["iota16"][:, :]), adds=[d_c])
        FNW = K.sb([128, D], F32, "FNW", es)
        K.sp.dma(lambda e: e.dma_start(out=FNW[:], in_=io["final_norm_w"][0:1, :].broadcast_to([128, D])), adds=[d_c])
        WQ = K.sb([128, 8, 2048], BF16, "WQ", es)
        KT = K.sb([128, 16, 128], BF16, "KT", es)
        PQ = Rot(K, 1, [128, 8, 128], F32, "fPQ", psum=True, es=es)
        PSc = Rot(K, 1, [128, 8, 128], F32, "fPSc", psum=True, es=es)
        PKT = Rot(K, 1, [128, 8, 128], BF16, "fPKT", psum=True, es=es)
        es_setup = ExitStack()
        stg = Rot(K, 2, [128, 1024], F32, "f_stg", es=es_setup)
        q = [0]
        load_w_bf16(K, es, io["peer_wq"][0], D, 2048, "WQ", d_c, stg, q, W=WQ)
        KF = K.sb([128, 16, 128], F32, "KF", es_setup)
        dKF = Dep()
        K.sp.dma(lambda e: e.dma_start(out=KF[:], in_=io["peer_keys"][0].rearrange("h c n d -> n (h c) d")), writes=[dKF])
        KB = K.sb([128, 16, 128], BF16, "KB", es_setup)
        K.dve.op(lambda e: e.tensor_copy(out=KB[:], in_=KF[:]), reads=[dKF], writes=[dKF])
        for half in range(2):
            pk, dpk = PKT.next()
            for i in range(8):
                K.pe.op(lambda e, pk=pk, i=i, half=half: e.transpose(out=pk[:, i, :], in_=KB[:, half * 8 + i, :], identity=identb[:]),
                        reads=[dKF, d_c], writes=[dpk] if i == 0 else [], adds=[] if i == 0 else [dpk])
            K.act.op(lambda e, pk=pk, half=half: e.activation(out=KT[:, half * 8:(half + 1) * 8, :], in_=pk[:], func=AF.Copy),
                     reads=[dpk], adds=[d_c])

        K.barrier()
        es_setup.close()
        XTr = Rot(K, 2, [128, 8, 128], BF16, "fXT", es=es)
        XHr = Rot(K, 2, [128, D], F32, "fXH", es=es)
        X1r = Rot(K, 2, [128, D], F32, "fX1", es=es)
        QTr = Rot(K, 1, [128, 16, 128], BF16, "fQT", es=es)
        SCr = Rot(K, 1, [128, 16, 128], F32, "fSC", es=es)
        SC2 = K.sb([128, 256], F32, "fSC2", es)
        dSC2 = Dep()
        M16r = Rot(K, 1, [128, 16, 16], F32, "fM16", es=es)
        I16r = Rot(K, 1, [128, 16, 16], U32, "fI16", es=es)
        I16fr = Rot(K, 1, [128, 16, 16], F32, "fI16f", es=es)
        CANDr = Rot(K, 1, [128, 8, 256], F32, "fCAND", es=es)
        VALr = Rot(K, 1, [128, 8, 16], F32, "fVAL", es=es)
        CIr = Rot(K, 1, [128, 3, 128], U32, "fCI", es=es)
        ABr = Rot(K, 1, [128, 2, 128], F32, "fAB", es=es)
        OHr = CANDr
        E12r = Rot(K, 1, [128, 3, 128], F32, "fE12", es=es)
        IDSr = Rot(K, 2, [128, 128], I32, "fIDS", es=es)
        GTr = Rot(K, 2, [128, 4, 128], F32, "fGT", es=es)
        S8r = Rot(K, 2, [128, 16], F32, "fS8", es=es)
        GRP = cfg.get("grp", 4)
        ACTDOT = tuple(cfg.get("actdot", (0, 2)))
        if isinstance(cfg.get("actdot_mask"), int):
            ACTDOT = tuple(i for i in range(GRP) if (cfg["actdot_mask"] >> i) & 1)
        junk3r = Rot(K, 2, [128, D], BF16, "fjunk3", es=es)
        junkr = Rot(K, 3, [128, D], BF16, "fjunkr", es=es)
        PRDr = Rot(K, 3, [128, D], BF16, "fPRD", es=es)
        GBr = Rot(K, cfg.get("ngbuf", 22), [128, 2 * D], BF16, "fGB", es=es)
        junk2 = K.sb([128, D], BF16, "fjunk2", es)
        d_junk2 = Dep()
        XHbr = Rot(K, 2, [128, D], BF16, "fXHb", es=es)
        DGr = Rot(K, 4, [128, 128], BF16, "fDG", es=es)
        PY = Rot(K, 1, [128, 2, 512], F32, "fPY", psum=True, es=es)

        RES = {}

        def routing(ti):
            t0 = ti * 128
            XT, dXT = XTr.next()
            XH, dXH = XHr.next()
            X1, dX1 = X1r.next()
            K.sp.dma(lambda e, XT=XT, t0=t0: e.dma_start(out=XT[:], in_=dap(scr["xhT_fm"], t0, [[T, 128], [128 * T, 8], [1, 128]])),
                     reads=[scr["d_p4"]], writes=[dXT])
            K.sp.dma(lambda e, XH=XH, t0=t0: e.dma_start(out=XH[:], in_=scr["xh_tm"][t0:t0 + 128, :]), reads=[scr["d_p4"]], writes=[dXH])
            K.sp.dma(lambda e, X1=X1, t0=t0: e.dma_start(out=X1[:], in_=scr["x1_tm"][t0:t0 + 128, :]), reads=[scr["d_p4"]], writes=[dX1])
            QT, dQT = QTr.next()
            for half in range(2):
                pq, dpq = PQ.next()
                for i in range(8):
                    hc = half * 8 + i
                    for kc in range(8):
                        K.pe.op(lambda e, pq=pq, i=i, hc=hc, kc=kc, XT=XT: e.matmul(
                            pq[:, i, :], WQ[:, kc, hc * 128:(hc + 1) * 128], XT[:, kc, :], start=(kc == 0), stop=(kc == 7)),
                            reads=[dXT, d_c], writes=[dpq] if (i == 0 and kc == 0) else [], adds=[] if (i == 0 and kc == 0) else [dpq])
                K.act.op(lambda e, pq=pq, QT=QT, half=half: e.activation(out=QT[:, half * 8:(half + 1) * 8, :], in_=pq[:], func=AF.Copy),
                         reads=[dpq], writes=[dQT] if half == 0 else [], adds=[] if half == 0 else [dQT])
            SC, dSC = SCr.next()
            for half in range(2):
                psc, dpsc = PSc.next()
                for i in range(8):
                    hc = half * 8 + i
                    K.pe.op(lambda e, psc=psc, i=i, hc=hc, QT=QT: e.matmul(psc[:, i, :], QT[:, hc, :], KT[:, hc, :], start=True, stop=True),
                            reads=[dQT, d_c], writes=[dpsc] if i == 0 else [], adds=[] if i == 0 else [dpsc])
                K.act.op(lambda e, psc=psc, SC=SC, half=half: e.activation(out=SC[:, half * 8:(half + 1) * 8, :], in_=psc[:], func=AF.Copy),
                         reads=[dpsc], writes=[dSC] if half == 0 else [], adds=[] if half == 0 else [dSC])
            yield
            M16, dM = M16r.next()
            I16, dI = I16r.next()
            for hc in range(16):
                if hc % 4 == 0 and hc > 0:
                    yield
                K.dve.op(lambda e, M16=M16, SC=SC, hc=hc: e.max(out=M16[:, hc, 0:8], in_=SC[:, hc, :]), reads=[dSC],
                         writes=[dM] if hc == 0 else [], adds=[] if hc == 0 else [dM])
                K.dve.op(lambda e, M16=M16, SC=SC, hc=hc: e.match_replace(out=SC2[:, 0:128], in_to_replace=M16[:, hc, 0:8],
                                                                         in_values=SC[:, hc, :], imm_value=-1e30),
                         reads=[dSC, dM], writes=[dSC2])
                K.dve.op(lambda e, M16=M16, hc=hc: e.max(out=M16[:, hc, 8:16], in_=SC2[:, 0:128]), reads=[dSC2], adds=[dM])
                K.dve.op(lambda e, M16=M16, I16=I16, SC=SC, hc=hc: e.max_index(out=I16[:, hc, 0:8], in_max=M16[:, hc, 0:8],
                                                                              in_values=SC[:, hc, :]),
                         reads=[dSC, dM], writes=[dI] if hc == 0 else [], adds=[] if hc == 0 else [dI])
                K.dve.op(lambda e, M16=M16, I16=I16, SC=SC, hc=hc: e.max_index(out=I16[:, hc, 8:16], in_max=M16[:, hc, 8:16],
                                                                              in_values=SC[:, hc, :]),
                         reads=[dSC, dM], adds=[dI])
            yield
            I16f, dIf = I16fr.next()
            K.dve.op(lambda e, I16f=I16f, I16=I16: e.tensor_copy(out=I16f[:], in_=I16[:]), reads=[dI], writes=[dIf])
            I16fv = I16f[:].rearrange("p (h c) k -> p h c k", c=2)
            K.dve.op(lambda e, I16fv=I16fv: e.tensor_scalar(out=I16fv[:, :, 0, :], in0=I16fv[:, :, 0, :], scalar1=128.0, scalar2=None,
                                                            op0=ALU.mult), writes=[dIf])
            CAND, dCA = CANDr.next()
            M16v = M16[:].rearrange("p (h c) k -> p h c k", c=2)
            K.dve.op(lambda e, CAND=CAND, M16v=M16v: e.tensor_tensor(
                out=CAND[:].rearrange("p h (a b) -> p h a b", b=16),
                in0=M16v[:, :, 0, :].unsqueeze(3).broadcast_to([128, 8, 16, 16]),
                in1=M16v[:, :, 1, :].unsqueeze(2).broadcast_to([128, 8, 16, 16]), op=ALU.add),
                reads=[dM], writes=[dCA])
            VAL, dVAL = VALr.next()
            CI, dCI = CIr.next()
            CIv = CI[:, 0, :].rearrange("p (h k) -> p h k", k=16)
            for h in range(8):
                if h % 4 == 0:
                    yield
                K.dve.op(lambda e, VAL=VAL, CAND=CAND, h=h: e.max(out=VAL[:, h, 0:8], in_=CAND[:, h, :]), reads=[dCA],
                         writes=[dVAL] if h == 0 else [], adds=[] if h == 0 else [dVAL])
                K.dve.op(lambda e, VAL=VAL, CAND=CAND, h=h: e.match_replace(out=SC2[:, :], in_to_replace=VAL[:, h, 0:8],
                                                                           in_values=CAND[:, h, :], imm_value=-1e30),
                         reads=[dCA, dVAL], writes=[dSC2])
                K.dve.op(lambda e, VAL=VAL, h=h: e.max(out=VAL[:, h, 8:16], in_=SC2[:, :]), reads=[dSC2], adds=[dVAL])
                K.dve.op(lambda e, VAL=VAL, CIv=CIv, CAND=CAND, h=h: e.max_index(out=CIv[:, h, 0:8], in_max=VAL[:, h, 0:8],
                                                                                in_values=CAND[:, h, :]),
                         reads=[dCA, dVAL], writes=[dCI] if h == 0 else [], adds=[] if h == 0 else [dCI])
                K.dve.op(lambda e, VAL=VAL, CIv=CIv, CAND=CAND, h=h: e.max_index(out=CIv[:, h, 8:16], in_max=VAL[:, h, 8:16],
                                                                                in_values=CAND[:, h, :]),
                         reads=[dCA, dVAL], adds=[dCI])
            yield
            GT, dGT = GTr.next()
            S8, dS8 = S8r.next()
            Ev = GT[:, 0, :].rearrange("p (h k) -> p h k", k=16)
            Gv = GT[:, 1, :].rearrange("p (h k) -> p h k", k=16)
            K.dve.op(lambda e, Ev=Ev, VAL=VAL: e.tensor_tensor(out=Ev, in0=VAL[:], in1=VAL[:, :, 0:1].broadcast_to([128, 8, 16]),
                                                              op=ALU.subtract), reads=[dVAL], writes=[dGT])
            K.act.op(lambda e, GT=GT: e.activation(out=GT[:, 0, :], in_=GT[:, 0, :], func=AF.Exp), writes=[dGT])
            K.dve.op(lambda e, Ev=Ev, S8=S8: e.tensor_reduce(out=S8[:, 0:8], in_=Ev, axis=AX.X, op=ALU.add), reads=[dGT], writes=[dS8])
            K.dve.op(lambda e, S8=S8: e.reciprocal(out=S8[:, 8:16], in_=S8[:, 0:8]), writes=[dS8])
            K.dve.op(lambda e, Ev=Ev, Gv=Gv, S8=S8: e.tensor_tensor(out=Gv, in0=Ev, in1=S8[:, 8:16].unsqueeze(2).broadcast_to([128, 8, 16]),
                                                                   op=ALU.mult), reads=[dS8], writes=[dGT])
            yield
            K.dve.op(lambda e, CI=CI: e.tensor_single_scalar(out=CI[:, 1, :], in_=CI[:, 0, :], scalar=4, op=ALU.logical_shift_right),
                     writes=[dCI])
            K.dve.op(lambda e, CI=CI: e.tensor_single_scalar(out=CI[:, 2, :], in_=CI[:, 0, :], scalar=15, op=ALU.bitwise_and),
                     writes=[dCI])
            AB, dAB = ABr.next()
            K.dve.op(lambda e, AB=AB, CI=CI: e.tensor_copy(out=AB[:], in_=CI[:, 1:3, :]), reads=[dCI], writes=[dAB])
            OH, dOH = OHr.next()
            E12, dE12 = E12r.next()
            OHv = OH[:].rearrange("p h (j a) -> p h j a", a=16)
            for c in range(2):
                ABv = AB[:, c, :].rearrange("p (h j) -> p h j", j=16)
                K.dve.op(lambda e, OHv=OHv, ABv=ABv: e.tensor_tensor(
                    out=OHv, in0=ABv.unsqueeze(3).broadcast_to([128, 8, 16, 16]),
                    in1=IOTA[:].unsqueeze(1).unsqueeze(1).broadcast_to([128, 8, 16, 16]), op=ALU.is_equal),
                    reads=[dAB, d_c], writes=[dOH])
                K.dve.op(lambda e, OHv=OHv, I16fv=I16fv, c=c: e.tensor_tensor(
                    out=OHv, in0=OHv, in1=I16fv[:, :, c, :].unsqueeze(2).broadcast_to([128, 8, 16, 16]), op=ALU.mult),
                    reads=[dIf], writes=[dOH])
                K.dve.op(lambda e, OHv=OHv, E12=E12, c=c: e.tensor_reduce(
                    out=E12[:, c, :].rearrange("p (h j) -> p h j", j=16), in_=OHv, axis=AX.X, op=ALU.add),
                    reads=[dOH], writes=[dE12] if c == 0 else [], adds=[] if c == 0 else [dE12])
            K.dve.op(lambda e, E12=E12: e.tensor_tensor(out=E12[:, 2, :], in0=E12[:, 0, :], in1=E12[:, 1, :], op=ALU.add), writes=[dE12])
            IDS, dIDS = IDSr.next()
            K.dve.op(lambda e, IDS=IDS, E12=E12: e.tensor_copy(out=IDS[:], in_=E12[:, 2, :]), reads=[dE12], writes=[dIDS])
            if "ids_dbg" in scr:
                K.sp.dma(lambda e, IDS=IDS, t0=t0: e.dma_start(out=scr["ids_dbg"][t0:t0 + 128, :], in_=IDS[:]), reads=[dIDS])
                K.sp.dma(lambda e, GT=GT, t0=t0: e.dma_start(out=scr["gate_dbg"][t0:t0 + 128, :], in_=GT[:, 1, :]), reads=[dGT])
            RES[ti] = dict(t0=t0, XH=XH, dXH=dXH, X1=X1, dX1=dX1, IDS=IDS, dIDS=dIDS, GT=GT, dGT=dGT, S8=S8, dS8=dS8)

        GDEPS = {}

        def expert(R):
            t0, XH, dXH, X1, dX1, IDS, dIDS, GT, dGT, S8, dS8 = (R[k] for k in
                ("t0", "XH", "dXH", "X1", "dX1", "IDS", "dIDS", "GT", "dGT", "S8", "dS8"))
            XHb, dXHb = XHbr.next()
            K.act.op(lambda e: e.activation(out=XHb[:], in_=XH[:], func=AF.Copy), reads=[dXH], writes=[dXHb])
            py, dpy = PY.next()
            NGRP = 128 // GRP
            bufs = {}
            gd = GDEPS.setdefault(id(GT), [(Dep(), Dep()) for _ in range(NGRP)])

            def stage_a(g):
                for jj in range(GRP):
                    j = g * GRP + jj
                    GB, dGB = GBr.next()
                    bufs[j] = (GB, dGB)
                    K.pool.dma(lambda e, GB=GB, j=j: e.indirect_dma_start(
                        out=GB[:], out_offset=None, in_=scr["uv_tab"][:, :],
                        in_offset=bass.IndirectOffsetOnAxis(ap=IDS[:, j:j + 1], axis=0)), reads=[dIDS, scr["d_uv"]], writes=[dGB])
                    if jj in ACTDOT:
                        PRD, dPRD = PRDr.next()
                        K.dve.op(lambda e, GB=GB, PRD=PRD: e.tensor_tensor(out=PRD[:], in0=GB[:, 0:D], in1=XHb[:], op=ALU.mult),
                                 reads=[dGB, dXHb], writes=[dPRD])
                        j3, dj3 = junk3r.next()
                        K.act.op(lambda e, PRD=PRD, j=j, j3=j3: e.activation(out=j3[:], in_=PRD[:], func=AF.Copy, accum_out=GT[:, 2, j:j + 1]),
                                 reads=[dPRD], writes=[dj3] + ([gd[g][0]] if jj == 0 else []), adds=[] if jj == 0 else [gd[g][0]])
                    else:
                        j1, dj1 = junkr.next()
                        K.dve.op(lambda e, GB=GB, j=j, j1=j1: e.scalar_tensor_tensor(
                            out=j1[:], in0=GB[:, 0:D], scalar=1.0, in1=XHb[:], op0=ALU.mult, op1=ALU.mult, accum_out=GT[:, 2, j:j + 1]),
                            reads=[dGB, dXHb], writes=[dj1] + ([gd[g][0]] if jj == 0 else []), adds=[] if jj == 0 else [gd[g][0]])
                gs = slice(g * GRP, (g + 1) * GRP)
                K.act.op(lambda e: e.activation(out=GT[:, 3, gs], in_=GT[:, 2, gs], func=AF.Gelu), reads=[gd[g][0]], writes=[gd[g][1]])

            def stage_b(g):
                gs = slice(g * GRP, (g + 1) * GRP)
                K.dve.op(lambda e: e.tensor_tensor(out=GT[:, 3, gs], in0=GT[:, 3, gs], in1=GT[:, 1, gs], op=ALU.mult), reads=[dGT], writes=[gd[g][1]])
                for jj in range(GRP):
                    j = g * GRP + jj
                    GB, dGB = bufs.pop(j)
                    DG, dDG = DGr.next()
                    K.act.op(lambda e, DG=DG, j=j: e.activation(out=DG[:], in_=identb[:], func=AF.Copy, scale=GT[:, 3, j:j + 1]),
                             reads=[gd[g][1], d_c], writes=[dDG])
                    for n in range(2):
                        K.pe.op(lambda e, DG=DG, GB=GB, n=n, j=j: e.matmul(
                            py[:, n, :], DG[:], GB[:, D + n * 512:D + (n + 1) * 512], start=(j == 0), stop=(j == 127)),
                            reads=[dDG, dGB], writes=[dpy] if (j == 0 and n == 0) else [], adds=[] if (j == 0 and n == 0) else [dpy])

            gen = routing(R["next"]) if R.get("next") is not None else None
            for g in range(NGRP):
                stage_a(g)
                if g >= 1:
                    stage_b(g - 1)
                if gen is not None and g >= 2 and g % 2 == 0:
                    try:
                        next(gen)
                    except StopIteration:
                        gen = None
            stage_b(NGRP - 1)
            if gen is not None:
                for _ in gen:
                    pass
            K.dve.op(lambda e: e.tensor_tensor(out=X1[:], in0=X1[:], in1=py[:].rearrange("p a b -> p (a b)"), op=ALU.add),
                     reads=[dpy], writes=[dX1])
            K.act.op(lambda e: e.activation(out=junk2[:], in_=X1[:], func=AF.Square, accum_out=S8[:, 0:1]),
                     reads=[dX1], writes=[d_junk2, dS8])
            K.dve.op(lambda e: e.tensor_scalar(out=S8[:, 1:2], in0=S8[:, 0:1], scalar1=1.0 / D, scalar2=1e-6,
                                               op0=ALU.mult, op1=ALU.add), writes=[dS8])
            K.act.op(lambda e: e.activation(out=S8[:, 2:3], in_=S8[:, 1:2], func=AF.Sqrt), writes=[dS8])
            K.dve.op(lambda e: e.reciprocal(out=S8[:, 3:4], in_=S8[:, 2:3]), writes=[dS8])
            K.dve.op(lambda e: e.scalar_tensor_tensor(
                out=XH[:], in0=X1[:], scalar=S8[:, 3:4], in1=FNW[:], op0=ALU.mult, op1=ALU.mult),
                reads=[dX1, dS8, d_c], writes=[dXH])
            K.sp.dma(lambda e: e.dma_start(out=io["out"][t0:t0 + 128, :], in_=XH[:]), reads=[dXH])

        for _ in routing(0):
            pass
        for ti in range(NTT):
            R = RES.pop(ti)
            R["next"] = ti + 1 if ti + 1 < NTT else None
            expert(R)


def make_consts():
    c = {}
    c["ident_bf"] = np.eye(128, dtype=np.float32).astype(ml_dtypes.bfloat16)
    c["ident_f"] = np.eye(128, dtype=np.float32)
    bo = np.zeros((128, 128), np.float32)
    bo[:64, :64] = 1.0 / 64
    bo[64:, 64:] = 1.0 / 64
    c["blockones"] = bo
    pp = np.arange(128)[:, None]
    ff = np.arange(128)[None, :]
    c["tri"] = ((pp <= ff) & (pp // 64 == ff // 64)).astype(np.float32)
    p6 = np.arange(64)[:, None]
    f6 = np.arange(64)[None, :]
    mk = np.zeros((64, 3, 64), np.float32)
    mk[:, 0, :] = (f6 < p6)
    mk[:, 1, :] = (f6 > p6)
    mk[:, 2, :] = (f6 >= p6)
    c["masks"] = mk
    c["ident64"] = np.eye(64, dtype=np.float32).astype(ml_dtypes.bfloat16)
    c["ones64"] = np.ones((64, 1), np.float32)
    c["iota16"] = np.tile(np.arange(16, dtype=np.float32)[None, :], (128, 1))
    return c


def make_alibi(S):
    al = np.zeros((4, 128, S), np.float32)
    ql = np.arange(128)[:, None]
    m = np.arange(S)[None, :]
    for h in range(4):
        slope = 2.0 ** (-8.0 * (h + 1) / 4)
        v = -slope * (ql - m + (S - 128)).astype(np.float32)
        al[h] = np.where(m <= ql + (S - 128), v, -30000.0)
    return al


def _unused():
    c = {}
    return c


def build(cfg):
    NB, S = cfg["NB"], cfg["S"]
    T = NB * S
    cfg["T"] = T
    dbg = set(cfg.get("debug", ()))
    phases = cfg.get("phases", (1,))
    nc = bass.Bass("TRN2", target_bir_lowering=False)
    io = {}

    def inp(name, shape, dt=F32):
        io[name] = nc.dram_tensor(name, list(shape), dt, kind="ExternalInput").ap()

    inp("x", [T, D])
    inp("norm_mix_w", [1, D])
    inp("w_in", [1, D, IN_COLS])
    inp("ident_bf", [128, 128], BF16)
    inp("ident_f", [128, 128])
    inp("blockones", [128, 128])
    inp("tri", [128, 128])
    inp("masks", [64, 3, 64])
    inp("ident64", [64, 64], BF16)
    inp("ones64", [64, 1])
    inp("alibi", [4, 128, S])
    for nm, shp in [("lam_q1", [1, 64]), ("lam_k1", [1, 64]), ("lam_q2", [1, 64]), ("lam_k2", [1, 64]),
                    ("subln_w", [1, 128])]:
        inp(nm, shp)
    scr = {}

    def scratch(name, shape, dt):
        kind = "ExternalOutput" if name in dbg else "Internal"
        scr[name] = nc.dram_tensor(name, list(shape), dt, kind=kind).ap()

    scratch("zs_tm", [T, SHIFT_COLS], F32)
    scratch("zv_fm", [512, T], F32)
    scratch("qk_fm", [1024, T], BF16)
    scratch("av_tm", [T, 512], BF16)
    scratch("sg_fm", [2048, T], BF16)
    if not cfg.get("chunked", True):
        scratch("rw_tm", [T, 5, 512], F32)
    scratch("ab_tm", [T, 5, 512], BF16)
    scratch("lw_tm", [T, 512], F32)
    scratch("v_fm", [512, T], F32)
    scratch("g_fm", [512, T], F32)
    scratch("coef_fm", [8, T], F32)
    scratch("y_fm", [512, T], F32)
    scratch("ya_fm", [512, T], BF16)
    scr["d_p1"] = Dep()
    scr["d_p2a"] = Dep()
    scr["d_p2b"] = Dep()
    scr["d_p2c"] = Dep()
    scr["d_p3"] = Dep()
    scr["d_p4"] = Dep()
    scr["d_uv"] = Dep()
    scratch("uv_tab", [16384, 2 * D], BF16)
    if "ids_dbg" in dbg:
        scratch("ids_dbg", [T, 128], I32)
        scratch("gate_dbg", [T, 128], F32)
    inp("peer_wq", [1, D, 2048])
    inp("peer_keys", [1, 8, 2, 128, 128])
    inp("peer_u", [1, 16384, D])
    inp("peer_v", [1, 16384, D])
    inp("final_norm_w", [1, D])
    inp("iota16", [128, 16])
    io["out"] = nc.dram_tensor("out", [T, D], F32, kind="ExternalOutput").ap()
    scratch("x1_tm", [T, D], F32)
    scratch("xh_tm", [T, D], F32)
    scratch("xhT_fm", [D, T], BF16)
    for nm, shp in [("proj_a", [1, 512, D]), ("proj_b", [1, 512, D]), ("w_out", [1, D, D]), ("norm_ffn_w", [1, D])]:
        inp(nm, shp)
    scratch("yb_fm", [512, T], BF16)
    for nm, shp in [("shift_mu", [1, SHIFT_COLS]), ("w0", [1, 512]), ("w2", [1, 64, 512]), ("a0", [1, 512]),
                    ("a2", [1, 64, 512]), ("g2", [1, 128, 512]), ("k_k", [1, 512]), ("k_a", [1, 512]),
                    ("r_k", [1, 8, 64]), ("lnx_w", [1, 512]), ("lnx_b", [1, 512])]:
        inp(nm, shp)
    with ExitStack() as es:
        K = Kern(nc, es, pool_slots=cfg.get("pool_slots", 8))
        K.scopes = bool(cfg.get("scopes", False))
        if 5 in phases:
            K.phase = "p0_uvtab"
            RB = 2048
            for r0 in range(0, 16384, RB):
                K.pool.dma(lambda e, r0=r0: e.dma_start(out=scr["uv_tab"][r0:r0 + RB, 0:D], in_=io["peer_u"][0, r0:r0 + RB, :]),
                           adds=[scr["d_uv"]])
                K.pool.dma(lambda e, r0=r0: e.dma_start(out=scr["uv_tab"][r0:r0 + RB, D:2 * D], in_=io["peer_v"][0, r0:r0 + RB, :]),
                           adds=[scr["d_uv"]])
        if 1 in phases:
            K.phase = "p1_inproj"
            phase1(K, cfg, io, scr)
            K.barrier()
        if 2 in phases:
            K.phase = "p2a_prep"
            phase2_prep(K, cfg, io, scr)
            K.barrier()
            K.phase = "p2b_scan"
            if cfg.get("chunked", True):
                phase2_chunk(K, cfg, io, scr)
            else:
                phase2_scan(K, cfg, io, scr)
            K.barrier()
            K.phase = "p2c_post"
            phase2_post(K, cfg, io, scr)
            K.barrier()
        if 3 in phases:
            K.phase = "p3_attn"
            phase3(K, cfg, io, scr)
            K.barrier()
        if 4 in phases:
            K.phase = "p4_merge"
            phase4(K, cfg, io, scr)
            K.barrier()
        if 5 in phases:
            K.phase = "p5_peer"
            phase5(K, cfg, io, scr)
            K.barrier()
        K.finish()
    return nc, io, scr


def kernel(**inputs):
    NB, S = 4, 2048
    cfg = dict(NB=NB, S=S, phases=(1, 2, 3, 4, 5))
    nc, io, scr = build(cfg)
    consts = make_consts()
    consts["alibi"] = make_alibi(S)
    x = np.ascontiguousarray(np.asarray(inputs["x"], dtype=np.float32))
    shared = {}
    for name in io:
        if name in ("x", "out"):
            continue
        if name in consts:
            shared[name] = consts[name]
        elif name == "final_norm_w":
            shared[name] = np.ascontiguousarray(np.asarray(inputs[name], dtype=np.float32).reshape(1, D))
        else:
            shared[name] = np.ascontiguousarray(np.asarray(inputs[name], dtype=np.float32))
    in_maps = []
    for c in range(NCORES):
        m = dict(shared)
        m["x"] = x[c * NB:(c + 1) * NB].reshape(NB * S, D)
        in_maps.append(m)
    res = run_bass_kernel_spmd(nc, in_maps, core_ids=list(range(NCORES)))
    out = np.concatenate([np.asarray(r["out"]).reshape(NB, S, D) for r in res.results], axis=0)
    return out.astype(np.float32)
```

```python
import numpy as np
import ml_dtypes
from contextlib import ExitStack
import concourse.bass as bass
import concourse.mybir as mybir
from concourse.bass_utils import run_bass_kernel_spmd

F32 = mybir.dt.float32
BF16 = mybir.dt.bfloat16
I32 = mybir.dt.int32
U32 = mybir.dt.uint32
ALU = mybir.AluOpType
AF = mybir.ActivationFunctionType
AX = mybir.AxisListType

D = 1024
IN_COLS = 5376
SHIFT_COLS = 1792
NCORES = 8


class Dep:
    __slots__ = ("w", "r", "pw", "pr")

    def __init__(self):
        self.w = {}
        self.r = {}
        self.pw = {}
        self.pr = {}


class Stream:
    def __init__(self, K, name, is_pe=False, ndma=0):
        self.K = K
        self.name = name
        self.sem = K.new_sem("s_" + name)
        self.cnt = 0
        self.items = []
        self.waited = {}
        self.is_pe = is_pe
        self.dsems = [K.new_sem("d_%s%d" % (name, i)) for i in range(ndma)]
        self.duses = [0] * ndma
        self.dj = 0

    def wait_tok(self, tok):
        if tok is None:
            return
        sem, val = tok
        if sem is self.sem and self.is_pe:
            return
        key = id(sem)
        if self.waited.get(key, 0) >= val:
            return
        self.waited[key] = val
        self.items.append(("w", sem, val, self.K.phase))

    def _pre(self, reads, writes, adds):
        for d in reads:
            for t in list(d.w.values()):
                self.wait_tok(t)
        for d in writes:
            for t in list(d.w.values()):
                self.wait_tok(t)
            for t in list(d.r.values()):
                self.wait_tok(t)
        for d in adds:
            for t in list(d.r.values()) + list(d.pr.values()) + list(d.pw.values()):
                self.wait_tok(t)

    def _post(self, tok, reads, writes, adds):
        for d in reads:
            d.r[id(tok[0])] = tok
        for d in writes:
            d.pw = d.w
            d.pr = d.r
            d.w = {id(tok[0]): tok}
            d.r = {}
        for d in adds:
            d.w[id(tok[0])] = tok

    def op(self, fn, reads=(), writes=(), adds=()):
        self._pre(reads, writes, adds)
        self.cnt += 1
        tok = (self.sem, self.cnt)
        self.items.append(("o", fn, self.sem, 1, self.K.phase))
        self._post(tok, reads, writes, adds)
        return tok

    def dma(self, fn, reads=(), writes=(), adds=()):
        self._pre(reads, writes, adds)
        n = len(self.dsems)
        slot = self.dj % n
        self.dj += 1
        if self.duses[slot] > 0:
            self.wait_tok((self.dsems[slot], 16 * self.duses[slot]))
        self.duses[slot] += 1
        tok = (self.dsems[slot], 16 * self.duses[slot])
        self.items.append(("o", fn, self.dsems[slot], 16, self.K.phase))
        self._post(tok, reads, writes, adds)
        return tok

    def replay(self, eng):
        nc = self.K.nc
        cur = None
        ctx = None
        for it in self.items:
            ph = it[-1]
            if self.K.scopes and ph != cur:
                if ctx is not None:
                    ctx.__exit__(None, None, None)
                ctx = nc.named_scope(ph)
                ctx.__enter__()
                cur = ph
            if it[0] == "w":
                eng.wait_ge(it[1], it[2])
            else:
                ins = it[1](eng)
                ins.then_inc(it[2], it[3])
        if ctx is not None:
            ctx.__exit__(None, None, None)


class Kern:
    def __init__(self, nc, es, pool_slots=8):
        self.nc = nc
        self.es = es
        self.nsem = 0
        self.phase = "init"
        self.scopes = False
        self.pe = Stream(self, "pe", is_pe=True)
        self.act = Stream(self, "act", ndma=4)
        self.dve = Stream(self, "dve")
        self.pool = Stream(self, "pool", ndma=pool_slots)
        self.sp = Stream(self, "sp", ndma=8)
        self.uid = 0

    def new_sem(self, name):
        self.nsem += 1
        return self.es.enter_context(self.nc.semaphore(name))

    def sb(self, shape, dt, name=None, es=None):
        self.uid += 1
        nm = "%s_%d" % (name or "t", self.uid)
        return (es or self.es).enter_context(self.nc.sbuf_tensor(nm, list(shape), dt))

    def ps(self, shape, dt, name=None, es=None):
        self.uid += 1
        nm = "%s_%d" % (name or "p", self.uid)
        return (es or self.es).enter_context(self.nc.psum_tensor(nm, list(shape), dt))

    def dram(self, name, shape, dt, kind="Internal"):
        return self.nc.dram_tensor(name, list(shape), dt, kind=kind)

    def streams(self):
        return [self.pe, self.act, self.dve, self.pool, self.sp]

    def barrier(self):
        st = self.streams()
        toks = []
        for q in st:
            if q.cnt > 0:
                toks.append((q.sem, q.cnt))
            for i, sem in enumerate(q.dsems):
                if q.duses[i] > 0:
                    toks.append((sem, 16 * q.duses[i]))
        for s_ in st:
            for t in toks:
                s_.wait_tok(t)

    def finish(self):
        streams = [self.pe, self.act, self.dve, self.pool, self.sp]
        for s in streams:
            for q in streams:
                for i, sem in enumerate(q.dsems):
                    if q.duses[i] > 0:
                        s.wait_tok((sem, 16 * q.duses[i]))
        with self.nc.allow_non_contiguous_dma(reason="small strided param loads"), self.nc.Block() as block:
            @block.tensor
            def _(e):
                self.pe.replay(e)

            @block.scalar
            def _(e):
                self.act.replay(e)

            @block.vector
            def _(e):
                self.dve.replay(e)

            @block.gpsimd
            def _(e):
                self.pool.replay(e)

            @block.sync
            def _(e):
                self.sp.replay(e)


class Rot:
    def __init__(self, K, n, shape, dt, name, psum=False, es=None):
        self.t = [(K.ps if psum else K.sb)(shape, dt, name, es=es) for _ in range(n)]
        self.d = [Dep() for _ in range(n)]
        self.i = 0

    def next(self):
        j = self.i % len(self.t)
        self.i += 1
        return self.t[j], self.d[j]


def phase1(K, cfg, io, scr):
    nc = K.nc
    T = cfg["T"]
    NT = T // 512
    with ExitStack() as es:
        ident = K.sb([128, 128], BF16, "ident", es)
        d_ident = Dep()
        K.sp.dma(lambda e: e.dma_start(out=ident[:], in_=io["ident_bf"][:, :]), writes=[d_ident])
        nw = K.sb([128, 8], F32, "nw", es)
        d_nw = Dep()
        K.sp.dma(lambda e: e.dma_start(out=nw[:], in_=io["norm_mix_w"].rearrange("o (c p) -> p (o c)", p=128)),
                 writes=[d_nw])
        wt = K.sb([128, 8, IN_COLS], BF16, "wt", es)
        d_wt = Dep()
        wst = Rot(K, 2, [128, 1344], F32, "wst", es=es)
        q = 0
        for kc in range(8):
            for cp in range(4):
                st, dst = wst.next()
                eng = K.sp if q % 2 == 0 else K.pool
                q += 1
                eng.dma(lambda e, st=st, kc=kc, cp=cp: e.dma_start(
                    out=st[:], in_=io["w_in"][0, kc * 128:(kc + 1) * 128, cp * 1344:(cp + 1) * 1344]), writes=[dst])
                K.act.op(lambda e, st=st, kc=kc, cp=cp: e.activation(
                    out=wt[:, kc, cp * 1344:(cp + 1) * 1344], in_=st[:], func=AF.Copy, scale=nw[:, kc:kc + 1]),
                    reads=[dst, d_nw], writes=[d_wt])

        xs = Rot(K, 2, [128, D], F32, "xs", es=es)
        junk = K.sb([128, D], BF16, "junk", es)
        d_junk = Dep()
        xn = Rot(K, 2, [128, D], BF16, "xn", es=es)
        st4 = Rot(K, 4, [128, 4], F32, "st4", es=es)
        hT = Rot(K, 2, [128, 8, 512], BF16, "hT", es=es)
        ptr = Rot(K, 2, [128, 8, 128], BF16, "ptr", psum=True, es=es)
        pmm = Rot(K, 4, [128, 512], F32, "pmm", psum=True, es=es)
        o32 = Rot(K, 3, [128, 512], F32, "o32", es=es)
        o16 = Rot(K, 3, [128, 512], BF16, "o16", es=es)
        ev = [0]

        def evac(pt, pd, ncols, kind, dst_ap):
            if kind == "f32":
                ot, od = o32.next()
            else:
                ot, od = o16.next()
            use_act = (kind == "sig") or (ev[0] % 2 == 0)
            ev[0] += 1
            if kind == "sig":
                K.act.op(lambda e: e.activation(out=ot[:, :ncols], in_=pt[:, :ncols], func=AF.Sigmoid),
                         reads=[pd], writes=[od])
            elif use_act:
                K.act.op(lambda e: e.activation(out=ot[:, :ncols], in_=pt[:, :ncols], func=AF.Copy),
                         reads=[pd], writes=[od])
            else:
                K.dve.op(lambda e: e.tensor_copy(out=ot[:, :ncols], in_=pt[:, :ncols]), reads=[pd], writes=[od])
            K.sp.dma(lambda e: e.dma_start(out=dst_ap, in_=ot[:, :ncols]), reads=[od], adds=[scr["d_p1"]])

        for ti in range(NT):
            h_t, h_d = hT.next()
            for sub in range(4):
                t0 = ti * 512 + sub * 128
                x_t, x_d = xs.next()
                K.pool.dma(lambda e, x_t=x_t, t0=t0: e.dma_start(out=x_t[:], in_=io["x"][t0:t0 + 128, :]),
                           writes=[x_d])
                s_t, s_d = st4.next()
                K.dve.op(lambda e, s_t=s_t: e.memset(s_t[:], 0.0), writes=[s_d])
                K.act.op(lambda e, x_t=x_t, s_t=s_t: e.activation(out=junk[:], in_=x_t[:], func=AF.Square,
                                                                    accum_out=s_t[:, 0:1]),
                         reads=[x_d], writes=[d_junk, s_d])
                K.dve.op(lambda e, s_t=s_t: e.tensor_scalar(out=s_t[:, 1:2], in0=s_t[:, 0:1], scalar1=1.0 / D,
                                                            scalar2=1e-6, op0=ALU.mult, op1=ALU.add),
                         reads=[s_d], writes=[s_d])
                K.act.op(lambda e, s_t=s_t: e.activation(out=s_t[:, 3:4], in_=s_t[:, 1:2], func=AF.Sqrt),
                         reads=[s_d], writes=[s_d])
                K.dve.op(lambda e, s_t=s_t: e.reciprocal(out=s_t[:, 2:3], in_=s_t[:, 3:4]),
                         reads=[s_d], writes=[s_d])
                n_t, n_d = xn.next()
                K.dve.op(lambda e, x_t=x_t, s_t=s_t, n_t=n_t: e.tensor_scalar(
                    out=n_t[:], in0=x_t[:], scalar1=s_t[:, 2:3], scalar2=None, op0=ALU.mult),
                    reads=[x_d, s_d], writes=[n_d])
                p_t, p_d = ptr.next()
                for kc in range(8):
                    K.pe.op(lambda e, p_t=p_t, n_t=n_t, kc=kc: e.transpose(
                        out=p_t[:, kc, :], in_=n_t[:, kc * 128:(kc + 1) * 128], identity=ident[:]),
                        reads=[n_d, d_ident], writes=[p_d])
                K.act.op(lambda e, p_t=p_t, h_t=h_t, sub=sub: e.activation(
                    out=h_t[:, :, sub * 128:(sub + 1) * 128], in_=p_t[:], func=AF.Copy),
                    reads=[p_d], writes=[h_d])
            tsl = slice(ti * 512, (ti + 1) * 512)
            for sub in range(4):
                r0 = ti * 512 + sub * 128
                for (c0, ncols, kind, name, dc0) in [(0, 512, "f32", "zs_tm", 0), (512, 512, "f32", "zs_tm", 512),
                                                    (1024, 512, "f32", "zs_tm", 1024),
                                                    (1536, 256, "f32", "zs_tm", 1536),
                                                    (2816, 512, "bf16", "av_tm", 0)]:
                    pt, pd = pmm.next()
                    for kc in range(8):
                        K.pe.op(lambda e, pt=pt, kc=kc, sub=sub, c0=c0, ncols=ncols, h_t=h_t: e.matmul(
                            pt[:, :ncols], h_t[:, kc, sub * 128:(sub + 1) * 128], wt[:, kc, c0:c0 + ncols],
                            start=(kc == 0), stop=(kc == 7)), reads=[h_d, d_wt], writes=[pd])
                    evac(pt, pd, ncols, kind, scr[name][r0:r0 + 128, dc0:dc0 + ncols])
            fm = []
            for j in range(8):
                fm.append((1792 + j * 128, "bf16", "qk_fm", j * 128))
            for j in range(16):
                fm.append((3328 + j * 128, "sig", "sg_fm", j * 128))
            for (c0, kind, name, r0) in fm:
                pt, pd = pmm.next()
                for kc in range(8):
                    K.pe.op(lambda e, pt=pt, kc=kc, c0=c0, h_t=h_t: e.matmul(
                        pt[:, :], wt[:, kc, c0:c0 + 128], h_t[:, kc, :], start=(kc == 0), stop=(kc == 7)),
                        reads=[h_d, d_wt], writes=[pd])
                evac(pt, pd, 512, kind, scr[name][r0:r0 + 128, tsl])


def dap(apobj, offset, dims):
    return bass.AP(tensor=apobj.tensor, offset=offset, ap=[list(d) for d in dims])


def bcast_load(K, eng, dst, src_row_ap, n, dep):
    eng.dma(lambda e: e.dma_start(out=dst, in_=src_row_ap.broadcast_to([128, n])), writes=[dep])


def phase2_prep(K, cfg, io, scr):
    T, S, NB = cfg["T"], cfg["S"], cfg["NB"]
    NTT = T // 128
    with ExitStack() as es:
        identb = K.sb([128, 128], BF16, "identb", es)
        identf = K.sb([128, 128], F32, "identf", es)
        d_c = Dep()
        K.sp.dma(lambda e: e.dma_start(out=identb[:], in_=io["ident_bf"][:, :]), adds=[d_c])
        K.sp.dma(lambda e: e.dma_start(out=identf[:], in_=io["ident_f"][:, :]), adds=[d_c])
        MU = K.sb([128, SHIFT_COLS], F32, "MU", es)
        PR = K.sb([128, 5, 512], F32, "PR", es)
        K.sp.dma(lambda e: e.dma_start(out=MU[:], in_=io["shift_mu"][0:1, :].broadcast_to([128, SHIFT_COLS])), adds=[d_c])
        for j, nm in enumerate(["w0", "a0", "k_k", "k_a"]):
            K.pool.dma(lambda e, j=j, nm=nm: e.dma_start(out=PR[:, j, :], in_=io[nm][0:1, :].broadcast_to([128, 512])),
                       adds=[d_c])
        K.pool.dma(lambda e: e.dma_start(out=PR[:, 4, :], in_=io["r_k"].rearrange("o h k -> o (h k)").broadcast_to([128, 512])),
                   adds=[d_c])
        cst = K.sb([128, 2], F32, "cst", es)
        K.dve.op(lambda e: e.memset(cst[:, 0:1], 1.0), adds=[d_c])
        K.dve.op(lambda e: e.memset(cst[:, 1:2], -0.5), adds=[d_c])
        wst = K.sb([128, 3, 512], F32, "lwst", es)
        d_wst = Dep()
        K.dve.op(lambda e: e.memset(wst[:], 0.0), writes=[d_wst])
        K.sp.dma(lambda e: e.dma_start(out=wst[0:64, 0, :], in_=io["w2"][0, :, :]), reads=[d_wst], adds=[d_wst])
        K.sp.dma(lambda e: e.dma_start(out=wst[64:128, 1, :], in_=io["a2"][0, :, :]), reads=[d_wst], adds=[d_wst])
        K.sp.dma(lambda e: e.dma_start(out=wst[:, 2, :], in_=io["g2"][0, :, :]), reads=[d_wst], adds=[d_wst])
        LW = K.sb([128, 3, 512], BF16, "LW", es)
        K.dve.op(lambda e: e.tensor_copy(out=LW[:], in_=wst[:]), reads=[d_wst], adds=[d_c])

        Zr = Rot(K, 2, [128, SHIFT_COLS], F32, "Z", es=es)
        Zpr = Rot(K, 2, [128, SHIFT_COLS], F32, "Zp", es=es)
        ZSr = Rot(K, 2, [128, SHIFT_COLS], F32, "ZS", es=es)
        OUTr = Rot(K, 2, [128, 5, 512], F32, "OUT", es=es)
        Er = Rot(K, 2, [128, 192], F32, "E", es=es)
        Lr = Rot(K, 2, [128, 256], BF16, "L", es=es)
        LTr = Rot(K, 2, [128, 2, 128], BF16, "LT", es=es)
        Ur = Rot(K, 2, [128, 512], F32, "U", es=es)
        UAr = Rot(K, 2, [128, 512], F32, "UA", es=es)
        KKr = Rot(K, 2, [128, 512], F32, "KKt", es=es)
        SQr = Rot(K, 2, [128, 512], F32, "SQ", es=es)
        T1r = Rot(K, 2, [128, 512], F32, "T1", es=es)
        T2r = Rot(K, 2, [128, 512], F32, "T2", es=es)
        S8r = Rot(K, 2, [128, 4, 8], F32, "S8", es=es)
        VTr = Rot(K, 2, [128, 4, 128], F32, "VT", es=es)
        GTr = Rot(K, 2, [128, 4, 128], F32, "GT", es=es)
        CTr = Rot(K, 2, [8, 128], F32, "CT", es=es)
        PT = Rot(K, 1, [128, 2, 128], BF16, "PT", psum=True, es=es)
        PW = Rot(K, 1, [128, 512], F32, "PW", psum=True, es=es)
        PA = Rot(K, 1, [128, 512], F32, "PA", psum=True, es=es)
        PG = Rot(K, 1, [128, 4, 128], F32, "PG", psum=True, es=es)
        PV = Rot(K, 1, [128, 4, 128], F32, "PV", psum=True, es=es)
        PC = Rot(K, 1, [8, 128], F32, "PC", psum=True, es=es)
        chunked = cfg.get("chunked", True)
        if chunked:
            PL = Rot(K, 1, [128, 512], F32, "PL", psum=True, es=es)
            TRI = K.sb([128, 128], F32, "TRI", es)
            K.sp.dma(lambda e: e.dma_start(out=TRI[:], in_=io["tri"][:, :]), adds=[d_c])
            LWr = Rot(K, 2, [128, 512], F32, "LWt", es=es)
            ELr = Rot(K, 2, [128, 3, 512], F32, "EL", es=es)
            ABr = Rot(K, 2, [128, 5, 512], BF16, "AB", es=es)
        dq = [0]

        def ldq():
            dq[0] += 1
            return K.sp if dq[0] % 2 == 0 else K.pool

        def tile_gen(ti):
            t0 = ti * 128
            first = (t0 % S == 0)
            Z, dZ = Zr.next()
            Zp, dZp = Zpr.next()
            ZS, dZS = ZSr.next()
            OUT, dO = OUTr.next()
            ldq().dma(lambda e, Z=Z, t0=t0: e.dma_start(out=Z[:], in_=scr["zs_tm"][t0:t0 + 128, :]),
                      reads=[scr["d_p1"]], writes=[dZ])
            if first:
                K.pool.op(lambda e, Zp=Zp: e.memset(Zp[0:32, :], 0.0), writes=[dZp])
                ldq().dma(lambda e, Zp=Zp, t0=t0: e.dma_start(out=Zp[1:128, :], in_=scr["zs_tm"][t0:t0 + 127, :]),
                          reads=[scr["d_p1"], dZp], adds=[dZp])
            else:
                ldq().dma(lambda e, Zp=Zp, t0=t0: e.dma_start(out=Zp[:], in_=scr["zs_tm"][t0 - 1:t0 + 127, :]),
                          reads=[scr["d_p1"]], writes=[dZp])
            CS = 1216
            dZSa, dZSb = Dep(), Dep()
            K.dve.op(lambda e, Z=Z, Zp=Zp, ZS=ZS: e.tensor_tensor(out=ZS[:, :CS], in0=Zp[:, :CS], in1=Z[:, :CS], op=ALU.subtract),
                     reads=[dZ, dZp], writes=[dZS])
            K.pool.op(lambda e, Z=Z, Zp=Zp, ZS=ZS: e.tensor_tensor(out=ZS[:, CS:], in0=Zp[:, CS:], in1=Z[:, CS:], op=ALU.subtract),
                      reads=[dZ, dZp, dZS], writes=[dZSb])
            K.dve.op(lambda e, ZS=ZS: e.tensor_tensor(out=ZS[:, :CS], in0=ZS[:, :CS], in1=MU[:, :CS], op=ALU.mult),
                     reads=[d_c, dZS], writes=[dZSa])
            K.pool.op(lambda e, ZS=ZS: e.tensor_tensor(out=ZS[:, CS:], in0=ZS[:, CS:], in1=MU[:, CS:], op=ALU.mult),
                      reads=[d_c], writes=[dZSb])
            K.dve.op(lambda e, Z=Z, ZS=ZS: e.tensor_tensor(out=ZS[:, :CS], in0=ZS[:, :CS], in1=Z[:, :CS], op=ALU.add),
                     reads=[dZ], writes=[dZSa])
            K.pool.op(lambda e, Z=Z, ZS=ZS: e.tensor_tensor(out=ZS[:, CS:], in0=ZS[:, CS:], in1=Z[:, CS:], op=ALU.add),
                      reads=[dZ], writes=[dZSb])
            K.dve.op(lambda e, ZS=ZS: e.tensor_copy(out=ZS[:, 0:1], in_=ZS[:, 0:1]), reads=[dZSa, dZSb], writes=[dZS])
            yield
            r_ap = ZS[:, 0:512]
            k_ap = ZS[:, 512:1024]
            K.act.op(lambda e, OUT=OUT, ZS=ZS: e.activation(out=OUT[:, 4, :], in_=ZS[:, 0:512], func=AF.Copy),
                     reads=[dZS], writes=[dO])
            yield
            E, dE = Er.next()
            L, dL = Lr.next()
            K.act.op(lambda e, E=E, ZS=ZS: e.activation(out=E[:, 0:64], in_=ZS[:, 1536:1600], func=AF.Exp, scale=-2.0),
                     reads=[dZS], writes=[dE])
            K.act.op(lambda e, E=E, ZS=ZS: e.activation(out=E[:, 64:192], in_=ZS[:, 1664:1792], func=AF.Exp, scale=-1.0),
                     reads=[dZS], adds=[dE])
            K.act.op(lambda e, E=E: e.activation(out=E[:], in_=E[:], func=AF.Ln, bias=cst[:, 0:1]), reads=[d_c], writes=[dE])
            K.act.op(lambda e, E=E: e.activation(out=E[:], in_=E[:], func=AF.Exp, scale=-1.0), writes=[dE])
            yield
            K.dve.op(lambda e, E=E, L=L: e.tensor_scalar(out=L[:, 0:64], in0=E[:, 0:64], scalar1=2.0, scalar2=-1.0,
                                                        op0=ALU.mult, op1=ALU.add), reads=[dE], writes=[dL])
            K.act.op(lambda e, L=L, ZS=ZS: e.activation(out=L[:, 64:128], in_=ZS[:, 1600:1664], func=AF.Copy),
                     reads=[dZS, dL], adds=[dL])
            K.act.op(lambda e, L=L, E=E: e.activation(out=L[:, 128:256], in_=E[:, 64:192], func=AF.Copy),
                     reads=[dE, dL], adds=[dL])
            yield
            pt, dpt = PT.next()
            K.pe.op(lambda e, pt=pt, L=L: e.transpose(out=pt[:, 0, :], in_=L[:, 0:128], identity=identb[:]),
                    reads=[dL, d_c], writes=[dpt])
            K.pe.op(lambda e, pt=pt, L=L: e.transpose(out=pt[:, 1, :], in_=L[:, 128:256], identity=identb[:]),
                    reads=[dL, d_c], adds=[dpt])
            LT, dLT = LTr.next()
            K.act.op(lambda e, pt=pt, LT=LT: e.activation(out=LT[:], in_=pt[:], func=AF.Copy), reads=[dpt], writes=[dLT])
            yield "pre_pw"
            pw, dpw = PW.next()
            pa, dpa = PA.next()
            pg, dpg = PG.next()
            K.pe.op(lambda e, pw=pw, LT=LT: e.matmul(pw[:], LT[:, 0, :], LW[:, 0, :], start=True, stop=True),
                    reads=[dLT, d_c], writes=[dpw])
            K.pe.op(lambda e, pa=pa, LT=LT: e.matmul(pa[:], LT[:, 0, :], LW[:, 1, :], start=True, stop=True),
                    reads=[dLT, d_c], writes=[dpa])
            for j in range(4):
                K.pe.op(lambda e, pg=pg, LT=LT, j=j: e.matmul(pg[:, j, :], LW[:, 2, j * 128:(j + 1) * 128], LT[:, 1, :],
                                                             start=True, stop=True),
                        reads=[dLT, d_c], writes=[dpg] if j == 0 else [], adds=[] if j == 0 else [dpg])
            GT, dGT = GTr.next()
            K.act.op(lambda e, pg=pg, GT=GT: e.activation(out=GT[:], in_=pg[:], func=AF.Copy), reads=[dpg], writes=[dGT])
            K.sp.dma(lambda e, GT=GT, t0=t0: e.dma_start(
                out=dap(scr["g_fm"], t0, [[T, 128], [128 * T, 4], [1, 128]]), in_=GT[:]),
                reads=[dGT], adds=[scr["d_p2a"]])
            yield
            U, dU = Ur.next()
            K.dve.op(lambda e, U=U, pw=pw: e.tensor_tensor(out=U[:], in0=pw[:], in1=PR[:, 0, :], op=ALU.add),
                     reads=[dpw, d_c], writes=[dU])
            yield
            K.act.op(lambda e, U=U: e.activation(out=U[:], in_=U[:], func=AF.Exp, scale=-1.0), writes=[dU])
            K.act.op(lambda e, U=U: e.activation(out=U[:], in_=U[:], func=AF.Ln, bias=cst[:, 0:1]), reads=[d_c], writes=[dU])
            yield
            K.act.op(lambda e, U=U: e.activation(out=U[:], in_=U[:], func=AF.Exp, scale=-1.0, bias=cst[:, 1:2]),
                     reads=[d_c], writes=[dU])
            K.act.op(lambda e, U=U, OUT=OUT: e.activation(out=OUT[:, 0, :], in_=U[:], func=AF.Exp, scale=-1.0),
                     reads=[dU], adds=[dO])
            yield
            UA, dUA = UAr.next()
            K.dve.op(lambda e, UA=UA, pa=pa: e.tensor_tensor(out=UA[:], in0=pa[:], in1=PR[:, 1, :], op=ALU.add),
                     reads=[dpa, d_c], writes=[dUA])
            yield
            K.act.op(lambda e, UA=UA: e.activation(out=UA[:], in_=UA[:], func=AF.Exp, scale=-1.0), writes=[dUA])
            K.act.op(lambda e, UA=UA: e.activation(out=UA[:], in_=UA[:], func=AF.Ln, bias=cst[:, 0:1]), reads=[d_c], writes=[dUA])
            K.act.op(lambda e, UA=UA: e.activation(out=UA[:], in_=UA[:], func=AF.Exp, scale=-1.0), writes=[dUA])
            yield "post_a"
            KKt, dKK = KKr.next()
            SQ, dSQ = SQr.next()
            S8, dS8 = S8r.next()
            K.dve.op(lambda e, KKt=KKt, ZS=ZS: e.tensor_tensor(out=KKt[:], in0=ZS[:, 512:1024], in1=PR[:, 2, :], op=ALU.mult),
                     reads=[dZS, d_c], writes=[dKK])
            K.pool.op(lambda e, KKt=KKt, SQ=SQ: e.tensor_tensor(out=SQ[:], in0=KKt[:], in1=KKt[:], op=ALU.mult),
                      reads=[dKK], writes=[dSQ])
            yield
            K.dve.op(lambda e, SQ=SQ, S8=S8: e.tensor_reduce(out=S8[:, 0, :], in_=SQ[:].rearrange("p (h k) -> p h k", k=64),
                                                            axis=AX.X, op=ALU.add), reads=[dSQ], writes=[dS8])
            K.dve.op(lambda e, S8=S8: e.tensor_scalar(out=S8[:, 0, :], in0=S8[:, 0, :], scalar1=1e-24, scalar2=None,
                                                      op0=ALU.max), writes=[dS8])
            K.act.op(lambda e, S8=S8: e.activation(out=S8[:, 1, :], in_=S8[:, 0, :], func=AF.Ln), writes=[dS8])
            K.act.op(lambda e, S8=S8: e.activation(out=S8[:, 2, :], in_=S8[:, 1, :], func=AF.Exp, scale=-0.5), writes=[dS8])
            yield
            K.dve.op(lambda e, KKt=KKt, S8=S8, OUT=OUT: e.tensor_tensor(
                out=OUT[:, 1, :].rearrange("p (h k) -> p h k", k=64), in0=KKt[:].rearrange("p (h k) -> p h k", k=64),
                in1=S8[:, 2, :].unsqueeze(2).broadcast_to([128, 8, 64]), op=ALU.mult),
                reads=[dKK, dS8, dO], adds=[dO])
            K.dve.op(lambda e, OUT=OUT, UA=UA: e.scalar_tensor_tensor(
                out=OUT[:, 2, :], in0=OUT[:, 1, :], scalar=-1.0, in1=UA[:], op0=ALU.mult, op1=ALU.mult),
                reads=[dUA, dO], adds=[dO])
            yield
            T1, dT1 = T1r.next()
            K.dve.op(lambda e, T1=T1, UA=UA: e.scalar_tensor_tensor(
                out=T1[:], in0=UA[:], scalar=-1.0, in1=PR[:, 3, :], op0=ALU.add, op1=ALU.mult),
                reads=[dUA, d_c], writes=[dT1])
            K.dve.op(lambda e, T1=T1, OUT=OUT, ZS=ZS: e.scalar_tensor_tensor(
                out=OUT[:, 3, :], in0=T1[:], scalar=1.0, in1=ZS[:, 512:1024], op0=ALU.add, op1=ALU.mult),
                reads=[dT1, dZS, dO], adds=[dO])
            T2, dT2 = T2r.next()
            K.pool.op(lambda e, T2=T2, OUT=OUT, ZS=ZS: e.tensor_tensor(out=T2[:], in0=OUT[:, 3, :], in1=ZS[:, 0:512], op=ALU.mult),
                      reads=[dO, dZS], writes=[dT2])
            K.pool.op(lambda e, T2=T2: e.tensor_tensor(out=T2[:], in0=T2[:], in1=PR[:, 4, :], op=ALU.mult),
                      reads=[d_c], writes=[dT2])
            yield
            K.dve.op(lambda e, T2=T2, S8=S8: e.tensor_reduce(out=S8[:, 3, :], in_=T2[:].rearrange("p (h k) -> p h k", k=64),
                                                            axis=AX.X, op=ALU.add), reads=[dT2], writes=[dS8])
            yield
            pv, dpv = PV.next()
            pc, dpc = PC.next()
            for j in range(4):
                K.pe.op(lambda e, pv=pv, ZS=ZS, j=j: e.transpose(out=pv[:, j, :], in_=ZS[:, 1024 + j * 128:1024 + (j + 1) * 128],
                                                                identity=identf[:]),
                        reads=[dZS, d_c], writes=[dpv] if j == 0 else [], adds=[] if j == 0 else [dpv])
            K.pe.op(lambda e, pc=pc, S8=S8: e.transpose(out=pc[:, :], in_=S8[:, 3, :], identity=identf[:]),
                    reads=[dS8, d_c], writes=[dpc])
            VT, dVT = VTr.next()
            CT, dCT = CTr.next()
            K.act.op(lambda e, pv=pv, VT=VT: e.activation(out=VT[:], in_=pv[:], func=AF.Copy), reads=[dpv], writes=[dVT])
            K.act.op(lambda e, pc=pc, CT=CT: e.activation(out=CT[:], in_=pc[:], func=AF.Copy), reads=[dpc], writes=[dCT])
            K.sp.dma(lambda e, VT=VT, t0=t0: e.dma_start(
                out=dap(scr["v_fm"], t0, [[T, 128], [128 * T, 4], [1, 128]]), in_=VT[:]),
                reads=[dVT], adds=[scr["d_p2a"]])
            K.sp.dma(lambda e, CT=CT, t0=t0: e.dma_start(out=scr["coef_fm"][:, t0:t0 + 128], in_=CT[:]),
                     reads=[dCT], adds=[scr["d_p2a"]])
            yield
            if not chunked:
                K.pool.dma(lambda e, OUT=OUT, t0=t0: e.dma_start(out=scr["rw_tm"][t0:t0 + 128, :, :], in_=OUT[:]),
                           reads=[dO], adds=[scr["d_p2a"]])
            else:
                LWt, dLW = LWr.next()
                K.dve.op(lambda e, LWt=LWt, U=U: e.tensor_scalar(out=LWt[:], in0=U[:], scalar1=-1.0, scalar2=None, op0=ALU.mult),
                         reads=[dU], writes=[dLW])
                pl, dpl = PL.next()
                K.pe.op(lambda e, pl=pl, LWt=LWt: e.matmul(pl[:], TRI[:], LWt[:], start=True, stop=True),
                        reads=[dLW, d_c], writes=[dpl])
                EL, dEL = ELr.next()
                K.act.op(lambda e, EL=EL, pl=pl: e.activation(out=EL[:, 0, :], in_=pl[:], func=AF.Exp), reads=[dpl], writes=[dEL])
                K.act.op(lambda e, EL=EL, pl=pl: e.activation(out=EL[:, 1, :], in_=pl[:], func=AF.Exp, scale=-1.0),
                         reads=[dpl], adds=[dEL])
                K.dve.op(lambda e, EL=EL, pl=pl, U=U: e.tensor_tensor(out=EL[:, 2, :], in0=pl[:], in1=U[:], op=ALU.add),
                         reads=[dpl, dU, dEL], adds=[dEL])
                K.act.op(lambda e, EL=EL: e.activation(out=EL[:, 2, :], in_=EL[:, 2, :], func=AF.Exp), reads=[dEL], adds=[dEL])
                AB, dAB = ABr.next()
                K.dve.op(lambda e, AB=AB, OUT=OUT, EL=EL: e.tensor_tensor(out=AB[:, 0, :], in0=OUT[:, 1, :], in1=EL[:, 2, :], op=ALU.mult),
                         reads=[dO, dEL], writes=[dAB])
                K.dve.op(lambda e, AB=AB, OUT=OUT, EL=EL: e.scalar_tensor_tensor(
                    out=AB[:, 1, :], in0=OUT[:, 2, :], scalar=-1.0, in1=EL[:, 1, :], op0=ALU.mult, op1=ALU.mult),
                    reads=[dO, dEL, dAB], adds=[dAB])
                K.pool.op(lambda e, AB=AB, OUT=OUT, EL=EL: e.tensor_tensor(out=AB[:, 2, :], in0=OUT[:, 3, :], in1=EL[:, 1, :], op=ALU.mult),
                          reads=[dO, dEL, dAB], adds=[dAB])
                K.pool.op(lambda e, AB=AB, OUT=OUT, EL=EL: e.tensor_tensor(out=AB[:, 3, :], in0=OUT[:, 4, :], in1=EL[:, 0, :], op=ALU.mult),
                          reads=[dO, dEL, dAB], adds=[dAB])
                K.act.op(lambda e, AB=AB, ZS=ZS: e.activation(out=AB[:, 4, :], in_=ZS[:, 1024:1536], func=AF.Copy),
                         reads=[dZS, dAB], adds=[dAB])
                K.pool.dma(lambda e, AB=AB, t0=t0: e.dma_start(out=scr["ab_tm"][t0:t0 + 128, :, :], in_=AB[:]),
                           reads=[dAB], adds=[scr["d_p2a"]])
                K.sp.dma(lambda e, LWt=LWt, t0=t0: e.dma_start(out=scr["lw_tm"][t0:t0 + 128, :], in_=LWt[:]),
                         reads=[dLW], adds=[scr["d_p2a"]])

        active = []
        nxt = 0
        while nxt < NTT or active:
            if len(active) < 2 and nxt < NTT and (not active or active[0][2] or active[0][3] >= 2):
                active.append([tile_gen(nxt), None, False, 0])
                nxt += 1
            for idx, a_ in enumerate(list(active)):
                if a_[1] == "pre_pw" and idx > 0 and not active[0][2]:
                    continue
                try:
                    tok = next(a_[0])
                    a_[1] = tok
                    a_[3] += 1
                    if tok == "post_a":
                        a_[2] = True
                except StopIteration:
                    active.remove(a_)


def phase2_scan(K, cfg, io, scr):
    T, S, NB = cfg["T"], cfg["S"], cfg["NB"]
    NBH = 2 if NB >= 2 else 1
    NBL = NB // NBH
    NP = 64 * NBH
    TS = 2
    TC = 128
    RW = 2560
    with ExitStack() as es:
        St = K.sb([128, NBL, 8, 64], F32, "St", es)
        dS = Dep()
        TMP = K.sb([128, NBL, 8, 64], F32, "TMP", es)
        dT = Dep()
        SA = K.sb([128, NBL, 8], F32, "SA", es)
        dSA = Dep()
        T2r = Rot(K, 2, [128, NBL, 8, 64], F32, "TMP2", es=es)
        T3r = Rot(K, 2, [128, NBL, 8, 64], F32, "TMP3", es=es)
        BCr = Rot(K, 3, [128, TS, NBL, 5, 8, 64], F32, "BC", es=es)
        Vr = Rot(K, 2, [128, NBL, 8, TC], F32, "Vf", es=es)
        Yr = Rot(K, 2, [128, NBL, 8, TC], F32, "Yf", es=es)
        K.dve.op(lambda e: e.memset(St[:], 0.0), writes=[dS])
        qi = [0]

        def q():
            qi[0] += 1
            return K.sp if qi[0] % 2 == 0 else K.act

        def load_bc(ci):
            t = ci * TS
            BC, dBC = BCr.next()
            first = True
            for bhi in range(NBH):
                for blo in range(NBL):
                    src = dap(scr["rw_tm"], ((bhi * NBL + blo) * S + t) * RW, [[0, 64], [RW, TS], [1, RW]])
                    dst = BC[bhi * 64:(bhi + 1) * 64, :, blo].rearrange("p t j h k -> p t (j h k)")
                    q().dma(lambda e, src=src, dst=dst: e.dma_start(out=dst, in_=src), reads=[scr["d_p2a"]],
                            writes=[dBC] if first else [], adds=[] if first else [dBC])
                    first = False
            return BC, dBC

        def vy_ap(name, bhi, blo, t):
            return dap(scr[name], (bhi * NBL + blo) * S + t, [[T, 64], [64 * T, 8], [1, TC]])

        def load_v(ni):
            Vf, dV = Vr.next()
            first = True
            for bhi in range(NBH):
                for blo in range(NBL):
                    src = vy_ap("v_fm", bhi, blo, ni * TC)
                    dst = Vf[bhi * 64:(bhi + 1) * 64, blo]
                    q().dma(lambda e, src=src, dst=dst: e.dma_start(out=dst, in_=src), reads=[scr["d_p2a"]],
                            writes=[dV] if first else [], adds=[] if first else [dV])
                    first = False
            return Vf, dV

        nch = S // TS
        bcs = {}
        bcs[0] = load_bc(0)
        if nch > 1:
            bcs[1] = load_bc(1)
        vs = {0: load_v(0)}
        P = slice(0, NP)
        for t in range(S):
            ci, ts = divmod(t, TS)
            ni, tt = divmod(t, TC)
            if ts == 0 and ci + 2 < nch:
                bcs[ci + 2] = load_bc(ci + 2)
            if tt == 0:
                if (ni + 1) * TC < S:
                    vs[ni + 1] = load_v(ni + 1)
                Yf, dY = Yr.next()
            BC, dBC = bcs[ci]
            Vf, dV = vs[ni]
            W_ = BC[P, ts, :, 0]
            KN = BC[P, ts, :, 1]
            KA = BC[P, ts, :, 2]
            KP = BC[P, ts, :, 3]
            R_ = BC[P, ts, :, 4]
            shp = [NP, NBL, 8, 64]
            K.dve.op(lambda e, KN=KN: e.tensor_tensor(out=TMP[P], in0=St[P], in1=KN, op=ALU.mult),
                     reads=[dS, dBC], writes=[dT])
            K.dve.op(lambda e: e.tensor_reduce(out=SA[P], in_=TMP[P], axis=AX.X, op=ALU.add), reads=[dT], writes=[dSA])
            K.dve.op(lambda e, W_=W_: e.tensor_tensor(out=St[P], in0=St[P], in1=W_, op=ALU.mult),
                     reads=[dBC], writes=[dS])
            K.dve.op(lambda e, KA=KA: e.tensor_tensor(out=TMP[P], in0=KA, in1=SA[P].unsqueeze(3).broadcast_to(shp),
                                                     op=ALU.mult), reads=[dBC, dSA], writes=[dT])
            K.dve.op(lambda e: e.tensor_tensor(out=St[P], in0=St[P], in1=TMP[P], op=ALU.add), reads=[dT], writes=[dS])
            T2, dT2 = T2r.next()
            K.pool.op(lambda e, KP=KP, T2=T2, Vf=Vf, tt=tt: e.tensor_tensor(
                out=T2[P], in0=KP, in1=Vf[P, :, :, tt:tt + 1].broadcast_to(shp), op=ALU.mult),
                reads=[dBC, dV], writes=[dT2])
            K.dve.op(lambda e, T2=T2: e.tensor_tensor(out=St[P], in0=St[P], in1=T2[P], op=ALU.add),
                     reads=[dT2], writes=[dS])
            T3, dT3 = T3r.next()
            K.pool.op(lambda e, T3=T3, R_=R_: e.tensor_tensor(out=T3[P], in0=St[P], in1=R_, op=ALU.mult),
                      reads=[dS, dBC], writes=[dT3])
            K.dve.op(lambda e, T3=T3, Yf=Yf, tt=tt: e.tensor_reduce(out=Yf[P, :, :, tt], in_=T3[P], axis=AX.X, op=ALU.add),
                      reads=[dT3], writes=[dY] if tt == 0 else [], adds=[] if tt == 0 else [dY])
            if tt == TC - 1:
                for bhi in range(NBH):
                    for blo in range(NBL):
                        dst = vy_ap("y_fm", bhi, blo, ni * TC)
                        srcp = Yf[bhi * 64:(bhi + 1) * 64, blo]
                        K.sp.dma(lambda e, dst=dst, srcp=srcp: e.dma_start(out=dst, in_=srcp), reads=[dY],
                                 adds=[scr["d_p2b"]])


def phase2_chunk(K, cfg, io, scr):
    T, S, NB = cfg["T"], cfg["S"], cfg["NB"]
    C = 64
    NCH = S // C
    with ExitStack() as es:
        d_c = Dep()
        id64 = K.sb([64, 64], BF16, "c_id64", es)
        K.sp.dma(lambda e: e.dma_start(out=id64[:], in_=io["ident64"][:, :]), adds=[d_c])
        MK = K.sb([64, 3, 64], F32, "c_MK", es)
        K.sp.dma(lambda e: e.dma_start(out=MK[:], in_=io["masks"][:, :, :]), adds=[d_c])
        ONES = K.sb([64, 1], F32, "c_ones", es)
        K.sp.dma(lambda e: e.dma_start(out=ONES[:], in_=io["ones64"][:, :]), adds=[d_c])
        IDF = K.sb([64, 8, 64], F32, "c_IDF", es)
        K.sp.dma(lambda e: e.dma_start(out=IDF[:], in_=io["ident_f"][0:64, 0:64].unsqueeze(1).broadcast_to([64, 8, 64])), adds=[d_c])
        ST = [K.sb([64, 8, 64], F32, "c_S%d" % b, es) for b in range(NB)]
        STb = [K.sb([64, 8, 64], BF16, "c_Sb%d" % b, es) for b in range(NB)]
        dST = [Dep() for _ in range(NB)]
        dSTb = [Dep() for _ in range(NB)]
        for b in range(NB):
            K.dve.op(lambda e, b=b: e.memset(ST[b][:], 0.0), writes=[dST[b]])
            K.pool.op(lambda e, b=b: e.memset(STb[b][:], 0.0), writes=[dSTb[b]])
        TMr = Rot(K, 3, [64, 5, 512], BF16, "c_TM", es=es)
        LWr = Rot(K, 3, [64, 512], F32, "c_LW", es=es)
        FMr = Rot(K, 2, [64, 4, 8, 64], BF16, "c_FM", es=es)
        PCr = Rot(K, 2, [64, 8], F32, "c_PC", es=es)
        Nr = Rot(K, 3, [64, 8, 64], BF16, "c_N", es=es)
        NTr = Rot(K, 3, [64, 8, 64], BF16, "c_NT", es=es)
        MTr = Rot(K, 3, [64, 8, 64], BF16, "c_MT", es=es)
        MTfr = Rot(K, 2, [64, 8, 64], F32, "c_MTf", es=es)
        NAKr = Rot(K, 2, [64, 8, 64], BF16, "c_NAK", es=es)
        MRBr = Rot(K, 2, [64, 8, 64], BF16, "c_MRB", es=es)
        MRKr = Rot(K, 2, [64, 8, 64], BF16, "c_MRK", es=es)
        Xr = Rot(K, 2, [64, 8, 64], BF16, "c_X", es=es)
        NUr = Rot(K, 2, [64, 8, 64], BF16, "c_NU", es=es)
        Yr = Rot(K, 2, [64, 8, 64], F32, "c_Y", es=es)
        TSr = Rot(K, 2, [64, 8, 64], F32, "c_TS", es=es)
        PTf = Rot(K, 1, [64, 4, 8, 64], BF16, "c_PTf", psum=True, es=es)
        PA = Rot(K, 4, [64, 8, 64], F32, "c_PA", psum=True, es=es)
        PPC = Rot(K, 1, [64, 8], F32, "c_PPC", psum=True, es=es)
        ce = [0]

        def evac_copy(dst_ap, src_ap, reads, writes=(), adds=(), scale=None):
            ce[0] += 1
            if scale is not None or ce[0] % 2 == 0:
                if scale is None:
                    K.act.op(lambda e: e.activation(out=dst_ap, in_=src_ap, func=AF.Copy), reads=reads, writes=writes, adds=adds)
                else:
                    K.act.op(lambda e: e.activation(out=dst_ap, in_=src_ap, func=AF.Copy, scale=scale), reads=reads, writes=writes, adds=adds)
            else:
                K.dve.op(lambda e: e.tensor_copy(out=dst_ap, in_=src_ap), reads=reads, writes=writes, adds=adds)

        def mm8(pt, dpt, lhs_fn, rhs_fn, reads, first=True, last=True, wr=True):
            mmN(pt, dpt, [(lhs_fn, rhs_fn)], reads)

        def mmN(pt, dpt, terms, reads):
            n = len(terms)
            for h in range(8):
                for i, (lf, rf) in enumerate(terms):
                    K.pe.op(lambda e, h=h, lf=lf, rf=rf, i=i: e.matmul(pt[:, h, :], lf(h), rf(h), start=(i == 0), stop=(i == n - 1)),
                            reads=reads, writes=[dpt] if (h == 0 and i == 0) else [], adds=[] if (h == 0 and i == 0) else [dpt])

        q = [0]

        def dq():
            q[0] += 1
            return K.sp if q[0] % 2 == 0 else K.pool

        for ci in range(NCH):
            for b in range(NB):
                t0 = b * S + ci * C
                TM, dTM = TMr.next()
                LW, dLW = LWr.next()
                dq().dma(lambda e, TM=TM, t0=t0: e.dma_start(out=TM[:], in_=scr["ab_tm"][t0:t0 + C, :, :]),
                         reads=[scr["d_p2a"]], writes=[dTM])
                dq().dma(lambda e, LW=LW, t0=t0: e.dma_start(out=LW[:], in_=scr["lw_tm"][t0:t0 + C, :]),
                         reads=[scr["d_p2a"]], writes=[dLW])
                ptf, dptf = PTf.next()
                first = True
                for j in range(4):
                    for h in range(8):
                        K.pe.op(lambda e, ptf=ptf, TM=TM, j=j, h=h: e.transpose(
                            out=ptf[:, j, h, :], in_=TM[:, j, h * 64:(h + 1) * 64], identity=id64[:]),
                            reads=[dTM, d_c], writes=[dptf] if first else [], adds=[] if first else [dptf])
                        first = False
                FM, dFM = FMr.next()
                K.act.op(lambda e, FM=FM, ptf=ptf: e.activation(out=FM[:, 0:2], in_=ptf[:, 0:2], func=AF.Copy), reads=[dptf], writes=[dFM])
                K.dve.op(lambda e, FM=FM, ptf=ptf: e.tensor_copy(out=FM[:, 2:4], in_=ptf[:, 2:4]), reads=[dptf, dFM], adds=[dFM])
                Af = lambda h, FM=FM: FM[:, 0, h, :]
                Bf = lambda h, FM=FM: FM[:, 1, h, :]
                Kf = lambda h, FM=FM: FM[:, 2, h, :]
                Rf = lambda h, FM=FM: FM[:, 3, h, :]
                Vt = lambda h, TM=TM: TM[:, 4, h * 64:(h + 1) * 64]
                Bt = lambda h, TM=TM: TM[:, 1, h * 64:(h + 1) * 64]
                Kt = lambda h, TM=TM: TM[:, 2, h * 64:(h + 1) * 64]
                ppc, dppc = PPC.next()
                for h in range(8):
                    K.pe.op(lambda e, ppc=ppc, LW=LW, h=h: e.matmul(ppc[:, h:h + 1], LW[:, h * 64:(h + 1) * 64], ONES[:], start=True, stop=True),
                            reads=[dLW, d_c], writes=[dppc] if h == 0 else [], adds=[] if h == 0 else [dppc])
                PCt, dPC = PCr.next()
                K.act.op(lambda e, PCt=PCt, ppc=ppc: e.activation(out=PCt[:], in_=ppc[:], func=AF.Exp), reads=[dppc], writes=[dPC])
                mbc = lambda i: MK[:, i, :].unsqueeze(1).broadcast_to([64, 8, 64])
                pa, dpa = PA.next()
                mm8(pa, dpa, Af, Bf, [dFM])
                N0, dN0 = Nr.next()
                K.dve.op(lambda e, N0=N0, pa=pa: e.tensor_tensor(out=N0[:], in0=pa[:], in1=mbc(0), op=ALU.mult), reads=[dpa, d_c], writes=[dN0])
                pa, dpa = PA.next()
                mm8(pa, dpa, Bf, Af, [dFM])
                NT0, dNT0 = NTr.next()
                MTf, dMTf = MTfr.next()
                K.dve.op(lambda e, NT0=NT0, pa=pa: e.tensor_tensor(out=NT0[:], in0=pa[:], in1=mbc(1), op=ALU.mult), reads=[dpa, d_c], writes=[dNT0])
                K.pool.op(lambda e, MTf=MTf, NT0=NT0: e.tensor_tensor(out=MTf[:], in0=IDF[:], in1=NT0[:], op=ALU.subtract),
                          reads=[dNT0, d_c], writes=[dMTf])
                MT, dMT = MTr.next()
                K.act.op(lambda e, MT=MT, MTf=MTf: e.activation(out=MT[:], in_=MTf[:], func=AF.Copy), reads=[dMTf], writes=[dMT])
                pa, dpa = PA.next()
                mm8(pa, dpa, Kf, Af, [dFM])
                NAK, dNAK = NAKr.next()
                K.dve.op(lambda e, NAK=NAK, pa=pa: e.tensor_tensor(out=NAK[:], in0=pa[:], in1=mbc(1), op=ALU.mult), reads=[dpa, d_c], writes=[dNAK])
                pa, dpa = PA.next()
                mm8(pa, dpa, Bf, Rf, [dFM])
                MRB, dMRB = MRBr.next()
                K.dve.op(lambda e, MRB=MRB, pa=pa: e.tensor_tensor(out=MRB[:], in0=pa[:], in1=mbc(2), op=ALU.mult), reads=[dpa, d_c], writes=[dMRB])
                pa, dpa = PA.next()
                mm8(pa, dpa, Kf, Rf, [dFM])
                MRK, dMRK = MRKr.next()
                K.dve.op(lambda e, MRK=MRK, pa=pa: e.tensor_tensor(out=MRK[:], in0=pa[:], in1=mbc(2), op=ALU.mult), reads=[dpa, d_c], writes=[dMRK])
                Np, dNp, NTp, dNTp = N0, dN0, NT0, dNT0
                for lvl in range(1, 6):
                    pa, dpa = PA.next()
                    mm8(pa, dpa, lambda h, NTp=NTp: NTp[:, h, :], lambda h, Np=Np: Np[:, h, :], [dNp, dNTp])
                    Nn, dNn = Nr.next()
                    evac_copy(Nn[:], pa[:], [dpa], writes=[dNn])
                    if lvl < 5:
                        pa2, dpa2 = PA.next()
                        mm8(pa2, dpa2, lambda h, Np=Np: Np[:, h, :], lambda h, NTp=NTp: NTp[:, h, :], [dNp, dNTp])
                        NTn, dNTn = NTr.next()
                        evac_copy(NTn[:], pa2[:], [dpa2], writes=[dNTn])
                    pa3, dpa3 = PA.next()
                    mm8(pa3, dpa3, lambda h, Nn=Nn: Nn[:, h, :], lambda h, MT=MT: MT[:, h, :], [dNn, dMT])
                    K.dve.op(lambda e, MTf=MTf, pa3=pa3: e.tensor_tensor(out=MTf[:], in0=MTf[:], in1=pa3[:], op=ALU.add),
                             reads=[dpa3], writes=[dMTf])
                    MT, dMT = MTr.next()
                    K.act.op(lambda e, MT=MT, MTf=MTf: e.activation(out=MT[:], in_=MTf[:], func=AF.Copy), reads=[dMTf], writes=[dMT])
                    Np, dNp = Nn, dNn
                    if lvl < 5:
                        NTp, dNTp = NTn, dNTn
                Sb = STb[b]
                pa, dpa = PA.next()
                mmN(pa, dpa, [(Af, lambda h, Sb=Sb: Sb[:, h, :]), (lambda h, NAK=NAK: NAK[:, h, :], Vt)], [dFM, dSTb[b], dNAK, dTM])
                X, dX = Xr.next()
                K.act.op(lambda e, X=X, pa=pa: e.activation(out=X[:], in_=pa[:], func=AF.Copy), reads=[dpa], writes=[dX])
                pa, dpa = PA.next()
                mm8(pa, dpa, lambda h, MT=MT: MT[:, h, :], lambda h, X=X: X[:, h, :], [dMT, dX])
                NU, dNU = NUr.next()
                K.act.op(lambda e, NU=NU, pa=pa: e.activation(out=NU[:], in_=pa[:], func=AF.Copy, scale=-1.0), reads=[dpa], writes=[dNU])
                pa, dpa = PA.next()
                mmN(pa, dpa, [(lambda h, Sb=Sb: Sb[:, h, :], Rf), (lambda h, NU=NU: NU[:, h, :], lambda h, MRB=MRB: MRB[:, h, :]),
                              (Vt, lambda h, MRK=MRK: MRK[:, h, :])], [dFM, dSTb[b], dNU, dMRB, dTM, dMRK])
                Y, dY = Yr.next()
                K.dve.op(lambda e, Y=Y, pa=pa: e.tensor_copy(out=Y[:], in_=pa[:]), reads=[dpa], writes=[dY])
                K.sp.dma(lambda e, Y=Y, t0=t0: e.dma_start(out=dap(scr["y_fm"], t0, [[T, 64], [64 * T, 8], [1, 64]]), in_=Y[:]),
                         reads=[dY], adds=[scr["d_p2b"]])
                pa, dpa = PA.next()
                mmN(pa, dpa, [(Bt, lambda h, NU=NU: NU[:, h, :]), (Kt, Vt)], [dTM, dNU])
                TS_, dTS = TSr.next()
                K.dve.op(lambda e, TS_=TS_, pa=pa, b=b: e.tensor_tensor(out=TS_[:], in0=pa[:], in1=ST[b][:], op=ALU.add),
                         reads=[dpa, dST[b]], writes=[dTS])
                K.dve.op(lambda e, TS_=TS_, PCt=PCt, b=b: e.tensor_tensor(
                    out=ST[b][:], in0=TS_[:], in1=PCt[:].unsqueeze(2).broadcast_to([64, 8, 64]), op=ALU.mult),
                    reads=[dTS, dPC], writes=[dST[b]])
                K.act.op(lambda e, b=b: e.activation(out=STb[b][:], in_=ST[b][:], func=AF.Copy), reads=[dST[b]], writes=[dSTb[b]])


def phase2_post(K, cfg, io, scr):
    T, S, NB = cfg["T"], cfg["S"], cfg["NB"]
    NT = T // 512
    with ExitStack() as es:
        BO = K.sb([128, 128], F32, "BO", es)
        d_c = Dep()
        K.sp.dma(lambda e: e.dma_start(out=BO[:], in_=io["blockones"][:, :]), adds=[d_c])
        LN = K.sb([128, 2, 4], F32, "LN", es)
        K.sp.dma(lambda e: e.dma_start(out=LN[:, 0, :], in_=io["lnx_w"].rearrange("o (j p) -> p (o j)", p=128)), adds=[d_c])
        K.sp.dma(lambda e: e.dma_start(out=LN[:, 1, :], in_=io["lnx_b"].rearrange("o (j p) -> p (o j)", p=128)), adds=[d_c])
        Yr = Rot(K, 2, [128, 512], F32, "pY", es=es)
        Vr = Rot(K, 2, [128, 512], F32, "pV", es=es)
        Gr = Rot(K, 2, [128, 512], F32, "pG", es=es)
        Cr = Rot(K, 2, [128, 512], F32, "pC", es=es)
        YCr = Rot(K, 2, [128, 512], F32, "pYC", es=es)
        SQr = Rot(K, 2, [128, 512], F32, "pSQ", es=es)
        Rr = Rot(K, 2, [128, 512], F32, "pR", es=es)
        Or = Rot(K, 2, [128, 512], BF16, "pO", es=es)
        PM = Rot(K, 2, [128, 512], F32, "pPM", psum=True, es=es)
        PVr = Rot(K, 2, [128, 512], F32, "pPV", psum=True, es=es)
        def make_gen(ti, j):
            cs = slice(ti * 512, (ti + 1) * 512)
            if True:
                rs = slice(j * 128, (j + 1) * 128)
                Y, dY = Yr.next()
                V, dV = Vr.next()
                G, dG = Gr.next()
                C, dC = Cr.next()
                K.sp.dma(lambda e, Y=Y, rs=rs, cs=cs: e.dma_start(out=Y[:], in_=scr["y_fm"][rs, cs]),
                         reads=[scr["d_p2b"]], writes=[dY])
                K.pool.dma(lambda e, V=V, rs=rs, cs=cs: e.dma_start(out=V[:], in_=scr["v_fm"][rs, cs]),
                           reads=[scr["d_p2a"]], writes=[dV])
                K.sp.dma(lambda e, G=G, rs=rs, cs=cs: e.dma_start(out=G[:], in_=scr["g_fm"][rs, cs]),
                         reads=[scr["d_p2a"]], writes=[dG])
                K.pool.dma(lambda e, C=C, j=j, cs=cs: e.dma_start(
                    out=C[0:64, :], in_=scr["coef_fm"][2 * j:2 * j + 1, cs].broadcast_to([64, 512])),
                    reads=[scr["d_p2a"]], writes=[dC])
                K.pool.dma(lambda e, C=C, j=j, cs=cs: e.dma_start(
                    out=C[64:128, :], in_=scr["coef_fm"][2 * j + 1:2 * j + 2, cs].broadcast_to([64, 512])),
                    reads=[scr["d_p2a"]], adds=[dC])
                pm, dpm = PM.next()
                K.pe.op(lambda e, pm=pm, Y=Y: e.matmul(pm[:], BO[:], Y[:], start=True, stop=True),
                        reads=[dY, d_c], writes=[dpm])
                YC, dYC = YCr.next()
                K.dve.op(lambda e, YC=YC, Y=Y, pm=pm: e.tensor_tensor(out=YC[:], in0=Y[:], in1=pm[:], op=ALU.subtract),
                         reads=[dY, dpm], writes=[dYC])
                yield
                SQ, dSQ = SQr.next()
                K.act.op(lambda e, SQ=SQ, YC=YC: e.activation(out=SQ[:], in_=YC[:], func=AF.Square),
                         reads=[dYC], writes=[dSQ])
                yield
                pv, dpv = PVr.next()
                K.pe.op(lambda e, pv=pv, SQ=SQ: e.matmul(pv[:], BO[:], SQ[:], start=True, stop=True),
                        reads=[dSQ, d_c], writes=[dpv])
                yield
                R, dR = Rr.next()
                K.dve.op(lambda e, R=R, pv=pv: e.tensor_scalar(out=R[:], in0=pv[:], scalar1=64e-5, scalar2=None, op0=ALU.add),
                         reads=[dpv], writes=[dR])
                K.act.op(lambda e, R=R: e.activation(out=R[:], in_=R[:], func=AF.Ln), writes=[dR])
                K.act.op(lambda e, R=R: e.activation(out=R[:], in_=R[:], func=AF.Exp, scale=-0.5), writes=[dR])
                yield
                K.dve.op(lambda e, YC=YC, R=R: e.tensor_tensor(out=YC[:], in0=YC[:], in1=R[:], op=ALU.mult),
                         reads=[dR], writes=[dYC])
                K.dve.op(lambda e, YC=YC, j=j: e.tensor_scalar(out=YC[:], in0=YC[:], scalar1=LN[:, 0, j:j + 1],
                                                              scalar2=LN[:, 1, j:j + 1], op0=ALU.mult, op1=ALU.add),
                         reads=[d_c], writes=[dYC])
                yield
                K.pool.op(lambda e, C=C, V=V: e.tensor_tensor(out=C[:], in0=C[:], in1=V[:], op=ALU.mult),
                          reads=[dV], writes=[dC])
                yield
                K.dve.op(lambda e, YC=YC, C=C: e.tensor_tensor(out=YC[:], in0=YC[:], in1=C[:], op=ALU.add),
                         reads=[dC], writes=[dYC])
                O, dO = Or.next()
                K.dve.op(lambda e, O=O, YC=YC, G=G: e.tensor_tensor(out=O[:], in0=YC[:], in1=G[:], op=ALU.mult),
                         reads=[dYC, dG], writes=[dO])
                K.sp.dma(lambda e, O=O, rs=rs, cs=cs: e.dma_start(out=scr["ya_fm"][rs, cs], in_=O[:]),
                         reads=[dO], adds=[scr["d_p2c"]])

        work = [(ti, j) for ti in range(NT) for j in range(4)]

        active = []
        nxt = 0
        while nxt < len(work) or active:
            if len(active) < 2 and nxt < len(work):
                active.append(make_gen(*work[nxt]))
                nxt += 1
            for a in list(active):
                try:
                    next(a)
                except StopIteration:
                    active.remove(a)


def phase3(K, cfg, io, scr):
    T, S, NB = cfg["T"], cfg["S"], cfg["NB"]
    NQ = S // 128
    lam_init = 0.2
    with ExitStack() as es:
        d_c = Dep()
        identb = K.sb([128, 128], BF16, "a_identb", es)
        K.sp.dma(lambda e: e.dma_start(out=identb[:], in_=io["ident_bf"][:, :]), adds=[d_c])
        TB = K.sb([128, 4, S], F32, "TB", es)
        for h in range(4):
            (K.sp if h % 2 == 0 else K.pool).dma(lambda e, h=h: e.dma_start(out=TB[:, h, :], in_=io["alibi"][h, :, :]), adds=[d_c])
        SW = K.sb([128, 128], F32, "SW", es)
        K.sp.dma(lambda e: e.dma_start(out=SW[:], in_=io["subln_w"][0:1, :].broadcast_to([128, 128])), adds=[d_c])
        LQ = K.sb([128, 4, 64], F32, "LQ", es)
        for j, nm in enumerate(["lam_q1", "lam_k1", "lam_q2", "lam_k2"]):
            K.pool.dma(lambda e, j=j, nm=nm: e.dma_start(out=LQ[:, j, :], in_=io[nm][0:1, :].broadcast_to([128, 64])), adds=[d_c])
        LM = K.sb([128, 8], F32, "LM", es)
        d_lm = Dep()
        LT_ = K.sb([128, 2, 64], F32, "LTt", es)
        K.dve.op(lambda e: e.tensor_tensor(out=LT_[:, 0, :], in0=LQ[:, 0, :], in1=LQ[:, 1, :], op=ALU.mult), reads=[d_c], writes=[d_lm])
        K.dve.op(lambda e: e.tensor_tensor(out=LT_[:, 1, :], in0=LQ[:, 2, :], in1=LQ[:, 3, :], op=ALU.mult), reads=[d_c], writes=[d_lm])
        K.dve.op(lambda e: e.tensor_reduce(out=LM[:, 0:2], in_=LT_[:], axis=AX.X, op=ALU.add), writes=[d_lm])
        K.act.op(lambda e: e.activation(out=LM[:, 2:4], in_=LM[:, 0:2], func=AF.Exp), writes=[d_lm])
        K.dve.op(lambda e: e.tensor_tensor(out=LM[:, 4:5], in0=LM[:, 3:4], in1=LM[:, 2:3], op=ALU.subtract), writes=[d_lm])
        K.dve.op(lambda e: e.tensor_scalar(out=LM[:, 4:5], in0=LM[:, 4:5], scalar1=-lam_init, scalar2=None, op0=ALU.add), writes=[d_lm])
        K.dve.op(lambda e: e.tensor_scalar(out=SW[:], in0=SW[:], scalar1=1.0 - lam_init, scalar2=None, op0=ALU.mult),
                 reads=[d_c], writes=[d_c])

        Vr = Rot(K, 2, [128, NQ, 512], BF16, "aV", es=es)
        QKr = Rot(K, 2, [64, 4, S], BF16, "aQK", es=es)
        SSr = Rot(K, 3, [128, 512], F32, "aSS", es=es)
        Pr = Rot(K, 3, [128, 512], BF16, "aP", es=es)
        PTsr = Rot(K, 4, [128, 4, 128], BF16, "aPTs", es=es)
        YB = K.sb([128, NQ, 512], BF16, "aYB", es)
        dYB = Dep()
        STr = Rot(K, 4, [128, 24], F32, "aST", es=es)
        O1r = Rot(K, 2, [128, 128], F32, "aO1", es=es)
        Or_ = Rot(K, 2, [128, 128], F32, "aO", es=es)
        junk = K.sb([128, 128], F32, "ajunk", es)
        d_junk = Dep()
        YTr = Rot(K, 2, [128, 4, 128], BF16, "aYT", es=es)
        PS = Rot(K, 3, [128, 512], F32, "aPS", psum=True, es=es)
        PTp = Rot(K, 2, [128, 4, 128], BF16, "aPTp", psum=True, es=es)
        PO = Rot(K, 2, [128, 2, 128], F32, "aPO", psum=True, es=es)
        cp = [0]

        def copy_eng():
            cp[0] += 1
            return cp[0] % 2

        SSQ = K.sb([128, NQ * 4], F32, "aSSQ", es)
        dSSQ = Dep()
        SWb = K.sb([128, 128], BF16, "aSWb", es)
        K.act.op(lambda e: e.activation(out=SWb[:], in_=SW[:], func=AF.Copy), reads=[d_c], adds=[d_c])
        pipe = []
        pidx = [0]

        def step_pipe():
            j = len(pipe) - 1
            pipe[j][0]()
            if j - 1 >= pidx[0]:
                pipe[j - 1][2]()
            if j - 2 >= pidx[0]:
                pipe[j - 2][3]()
                if pipe[j - 2][4] is not None:
                    pipe[j - 2][4]()
            pipe[j][1]()

        def flush_pipe():
            j = len(pipe) - 1
            if j - 0 >= pidx[0] and j >= 0:
                pipe[j][2]()
            for k in (j - 1, j):
                if k >= pidx[0] and k >= 0:
                    pipe[k][3]()
                    if pipe[k][4] is not None:
                        pipe[k][4]()
            pidx[0] = len(pipe)
        for b in range(NB):
            V, dV = Vr.next()
            K.sp.dma(lambda e, V=V, b=b: e.dma_start(
                out=V[:], in_=scr["av_tm"][b * S:(b + 1) * S, :].rearrange("(n p) c -> p n c", p=128)),
                reads=[scr["d_p1"]], writes=[dV])
            first_yb = True
            for h in range(4):
                QK, dQK = QKr.next()
                for j in range(4):
                    r0 = (0 if j < 2 else 512) + h * 128 + (j % 2) * 64
                    (K.sp if j % 2 == 0 else K.pool).dma(lambda e, QK=QK, j=j, r0=r0, b=b: e.dma_start(
                        out=QK[:, j, :], in_=scr["qk_fm"][r0:r0 + 64, b * S:(b + 1) * S]),
                        reads=[scr["d_p1"]], writes=[dQK] if j == 0 else [], adds=[] if j == 0 else [dQK])
                for qi in range(NQ):
                    nk = (qi + 1) * 128
                    off = (S - 128) - qi * 128
                    ST, dST = STr.next()
                    K.pool.op(lambda e, ST=ST: e.memset(ST[:], 0.0), writes=[dST])
                    po, dpo = PO.next()
                    items = []
                    for c in range(2):
                        nch = (nk + 511) // 512
                        for ch in range(nch):
                            items.append((c, ch))
                    for ii, (c, ch) in enumerate(items):
                        kb0 = ch * 512
                        n = min(512, nk - kb0)
                        nb = n // 128
                        ps, dps = PS.next()
                        SS, dSS = SSr.next()
                        Pt, dP = Pr.next()
                        hold = {}

                        def stA_pe(ps=ps, dps=dps, c=c, qi=qi, kb0=kb0, n=n, QK=QK, dQK=dQK):
                            K.pe.op(lambda e: e.matmul(
                                ps[:, :n], QK[:, c, qi * 128:(qi + 1) * 128], QK[:, 2 + c, kb0:kb0 + n],
                                start=True, stop=True), reads=[dQK], writes=[dps])

                        def stA_rest(ps=ps, dps=dps, SS=SS, dSS=dSS, Pt=Pt, dP=dP, ST=ST, dST=dST, n=n, off=off, h=h, kb0=kb0, c=c, ch=ch):
                            K.dve.op(lambda e: e.scalar_tensor_tensor(
                                out=SS[:, :n], in0=ps[:, :n], scalar=0.125, in1=TB[:, h, off + kb0:off + kb0 + n],
                                op0=ALU.mult, op1=ALU.add), reads=[dps, d_c], writes=[dSS])
                            K.act.op(lambda e: e.activation(
                                out=Pt[:, :n], in_=SS[:, :n], func=AF.Exp,
                                accum_out=ST[:, 4 * c + ch:4 * c + ch + 1]), reads=[dSS, dST], writes=[dP], adds=[dST])

                        def stB(Pt=Pt, dP=dP, nb=nb, hold=hold):
                            ptp, dptp = PTp.next()
                            for kk_ in range(nb):
                                K.pe.op(lambda e, kk_=kk_: e.transpose(
                                    out=ptp[:, kk_, :], in_=Pt[:, kk_ * 128:(kk_ + 1) * 128], identity=identb[:]),
                                    reads=[dP, d_c], writes=[dptp] if kk_ == 0 else [], adds=[] if kk_ == 0 else [dptp])
                            PTs, dPTs = PTsr.next()
                            hold["PTs"] = (PTs, dPTs)
                            if copy_eng():
                                K.act.op(lambda e: e.activation(
                                    out=PTs[:, :nb, :], in_=ptp[:, :nb, :], func=AF.Copy), reads=[dptp], writes=[dPTs])
                            else:
                                K.dve.op(lambda e: e.tensor_copy(
                                    out=PTs[:, :nb, :], in_=ptp[:, :nb, :]), reads=[dptp], writes=[dPTs])

                        def stC(nb=nb, kb0=kb0, c=c, h=h, qi=qi, po=po, dpo=dpo, V=V, dV=dV, hold=hold):
                            PTs, dPTs = hold["PTs"]
                            for kk_ in range(nb):
                                kb = kb0 // 128 + kk_
                                K.pe.op(lambda e, kb=kb, kk_=kk_: e.matmul(
                                    po[:, c, :], PTs[:, kk_, :], V[:, kb, h * 128:(h + 1) * 128],
                                    start=(kb == 0), stop=(kb == qi)), reads=[dPTs, dV],
                                    writes=[dpo] if (kb == 0 and c == 0) else [], adds=[] if (kb == 0 and c == 0) else [dpo])

                        def combine(ST=ST, dST=dST, po=po, dpo=dpo, qi=qi, h=h, fy=first_yb):
                            K.dve.op(lambda e: e.tensor_reduce(out=ST[:, 8:10], in_=ST[:, 0:8].rearrange("p (c k) -> p c k", k=4),
                                                               axis=AX.X, op=ALU.add), reads=[dST], writes=[dST])
                            K.dve.op(lambda e: e.reciprocal(out=ST[:, 10:12], in_=ST[:, 8:10]), writes=[dST])
                            K.dve.op(lambda e: e.tensor_tensor(out=ST[:, 12:13], in0=ST[:, 11:12], in1=LM[:, 4:5], op=ALU.mult),
                                     reads=[d_lm], writes=[dST])
                            O1, dO1 = O1r.next()
                            K.dve.op(lambda e: e.tensor_scalar(out=O1[:], in0=po[:, 1, :], scalar1=ST[:, 12:13],
                                                               scalar2=None, op0=ALU.mult), reads=[dpo, dST], writes=[dO1])
                            K.dve.op(lambda e: e.scalar_tensor_tensor(
                                out=YB[:, qi, h * 128:(h + 1) * 128], in0=po[:, 0, :], scalar=ST[:, 10:11], in1=O1[:], op0=ALU.mult, op1=ALU.add),
                                reads=[dpo, dST, dO1], writes=[dYB] if fy else [], adds=[] if fy else [dYB])
                            K.act.op(lambda e: e.activation(out=junk[:], in_=YB[:, qi, h * 128:(h + 1) * 128], func=AF.Square,
                                                            accum_out=SSQ[:, qi * 4 + h:qi * 4 + h + 1]),
                                     reads=[dYB], writes=[d_junk], adds=[dSSQ])

                        last = (ii == len(items) - 1)
                        pipe.append([stA_pe, stA_rest, stB, stC, combine if last else None])
                        step_pipe()
                    first_yb = False
            flush_pipe()
            K.dve.op(lambda e: e.tensor_scalar(out=SSQ[:], in0=SSQ[:], scalar1=1.0 / 128, scalar2=1e-5, op0=ALU.mult, op1=ALU.add),
                     reads=[dSSQ], writes=[dSSQ])
            K.act.op(lambda e: e.activation(out=SSQ[:], in_=SSQ[:], func=AF.Ln), writes=[dSSQ])
            K.act.op(lambda e: e.activation(out=SSQ[:], in_=SSQ[:], func=AF.Exp, scale=-0.5), writes=[dSSQ])
            YBv = YB[:].rearrange("p q (h e) -> p (q h) e", e=128)
            K.dve.op(lambda e: e.tensor_tensor(out=YBv, in0=YBv, in1=SSQ[:].unsqueeze(2).broadcast_to([128, NQ * 4, 128]), op=ALU.mult),
                     reads=[dSSQ], writes=[dYB])
            K.pool.op(lambda e: e.tensor_tensor(out=YBv, in0=YBv, in1=SWb[:].unsqueeze(1).broadcast_to([128, NQ * 4, 128]), op=ALU.mult),
                      reads=[d_c], writes=[dYB])
            for qi in range(NQ):
                ptp, dptp = PTp.next()
                for h in range(4):
                    K.pe.op(lambda e, ptp=ptp, qi=qi, h=h: e.transpose(out=ptp[:, h, :], in_=YB[:, qi, h * 128:(h + 1) * 128],
                                                                      identity=identb[:]),
                            reads=[dYB, d_c], writes=[dptp] if h == 0 else [], adds=[] if h == 0 else [dptp])
                YT, dYT = YTr.next()
                K.act.op(lambda e, ptp=ptp, YT=YT: e.activation(out=YT[:], in_=ptp[:, 0:4, :], func=AF.Copy),
                         reads=[dptp], writes=[dYT])
                t0 = b * S + qi * 128
                K.sp.dma(lambda e, YT=YT, t0=t0: e.dma_start(
                    out=dap(scr["yb_fm"], t0, [[T, 128], [128 * T, 4], [1, 128]]), in_=YT[:]),
                    reads=[dYT], adds=[scr["d_p3"]])


def load_w_bf16(K, es, src2d, rows, cols, name, dep, stage_rot, q, W=None):
    nk = rows // 128
    if W is None:
        W = K.sb([128, nk, cols], BF16, name, es)
    for kc in range(nk):
        for c0 in range(0, cols, 1024):
            n = min(1024, cols - c0)
            st, dst = stage_rot.next()
            q[0] += 1
            (K.sp if q[0] % 2 == 0 else K.pool).dma(lambda e, st=st, kc=kc, c0=c0, n=n: e.dma_start(
                out=st[:, :n], in_=src2d[kc * 128:(kc + 1) * 128, c0:c0 + n]), writes=[dst])
            if q[0] % 2 == 0:
                K.act.op(lambda e, st=st, kc=kc, c0=c0, n=n: e.activation(out=W[:, kc, c0:c0 + n], in_=st[:, :n], func=AF.Copy),
                         reads=[dst], adds=[dep])
            else:
                K.dve.op(lambda e, st=st, kc=kc, c0=c0, n=n: e.tensor_copy(out=W[:, kc, c0:c0 + n], in_=st[:, :n]),
                         reads=[dst], adds=[dep])
    return W


def phase4(K, cfg, io, scr):
    T, S, NB = cfg["T"], cfg["S"], cfg["NB"]
    NT = T // 512
    with ExitStack() as es:
        d_c = Dep()
        identb = K.sb([128, 128], BF16, "m_identb", es)
        K.sp.dma(lambda e: e.dma_start(out=identb[:], in_=io["ident_bf"][:, :]), adds=[d_c])
        NF = K.sb([128, D], F32, "NF", es)
        K.sp.dma(lambda e: e.dma_start(out=NF[:], in_=io["norm_ffn_w"][0:1, :].broadcast_to([128, D])), adds=[d_c])
        stg = Rot(K, 2, [128, 1024], F32, "m_stg", es=es)
        q = [0]
        PAw = load_w_bf16(K, es, io["proj_a"][0], 512, D, "PAw", d_c, stg, q)
        PBw = load_w_bf16(K, es, io["proj_b"][0], 512, D, "PBw", d_c, stg, q)
        WO = load_w_bf16(K, es, io["w_out"][0], D, D, "WO", d_c, stg, q)
        YAr = Rot(K, 2, [128, 4, 512], BF16, "mYA", es=es)
        YBr = Rot(K, 2, [128, 4, 512], BF16, "mYB", es=es)
        SGr = Rot(K, 2, [128, 16, 512], BF16, "mSG", es=es)
        MGr = Rot(K, 2, [128, 8, 512], BF16, "mMG", es=es)
        t1r = Rot(K, 2, [128, 512], F32, "mt1", es=es)
        t2r = Rot(K, 2, [128, 512], F32, "mt2", es=es)
        Xr = Rot(K, 2, [128, D], F32, "mX", es=es)
        X1r = Rot(K, 2, [128, D], F32, "mX1", es=es)
        XHr = Rot(K, 2, [128, D], F32, "mXH", es=es)
        XBr = Rot(K, 2, [128, D], BF16, "mXB", es=es)
        XTr = Rot(K, 2, [128, 8, 128], BF16, "mXT", es=es)
        junk = K.sb([128, D], BF16, "mjunk", es)
        d_junk = Dep()
        STr = Rot(K, 4, [128, 4], F32, "mST", es=es)
        PP = Rot(K, 2, [128, 2, 512], F32, "mPP", psum=True, es=es)
        PO2 = Rot(K, 1, [128, 2, 512], F32, "mPO", psum=True, es=es)
        PTp = Rot(K, 1, [128, 8, 128], BF16, "mPTp", psum=True, es=es)
        def make_gen(ti):
            cs = slice(ti * 512, (ti + 1) * 512)
            YA, dYA = YAr.next()
            YB, dYB = YBr.next()
            SG, dSG = SGr.next()
            K.sp.dma(lambda e, YA=YA, cs=cs: e.dma_start(out=YA[:], in_=scr["ya_fm"][:, cs].rearrange("(c p) t -> p c t", p=128)),
                     reads=[scr["d_p2c"]], writes=[dYA])
            K.pool.dma(lambda e, YB=YB, cs=cs: e.dma_start(out=YB[:], in_=scr["yb_fm"][:, cs].rearrange("(c p) t -> p c t", p=128)),
                       reads=[scr["d_p3"]], writes=[dYB])
            K.sp.dma(lambda e, SG=SG, cs=cs: e.dma_start(out=SG[:], in_=scr["sg_fm"][:, cs].rearrange("(c p) t -> p c t", p=128)),
                     reads=[scr["d_p1"]], writes=[dSG])
            MG, dMG = MGr.next()
            for m in range(8):
                pp, dpp = PP.next()
                for c in range(4):
                    K.pe.op(lambda e, pp=pp, YA=YA, c=c, m=m: e.matmul(pp[:, 0, :], PAw[:, c, m * 128:(m + 1) * 128], YA[:, c, :],
                                                                      start=(c == 0), stop=(c == 3)),
                            reads=[dYA, d_c], writes=[dpp] if c == 0 else [], adds=[] if c == 0 else [dpp])
                for c in range(4):
                    K.pe.op(lambda e, pp=pp, YB=YB, c=c, m=m: e.matmul(pp[:, 1, :], PBw[:, c, m * 128:(m + 1) * 128], YB[:, c, :],
                                                                      start=(c == 0), stop=(c == 3)),
                            reads=[dYB, d_c], adds=[dpp])
                t1, dt1 = t1r.next()
                t2, dt2 = t2r.next()
                K.dve.op(lambda e, t1=t1, pp=pp, SG=SG, m=m: e.tensor_tensor(out=t1[:], in0=pp[:, 0, :], in1=SG[:, m, :], op=ALU.mult),
                         reads=[dpp, dSG], writes=[dt1])
                K.dve.op(lambda e, t2=t2, pp=pp, SG=SG, m=m: e.tensor_tensor(out=t2[:], in0=pp[:, 1, :], in1=SG[:, 8 + m, :], op=ALU.mult),
                         reads=[dpp, dSG], writes=[dt2])
                K.pool.op(lambda e, t1=t1, t2=t2, MG=MG, m=m: e.tensor_tensor(out=MG[:, m, :], in0=t1[:], in1=t2[:], op=ALU.add),
                          reads=[dt1, dt2], writes=[dMG] if m == 0 else [], adds=[] if m == 0 else [dMG])
                if m % 2 == 1:
                    yield
            for sub in range(4):
                t0 = ti * 512 + sub * 128
                X, dX = Xr.next()
                K.pool.dma(lambda e, X=X, t0=t0: e.dma_start(out=X[:], in_=io["x"][t0:t0 + 128, :]), writes=[dX])
                po, dpo = PO2.next()
                for n in range(2):
                    for m in range(8):
                        K.pe.op(lambda e, po=po, MG=MG, m=m, n=n, sub=sub: e.matmul(
                            po[:, n, :], MG[:, m, sub * 128:(sub + 1) * 128], WO[:, m, n * 512:(n + 1) * 512],
                            start=(m == 0), stop=(m == 7)), reads=[dMG, d_c],
                            writes=[dpo] if (m == 0 and n == 0) else [], adds=[] if (m == 0 and n == 0) else [dpo])
                X1, dX1 = X1r.next()
                K.dve.op(lambda e, X1=X1, X=X, po=po: e.tensor_tensor(out=X1[:], in0=X[:], in1=po[:].rearrange("p a b -> p (a b)"),
                                                                     op=ALU.add), reads=[dX, dpo], writes=[dX1])
                yield
                K.sp.dma(lambda e, X1=X1, t0=t0: e.dma_start(out=scr["x1_tm"][t0:t0 + 128, :], in_=X1[:]),
                         reads=[dX1], adds=[scr["d_p4"]])
                ST, dST = STr.next()
                K.act.op(lambda e, X1=X1, ST=ST: e.activation(out=junk[:], in_=X1[:], func=AF.Square, accum_out=ST[:, 0:1]),
                         reads=[dX1], writes=[d_junk, dST])
                K.dve.op(lambda e, ST=ST: e.tensor_scalar(out=ST[:, 1:2], in0=ST[:, 0:1], scalar1=1.0 / D, scalar2=1e-6,
                                                          op0=ALU.mult, op1=ALU.add), writes=[dST])
                K.act.op(lambda e, ST=ST: e.activation(out=ST[:, 2:3], in_=ST[:, 1:2], func=AF.Ln), writes=[dST])
                K.act.op(lambda e, ST=ST: e.activation(out=ST[:, 3:4], in_=ST[:, 2:3], func=AF.Exp, scale=-0.5), writes=[dST])
                yield
                XH, dXH = XHr.next()
                K.dve.op(lambda e, XH=XH, X1=X1, ST=ST: e.scalar_tensor_tensor(
                    out=XH[:], in0=X1[:], scalar=ST[:, 3:4], in1=NF[:], op0=ALU.mult, op1=ALU.mult),
                    reads=[dX1, dST, d_c], writes=[dXH])
                K.sp.dma(lambda e, XH=XH, t0=t0: e.dma_start(out=scr["xh_tm"][t0:t0 + 128, :], in_=XH[:]),
                         reads=[dXH], adds=[scr["d_p4"]])
                XB, dXB = XBr.next()
                K.act.op(lambda e, XB=XB, XH=XH: e.activation(out=XB[:], in_=XH[:], func=AF.Copy), reads=[dXH], writes=[dXB])
                yield
                ptp, dptp = PTp.next()
                for kc in range(8):
                    K.pe.op(lambda e, ptp=ptp, XB=XB, kc=kc: e.transpose(out=ptp[:, kc, :], in_=XB[:, kc * 128:(kc + 1) * 128],
                                                                        identity=identb[:]),
                            reads=[dXB, d_c], writes=[dptp] if kc == 0 else [], adds=[] if kc == 0 else [dptp])
                XT, dXT = XTr.next()
                K.act.op(lambda e, ptp=ptp, XT=XT: e.activation(out=XT[:], in_=ptp[:], func=AF.Copy), reads=[dptp], writes=[dXT])
                K.sp.dma(lambda e, XT=XT, t0=t0: e.dma_start(
                    out=dap(scr["xhT_fm"], t0, [[T, 128], [128 * T, 8], [1, 128]]), in_=XT[:]),
                    reads=[dXT], adds=[scr["d_p4"]])
                yield

        work = [(ti,) for ti in range(NT)]

        active = []
        nxt = 0
        while nxt < len(work) or active:
            if len(active) < 2 and nxt < len(work):
                active.append(make_gen(*work[nxt]))
                nxt += 1
            for a in list(active):
                try:
                    next(a)
                except StopIteration:
                    active.remove(a)


def phase5(K, cfg, io, scr):
    T, S, NB = cfg["T"], cfg["S"], cfg["NB"]
    NTT = T // 128
    with ExitStack() as es:
        d_c = Dep()
        identb = K.sb([128, 128], BF16, "f_identb", es)
        K.sp.dma(lambda e: e.dma_start(out=identb[:], in_=io["ident_bf"][:, :]), adds=[d_c])
        IOTA = K.sb([128, 16], F32, "IOTA", es)
        K.sp.dma(lambda e: e.dma_start(out=IOTA[:], in_=io["iota16"][:, :]), adds=[d_c])
        FNW = K.sb([128, D], F32, "FNW", es)
        K.sp.dma(lambda e: e.dma_start(out=FNW[:], in_=io["final_norm_w"][0:1, :].broadcast_to([128, D])), adds=[d_c])
        WQ = K.sb([128, 8, 2048], BF16, "WQ", es)
        KT = K.sb([128, 16, 128], BF16, "KT", es)
        PQ = Rot(K, 1, [128, 8, 128], F32, "fPQ", psum=True, es=es)
        PSc = Rot(K, 1, [128, 8, 128], F32, "fPSc", psum=True, es=es)
        PKT = Rot(K, 1, [128, 8, 128], BF16, "fPKT", psum=True, es=es)
        es_setup = ExitStack()
        stg = Rot(K, 2, [128, 1024], F32, "f_stg", es=es_setup)
        q = [0]
        load_w_bf16(K, es, io["peer_wq"][0], D, 2048, "WQ", d_c, stg, q, W=WQ)
        KF = K.sb([128, 16, 128], F32, "KF", es_setup)
        dKF = Dep()
        K.sp.dma(lambda e: e.dma_start(out=KF[:], in_=io["peer_keys"][0].rearrange("h c n d -> n (h c) d")), writes=[dKF])
        KB = K.sb([128, 16, 128], BF16, "KB", es_setup)
        K.dve.op(lambda e: e.tensor_copy(out=KB[:], in_=KF[:]), reads=[dKF], writes=[dKF])
        for half in range(2):
            pk, dpk = PKT.next()
            for i in range(8):
                K.pe.op(lambda e, pk=pk, i=i, half=half: e.transpose(out=pk[:, i, :], in_=KB[:, half * 8 + i, :], identity=identb[:]),
                        reads=[dKF, d_c], writes=[dpk] if i == 0 else [], adds=[] if i == 0 else [dpk])
            K.act.op(lambda e, pk=pk, half=half: e.activation(out=KT[:, half * 8:(half + 1) * 8, :], in_=pk[:], func=AF.Copy),
                     reads=[dpk], adds=[d_c])

        K.barrier()
        es_setup.close()
        XTr = Rot(K, 2, [128, 8, 128], BF16, "fXT", es=es)
        XHr = Rot(K, 2, [128, D], F32, "fXH", es=es)
        X1r = Rot(K, 2, [128, D], F32, "fX1", es=es)
        QTr = Rot(K, 1, [128, 16, 128], BF16, "fQT", es=es)
        SCr = Rot(K, 1, [128, 16, 128], F32, "fSC", es=es)
        SC2 = K.sb([128, 256], F32, "fSC2", es)
        dSC2 = Dep()
        M16r = Rot(K, 1, [128, 16, 16], F32, "fM16", es=es)
        I16r = Rot(K, 1, [128, 16, 16], U32, "fI16", es=es)
        I16fr = Rot(K, 1, [128, 16, 16], F32, "fI16f", es=es)
        CANDr = Rot(K, 1, [128, 8, 256], F32, "fCAND", es=es)
        VALr = Rot(K, 1, [128, 8, 16], F32, "fVAL", es=es)
        CIr = Rot(K, 1, [128, 3, 128], U32, "fCI", es=es)
        ABr = Rot(K, 1, [128, 2, 128], F32, "fAB", es=es)
        OHr = CANDr
        E12r = Rot(K, 1, [128, 3, 128], F32, "fE12", es=es)
        IDSr = Rot(K, 2, [128, 128], I32, "fIDS", es=es)
        GTr = Rot(K, 2, [128, 4, 128], F32, "fGT", es=es)
        S8r = Rot(K, 2, [128, 16], F32, "fS8", es=es)
        GRP = cfg.get("grp", 4)
        ACTDOT = tuple(cfg.get("actdot", (0, 2)))
        if isinstance(cfg.get("actdot_mask"), int):
            ACTDOT = tuple(i for i in range(GRP) if (cfg["actdot_mask"] >> i) & 1)
        junk3r = Rot(K, 2, [128, D], BF16, "fjunk3", es=es)
        junkr = Rot(K, 3, [128, D], BF16, "fjunkr", es=es)
        PRDr = Rot(K, 3, [128, D], BF16, "fPRD", es=es)
        GBr = Rot(K, cfg.get("ngbuf", 22), [128, 2 * D], BF16, "fGB", es=es)
        junk2 = K.sb([128, D], BF16, "fjunk2", es)
        d_junk2 = Dep()
        XHbr = Rot(K, 2, [128, D], BF16, "fXHb", es=es)
        DGr = Rot(K, 4, [128, 128], BF16, "fDG", es=es)
        PY = Rot(K, 1, [128, 2, 512], F32, "fPY", psum=True, es=es)

        RES = {}

        def routing(ti):
            t0 = ti * 128
            XT, dXT = XTr.next()
            XH, dXH = XHr.next()
            X1, dX1 = X1r.next()
            K.sp.dma(lambda e, XT=XT, t0=t0: e.dma_start(out=XT[:], in_=dap(scr["xhT_fm"], t0, [[T, 128], [128 * T, 8], [1, 128]])),
                     reads=[scr["d_p4"]], writes=[dXT])
            K.sp.dma(lambda e, XH=XH, t0=t0: e.dma_start(out=XH[:], in_=scr["xh_tm"][t0:t0 + 128, :]), reads=[scr["d_p4"]], writes=[dXH])
            K.sp.dma(lambda e, X1=X1, t0=t0: e.dma_start(out=X1[:], in_=scr["x1_tm"][t0:t0 + 128, :]), reads=[scr["d_p4"]], writes=[dX1])
            QT, dQT = QTr.next()
            for half in range(2):
                pq, dpq = PQ.next()
                for i in range(8):
                    hc = half * 8 + i
                    for kc in range(8):
                        K.pe.op(lambda e, pq=pq, i=i, hc=hc, kc=kc, XT=XT: e.matmul(
                            pq[:, i, :], WQ[:, kc, hc * 128:(hc + 1) * 128], XT[:, kc, :], start=(kc == 0), stop=(kc == 7)),
                            reads=[dXT, d_c], writes=[dpq] if (i == 0 and kc == 0) else [], adds=[] if (i == 0 and kc == 0) else [dpq])
                K.act.op(lambda e, pq=pq, QT=QT, half=half: e.activation(out=QT[:, half * 8:(half + 1) * 8, :], in_=pq[:], func=AF.Copy),
                         reads=[dpq], writes=[dQT] if half == 0 else [], adds=[] if half == 0 else [dQT])
            SC, dSC = SCr.next()
            for half in range(2):
                psc, dpsc = PSc.next()
                for i in range(8):
                    hc = half * 8 + i
                    K.pe.op(lambda e, psc=psc, i=i, hc=hc, QT=QT: e.matmul(psc[:, i, :], QT[:, hc, :], KT[:, hc, :], start=True, stop=True),
                            reads=[dQT, d_c], writes=[dpsc] if i == 0 else [], adds=[] if i == 0 else [dpsc])
                K.act.op(lambda e, psc=psc, SC=SC, half=half: e.activation(out=SC[:, half * 8:(half + 1) * 8, :], in_=psc[:], func=AF.Copy),
                         reads=[dpsc], writes=[dSC] if half == 0 else [], adds=[] if half == 0 else [dSC])
            yield
            M16, dM = M16r.next()
            I16, dI = I16r.next()
            for hc in range(16):
                if hc % 4 == 0 and hc > 0:
                    yield
                K.dve.op(lambda e, M16=M16, SC=SC, hc=hc: e.max(out=M16[:, hc, 0:8], in_=SC[:, hc, :]), reads=[dSC],
                         writes=[dM] if hc == 0 else [], adds=[] if hc == 0 else [dM])
                K.dve.op(lambda e, M16=M16, SC=SC, hc=hc: e.match_replace(out=SC2[:, 0:128], in_to_replace=M16[:, hc, 0:8],
                                                                         in_values=SC[:, hc, :], imm_value=-1e30),
                         reads=[dSC, dM], writes=[dSC2])
                K.dve.op(lambda e, M16=M16, hc=hc: e.max(out=M16[:, hc, 8:16], in_=SC2[:, 0:128]), reads=[dSC2], adds=[dM])
                K.dve.op(lambda e, M16=M16, I16=I16, SC=SC, hc=hc: e.max_index(out=I16[:, hc, 0:8], in_max=M16[:, hc, 0:8],
                                                                              in_values=SC[:, hc, :]),
                         reads=[dSC, dM], writes=[dI] if hc == 0 else [], adds=[] if hc == 0 else [dI])
                K.dve.op(lambda e, M16=M16, I16=I16, SC=SC, hc=hc: e.max_index(out=I16[:, hc, 8:16], in_max=M16[:, hc, 8:16],
                                                                              in_values=SC[:, hc, :]),
                         reads=[dSC, dM], adds=[dI])
            yield
            I16f, dIf = I16fr.next()
            K.dve.op(lambda e, I16f=I16f, I16=I16: e.tensor_copy(out=I16f[:], in_=I16[:]), reads=[dI], writes=[dIf])
            I16fv = I16f[:].rearrange("p (h c) k -> p h c k", c=2)
            K.dve.op(lambda e, I16fv=I16fv: e.tensor_scalar(out=I16fv[:, :, 0, :], in0=I16fv[:, :, 0, :], scalar1=128.0, scalar2=None,
                                                            op0=ALU.mult), writes=[dIf])
            CAND, dCA = CANDr.next()
            M16v = M16[:].rearrange("p (h c) k -> p h c k", c=2)
            K.dve.op(lambda e, CAND=CAND, M16v=M16v: e.tensor_tensor(
                out=CAND[:].rearrange("p h (a b) -> p h a b", b=16),
                in0=M16v[:, :, 0, :].unsqueeze(3).broadcast_to([128, 8, 16, 16]),
                in1=M16v[:, :, 1, :].unsqueeze(2).broadcast_to([128, 8, 16, 16]), op=ALU.add),
                reads=[dM], writes=[dCA])
            VAL, dVAL = VALr.next()
            CI, dCI = CIr.next()
            CIv = CI[:, 0, :].rearrange("p (h k) -> p h k", k=16)
            for h in range(8):
                if h % 4 == 0:
                    yield
                K.dve.op(lambda e, VAL=VAL, CAND=CAND, h=h: e.max(out=VAL[:, h, 0:8], in_=CAND[:, h, :]), reads=[dCA],
                         writes=[dVAL] if h == 0 else [], adds=[] if h == 0 else [dVAL])
                K.dve.op(lambda e, VAL=VAL, CAND=CAND, h=h: e.match_replace(out=SC2[:, :], in_to_replace=VAL[:, h, 0:8],
                                                                           in_values=CAND[:, h, :], imm_value=-1e30),
                         reads=[dCA, dVAL], writes=[dSC2])
                K.dve.op(lambda e, VAL=VAL, h=h: e.max(out=VAL[:, h, 8:16], in_=SC2[:, :]), reads=[dSC2], adds=[dVAL])
                K.dve.op(lambda e, VAL=VAL, CIv=CIv, CAND=CAND, h=h: e.max_index(out=CIv[:, h, 0:8], in_max=VAL[:, h, 0:8],
                                                                                in_values=CAND[:, h, :]),
                         reads=[dCA, dVAL], writes=[dCI] if h == 0 else [], adds=[] if h == 0 else [dCI])
                K.dve.op(lambda e, VAL=VAL, CIv=CIv, CAND=CAND, h=h: e.max_index(out=CIv[:, h, 8:16], in_max=VAL[:, h, 8:16],
                                                                                in_values=CAND[:, h, :]),
                         reads=[dCA, dVAL], adds=[dCI])
            yield
            GT, dGT = GTr.next()
            S8, dS8 = S8r.next()
            Ev = GT[:, 0, :].rearrange("p (h k) -> p h k", k=16)
            Gv = GT[:, 1, :].rearrange("p (h k) -> p h k", k=16)
            K.dve.op(lambda e, Ev=Ev, VAL=VAL: e.tensor_tensor(out=Ev, in0=VAL[:], in1=VAL[:, :, 0:1].broadcast_to([128, 8, 16]),
                                                              op=ALU.subtract), reads=[dVAL], writes=[dGT])
            K.act.op(lambda e, GT=GT: e.activation(out=GT[:, 0, :], in_=GT[:, 0, :], func=AF.Exp), writes=[dGT])
            K.dve.op(lambda e, Ev=Ev, S8=S8: e.tensor_reduce(out=S8[:, 0:8], in_=Ev, axis=AX.X, op=ALU.add), reads=[dGT], writes=[dS8])
            K.dve.op(lambda e, S8=S8: e.reciprocal(out=S8[:, 8:16], in_=S8[:, 0:8]), writes=[dS8])
            K.dve.op(lambda e, Ev=Ev, Gv=Gv, S8=S8: e.tensor_tensor(out=Gv, in0=Ev, in1=S8[:, 8:16].unsqueeze(2).broadcast_to([128, 8, 16]),
                                                                   op=ALU.mult), reads=[dS8], writes=[dGT])
            yield
            K.dve.op(lambda e, CI=CI: e.tensor_single_scalar(out=CI[:, 1, :], in_=CI[:, 0, :], scalar=4, op=ALU.logical_shift_right),
                     writes=[dCI])
            K.dve.op(lambda e, CI=CI: e.tensor_single_scalar(out=CI[:, 2, :], in_=CI[:, 0, :], scalar=15, op=ALU.bitwise_and),
                     writes=[dCI])
            AB, dAB = ABr.next()
            K.dve.op(lambda e, AB=AB, CI=CI: e.tensor_copy(out=AB[:], in_=CI[:, 1:3, :]), reads=[dCI], writes=[dAB])
            OH, dOH = OHr.next()
            E12, dE12 = E12r.next()
            OHv = OH[:].rearrange("p h (j a) -> p h j a", a=16)
            for c in range(2):
                ABv = AB[:, c, :].rearrange("p (h j) -> p h j", j=16)
                K.dve.op(lambda e, OHv=OHv, ABv=ABv: e.tensor_tensor(
                    out=OHv, in0=ABv.unsqueeze(3).broadcast_to([128, 8, 16, 16]),
                    in1=IOTA[:].unsqueeze(1).unsqueeze(1).broadcast_to([128, 8, 16, 16]), op=ALU.is_equal),
                    reads=[dAB, d_c], writes=[dOH])
                K.dve.op(lambda e, OHv=OHv, I16fv=I16fv, c=c: e.tensor_tensor(
                    out=OHv, in0=OHv, in1=I16fv[:, :, c, :].unsqueeze(2).broadcast_to([128, 8, 16, 16]), op=ALU.mult),
                    reads=[dIf], writes=[dOH])
                K.dve.op(lambda e, OHv=OHv, E12=E12, c=c: e.tensor_reduce(
                    out=E12[:, c, :].rearrange("p (h j) -> p h j", j=16), in_=OHv, axis=AX.X, op=ALU.add),
                    reads=[dOH], writes=[dE12] if c == 0 else [], adds=[] if c == 0 else [dE12])
            K.dve.op(lambda e, E12=E12: e.tensor_tensor(out=E12[:, 2, :], in0=E12[:, 0, :], in1=E12[:, 1, :], op=ALU.add), writes=[dE12])
            IDS, dIDS = IDSr.next()
            K.dve.op(lambda e, IDS=IDS, E12=E12: e.tensor_copy(out=IDS[:], in_=E12[:, 2, :]), reads=[dE12], writes=[dIDS])
            if "ids_dbg" in scr:
                K.sp.dma(lambda e, IDS=IDS, t0=t0: e.dma_start(out=scr["ids_dbg"][t0:t0 + 128, :], in_=IDS[:]), reads=[dIDS])
                K.sp.dma(lambda e, GT=GT, t0=t0: e.dma_start(out=scr["gate_dbg"][t0:t0 + 128, :], in_=GT[:, 1, :]), reads=[dGT])
            RES[ti] = dict(t0=t0, XH=XH, dXH=dXH, X1=X1, dX1=dX1, IDS=IDS, dIDS=dIDS, GT=GT, dGT=dGT, S8=S8, dS8=dS8)

        GDEPS = {}

        def expert(R):
            t0, XH, dXH, X1, dX1, IDS, dIDS, GT, dGT, S8, dS8 = (R[k] for k in
                ("t0", "XH", "dXH", "X1", "dX1", "IDS", "dIDS", "GT", "dGT", "S8", "dS8"))
            XHb, dXHb = XHbr.next()
            K.act.op(lambda e: e.activation(out=XHb[:], in_=XH[:], func=AF.Copy), reads=[dXH], writes=[dXHb])
            py, dpy = PY.next()
            NGRP = 128 // GRP
            bufs = {}
            gd = GDEPS.setdefault(id(GT), [(Dep(), Dep()) for _ in range(NGRP)])

            def stage_a(g):
                for jj in range(GRP):
                    j = g * GRP + jj
                    GB, dGB = GBr.next()
                    bufs[j] = (GB, dGB)
                    K.pool.dma(lambda e, GB=GB, j=j: e.indirect_dma_start(
                        out=GB[:], out_offset=None, in_=scr["uv_tab"][:, :],
                        in_offset=bass.IndirectOffsetOnAxis(ap=IDS[:, j:j + 1], axis=0)), reads=[dIDS, scr["d_uv"]], writes=[dGB])
                    if jj in ACTDOT:
                        PRD, dPRD = PRDr.next()
                        K.dve.op(lambda e, GB=GB, PRD=PRD: e.tensor_tensor(out=PRD[:], in0=GB[:, 0:D], in1=XHb[:], op=ALU.mult),
                                 reads=[dGB, dXHb], writes=[dPRD])
                        j3, dj3 = junk3r.next()
                        K.act.op(lambda e, PRD=PRD, j=j, j3=j3: e.activation(out=j3[:], in_=PRD[:], func=AF.Copy, accum_out=GT[:, 2, j:j + 1]),
                                 reads=[dPRD], writes=[dj3] + ([gd[g][0]] if jj == 0 else []), adds=[] if jj == 0 else [gd[g][0]])
                    else:
                        j1, dj1 = junkr.next()
                        K.dve.op(lambda e, GB=GB, j=j, j1=j1: e.scalar_tensor_tensor(
                            out=j1[:], in0=GB[:, 0:D], scalar=1.0, in1=XHb[:], op0=ALU.mult, op1=ALU.mult, accum_out=GT[:, 2, j:j + 1]),
                            reads=[dGB, dXHb], writes=[dj1] + ([gd[g][0]] if jj == 0 else []), adds=[] if jj == 0 else [gd[g][0]])
                gs = slice(g * GRP, (g + 1) * GRP)
                K.act.op(lambda e: e.activation(out=GT[:, 3, gs], in_=GT[:, 2, gs], func=AF.Gelu), reads=[gd[g][0]], writes=[gd[g][1]])

            def stage_b(g):
                gs = slice(g * GRP, (g + 1) * GRP)
                K.dve.op(lambda e: e.tensor_tensor(out=GT[:, 3, gs], in0=GT[:, 3, gs], in1=GT[:, 1, gs], op=ALU.mult), reads=[dGT], writes=[gd[g][1]])
                for jj in range(GRP):
                    j = g * GRP + jj
                    GB, dGB = bufs.pop(j)
                    DG, dDG = DGr.next()
                    K.act.op(lambda e, DG=DG, j=j: e.activation(out=DG[:], in_=identb[:], func=AF.Copy, scale=GT[:, 3, j:j + 1]),
                             reads=[gd[g][1], d_c], writes=[dDG])
                    for n in range(2):
                        K.pe.op(lambda e, DG=DG, GB=GB, n=n, j=j: e.matmul(
                            py[:, n, :], DG[:], GB[:, D + n * 512:D + (n + 1) * 512], start=(j == 0), stop=(j == 127)),
                            reads=[dDG, dGB], writes=[dpy] if (j == 0 and n == 0) else [], adds=[] if (j == 0 and n == 0) else [dpy])

            gen = routing(R["next"]) if R.get("next") is not None else None
            for g in range(NGRP):
                stage_a(g)
                if g >= 1:
                    stage_b(g - 1)
                if gen is not None and g >= 2 and g % 2 == 0:
                    try:
                        next(gen)
                    except StopIteration:
                        gen = None
            stage_b(NGRP - 1)
            if gen is not None:
                for _ in gen:
                    pass
            K.dve.op(lambda e: e.tensor_tensor(out=X1[:], in0=X1[:], in1=py[:].rearrange("p a b -> p (a b)"), op=ALU.add),
                     reads=[dpy], writes=[dX1])
            K.act.op(lambda e: e.activation(out=junk2[:], in_=X1[:], func=AF.Square, accum_out=S8[:, 0:1]),
                     reads=[dX1], writes=[d_junk2, dS8])
            K.dve.op(lambda e: e.tensor_scalar(out=S8[:, 1:2], in0=S8[:, 0:1], scalar1=1.0 / D, scalar2=1e-6,
                                               op0=ALU.mult, op1=ALU.add), writes=[dS8])
            K.act.op(lambda e: e.activation(out=S8[:, 2:3], in_=S8[:, 1:2], func=AF.Sqrt), writes=[dS8])
            K.dve.op(lambda e: e.reciprocal(out=S8[:, 3:4], in_=S8[:, 2:3]), writes=[dS8])
            K.dve.op(lambda e: e.scalar_tensor_tensor(
                out=XH[:], in0=X1[:], scalar=S8[:, 3:4], in1=FNW[:], op0=ALU.mult, op1=ALU.mult),
                reads=[dX1, dS8, d_c], writes=[dXH])
            K.sp.dma(lambda e: e.dma_start(out=io["out"][t0:t0 + 128, :], in_=XH[:]), reads=[dXH])

        for _ in routing(0):
            pass
        for ti in range(NTT):
            R = RES.pop(ti)
            R["next"] = ti + 1 if ti + 1 < NTT else None
            expert(R)


def make_consts():
    c = {}
    c["ident_bf"] = np.eye(128, dtype=np.float32).astype(ml_dtypes.bfloat16)
    c["ident_f"] = np.eye(128, dtype=np.float32)
    bo = np.zeros((128, 128), np.float32)
    bo[:64, :64] = 1.0 / 64
    bo[64:, 64:] = 1.0 / 64
    c["blockones"] = bo
    pp = np.arange(128)[:, None]
    ff = np.arange(128)[None, :]
    c["tri"] = ((pp <= ff) & (pp // 64 == ff // 64)).astype(np.float32)
    p6 = np.arange(64)[:, None]
    f6 = np.arange(64)[None, :]
    mk = np.zeros((64, 3, 64), np.float32)
    mk[:, 0, :] = (f6 < p6)
    mk[:, 1, :] = (f6 > p6)
    mk[:, 2, :] = (f6 >= p6)
    c["masks"] = mk
    c["ident64"] = np.eye(64, dtype=np.float32).astype(ml_dtypes.bfloat16)
    c["ones64"] = np.ones((64, 1), np.float32)
    c["iota16"] = np.tile(np.arange(16, dtype=np.float32)[None, :], (128, 1))
    return c


def make_alibi(S):
    al = np.zeros((4, 128, S), np.float32)
    ql = np.arange(128)[:, None]
    m = np.arange(S)[None, :]
    for h in range(4):
        slope = 2.0 ** (-8.0 * (h + 1) / 4)
        v = -slope * (ql - m + (S - 128)).astype(np.float32)
        al[h] = np.where(m <= ql + (S - 128), v, -30000.0)
    return al


def _unused():
    c = {}
    return c


def build(cfg):
    NB, S = cfg["NB"], cfg["S"]
    T = NB * S
    cfg["T"] = T
    dbg = set(cfg.get("debug", ()))
    phases = cfg.get("phases", (1,))
    nc = bass.Bass("TRN2", target_bir_lowering=False)
    io = {}

    def inp(name, shape, dt=F32):
        io[name] = nc.dram_tensor(name, list(shape), dt, kind="ExternalInput").ap()

    inp("x", [T, D])
    inp("norm_mix_w", [1, D])
    inp("w_in", [1, D, IN_COLS])
    inp("ident_bf", [128, 128], BF16)
    inp("ident_f", [128, 128])
    inp("blockones", [128, 128])
    inp("tri", [128, 128])
    inp("masks", [64, 3, 64])
    inp("ident64", [64, 64], BF16)
    inp("ones64", [64, 1])
    inp("alibi", [4, 128, S])
    for nm, shp in [("lam_q1", [1, 64]), ("lam_k1", [1, 64]), ("lam_q2", [1, 64]), ("lam_k2", [1, 64]),
                    ("subln_w", [1, 128])]:
        inp(nm, shp)
    scr = {}

    def scratch(name, shape, dt):
        kind = "ExternalOutput" if name in dbg else "Internal"
        scr[name] = nc.dram_tensor(name, list(shape), dt, kind=kind).ap()

    scratch("zs_tm", [T, SHIFT_COLS], F32)
    scratch("zv_fm", [512, T], F32)
    scratch("qk_fm", [1024, T], BF16)
    scratch("av_tm", [T, 512], BF16)
    scratch("sg_fm", [2048, T], BF16)
    if not cfg.get("chunked", True):
        scratch("rw_tm", [T, 5, 512], F32)
    scratch("ab_tm", [T, 5, 512], BF16)
    scratch("lw_tm", [T, 512], F32)
    scratch("v_fm", [512, T], F32)
    scratch("g_fm", [512, T], F32)
    scratch("coef_fm", [8, T], F32)
    scratch("y_fm", [512, T], F32)
    scratch("ya_fm", [512, T], BF16)
    scr["d_p1"] = Dep()
    scr["d_p2a"] = Dep()
    scr["d_p2b"] = Dep()
    scr["d_p2c"] = Dep()
    scr["d_p3"] = Dep()
    scr["d_p4"] = Dep()
    scr["d_uv"] = Dep()
    scratch("uv_tab", [16384, 2 * D], BF16)
    if "ids_dbg" in dbg:
        scratch("ids_dbg", [T, 128], I32)
        scratch("gate_dbg", [T, 128], F32)
    inp("peer_wq", [1, D, 2048])
    inp("peer_keys", [1, 8, 2, 128, 128])
    inp("peer_u", [1, 16384, D])
    inp("peer_v", [1, 16384, D])
    inp("final_norm_w", [1, D])
    inp("iota16", [128, 16])
    io["out"] = nc.dram_tensor("out", [T, D], F32, kind="ExternalOutput").ap()
    scratch("x1_tm", [T, D], F32)
    scratch("xh_tm", [T, D], F32)
    scratch("xhT_fm", [D, T], BF16)
    for nm, shp in [("proj_a", [1, 512, D]), ("proj_b", [1, 512, D]), ("w_out", [1, D, D]), ("norm_ffn_w", [1, D])]:
        inp(nm, shp)
    scratch("yb_fm", [512, T], BF16)
    for nm, shp in [("shift_mu", [1, SHIFT_COLS]), ("w0", [1, 512]), ("w2", [1, 64, 512]), ("a0", [1, 512]),
                    ("a2", [1, 64, 512]), ("g2", [1, 128, 512]), ("k_k", [1, 512]), ("k_a", [1, 512]),
                    ("r_k", [1, 8, 64]), ("lnx_w", [1, 512]), ("lnx_b", [1, 512])]:
        inp(nm, shp)
    with ExitStack() as es:
        K = Kern(nc, es, pool_slots=cfg.get("pool_slots", 8))
        K.scopes = bool(cfg.get("scopes", False))
        if 5 in phases:
            K.phase = "p0_uvtab"
            RB = 2048
            for r0 in range(0, 16384, RB):
                K.pool.dma(lambda e, r0=r0: e.dma_start(out=scr["uv_tab"][r0:r0 + RB, 0:D], in_=io["peer_u"][0, r0:r0 + RB, :]),
                           adds=[scr["d_uv"]])
                K.pool.dma(lambda e, r0=r0: e.dma_start(out=scr["uv_tab"][r0:r0 + RB, D:2 * D], in_=io["peer_v"][0, r0:r0 + RB, :]),
                           adds=[scr["d_uv"]])
        if 1 in phases:
            K.phase = "p1_inproj"
            phase1(K, cfg, io, scr)
            K.barrier()
        if 2 in phases:
            K.phase = "p2a_prep"
            phase2_prep(K, cfg, io, scr)
            K.barrier()
            K.phase = "p2b_scan"
            if cfg.get("chunked", True):
                phase2_chunk(K, cfg, io, scr)
            else:
                phase2_scan(K, cfg, io, scr)
            K.barrier()
            K.phase = "p2c_post"
            phase2_post(K, cfg, io, scr)
            K.barrier()
        if 3 in phases:
            K.phase = "p3_attn"
            phase3(K, cfg, io, scr)
            K.barrier()
        if 4 in phases:
            K.phase = "p4_merge"
            phase4(K, cfg, io, scr)
            K.barrier()
        if 5 in phases:
            K.phase = "p5_peer"
            phase5(K, cfg, io, scr)
            K.barrier()
        K.finish()
    return nc, io, scr


def kernel(**inputs):
    NB, S = 4, 2048
    cfg = dict(NB=NB, S=S, phases=(1, 2, 3, 4, 5))
    nc, io, scr = build(cfg)
    consts = make_consts()
    consts["alibi"] = make_alibi(S)
    x = np.ascontiguousarray(np.asarray(inputs["x"], dtype=np.float32))
    shared = {}
    for name in io:
        if name in ("x", "out"):
            continue
        if name in consts:
            shared[name] = consts[name]
        elif name == "final_norm_w":
            shared[name] = np.ascontiguousarray(np.asarray(inputs[name], dtype=np.float32).reshape(1, D))
        else:
            shared[name] = np.ascontiguousarray(np.asarray(inputs[name], dtype=np.float32))
    in_maps = []
    for c in range(NCORES):
        m = dict(shared)
        m["x"] = x[c * NB:(c + 1) * NB].reshape(NB * S, D)
        in_maps.append(m)
    res = run_bass_kernel_spmd(nc, in_maps, core_ids=list(range(NCORES)))
    out = np.concatenate([np.asarray(r["out"]).reshape(NB, S, D) for r in res.results], axis=0)
    return out.astype(np.float32)
```

```python
import numpy as np
import ml_dtypes
from contextlib import ExitStack
import concourse.bass as bass
import concourse.mybir as mybir
from concourse.bass_utils import run_bass_kernel_spmd

F32 = mybir.dt.float32
BF16 = mybir.dt.bfloat16
I32 = mybir.dt.int32
U32 = mybir.dt.uint32
ALU = mybir.AluOpType
AF = mybir.ActivationFunctionType
AX = mybir.AxisListType

D = 1024
IN_COLS = 5376
SHIFT_COLS = 1792
NCORES = 8


class Dep:
    __slots__ = ("w", "r", "pw", "pr")

    def __init__(self):
        self.w = {}
        self.r = {}
        self.pw = {}
        self.pr = {}


class Stream:
    def __init__(self, K, name, is_pe=False, ndma=0):
        self.K = K
        self.name = name
        self.sem = K.new_sem("s_" + name)
        self.cnt = 0
        self.items = []
        self.waited = {}
        self.is_pe = is_pe
        self.dsems = [K.new_sem("d_%s%d" % (name, i)) for i in range(ndma)]
        self.duses = [0] * ndma
        self.dj = 0

    def wait_tok(self, tok):
        if tok is None:
            return
        sem, val = tok
        if sem is self.sem and self.is_pe:
            return
        key = id(sem)
        if self.waited.get(key, 0) >= val:
            return
        self.waited[key] = val
        self.items.append(("w", sem, val, self.K.phase))

    def _pre(self, reads, writes, adds):
        for d in reads:
            for t in list(d.w.values()):
                self.wait_tok(t)
        for d in writes:
            for t in list(d.w.values()):
                self.wait_tok(t)
            for t in list(d.r.values()):
                self.wait_tok(t)
        for d in adds:
            for t in list(d.r.values()) + list(d.pr.values()) + list(d.pw.values()):
                self.wait_tok(t)

    def _post(self, tok, reads, writes, adds):
        for d in reads:
            d.r[id(tok[0])] = tok
        for d in writes:
            d.pw = d.w
            d.pr = d.r
            d.w = {id(tok[0]): tok}
            d.r = {}
        for d in adds:
            d.w[id(tok[0])] = tok

    def op(self, fn, reads=(), writes=(), adds=()):
        self._pre(reads, writes, adds)
        self.cnt += 1
        tok = (self.sem, self.cnt)
        self.items.append(("o", fn, self.sem, 1, self.K.phase))
        self._post(tok, reads, writes, adds)
        return tok

    def dma(self, fn, reads=(), writes=(), adds=()):
        self._pre(reads, writes, adds)
        n = len(self.dsems)
        slot = self.dj % n
        self.dj += 1
        if self.duses[slot] > 0:
            self.wait_tok((self.dsems[slot], 16 * self.duses[slot]))
        self.duses[slot] += 1
        tok = (self.dsems[slot], 16 * self.duses[slot])
        self.items.append(("o", fn, self.dsems[slot], 16, self.K.phase))
        self._post(tok, reads, writes, adds)
        return tok

    def replay(self, eng):
        nc = self.K.nc
        cur = None
        ctx = None
        for it in self.items:
            ph = it[-1]
            if self.K.scopes and ph != cur:
                if ctx is not None:
                    ctx.__exit__(None, None, None)
                ctx = nc.named_scope(ph)
                ctx.__enter__()
                cur = ph
            if it[0] == "w":
                eng.wait_ge(it[1], it[2])
            else:
                ins = it[1](eng)
                ins.then_inc(it[2], it[3])
        if ctx is not None:
            ctx.__exit__(None, None, None)


class Kern:
    def __init__(self, nc, es, pool_slots=8):
        self.nc = nc
        self.es = es
        self.nsem = 0
        self.phase = "init"
        self.scopes = False
        self.pe = Stream(self, "pe", is_pe=True)
        self.act = Stream(self, "act", ndma=4)
        self.dve = Stream(self, "dve")
        self.pool = Stream(self, "pool", ndma=pool_slots)
        self.sp = Stream(self, "sp", ndma=8)
        self.uid = 0

    def new_sem(self, name):
        self.nsem += 1
        return self.es.enter_context(self.nc.semaphore(name))

    def sb(self, shape, dt, name=None, es=None):
        self.uid += 1
        nm = "%s_%d" % (name or "t", self.uid)
        return (es or self.es).enter_context(self.nc.sbuf_tensor(nm, list(shape), dt))

    def ps(self, shape, dt, name=None, es=None):
        self.uid += 1
        nm = "%s_%d" % (name or "p", self.uid)
        return (es or self.es).enter_context(self.nc.psum_tensor(nm, list(shape), dt))

    def dram(self, name, shape, dt, kind="Internal"):
        return self.nc.dram_tensor(name, list(shape), dt, kind=kind)

    def streams(self):
        return [self.pe, self.act, self.dve, self.pool, self.sp]

    def barrier(self):
        st = self.streams()
        toks = []
        for q in st:
            if q.cnt > 0:
                toks.append((q.sem, q.cnt))
            for i, sem in enumerate(q.dsems):
                if q.duses[i] > 0:
                    toks.append((sem, 16 * q.duses[i]))
        for s_ in st:
            for t in toks:
                s_.wait_tok(t)

    def finish(self):
        streams = [self.pe, self.act, self.dve, self.pool, self.sp]
        for s in streams:
            for q in streams:
                for i, sem in enumerate(q.dsems):
                    if q.duses[i] > 0:
                        s.wait_tok((sem, 16 * q.duses[i]))
        with self.nc.allow_non_contiguous_dma(reason="small strided param loads"), self.nc.Block() as block:
            @block.tensor
            def _(e):
                self.pe.replay(e)

            @block.scalar
            def _(e):
                self.act.replay(e)

            @block.vector
            def _(e):
                self.dve.replay(e)

            @block.gpsimd
            def _(e):
                self.pool.replay(e)

            @block.sync
            def _(e):
                self.sp.replay(e)


class Rot:
    def __init__(self, K, n, shape, dt, name, psum=False, es=None):
        self.t = [(K.ps if psum else K.sb)(shape, dt, name, es=es) for _ in range(n)]
        self.d = [Dep() for _ in range(n)]
        self.i = 0

    def next(self):
        j = self.i % len(self.t)
        self.i += 1
        return self.t[j], self.d[j]


def phase1(K, cfg, io, scr):
    nc = K.nc
    T = cfg["T"]
    NT = T // 512
    with ExitStack() as es:
        ident = K.sb([128, 128], BF16, "ident", es)
        d_ident = Dep()
        K.sp.dma(lambda e: e.dma_start(out=ident[:], in_=io["ident_bf"][:, :]), writes=[d_ident])
        nw = K.sb([128, 8], F32, "nw", es)
        d_nw = Dep()
        K.sp.dma(lambda e: e.dma_start(out=nw[:], in_=io["norm_mix_w"].rearrange("o (c p) -> p (o c)", p=128)),
                 writes=[d_nw])
        wt = K.sb([128, 8, IN_COLS], BF16, "wt", es)
        d_wt = Dep()
        wst = Rot(K, 2, [128, 1344], F32, "wst", es=es)
        q = 0
        for kc in range(8):
            for cp in range(4):
                st, dst = wst.next()
                eng = K.sp if q % 2 == 0 else K.act
                q += 1
                eng.dma(lambda e, st=st, kc=kc, cp=cp: e.dma_start(
                    out=st[:], in_=io["w_in"][0, kc * 128:(kc + 1) * 128, cp * 1344:(cp + 1) * 1344]), writes=[dst])
                K.act.op(lambda e, st=st, kc=kc, cp=cp: e.activation(
                    out=wt[:, kc, cp * 1344:(cp + 1) * 1344], in_=st[:], func=AF.Copy, scale=nw[:, kc:kc + 1]),
                    reads=[dst, d_nw], writes=[d_wt])

        xs = Rot(K, 2, [128, D], F32, "xs", es=es)
        junk = K.sb([128, D], BF16, "junk", es)
        d_junk = Dep()
        xn = Rot(K, 2, [128, D], BF16, "xn", es=es)
        st4 = Rot(K, 4, [128, 4], F32, "st4", es=es)
        hT = Rot(K, 2, [128, 8, 512], BF16, "hT", es=es)
        ptr = Rot(K, 2, [128, 8, 128], BF16, "ptr", psum=True, es=es)
        pmm = Rot(K, 4, [128, 512], F32, "pmm", psum=True, es=es)
        o32 = Rot(K, 3, [128, 512], F32, "o32", es=es)
        o16 = Rot(K, 3, [128, 512], BF16, "o16", es=es)
        ev = [0]

        def evac(pt, pd, ncols, kind, dst_ap):
            if kind == "f32":
                ot, od = o32.next()
            else:
                ot, od = o16.next()
            use_act = (kind == "sig") or (ev[0] % 2 == 0)
            ev[0] += 1
            if kind == "sig":
                K.act.op(lambda e: e.activation(out=ot[:, :ncols], in_=pt[:, :ncols], func=AF.Sigmoid),
                         reads=[pd], writes=[od])
            elif use_act:
                K.act.op(lambda e: e.activation(out=ot[:, :ncols], in_=pt[:, :ncols], func=AF.Copy),
                         reads=[pd], writes=[od])
            else:
                K.dve.op(lambda e: e.tensor_copy(out=ot[:, :ncols], in_=pt[:, :ncols]), reads=[pd], writes=[od])
            K.sp.dma(lambda e: e.dma_start(out=dst_ap, in_=ot[:, :ncols]), reads=[od], adds=[scr["d_p1"]])

        for ti in range(NT):
            h_t, h_d = hT.next()
            for sub in range(4):
                t0 = ti * 512 + sub * 128
                x_t, x_d = xs.next()
                K.sp.dma(lambda e, x_t=x_t, t0=t0: e.dma_start(out=x_t[:], in_=io["x"][t0:t0 + 128, :]),
                           writes=[x_d])
                s_t, s_d = st4.next()
                K.dve.op(lambda e, s_t=s_t: e.memset(s_t[:], 0.0), writes=[s_d])
                K.act.op(lambda e, x_t=x_t, s_t=s_t: e.activation(out=junk[:], in_=x_t[:], func=AF.Square,
                                                                    accum_out=s_t[:, 0:1]),
                         reads=[x_d], writes=[d_junk, s_d])
                K.dve.op(lambda e, s_t=s_t: e.tensor_scalar(out=s_t[:, 1:2], in0=s_t[:, 0:1], scalar1=1.0 / D,
                                                            scalar2=1e-6, op0=ALU.mult, op1=ALU.add),
                         reads=[s_d], writes=[s_d])
                K.act.op(lambda e, s_t=s_t: e.activation(out=s_t[:, 3:4], in_=s_t[:, 1:2], func=AF.Sqrt),
                         reads=[s_d], writes=[s_d])
                K.dve.op(lambda e, s_t=s_t: e.reciprocal(out=s_t[:, 2:3], in_=s_t[:, 3:4]),
                         reads=[s_d], writes=[s_d])
                n_t, n_d = xn.next()
                K.dve.op(lambda e, x_t=x_t, s_t=s_t, n_t=n_t: e.tensor_scalar(
                    out=n_t[:], in0=x_t[:], scalar1=s_t[:, 2:3], scalar2=None, op0=ALU.mult),
                    reads=[x_d, s_d], writes=[n_d])
                p_t, p_d = ptr.next()
                for kc in range(8):
                    K.pe.op(lambda e, p_t=p_t, n_t=n_t, kc=kc: e.transpose(
                        out=p_t[:, kc, :], in_=n_t[:, kc * 128:(kc + 1) * 128], identity=ident[:]),
                        reads=[n_d, d_ident], writes=[p_d])
                K.act.op(lambda e, p_t=p_t, h_t=h_t, sub=sub: e.activation(
                    out=h_t[:, :, sub * 128:(sub + 1) * 128], in_=p_t[:], func=AF.Copy),
                    reads=[p_d], writes=[h_d])
            tsl = slice(ti * 512, (ti + 1) * 512)
            for sub in range(4):
                r0 = ti * 512 + sub * 128
                for (c0, ncols, kind, name, dc0) in [(0, 512, "f32", "zs_tm", 0), (512, 512, "f32", "zs_tm", 512),
                                                    (1024, 512, "f32", "zs_tm", 1024),
                                                    (1536, 256, "f32", "zs_tm", 1536),
                                                    (2816, 512, "bf16", "av_tm", 0)]:
                    pt, pd = pmm.next()
                    for kc in range(8):
                        K.pe.op(lambda e, pt=pt, kc=kc, sub=sub, c0=c0, ncols=ncols, h_t=h_t: e.matmul(
                            pt[:, :ncols], h_t[:, kc, sub * 128:(sub + 1) * 128], wt[:, kc, c0:c0 + ncols],
                            start=(kc == 0), stop=(kc == 7)), reads=[h_d, d_wt], writes=[pd])
                    evac(pt, pd, ncols, kind, scr[name][r0:r0 + 128, dc0:dc0 + ncols])
            fm = []
            for j in range(8):
                fm.append((1792 + j * 128, "bf16", "qk_fm", j * 128))
            for j in range(16):
                fm.append((3328 + j * 128, "sig", "sg_fm", j * 128))
            for (c0, kind, name, r0) in fm:
                pt, pd = pmm.next()
                for kc in range(8):
                    K.pe.op(lambda e, pt=pt, kc=kc, c0=c0, h_t=h_t: e.matmul(
                        pt[:, :], wt[:, kc, c0:c0 + 128], h_t[:, kc, :], start=(kc == 0), stop=(kc == 7)),
                        reads=[h_d, d_wt], writes=[pd])
                evac(pt, pd, 512, kind, scr[name][r0:r0 + 128, tsl])


def dap(apobj, offset, dims):
    return bass.AP(tensor=apobj.tensor, offset=offset, ap=[list(d) for d in dims])


def bcast_load(K, eng, dst, src_row_ap, n, dep):
    eng.dma(lambda e: e.dma_start(out=dst, in_=src_row_ap.broadcast_to([128, n])), writes=[dep])


def phase2_prep(K, cfg, io, scr):
    T, S, NB = cfg["T"], cfg["S"], cfg["NB"]
    NTT = T // 128
    with ExitStack() as es:
        identb = K.sb([128, 128], BF16, "identb", es)
        identf = K.sb([128, 128], F32, "identf", es)
        d_c = Dep()
        K.sp.dma(lambda e: e.dma_start(out=identb[:], in_=io["ident_bf"][:, :]), adds=[d_c])
        K.sp.dma(lambda e: e.dma_start(out=identf[:], in_=io["ident_f"][:, :]), adds=[d_c])
        MU = K.sb([128, SHIFT_COLS], F32, "MU", es)
        PR = K.sb([128, 5, 512], F32, "PR", es)
        K.sp.dma(lambda e: e.dma_start(out=MU[:], in_=io["shift_mu"][0:1, :].broadcast_to([128, SHIFT_COLS])), adds=[d_c])
        for j, nm in enumerate(["w0", "a0", "k_k", "k_a"]):
            K.pool.dma(lambda e, j=j, nm=nm: e.dma_start(out=PR[:, j, :], in_=io[nm][0:1, :].broadcast_to([128, 512])),
                       adds=[d_c])
        K.pool.dma(lambda e: e.dma_start(out=PR[:, 4, :], in_=io["r_k"].rearrange("o h k -> o (h k)").broadcast_to([128, 512])),
                   adds=[d_c])
        cst = K.sb([128, 2], F32, "cst", es)
        K.dve.op(lambda e: e.memset(cst[:, 0:1], 1.0), adds=[d_c])
        K.dve.op(lambda e: e.memset(cst[:, 1:2], -0.5), adds=[d_c])
        wst = K.sb([128, 3, 512], F32, "lwst", es)
        d_wst = Dep()
        K.dve.op(lambda e: e.memset(wst[:], 0.0), writes=[d_wst])
        K.sp.dma(lambda e: e.dma_start(out=wst[0:64, 0, :], in_=io["w2"][0, :, :]), reads=[d_wst], adds=[d_wst])
        K.sp.dma(lambda e: e.dma_start(out=wst[64:128, 1, :], in_=io["a2"][0, :, :]), reads=[d_wst], adds=[d_wst])
        K.sp.dma(lambda e: e.dma_start(out=wst[:, 2, :], in_=io["g2"][0, :, :]), reads=[d_wst], adds=[d_wst])
        LW = K.sb([128, 3, 512], BF16, "LW", es)
        K.dve.op(lambda e: e.tensor_copy(out=LW[:], in_=wst[:]), reads=[d_wst], adds=[d_c])

        Zr = Rot(K, 2, [128, SHIFT_COLS], F32, "Z", es=es)
        Zpr = Rot(K, 2, [128, SHIFT_COLS], F32, "Zp", es=es)
        ZSr = Rot(K, 2, [128, SHIFT_COLS], F32, "ZS", es=es)
        OUTr = Rot(K, 2, [128, 5, 512], F32, "OUT", es=es)
        Er = Rot(K, 2, [128, 192], F32, "E", es=es)
        Lr = Rot(K, 2, [128, 256], BF16, "L", es=es)
        LTr = Rot(K, 2, [128, 2, 128], BF16, "LT", es=es)
        Ur = Rot(K, 2, [128, 512], F32, "U", es=es)
        UAr = Rot(K, 2, [128, 512], F32, "UA", es=es)
        KKr = Rot(K, 2, [128, 512], F32, "KKt", es=es)
        SQr = Rot(K, 2, [128, 512], F32, "SQ", es=es)
        T1r = Rot(K, 2, [128, 512], F32, "T1", es=es)
        T2r = Rot(K, 2, [128, 512], F32, "T2", es=es)
        S8r = Rot(K, 2, [128, 4, 8], F32, "S8", es=es)
        VTr = Rot(K, 2, [128, 4, 128], F32, "VT", es=es)
        GTr = Rot(K, 2, [128, 4, 128], F32, "GT", es=es)
        CTr = Rot(K, 2, [8, 128], F32, "CT", es=es)
        PT = Rot(K, 1, [128, 2, 128], BF16, "PT", psum=True, es=es)
        PW = Rot(K, 1, [128, 512], F32, "PW", psum=True, es=es)
        PA = Rot(K, 1, [128, 512], F32, "PA", psum=True, es=es)
        PG = Rot(K, 1, [128, 4, 128], F32, "PG", psum=True, es=es)
        PV = Rot(K, 1, [128, 4, 128], F32, "PV", psum=True, es=es)
        PC = Rot(K, 1, [8, 128], F32, "PC", psum=True, es=es)
        chunked = cfg.get("chunked", True)
        if chunked:
            PL = Rot(K, 1, [128, 512], F32, "PL", psum=True, es=es)
            TRI = K.sb([128, 128], F32, "TRI", es)
            K.sp.dma(lambda e: e.dma_start(out=TRI[:], in_=io["tri"][:, :]), adds=[d_c])
            LWr = Rot(K, 2, [128, 512], F32, "LWt", es=es)
            ELr = Rot(K, 2, [128, 3, 512], F32, "EL", es=es)
            ABr = Rot(K, 2, [128, 5, 512], BF16, "AB", es=es)
        dq = [0]

        def ldq():
            dq[0] += 1
            return K.sp if dq[0] % 2 == 0 else K.pool

        def tile_gen(ti):
            t0 = ti * 128
            first = (t0 % S == 0)
            Z, dZ = Zr.next()
            Zp, dZp = Zpr.next()
            ZS, dZS = ZSr.next()
            OUT, dO = OUTr.next()
            ldq().dma(lambda e, Z=Z, t0=t0: e.dma_start(out=Z[:], in_=scr["zs_tm"][t0:t0 + 128, :]),
                      reads=[scr["d_p1"]], writes=[dZ])
            if first:
                K.pool.op(lambda e, Zp=Zp: e.memset(Zp[0:32, :], 0.0), writes=[dZp])
                ldq().dma(lambda e, Zp=Zp, t0=t0: e.dma_start(out=Zp[1:128, :], in_=scr["zs_tm"][t0:t0 + 127, :]),
                          reads=[scr["d_p1"], dZp], adds=[dZp])
            else:
                ldq().dma(lambda e, Zp=Zp, t0=t0: e.dma_start(out=Zp[:], in_=scr["zs_tm"][t0 - 1:t0 + 127, :]),
                          reads=[scr["d_p1"]], writes=[dZp])
            CS = 1216
            dZSa, dZSb = Dep(), Dep()
            K.dve.op(lambda e, Z=Z, Zp=Zp, ZS=ZS: e.tensor_tensor(out=ZS[:, :CS], in0=Zp[:, :CS], in1=Z[:, :CS], op=ALU.subtract),
                     reads=[dZ, dZp], writes=[dZS])
            K.pool.op(lambda e, Z=Z, Zp=Zp, ZS=ZS: e.tensor_tensor(out=ZS[:, CS:], in0=Zp[:, CS:], in1=Z[:, CS:], op=ALU.subtract),
                      reads=[dZ, dZp, dZS], writes=[dZSb])
            K.dve.op(lambda e, ZS=ZS: e.tensor_tensor(out=ZS[:, :CS], in0=ZS[:, :CS], in1=MU[:, :CS], op=ALU.mult),
                     reads=[d_c, dZS], writes=[dZSa])
            K.pool.op(lambda e, ZS=ZS: e.tensor_tensor(out=ZS[:, CS:], in0=ZS[:, CS:], in1=MU[:, CS:], op=ALU.mult),
                      reads=[d_c], writes=[dZSb])
            K.dve.op(lambda e, Z=Z, ZS=ZS: e.tensor_tensor(out=ZS[:, :CS], in0=ZS[:, :CS], in1=Z[:, :CS], op=ALU.add),
                     reads=[dZ], writes=[dZSa])
            K.pool.op(lambda e, Z=Z, ZS=ZS: e.tensor_tensor(out=ZS[:, CS:], in0=ZS[:, CS:], in1=Z[:, CS:], op=ALU.add),
                      reads=[dZ], writes=[dZSb])
            K.dve.op(lambda e, ZS=ZS: e.tensor_copy(out=ZS[:, 0:1], in_=ZS[:, 0:1]), reads=[dZSa, dZSb], writes=[dZS])
            yield
            r_ap = ZS[:, 0:512]
            k_ap = ZS[:, 512:1024]
            K.act.op(lambda e, OUT=OUT, ZS=ZS: e.activation(out=OUT[:, 4, :], in_=ZS[:, 0:512], func=AF.Copy),
                     reads=[dZS], writes=[dO])
            yield
            E, dE = Er.next()
            L, dL = Lr.next()
            K.act.op(lambda e, E=E, ZS=ZS: e.activation(out=E[:, 0:64], in_=ZS[:, 1536:1600], func=AF.Exp, scale=-2.0),
                     reads=[dZS], writes=[dE])
            K.act.op(lambda e, E=E, ZS=ZS: e.activation(out=E[:, 64:192], in_=ZS[:, 1664:1792], func=AF.Exp, scale=-1.0),
                     reads=[dZS], adds=[dE])
            K.act.op(lambda e, E=E: e.activation(out=E[:], in_=E[:], func=AF.Ln, bias=cst[:, 0:1]), reads=[d_c], writes=[dE])
            K.act.op(lambda e, E=E: e.activation(out=E[:], in_=E[:], func=AF.Exp, scale=-1.0), writes=[dE])
            yield
            K.dve.op(lambda e, E=E, L=L: e.tensor_scalar(out=L[:, 0:64], in0=E[:, 0:64], scalar1=2.0, scalar2=-1.0,
                                                        op0=ALU.mult, op1=ALU.add), reads=[dE], writes=[dL])
            K.act.op(lambda e, L=L, ZS=ZS: e.activation(out=L[:, 64:128], in_=ZS[:, 1600:1664], func=AF.Copy),
                     reads=[dZS, dL], adds=[dL])
            K.act.op(lambda e, L=L, E=E: e.activation(out=L[:, 128:256], in_=E[:, 64:192], func=AF.Copy),
                     reads=[dE, dL], adds=[dL])
            yield
            pt, dpt = PT.next()
            K.pe.op(lambda e, pt=pt, L=L: e.transpose(out=pt[:, 0, :], in_=L[:, 0:128], identity=identb[:]),
                    reads=[dL, d_c], writes=[dpt])
            K.pe.op(lambda e, pt=pt, L=L: e.transpose(out=pt[:, 1, :], in_=L[:, 128:256], identity=identb[:]),
                    reads=[dL, d_c], adds=[dpt])
            LT, dLT = LTr.next()
            K.act.op(lambda e, pt=pt, LT=LT: e.activation(out=LT[:], in_=pt[:], func=AF.Copy), reads=[dpt], writes=[dLT])
            yield "pre_pw"
            pw, dpw = PW.next()
            pa, dpa = PA.next()
            pg, dpg = PG.next()
            K.pe.op(lambda e, pw=pw, LT=LT: e.matmul(pw[:], LT[:, 0, :], LW[:, 0, :], start=True, stop=True),
                    reads=[dLT, d_c], writes=[dpw])
            K.pe.op(lambda e, pa=pa, LT=LT: e.matmul(pa[:], LT[:, 0, :], LW[:, 1, :], start=True, stop=True),
                    reads=[dLT, d_c], writes=[dpa])
            for j in range(4):
                K.pe.op(lambda e, pg=pg, LT=LT, j=j: e.matmul(pg[:, j, :], LW[:, 2, j * 128:(j + 1) * 128], LT[:, 1, :],
                                                             start=True, stop=True),
                        reads=[dLT, d_c], writes=[dpg] if j == 0 else [], adds=[] if j == 0 else [dpg])
            GT, dGT = GTr.next()
            K.act.op(lambda e, pg=pg, GT=GT: e.activation(out=GT[:], in_=pg[:], func=AF.Copy), reads=[dpg], writes=[dGT])
            K.sp.dma(lambda e, GT=GT, t0=t0: e.dma_start(
                out=dap(scr["g_fm"], t0, [[T, 128], [128 * T, 4], [1, 128]]), in_=GT[:]),
                reads=[dGT], adds=[scr["d_p2a"]])
            yield
            U, dU = Ur.next()
            K.dve.op(lambda e, U=U, pw=pw: e.tensor_tensor(out=U[:], in0=pw[:], in1=PR[:, 0, :], op=ALU.add),
                     reads=[dpw, d_c], writes=[dU])
            yield
            K.act.op(lambda e, U=U: e.activation(out=U[:], in_=U[:], func=AF.Exp, scale=-1.0), writes=[dU])
            K.act.op(lambda e, U=U: e.activation(out=U[:], in_=U[:], func=AF.Ln, bias=cst[:, 0:1]), reads=[d_c], writes=[dU])
            yield
            K.act.op(lambda e, U=U: e.activation(out=U[:], in_=U[:], func=AF.Exp, scale=-1.0, bias=cst[:, 1:2]),
                     reads=[d_c], writes=[dU])
            K.act.op(lambda e, U=U, OUT=OUT: e.activation(out=OUT[:, 0, :], in_=U[:], func=AF.Exp, scale=-1.0),
                     reads=[dU], adds=[dO])
            yield
            UA, dUA = UAr.next()
            K.dve.op(lambda e, UA=UA, pa=pa: e.tensor_tensor(out=UA[:], in0=pa[:], in1=PR[:, 1, :], op=ALU.add),
                     reads=[dpa, d_c], writes=[dUA])
            yield
            K.act.op(lambda e, UA=UA: e.activation(out=UA[:], in_=UA[:], func=AF.Exp, scale=-1.0), writes=[dUA])
            K.act.op(lambda e, UA=UA: e.activation(out=UA[:], in_=UA[:], func=AF.Ln, bias=cst[:, 0:1]), reads=[d_c], writes=[dUA])
            K.act.op(lambda e, UA=UA: e.activation(out=UA[:], in_=UA[:], func=AF.Exp, scale=-1.0), writes=[dUA])
            yield "post_a"
            KKt, dKK = KKr.next()
            SQ, dSQ = SQr.next()
            S8, dS8 = S8r.next()
            K.dve.op(lambda e, KKt=KKt, ZS=ZS: e.tensor_tensor(out=KKt[:], in0=ZS[:, 512:1024], in1=PR[:, 2, :], op=ALU.mult),
                     reads=[dZS, d_c], writes=[dKK])
            K.pool.op(lambda e, KKt=KKt, SQ=SQ: e.tensor_tensor(out=SQ[:], in0=KKt[:], in1=KKt[:], op=ALU.mult),
                      reads=[dKK], writes=[dSQ])
            yield
            K.dve.op(lambda e, SQ=SQ, S8=S8: e.tensor_reduce(out=S8[:, 0, :], in_=SQ[:].rearrange("p (h k) -> p h k", k=64),
                                                            axis=AX.X, op=ALU.add), reads=[dSQ], writes=[dS8])
            K.dve.op(lambda e, S8=S8: e.tensor_scalar(out=S8[:, 0, :], in0=S8[:, 0, :], scalar1=1e-24, scalar2=None,
                                                      op0=ALU.max), writes=[dS8])
            K.act.op(lambda e, S8=S8: e.activation(out=S8[:, 1, :], in_=S8[:, 0, :], func=AF.Ln), writes=[dS8])
            K.act.op(lambda e, S8=S8: e.activation(out=S8[:, 2, :], in_=S8[:, 1, :], func=AF.Exp, scale=-0.5), writes=[dS8])
            yield
            K.dve.op(lambda e, KKt=KKt, S8=S8, OUT=OUT: e.tensor_tensor(
                out=OUT[:, 1, :].rearrange("p (h k) -> p h k", k=64), in0=KKt[:].rearrange("p (h k) -> p h k", k=64),
                in1=S8[:, 2, :].unsqueeze(2).broadcast_to([128, 8, 64]), op=ALU.mult),
                reads=[dKK, dS8, dO], adds=[dO])
            K.dve.op(lambda e, OUT=OUT, UA=UA: e.scalar_tensor_tensor(
                out=OUT[:, 2, :], in0=OUT[:, 1, :], scalar=-1.0, in1=UA[:], op0=ALU.mult, op1=ALU.mult),
                reads=[dUA, dO], adds=[dO])
            yield
            T1, dT1 = T1r.next()
            K.dve.op(lambda e, T1=T1, UA=UA: e.scalar_tensor_tensor(
                out=T1[:], in0=UA[:], scalar=-1.0, in1=PR[:, 3, :], op0=ALU.add, op1=ALU.mult),
                reads=[dUA, d_c], writes=[dT1])
            K.dve.op(lambda e, T1=T1, OUT=OUT, ZS=ZS: e.scalar_tensor_tensor(
                out=OUT[:, 3, :], in0=T1[:], scalar=1.0, in1=ZS[:, 512:1024], op0=ALU.add, op1=ALU.mult),
                reads=[dT1, dZS, dO], adds=[dO])
            T2, dT2 = T2r.next()
            K.pool.op(lambda e, T2=T2, OUT=OUT, ZS=ZS: e.tensor_tensor(out=T2[:], in0=OUT[:, 3, :], in1=ZS[:, 0:512], op=ALU.mult),
                      reads=[dO, dZS], writes=[dT2])
            K.pool.op(lambda e, T2=T2: e.tensor_tensor(out=T2[:], in0=T2[:], in1=PR[:, 4, :], op=ALU.mult),
                      reads=[d_c], writes=[dT2])
            yield
            K.dve.op(lambda e, T2=T2, S8=S8: e.tensor_reduce(out=S8[:, 3, :], in_=T2[:].rearrange("p (h k) -> p h k", k=64),
                                                            axis=AX.X, op=ALU.add), reads=[dT2], writes=[dS8])
            yield
            pv, dpv = PV.next()
            pc, dpc = PC.next()
            for j in range(4):
                K.pe.op(lambda e, pv=pv, ZS=ZS, j=j: e.transpose(out=pv[:, j, :], in_=ZS[:, 1024 + j * 128:1024 + (j + 1) * 128],
                                                                identity=identf[:]),
                        reads=[dZS, d_c], writes=[dpv] if j == 0 else [], adds=[] if j == 0 else [dpv])
            K.pe.op(lambda e, pc=pc, S8=S8: e.transpose(out=pc[:, :], in_=S8[:, 3, :], identity=identf[:]),
                    reads=[dS8, d_c], writes=[dpc])
            VT, dVT = VTr.next()
            CT, dCT = CTr.next()
            K.act.op(lambda e, pv=pv, VT=VT: e.activation(out=VT[:], in_=pv[:], func=AF.Copy), reads=[dpv], writes=[dVT])
            K.act.op(lambda e, pc=pc, CT=CT: e.activation(out=CT[:], in_=pc[:], func=AF.Copy), reads=[dpc], writes=[dCT])
            K.sp.dma(lambda e, VT=VT, t0=t0: e.dma_start(
                out=dap(scr["v_fm"], t0, [[T, 128], [128 * T, 4], [1, 128]]), in_=VT[:]),
                reads=[dVT], adds=[scr["d_p2a"]])
            K.sp.dma(lambda e, CT=CT, t0=t0: e.dma_start(out=scr["coef_fm"][:, t0:t0 + 128], in_=CT[:]),
                     reads=[dCT], adds=[scr["d_p2a"]])
            yield
            if not chunked:
                K.pool.dma(lambda e, OUT=OUT, t0=t0: e.dma_start(out=scr["rw_tm"][t0:t0 + 128, :, :], in_=OUT[:]),
                           reads=[dO], adds=[scr["d_p2a"]])
            else:
                LWt, dLW = LWr.next()
                K.dve.op(lambda e, LWt=LWt, U=U: e.tensor_scalar(out=LWt[:], in0=U[:], scalar1=-1.0, scalar2=None, op0=ALU.mult),
                         reads=[dU], writes=[dLW])
                pl, dpl = PL.next()
                K.pe.op(lambda e, pl=pl, LWt=LWt: e.matmul(pl[:], TRI[:], LWt[:], start=True, stop=True),
                        reads=[dLW, d_c], writes=[dpl])
                EL, dEL = ELr.next()
                K.act.op(lambda e, EL=EL, pl=pl: e.activation(out=EL[:, 0, :], in_=pl[:], func=AF.Exp), reads=[dpl], writes=[dEL])
                K.act.op(lambda e, EL=EL, pl=pl: e.activation(out=EL[:, 1, :], in_=pl[:], func=AF.Exp, scale=-1.0),
                         reads=[dpl], adds=[dEL])
                K.dve.op(lambda e, EL=EL, pl=pl, U=U: e.tensor_tensor(out=EL[:, 2, :], in0=pl[:], in1=U[:], op=ALU.add),
                         reads=[dpl, dU, dEL], adds=[dEL])
                K.act.op(lambda e, EL=EL: e.activation(out=EL[:, 2, :], in_=EL[:, 2, :], func=AF.Exp), reads=[dEL], adds=[dEL])
                AB, dAB = ABr.next()
                K.dve.op(lambda e, AB=AB, OUT=OUT, EL=EL: e.tensor_tensor(out=AB[:, 0, :], in0=OUT[:, 1, :], in1=EL[:, 2, :], op=ALU.mult),
                         reads=[dO, dEL], writes=[dAB])
                K.dve.op(lambda e, AB=AB, OUT=OUT, EL=EL: e.scalar_tensor_tensor(
                    out=AB[:, 1, :], in0=OUT[:, 2, :], scalar=-1.0, in1=EL[:, 1, :], op0=ALU.mult, op1=ALU.mult),
                    reads=[dO, dEL, dAB], adds=[dAB])
                K.pool.op(lambda e, AB=AB, OUT=OUT, EL=EL: e.tensor_tensor(out=AB[:, 2, :], in0=OUT[:, 3, :], in1=EL[:, 1, :], op=ALU.mult),
                          reads=[dO, dEL, dAB], adds=[dAB])
                K.pool.op(lambda e, AB=AB, OUT=OUT, EL=EL: e.tensor_tensor(out=AB[:, 3, :], in0=OUT[:, 4, :], in1=EL[:, 0, :], op=ALU.mult),
                          reads=[dO, dEL, dAB], adds=[dAB])
                K.act.op(lambda e, AB=AB, ZS=ZS: e.activation(out=AB[:, 4, :], in_=ZS[:, 1024:1536], func=AF.Copy),
                         reads=[dZS, dAB], adds=[dAB])
                K.pool.dma(lambda e, AB=AB, t0=t0: e.dma_start(out=scr["ab_tm"][t0:t0 + 128, :, :], in_=AB[:]),
                           reads=[dAB], adds=[scr["d_p2a"]])
                K.sp.dma(lambda e, LWt=LWt, t0=t0: e.dma_start(out=scr["lw_tm"][t0:t0 + 128, :], in_=LWt[:]),
                         reads=[dLW], adds=[scr["d_p2a"]])

        active = []
        nxt = 0
        while nxt < NTT or active:
            if len(active) < 2 and nxt < NTT and (not active or active[0][2] or active[0][3] >= 2):
                active.append([tile_gen(nxt), None, False, 0])
                nxt += 1
            for idx, a_ in enumerate(list(active)):
                if a_[1] == "pre_pw" and idx > 0 and not active[0][2]:
                    continue
                try:
                    tok = next(a_[0])
                    a_[1] = tok
                    a_[3] += 1
                    if tok == "post_a":
                        a_[2] = True
                except StopIteration:
                    active.remove(a_)


def phase2_scan(K, cfg, io, scr):
    T, S, NB = cfg["T"], cfg["S"], cfg["NB"]
    NBH = 2 if NB >= 2 else 1
    NBL = NB // NBH
    NP = 64 * NBH
    TS = 2
    TC = 128
    RW = 2560
    with ExitStack() as es:
        St = K.sb([128, NBL, 8, 64], F32, "St", es)
        dS = Dep()
        TMP = K.sb([128, NBL, 8, 64], F32, "TMP", es)
        dT = Dep()
        SA = K.sb([128, NBL, 8], F32, "SA", es)
        dSA = Dep()
        T2r = Rot(K, 2, [128, NBL, 8, 64], F32, "TMP2", es=es)
        T3r = Rot(K, 2, [128, NBL, 8, 64], F32, "TMP3", es=es)
        BCr = Rot(K, 3, [128, TS, NBL, 5, 8, 64], F32, "BC", es=es)
        Vr = Rot(K, 2, [128, NBL, 8, TC], F32, "Vf", es=es)
        Yr = Rot(K, 2, [128, NBL, 8, TC], F32, "Yf", es=es)
        K.dve.op(lambda e: e.memset(St[:], 0.0), writes=[dS])
        qi = [0]

        def q():
            qi[0] += 1
            return K.sp if qi[0] % 2 == 0 else K.act

        def load_bc(ci):
            t = ci * TS
            BC, dBC = BCr.next()
            first = True
            for bhi in range(NBH):
                for blo in range(NBL):
                    src = dap(scr["rw_tm"], ((bhi * NBL + blo) * S + t) * RW, [[0, 64], [RW, TS], [1, RW]])
                    dst = BC[bhi * 64:(bhi + 1) * 64, :, blo].rearrange("p t j h k -> p t (j h k)")
                    q().dma(lambda e, src=src, dst=dst: e.dma_start(out=dst, in_=src), reads=[scr["d_p2a"]],
                            writes=[dBC] if first else [], adds=[] if first else [dBC])
                    first = False
            return BC, dBC

        def vy_ap(name, bhi, blo, t):
            return dap(scr[name], (bhi * NBL + blo) * S + t, [[T, 64], [64 * T, 8], [1, TC]])

        def load_v(ni):
            Vf, dV = Vr.next()
            first = True
            for bhi in range(NBH):
                for blo in range(NBL):
                    src = vy_ap("v_fm", bhi, blo, ni * TC)
                    dst = Vf[bhi * 64:(bhi + 1) * 64, blo]
                    q().dma(lambda e, src=src, dst=dst: e.dma_start(out=dst, in_=src), reads=[scr["d_p2a"]],
                            writes=[dV] if first else [], adds=[] if first else [dV])
                    first = False
            return Vf, dV

        nch = S // TS
        bcs = {}
        bcs[0] = load_bc(0)
        if nch > 1:
            bcs[1] = load_bc(1)
        vs = {0: load_v(0)}
        P = slice(0, NP)
        for t in range(S):
            ci, ts = divmod(t, TS)
            ni, tt = divmod(t, TC)
            if ts == 0 and ci + 2 < nch:
                bcs[ci + 2] = load_bc(ci + 2)
            if tt == 0:
                if (ni + 1) * TC < S:
                    vs[ni + 1] = load_v(ni + 1)
                Yf, dY = Yr.next()
            BC, dBC = bcs[ci]
            Vf, dV = vs[ni]
            W_ = BC[P, ts, :, 0]
            KN = BC[P, ts, :, 1]
            KA = BC[P, ts, :, 2]
            KP = BC[P, ts, :, 3]
            R_ = BC[P, ts, :, 4]
            shp = [NP, NBL, 8, 64]
            K.dve.op(lambda e, KN=KN: e.tensor_tensor(out=TMP[P], in0=St[P], in1=KN, op=ALU.mult),
                     reads=[dS, dBC], writes=[dT])
            K.dve.op(lambda e: e.tensor_reduce(out=SA[P], in_=TMP[P], axis=AX.X, op=ALU.add), reads=[dT], writes=[dSA])
            K.dve.op(lambda e, W_=W_: e.tensor_tensor(out=St[P], in0=St[P], in1=W_, op=ALU.mult),
                     reads=[dBC], writes=[dS])
            K.dve.op(lambda e, KA=KA: e.tensor_tensor(out=TMP[P], in0=KA, in1=SA[P].unsqueeze(3).broadcast_to(shp),
                                                     op=ALU.mult), reads=[dBC, dSA], writes=[dT])
            K.dve.op(lambda e: e.tensor_tensor(out=St[P], in0=St[P], in1=TMP[P], op=ALU.add), reads=[dT], writes=[dS])
            T2, dT2 = T2r.next()
            K.pool.op(lambda e, KP=KP, T2=T2, Vf=Vf, tt=tt: e.tensor_tensor(
                out=T2[P], in0=KP, in1=Vf[P, :, :, tt:tt + 1].broadcast_to(shp), op=ALU.mult),
                reads=[dBC, dV], writes=[dT2])
            K.dve.op(lambda e, T2=T2: e.tensor_tensor(out=St[P], in0=St[P], in1=T2[P], op=ALU.add),
                     reads=[dT2], writes=[dS])
            T3, dT3 = T3r.next()
            K.pool.op(lambda e, T3=T3, R_=R_: e.tensor_tensor(out=T3[P], in0=St[P], in1=R_, op=ALU.mult),
                      reads=[dS, dBC], writes=[dT3])
            K.dve.op(lambda e, T3=T3, Yf=Yf, tt=tt: e.tensor_reduce(out=Yf[P, :, :, tt], in_=T3[P], axis=AX.X, op=ALU.add),
                      reads=[dT3], writes=[dY] if tt == 0 else [], adds=[] if tt == 0 else [dY])
            if tt == TC - 1:
                for bhi in range(NBH):
                    for blo in range(NBL):
                        dst = vy_ap("y_fm", bhi, blo, ni * TC)
                        srcp = Yf[bhi * 64:(bhi + 1) * 64, blo]
                        K.sp.dma(lambda e, dst=dst, srcp=srcp: e.dma_start(out=dst, in_=srcp), reads=[dY],
                                 adds=[scr["d_p2b"]])


def phase2_chunk(K, cfg, io, scr):
    T, S, NB = cfg["T"], cfg["S"], cfg["NB"]
    C = 64
    NCH = S // C
    with ExitStack() as es:
        d_c = Dep()
        id64 = K.sb([64, 64], BF16, "c_id64", es)
        K.sp.dma(lambda e: e.dma_start(out=id64[:], in_=io["ident64"][:, :]), adds=[d_c])
        MK = K.sb([64, 3, 64], F32, "c_MK", es)
        K.sp.dma(lambda e: e.dma_start(out=MK[:], in_=io["masks"][:, :, :]), adds=[d_c])
        ONES = K.sb([64, 1], F32, "c_ones", es)
        K.sp.dma(lambda e: e.dma_start(out=ONES[:], in_=io["ones64"][:, :]), adds=[d_c])
        IDF = K.sb([64, 8, 64], F32, "c_IDF", es)
        K.sp.dma(lambda e: e.dma_start(out=IDF[:], in_=io["ident_f"][0:64, 0:64].unsqueeze(1).broadcast_to([64, 8, 64])), adds=[d_c])
        ST = [K.sb([64, 8, 64], F32, "c_S%d" % b, es) for b in range(NB)]
        STb = [K.sb([64, 8, 64], BF16, "c_Sb%d" % b, es) for b in range(NB)]
        dST = [Dep() for _ in range(NB)]
        dSTb = [Dep() for _ in range(NB)]
        for b in range(NB):
            K.dve.op(lambda e, b=b: e.memset(ST[b][:], 0.0), writes=[dST[b]])
            K.pool.op(lambda e, b=b: e.memset(STb[b][:], 0.0), writes=[dSTb[b]])
        TMr = Rot(K, 3, [64, 5, 512], BF16, "c_TM", es=es)
        LWr = Rot(K, 3, [64, 512], F32, "c_LW", es=es)
        FMr = Rot(K, 2, [64, 4, 8, 64], BF16, "c_FM", es=es)
        PCr = Rot(K, 2, [64, 8], F32, "c_PC", es=es)
        Nr = Rot(K, 3, [64, 8, 64], BF16, "c_N", es=es)
        NTr = Rot(K, 3, [64, 8, 64], BF16, "c_NT", es=es)
        MTr = Rot(K, 3, [64, 8, 64], BF16, "c_MT", es=es)
        MTfr = Rot(K, 2, [64, 8, 64], F32, "c_MTf", es=es)
        NAKr = Rot(K, 2, [64, 8, 64], BF16, "c_NAK", es=es)
        MRBr = Rot(K, 2, [64, 8, 64], BF16, "c_MRB", es=es)
        MRKr = Rot(K, 2, [64, 8, 64], BF16, "c_MRK", es=es)
        Xr = Rot(K, 2, [64, 8, 64], BF16, "c_X", es=es)
        NUr = Rot(K, 2, [64, 8, 64], BF16, "c_NU", es=es)
        Yr = Rot(K, 2, [64, 8, 64], F32, "c_Y", es=es)
        TSr = Rot(K, 2, [64, 8, 64], F32, "c_TS", es=es)
        PTf = Rot(K, 1, [64, 4, 8, 64], BF16, "c_PTf", psum=True, es=es)
        PA = Rot(K, 4, [64, 8, 64], F32, "c_PA", psum=True, es=es)
        PPC = Rot(K, 1, [64, 8], F32, "c_PPC", psum=True, es=es)
        ce = [0]

        def evac_copy(dst_ap, src_ap, reads, writes=(), adds=(), scale=None):
            ce[0] += 1
            if scale is not None or ce[0] % 2 == 0:
                if scale is None:
                    K.act.op(lambda e: e.activation(out=dst_ap, in_=src_ap, func=AF.Copy), reads=reads, writes=writes, adds=adds)
                else:
                    K.act.op(lambda e: e.activation(out=dst_ap, in_=src_ap, func=AF.Copy, scale=scale), reads=reads, writes=writes, adds=adds)
            else:
                K.dve.op(lambda e: e.tensor_copy(out=dst_ap, in_=src_ap), reads=reads, writes=writes, adds=adds)

        def mm8(pt, dpt, lhs_fn, rhs_fn, reads, first=True, last=True, wr=True):
            mmN(pt, dpt, [(lhs_fn, rhs_fn)], reads)

        def mmN(pt, dpt, terms, reads):
            n = len(terms)
            for h in range(8):
                for i, (lf, rf) in enumerate(terms):
                    K.pe.op(lambda e, h=h, lf=lf, rf=rf, i=i: e.matmul(pt[:, h, :], lf(h), rf(h), start=(i == 0), stop=(i == n - 1)),
                            reads=reads, writes=[dpt] if (h == 0 and i == 0) else [], adds=[] if (h == 0 and i == 0) else [dpt])

        q = [0]

        def dq():
            q[0] += 1
            return K.sp if q[0] % 2 == 0 else K.pool

        for ci in range(NCH):
            for b in range(NB):
                t0 = b * S + ci * C
                TM, dTM = TMr.next()
                LW, dLW = LWr.next()
                dq().dma(lambda e, TM=TM, t0=t0: e.dma_start(out=TM[:], in_=scr["ab_tm"][t0:t0 + C, :, :]),
                         reads=[scr["d_p2a"]], writes=[dTM])
                dq().dma(lambda e, LW=LW, t0=t0: e.dma_start(out=LW[:], in_=scr["lw_tm"][t0:t0 + C, :]),
                         reads=[scr["d_p2a"]], writes=[dLW])
                ptf, dptf = PTf.next()
                first = True
                for j in range(4):
                    for h in range(8):
                        K.pe.op(lambda e, ptf=ptf, TM=TM, j=j, h=h: e.transpose(
                            out=ptf[:, j, h, :], in_=TM[:, j, h * 64:(h + 1) * 64], identity=id64[:]),
                            reads=[dTM, d_c], writes=[dptf] if first else [], adds=[] if first else [dptf])
                        first = False
                FM, dFM = FMr.next()
                K.act.op(lambda e, FM=FM, ptf=ptf: e.activation(out=FM[:, 0:2], in_=ptf[:, 0:2], func=AF.Copy), reads=[dptf], writes=[dFM])
                K.dve.op(lambda e, FM=FM, ptf=ptf: e.tensor_copy(out=FM[:, 2:4], in_=ptf[:, 2:4]), reads=[dptf, dFM], adds=[dFM])
                Af = lambda h, FM=FM: FM[:, 0, h, :]
                Bf = lambda h, FM=FM: FM[:, 1, h, :]
                Kf = lambda h, FM=FM: FM[:, 2, h, :]
                Rf = lambda h, FM=FM: FM[:, 3, h, :]
                Vt = lambda h, TM=TM: TM[:, 4, h * 64:(h + 1) * 64]
                Bt = lambda h, TM=TM: TM[:, 1, h * 64:(h + 1) * 64]
                Kt = lambda h, TM=TM: TM[:, 2, h * 64:(h + 1) * 64]
                ppc, dppc = PPC.next()
                for h in range(8):
                    K.pe.op(lambda e, ppc=ppc, LW=LW, h=h: e.matmul(ppc[:, h:h + 1], LW[:, h * 64:(h + 1) * 64], ONES[:], start=True, stop=True),
                            reads=[dLW, d_c], writes=[dppc] if h == 0 else [], adds=[] if h == 0 else [dppc])
                PCt, dPC = PCr.next()
                K.act.op(lambda e, PCt=PCt, ppc=ppc: e.activation(out=PCt[:], in_=ppc[:], func=AF.Exp), reads=[dppc], writes=[dPC])
                mbc = lambda i: MK[:, i, :].unsqueeze(1).broadcast_to([64, 8, 64])
                pa, dpa = PA.next()
                mm8(pa, dpa, Af, Bf, [dFM])
                N0, dN0 = Nr.next()
                K.dve.op(lambda e, N0=N0, pa=pa: e.tensor_tensor(out=N0[:], in0=pa[:], in1=mbc(0), op=ALU.mult), reads=[dpa, d_c], writes=[dN0])
                pa, dpa = PA.next()
                mm8(pa, dpa, Bf, Af, [dFM])
                NT0, dNT0 = NTr.next()
                MTf, dMTf = MTfr.next()
                K.dve.op(lambda e, NT0=NT0, pa=pa: e.tensor_tensor(out=NT0[:], in0=pa[:], in1=mbc(1), op=ALU.mult), reads=[dpa, d_c], writes=[dNT0])
                K.pool.op(lambda e, MTf=MTf, NT0=NT0: e.tensor_tensor(out=MTf[:], in0=IDF[:], in1=NT0[:], op=ALU.subtract),
                          reads=[dNT0, d_c], writes=[dMTf])
                MT, dMT = MTr.next()
                K.act.op(lambda e, MT=MT, MTf=MTf: e.activation(out=MT[:], in_=MTf[:], func=AF.Copy), reads=[dMTf], writes=[dMT])
                pa, dpa = PA.next()
                mm8(pa, dpa, Kf, Af, [dFM])
                NAK, dNAK = NAKr.next()
                K.dve.op(lambda e, NAK=NAK, pa=pa: e.tensor_tensor(out=NAK[:], in0=pa[:], in1=mbc(1), op=ALU.mult), reads=[dpa, d_c], writes=[dNAK])
                pa, dpa = PA.next()
                mm8(pa, dpa, Bf, Rf, [dFM])
                MRB, dMRB = MRBr.next()
                K.dve.op(lambda e, MRB=MRB, pa=pa: e.tensor_tensor(out=MRB[:], in0=pa[:], in1=mbc(2), op=ALU.mult), reads=[dpa, d_c], writes=[dMRB])
                pa, dpa = PA.next()
                mm8(pa, dpa, Kf, Rf, [dFM])
                MRK, dMRK = MRKr.next()
                K.dve.op(lambda e, MRK=MRK, pa=pa: e.tensor_tensor(out=MRK[:], in0=pa[:], in1=mbc(2), op=ALU.mult), reads=[dpa, d_c], writes=[dMRK])
                Np, dNp, NTp, dNTp = N0, dN0, NT0, dNT0
                for lvl in range(1, 6):
                    pa, dpa = PA.next()
                    mm8(pa, dpa, lambda h, NTp=NTp: NTp[:, h, :], lambda h, Np=Np: Np[:, h, :], [dNp, dNTp])
                    Nn, dNn = Nr.next()
                    evac_copy(Nn[:], pa[:], [dpa], writes=[dNn])
                    if lvl < 5:
                        pa2, dpa2 = PA.next()
                        mm8(pa2, dpa2, lambda h, Np=Np: Np[:, h, :], lambda h, NTp=NTp: NTp[:, h, :], [dNp, dNTp])
                        NTn, dNTn = NTr.next()
                        evac_copy(NTn[:], pa2[:], [dpa2], writes=[dNTn])
                    pa3, dpa3 = PA.next()
                    mm8(pa3, dpa3, lambda h, Nn=Nn: Nn[:, h, :], lambda h, MT=MT: MT[:, h, :], [dNn, dMT])
                    K.dve.op(lambda e, MTf=MTf, pa3=pa3: e.tensor_tensor(out=MTf[:], in0=MTf[:], in1=pa3[:], op=ALU.add),
                             reads=[dpa3], writes=[dMTf])
                    MT, dMT = MTr.next()
                    K.act.op(lambda e, MT=MT, MTf=MTf: e.activation(out=MT[:], in_=MTf[:], func=AF.Copy), reads=[dMTf], writes=[dMT])
                    Np, dNp = Nn, dNn
                    if lvl < 5:
                        NTp, dNTp = NTn, dNTn
                Sb = STb[b]
                pa, dpa = PA.next()
                mmN(pa, dpa, [(Af, lambda h, Sb=Sb: Sb[:, h, :]), (lambda h, NAK=NAK: NAK[:, h, :], Vt)], [dFM, dSTb[b], dNAK, dTM])
                X, dX = Xr.next()
                K.act.op(lambda e, X=X, pa=pa: e.activation(out=X[:], in_=pa[:], func=AF.Copy), reads=[dpa], writes=[dX])
                pa, dpa = PA.next()
                mm8(pa, dpa, lambda h, MT=MT: MT[:, h, :], lambda h, X=X: X[:, h, :], [dMT, dX])
                NU, dNU = NUr.next()
                K.act.op(lambda e, NU=NU, pa=pa: e.activation(out=NU[:], in_=pa[:], func=AF.Copy, scale=-1.0), reads=[dpa], writes=[dNU])
                pa, dpa = PA.next()
                mmN(pa, dpa, [(lambda h, Sb=Sb: Sb[:, h, :], Rf), (lambda h, NU=NU: NU[:, h, :], lambda h, MRB=MRB: MRB[:, h, :]),
                              (Vt, lambda h, MRK=MRK: MRK[:, h, :])], [dFM, dSTb[b], dNU, dMRB, dTM, dMRK])
                Y, dY = Yr.next()
                K.dve.op(lambda e, Y=Y, pa=pa: e.tensor_copy(out=Y[:], in_=pa[:]), reads=[dpa], writes=[dY])
                K.sp.dma(lambda e, Y=Y, t0=t0: e.dma_start(out=dap(scr["y_fm"], t0, [[T, 64], [64 * T, 8], [1, 64]]), in_=Y[:]),
                         reads=[dY], adds=[scr["d_p2b"]])
                pa, dpa = PA.next()
                mmN(pa, dpa, [(Bt, lambda h, NU=NU: NU[:, h, :]), (Kt, Vt)], [dTM, dNU])
                TS_, dTS = TSr.next()
                K.dve.op(lambda e, TS_=TS_, pa=pa, b=b: e.tensor_tensor(out=TS_[:], in0=pa[:], in1=ST[b][:], op=ALU.add),
                         reads=[dpa, dST[b]], writes=[dTS])
                K.dve.op(lambda e, TS_=TS_, PCt=PCt, b=b: e.tensor_tensor(
                    out=ST[b][:], in0=TS_[:], in1=PCt[:].unsqueeze(2).broadcast_to([64, 8, 64]), op=ALU.mult),
                    reads=[dTS, dPC], writes=[dST[b]])
                K.act.op(lambda e, b=b: e.activation(out=STb[b][:], in_=ST[b][:], func=AF.Copy), reads=[dST[b]], writes=[dSTb[b]])


def phase2_post(K, cfg, io, scr):
    T, S, NB = cfg["T"], cfg["S"], cfg["NB"]
    NT = T // 512
    with ExitStack() as es:
        BO = K.sb([128, 128], F32, "BO", es)
        d_c = Dep()
        K.sp.dma(lambda e: e.dma_start(out=BO[:], in_=io["blockones"][:, :]), adds=[d_c])
        LN = K.sb([128, 2, 4], F32, "LN", es)
        K.sp.dma(lambda e: e.dma_start(out=LN[:, 0, :], in_=io["lnx_w"].rearrange("o (j p) -> p (o j)", p=128)), adds=[d_c])
        K.sp.dma(lambda e: e.dma_start(out=LN[:, 1, :], in_=io["lnx_b"].rearrange("o (j p) -> p (o j)", p=128)), adds=[d_c])
        Yr = Rot(K, 2, [128, 512], F32, "pY", es=es)
        Vr = Rot(K, 2, [128, 512], F32, "pV", es=es)
        Gr = Rot(K, 2, [128, 512], F32, "pG", es=es)
        Cr = Rot(K, 2, [128, 512], F32, "pC", es=es)
        YCr = Rot(K, 2, [128, 512], F32, "pYC", es=es)
        SQr = Rot(K, 2, [128, 512], F32, "pSQ", es=es)
        Rr = Rot(K, 2, [128, 512], F32, "pR", es=es)
        Or = Rot(K, 2, [128, 512], BF16, "pO", es=es)
        PM = Rot(K, 2, [128, 512], F32, "pPM", psum=True, es=es)
        PVr = Rot(K, 2, [128, 512], F32, "pPV", psum=True, es=es)
        def make_gen(ti, j):
            cs = slice(ti * 512, (ti + 1) * 512)
            if True:
                rs = slice(j * 128, (j + 1) * 128)
                Y, dY = Yr.next()
                V, dV = Vr.next()
                G, dG = Gr.next()
                C, dC = Cr.next()
                K.sp.dma(lambda e, Y=Y, rs=rs, cs=cs: e.dma_start(out=Y[:], in_=scr["y_fm"][rs, cs]),
                         reads=[scr["d_p2b"]], writes=[dY])
                K.pool.dma(lambda e, V=V, rs=rs, cs=cs: e.dma_start(out=V[:], in_=scr["v_fm"][rs, cs]),
                           reads=[scr["d_p2a"]], writes=[dV])
                K.sp.dma(lambda e, G=G, rs=rs, cs=cs: e.dma_start(out=G[:], in_=scr["g_fm"][rs, cs]),
                         reads=[scr["d_p2a"]], writes=[dG])
                K.pool.dma(lambda e, C=C, j=j, cs=cs: e.dma_start(
                    out=C[0:64, :], in_=scr["coef_fm"][2 * j:2 * j + 1, cs].broadcast_to([64, 512])),
                    reads=[scr["d_p2a"]], writes=[dC])
                K.pool.dma(lambda e, C=C, j=j, cs=cs: e.dma_start(
                    out=C[64:128, :], in_=scr["coef_fm"][2 * j + 1:2 * j + 2, cs].broadcast_to([64, 512])),
                    reads=[scr["d_p2a"]], adds=[dC])
                pm, dpm = PM.next()
                K.pe.op(lambda e, pm=pm, Y=Y: e.matmul(pm[:], BO[:], Y[:], start=True, stop=True),
                        reads=[dY, d_c], writes=[dpm])
                YC, dYC = YCr.next()
                K.dve.op(lambda e, YC=YC, Y=Y, pm=pm: e.tensor_tensor(out=YC[:], in0=Y[:], in1=pm[:], op=ALU.subtract),
                         reads=[dY, dpm], writes=[dYC])
                SQ, dSQ = SQr.next()
                K.act.op(lambda e, SQ=SQ, YC=YC: e.activation(out=SQ[:], in_=YC[:], func=AF.Square),
                         reads=[dYC], writes=[dSQ])
                yield
                pv, dpv = PVr.next()
                K.pe.op(lambda e, pv=pv, SQ=SQ: e.matmul(pv[:], BO[:], SQ[:], start=True, stop=True),
                        reads=[dSQ, d_c], writes=[dpv])
                yield
                R, dR = Rr.next()
                K.dve.op(lambda e, R=R, pv=pv: e.tensor_scalar(out=R[:], in0=pv[:], scalar1=64e-5, scalar2=None, op0=ALU.add),
                         reads=[dpv], writes=[dR])
                K.act.op(lambda e, R=R: e.activation(out=R[:], in_=R[:], func=AF.Ln), writes=[dR])
                K.act.op(lambda e, R=R: e.activation(out=R[:], in_=R[:], func=AF.Exp, scale=-0.5), writes=[dR])
                K.dve.op(lambda e, YC=YC, R=R: e.tensor_tensor(out=YC[:], in0=YC[:], in1=R[:], op=ALU.mult),
                         reads=[dR], writes=[dYC])
                K.dve.op(lambda e, YC=YC, j=j: e.tensor_scalar(out=YC[:], in0=YC[:], scalar1=LN[:, 0, j:j + 1],
                                                              scalar2=LN[:, 1, j:j + 1], op0=ALU.mult, op1=ALU.add),
                         reads=[d_c], writes=[dYC])
                yield
                K.pool.op(lambda e, C=C, V=V: e.tensor_tensor(out=C[:], in0=C[:], in1=V[:], op=ALU.mult),
                          reads=[dV], writes=[dC])
                K.dve.op(lambda e, YC=YC, C=C: e.tensor_tensor(out=YC[:], in0=YC[:], in1=C[:], op=ALU.add),
                         reads=[dC], writes=[dYC])
                O, dO = Or.next()
                K.dve.op(lambda e, O=O, YC=YC, G=G: e.tensor_tensor(out=O[:], in0=YC[:], in1=G[:], op=ALU.mult),
                         reads=[dYC, dG], writes=[dO])
                K.sp.dma(lambda e, O=O, rs=rs, cs=cs: e.dma_start(out=scr["ya_fm"][rs, cs], in_=O[:]),
                         reads=[dO], adds=[scr["d_p2c"]])

        work = [(ti, j) for ti in range(NT) for j in range(4)]

        active = []
        nxt = 0
        while nxt < len(work) or active:
            if len(active) < 2 and nxt < len(work):
                active.append(make_gen(*work[nxt]))
                nxt += 1
            for a in list(active):
                try:
                    next(a)
                except StopIteration:
                    active.remove(a)


def phase3(K, cfg, io, scr):
    T, S, NB = cfg["T"], cfg["S"], cfg["NB"]
    NQ = S // 128
    lam_init = 0.2
    with ExitStack() as es:
        d_c = Dep()
        identb = K.sb([128, 128], BF16, "a_identb", es)
        K.sp.dma(lambda e: e.dma_start(out=identb[:], in_=io["ident_bf"][:, :]), adds=[d_c])
        TB = K.sb([128, 4, S], F32, "TB", es)
        for h in range(4):
            (K.sp if h % 2 == 0 else K.pool).dma(lambda e, h=h: e.dma_start(out=TB[:, h, :], in_=io["alibi"][h, :, :]), adds=[d_c])
        SW = K.sb([128, 128], F32, "SW", es)
        K.sp.dma(lambda e: e.dma_start(out=SW[:], in_=io["subln_w"][0:1, :].broadcast_to([128, 128])), adds=[d_c])
        LQ = K.sb([128, 4, 64], F32, "LQ", es)
        for j, nm in enumerate(["lam_q1", "lam_k1", "lam_q2", "lam_k2"]):
            K.pool.dma(lambda e, j=j, nm=nm: e.dma_start(out=LQ[:, j, :], in_=io[nm][0:1, :].broadcast_to([128, 64])), adds=[d_c])
        LM = K.sb([128, 8], F32, "LM", es)
        d_lm = Dep()
        LT_ = K.sb([128, 2, 64], F32, "LTt", es)
        K.dve.op(lambda e: e.tensor_tensor(out=LT_[:, 0, :], in0=LQ[:, 0, :], in1=LQ[:, 1, :], op=ALU.mult), reads=[d_c], writes=[d_lm])
        K.dve.op(lambda e: e.tensor_tensor(out=LT_[:, 1, :], in0=LQ[:, 2, :], in1=LQ[:, 3, :], op=ALU.mult), reads=[d_c], writes=[d_lm])
        K.dve.op(lambda e: e.tensor_reduce(out=LM[:, 0:2], in_=LT_[:], axis=AX.X, op=ALU.add), writes=[d_lm])
        K.act.op(lambda e: e.activation(out=LM[:, 2:4], in_=LM[:, 0:2], func=AF.Exp), writes=[d_lm])
        K.dve.op(lambda e: e.tensor_tensor(out=LM[:, 4:5], in0=LM[:, 3:4], in1=LM[:, 2:3], op=ALU.subtract), writes=[d_lm])
        K.dve.op(lambda e: e.tensor_scalar(out=LM[:, 4:5], in0=LM[:, 4:5], scalar1=-lam_init, scalar2=None, op0=ALU.add), writes=[d_lm])
        K.dve.op(lambda e: e.tensor_scalar(out=SW[:], in0=SW[:], scalar1=1.0 - lam_init, scalar2=None, op0=ALU.mult),
                 reads=[d_c], writes=[d_c])

        Vr = Rot(K, 2, [128, NQ, 512], BF16, "aV", es=es)
        QKr = Rot(K, 2, [64, 4, S], BF16, "aQK", es=es)
        SSr = Rot(K, 3, [128, 512], F32, "aSS", es=es)
        Pr = Rot(K, 3, [128, 512], BF16, "aP", es=es)
        PTsr = Rot(K, 4, [128, 4, 128], BF16, "aPTs", es=es)
        YB = K.sb([128, NQ, 512], BF16, "aYB", es)
        dYB = Dep()
        STr = Rot(K, 4, [128, 24], F32, "aST", es=es)
        O1r = Rot(K, 2, [128, 128], F32, "aO1", es=es)
        Or_ = Rot(K, 2, [128, 128], F32, "aO", es=es)
        junk = K.sb([128, 128], F32, "ajunk", es)
        d_junk = Dep()
        YTr = Rot(K, 2, [128, 4, 128], BF16, "aYT", es=es)
        PS = Rot(K, 3, [128, 512], F32, "aPS", psum=True, es=es)
        PTp = Rot(K, 2, [128, 4, 128], BF16, "aPTp", psum=True, es=es)
        PO = Rot(K, 2, [128, 2, 128], F32, "aPO", psum=True, es=es)
        cp = [0]

        def copy_eng():
            cp[0] += 1
            return cp[0] % 2

        SSQ = K.sb([128, NQ * 4], F32, "aSSQ", es)
        dSSQ = Dep()
        SWb = K.sb([128, 128], BF16, "aSWb", es)
        K.act.op(lambda e: e.activation(out=SWb[:], in_=SW[:], func=AF.Copy), reads=[d_c], adds=[d_c])
        pipe = []
        pidx = [0]

        def step_pipe():
            j = len(pipe) - 1
            pipe[j][0]()
            if j - 1 >= pidx[0]:
                pipe[j - 1][2]()
            if j - 2 >= pidx[0]:
                pipe[j - 2][3]()
                if pipe[j - 2][4] is not None:
                    pipe[j - 2][4]()
            pipe[j][1]()

        def flush_pipe():
            j = len(pipe) - 1
            if j - 0 >= pidx[0] and j >= 0:
                pipe[j][2]()
            for k in (j - 1, j):
                if k >= pidx[0] and k >= 0:
                    pipe[k][3]()
                    if pipe[k][4] is not None:
                        pipe[k][4]()
            pidx[0] = len(pipe)
        for b in range(NB):
            V, dV = Vr.next()
            K.sp.dma(lambda e, V=V, b=b: e.dma_start(
                out=V[:], in_=scr["av_tm"][b * S:(b + 1) * S, :].rearrange("(n p) c -> p n c", p=128)),
                reads=[scr["d_p1"]], writes=[dV])
            first_yb = True
            for h in range(4):
                QK, dQK = QKr.next()
                for j in range(4):
                    r0 = (0 if j < 2 else 512) + h * 128 + (j % 2) * 64
                    (K.sp if j % 2 == 0 else K.pool).dma(lambda e, QK=QK, j=j, r0=r0, b=b: e.dma_start(
                        out=QK[:, j, :], in_=scr["qk_fm"][r0:r0 + 64, b * S:(b + 1) * S]),
                        reads=[scr["d_p1"]], writes=[dQK] if j == 0 else [], adds=[] if j == 0 else [dQK])
                for qi in range(NQ):
                    nk = (qi + 1) * 128
                    off = (S - 128) - qi * 128
                    ST, dST = STr.next()
                    K.pool.op(lambda e, ST=ST: e.memset(ST[:], 0.0), writes=[dST])
                    po, dpo = PO.next()
                    items = []
                    for c in range(2):
                        nch = (nk + 511) // 512
                        for ch in range(nch):
                            items.append((c, ch))
                    for ii, (c, ch) in enumerate(items):
                        kb0 = ch * 512
                        n = min(512, nk - kb0)
                        nb = n // 128
                        ps, dps = PS.next()
                        SS, dSS = SSr.next()
                        Pt, dP = Pr.next()
                        hold = {}

                        def stA_pe(ps=ps, dps=dps, c=c, qi=qi, kb0=kb0, n=n, QK=QK, dQK=dQK):
                            K.pe.op(lambda e: e.matmul(
                                ps[:, :n], QK[:, c, qi * 128:(qi + 1) * 128], QK[:, 2 + c, kb0:kb0 + n],
                                start=True, stop=True), reads=[dQK], writes=[dps])

                        def stA_rest(ps=ps, dps=dps, SS=SS, dSS=dSS, Pt=Pt, dP=dP, ST=ST, dST=dST, n=n, off=off, h=h, kb0=kb0, c=c, ch=ch):
                            K.dve.op(lambda e: e.scalar_tensor_tensor(
                                out=SS[:, :n], in0=ps[:, :n], scalar=0.125, in1=TB[:, h, off + kb0:off + kb0 + n],
                                op0=ALU.mult, op1=ALU.add), reads=[dps, d_c], writes=[dSS])
                            K.act.op(lambda e: e.activation(
                                out=Pt[:, :n], in_=SS[:, :n], func=AF.Exp,
                                accum_out=ST[:, 4 * c + ch:4 * c + ch + 1]), reads=[dSS, dST], writes=[dP], adds=[dST])

                        def stB(Pt=Pt, dP=dP, nb=nb, hold=hold):
                            ptp, dptp = PTp.next()
                            for kk_ in range(nb):
                                K.pe.op(lambda e, kk_=kk_: e.transpose(
                                    out=ptp[:, kk_, :], in_=Pt[:, kk_ * 128:(kk_ + 1) * 128], identity=identb[:]),
                                    reads=[dP, d_c], writes=[dptp] if kk_ == 0 else [], adds=[] if kk_ == 0 else [dptp])
                            PTs, dPTs = PTsr.next()
                            hold["PTs"] = (PTs, dPTs)
                            if copy_eng():
                                K.act.op(lambda e: e.activation(
                                    out=PTs[:, :nb, :], in_=ptp[:, :nb, :], func=AF.Copy), reads=[dptp], writes=[dPTs])
                            else:
                                K.dve.op(lambda e: e.tensor_copy(
                                    out=PTs[:, :nb, :], in_=ptp[:, :nb, :]), reads=[dptp], writes=[dPTs])

                        def stC(nb=nb, kb0=kb0, c=c, h=h, qi=qi, po=po, dpo=dpo, V=V, dV=dV, hold=hold):
                            PTs, dPTs = hold["PTs"]
                            for kk_ in range(nb):
                                kb = kb0 // 128 + kk_
                                K.pe.op(lambda e, kb=kb, kk_=kk_: e.matmul(
                                    po[:, c, :], PTs[:, kk_, :], V[:, kb, h * 128:(h + 1) * 128],
                                    start=(kb == 0), stop=(kb == qi)), reads=[dPTs, dV],
                                    writes=[dpo] if (kb == 0 and c == 0) else [], adds=[] if (kb == 0 and c == 0) else [dpo])

                        def combine(ST=ST, dST=dST, po=po, dpo=dpo, qi=qi, h=h, fy=first_yb):
                            K.dve.op(lambda e: e.tensor_reduce(out=ST[:, 8:10], in_=ST[:, 0:8].rearrange("p (c k) -> p c k", k=4),
                                                               axis=AX.X, op=ALU.add), reads=[dST], writes=[dST])
                            K.dve.op(lambda e: e.reciprocal(out=ST[:, 10:12], in_=ST[:, 8:10]), writes=[dST])
                            K.dve.op(lambda e: e.tensor_tensor(out=ST[:, 12:13], in0=ST[:, 11:12], in1=LM[:, 4:5], op=ALU.mult),
                                     reads=[d_lm], writes=[dST])
                            O1, dO1 = O1r.next()
                            K.dve.op(lambda e: e.tensor_scalar(out=O1[:], in0=po[:, 1, :], scalar1=ST[:, 12:13],
                                                               scalar2=None, op0=ALU.mult), reads=[dpo, dST], writes=[dO1])
                            K.dve.op(lambda e: e.scalar_tensor_tensor(
                                out=YB[:, qi, h * 128:(h + 1) * 128], in0=po[:, 0, :], scalar=ST[:, 10:11], in1=O1[:], op0=ALU.mult, op1=ALU.add),
                                reads=[dpo, dST, dO1], writes=[dYB] if fy else [], adds=[] if fy else [dYB])
                            K.act.op(lambda e: e.activation(out=junk[:], in_=YB[:, qi, h * 128:(h + 1) * 128], func=AF.Square,
                                                            accum_out=SSQ[:, qi * 4 + h:qi * 4 + h + 1]),
                                     reads=[dYB], writes=[d_junk], adds=[dSSQ])

                        last = (ii == len(items) - 1)
                        pipe.append([stA_pe, stA_rest, stB, stC, combine if last else None])
                        step_pipe()
                    first_yb = False
            flush_pipe()
            K.dve.op(lambda e: e.tensor_scalar(out=SSQ[:], in0=SSQ[:], scalar1=1.0 / 128, scalar2=1e-5, op0=ALU.mult, op1=ALU.add),
                     reads=[dSSQ], writes=[dSSQ])
            K.act.op(lambda e: e.activation(out=SSQ[:], in_=SSQ[:], func=AF.Ln), writes=[dSSQ])
            K.act.op(lambda e: e.activation(out=SSQ[:], in_=SSQ[:], func=AF.Exp, scale=-0.5), writes=[dSSQ])
            YBv = YB[:].rearrange("p q (h e) -> p (q h) e", e=128)
            K.dve.op(lambda e: e.tensor_tensor(out=YBv, in0=YBv, in1=SSQ[:].unsqueeze(2).broadcast_to([128, NQ * 4, 128]), op=ALU.mult),
                     reads=[dSSQ], writes=[dYB])
            K.pool.op(lambda e: e.tensor_tensor(out=YBv, in0=YBv, in1=SWb[:].unsqueeze(1).broadcast_to([128, NQ * 4, 128]), op=ALU.mult),
                      reads=[d_c], writes=[dYB])
            for qi in range(NQ):
                ptp, dptp = PTp.next()
                for h in range(4):
                    K.pe.op(lambda e, ptp=ptp, qi=qi, h=h: e.transpose(out=ptp[:, h, :], in_=YB[:, qi, h * 128:(h + 1) * 128],
                                                                      identity=identb[:]),
                            reads=[dYB, d_c], writes=[dptp] if h == 0 else [], adds=[] if h == 0 else [dptp])
                YT, dYT = YTr.next()
                K.act.op(lambda e, ptp=ptp, YT=YT: e.activation(out=YT[:], in_=ptp[:, 0:4, :], func=AF.Copy),
                         reads=[dptp], writes=[dYT])
                t0 = b * S + qi * 128
                K.sp.dma(lambda e, YT=YT, t0=t0: e.dma_start(
                    out=dap(scr["yb_fm"], t0, [[T, 128], [128 * T, 4], [1, 128]]), in_=YT[:]),
                    reads=[dYT], adds=[scr["d_p3"]])


def load_w_bf16(K, es, src2d, rows, cols, name, dep, stage_rot, q, W=None):
    nk = rows // 128
    if W is None:
        W = K.sb([128, nk, cols], BF16, name, es)
    for kc in range(nk):
        for c0 in range(0, cols, 1024):
            n = min(1024, cols - c0)
            st, dst = stage_rot.next()
            q[0] += 1
            (K.sp if q[0] % 2 == 0 else K.pool).dma(lambda e, st=st, kc=kc, c0=c0, n=n: e.dma_start(
                out=st[:, :n], in_=src2d[kc * 128:(kc + 1) * 128, c0:c0 + n]), writes=[dst])
            if q[0] % 2 == 0:
                K.act.op(lambda e, st=st, kc=kc, c0=c0, n=n: e.activation(out=W[:, kc, c0:c0 + n], in_=st[:, :n], func=AF.Copy),
                         reads=[dst], adds=[dep])
            else:
                K.dve.op(lambda e, st=st, kc=kc, c0=c0, n=n: e.tensor_copy(out=W[:, kc, c0:c0 + n], in_=st[:, :n]),
                         reads=[dst], adds=[dep])
    return W


def phase4(K, cfg, io, scr):
    T, S, NB = cfg["T"], cfg["S"], cfg["NB"]
    NT = T // 512
    with ExitStack() as es:
        d_c = Dep()
        identb = K.sb([128, 128], BF16, "m_identb", es)
        K.sp.dma(lambda e: e.dma_start(out=identb[:], in_=io["ident_bf"][:, :]), adds=[d_c])
        NF = K.sb([128, D], F32, "NF", es)
        K.sp.dma(lambda e: e.dma_start(out=NF[:], in_=io["norm_ffn_w"][0:1, :].broadcast_to([128, D])), adds=[d_c])
        stg = Rot(K, 2, [128, 1024], F32, "m_stg", es=es)
        q = [0]
        PAw = load_w_bf16(K, es, io["proj_a"][0], 512, D, "PAw", d_c, stg, q)
        PBw = load_w_bf16(K, es, io["proj_b"][0], 512, D, "PBw", d_c, stg, q)
        WO = load_w_bf16(K, es, io["w_out"][0], D, D, "WO", d_c, stg, q)
        YAr = Rot(K, 2, [128, 4, 512], BF16, "mYA", es=es)
        YBr = Rot(K, 2, [128, 4, 512], BF16, "mYB", es=es)
        SGr = Rot(K, 2, [128, 16, 512], BF16, "mSG", es=es)
        MGr = Rot(K, 2, [128, 8, 512], BF16, "mMG", es=es)
        t1r = Rot(K, 2, [128, 512], F32, "mt1", es=es)
        t2r = Rot(K, 2, [128, 512], F32, "mt2", es=es)
        Xr = Rot(K, 2, [128, D], F32, "mX", es=es)
        X1r = Rot(K, 2, [128, D], F32, "mX1", es=es)
        XHr = Rot(K, 2, [128, D], F32, "mXH", es=es)
        XBr = Rot(K, 2, [128, D], BF16, "mXB", es=es)
        XTr = Rot(K, 2, [128, 8, 128], BF16, "mXT", es=es)
        junk = K.sb([128, D], BF16, "mjunk", es)
        d_junk = Dep()
        STr = Rot(K, 4, [128, 4], F32, "mST", es=es)
        PP = Rot(K, 2, [128, 2, 512], F32, "mPP", psum=True, es=es)
        PO2 = Rot(K, 1, [128, 2, 512], F32, "mPO", psum=True, es=es)
        PTp = Rot(K, 1, [128, 8, 128], BF16, "mPTp", psum=True, es=es)
        def make_gen(ti):
            cs = slice(ti * 512, (ti + 1) * 512)
            YA, dYA = YAr.next()
            YB, dYB = YBr.next()
            SG, dSG = SGr.next()
            K.sp.dma(lambda e, YA=YA, cs=cs: e.dma_start(out=YA[:], in_=scr["ya_fm"][:, cs].rearrange("(c p) t -> p c t", p=128)),
                     reads=[scr["d_p2c"]], writes=[dYA])
            K.pool.dma(lambda e, YB=YB, cs=cs: e.dma_start(out=YB[:], in_=scr["yb_fm"][:, cs].rearrange("(c p) t -> p c t", p=128)),
                       reads=[scr["d_p3"]], writes=[dYB])
            K.sp.dma(lambda e, SG=SG, cs=cs: e.dma_start(out=SG[:], in_=scr["sg_fm"][:, cs].rearrange("(c p) t -> p c t", p=128)),
                     reads=[scr["d_p1"]], writes=[dSG])
            MG, dMG = MGr.next()
            for m in range(8):
                pp, dpp = PP.next()
                for c in range(4):
                    K.pe.op(lambda e, pp=pp, YA=YA, c=c, m=m: e.matmul(pp[:, 0, :], PAw[:, c, m * 128:(m + 1) * 128], YA[:, c, :],
                                                                      start=(c == 0), stop=(c == 3)),
                            reads=[dYA, d_c], writes=[dpp] if c == 0 else [], adds=[] if c == 0 else [dpp])
                for c in range(4):
                    K.pe.op(lambda e, pp=pp, YB=YB, c=c, m=m: e.matmul(pp[:, 1, :], PBw[:, c, m * 128:(m + 1) * 128], YB[:, c, :],
                                                                      start=(c == 0), stop=(c == 3)),
                            reads=[dYB, d_c], adds=[dpp])
                t1, dt1 = t1r.next()
                t2, dt2 = t2r.next()
                K.dve.op(lambda e, t1=t1, pp=pp, SG=SG, m=m: e.tensor_tensor(out=t1[:], in0=pp[:, 0, :], in1=SG[:, m, :], op=ALU.mult),
                         reads=[dpp, dSG], writes=[dt1])
                K.dve.op(lambda e, t2=t2, pp=pp, SG=SG, m=m: e.tensor_tensor(out=t2[:], in0=pp[:, 1, :], in1=SG[:, 8 + m, :], op=ALU.mult),
                         reads=[dpp, dSG], writes=[dt2])
                K.pool.op(lambda e, t1=t1, t2=t2, MG=MG, m=m: e.tensor_tensor(out=MG[:, m, :], in0=t1[:], in1=t2[:], op=ALU.add),
                          reads=[dt1, dt2], writes=[dMG] if m == 0 else [], adds=[] if m == 0 else [dMG])
                if m % 2 == 1:
                    yield
            for sub in range(4):
                t0 = ti * 512 + sub * 128
                X, dX = Xr.next()
                K.pool.dma(lambda e, X=X, t0=t0: e.dma_start(out=X[:], in_=io["x"][t0:t0 + 128, :]), writes=[dX])
                po, dpo = PO2.next()
                for n in range(2):
                    for m in range(8):
                        K.pe.op(lambda e, po=po, MG=MG, m=m, n=n, sub=sub: e.matmul(
                            po[:, n, :], MG[:, m, sub * 128:(sub + 1) * 128], WO[:, m, n * 512:(n + 1) * 512],
                            start=(m == 0), stop=(m == 7)), reads=[dMG, d_c],
                            writes=[dpo] if (m == 0 and n == 0) else [], adds=[] if (m == 0 and n == 0) else [dpo])
                X1, dX1 = X1r.next()
                K.dve.op(lambda e, X1=X1, X=X, po=po: e.tensor_tensor(out=X1[:], in0=X[:], in1=po[:].rearrange("p a b -> p (a b)"),
                                                                     op=ALU.add), reads=[dX, dpo], writes=[dX1])
                K.sp.dma(lambda e, X1=X1, t0=t0: e.dma_start(out=scr["x1_tm"][t0:t0 + 128, :], in_=X1[:]),
                         reads=[dX1], adds=[scr["d_p4"]])
                ST, dST = STr.next()
                K.act.op(lambda e, X1=X1, ST=ST: e.activation(out=junk[:], in_=X1[:], func=AF.Square, accum_out=ST[:, 0:1]),
                         reads=[dX1], writes=[d_junk, dST])
                K.dve.op(lambda e, ST=ST: e.tensor_scalar(out=ST[:, 1:2], in0=ST[:, 0:1], scalar1=1.0 / D, scalar2=1e-6,
                                                          op0=ALU.mult, op1=ALU.add), writes=[dST])
                K.act.op(lambda e, ST=ST: e.activation(out=ST[:, 2:3], in_=ST[:, 1:2], func=AF.Ln), writes=[dST])
                K.act.op(lambda e, ST=ST: e.activation(out=ST[:, 3:4], in_=ST[:, 2:3], func=AF.Exp, scale=-0.5), writes=[dST])
                XH, dXH = XHr.next()
                K.dve.op(lambda e, XH=XH, X1=X1, ST=ST: e.scalar_tensor_tensor(
                    out=XH[:], in0=X1[:], scalar=ST[:, 3:4], in1=NF[:], op0=ALU.mult, op1=ALU.mult),
                    reads=[dX1, dST, d_c], writes=[dXH])
                K.sp.dma(lambda e, XH=XH, t0=t0: e.dma_start(out=scr["xh_tm"][t0:t0 + 128, :], in_=XH[:]),
                         reads=[dXH], adds=[scr["d_p4"]])
                XB, dXB = XBr.next()
                K.act.op(lambda e, XB=XB, XH=XH: e.activation(out=XB[:], in_=XH[:], func=AF.Copy), reads=[dXH], writes=[dXB])
                yield
                ptp, dptp = PTp.next()
                for kc in range(8):
                    K.pe.op(lambda e, ptp=ptp, XB=XB, kc=kc: e.transpose(out=ptp[:, kc, :], in_=XB[:, kc * 128:(kc + 1) * 128],
                                                                        identity=identb[:]),
                            reads=[dXB, d_c], writes=[dptp] if kc == 0 else [], adds=[] if kc == 0 else [dptp])
                XT, dXT = XTr.next()
                K.act.op(lambda e, ptp=ptp, XT=XT: e.activation(out=XT[:], in_=ptp[:], func=AF.Copy), reads=[dptp], writes=[dXT])
                K.sp.dma(lambda e, XT=XT, t0=t0: e.dma_start(
                    out=dap(scr["xhT_fm"], t0, [[T, 128], [128 * T, 8], [1, 128]]), in_=XT[:]),
                    reads=[dXT], adds=[scr["d_p4"]])
                yield

        work = [(ti,) for ti in range(NT)]

        active = []
        nxt = 0
        while nxt < len(work) or active:
            if len(active) < 2 and nxt < len(work):
                active.append(make_gen(*work[nxt]))
                nxt += 1
            for a in list(active):
                try:
                    next(a)
                except StopIteration:
                    active.remove(a)


def phase5(K, cfg, io, scr):
    T, S, NB = cfg["T"], cfg["S"], cfg["NB"]
    NTT = T // 128
    with ExitStack() as es:
        d_c = Dep()
        identb = K.sb([128, 128], BF16, "f_identb", es)
        K.sp.dma(lambda e: e.dma_start(out=identb[:], in_=io["ident_bf"][:, :]), adds=[d_c])
        IOTA = K.sb([128, 16], F32, "IOTA", es)
        K.sp.dma(lambda e: e.dma_start(out=IOTA[:], in_=io["iota16"][:, :]), adds=[d_c])
        FNW = K.sb([128, D], F32, "FNW", es)
        K.sp.dma(lambda e: e.dma_start(out=FNW[:], in_=io["final_norm_w"][0:1, :].broadcast_to([128, D])), adds=[d_c])
        WQ = K.sb([128, 8, 2048], BF16, "WQ", es)
        KT = K.sb([128, 16, 128], BF16, "KT", es)
        PQ = Rot(K, 1, [128, 8, 128], F32, "fPQ", psum=True, es=es)
        PSc = Rot(K, 1, [128, 8, 128], F32, "fPSc", psum=True, es=es)
        PKT = Rot(K, 1, [128, 8, 128], BF16, "fPKT", psum=True, es=es)
        es_setup = ExitStack()
        stg = Rot(K, 2, [128, 1024], F32, "f_stg", es=es_setup)
        q = [0]
        load_w_bf16(K, es, io["peer_wq"][0], D, 2048, "WQ", d_c, stg, q, W=WQ)
        KF = K.sb([128, 16, 128], F32, "KF", es_setup)
        dKF = Dep()
        K.sp.dma(lambda e: e.dma_start(out=KF[:], in_=io["peer_keys"][0].rearrange("h c n d -> n (h c) d")), writes=[dKF])
        KB = K.sb([128, 16, 128], BF16, "KB", es_setup)
        K.dve.op(lambda e: e.tensor_copy(out=KB[:], in_=KF[:]), reads=[dKF], writes=[dKF])
        for half in range(2):
            pk, dpk = PKT.next()
            for i in range(8):
                K.pe.op(lambda e, pk=pk, i=i, half=half: e.transpose(out=pk[:, i, :], in_=KB[:, half * 8 + i, :], identity=identb[:]),
                        reads=[dKF, d_c], writes=[dpk] if i == 0 else [], adds=[] if i == 0 else [dpk])
            K.act.op(lambda e, pk=pk, half=half: e.activation(out=KT[:, half * 8:(half + 1) * 8, :], in_=pk[:], func=AF.Copy),
                     reads=[dpk], adds=[d_c])

        K.barrier()
        es_setup.close()
        XTr = Rot(K, 2, [128, 8, 128], BF16, "fXT", es=es)
        XHr = Rot(K, 2, [128, D], F32, "fXH", es=es)
        X1r = Rot(K, 2, [128, D], F32, "fX1", es=es)
        QTr = Rot(K, 1, [128, 16, 128], BF16, "fQT", es=es)
        SCr = Rot(K, 1, [128, 16, 128], F32, "fSC", es=es)
        SC2 = K.sb([128, 256], F32, "fSC2", es)
        dSC2 = Dep()
        M16r = Rot(K, 1, [128, 16, 16], F32, "fM16", es=es)
        I16r = Rot(K, 1, [128, 16, 16], U32, "fI16", es=es)
        I16fr = Rot(K, 1, [128, 16, 16], F32, "fI16f", es=es)
        CANDr = Rot(K, 1, [128, 8, 256], F32, "fCAND", es=es)
        VALr = Rot(K, 1, [128, 8, 16], F32, "fVAL", es=es)
        CIr = Rot(K, 1, [128, 3, 128], U32, "fCI", es=es)
        ABr = Rot(K, 1, [128, 2, 128], F32, "fAB", es=es)
        OHr = CANDr
        E12r = Rot(K, 1, [128, 3, 128], F32, "fE12", es=es)
        IDSr = Rot(K, 2, [128, 128], I32, "fIDS", es=es)
        GTr = Rot(K, 2, [128, 4, 128], F32, "fGT", es=es)
        S8r = Rot(K, 2, [128, 16], F32, "fS8", es=es)
        GRP = cfg.get("grp", 4)
        ACTDOT = tuple(cfg.get("actdot", (0, 2)))
        if isinstance(cfg.get("actdot_mask"), int):
            ACTDOT = tuple(i for i in range(GRP) if (cfg["actdot_mask"] >> i) & 1)
        junk3r = Rot(K, 2, [128, D], BF16, "fjunk3", es=es)
        junkr = Rot(K, 3, [128, D], BF16, "fjunkr", es=es)
        PRDr = Rot(K, 3, [128, D], BF16, "fPRD", es=es)
        GBr = Rot(K, cfg.get("ngbuf", 22), [128, 2 * D], BF16, "fGB", es=es)
        junk2 = K.sb([128, D], BF16, "fjunk2", es)
        d_junk2 = Dep()
        XHbr = Rot(K, 2, [128, D], BF16, "fXHb", es=es)
        DGr = Rot(K, 4, [128, 128], BF16, "fDG", es=es)
        PY = Rot(K, 1, [128, 2, 512], F32, "fPY", psum=True, es=es)

        RES = {}

        def routing(ti):
            t0 = ti * 128
            XT, dXT = XTr.next()
            XH, dXH = XHr.next()
            X1, dX1 = X1r.next()
            K.sp.dma(lambda e, XT=XT, t0=t0: e.dma_start(out=XT[:], in_=dap(scr["xhT_fm"], t0, [[T, 128], [128 * T, 8], [1, 128]])),
                     reads=[scr["d_p4"]], writes=[dXT])
            K.sp.dma(lambda e, XH=XH, t0=t0: e.dma_start(out=XH[:], in_=scr["xh_tm"][t0:t0 + 128, :]), reads=[scr["d_p4"]], writes=[dXH])
            K.sp.dma(lambda e, X1=X1, t0=t0: e.dma_start(out=X1[:], in_=scr["x1_tm"][t0:t0 + 128, :]), reads=[scr["d_p4"]], writes=[dX1])
            QT, dQT = QTr.next()
            for half in range(2):
                pq, dpq = PQ.next()
                for i in range(8):
                    hc = half * 8 + i
                    for kc in range(8):
                        K.pe.op(lambda e, pq=pq, i=i, hc=hc, kc=kc, XT=XT: e.matmul(
                            pq[:, i, :], WQ[:, kc, hc * 128:(hc + 1) * 128], XT[:, kc, :], start=(kc == 0), stop=(kc == 7)),
                            reads=[dXT, d_c], writes=[dpq] if (i == 0 and kc == 0) else [], adds=[] if (i == 0 and kc == 0) else [dpq])
                K.act.op(lambda e, pq=pq, QT=QT, half=half: e.activation(out=QT[:, half * 8:(half + 1) * 8, :], in_=pq[:], func=AF.Copy),
                         reads=[dpq], writes=[dQT] if half == 0 else [], adds=[] if half == 0 else [dQT])
            SC, dSC = SCr.next()
            for half in range(2):
                psc, dpsc = PSc.next()
                for i in range(8):
                    hc = half * 8 + i
                    K.pe.op(lambda e, psc=psc, i=i, hc=hc, QT=QT: e.matmul(psc[:, i, :], QT[:, hc, :], KT[:, hc, :], start=True, stop=True),
                            reads=[dQT, d_c], writes=[dpsc] if i == 0 else [], adds=[] if i == 0 else [dpsc])
                K.act.op(lambda e, psc=psc, SC=SC, half=half: e.activation(out=SC[:, half * 8:(half + 1) * 8, :], in_=psc[:], func=AF.Copy),
                         reads=[dpsc], writes=[dSC] if half == 0 else [], adds=[] if half == 0 else [dSC])
            yield
            M16, dM = M16r.next()
            I16, dI = I16r.next()
            for hc in range(16):
                if hc % 4 == 0 and hc > 0:
                    yield
                K.dve.op(lambda e, M16=M16, SC=SC, hc=hc: e.max(out=M16[:, hc, 0:8], in_=SC[:, hc, :]), reads=[dSC],
                         writes=[dM] if hc == 0 else [], adds=[] if hc == 0 else [dM])
                K.dve.op(lambda e, M16=M16, SC=SC, hc=hc: e.match_replace(out=SC2[:, 0:128], in_to_replace=M16[:, hc, 0:8],
                                                                         in_values=SC[:, hc, :], imm_value=-1e30),
                         reads=[dSC, dM], writes=[dSC2])
                K.dve.op(lambda e, M16=M16, hc=hc: e.max(out=M16[:, hc, 8:16], in_=SC2[:, 0:128]), reads=[dSC2], adds=[dM])
                K.dve.op(lambda e, M16=M16, I16=I16, SC=SC, hc=hc: e.max_index(out=I16[:, hc, 0:8], in_max=M16[:, hc, 0:8],
                                                                              in_values=SC[:, hc, :]),
                         reads=[dSC, dM], writes=[dI] if hc == 0 else [], adds=[] if hc == 0 else [dI])
                K.dve.op(lambda e, M16=M16, I16=I16, SC=SC, hc=hc: e.max_index(out=I16[:, hc, 8:16], in_max=M16[:, hc, 8:16],
                                                                              in_values=SC[:, hc, :]),
                         reads=[dSC, dM], adds=[dI])
            yield
            I16f, dIf = I16fr.next()
            K.dve.op(lambda e, I16f=I16f, I16=I16: e.tensor_copy(out=I16f[:], in_=I16[:]), reads=[dI], writes=[dIf])
            I16fv = I16f[:].rearrange("p (h c) k -> p h c k", c=2)
            K.dve.op(lambda e, I16fv=I16fv: e.tensor_scalar(out=I16fv[:, :, 0, :], in0=I16fv[:, :, 0, :], scalar1=128.0, scalar2=None,
                                                            op0=ALU.mult), writes=[dIf])
            CAND, dCA = CANDr.next()
            M16v = M16[:].rearrange("p (h c) k -> p h c k", c=2)
            K.dve.op(lambda e, CAND=CAND, M16v=M16v: e.tensor_tensor(
                out=CAND[:].rearrange("p h (a b) -> p h a b", b=16),
                in0=M16v[:, :, 0, :].unsqueeze(3).broadcast_to([128, 8, 16, 16]),
                in1=M16v[:, :, 1, :].unsqueeze(2).broadcast_to([128, 8, 16, 16]), op=ALU.add),
                reads=[dM], writes=[dCA])
            VAL, dVAL = VALr.next()
            CI, dCI = CIr.next()
            CIv = CI[:, 0, :].rearrange("p (h k) -> p h k", k=16)
            for h in range(8):
                if h % 4 == 0:
                    yield
                K.dve.op(lambda e, VAL=VAL, CAND=CAND, h=h: e.max(out=VAL[:, h, 0:8], in_=CAND[:, h, :]), reads=[dCA],
                         writes=[dVAL] if h == 0 else [], adds=[] if h == 0 else [dVAL])
                K.dve.op(lambda e, VAL=VAL, CAND=CAND, h=h: e.match_replace(out=SC2[:, :], in_to_replace=VAL[:, h, 0:8],
                                                                           in_values=CAND[:, h, :], imm_value=-1e30),
                         reads=[dCA, dVAL], writes=[dSC2])
                K.dve.op(lambda e, VAL=VAL, h=h: e.max(out=VAL[:, h, 8:16], in_=SC2[:, :]), reads=[dSC2], adds=[dVAL])
                K.dve.op(lambda e, VAL=VAL, CIv=CIv, CAND=CAND, h=h: e.max_index(out=CIv[:, h, 0:8], in_max=VAL[:, h, 0:8],
                                                                                in_values=CAND[:, h, :]),
                         reads=[dCA, dVAL], writes=[dCI] if h == 0 else [], adds=[] if h == 0 else [dCI])
                K.dve.op(lambda e, VAL=VAL, CIv=CIv, CAND=CAND, h=h: e.max_index(out=CIv[:, h, 8:16], in_max=VAL[:, h, 8:16],
                                                                                in_values=CAND[:, h, :]),
                         reads=[dCA, dVAL], adds=[dCI])
            yield
            GT, dGT = GTr.next()
            S8, dS8 = S8r.next()
            Ev = GT[:, 0, :].rearrange("p (h k) -> p h k", k=16)
            Gv = GT[:, 1, :].rearrange("p (h k) -> p h k", k=16)
            K.dve.op(lambda e, Ev=Ev, VAL=VAL: e.tensor_tensor(out=Ev, in0=VAL[:], in1=VAL[:, :, 0:1].broadcast_to([128, 8, 16]),
                                                              op=ALU.subtract), reads=[dVAL], writes=[dGT])
            K.act.op(lambda e, GT=GT: e.activation(out=GT[:, 0, :], in_=GT[:, 0, :], func=AF.Exp), writes=[dGT])
            K.dve.op(lambda e, Ev=Ev, S8=S8: e.tensor_reduce(out=S8[:, 0:8], in_=Ev, axis=AX.X, op=ALU.add), reads=[dGT], writes=[dS8])
            K.dve.op(lambda e, S8=S8: e.reciprocal(out=S8[:, 8:16], in_=S8[:, 0:8]), writes=[dS8])
            K.dve.op(lambda e, Ev=Ev, Gv=Gv, S8=S8: e.tensor_tensor(out=Gv, in0=Ev, in1=S8[:, 8:16].unsqueeze(2).broadcast_to([128, 8, 16]),
                                                                   op=ALU.mult), reads=[dS8], writes=[dGT])
            yield
            K.dve.op(lambda e, CI=CI: e.tensor_single_scalar(out=CI[:, 1, :], in_=CI[:, 0, :], scalar=4, op=ALU.logical_shift_right),
                     writes=[dCI])
            K.dve.op(lambda e, CI=CI: e.tensor_single_scalar(out=CI[:, 2, :], in_=CI[:, 0, :], scalar=15, op=ALU.bitwise_and),
                     writes=[dCI])
            AB, dAB = ABr.next()
            K.dve.op(lambda e, AB=AB, CI=CI: e.tensor_copy(out=AB[:], in_=CI[:, 1:3, :]), reads=[dCI], writes=[dAB])
            OH, dOH = OHr.next()
            E12, dE12 = E12r.next()
            OHv = OH[:].rearrange("p h (j a) -> p h j a", a=16)
            for c in range(2):
                ABv = AB[:, c, :].rearrange("p (h j) -> p h j", j=16)
                K.dve.op(lambda e, OHv=OHv, ABv=ABv: e.tensor_tensor(
                    out=OHv, in0=ABv.unsqueeze(3).broadcast_to([128, 8, 16, 16]),
                    in1=IOTA[:].unsqueeze(1).unsqueeze(1).broadcast_to([128, 8, 16, 16]), op=ALU.is_equal),
                    reads=[dAB, d_c], writes=[dOH])
                K.dve.op(lambda e, OHv=OHv, I16fv=I16fv, c=c: e.tensor_tensor(
                    out=OHv, in0=OHv, in1=I16fv[:, :, c, :].unsqueeze(2).broadcast_to([128, 8, 16, 16]), op=ALU.mult),
                    reads=[dIf], writes=[dOH])
                K.dve.op(lambda e, OHv=OHv, E12=E12, c=c: e.tensor_reduce(
                    out=E12[:, c, :].rearrange("p (h j) -> p h j", j=16), in_=OHv, axis=AX.X, op=ALU.add),
                    reads=[dOH], writes=[dE12] if c == 0 else [], adds=[] if c == 0 else [dE12])
            K.dve.op(lambda e, E12=E12: e.tensor_tensor(out=E12[:, 2, :], in0=E12[:, 0, :], in1=E12[:, 1, :], op=ALU.add), writes=[dE12])
            IDS, dIDS = IDSr.next()
            K.dve.op(lambda e, IDS=IDS, E12=E12: e.tensor_copy(out=IDS[:], in_=E12[:, 2, :]), reads=[dE12], writes=[dIDS])
            if "ids_dbg" in scr:
                K.sp.dma(lambda e, IDS=IDS, t0=t0: e.dma_start(out=scr["ids_dbg"][t0:t0 + 128, :], in_=IDS[:]), reads=[dIDS])
                K.sp.dma(lambda e, GT=GT, t0=t0: e.dma_start(out=scr["gate_dbg"][t0:t0 + 128, :], in_=GT[:, 1, :]), reads=[dGT])
            RES[ti] = dict(t0=t0, XH=XH, dXH=dXH, X1=X1, dX1=dX1, IDS=IDS, dIDS=dIDS, GT=GT, dGT=dGT, S8=S8, dS8=dS8)

        GDEPS = {}

        def expert(R):
            t0, XH, dXH, X1, dX1, IDS, dIDS, GT, dGT, S8, dS8 = (R[k] for k in
                ("t0", "XH", "dXH", "X1", "dX1", "IDS", "dIDS", "GT", "dGT", "S8", "dS8"))
            XHb, dXHb = XHbr.next()
            K.act.op(lambda e: e.activation(out=XHb[:], in_=XH[:], func=AF.Copy), reads=[dXH], writes=[dXHb])
            py, dpy = PY.next()
            NGRP = 128 // GRP
            bufs = {}
            gd = GDEPS.setdefault(id(GT), [(Dep(), Dep()) for _ in range(NGRP)])

            def stage_a(g):
                for jj in range(GRP):
                    j = g * GRP + jj
                    GB, dGB = GBr.next()
                    bufs[j] = (GB, dGB)
                    K.pool.dma(lambda e, GB=GB, j=j: e.indirect_dma_start(
                        out=GB[:], out_offset=None, in_=scr["uv_tab"][:, :],
                        in_offset=bass.IndirectOffsetOnAxis(ap=IDS[:, j:j + 1], axis=0)), reads=[dIDS, scr["d_uv"]], writes=[dGB])
                    if jj in ACTDOT:
                        PRD, dPRD = PRDr.next()
                        K.dve.op(lambda e, GB=GB, PRD=PRD: e.tensor_tensor(out=PRD[:], in0=GB[:, 0:D], in1=XHb[:], op=ALU.mult),
                                 reads=[dGB, dXHb], writes=[dPRD])
                        j3, dj3 = junk3r.next()
                        K.act.op(lambda e, PRD=PRD, j=j, j3=j3: e.activation(out=j3[:], in_=PRD[:], func=AF.Copy, accum_out=GT[:, 2, j:j + 1]),
                                 reads=[dPRD], writes=[dj3] + ([gd[g][0]] if jj == 0 else []), adds=[] if jj == 0 else [gd[g][0]])
                    else:
                        j1, dj1 = junkr.next()
                        K.dve.op(lambda e, GB=GB, j=j, j1=j1: e.scalar_tensor_tensor(
                            out=j1[:], in0=GB[:, 0:D], scalar=1.0, in1=XHb[:], op0=ALU.mult, op1=ALU.mult, accum_out=GT[:, 2, j:j + 1]),
                            reads=[dGB, dXHb], writes=[dj1] + ([gd[g][0]] if jj == 0 else []), adds=[] if jj == 0 else [gd[g][0]])
                gs = slice(g * GRP, (g + 1) * GRP)
                K.act.op(lambda e: e.activation(out=GT[:, 3, gs], in_=GT[:, 2, gs], func=AF.Gelu), reads=[gd[g][0]], writes=[gd[g][1]])

            def stage_b(g):
                gs = slice(g * GRP, (g + 1) * GRP)
                K.dve.op(lambda e: e.tensor_tensor(out=GT[:, 3, gs], in0=GT[:, 3, gs], in1=GT[:, 1, gs], op=ALU.mult), reads=[dGT], writes=[gd[g][1]])
                for jj in range(GRP):
                    j = g * GRP + jj
                    GB, dGB = bufs.pop(j)
                    DG, dDG = DGr.next()
                    K.act.op(lambda e, DG=DG, j=j: e.activation(out=DG[:], in_=identb[:], func=AF.Copy, scale=GT[:, 3, j:j + 1]),
                             reads=[gd[g][1], d_c], writes=[dDG])
                    for n in range(2):
                        K.pe.op(lambda e, DG=DG, GB=GB, n=n, j=j: e.matmul(
                            py[:, n, :], DG[:], GB[:, D + n * 512:D + (n + 1) * 512], start=(j == 0), stop=(j == 127)),
                            reads=[dDG, dGB], writes=[dpy] if (j == 0 and n == 0) else [], adds=[] if (j == 0 and n == 0) else [dpy])

            gen = routing(R["next"]) if R.get("next") is not None else None
            for g in range(NGRP):
                stage_a(g)
                if g >= 1:
                    stage_b(g - 1)
                if gen is not None and g >= 2 and g % 2 == 0:
                    try:
                        next(gen)
                    except StopIteration:
                        gen = None
            stage_b(NGRP - 1)
            if gen is not None:
                for _ in gen:
                    pass
            K.dve.op(lambda e: e.tensor_tensor(out=X1[:], in0=X1[:], in1=py[:].rearrange("p a b -> p (a b)"), op=ALU.add),
                     reads=[dpy], writes=[dX1])
            K.act.op(lambda e: e.activation(out=junk2[:], in_=X1[:], func=AF.Square, accum_out=S8[:, 0:1]),
                     reads=[dX1], writes=[d_junk2, dS8])
            K.dve.op(lambda e: e.tensor_scalar(out=S8[:, 1:2], in0=S8[:, 0:1], scalar1=1.0 / D, scalar2=1e-6,
                                               op0=ALU.mult, op1=ALU.add), writes=[dS8])
            K.act.op(lambda e: e.activation(out=S8[:, 2:3], in_=S8[:, 1:2], func=AF.Sqrt), writes=[dS8])
            K.dve.op(lambda e: e.reciprocal(out=S8[:, 3:4], in_=S8[:, 2:3]), writes=[dS8])
            K.dve.op(lambda e: e.scalar_tensor_tensor(
                out=XH[:], in0=X1[:], scalar=S8[:, 3:4], in1=FNW[:], op0=ALU.mult, op1=ALU.mult),
                reads=[dX1, dS8, d_c], writes=[dXH])
            K.sp.dma(lambda e: e.dma_start(out=io["out"][t0:t0 + 128, :], in_=XH[:]), reads=[dXH])

        for _ in routing(0):
            pass
        for ti in range(NTT):
            R = RES.pop(ti)
            R["next"] = ti + 1 if ti + 1 < NTT else None
            expert(R)


def make_consts():
    c = {}
    c["ident_bf"] = np.eye(128, dtype=np.float32).astype(ml_dtypes.bfloat16)
    c["ident_f"] = np.eye(128, dtype=np.float32)
    bo = np.zeros((128, 128), np.float32)
    bo[:64, :64] = 1.0 / 64
    bo[64:, 64:] = 1.0 / 64
    c["blockones"] = bo
    pp = np.arange(128)[:, None]
    ff = np.arange(128)[None, :]
    c["tri"] = ((pp <= ff) & (pp // 64 == ff // 64)).astype(np.float32)
    p6 = np.arange(64)[:, None]
    f6 = np.arange(64)[None, :]
    mk = np.zeros((64, 3, 64), np.float32)
    mk[:, 0, :] = (f6 < p6)
    mk[:, 1, :] = (f6 > p6)
    mk[:, 2, :] = (f6 >= p6)
    c["masks"] = mk
    c["ident64"] = np.eye(64, dtype=np.float32).astype(ml_dtypes.bfloat16)
    c["ones64"] = np.ones((64, 1), np.float32)
    c["iota16"] = np.tile(np.arange(16, dtype=np.float32)[None, :], (128, 1))
    return c


def make_alibi(S):
    al = np.zeros((4, 128, S), np.float32)
    ql = np.arange(128)[:, None]
    m = np.arange(S)[None, :]
    for h in range(4):
        slope = 2.0 ** (-8.0 * (h + 1) / 4)
        v = -slope * (ql - m + (S - 128)).astype(np.float32)
        al[h] = np.where(m <= ql + (S - 128), v, -30000.0)
    return al


def _unused():
    c = {}
    return c


def build(cfg):
    NB, S = cfg["NB"], cfg["S"]
    T = NB * S
    cfg["T"] = T
    dbg = set(cfg.get("debug", ()))
    phases = cfg.get("phases", (1,))
    nc = bass.Bass("TRN2", target_bir_lowering=False)
    io = {}

    def inp(name, shape, dt=F32):
        io[name] = nc.dram_tensor(name, list(shape), dt, kind="ExternalInput").ap()

    inp("x", [T, D])
    inp("norm_mix_w", [1, D])
    inp("w_in", [1, D, IN_COLS])
    inp("ident_bf", [128, 128], BF16)
    inp("ident_f", [128, 128])
    inp("blockones", [128, 128])
    inp("tri", [128, 128])
    inp("masks", [64, 3, 64])
    inp("ident64", [64, 64], BF16)
    inp("ones64", [64, 1])
    inp("alibi", [4, 128, S])
    for nm, shp in [("lam_q1", [1, 64]), ("lam_k1", [1, 64]), ("lam_q2", [1, 64]), ("lam_k2", [1, 64]),
                    ("subln_w", [1, 128])]:
        inp(nm, shp)
    scr = {}

    def scratch(name, shape, dt):
        kind = "ExternalOutput" if name in dbg else "Internal"
        scr[name] = nc.dram_tensor(name, list(shape), dt, kind=kind).ap()

    scratch("zs_tm", [T, SHIFT_COLS], F32)
    scratch("zv_fm", [512, T], F32)
    scratch("qk_fm", [1024, T], BF16)
    scratch("av_tm", [T, 512], BF16)
    scratch("sg_fm", [2048, T], BF16)
    if not cfg.get("chunked", True):
        scratch("rw_tm", [T, 5, 512], F32)
    scratch("ab_tm", [T, 5, 512], BF16)
    scratch("lw_tm", [T, 512], F32)
    scratch("v_fm", [512, T], F32)
    scratch("g_fm", [512, T], F32)
    scratch("coef_fm", [8, T], F32)
    scratch("y_fm", [512, T], F32)
    scratch("ya_fm", [512, T], BF16)
    scr["d_p1"] = Dep()
    scr["d_p2a"] = Dep()
    scr["d_p2b"] = Dep()
    scr["d_p2c"] = Dep()
    scr["d_p3"] = Dep()
    scr["d_p4"] = Dep()
    scr["d_uv"] = Dep()
    scratch("uv_tab", [16384, 2 * D], BF16)
    if "ids_dbg" in dbg:
        scratch("ids_dbg", [T, 128], I32)
        scratch("gate_dbg", [T, 128], F32)
    inp("peer_wq", [1, D, 2048])
    inp("peer_keys", [1, 8, 2, 128, 128])
    inp("peer_u", [1, 16384, D])
    inp("peer_v", [1, 16384, D])
    inp("final_norm_w", [1, D])
    inp("iota16", [128, 16])
    io["out"] = nc.dram_tensor("out", [T, D], F32, kind="ExternalOutput").ap()
    scratch("x1_tm", [T, D], F32)
    scratch("xh_tm", [T, D], F32)
    scratch("xhT_fm", [D, T], BF16)
    for nm, shp in [("proj_a", [1, 512, D]), ("proj_b", [1, 512, D]), ("w_out", [1, D, D]), ("norm_ffn_w", [1, D])]:
        inp(nm, shp)
    scratch("yb_fm", [512, T], BF16)
    for nm, shp in [("shift_mu", [1, SHIFT_COLS]), ("w0", [1, 512]), ("w2", [1, 64, 512]), ("a0", [1, 512]),
                    ("a2", [1, 64, 512]), ("g2", [1, 128, 512]), ("k_k", [1, 512]), ("k_a", [1, 512]),
                    ("r_k", [1, 8, 64]), ("lnx_w", [1, 512]), ("lnx_b", [1, 512])]:
        inp(nm, shp)
    with ExitStack() as es:
        K = Kern(nc, es, pool_slots=cfg.get("pool_slots", 8))
        K.scopes = bool(cfg.get("scopes", False))
        if 5 in phases:
            K.phase = "p0_uvtab"
            RB = 2048
            for r0 in range(0, 16384, RB):
                K.pool.dma(lambda e, r0=r0: e.dma_start(out=scr["uv_tab"][r0:r0 + RB, 0:D], in_=io["peer_u"][0, r0:r0 + RB, :]),
                           adds=[scr["d_uv"]])
                K.pool.dma(lambda e, r0=r0: e.dma_start(out=scr["uv_tab"][r0:r0 + RB, D:2 * D], in_=io["peer_v"][0, r0:r0 + RB, :]),
                           adds=[scr["d_uv"]])
        if 1 in phases:
            K.phase = "p1_inproj"
            phase1(K, cfg, io, scr)
            K.barrier()
        if 2 in phases:
            K.phase = "p2a_prep"
            phase2_prep(K, cfg, io, scr)
            K.barrier()
            K.phase = "p2b_scan"
            if cfg.get("chunked", True):
                phase2_chunk(K, cfg, io, scr)
            else:
                phase2_scan(K, cfg, io, scr)
            K.barrier()
            K.phase = "p2c_post"
            phase2_post(K, cfg, io, scr)
            K.barrier()
        if 3 in phases:
            K.phase = "p3_attn"
            phase3(K, cfg, io, scr)
            K.barrier()
        if 4 in phases:
            K.phase = "p4_merge"
            phase4(K, cfg, io, scr)
            K.barrier()
        if 5 in phases:
            K.phase = "p5_peer"
            phase5(K, cfg, io, scr)
            K.barrier()
        K.finish()
    return nc, io, scr


def kernel(**inputs):
    NB, S = 4, 2048
    cfg = dict(NB=NB, S=S, phases=(1, 2, 3, 4, 5))
    nc, io, scr = build(cfg)
    consts = make_consts()
    consts["alibi"] = make_alibi(S)
    x = np.ascontiguousarray(np.asarray(inputs["x"], dtype=np.float32))
    shared = {}
    for name in io:
        if name in ("x", "out"):
            continue
        if name in consts:
            shared[name] = consts[name]
        elif name == "final_norm_w":
            shared[name] = np.ascontiguousarray(np.asarray(inputs[name], dtype=np.float32).reshape(1, D))
        else:
            shared[name] = np.ascontiguousarray(np.asarray(inputs[name], dtype=np.float32))
    in_maps = []
    for c in range(NCORES):
        m = dict(shared)
        m["x"] = x[c * NB:(c + 1) * NB].reshape(NB * S, D)
        in_maps.append(m)
    res = run_bass_kernel_spmd(nc, in_maps, core_ids=list(range(NCORES)))
    out = np.concatenate([np.asarray(r["out"]).reshape(NB, S, D) for r in res.results], axis=0)
    return out.astype(np.float32)
```

```python
import numpy as np
import ml_dtypes
from contextlib import ExitStack
import concourse.bass as bass
import concourse.mybir as mybir
from concourse.bass_utils import run_bass_kernel_spmd

F32 = mybir.dt.float32
BF16 = mybir.dt.bfloat16
I32 = mybir.dt.int32
U32 = mybir.dt.uint32
ALU = mybir.AluOpType
AF = mybir.ActivationFunctionType
AX = mybir.AxisListType

D = 1024
IN_COLS = 5376
SHIFT_COLS = 1792
NCORES = 8


class Dep:
    __slots__ = ("w", "r", "pw", "pr")

    def __init__(self):
        self.w = {}
        self.r = {}
        self.pw = {}
        self.pr = {}


class Stream:
    def __init__(self, K, name, is_pe=False, ndma=0):
        self.K = K
        self.name = name
        self.sem = K.new_sem("s_" + name)
        self.cnt = 0
        self.items = []
        self.waited = {}
        self.is_pe = is_pe
        self.dsems = [K.new_sem("d_%s%d" % (name, i)) for i in range(ndma)]
        self.duses = [0] * ndma
        self.dj = 0

    def wait_tok(self, tok):
        if tok is None:
            return
        sem, val = tok
        if sem is self.sem and self.is_pe:
            return
        key = id(sem)
        if self.waited.get(key, 0) >= val:
            return
        self.waited[key] = val
        self.items.append(("w", sem, val, self.K.phase))

    def _pre(self, reads, writes, adds):
        for d in reads:
            for t in list(d.w.values()):
                self.wait_tok(t)
        for d in writes:
            for t in list(d.w.values()):
                self.wait_tok(t)
            for t in list(d.r.values()):
                self.wait_tok(t)
        for d in adds:
            for t in list(d.r.values()) + list(d.pr.values()) + list(d.pw.values()):
                self.wait_tok(t)

    def _post(self, tok, reads, writes, adds):
        for d in reads:
            d.r[id(tok[0])] = tok
        for d in writes:
            d.pw = d.w
            d.pr = d.r
            d.w = {id(tok[0]): tok}
            d.r = {}
        for d in adds:
            d.w[id(tok[0])] = tok

    def op(self, fn, reads=(), writes=(), adds=()):
        self._pre(reads, writes, adds)
        self.cnt += 1
        tok = (self.sem, self.cnt)
        self.items.append(("o", fn, self.sem, 1, self.K.phase))
        self._post(tok, reads, writes, adds)
        return tok

    def dma(self, fn, reads=(), writes=(), adds=()):
        self._pre(reads, writes, adds)
        n = len(self.dsems)
        slot = self.dj % n
        self.dj += 1
        if self.duses[slot] > 0:
            self.wait_tok((self.dsems[slot], 16 * self.duses[slot]))
        self.duses[slot] += 1
        tok = (self.dsems[slot], 16 * self.duses[slot])
        self.items.append(("o", fn, self.dsems[slot], 16, self.K.phase))
        self._post(tok, reads, writes, adds)
        return tok

    def replay(self, eng):
        nc = self.K.nc
        cur = None
        ctx = None
        for it in self.items:
            ph = it[-1]
            if self.K.scopes and ph != cur:
                if ctx is not None:
                    ctx.__exit__(None, None, None)
                ctx = nc.named_scope(ph)
                ctx.__enter__()
                cur = ph
            if it[0] == "w":
                eng.wait_ge(it[1], it[2])
            else:
                ins = it[1](eng)
                ins.then_inc(it[2], it[3])
        if ctx is not None:
            ctx.__exit__(None, None, None)


class Kern:
    def __init__(self, nc, es, pool_slots=8):
        self.nc = nc
        self.es = es
        self.nsem = 0
        self.phase = "init"
        self.scopes = False
        self.pe = Stream(self, "pe", is_pe=True)
        self.act = Stream(self, "act", ndma=4)
        self.dve = Stream(self, "dve")
        self.pool = Stream(self, "pool", ndma=pool_slots)
        self.sp = Stream(self, "sp", ndma=8)
        self.uid = 0

    def new_sem(self, name):
        self.nsem += 1
        return self.es.enter_context(self.nc.semaphore(name))

    def sb(self, shape, dt, name=None, es=None):
        self.uid += 1
        nm = "%s_%d" % (name or "t", self.uid)
        return (es or self.es).enter_context(self.nc.sbuf_tensor(nm, list(shape), dt))

    def ps(self, shape, dt, name=None, es=None):
        self.uid += 1
        nm = "%s_%d" % (name or "p", self.uid)
        return (es or self.es).enter_context(self.nc.psum_tensor(nm, list(shape), dt))

    def dram(self, name, shape, dt, kind="Internal"):
        return self.nc.dram_tensor(name, list(shape), dt, kind=kind)

    def streams(self):
        return [self.pe, self.act, self.dve, self.pool, self.sp]

    def barrier(self):
        st = self.streams()
        toks = []
        for q in st:
            if q.cnt > 0:
                toks.append((q.sem, q.cnt))
            for i, sem in enumerate(q.dsems):
                if q.duses[i] > 0:
                    toks.append((sem, 16 * q.duses[i]))
        for s_ in st:
            for t in toks:
                s_.wait_tok(t)

    def finish(self):
        streams = [self.pe, self.act, self.dve, self.pool, self.sp]
        for s in streams:
            for q in streams:
                for i, sem in enumerate(q.dsems):
                    if q.duses[i] > 0:
                        s.wait_tok((sem, 16 * q.duses[i]))
        with self.nc.allow_non_contiguous_dma(reason="small strided param loads"), self.nc.Block() as block:
            @block.tensor
            def _(e):
                self.pe.replay(e)

            @block.scalar
            def _(e):
                self.act.replay(e)

            @block.vector
            def _(e):
                self.dve.replay(e)

            @block.gpsimd
            def _(e):
                self.pool.replay(e)

            @block.sync
            def _(e):
                self.sp.replay(e)


class Rot:
    def __init__(self, K, n, shape, dt, name, psum=False, es=None):
        self.t = [(K.ps if psum else K.sb)(shape, dt, name, es=es) for _ in range(n)]
        self.d = [Dep() for _ in range(n)]
        self.i = 0

    def next(self):
        j = self.i % len(self.t)
        self.i += 1
        return self.t[j], self.d[j]


def phase1(K, cfg, io, scr):
    nc = K.nc
    T = cfg["T"]
    NT = T // 512
    with ExitStack() as es:
        ident = K.sb([128, 128], BF16, "ident", es)
        d_ident = Dep()
        K.sp.dma(lambda e: e.dma_start(out=ident[:], in_=io["ident_bf"][:, :]), writes=[d_ident])
        nw = K.sb([128, 8], F32, "nw", es)
        d_nw = Dep()
        K.sp.dma(lambda e: e.dma_start(out=nw[:], in_=io["norm_mix_w"].rearrange("o (c p) -> p (o c)", p=128)),
                 writes=[d_nw])
        wt = K.sb([128, 8, IN_COLS], BF16, "wt", es)
        d_wt = Dep()
        wst = Rot(K, 2, [128, 1344], F32, "wst", es=es)
        q = 0
        for kc in range(8):
            for cp in range(4):
                st, dst = wst.next()
                eng = K.sp if q % 2 == 0 else K.pool
                q += 1
                eng.dma(lambda e, st=st, kc=kc, cp=cp: e.dma_start(
                    out=st[:], in_=io["w_in"][0, kc * 128:(kc + 1) * 128, cp * 1344:(cp + 1) * 1344]), writes=[dst])
                K.act.op(lambda e, st=st, kc=kc, cp=cp: e.activation(
                    out=wt[:, kc, cp * 1344:(cp + 1) * 1344], in_=st[:], func=AF.Copy, scale=nw[:, kc:kc + 1]),
                    reads=[dst, d_nw], writes=[d_wt])

        xs = Rot(K, 2, [128, D], F32, "xs", es=es)
        junk = K.sb([128, D], BF16, "junk", es)
        d_junk = Dep()
        xn = Rot(K, 2, [128, D], BF16, "xn", es=es)
        st4 = Rot(K, 4, [128, 4], F32, "st4", es=es)
        hT = Rot(K, 2, [128, 8, 512], BF16, "hT", es=es)
        ptr = Rot(K, 2, [128, 8, 128], BF16, "ptr", psum=True, es=es)
        pmm = Rot(K, 4, [128, 512], F32, "pmm", psum=True, es=es)
        o32 = Rot(K, 3, [128, 512], F32, "o32", es=es)
        o16 = Rot(K, 3, [128, 512], BF16, "o16", es=es)
        ev = [0]

        def evac(pt, pd, ncols, kind, dst_ap):
            if kind == "f32":
                ot, od = o32.next()
            else:
                ot, od = o16.next()
            use_act = (kind == "sig") or (ev[0] % 2 == 0)
            ev[0] += 1
            if kind == "sig":
                K.act.op(lambda e: e.activation(out=ot[:, :ncols], in_=pt[:, :ncols], func=AF.Sigmoid),
                         reads=[pd], writes=[od])
            elif use_act:
                K.act.op(lambda e: e.activation(out=ot[:, :ncols], in_=pt[:, :ncols], func=AF.Copy),
                         reads=[pd], writes=[od])
            else:
                K.dve.op(lambda e: e.tensor_copy(out=ot[:, :ncols], in_=pt[:, :ncols]), reads=[pd], writes=[od])
            K.sp.dma(lambda e: e.dma_start(out=dst_ap, in_=ot[:, :ncols]), reads=[od], adds=[scr["d_p1"]])

        for ti in range(NT):
            h_t, h_d = hT.next()
            for sub in range(4):
                t0 = ti * 512 + sub * 128
                x_t, x_d = xs.next()
                K.pool.dma(lambda e, x_t=x_t, t0=t0: e.dma_start(out=x_t[:], in_=io["x"][t0:t0 + 128, :]),
                           writes=[x_d])
                s_t, s_d = st4.next()
                K.dve.op(lambda e, s_t=s_t: e.memset(s_t[:], 0.0), writes=[s_d])
                K.act.op(lambda e, x_t=x_t, s_t=s_t: e.activation(out=junk[:], in_=x_t[:], func=AF.Square,
                                                                    accum_out=s_t[:, 0:1]),
                         reads=[x_d], writes=[d_junk, s_d])
                K.dve.op(lambda e, s_t=s_t: e.tensor_scalar(out=s_t[:, 1:2], in0=s_t[:, 0:1], scalar1=1.0 / D,
                                                            scalar2=1e-6, op0=ALU.mult, op1=ALU.add),
                         reads=[s_d], writes=[s_d])
                K.act.op(lambda e, s_t=s_t: e.activation(out=s_t[:, 3:4], in_=s_t[:, 1:2], func=AF.Sqrt),
                         reads=[s_d], writes=[s_d])
                K.dve.op(lambda e, s_t=s_t: e.reciprocal(out=s_t[:, 2:3], in_=s_t[:, 3:4]),
                         reads=[s_d], writes=[s_d])
                n_t, n_d = xn.next()
                K.dve.op(lambda e, x_t=x_t, s_t=s_t, n_t=n_t: e.tensor_scalar(
                    out=n_t[:], in0=x_t[:], scalar1=s_t[:, 2:3], scalar2=None, op0=ALU.mult),
                    reads=[x_d, s_d], writes=[n_d])
                p_t, p_d = ptr.next()
                for kc in range(8):
                    K.pe.op(lambda e, p_t=p_t, n_t=n_t, kc=kc: e.transpose(
                        out=p_t[:, kc, :], in_=n_t[:, kc * 128:(kc + 1) * 128], identity=ident[:]),
                        reads=[n_d, d_ident], writes=[p_d])
                K.act.op(lambda e, p_t=p_t, h_t=h_t, sub=sub: e.activation(
                    out=h_t[:, :, sub * 128:(sub + 1) * 128], in_=p_t[:], func=AF.Copy),
                    reads=[p_d], writes=[h_d])
            tsl = slice(ti * 512, (ti + 1) * 512)
            for sub in range(4):
                r0 = ti * 512 + sub * 128
                for (c0, ncols, kind, name, dc0) in [(0, 512, "f32", "zs_tm", 0), (512, 512, "f32", "zs_tm", 512),
                                                    (1024, 512, "f32", "zs_tm", 1024),
                                                    (1536, 256, "f32", "zs_tm", 1536),
                                                    (2816, 512, "bf16", "av_tm", 0)]:
                    pt, pd = pmm.next()
                    for kc in range(8):
                        K.pe.op(lambda e, pt=pt, kc=kc, sub=sub, c0=c0, ncols=ncols, h_t=h_t: e.matmul(
                            pt[:, :ncols], h_t[:, kc, sub * 128:(sub + 1) * 128], wt[:, kc, c0:c0 + ncols],
                            start=(kc == 0), stop=(kc == 7)), reads=[h_d, d_wt], writes=[pd])
                    evac(pt, pd, ncols, kind, scr[name][r0:r0 + 128, dc0:dc0 + ncols])
            fm = []
            for j in range(8):
                fm.append((1792 + j * 128, "bf16", "qk_fm", j * 128))
            for j in range(16):
                fm.append((3328 + j * 128, "sig", "sg_fm", j * 128))
            for (c0, kind, name, r0) in fm:
                pt, pd = pmm.next()
                for kc in range(8):
                    K.pe.op(lambda e, pt=pt, kc=kc, c0=c0, h_t=h_t: e.matmul(
                        pt[:, :], wt[:, kc, c0:c0 + 128], h_t[:, kc, :], start=(kc == 0), stop=(kc == 7)),
                        reads=[h_d, d_wt], writes=[pd])
                evac(pt, pd, 512, kind, scr[name][r0:r0 + 128, tsl])


def dap(apobj, offset, dims):
    return bass.AP(tensor=apobj.tensor, offset=offset, ap=[list(d) for d in dims])


def bcast_load(K, eng, dst, src_row_ap, n, dep):
    eng.dma(lambda e: e.dma_start(out=dst, in_=src_row_ap.broadcast_to([128, n])), writes=[dep])


def phase2_prep(K, cfg, io, scr):
    T, S, NB = cfg["T"], cfg["S"], cfg["NB"]
    NTT = T // 128
    with ExitStack() as es:
        identb = K.sb([128, 128], BF16, "identb", es)
        identf = K.sb([128, 128], F32, "identf", es)
        d_c = Dep()
        K.sp.dma(lambda e: e.dma_start(out=identb[:], in_=io["ident_bf"][:, :]), adds=[d_c])
        K.sp.dma(lambda e: e.dma_start(out=identf[:], in_=io["ident_f"][:, :]), adds=[d_c])
        MU = K.sb([128, SHIFT_COLS], F32, "MU", es)
        PR = K.sb([128, 5, 512], F32, "PR", es)
        K.sp.dma(lambda e: e.dma_start(out=MU[:], in_=io["shift_mu"][0:1, :].broadcast_to([128, SHIFT_COLS])), adds=[d_c])
        for j, nm in enumerate(["w0", "a0", "k_k", "k_a"]):
            K.pool.dma(lambda e, j=j, nm=nm: e.dma_start(out=PR[:, j, :], in_=io[nm][0:1, :].broadcast_to([128, 512])),
                       adds=[d_c])
        K.pool.dma(lambda e: e.dma_start(out=PR[:, 4, :], in_=io["r_k"].rearrange("o h k -> o (h k)").broadcast_to([128, 512])),
                   adds=[d_c])
        cst = K.sb([128, 2], F32, "cst", es)
        K.dve.op(lambda e: e.memset(cst[:, 0:1], 1.0), adds=[d_c])
        K.dve.op(lambda e: e.memset(cst[:, 1:2], -0.5), adds=[d_c])
        wst = K.sb([128, 3, 512], F32, "lwst", es)
        d_wst = Dep()
        K.dve.op(lambda e: e.memset(wst[:], 0.0), writes=[d_wst])
        K.sp.dma(lambda e: e.dma_start(out=wst[0:64, 0, :], in_=io["w2"][0, :, :]), reads=[d_wst], adds=[d_wst])
        K.sp.dma(lambda e: e.dma_start(out=wst[64:128, 1, :], in_=io["a2"][0, :, :]), reads=[d_wst], adds=[d_wst])
        K.sp.dma(lambda e: e.dma_start(out=wst[:, 2, :], in_=io["g2"][0, :, :]), reads=[d_wst], adds=[d_wst])
        LW = K.sb([128, 3, 512], BF16, "LW", es)
        K.dve.op(lambda e: e.tensor_copy(out=LW[:], in_=wst[:]), reads=[d_wst], adds=[d_c])

        Zr = Rot(K, 2, [128, SHIFT_COLS], F32, "Z", es=es)
        Zpr = Rot(K, 2, [128, SHIFT_COLS], F32, "Zp", es=es)
        ZSr = Rot(K, 2, [128, SHIFT_COLS], F32, "ZS", es=es)
        OUTr = Rot(K, 2, [128, 5, 512], F32, "OUT", es=es)
        Er = Rot(K, 2, [128, 192], F32, "E", es=es)
        Lr = Rot(K, 2, [128, 256], BF16, "L", es=es)
        LTr = Rot(K, 2, [128, 2, 128], BF16, "LT", es=es)
        Ur = Rot(K, 2, [128, 512], F32, "U", es=es)
        UAr = Rot(K, 2, [128, 512], F32, "UA", es=es)
        KKr = Rot(K, 2, [128, 512], F32, "KKt", es=es)
        SQr = Rot(K, 2, [128, 512], F32, "SQ", es=es)
        T1r = Rot(K, 2, [128, 512], F32, "T1", es=es)
        T2r = Rot(K, 2, [128, 512], F32, "T2", es=es)
        S8r = Rot(K, 2, [128, 4, 8], F32, "S8", es=es)
        VTr = Rot(K, 2, [128, 4, 128], F32, "VT", es=es)
        GTr = Rot(K, 2, [128, 4, 128], F32, "GT", es=es)
        CTr = Rot(K, 2, [8, 128], F32, "CT", es=es)
        PT = Rot(K, 1, [128, 2, 128], BF16, "PT", psum=True, es=es)
        PW = Rot(K, 1, [128, 512], F32, "PW", psum=True, es=es)
        PA = Rot(K, 1, [128, 512], F32, "PA", psum=True, es=es)
        PG = Rot(K, 1, [128, 4, 128], F32, "PG", psum=True, es=es)
        PV = Rot(K, 1, [128, 4, 128], F32, "PV", psum=True, es=es)
        PC = Rot(K, 1, [8, 128], F32, "PC", psum=True, es=es)
        chunked = cfg.get("chunked", True)
        if chunked:
            PL = Rot(K, 1, [128, 512], F32, "PL", psum=True, es=es)
            TRI = K.sb([128, 128], F32, "TRI", es)
            K.sp.dma(lambda e: e.dma_start(out=TRI[:], in_=io["tri"][:, :]), adds=[d_c])
            LWr = Rot(K, 2, [128, 512], F32, "LWt", es=es)
            ELr = Rot(K, 2, [128, 3, 512], F32, "EL", es=es)
            ABr = Rot(K, 2, [128, 5, 512], BF16, "AB", es=es)
        dq = [0]

        def ldq():
            dq[0] += 1
            return K.sp if dq[0] % 2 == 0 else K.pool

        def tile_gen(ti):
            t0 = ti * 128
            first = (t0 % S == 0)
            Z, dZ = Zr.next()
            Zp, dZp = Zpr.next()
            ZS, dZS = ZSr.next()
            OUT, dO = OUTr.next()
            ldq().dma(lambda e, Z=Z, t0=t0: e.dma_start(out=Z[:], in_=scr["zs_tm"][t0:t0 + 128, :]),
                      reads=[scr["d_p1"]], writes=[dZ])
            if first:
                K.pool.op(lambda e, Zp=Zp: e.memset(Zp[0:32, :], 0.0), writes=[dZp])
                ldq().dma(lambda e, Zp=Zp, t0=t0: e.dma_start(out=Zp[1:128, :], in_=scr["zs_tm"][t0:t0 + 127, :]),
                          reads=[scr["d_p1"], dZp], adds=[dZp])
            else:
                ldq().dma(lambda e, Zp=Zp, t0=t0: e.dma_start(out=Zp[:], in_=scr["zs_tm"][t0 - 1:t0 + 127, :]),
                          reads=[scr["d_p1"]], writes=[dZp])
            CS = 1216
            dZSa, dZSb = Dep(), Dep()
            K.dve.op(lambda e, Z=Z, Zp=Zp, ZS=ZS: e.tensor_tensor(out=ZS[:, :CS], in0=Zp[:, :CS], in1=Z[:, :CS], op=ALU.subtract),
                     reads=[dZ, dZp], writes=[dZS])
            K.pool.op(lambda e, Z=Z, Zp=Zp, ZS=ZS: e.tensor_tensor(out=ZS[:, CS:], in0=Zp[:, CS:], in1=Z[:, CS:], op=ALU.subtract),
                      reads=[dZ, dZp, dZS], writes=[dZSb])
            K.dve.op(lambda e, ZS=ZS: e.tensor_tensor(out=ZS[:, :CS], in0=ZS[:, :CS], in1=MU[:, :CS], op=ALU.mult),
                     reads=[d_c, dZS], writes=[dZSa])
            K.pool.op(lambda e, ZS=ZS: e.tensor_tensor(out=ZS[:, CS:], in0=ZS[:, CS:], in1=MU[:, CS:], op=ALU.mult),
                      reads=[d_c], writes=[dZSb])
            K.dve.op(lambda e, Z=Z, ZS=ZS: e.tensor_tensor(out=ZS[:, :CS], in0=ZS[:, :CS], in1=Z[:, :CS], op=ALU.add),
                     reads=[dZ], writes=[dZSa])
            K.pool.op(lambda e, Z=Z, ZS=ZS: e.tensor_tensor(out=ZS[:, CS:], in0=ZS[:, CS:], in1=Z[:, CS:], op=ALU.add),
                      reads=[dZ], writes=[dZSb])
            K.dve.op(lambda e, ZS=ZS: e.tensor_copy(out=ZS[:, 0:1], in_=ZS[:, 0:1]), reads=[dZSa, dZSb], writes=[dZS])
            yield
            r_ap = ZS[:, 0:512]
            k_ap = ZS[:, 512:1024]
            K.act.op(lambda e, OUT=OUT, ZS=ZS: e.activation(out=OUT[:, 4, :], in_=ZS[:, 0:512], func=AF.Copy),
                     reads=[dZS], writes=[dO])
            yield
            E, dE = Er.next()
            L, dL = Lr.next()
            K.act.op(lambda e, E=E, ZS=ZS: e.activation(out=E[:, 0:64], in_=ZS[:, 1536:1600], func=AF.Exp, scale=-2.0),
                     reads=[dZS], writes=[dE])
            K.act.op(lambda e, E=E, ZS=ZS: e.activation(out=E[:, 64:192], in_=ZS[:, 1664:1792], func=AF.Exp, scale=-1.0),
                     reads=[dZS], adds=[dE])
            K.act.op(lambda e, E=E: e.activation(out=E[:], in_=E[:], func=AF.Ln, bias=cst[:, 0:1]), reads=[d_c], writes=[dE])
            K.act.op(lambda e, E=E: e.activation(out=E[:], in_=E[:], func=AF.Exp, scale=-1.0), writes=[dE])
            yield
            K.dve.op(lambda e, E=E, L=L: e.tensor_scalar(out=L[:, 0:64], in0=E[:, 0:64], scalar1=2.0, scalar2=-1.0,
                                                        op0=ALU.mult, op1=ALU.add), reads=[dE], writes=[dL])
            K.act.op(lambda e, L=L, ZS=ZS: e.activation(out=L[:, 64:128], in_=ZS[:, 1600:1664], func=AF.Copy),
                     reads=[dZS, dL], adds=[dL])
            K.act.op(lambda e, L=L, E=E: e.activation(out=L[:, 128:256], in_=E[:, 64:192], func=AF.Copy),
                     reads=[dE, dL], adds=[dL])
            yield
            pt, dpt = PT.next()
            K.pe.op(lambda e, pt=pt, L=L: e.transpose(out=pt[:, 0, :], in_=L[:, 0:128], identity=identb[:]),
                    reads=[dL, d_c], writes=[dpt])
            K.pe.op(lambda e, pt=pt, L=L: e.transpose(out=pt[:, 1, :], in_=L[:, 128:256], identity=identb[:]),
                    reads=[dL, d_c], adds=[dpt])
            LT, dLT = LTr.next()
            K.act.op(lambda e, pt=pt, LT=LT: e.activation(out=LT[:], in_=pt[:], func=AF.Copy), reads=[dpt], writes=[dLT])
            yield "pre_pw"
            pw, dpw = PW.next()
            pa, dpa = PA.next()
            pg, dpg = PG.next()
            K.pe.op(lambda e, pw=pw, LT=LT: e.matmul(pw[:], LT[:, 0, :], LW[:, 0, :], start=True, stop=True),
                    reads=[dLT, d_c], writes=[dpw])
            K.pe.op(lambda e, pa=pa, LT=LT: e.matmul(pa[:], LT[:, 0, :], LW[:, 1, :], start=True, stop=True),
                    reads=[dLT, d_c], writes=[dpa])
            for j in range(4):
                K.pe.op(lambda e, pg=pg, LT=LT, j=j: e.matmul(pg[:, j, :], LW[:, 2, j * 128:(j + 1) * 128], LT[:, 1, :],
                                                             start=True, stop=True),
                        reads=[dLT, d_c], writes=[dpg] if j == 0 else [], adds=[] if j == 0 else [dpg])
            GT, dGT = GTr.next()
            K.act.op(lambda e, pg=pg, GT=GT: e.activation(out=GT[:], in_=pg[:], func=AF.Copy), reads=[dpg], writes=[dGT])
            K.sp.dma(lambda e, GT=GT, t0=t0: e.dma_start(
                out=dap(scr["g_fm"], t0, [[T, 128], [128 * T, 4], [1, 128]]), in_=GT[:]),
                reads=[dGT], adds=[scr["d_p2a"]])
            yield
            U, dU = Ur.next()
            K.dve.op(lambda e, U=U, pw=pw: e.tensor_tensor(out=U[:], in0=pw[:], in1=PR[:, 0, :], op=ALU.add),
                     reads=[dpw, d_c], writes=[dU])
            yield
            K.act.op(lambda e, U=U: e.activation(out=U[:], in_=U[:], func=AF.Exp, scale=-1.0), writes=[dU])
            K.act.op(lambda e, U=U: e.activation(out=U[:], in_=U[:], func=AF.Ln, bias=cst[:, 0:1]), reads=[d_c], writes=[dU])
            yield
            K.act.op(lambda e, U=U: e.activation(out=U[:], in_=U[:], func=AF.Exp, scale=-1.0, bias=cst[:, 1:2]),
                     reads=[d_c], writes=[dU])
            K.act.op(lambda e, U=U, OUT=OUT: e.activation(out=OUT[:, 0, :], in_=U[:], func=AF.Exp, scale=-1.0),
                     reads=[dU], adds=[dO])
            yield
            UA, dUA = UAr.next()
            K.dve.op(lambda e, UA=UA, pa=pa: e.tensor_tensor(out=UA[:], in0=pa[:], in1=PR[:, 1, :], op=ALU.add),
                     reads=[dpa, d_c], writes=[dUA])
            yield
            K.act.op(lambda e, UA=UA: e.activation(out=UA[:], in_=UA[:], func=AF.Exp, scale=-1.0), writes=[dUA])
            K.act.op(lambda e, UA=UA: e.activation(out=UA[:], in_=UA[:], func=AF.Ln, bias=cst[:, 0:1]), reads=[d_c], writes=[dUA])
            K.act.op(lambda e, UA=UA: e.activation(out=UA[:], in_=UA[:], func=AF.Exp, scale=-1.0), writes=[dUA])
            yield "post_a"
            KKt, dKK = KKr.next()
            SQ, dSQ = SQr.next()
            S8, dS8 = S8r.next()
            K.dve.op(lambda e, KKt=KKt, ZS=ZS: e.tensor_tensor(out=KKt[:], in0=ZS[:, 512:1024], in1=PR[:, 2, :], op=ALU.mult),
                     reads=[dZS, d_c], writes=[dKK])
            K.pool.op(lambda e, KKt=KKt, SQ=SQ: e.tensor_tensor(out=SQ[:], in0=KKt[:], in1=KKt[:], op=ALU.mult),
                      reads=[dKK], writes=[dSQ])
            yield
            K.dve.op(lambda e, SQ=SQ, S8=S8: e.tensor_reduce(out=S8[:, 0, :], in_=SQ[:].rearrange("p (h k) -> p h k", k=64),
                                                            axis=AX.X, op=ALU.add), reads=[dSQ], writes=[dS8])
            K.dve.op(lambda e, S8=S8: e.tensor_scalar(out=S8[:, 0, :], in0=S8[:, 0, :], scalar1=1e-24, scalar2=None,
                                                      op0=ALU.max), writes=[dS8])
            K.act.op(lambda e, S8=S8: e.activation(out=S8[:, 1, :], in_=S8[:, 0, :], func=AF.Ln), writes=[dS8])
            K.act.op(lambda e, S8=S8: e.activation(out=S8[:, 2, :], in_=S8[:, 1, :], func=AF.Exp, scale=-0.5), writes=[dS8])
            yield
            K.dve.op(lambda e, KKt=KKt, S8=S8, OUT=OUT: e.tensor_tensor(
                out=OUT[:, 1, :].rearrange("p (h k) -> p h k", k=64), in0=KKt[:].rearrange("p (h k) -> p h k", k=64),
                in1=S8[:, 2, :].unsqueeze(2).broadcast_to([128, 8, 64]), op=ALU.mult),
                reads=[dKK, dS8, dO], adds=[dO])
            K.dve.op(lambda e, OUT=OUT, UA=UA: e.scalar_tensor_tensor(
                out=OUT[:, 2, :], in0=OUT[:, 1, :], scalar=-1.0, in1=UA[:], op0=ALU.mult, op1=ALU.mult),
                reads=[dUA, dO], adds=[dO])
            yield
            T1, dT1 = T1r.next()
            K.dve.op(lambda e, T1=T1, UA=UA: e.scalar_tensor_tensor(
                out=T1[:], in0=UA[:], scalar=-1.0, in1=PR[:, 3, :], op0=ALU.add, op1=ALU.mult),
                reads=[dUA, d_c], writes=[dT1])
            K.dve.op(lambda e, T1=T1, OUT=OUT, ZS=ZS: e.scalar_tensor_tensor(
                out=OUT[:, 3, :], in0=T1[:], scalar=1.0, in1=ZS[:, 512:1024], op0=ALU.add, op1=ALU.mult),
                reads=[dT1, dZS, dO], adds=[dO])
            T2, dT2 = T2r.next()
            K.pool.op(lambda e, T2=T2, OUT=OUT, ZS=ZS: e.tensor_tensor(out=T2[:], in0=OUT[:, 3, :], in1=ZS[:, 0:512], op=ALU.mult),
                      reads=[dO, dZS], writes=[dT2])
            K.pool.op(lambda e, T2=T2: e.tensor_tensor(out=T2[:], in0=T2[:], in1=PR[:, 4, :], op=ALU.mult),
                      reads=[d_c], writes=[dT2])
            yield
            K.dve.op(lambda e, T2=T2, S8=S8: e.tensor_reduce(out=S8[:, 3, :], in_=T2[:].rearrange("p (h k) -> p h k", k=64),
                                                            axis=AX.X, op=ALU.add), reads=[dT2], writes=[dS8])
            yield
            pv, dpv = PV.next()
            pc, dpc = PC.next()
            for j in range(4):
                K.pe.op(lambda e, pv=pv, ZS=ZS, j=j: e.transpose(out=pv[:, j, :], in_=ZS[:, 1024 + j * 128:1024 + (j + 1) * 128],
                                                                identity=identf[:]),
                        reads=[dZS, d_c], writes=[dpv] if j == 0 else [], adds=[] if j == 0 else [dpv])
            K.pe.op(lambda e, pc=pc, S8=S8: e.transpose(out=pc[:, :], in_=S8[:, 3, :], identity=identf[:]),
                    reads=[dS8, d_c], writes=[dpc])
            VT, dVT = VTr.next()
            CT, dCT = CTr.next()
            K.act.op(lambda e, pv=pv, VT=VT: e.activation(out=VT[:], in_=pv[:], func=AF.Copy), reads=[dpv], writes=[dVT])
            K.act.op(lambda e, pc=pc, CT=CT: e.activation(out=CT[:], in_=pc[:], func=AF.Copy), reads=[dpc], writes=[dCT])
            K.sp.dma(lambda e, VT=VT, t0=t0: e.dma_start(
                out=dap(scr["v_fm"], t0, [[T, 128], [128 * T, 4], [1, 128]]), in_=VT[:]),
                reads=[dVT], adds=[scr["d_p2a"]])
            K.sp.dma(lambda e, CT=CT, t0=t0: e.dma_start(out=scr["coef_fm"][:, t0:t0 + 128], in_=CT[:]),
                     reads=[dCT], adds=[scr["d_p2a"]])
            yield
            if not chunked:
                K.pool.dma(lambda e, OUT=OUT, t0=t0: e.dma_start(out=scr["rw_tm"][t0:t0 + 128, :, :], in_=OUT[:]),
                           reads=[dO], adds=[scr["d_p2a"]])
            else:
                LWt, dLW = LWr.next()
                K.dve.op(lambda e, LWt=LWt, U=U: e.tensor_scalar(out=LWt[:], in0=U[:], scalar1=-1.0, scalar2=None, op0=ALU.mult),
                         reads=[dU], writes=[dLW])
                pl, dpl = PL.next()
                K.pe.op(lambda e, pl=pl, LWt=LWt: e.matmul(pl[:], TRI[:], LWt[:], start=True, stop=True),
                        reads=[dLW, d_c], writes=[dpl])
                EL, dEL = ELr.next()
                K.act.op(lambda e, EL=EL, pl=pl: e.activation(out=EL[:, 0, :], in_=pl[:], func=AF.Exp), reads=[dpl], writes=[dEL])
                K.act.op(lambda e, EL=EL, pl=pl: e.activation(out=EL[:, 1, :], in_=pl[:], func=AF.Exp, scale=-1.0),
                         reads=[dpl], adds=[dEL])
                K.dve.op(lambda e, EL=EL, pl=pl, U=U: e.tensor_tensor(out=EL[:, 2, :], in0=pl[:], in1=U[:], op=ALU.add),
                         reads=[dpl, dU, dEL], adds=[dEL])
                K.act.op(lambda e, EL=EL: e.activation(out=EL[:, 2, :], in_=EL[:, 2, :], func=AF.Exp), reads=[dEL], adds=[dEL])
                AB, dAB = ABr.next()
                K.dve.op(lambda e, AB=AB, OUT=OUT, EL=EL: e.tensor_tensor(out=AB[:, 0, :], in0=OUT[:, 1, :], in1=EL[:, 2, :], op=ALU.mult),
                         reads=[dO, dEL], writes=[dAB])
                K.dve.op(lambda e, AB=AB, OUT=OUT, EL=EL: e.scalar_tensor_tensor(
                    out=AB[:, 1, :], in0=OUT[:, 2, :], scalar=-1.0, in1=EL[:, 1, :], op0=ALU.mult, op1=ALU.mult),
                    reads=[dO, dEL, dAB], adds=[dAB])
                K.pool.op(lambda e, AB=AB, OUT=OUT, EL=EL: e.tensor_tensor(out=AB[:, 2, :], in0=OUT[:, 3, :], in1=EL[:, 1, :], op=ALU.mult),
                          reads=[dO, dEL, dAB], adds=[dAB])
                K.pool.op(lambda e, AB=AB, OUT=OUT, EL=EL: e.tensor_tensor(out=AB[:, 3, :], in0=OUT[:, 4, :], in1=EL[:, 0, :], op=ALU.mult),
                          reads=[dO, dEL, dAB], adds=[dAB])
                K.act.op(lambda e, AB=AB, ZS=ZS: e.activation(out=AB[:, 4, :], in_=ZS[:, 1024:1536], func=AF.Copy),
                         reads=[dZS, dAB], adds=[dAB])
                K.pool.dma(lambda e, AB=AB, t0=t0: e.dma_start(out=scr["ab_tm"][t0:t0 + 128, :, :], in_=AB[:]),
                           reads=[dAB], adds=[scr["d_p2a"]])
                K.sp.dma(lambda e, LWt=LWt, t0=t0: e.dma_start(out=scr["lw_tm"][t0:t0 + 128, :], in_=LWt[:]),
                         reads=[dLW], adds=[scr["d_p2a"]])

        active = []
        nxt = 0
        while nxt < NTT or active:
            if len(active) < 2 and nxt < NTT and (not active or active[0][2] or active[0][3] >= 2):
                active.append([tile_gen(nxt), None, False, 0])
                nxt += 1
            for idx, a_ in enumerate(list(active)):
                if a_[1] == "pre_pw" and idx > 0 and not active[0][2]:
                    continue
                try:
                    tok = next(a_[0])
                    a_[1] = tok
                    a_[3] += 1
                    if tok == "post_a":
                        a_[2] = True
                except StopIteration:
                    active.remove(a_)


def phase2_scan(K, cfg, io, scr):
    T, S, NB = cfg["T"], cfg["S"], cfg["NB"]
    NBH = 2 if NB >= 2 else 1
    NBL = NB // NBH
    NP = 64 * NBH
    TS = 2
    TC = 128
    RW = 2560
    with ExitStack() as es:
        St = K.sb([128, NBL, 8, 64], F32, "St", es)
        dS = Dep()
        TMP = K.sb([128, NBL, 8, 64], F32, "TMP", es)
        dT = Dep()
        SA = K.sb([128, NBL, 8], F32, "SA", es)
        dSA = Dep()
        T2r = Rot(K, 2, [128, NBL, 8, 64], F32, "TMP2", es=es)
        T3r = Rot(K, 2, [128, NBL, 8, 64], F32, "TMP3", es=es)
        BCr = Rot(K, 3, [128, TS, NBL, 5, 8, 64], F32, "BC", es=es)
        Vr = Rot(K, 2, [128, NBL, 8, TC], F32, "Vf", es=es)
        Yr = Rot(K, 2, [128, NBL, 8, TC], F32, "Yf", es=es)
        K.dve.op(lambda e: e.memset(St[:], 0.0), writes=[dS])
        qi = [0]

        def q():
            qi[0] += 1
            return K.sp if qi[0] % 2 == 0 else K.act

        def load_bc(ci):
            t = ci * TS
            BC, dBC = BCr.next()
            first = True
            for bhi in range(NBH):
                for blo in range(NBL):
                    src = dap(scr["rw_tm"], ((bhi * NBL + blo) * S + t) * RW, [[0, 64], [RW, TS], [1, RW]])
                    dst = BC[bhi * 64:(bhi + 1) * 64, :, blo].rearrange("p t j h k -> p t (j h k)")
                    q().dma(lambda e, src=src, dst=dst: e.dma_start(out=dst, in_=src), reads=[scr["d_p2a"]],
                            writes=[dBC] if first else [], adds=[] if first else [dBC])
                    first = False
            return BC, dBC

        def vy_ap(name, bhi, blo, t):
            return dap(scr[name], (bhi * NBL + blo) * S + t, [[T, 64], [64 * T, 8], [1, TC]])

        def load_v(ni):
            Vf, dV = Vr.next()
            first = True
            for bhi in range(NBH):
                for blo in range(NBL):
                    src = vy_ap("v_fm", bhi, blo, ni * TC)
                    dst = Vf[bhi * 64:(bhi + 1) * 64, blo]
                    q().dma(lambda e, src=src, dst=dst: e.dma_start(out=dst, in_=src), reads=[scr["d_p2a"]],
                            writes=[dV] if first else [], adds=[] if first else [dV])
                    first = False
            return Vf, dV

        nch = S // TS
        bcs = {}
        bcs[0] = load_bc(0)
        if nch > 1:
            bcs[1] = load_bc(1)
        vs = {0: load_v(0)}
        P = slice(0, NP)
        for t in range(S):
            ci, ts = divmod(t, TS)
            ni, tt = divmod(t, TC)
            if ts == 0 and ci + 2 < nch:
                bcs[ci + 2] = load_bc(ci + 2)
            if tt == 0:
                if (ni + 1) * TC < S:
                    vs[ni + 1] = load_v(ni + 1)
                Yf, dY = Yr.next()
            BC, dBC = bcs[ci]
            Vf, dV = vs[ni]
            W_ = BC[P, ts, :, 0]
            KN = BC[P, ts, :, 1]
            KA = BC[P, ts, :, 2]
            KP = BC[P, ts, :, 3]
            R_ = BC[P, ts, :, 4]
            shp = [NP, NBL, 8, 64]
            K.dve.op(lambda e, KN=KN: e.tensor_tensor(out=TMP[P], in0=St[P], in1=KN, op=ALU.mult),
                     reads=[dS, dBC], writes=[dT])
            K.dve.op(lambda e: e.tensor_reduce(out=SA[P], in_=TMP[P], axis=AX.X, op=ALU.add), reads=[dT], writes=[dSA])
            K.dve.op(lambda e, W_=W_: e.tensor_tensor(out=St[P], in0=St[P], in1=W_, op=ALU.mult),
                     reads=[dBC], writes=[dS])
            K.dve.op(lambda e, KA=KA: e.tensor_tensor(out=TMP[P], in0=KA, in1=SA[P].unsqueeze(3).broadcast_to(shp),
                                                     op=ALU.mult), reads=[dBC, dSA], writes=[dT])
            K.dve.op(lambda e: e.tensor_tensor(out=St[P], in0=St[P], in1=TMP[P], op=ALU.add), reads=[dT], writes=[dS])
            T2, dT2 = T2r.next()
            K.pool.op(lambda e, KP=KP, T2=T2, Vf=Vf, tt=tt: e.tensor_tensor(
                out=T2[P], in0=KP, in1=Vf[P, :, :, tt:tt + 1].broadcast_to(shp), op=ALU.mult),
                reads=[dBC, dV], writes=[dT2])
            K.dve.op(lambda e, T2=T2: e.tensor_tensor(out=St[P], in0=St[P], in1=T2[P], op=ALU.add),
                     reads=[dT2], writes=[dS])
            T3, dT3 = T3r.next()
            K.pool.op(lambda e, T3=T3, R_=R_: e.tensor_tensor(out=T3[P], in0=St[P], in1=R_, op=ALU.mult),
                      reads=[dS, dBC], writes=[dT3])
            K.dve.op(lambda e, T3=T3, Yf=Yf, tt=tt: e.tensor_reduce(out=Yf[P, :, :, tt], in_=T3[P], axis=AX.X, op=ALU.add),
                      reads=[dT3], writes=[dY] if tt == 0 else [], adds=[] if tt == 0 else [dY])
            if tt == TC - 1:
                for bhi in range(NBH):
                    for blo in range(NBL):
                        dst = vy_ap("y_fm", bhi, blo, ni * TC)
                        srcp = Yf[bhi * 64:(bhi + 1) * 64, blo]
                        K.sp.dma(lambda e, dst=dst, srcp=srcp: e.dma_start(out=dst, in_=srcp), reads=[dY],
                                 adds=[scr["d_p2b"]])


def phase2_chunk(K, cfg, io, scr):
    T, S, NB = cfg["T"], cfg["S"], cfg["NB"]
    C = 64
    NCH = S // C
    with ExitStack() as es:
        d_c = Dep()
        id64 = K.sb([64, 64], BF16, "c_id64", es)
        K.sp.dma(lambda e: e.dma_start(out=id64[:], in_=io["ident64"][:, :]), adds=[d_c])
        MK = K.sb([64, 3, 64], F32, "c_MK", es)
        K.sp.dma(lambda e: e.dma_start(out=MK[:], in_=io["masks"][:, :, :]), adds=[d_c])
        ONES = K.sb([64, 1], F32, "c_ones", es)
        K.sp.dma(lambda e: e.dma_start(out=ONES[:], in_=io["ones64"][:, :]), adds=[d_c])
        IDF = K.sb([64, 8, 64], F32, "c_IDF", es)
        K.sp.dma(lambda e: e.dma_start(out=IDF[:], in_=io["ident_f"][0:64, 0:64].unsqueeze(1).broadcast_to([64, 8, 64])), adds=[d_c])
        ST = [K.sb([64, 8, 64], F32, "c_S%d" % b, es) for b in range(NB)]
        STb = [K.sb([64, 8, 64], BF16, "c_Sb%d" % b, es) for b in range(NB)]
        dST = [Dep() for _ in range(NB)]
        dSTb = [Dep() for _ in range(NB)]
        for b in range(NB):
            K.dve.op(lambda e, b=b: e.memset(ST[b][:], 0.0), writes=[dST[b]])
            K.pool.op(lambda e, b=b: e.memset(STb[b][:], 0.0), writes=[dSTb[b]])
        TMr = Rot(K, 3, [64, 5, 512], BF16, "c_TM", es=es)
        LWr = Rot(K, 3, [64, 512], F32, "c_LW", es=es)
        FMr = Rot(K, 2, [64, 4, 8, 64], BF16, "c_FM", es=es)
        PCr = Rot(K, 2, [64, 8], F32, "c_PC", es=es)
        Nr = Rot(K, 3, [64, 8, 64], BF16, "c_N", es=es)
        NTr = Rot(K, 3, [64, 8, 64], BF16, "c_NT", es=es)
        MTr = Rot(K, 3, [64, 8, 64], BF16, "c_MT", es=es)
        MTfr = Rot(K, 2, [64, 8, 64], F32, "c_MTf", es=es)
        NAKr = Rot(K, 2, [64, 8, 64], BF16, "c_NAK", es=es)
        MRBr = Rot(K, 2, [64, 8, 64], BF16, "c_MRB", es=es)
        MRKr = Rot(K, 2, [64, 8, 64], BF16, "c_MRK", es=es)
        Xr = Rot(K, 2, [64, 8, 64], BF16, "c_X", es=es)
        NUr = Rot(K, 2, [64, 8, 64], BF16, "c_NU", es=es)
        Yr = Rot(K, 2, [64, 8, 64], F32, "c_Y", es=es)
        TSr = Rot(K, 2, [64, 8, 64], F32, "c_TS", es=es)
        PTf = Rot(K, 1, [64, 4, 8, 64], BF16, "c_PTf", psum=True, es=es)
        PA = Rot(K, 4, [64, 8, 64], F32, "c_PA", psum=True, es=es)
        PPC = Rot(K, 1, [64, 8], F32, "c_PPC", psum=True, es=es)
        ce = [0]

        def evac_copy(dst_ap, src_ap, reads, writes=(), adds=(), scale=None):
            ce[0] += 1
            if scale is not None or ce[0] % 2 == 0:
                if scale is None:
                    K.act.op(lambda e: e.activation(out=dst_ap, in_=src_ap, func=AF.Copy), reads=reads, writes=writes, adds=adds)
                else:
                    K.act.op(lambda e: e.activation(out=dst_ap, in_=src_ap, func=AF.Copy, scale=scale), reads=reads, writes=writes, adds=adds)
            else:
                K.dve.op(lambda e: e.tensor_copy(out=dst_ap, in_=src_ap), reads=reads, writes=writes, adds=adds)

        def mm8(pt, dpt, lhs_fn, rhs_fn, reads, first=True, last=True, wr=True):
            mmN(pt, dpt, [(lhs_fn, rhs_fn)], reads)

        def mmN(pt, dpt, terms, reads):
            n = len(terms)
            for h in range(8):
                for i, (lf, rf) in enumerate(terms):
                    K.pe.op(lambda e, h=h, lf=lf, rf=rf, i=i: e.matmul(pt[:, h, :], lf(h), rf(h), start=(i == 0), stop=(i == n - 1)),
                            reads=reads, writes=[dpt] if (h == 0 and i == 0) else [], adds=[] if (h == 0 and i == 0) else [dpt])

        q = [0]

        def dq():
            q[0] += 1
            return K.sp if q[0] % 2 == 0 else K.pool

        for ci in range(NCH):
            for b in range(NB):
                t0 = b * S + ci * C
                TM, dTM = TMr.next()
                LW, dLW = LWr.next()
                dq().dma(lambda e, TM=TM, t0=t0: e.dma_start(out=TM[:], in_=scr["ab_tm"][t0:t0 + C, :, :]),
                         reads=[scr["d_p2a"]], writes=[dTM])
                dq().dma(lambda e, LW=LW, t0=t0: e.dma_start(out=LW[:], in_=scr["lw_tm"][t0:t0 + C, :]),
                         reads=[scr["d_p2a"]], writes=[dLW])
                ptf, dptf = PTf.next()
                first = True
                for j in range(4):
                    for h in range(8):
                        K.pe.op(lambda e, ptf=ptf, TM=TM, j=j, h=h: e.transpose(
                            out=ptf[:, j, h, :], in_=TM[:, j, h * 64:(h + 1) * 64], identity=id64[:]),
                            reads=[dTM, d_c], writes=[dptf] if first else [], adds=[] if first else [dptf])
                        first = False
                FM, dFM = FMr.next()
                K.act.op(lambda e, FM=FM, ptf=ptf: e.activation(out=FM[:, 0:2], in_=ptf[:, 0:2], func=AF.Copy), reads=[dptf], writes=[dFM])
                K.dve.op(lambda e, FM=FM, ptf=ptf: e.tensor_copy(out=FM[:, 2:4], in_=ptf[:, 2:4]), reads=[dptf, dFM], adds=[dFM])
                Af = lambda h, FM=FM: FM[:, 0, h, :]
                Bf = lambda h, FM=FM: FM[:, 1, h, :]
                Kf = lambda h, FM=FM: FM[:, 2, h, :]
                Rf = lambda h, FM=FM: FM[:, 3, h, :]
                Vt = lambda h, TM=TM: TM[:, 4, h * 64:(h + 1) * 64]
                Bt = lambda h, TM=TM: TM[:, 1, h * 64:(h + 1) * 64]
                Kt = lambda h, TM=TM: TM[:, 2, h * 64:(h + 1) * 64]
                ppc, dppc = PPC.next()
                for h in range(8):
                    K.pe.op(lambda e, ppc=ppc, LW=LW, h=h: e.matmul(ppc[:, h:h + 1], LW[:, h * 64:(h + 1) * 64], ONES[:], start=True, stop=True),
                            reads=[dLW, d_c], writes=[dppc] if h == 0 else [], adds=[] if h == 0 else [dppc])
                PCt, dPC = PCr.next()
                K.act.op(lambda e, PCt=PCt, ppc=ppc: e.activation(out=PCt[:], in_=ppc[:], func=AF.Exp), reads=[dppc], writes=[dPC])
                mbc = lambda i: MK[:, i, :].unsqueeze(1).broadcast_to([64, 8, 64])
                pa, dpa = PA.next()
                mm8(pa, dpa, Af, Bf, [dFM])
                N0, dN0 = Nr.next()
                K.dve.op(lambda e, N0=N0, pa=pa: e.tensor_tensor(out=N0[:], in0=pa[:], in1=mbc(0), op=ALU.mult), reads=[dpa, d_c], writes=[dN0])
                pa, dpa = PA.next()
                mm8(pa, dpa, Bf, Af, [dFM])
                NT0, dNT0 = NTr.next()
                MTf, dMTf = MTfr.next()
                K.dve.op(lambda e, NT0=NT0, pa=pa: e.tensor_tensor(out=NT0[:], in0=pa[:], in1=mbc(1), op=ALU.mult), reads=[dpa, d_c], writes=[dNT0])
                K.pool.op(lambda e, MTf=MTf, NT0=NT0: e.tensor_tensor(out=MTf[:], in0=IDF[:], in1=NT0[:], op=ALU.subtract),
                          reads=[dNT0, d_c], writes=[dMTf])
                MT, dMT = MTr.next()
                K.act.op(lambda e, MT=MT, MTf=MTf: e.activation(out=MT[:], in_=MTf[:], func=AF.Copy), reads=[dMTf], writes=[dMT])
                pa, dpa = PA.next()
                mm8(pa, dpa, Kf, Af, [dFM])
                NAK, dNAK = NAKr.next()
                K.dve.op(lambda e, NAK=NAK, pa=pa: e.tensor_tensor(out=NAK[:], in0=pa[:], in1=mbc(1), op=ALU.mult), reads=[dpa, d_c], writes=[dNAK])
                pa, dpa = PA.next()
                mm8(pa, dpa, Bf, Rf, [dFM])
                MRB, dMRB = MRBr.next()
                K.dve.op(lambda e, MRB=MRB, pa=pa: e.tensor_tensor(out=MRB[:], in0=pa[:], in1=mbc(2), op=ALU.mult), reads=[dpa, d_c], writes=[dMRB])
                pa, dpa = PA.next()
                mm8(pa, dpa, Kf, Rf, [dFM])
                MRK, dMRK = MRKr.next()
                K.dve.op(lambda e, MRK=MRK, pa=pa: e.tensor_tensor(out=MRK[:], in0=pa[:], in1=mbc(2), op=ALU.mult), reads=[dpa, d_c], writes=[dMRK])
                Np, dNp, NTp, dNTp = N0, dN0, NT0, dNT0
                for lvl in range(1, 6):
                    pa, dpa = PA.next()
                    mm8(pa, dpa, lambda h, NTp=NTp: NTp[:, h, :], lambda h, Np=Np: Np[:, h, :], [dNp, dNTp])
                    Nn, dNn = Nr.next()
                    evac_copy(Nn[:], pa[:], [dpa], writes=[dNn])
                    if lvl < 5:
                        pa2, dpa2 = PA.next()
                        mm8(pa2, dpa2, lambda h, Np=Np: Np[:, h, :], lambda h, NTp=NTp: NTp[:, h, :], [dNp, dNTp])
                        NTn, dNTn = NTr.next()
                        evac_copy(NTn[:], pa2[:], [dpa2], writes=[dNTn])
                    pa3, dpa3 = PA.next()
                    mm8(pa3, dpa3, lambda h, Nn=Nn: Nn[:, h, :], lambda h, MT=MT: MT[:, h, :], [dNn, dMT])
                    K.dve.op(lambda e, MTf=MTf, pa3=pa3: e.tensor_tensor(out=MTf[:], in0=MTf[:], in1=pa3[:], op=ALU.add),
                             reads=[dpa3], writes=[dMTf])
                    MT, dMT = MTr.next()
                    K.act.op(lambda e, MT=MT, MTf=MTf: e.activation(out=MT[:], in_=MTf[:], func=AF.Copy), reads=[dMTf], writes=[dMT])
                    Np, dNp = Nn, dNn
                    if lvl < 5:
                        NTp, dNTp = NTn, dNTn
                Sb = STb[b]
                pa, dpa = PA.next()
                mmN(pa, dpa, [(Af, lambda h, Sb=Sb: Sb[:, h, :]), (lambda h, NAK=NAK: NAK[:, h, :], Vt)], [dFM, dSTb[b], dNAK, dTM])
                X, dX = Xr.next()
                K.act.op(lambda e, X=X, pa=pa: e.activation(out=X[:], in_=pa[:], func=AF.Copy), reads=[dpa], writes=[dX])
                pa, dpa = PA.next()
                mm8(pa, dpa, lambda h, MT=MT: MT[:, h, :], lambda h, X=X: X[:, h, :], [dMT, dX])
                NU, dNU = NUr.next()
                K.act.op(lambda e, NU=NU, pa=pa: e.activation(out=NU[:], in_=pa[:], func=AF.Copy, scale=-1.0), reads=[dpa], writes=[dNU])
                pa, dpa = PA.next()
                mmN(pa, dpa, [(lambda h, Sb=Sb: Sb[:, h, :], Rf), (lambda h, NU=NU: NU[:, h, :], lambda h, MRB=MRB: MRB[:, h, :]),
                              (Vt, lambda h, MRK=MRK: MRK[:, h, :])], [dFM, dSTb[b], dNU, dMRB, dTM, dMRK])
                Y, dY = Yr.next()
                K.dve.op(lambda e, Y=Y, pa=pa: e.tensor_copy(out=Y[:], in_=pa[:]), reads=[dpa], writes=[dY])
                K.sp.dma(lambda e, Y=Y, t0=t0: e.dma_start(out=dap(scr["y_fm"], t0, [[T, 64], [64 * T, 8], [1, 64]]), in_=Y[:]),
                         reads=[dY], adds=[scr["d_p2b"]])
                pa, dpa = PA.next()
                mmN(pa, dpa, [(Bt, lambda h, NU=NU: NU[:, h, :]), (Kt, Vt)], [dTM, dNU])
                TS_, dTS = TSr.next()
                K.dve.op(lambda e, TS_=TS_, pa=pa, b=b: e.tensor_tensor(out=TS_[:], in0=pa[:], in1=ST[b][:], op=ALU.add),
                         reads=[dpa, dST[b]], writes=[dTS])
                K.dve.op(lambda e, TS_=TS_, PCt=PCt, b=b: e.tensor_tensor(
                    out=ST[b][:], in0=TS_[:], in1=PCt[:].unsqueeze(2).broadcast_to([64, 8, 64]), op=ALU.mult),
                    reads=[dTS, dPC], writes=[dST[b]])
                K.act.op(lambda e, b=b: e.activation(out=STb[b][:], in_=ST[b][:], func=AF.Copy), reads=[dST[b]], writes=[dSTb[b]])


def phase2_post(K, cfg, io, scr):
    T, S, NB = cfg["T"], cfg["S"], cfg["NB"]
    NT = T // 512
    with ExitStack() as es:
        BO = K.sb([128, 128], F32, "BO", es)
        d_c = Dep()
        K.sp.dma(lambda e: e.dma_start(out=BO[:], in_=io["blockones"][:, :]), adds=[d_c])
        LN = K.sb([128, 2, 4], F32, "LN", es)
        K.sp.dma(lambda e: e.dma_start(out=LN[:, 0, :], in_=io["lnx_w"].rearrange("o (j p) -> p (o j)", p=128)), adds=[d_c])
        K.sp.dma(lambda e: e.dma_start(out=LN[:, 1, :], in_=io["lnx_b"].rearrange("o (j p) -> p (o j)", p=128)), adds=[d_c])
        Yr = Rot(K, 2, [128, 512], F32, "pY", es=es)
        Vr = Rot(K, 2, [128, 512], F32, "pV", es=es)
        Gr = Rot(K, 2, [128, 512], F32, "pG", es=es)
        Cr = Rot(K, 2, [128, 512], F32, "pC", es=es)
        YCr = Rot(K, 2, [128, 512], F32, "pYC", es=es)
        SQr = Rot(K, 2, [128, 512], F32, "pSQ", es=es)
        Rr = Rot(K, 2, [128, 512], F32, "pR", es=es)
        Or = Rot(K, 2, [128, 512], BF16, "pO", es=es)
        PM = Rot(K, 2, [128, 512], F32, "pPM", psum=True, es=es)
        PVr = Rot(K, 2, [128, 512], F32, "pPV", psum=True, es=es)
        def make_gen(ti, j):
            cs = slice(ti * 512, (ti + 1) * 512)
            if True:
                rs = slice(j * 128, (j + 1) * 128)
                Y, dY = Yr.next()
                V, dV = Vr.next()
                G, dG = Gr.next()
                C, dC = Cr.next()
                K.sp.dma(lambda e, Y=Y, rs=rs, cs=cs: e.dma_start(out=Y[:], in_=scr["y_fm"][rs, cs]),
                         reads=[scr["d_p2b"]], writes=[dY])
                K.pool.dma(lambda e, V=V, rs=rs, cs=cs: e.dma_start(out=V[:], in_=scr["v_fm"][rs, cs]),
                           reads=[scr["d_p2a"]], writes=[dV])
                K.sp.dma(lambda e, G=G, rs=rs, cs=cs: e.dma_start(out=G[:], in_=scr["g_fm"][rs, cs]),
                         reads=[scr["d_p2a"]], writes=[dG])
                K.pool.dma(lambda e, C=C, j=j, cs=cs: e.dma_start(
                    out=C[0:64, :], in_=scr["coef_fm"][2 * j:2 * j + 1, cs].broadcast_to([64, 512])),
                    reads=[scr["d_p2a"]], writes=[dC])
                K.pool.dma(lambda e, C=C, j=j, cs=cs: e.dma_start(
                    out=C[64:128, :], in_=scr["coef_fm"][2 * j + 1:2 * j + 2, cs].broadcast_to([64, 512])),
                    reads=[scr["d_p2a"]], adds=[dC])
                pm, dpm = PM.next()
                K.pe.op(lambda e, pm=pm, Y=Y: e.matmul(pm[:], BO[:], Y[:], start=True, stop=True),
                        reads=[dY, d_c], writes=[dpm])
                YC, dYC = YCr.next()
                K.dve.op(lambda e, YC=YC, Y=Y, pm=pm: e.tensor_tensor(out=YC[:], in0=Y[:], in1=pm[:], op=ALU.subtract),
                         reads=[dY, dpm], writes=[dYC])
                SQ, dSQ = SQr.next()
                K.act.op(lambda e, SQ=SQ, YC=YC: e.activation(out=SQ[:], in_=YC[:], func=AF.Square),
                         reads=[dYC], writes=[dSQ])
                yield
                pv, dpv = PVr.next()
                K.pe.op(lambda e, pv=pv, SQ=SQ: e.matmul(pv[:], BO[:], SQ[:], start=True, stop=True),
                        reads=[dSQ, d_c], writes=[dpv])
                yield
                R, dR = Rr.next()
                K.dve.op(lambda e, R=R, pv=pv: e.tensor_scalar(out=R[:], in0=pv[:], scalar1=64e-5, scalar2=None, op0=ALU.add),
                         reads=[dpv], writes=[dR])
                K.act.op(lambda e, R=R: e.activation(out=R[:], in_=R[:], func=AF.Ln), writes=[dR])
                K.act.op(lambda e, R=R: e.activation(out=R[:], in_=R[:], func=AF.Exp, scale=-0.5), writes=[dR])
                K.dve.op(lambda e, YC=YC, R=R: e.tensor_tensor(out=YC[:], in0=YC[:], in1=R[:], op=ALU.mult),
                         reads=[dR], writes=[dYC])
                K.dve.op(lambda e, YC=YC, j=j: e.tensor_scalar(out=YC[:], in0=YC[:], scalar1=LN[:, 0, j:j + 1],
                                                              scalar2=LN[:, 1, j:j + 1], op0=ALU.mult, op1=ALU.add),
                         reads=[d_c], writes=[dYC])
                yield
                K.pool.op(lambda e, C=C, V=V: e.tensor_tensor(out=C[:], in0=C[:], in1=V[:], op=ALU.mult),
                          reads=[dV], writes=[dC])
                K.dve.op(lambda e, YC=YC, C=C: e.tensor_tensor(out=YC[:], in0=YC[:], in1=C[:], op=ALU.add),
                         reads=[dC], writes=[dYC])
                O, dO = Or.next()
                K.dve.op(lambda e, O=O, YC=YC, G=G: e.tensor_tensor(out=O[:], in0=YC[:], in1=G[:], op=ALU.mult),
                         reads=[dYC, dG], writes=[dO])
                K.sp.dma(lambda e, O=O, rs=rs, cs=cs: e.dma_start(out=scr["ya_fm"][rs, cs], in_=O[:]),
                         reads=[dO], adds=[scr["d_p2c"]])

        work = [(ti, j) for ti in range(NT) for j in range(4)]

        active = []
        nxt = 0
        while nxt < len(work) or active:
            if len(active) < 2 and nxt < len(work):
                active.append(make_gen(*work[nxt]))
                nxt += 1
            for a in list(active):
                try:
                    next(a)
                except StopIteration:
                    active.remove(a)


def phase3(K, cfg, io, scr):
    T, S, NB = cfg["T"], cfg["S"], cfg["NB"]
    NQ = S // 128
    lam_init = 0.2
    with ExitStack() as es:
        d_c = Dep()
        identb = K.sb([128, 128], BF16, "a_identb", es)
        K.sp.dma(lambda e: e.dma_start(out=identb[:], in_=io["ident_bf"][:, :]), adds=[d_c])
        TB = K.sb([128, 4, S], F32, "TB", es)
        for h in range(4):
            (K.sp if h % 2 == 0 else K.pool).dma(lambda e, h=h: e.dma_start(out=TB[:, h, :], in_=io["alibi"][h, :, :]), adds=[d_c])
        SW = K.sb([128, 128], F32, "SW", es)
        K.sp.dma(lambda e: e.dma_start(out=SW[:], in_=io["subln_w"][0:1, :].broadcast_to([128, 128])), adds=[d_c])
        LQ = K.sb([128, 4, 64], F32, "LQ", es)
        for j, nm in enumerate(["lam_q1", "lam_k1", "lam_q2", "lam_k2"]):
            K.pool.dma(lambda e, j=j, nm=nm: e.dma_start(out=LQ[:, j, :], in_=io[nm][0:1, :].broadcast_to([128, 64])), adds=[d_c])
        LM = K.sb([128, 8], F32, "LM", es)
        d_lm = Dep()
        LT_ = K.sb([128, 2, 64], F32, "LTt", es)
        K.dve.op(lambda e: e.tensor_tensor(out=LT_[:, 0, :], in0=LQ[:, 0, :], in1=LQ[:, 1, :], op=ALU.mult), reads=[d_c], writes=[d_lm])
        K.dve.op(lambda e: e.tensor_tensor(out=LT_[:, 1, :], in0=LQ[:, 2, :], in1=LQ[:, 3, :], op=ALU.mult), reads=[d_c], writes=[d_lm])
        K.dve.op(lambda e: e.tensor_reduce(out=LM[:, 0:2], in_=LT_[:], axis=AX.X, op=ALU.add), writes=[d_lm])
        K.act.op(lambda e: e.activation(out=LM[:, 2:4], in_=LM[:, 0:2], func=AF.Exp), writes=[d_lm])
        K.dve.op(lambda e: e.tensor_tensor(out=LM[:, 4:5], in0=LM[:, 3:4], in1=LM[:, 2:3], op=ALU.subtract), writes=[d_lm])
        K.dve.op(lambda e: e.tensor_scalar(out=LM[:, 4:5], in0=LM[:, 4:5], scalar1=-lam_init, scalar2=None, op0=ALU.add), writes=[d_lm])
        K.dve.op(lambda e: e.tensor_scalar(out=SW[:], in0=SW[:], scalar1=1.0 - lam_init, scalar2=None, op0=ALU.mult),
                 reads=[d_c], writes=[d_c])

        Vr = Rot(K, 2, [128, NQ, 512], BF16, "aV", es=es)
        QKr = Rot(K, 2, [64, 4, S], BF16, "aQK", es=es)
        SSr = Rot(K, 3, [128, 512], F32, "aSS", es=es)
        Pr = Rot(K, 3, [128, 512], BF16, "aP", es=es)
        PTsr = Rot(K, 4, [128, 4, 128], BF16, "aPTs", es=es)
        YB = K.sb([128, NQ, 512], BF16, "aYB", es)
        dYB = Dep()
        STr = Rot(K, 4, [128, 24], F32, "aST", es=es)
        O1r = Rot(K, 2, [128, 128], F32, "aO1", es=es)
        Or_ = Rot(K, 2, [128, 128], F32, "aO", es=es)
        junk = K.sb([128, 128], F32, "ajunk", es)
        d_junk = Dep()
        YTr = Rot(K, 2, [128, 4, 128], BF16, "aYT", es=es)
        PS = Rot(K, 3, [128, 512], F32, "aPS", psum=True, es=es)
        PTp = Rot(K, 2, [128, 4, 128], BF16, "aPTp", psum=True, es=es)
        PO = Rot(K, 2, [128, 2, 128], F32, "aPO", psum=True, es=es)
        cp = [0]

        def copy_eng():
            cp[0] += 1
            return cp[0] % 2

        SSQ = K.sb([128, NQ * 4], F32, "aSSQ", es)
        dSSQ = Dep()
        SWb = K.sb([128, 128], BF16, "aSWb", es)
        K.act.op(lambda e: e.activation(out=SWb[:], in_=SW[:], func=AF.Copy), reads=[d_c], adds=[d_c])
        pipe = []
        pidx = [0]

        def step_pipe():
            j = len(pipe) - 1
            pipe[j][0]()
            if j - 1 >= pidx[0]:
                pipe[j - 1][2]()
            if j - 2 >= pidx[0]:
                pipe[j - 2][3]()
                if pipe[j - 2][4] is not None:
                    pipe[j - 2][4]()
            pipe[j][1]()

        def flush_pipe():
            j = len(pipe) - 1
            if j - 0 >= pidx[0] and j >= 0:
                pipe[j][2]()
            for k in (j - 1, j):
                if k >= pidx[0] and k >= 0:
                    pipe[k][3]()
                    if pipe[k][4] is not None:
                        pipe[k][4]()
            pidx[0] = len(pipe)
        for b in range(NB):
            V, dV = Vr.next()
            K.sp.dma(lambda e, V=V, b=b: e.dma_start(
                out=V[:], in_=scr["av_tm"][b * S:(b + 1) * S, :].rearrange("(n p) c -> p n c", p=128)),
                reads=[scr["d_p1"]], writes=[dV])
            first_yb = True
            for h in range(4):
                QK, dQK = QKr.next()
                for j in range(4):
                    r0 = (0 if j < 2 else 512) + h * 128 + (j % 2) * 64
                    (K.sp if j % 2 == 0 else K.pool).dma(lambda e, QK=QK, j=j, r0=r0, b=b: e.dma_start(
                        out=QK[:, j, :], in_=scr["qk_fm"][r0:r0 + 64, b * S:(b + 1) * S]),
                        reads=[scr["d_p1"]], writes=[dQK] if j == 0 else [], adds=[] if j == 0 else [dQK])
                for qi in range(NQ):
                    nk = (qi + 1) * 128
                    off = (S - 128) - qi * 128
                    ST, dST = STr.next()
                    K.pool.op(lambda e, ST=ST: e.memset(ST[:], 0.0), writes=[dST])
                    po, dpo = PO.next()
                    items = []
                    for c in range(2):
                        nch = (nk + 511) // 512
                        for ch in range(nch):
                            items.append((c, ch))
                    for ii, (c, ch) in enumerate(items):
                        kb0 = ch * 512
                        n = min(512, nk - kb0)
                        nb = n // 128
                        ps, dps = PS.next()
                        SS, dSS = SSr.next()
                        Pt, dP = Pr.next()
                        hold = {}

                        def stA_pe(ps=ps, dps=dps, c=c, qi=qi, kb0=kb0, n=n, QK=QK, dQK=dQK):
                            K.pe.op(lambda e: e.matmul(
                                ps[:, :n], QK[:, c, qi * 128:(qi + 1) * 128], QK[:, 2 + c, kb0:kb0 + n],
                                start=True, stop=True), reads=[dQK], writes=[dps])

                        def stA_rest(ps=ps, dps=dps, SS=SS, dSS=dSS, Pt=Pt, dP=dP, ST=ST, dST=dST, n=n, off=off, h=h, kb0=kb0, c=c, ch=ch):
                            K.dve.op(lambda e: e.scalar_tensor_tensor(
                                out=SS[:, :n], in0=ps[:, :n], scalar=0.125, in1=TB[:, h, off + kb0:off + kb0 + n],
                                op0=ALU.mult, op1=ALU.add), reads=[dps, d_c], writes=[dSS])
                            K.act.op(lambda e: e.activation(
                                out=Pt[:, :n], in_=SS[:, :n], func=AF.Exp,
                                accum_out=ST[:, 4 * c + ch:4 * c + ch + 1]), reads=[dSS, dST], writes=[dP], adds=[dST])

                        def stB(Pt=Pt, dP=dP, nb=nb, hold=hold):
                            ptp, dptp = PTp.next()
                            for kk_ in range(nb):
                                K.pe.op(lambda e, kk_=kk_: e.transpose(
                                    out=ptp[:, kk_, :], in_=Pt[:, kk_ * 128:(kk_ + 1) * 128], identity=identb[:]),
                                    reads=[dP, d_c], writes=[dptp] if kk_ == 0 else [], adds=[] if kk_ == 0 else [dptp])
                            PTs, dPTs = PTsr.next()
                            hold["PTs"] = (PTs, dPTs)
                            if copy_eng():
                                K.act.op(lambda e: e.activation(
                                    out=PTs[:, :nb, :], in_=ptp[:, :nb, :], func=AF.Copy), reads=[dptp], writes=[dPTs])
                            else:
                                K.dve.op(lambda e: e.tensor_copy(
                                    out=PTs[:, :nb, :], in_=ptp[:, :nb, :]), reads=[dptp], writes=[dPTs])

                        def stC(nb=nb, kb0=kb0, c=c, h=h, qi=qi, po=po, dpo=dpo, V=V, dV=dV, hold=hold):
                            PTs, dPTs = hold["PTs"]
                            for kk_ in range(nb):
                                kb = kb0 // 128 + kk_
                                K.pe.op(lambda e, kb=kb, kk_=kk_: e.matmul(
                                    po[:, c, :], PTs[:, kk_, :], V[:, kb, h * 128:(h + 1) * 128],
                                    start=(kb == 0), stop=(kb == qi)), reads=[dPTs, dV],
                                    writes=[dpo] if (kb == 0 and c == 0) else [], adds=[] if (kb == 0 and c == 0) else [dpo])

                        def combine(ST=ST, dST=dST, po=po, dpo=dpo, qi=qi, h=h, fy=first_yb):
                            K.dve.op(lambda e: e.tensor_reduce(out=ST[:, 8:10], in_=ST[:, 0:8].rearrange("p (c k) -> p c k", k=4),
                                                               axis=AX.X, op=ALU.add), reads=[dST], writes=[dST])
                            K.dve.op(lambda e: e.reciprocal(out=ST[:, 10:12], in_=ST[:, 8:10]), writes=[dST])
                            K.dve.op(lambda e: e.tensor_tensor(out=ST[:, 12:13], in0=ST[:, 11:12], in1=LM[:, 4:5], op=ALU.mult),
                                     reads=[d_lm], writes=[dST])
                            O1, dO1 = O1r.next()
                            K.dve.op(lambda e: e.tensor_scalar(out=O1[:], in0=po[:, 1, :], scalar1=ST[:, 12:13],
                                                               scalar2=None, op0=ALU.mult), reads=[dpo, dST], writes=[dO1])
                            K.dve.op(lambda e: e.scalar_tensor_tensor(
                                out=YB[:, qi, h * 128:(h + 1) * 128], in0=po[:, 0, :], scalar=ST[:, 10:11], in1=O1[:], op0=ALU.mult, op1=ALU.add),
                                reads=[dpo, dST, dO1], writes=[dYB] if fy else [], adds=[] if fy else [dYB])
                            K.act.op(lambda e: e.activation(out=junk[:], in_=YB[:, qi, h * 128:(h + 1) * 128], func=AF.Square,
                                                            accum_out=SSQ[:, qi * 4 + h:qi * 4 + h + 1]),
                                     reads=[dYB], writes=[d_junk], adds=[dSSQ])

                        last = (ii == len(items) - 1)
                        pipe.append([stA_pe, stA_rest, stB, stC, combine if last else None])
                        step_pipe()
                    first_yb = False
            flush_pipe()
            K.dve.op(lambda e: e.tensor_scalar(out=SSQ[:], in0=SSQ[:], scalar1=1.0 / 128, scalar2=1e-5, op0=ALU.mult, op1=ALU.add),
                     reads=[dSSQ], writes=[dSSQ])
            K.act.op(lambda e: e.activation(out=SSQ[:], in_=SSQ[:], func=AF.Ln), writes=[dSSQ])
            K.act.op(lambda e: e.activation(out=SSQ[:], in_=SSQ[:], func=AF.Exp, scale=-0.5), writes=[dSSQ])
            YBv = YB[:].rearrange("p q (h e) -> p (q h) e", e=128)
            K.dve.op(lambda e: e.tensor_tensor(out=YBv, in0=YBv, in1=SSQ[:].unsqueeze(2).broadcast_to([128, NQ * 4, 128]), op=ALU.mult),
                     reads=[dSSQ], writes=[dYB])
            K.pool.op(lambda e: e.tensor_tensor(out=YBv, in0=YBv, in1=SWb[:].unsqueeze(1).broadcast_to([128, NQ * 4, 128]), op=ALU.mult),
                      reads=[d_c], writes=[dYB])
            for qi in range(NQ):
                ptp, dptp = PTp.next()
                for h in range(4):
                    K.pe.op(lambda e, ptp=ptp, qi=qi, h=h: e.transpose(out=ptp[:, h, :], in_=YB[:, qi, h * 128:(h + 1) * 128],
                                                                      identity=identb[:]),
                            reads=[dYB, d_c], writes=[dptp] if h == 0 else [], adds=[] if h == 0 else [dptp])
                YT, dYT = YTr.next()
                K.act.op(lambda e, ptp=ptp, YT=YT: e.activation(out=YT[:], in_=ptp[:, 0:4, :], func=AF.Copy),
                         reads=[dptp], writes=[dYT])
                t0 = b * S + qi * 128
                K.sp.dma(lambda e, YT=YT, t0=t0: e.dma_start(
                    out=dap(scr["yb_fm"], t0, [[T, 128], [128 * T, 4], [1, 128]]), in_=YT[:]),
                    reads=[dYT], adds=[scr["d_p3"]])


def load_w_bf16(K, es, src2d, rows, cols, name, dep, stage_rot, q, W=None):
    nk = rows // 128
    if W is None:
        W = K.sb([128, nk, cols], BF16, name, es)
    for kc in range(nk):
        for c0 in range(0, cols, 1024):
            n = min(1024, cols - c0)
            st, dst = stage_rot.next()
            q[0] += 1
            (K.sp if q[0] % 2 == 0 else K.pool).dma(lambda e, st=st, kc=kc, c0=c0, n=n: e.dma_start(
                out=st[:, :n], in_=src2d[kc * 128:(kc + 1) * 128, c0:c0 + n]), writes=[dst])
            if q[0] % 2 == 0:
                K.act.op(lambda e, st=st, kc=kc, c0=c0, n=n: e.activation(out=W[:, kc, c0:c0 + n], in_=st[:, :n], func=AF.Copy),
                         reads=[dst], adds=[dep])
            else:
                K.dve.op(lambda e, st=st, kc=kc, c0=c0, n=n: e.tensor_copy(out=W[:, kc, c0:c0 + n], in_=st[:, :n]),
                         reads=[dst], adds=[dep])
    return W


def phase4(K, cfg, io, scr):
    T, S, NB = cfg["T"], cfg["S"], cfg["NB"]
    NT = T // 512
    with ExitStack() as es:
        d_c = Dep()
        identb = K.sb([128, 128], BF16, "m_identb", es)
        K.sp.dma(lambda e: e.dma_start(out=identb[:], in_=io["ident_bf"][:, :]), adds=[d_c])
        NF = K.sb([128, D], F32, "NF", es)
        K.sp.dma(lambda e: e.dma_start(out=NF[:], in_=io["norm_ffn_w"][0:1, :].broadcast_to([128, D])), adds=[d_c])
        stg = Rot(K, 2, [128, 1024], F32, "m_stg", es=es)
        q = [0]
        PAw = load_w_bf16(K, es, io["proj_a"][0], 512, D, "PAw", d_c, stg, q)
        PBw = load_w_bf16(K, es, io["proj_b"][0], 512, D, "PBw", d_c, stg, q)
        WO = load_w_bf16(K, es, io["w_out"][0], D, D, "WO", d_c, stg, q)
        YAr = Rot(K, 2, [128, 4, 512], BF16, "mYA", es=es)
        YBr = Rot(K, 2, [128, 4, 512], BF16, "mYB", es=es)
        SGr = Rot(K, 2, [128, 16, 512], BF16, "mSG", es=es)
        MGr = Rot(K, 2, [128, 8, 512], BF16, "mMG", es=es)
        t1r = Rot(K, 2, [128, 512], F32, "mt1", es=es)
        t2r = Rot(K, 2, [128, 512], F32, "mt2", es=es)
        Xr = Rot(K, 2, [128, D], F32, "mX", es=es)
        X1r = Rot(K, 2, [128, D], F32, "mX1", es=es)
        XHr = Rot(K, 2, [128, D], F32, "mXH", es=es)
        XBr = Rot(K, 2, [128, D], BF16, "mXB", es=es)
        XTr = Rot(K, 2, [128, 8, 128], BF16, "mXT", es=es)
        junk = K.sb([128, D], BF16, "mjunk", es)
        d_junk = Dep()
        STr = Rot(K, 4, [128, 4], F32, "mST", es=es)
        PP = Rot(K, 2, [128, 2, 512], F32, "mPP", psum=True, es=es)
        PO2 = Rot(K, 1, [128, 2, 512], F32, "mPO", psum=True, es=es)
        PTp = Rot(K, 1, [128, 8, 128], BF16, "mPTp", psum=True, es=es)
        def make_gen(ti):
            cs = slice(ti * 512, (ti + 1) * 512)
            YA, dYA = YAr.next()
            YB, dYB = YBr.next()
            SG, dSG = SGr.next()
            K.sp.dma(lambda e, YA=YA, cs=cs: e.dma_start(out=YA[:], in_=scr["ya_fm"][:, cs].rearrange("(c p) t -> p c t", p=128)),
                     reads=[scr["d_p2c"]], writes=[dYA])
            K.pool.dma(lambda e, YB=YB, cs=cs: e.dma_start(out=YB[:], in_=scr["yb_fm"][:, cs].rearrange("(c p) t -> p c t", p=128)),
                       reads=[scr["d_p3"]], writes=[dYB])
            K.sp.dma(lambda e, SG=SG, cs=cs: e.dma_start(out=SG[:], in_=scr["sg_fm"][:, cs].rearrange("(c p) t -> p c t", p=128)),
                     reads=[scr["d_p1"]], writes=[dSG])
            MG, dMG = MGr.next()
            for m in range(8):
                pp, dpp = PP.next()
                for c in range(4):
                    K.pe.op(lambda e, pp=pp, YA=YA, c=c, m=m: e.matmul(pp[:, 0, :], PAw[:, c, m * 128:(m + 1) * 128], YA[:, c, :],
                                                                      start=(c == 0), stop=(c == 3)),
                            reads=[dYA, d_c], writes=[dpp] if c == 0 else [], adds=[] if c == 0 else [dpp])
                for c in range(4):
                    K.pe.op(lambda e, pp=pp, YB=YB, c=c, m=m: e.matmul(pp[:, 1, :], PBw[:, c, m * 128:(m + 1) * 128], YB[:, c, :],
                                                                      start=(c == 0), stop=(c == 3)),
                            reads=[dYB, d_c], adds=[dpp])
                t1, dt1 = t1r.next()
                t2, dt2 = t2r.next()
                K.dve.op(lambda e, t1=t1, pp=pp, SG=SG, m=m: e.tensor_tensor(out=t1[:], in0=pp[:, 0, :], in1=SG[:, m, :], op=ALU.mult),
                         reads=[dpp, dSG], writes=[dt1])
                K.dve.op(lambda e, t2=t2, pp=pp, SG=SG, m=m: e.tensor_tensor(out=t2[:], in0=pp[:, 1, :], in1=SG[:, 8 + m, :], op=ALU.mult),
                         reads=[dpp, dSG], writes=[dt2])
                K.pool.op(lambda e, t1=t1, t2=t2, MG=MG, m=m: e.tensor_tensor(out=MG[:, m, :], in0=t1[:], in1=t2[:], op=ALU.add),
                          reads=[dt1, dt2], writes=[dMG] if m == 0 else [], adds=[] if m == 0 else [dMG])
                if m % 2 == 1:
                    yield
            for sub in range(4):
                t0 = ti * 512 + sub * 128
                X, dX = Xr.next()
                K.pool.dma(lambda e, X=X, t0=t0: e.dma_start(out=X[:], in_=io["x"][t0:t0 + 128, :]), writes=[dX])
                po, dpo = PO2.next()
                for n in range(2):
                    for m in range(8):
                        K.pe.op(lambda e, po=po, MG=MG, m=m, n=n, sub=sub: e.matmul(
                            po[:, n, :], MG[:, m, sub * 128:(sub + 1) * 128], WO[:, m, n * 512:(n + 1) * 512],
                            start=(m == 0), stop=(m == 7)), reads=[dMG, d_c],
                            writes=[dpo] if (m == 0 and n == 0) else [], adds=[] if (m == 0 and n == 0) else [dpo])
                X1, dX1 = X1r.next()
                K.dve.op(lambda e, X1=X1, X=X, po=po: e.tensor_tensor(out=X1[:], in0=X[:], in1=po[:].rearrange("p a b -> p (a b)"),
                                                                     op=ALU.add), reads=[dX, dpo], writes=[dX1])
                K.sp.dma(lambda e, X1=X1, t0=t0: e.dma_start(out=scr["x1_tm"][t0:t0 + 128, :], in_=X1[:]),
                         reads=[dX1], adds=[scr["d_p4"]])
                ST, dST = STr.next()
                K.act.op(lambda e, X1=X1, ST=ST: e.activation(out=junk[:], in_=X1[:], func=AF.Square, accum_out=ST[:, 0:1]),
                         reads=[dX1], writes=[d_junk, dST])
                K.dve.op(lambda e, ST=ST: e.tensor_scalar(out=ST[:, 1:2], in0=ST[:, 0:1], scalar1=1.0 / D, scalar2=1e-6,
                                                          op0=ALU.mult, op1=ALU.add), writes=[dST])
                K.act.op(lambda e, ST=ST: e.activation(out=ST[:, 2:3], in_=ST[:, 1:2], func=AF.Ln), writes=[dST])
                K.act.op(lambda e, ST=ST: e.activation(out=ST[:, 3:4], in_=ST[:, 2:3], func=AF.Exp, scale=-0.5), writes=[dST])
                XH, dXH = XHr.next()
                K.dve.op(lambda e, XH=XH, X1=X1, ST=ST: e.scalar_tensor_tensor(
                    out=XH[:], in0=X1[:], scalar=ST[:, 3:4], in1=NF[:], op0=ALU.mult, op1=ALU.mult),
                    reads=[dX1, dST, d_c], writes=[dXH])
                K.sp.dma(lambda e, XH=XH, t0=t0: e.dma_start(out=scr["xh_tm"][t0:t0 + 128, :], in_=XH[:]),
                         reads=[dXH], adds=[scr["d_p4"]])
                XB, dXB = XBr.next()
                K.act.op(lambda e, XB=XB, XH=XH: e.activation(out=XB[:], in_=XH[:], func=AF.Copy), reads=[dXH], writes=[dXB])
                yield
                ptp, dptp = PTp.next()
                for kc in range(8):
                    K.pe.op(lambda e, ptp=ptp, XB=XB, kc=kc: e.transpose(out=ptp[:, kc, :], in_=XB[:, kc * 128:(kc + 1) * 128],
                                                                        identity=identb[:]),
                            reads=[dXB, d_c], writes=[dptp] if kc == 0 else [], adds=[] if kc == 0 else [dptp])
                XT, dXT = XTr.next()
                K.act.op(lambda e, ptp=ptp, XT=XT: e.activation(out=XT[:], in_=ptp[:], func=AF.Copy), reads=[dptp], writes=[dXT])
                K.sp.dma(lambda e, XT=XT, t0=t0: e.dma_start(
                    out=dap(scr["xhT_fm"], t0, [[T, 128], [128 * T, 8], [1, 128]]), in_=XT[:]),
                    reads=[dXT], adds=[scr["d_p4"]])
                yield

        work = [(ti,) for ti in range(NT)]

        active = []
        nxt = 0
        while nxt < len(work) or active:
            if len(active) < 2 and nxt < len(work):
                active.append(make_gen(*work[nxt]))
                nxt += 1
            for a in list(active):
                try:
                    next(a)
                except StopIteration:
                    active.remove(a)


def phase5(K, cfg, io, scr):
    T, S, NB = cfg["T"], cfg["S"], cfg["NB"]
    NTT = T // 128
    with ExitStack() as es:
        d_c = Dep()
        identb = K.sb([128, 128], BF16, "f_identb", es)
        K.sp.dma(lambda e: e.dma_start(out=identb[:], in_=io["ident_bf"][:, :]), adds=[d_c])
        IOTA = K.sb([128, 16], F32, "IOTA", es)
        K.sp.dma(lambda e: e.dma_start(out=IOTA[:], in_=io["iota16"][:, :]), adds=[d_c])
        FNW = K.sb([128, D], F32, "FNW", es)
        K.sp.dma(lambda e: e.dma_start(out=FNW[:], in_=io["final_norm_w"][0:1, :].broadcast_to([128, D])), adds=[d_c])
        WQ = K.sb([128, 8, 2048], BF16, "WQ", es)
        KT = K.sb([128, 16, 128], BF16, "KT", es)
        PQ = Rot(K, 1, [128, 8, 128], F32, "fPQ", psum=True, es=es)
        PSc = Rot(K, 1, [128, 8, 128], F32, "fPSc", psum=True, es=es)
        PKT = Rot(K, 1, [128, 8, 128], BF16, "fPKT", psum=True, es=es)
        es_setup = ExitStack()
        stg = Rot(K, 2, [128, 1024], F32, "f_stg", es=es_setup)
        q = [0]
        load_w_bf16(K, es, io["peer_wq"][0], D, 2048, "WQ", d_c, stg, q, W=WQ)
        KF = K.sb([128, 16, 128], F32, "KF", es_setup)
        dKF = Dep()
        K.sp.dma(lambda e: e.dma_start(out=KF[:], in_=io["peer_keys"][0].rearrange("h c n d -> n (h c) d")), writes=[dKF])
        KB = K.sb([128, 16, 128], BF16, "KB", es_setup)
        K.dve.op(lambda e: e.tensor_copy(out=KB[:], in_=KF[:]), reads=[dKF], writes=[dKF])
        for half in range(2):
            pk, dpk = PKT.next()
            for i in range(8):
                K.pe.op(lambda e, pk=pk, i=i, half=half: e.transpose(out=pk[:, i, :], in_=KB[:, half * 8 + i, :], identity=identb[:]),
                        reads=[dKF, d_c], writes=[dpk] if i == 0 else [], adds=[] if i == 0 else [dpk])
            K.act.op(lambda e, pk=pk, half=half: e.activation(out=KT[:, half * 8:(half + 1) * 8, :], in_=pk[:], func=AF.Copy),
                     reads=[dpk], adds=[d_c])

        K.barrier()
        es_setup.close()
        XTr = Rot(K, 2, [128, 8, 128], BF16, "fXT", es=es)
        XHr = Rot(K, 2, [128, D], F32, "fXH", es=es)
        X1r = Rot(K, 2, [128, D], F32, "fX1", es=es)
        QTr = Rot(K, 1, [128, 16, 128], BF16, "fQT", es=es)
        SCr = Rot(K, 1, [128, 16, 128], F32, "fSC", es=es)
        SC2 = K.sb([128, 256], F32, "fSC2", es)
        dSC2 = Dep()
        M16r = Rot(K, 1, [128, 16, 16], F32, "fM16", es=es)
        I16r = Rot(K, 1, [128, 16, 16], U32, "fI16", es=es)
        I16fr = Rot(K, 1, [128, 16, 16], F32, "fI16f", es=es)
        CANDr = Rot(K, 1, [128, 8, 256], F32, "fCAND", es=es)
        VALr = Rot(K, 1, [128, 8, 16], F32, "fVAL", es=es)
        CIr = Rot(K, 1, [128, 3, 128], U32, "fCI", es=es)
        ABr = Rot(K, 1, [128, 2, 128], F32, "fAB", es=es)
        OHr = CANDr
        E12r = Rot(K, 1, [128, 3, 128], F32, "fE12", es=es)
        IDSr = Rot(K, 2, [128, 128], I32, "fIDS", es=es)
        GTr = Rot(K, 2, [128, 4, 128], F32, "fGT", es=es)
        S8r = Rot(K, 2, [128, 16], F32, "fS8", es=es)
        GRP = cfg.get("grp", 4)
        ACTDOT = tuple(cfg.get("actdot", (0, 2)))
        if isinstance(cfg.get("actdot_mask"), int):
            ACTDOT = tuple(i for i in range(GRP) if (cfg["actdot_mask"] >> i) & 1)
        junk3r = Rot(K, 2, [128, D], BF16, "fjunk3", es=es)
        junkr = Rot(K, 3, [128, D], BF16, "fjunkr", es=es)
        PRDr = Rot(K, 3, [128, D], BF16, "fPRD", es=es)
        GBr = Rot(K, cfg.get("ngbuf", 22), [128, 2 * D], BF16, "fGB", es=es)
        junk2 = K.sb([128, D], BF16, "fjunk2", es)
        d_junk2 = Dep()
        XHbr = Rot(K, 2, [128, D], BF16, "fXHb", es=es)
        DGr = Rot(K, 4, [128, 128], BF16, "fDG", es=es)
        PY = Rot(K, 1, [128, 2, 512], F32, "fPY", psum=True, es=es)

        RES = {}

        def routing(ti):
            t0 = ti * 128
            XT, dXT = XTr.next()
            XH, dXH = XHr.next()
            X1, dX1 = X1r.next()
            K.sp.dma(lambda e, XT=XT, t0=t0: e.dma_start(out=XT[:], in_=dap(scr["xhT_fm"], t0, [[T, 128], [128 * T, 8], [1, 128]])),
                     reads=[scr["d_p4"]], writes=[dXT])
            K.sp.dma(lambda e, XH=XH, t0=t0: e.dma_start(out=XH[:], in_=scr["xh_tm"][t0:t0 + 128, :]), reads=[scr["d_p4"]], writes=[dXH])
            K.sp.dma(lambda e, X1=X1, t0=t0: e.dma_start(out=X1[:], in_=scr["x1_tm"][t0:t0 + 128, :]), reads=[scr["d_p4"]], writes=[dX1])
            QT, dQT = QTr.next()
            for half in range(2):
                pq, dpq = PQ.next()
                for i in range(8):
                    hc = half * 8 + i
                    for kc in range(8):
                        K.pe.op(lambda e, pq=pq, i=i, hc=hc, kc=kc, XT=XT: e.matmul(
                            pq[:, i, :], WQ[:, kc, hc * 128:(hc + 1) * 128], XT[:, kc, :], start=(kc == 0), stop=(kc == 7)),
                            reads=[dXT, d_c], writes=[dpq] if (i == 0 and kc == 0) else [], adds=[] if (i == 0 and kc == 0) else [dpq])
                K.act.op(lambda e, pq=pq, QT=QT, half=half: e.activation(out=QT[:, half * 8:(half + 1) * 8, :], in_=pq[:], func=AF.Copy),
                         reads=[dpq], writes=[dQT] if half == 0 else [], adds=[] if half == 0 else [dQT])
            SC, dSC = SCr.next()
            for half in range(2):
                psc, dpsc = PSc.next()
                for i in range(8):
                    hc = half * 8 + i
                    K.pe.op(lambda e, psc=psc, i=i, hc=hc, QT=QT: e.matmul(psc[:, i, :], QT[:, hc, :], KT[:, hc, :], start=True, stop=True),
                            reads=[dQT, d_c], writes=[dpsc] if i == 0 else [], adds=[] if i == 0 else [dpsc])
                K.act.op(lambda e, psc=psc, SC=SC, half=half: e.activation(out=SC[:, half * 8:(half + 1) * 8, :], in_=psc[:], func=AF.Copy),
                         reads=[dpsc], writes=[dSC] if half == 0 else [], adds=[] if half == 0 else [dSC])
            yield
            M16, dM = M16r.next()
            I16, dI = I16r.next()
            for hc in range(16):
                if hc % 4 == 0 and hc > 0:
                    yield
                K.dve.op(lambda e, M16=M16, SC=SC, hc=hc: e.max(out=M16[:, hc, 0:8], in_=SC[:, hc, :]), reads=[dSC],
                         writes=[dM] if hc == 0 else [], adds=[] if hc == 0 else [dM])
                K.dve.op(lambda e, M16=M16, SC=SC, hc=hc: e.match_replace(out=SC2[:, 0:128], in_to_replace=M16[:, hc, 0:8],
                                                                         in_values=SC[:, hc, :], imm_value=-1e30),
                         reads=[dSC, dM], writes=[dSC2])
                K.dve.op(lambda e, M16=M16, hc=hc: e.max(out=M16[:, hc, 8:16], in_=SC2[:, 0:128]), reads=[dSC2], adds=[dM])
                K.dve.op(lambda e, M16=M16, I16=I16, SC=SC, hc=hc: e.max_index(out=I16[:, hc, 0:8], in_max=M16[:, hc, 0:8],
                                                                              in_values=SC[:, hc, :]),
                         reads=[dSC, dM], writes=[dI] if hc == 0 else [], adds=[] if hc == 0 else [dI])
                K.dve.op(lambda e, M16=M16, I16=I16, SC=SC, hc=hc: e.max_index(out=I16[:, hc, 8:16], in_max=M16[:, hc, 8:16],
                                                                              in_values=SC[:, hc, :]),
                         reads=[dSC, dM], adds=[dI])
            yield
            I16f, dIf = I16fr.next()
            K.dve.op(lambda e, I16f=I16f, I16=I16: e.tensor_copy(out=I16f[:], in_=I16[:]), reads=[dI], writes=[dIf])
            I16fv = I16f[:].rearrange("p (h c) k -> p h c k", c=2)
            K.dve.op(lambda e, I16fv=I16fv: e.tensor_scalar(out=I16fv[:, :, 0, :], in0=I16fv[:, :, 0, :], scalar1=128.0, scalar2=None,
                                                            op0=ALU.mult), writes=[dIf])
            CAND, dCA = CANDr.next()
            M16v = M16[:].rearrange("p (h c) k -> p h c k", c=2)
            K.dve.op(lambda e, CAND=CAND, M16v=M16v: e.tensor_tensor(
                out=CAND[:].rearrange("p h (a b) -> p h a b", b=16),
                in0=M16v[:, :, 0, :].unsqueeze(3).broadcast_to([128, 8, 16, 16]),
                in1=M16v[:, :, 1, :].unsqueeze(2).broadcast_to([128, 8, 16, 16]), op=ALU.add),
                reads=[dM], writes=[dCA])
            VAL, dVAL = VALr.next()
            CI, dCI = CIr.next()
            CIv = CI[:, 0, :].rearrange("p (h k) -> p h k", k=16)
            for h in range(8):
                if h % 4 == 0:
                    yield
                K.dve.op(lambda e, VAL=VAL, CAND=CAND, h=h: e.max(out=VAL[:, h, 0:8], in_=CAND[:, h, :]), reads=[dCA],
                         writes=[dVAL] if h == 0 else [], adds=[] if h == 0 else [dVAL])
                K.dve.op(lambda e, VAL=VAL, CAND=CAND, h=h: e.match_replace(out=SC2[:, :], in_to_replace=VAL[:, h, 0:8],
                                                                           in_values=CAND[:, h, :], imm_value=-1e30),
                         reads=[dCA, dVAL], writes=[dSC2])
                K.dve.op(lambda e, VAL=VAL, h=h: e.max(out=VAL[:, h, 8:16], in_=SC2[:, :]), reads=[dSC2], adds=[dVAL])
                K.dve.op(lambda e, VAL=VAL, CIv=CIv, CAND=CAND, h=h: e.max_index(out=CIv[:, h, 0:8], in_max=VAL[:, h, 0:8],
                                                                                in_values=CAND[:, h, :]),
                         reads=[dCA, dVAL], writes=[dCI] if h == 0 else [], adds=[] if h == 0 else [dCI])
                K.dve.op(lambda e, VAL=VAL, CIv=CIv, CAND=CAND, h=h: e.max_index(out=CIv[:, h, 8:16], in_max=VAL[:, h, 8:16],
                                                                                in_values=CAND[:, h, :]),
                         reads=[dCA, dVAL], adds=[dCI])
            yield
            GT, dGT = GTr.next()
            S8, dS8 = S8r.next()
            Ev = GT[:, 0, :].rearrange("p (h k) -> p h k", k=16)
            Gv = GT[:, 1, :].rearrange("p (h k) -> p h k", k=16)
            K.dve.op(lambda e, Ev=Ev, VAL=VAL: e.tensor_tensor(out=Ev, in0=VAL[:], in1=VAL[:, :, 0:1].broadcast_to([128, 8, 16]),
                                                              op=ALU.subtract), reads=[dVAL], writes=[dGT])
            K.act.op(lambda e, GT=GT: e.activation(out=GT[:, 0, :], in_=GT[:, 0, :], func=AF.Exp), writes=[dGT])
            K.dve.op(lambda e, Ev=Ev, S8=S8: e.tensor_reduce(out=S8[:, 0:8], in_=Ev, axis=AX.X, op=ALU.add), reads=[dGT], writes=[dS8])
            K.dve.op(lambda e, S8=S8: e.reciprocal(out=S8[:, 8:16], in_=S8[:, 0:8]), writes=[dS8])
            K.dve.op(lambda e, Ev=Ev, Gv=Gv, S8=S8: e.tensor_tensor(out=Gv, in0=Ev, in1=S8[:, 8:16].unsqueeze(2).broadcast_to([128, 8, 16]),
                                                                   op=ALU.mult), reads=[dS8], writes=[dGT])
            yield
            K.dve.op(lambda e, CI=CI: e.tensor_single_scalar(out=CI[:, 1, :], in_=CI[:, 0, :], scalar=4, op=ALU.logical_shift_right),
                     writes=[dCI])
            K.dve.op(lambda e, CI=CI: e.tensor_single_scalar(out=CI[:, 2, :], in_=CI[:, 0, :], scalar=15, op=ALU.bitwise_and),
                     writes=[dCI])
            AB, dAB = ABr.next()
            K.dve.op(lambda e, AB=AB, CI=CI: e.tensor_copy(out=AB[:], in_=CI[:, 1:3, :]), reads=[dCI], writes=[dAB])
            OH, dOH = OHr.next()
            E12, dE12 = E12r.next()
            OHv = OH[:].rearrange("p h (j a) -> p h j a", a=16)
            for c in range(2):
                ABv = AB[:, c, :].rearrange("p (h j) -> p h j", j=16)
                K.dve.op(lambda e, OHv=OHv, ABv=ABv: e.tensor_tensor(
                    out=OHv, in0=ABv.unsqueeze(3).broadcast_to([128, 8, 16, 16]),
                    in1=IOTA[:].unsqueeze(1).unsqueeze(1).broadcast_to([128, 8, 16, 16]), op=ALU.is_equal),
                    reads=[dAB, d_c], writes=[dOH])
                K.dve.op(lambda e, OHv=OHv, I16fv=I16fv, c=c: e.tensor_tensor(
                    out=OHv, in0=OHv, in1=I16fv[:, :, c, :].unsqueeze(2).broadcast_to([128, 8, 16, 16]), op=ALU.mult),
                    reads=[dIf], writes=[dOH])
                K.dve.op(lambda e, OHv=OHv, E12=E12, c=c: e.tensor_reduce(
                    out=E12[:, c, :].rearrange("p (h j) -> p h j", j=16), in_=OHv, axis=AX.X, op=ALU.add),
                    reads=[dOH], writes=[dE12] if c == 0 else [], adds=[] if c == 0 else [dE12])
            K.dve.op(lambda e, E12=E12: e.tensor_tensor(out=E12[:, 2, :], in0=E12[:, 0, :], in1=E12[:, 1, :], op=ALU.add), writes=[dE12])
            IDS, dIDS = IDSr.next()
            K.dve.op(lambda e, IDS=IDS, E12=E12: e.tensor_copy(out=IDS[:], in_=E12[:, 2, :]), reads=[dE12], writes=[dIDS])
            if "ids_dbg" in scr:
                K.sp.dma(lambda e, IDS=IDS, t0=t0: e.dma_start(out=scr["ids_dbg"][t0:t0 + 128, :], in_=IDS[:]), reads=[dIDS])
                K.sp.dma(lambda e, GT=GT, t0=t0: e.dma_start(out=scr["gate_dbg"][t0:t0 + 128, :], in_=GT[:, 1, :]), reads=[dGT])
            RES[ti] = dict(t0=t0, XH=XH, dXH=dXH, X1=X1, dX1=dX1, IDS=IDS, dIDS=dIDS, GT=GT, dGT=dGT, S8=S8, dS8=dS8)

        GDEPS = {}

        def expert(R):
            t0, XH, dXH, X1, dX1, IDS, dIDS, GT, dGT, S8, dS8 = (R[k] for k in
                ("t0", "XH", "dXH", "X1", "dX1", "IDS", "dIDS", "GT", "dGT", "S8", "dS8"))
            XHb, dXHb = XHbr.next()
            K.act.op(lambda e: e.activation(out=XHb[:], in_=XH[:], func=AF.Copy), reads=[dXH], writes=[dXHb])
            py, dpy = PY.next()
            NGRP = 128 // GRP
            bufs = {}
            gd = GDEPS.setdefault(id(GT), [(Dep(), Dep()) for _ in range(NGRP)])

            def stage_a(g):
                for jj in range(GRP):
                    j = g * GRP + jj
                    GB, dGB = GBr.next()
                    bufs[j] = (GB, dGB)
                    K.pool.dma(lambda e, GB=GB, j=j: e.indirect_dma_start(
                        out=GB[:], out_offset=None, in_=scr["uv_tab"][:, :],
                        in_offset=bass.IndirectOffsetOnAxis(ap=IDS[:, j:j + 1], axis=0)), reads=[dIDS, scr["d_uv"]], writes=[dGB])
                    if jj in ACTDOT:
                        PRD, dPRD = PRDr.next()
                        K.dve.op(lambda e, GB=GB, PRD=PRD: e.tensor_tensor(out=PRD[:], in0=GB[:, 0:D], in1=XHb[:], op=ALU.mult),
                                 reads=[dGB, dXHb], writes=[dPRD])
                        j3, dj3 = junk3r.next()
                        K.act.op(lambda e, PRD=PRD, j=j, j3=j3: e.activation(out=j3[:], in_=PRD[:], func=AF.Copy, accum_out=GT[:, 2, j:j + 1]),
                                 reads=[dPRD], writes=[dj3] + ([gd[g][0]] if jj == 0 else []), adds=[] if jj == 0 else [gd[g][0]])
                    else:
                        j1, dj1 = junkr.next()
                        K.dve.op(lambda e, GB=GB, j=j, j1=j1: e.scalar_tensor_tensor(
                            out=j1[:], in0=GB[:, 0:D], scalar=1.0, in1=XHb[:], op0=ALU.mult, op1=ALU.mult, accum_out=GT[:, 2, j:j + 1]),
                            reads=[dGB, dXHb], writes=[dj1] + ([gd[g][0]] if jj == 0 else []), adds=[] if jj == 0 else [gd[g][0]])
                gs = slice(g * GRP, (g + 1) * GRP)
                K.act.op(lambda e: e.activation(out=GT[:, 3, gs], in_=GT[:, 2, gs], func=AF.Gelu), reads=[gd[g][0]], writes=[gd[g][1]])

            def stage_b(g):
                gs = slice(g * GRP, (g + 1) * GRP)
                K.dve.op(lambda e: e.tensor_tensor(out=GT[:, 3, gs], in0=GT[:, 3, gs], in1=GT[:, 1, gs], op=ALU.mult), reads=[dGT], writes=[gd[g][1]])
                for jj in range(GRP):
                    j = g * GRP + jj
                    GB, dGB = bufs.pop(j)
                    DG, dDG = DGr.next()
                    K.act.op(lambda e, DG=DG, j=j: e.activation(out=DG[:], in_=identb[:], func=AF.Copy, scale=GT[:, 3, j:j + 1]),
                             reads=[gd[g][1], d_c], writes=[dDG])
                    for n in range(2):
                        K.pe.op(lambda e, DG=DG, GB=GB, n=n, j=j: e.matmul(
                            py[:, n, :], DG[:], GB[:, D + n * 512:D + (n + 1) * 512], start=(j == 0), stop=(j == 127)),
                            reads=[dDG, dGB], writes=[dpy] if (j == 0 and n == 0) else [], adds=[] if (j == 0 and n == 0) else [dpy])

            gen = routing(R["next"]) if R.get("next") is not None else None
            for g in range(NGRP):
                stage_a(g)
                if g >= 1:
                    stage_b(g - 1)
                if gen is not None and g >= 2 and g % 2 == 0:
                    try:
                        next(gen)
                    except StopIteration:
                        gen = None
            stage_b(NGRP - 1)
            if gen is not None:
                for _ in gen:
                    pass
            K.dve.op(lambda e: e.tensor_tensor(out=X1[:], in0=X1[:], in1=py[:].rearrange("p a b -> p (a b)"), op=ALU.add),
                     reads=[dpy], writes=[dX1])
            K.act.op(lambda e: e.activation(out=junk2[:], in_=X1[:], func=AF.Square, accum_out=S8[:, 0:1]),
                     reads=[dX1], writes=[d_junk2, dS8])
            K.dve.op(lambda e: e.tensor_scalar(out=S8[:, 1:2], in0=S8[:, 0:1], scalar1=1.0 / D, scalar2=1e-6,
                                               op0=ALU.mult, op1=ALU.add), writes=[dS8])
            K.act.op(lambda e: e.activation(out=S8[:, 2:3], in_=S8[:, 1:2], func=AF.Sqrt), writes=[dS8])
            K.dve.op(lambda e: e.reciprocal(out=S8[:, 3:4], in_=S8[:, 2:3]), writes=[dS8])
            K.dve.op(lambda e: e.scalar_tensor_tensor(
                out=XH[:], in0=X1[:], scalar=S8[:, 3:4], in1=FNW[:], op0=ALU.mult, op1=ALU.mult),
                reads=[dX1, dS8, d_c], writes=[dXH])
            K.sp.dma(lambda e: e.dma_start(out=io["out"][t0:t0 + 128, :], in_=XH[:]), reads=[dXH])

        for _ in routing(0):
            pass
        for ti in range(NTT):
            R = RES.pop(ti)
            R["next"] = ti + 1 if ti + 1 < NTT else None
            expert(R)


def make_consts():
    c = {}
    c["ident_bf"] = np.eye(128, dtype=np.float32).astype(ml_dtypes.bfloat16)
    c["ident_f"] = np.eye(128, dtype=np.float32)
    bo = np.zeros((128, 128), np.float32)
    bo[:64, :64] = 1.0 / 64
    bo[64:, 64:] = 1.0 / 64
    c["blockones"] = bo
    pp = np.arange(128)[:, None]
    ff = np.arange(128)[None, :]
    c["tri"] = ((pp <= ff) & (pp // 64 == ff // 64)).astype(np.float32)
    p6 = np.arange(64)[:, None]
    f6 = np.arange(64)[None, :]
    mk = np.zeros((64, 3, 64), np.float32)
    mk[:, 0, :] = (f6 < p6)
    mk[:, 1, :] = (f6 > p6)
    mk[:, 2, :] = (f6 >= p6)
    c["masks"] = mk
    c["ident64"] = np.eye(64, dtype=np.float32).astype(ml_dtypes.bfloat16)
    c["ones64"] = np.ones((64, 1), np.float32)
    c["iota16"] = np.tile(np.arange(16, dtype=np.float32)[None, :], (128, 1))
    return c


def make_alibi(S):
    al = np.zeros((4, 128, S), np.float32)
    ql = np.arange(128)[:, None]
    m = np.arange(S)[None, :]
    for h in range(4):
        slope = 2.0 ** (-8.0 * (h + 1) / 4)
        v = -slope * (ql - m + (S - 128)).astype(np.float32)
        al[h] = np.where(m <= ql + (S - 128), v, -30000.0)
    return al


def _unused():
    c = {}
    return c


def build(cfg):
    NB, S = cfg["NB"], cfg["S"]
    T = NB * S
    cfg["T"] = T
    dbg = set(cfg.get("debug", ()))
    phases = cfg.get("phases", (1,))
    nc = bass.Bass("TRN2", target_bir_lowering=False)
    io = {}

    def inp(name, shape, dt=F32):
        io[name] = nc.dram_tensor(name, list(shape), dt, kind="ExternalInput").ap()

    inp("x", [T, D])
    inp("norm_mix_w", [1, D])
    inp("w_in", [1, D, IN_COLS])
    inp("ident_bf", [128, 128], BF16)
    inp("ident_f", [128, 128])
    inp("blockones", [128, 128])
    inp("tri", [128, 128])
    inp("masks", [64, 3, 64])
    inp("ident64", [64, 64], BF16)
    inp("ones64", [64, 1])
    inp("alibi", [4, 128, S])
    for nm, shp in [("lam_q1", [1, 64]), ("lam_k1", [1, 64]), ("lam_q2", [1, 64]), ("lam_k2", [1, 64]),
                    ("subln_w", [1, 128])]:
        inp(nm, shp)
    scr = {}

    def scratch(name, shape, dt):
        kind = "ExternalOutput" if name in dbg else "Internal"
        scr[name] = nc.dram_tensor(name, list(shape), dt, kind=kind).ap()

    scratch("zs_tm", [T, SHIFT_COLS], F32)
    scratch("zv_fm", [512, T], F32)
    scratch("qk_fm", [1024, T], BF16)
    scratch("av_tm", [T, 512], BF16)
    scratch("sg_fm", [2048, T], BF16)
    if not cfg.get("chunked", True):
        scratch("rw_tm", [T, 5, 512], F32)
    scratch("ab_tm", [T, 5, 512], BF16)
    scratch("lw_tm", [T, 512], F32)
    scratch("v_fm", [512, T], F32)
    scratch("g_fm", [512, T], F32)
    scratch("coef_fm", [8, T], F32)
    scratch("y_fm", [512, T], F32)
    scratch("ya_fm", [512, T], BF16)
    scr["d_p1"] = Dep()
    scr["d_p2a"] = Dep()
    scr["d_p2b"] = Dep()
    scr["d_p2c"] = Dep()
    scr["d_p3"] = Dep()
    scr["d_p4"] = Dep()
    scr["d_uv"] = Dep()
    scratch("uv_tab", [16384, 2 * D], BF16)
    if "ids_dbg" in dbg:
        scratch("ids_dbg", [T, 128], I32)
        scratch("gate_dbg", [T, 128], F32)
    inp("peer_wq", [1, D, 2048])
    inp("peer_keys", [1, 8, 2, 128, 128])
    inp("peer_u", [1, 16384, D])
    inp("peer_v", [1, 16384, D])
    inp("final_norm_w", [1, D])
    inp("iota16", [128, 16])
    io["out"] = nc.dram_tensor("out", [T, D], F32, kind="ExternalOutput").ap()
    scratch("x1_tm", [T, D], F32)
    scratch("xh_tm", [T, D], F32)
    scratch("xhT_fm", [D, T], BF16)
    for nm, shp in [("proj_a", [1, 512, D]), ("proj_b", [1, 512, D]), ("w_out", [1, D, D]), ("norm_ffn_w", [1, D])]:
        inp(nm, shp)
    scratch("yb_fm", [512, T], BF16)
    for nm, shp in [("shift_mu", [1, SHIFT_COLS]), ("w0", [1, 512]), ("w2", [1, 64, 512]), ("a0", [1, 512]),
                    ("a2", [1, 64, 512]), ("g2", [1, 128, 512]), ("k_k", [1, 512]), ("k_a", [1, 512]),
                    ("r_k", [1, 8, 64]), ("lnx_w", [1, 512]), ("lnx_b", [1, 512])]:
        inp(nm, shp)
    with ExitStack() as es:
        K = Kern(nc, es, pool_slots=cfg.get("pool_slots", 8))
        K.scopes = bool(cfg.get("scopes", False))
        if 1 in phases:
            K.phase = "p1_inproj"
            phase1(K, cfg, io, scr)
            K.barrier()
        if 5 in phases:
            K.phase = "p0_uvtab"
            RB = 2048
            for r0 in range(0, 16384, RB):
                K.pool.dma(lambda e, r0=r0: e.dma_start(out=scr["uv_tab"][r0:r0 + RB, 0:D], in_=io["peer_u"][0, r0:r0 + RB, :]),
                           adds=[scr["d_uv"]])
                K.pool.dma(lambda e, r0=r0: e.dma_start(out=scr["uv_tab"][r0:r0 + RB, D:2 * D], in_=io["peer_v"][0, r0:r0 + RB, :]),
                           adds=[scr["d_uv"]])
        if 2 in phases:
            K.phase = "p2a_prep"
            phase2_prep(K, cfg, io, scr)
            K.barrier()
            K.phase = "p2b_scan"
            if cfg.get("chunked", True):
                phase2_chunk(K, cfg, io, scr)
            else:
                phase2_scan(K, cfg, io, scr)
            K.barrier()
            K.phase = "p2c_post"
            phase2_post(K, cfg, io, scr)
            K.barrier()
        if 3 in phases:
            K.phase = "p3_attn"
            phase3(K, cfg, io, scr)
            K.barrier()
        if 4 in phases:
            K.phase = "p4_merge"
            phase4(K, cfg, io, scr)
            K.barrier()
        if 5 in phases:
            K.phase = "p5_peer"
            phase5(K, cfg, io, scr)
            K.barrier()
        K.finish()
    return nc, io, scr


def kernel(**inputs):
    NB, S = 4, 2048
    cfg = dict(NB=NB, S=S, phases=(1, 2, 3, 4, 5))
    nc, io, scr = build(cfg)
    consts = make_consts()
    consts["alibi"] = make_alibi(S)
    x = np.ascontiguousarray(np.asarray(inputs["x"], dtype=np.float32))
    shared = {}
    for name in io:
        if name in ("x", "out"):
            continue
        if name in consts:
            shared[name] = consts[name]
        elif name == "final_norm_w":
            shared[name] = np.ascontiguousarray(np.asarray(inputs[name], dtype=np.float32).reshape(1, D))
        else:
            shared[name] = np.ascontiguousarray(np.asarray(inputs[name], dtype=np.float32))
    in_maps = []
    for c in range(NCORES):
        m = dict(shared)
        m["x"] = x[c * NB:(c + 1) * NB].reshape(NB * S, D)
        in_maps.append(m)
    res = run_bass_kernel_spmd(nc, in_maps, core_ids=list(range(NCORES)))
    out = np.concatenate([np.asarray(r["out"]).reshape(NB, S, D) for r in res.results], axis=0)
    return out.astype(np.float32)
```

```python
import numpy as np
import ml_dtypes
from contextlib import ExitStack
import concourse.bass as bass
import concourse.mybir as mybir
from concourse.bass_utils import run_bass_kernel_spmd

F32 = mybir.dt.float32
BF16 = mybir.dt.bfloat16
I32 = mybir.dt.int32
U32 = mybir.dt.uint32
ALU = mybir.AluOpType
AF = mybir.ActivationFunctionType
AX = mybir.AxisListType

D = 1024
IN_COLS = 5376
SHIFT_COLS = 1792
NCORES = 8


class Dep:
    __slots__ = ("w", "r", "pw", "pr")

    def __init__(self):
        self.w = {}
        self.r = {}
        self.pw = {}
        self.pr = {}


class Stream:
    def __init__(self, K, name, is_pe=False, ndma=0):
        self.K = K
        self.name = name
        self.sem = K.new_sem("s_" + name)
        self.cnt = 0
        self.items = []
        self.waited = {}
        self.is_pe = is_pe
        self.dsems = [K.new_sem("d_%s%d" % (name, i)) for i in range(ndma)]
        self.duses = [0] * ndma
        self.dj = 0

    def wait_tok(self, tok):
        if tok is None:
            return
        sem, val = tok
        if sem is self.sem and self.is_pe:
            return
        key = id(sem)
        if self.waited.get(key, 0) >= val:
            return
        self.waited[key] = val
        self.items.append(("w", sem, val, self.K.phase))

    def _pre(self, reads, writes, adds):
        for d in reads:
            for t in list(d.w.values()):
                self.wait_tok(t)
        for d in writes:
            for t in list(d.w.values()):
                self.wait_tok(t)
            for t in list(d.r.values()):
                self.wait_tok(t)
        for d in adds:
            for t in list(d.r.values()) + list(d.pr.values()) + list(d.pw.values()):
                self.wait_tok(t)

    def _post(self, tok, reads, writes, adds):
        for d in reads:
            d.r[id(tok[0])] = tok
        for d in writes:
            d.pw = d.w
            d.pr = d.r
            d.w = {id(tok[0]): tok}
            d.r = {}
        for d in adds:
            d.w[id(tok[0])] = tok

    def op(self, fn, reads=(), writes=(), adds=()):
        self._pre(reads, writes, adds)
        self.cnt += 1
        tok = (self.sem, self.cnt)
        self.items.append(("o", fn, self.sem, 1, self.K.phase))
        self._post(tok, reads, writes, adds)
        return tok

    def dma(self, fn, reads=(), writes=(), adds=()):
        self._pre(reads, writes, adds)
        n = len(self.dsems)
        slot = self.dj % n
        self.dj += 1
        if self.duses[slot] > 0:
            self.wait_tok((self.dsems[slot], 16 * self.duses[slot]))
        self.duses[slot] += 1
        tok = (self.dsems[slot], 16 * self.duses[slot])
        self.items.append(("o", fn, self.dsems[slot], 16, self.K.phase))
        self._post(tok, reads, writes, adds)
        return tok

    def replay(self, eng):
        nc = self.K.nc
        cur = None
        ctx = None
        for it in self.items:
            ph = it[-1]
            if self.K.scopes and ph != cur:
                if ctx is not None:
                    ctx.__exit__(None, None, None)
                ctx = nc.named_scope(ph)
                ctx.__enter__()
                cur = ph
            if it[0] == "w":
                eng.wait_ge(it[1], it[2])
            else:
                ins = it[1](eng)
                ins.then_inc(it[2], it[3])
        if ctx is not None:
            ctx.__exit__(None, None, None)


class Kern:
    def __init__(self, nc, es, pool_slots=8):
        self.nc = nc
        self.es = es
        self.nsem = 0
        self.phase = "init"
        self.scopes = False
        self.pe = Stream(self, "pe", is_pe=True)
        self.act = Stream(self, "act", ndma=4)
        self.dve = Stream(self, "dve")
        self.pool = Stream(self, "pool", ndma=pool_slots)
        self.sp = Stream(self, "sp", ndma=8)
        self.uid = 0

    def new_sem(self, name):
        self.nsem += 1
        return self.es.enter_context(self.nc.semaphore(name))

    def sb(self, shape, dt, name=None, es=None):
        self.uid += 1
        nm = "%s_%d" % (name or "t", self.uid)
        return (es or self.es).enter_context(self.nc.sbuf_tensor(nm, list(shape), dt))

    def ps(self, shape, dt, name=None, es=None):
        self.uid += 1
        nm = "%s_%d" % (name or "p", self.uid)
        return (es or self.es).enter_context(self.nc.psum_tensor(nm, list(shape), dt))

    def dram(self, name, shape, dt, kind="Internal"):
        return self.nc.dram_tensor(name, list(shape), dt, kind=kind)

    def streams(self):
        return [self.pe, self.act, self.dve, self.pool, self.sp]

    def barrier(self):
        st = self.streams()
        toks = []
        for q in st:
            if q.cnt > 0:
                toks.append((q.sem, q.cnt))
            for i, sem in enumerate(q.dsems):
                if q.duses[i] > 0:
                    toks.append((sem, 16 * q.duses[i]))
        for s_ in st:
            for t in toks:
                s_.wait_tok(t)

    def finish(self):
        streams = [self.pe, self.act, self.dve, self.pool, self.sp]
        for s in streams:
            for q in streams:
                for i, sem in enumerate(q.dsems):
                    if q.duses[i] > 0:
                        s.wait_tok((sem, 16 * q.duses[i]))
        with self.nc.allow_non_contiguous_dma(reason="small strided param loads"), self.nc.Block() as block:
            @block.tensor
            def _(e):
                self.pe.replay(e)

            @block.scalar
            def _(e):
                self.act.replay(e)

            @block.vector
            def _(e):
                self.dve.replay(e)

            @block.gpsimd
            def _(e):
                self.pool.replay(e)

            @block.sync
            def _(e):
                self.sp.replay(e)


class Rot:
    def __init__(self, K, n, shape, dt, name, psum=False, es=None):
        self.t = [(K.ps if psum else K.sb)(shape, dt, name, es=es) for _ in range(n)]
        self.d = [Dep() for _ in range(n)]
        self.i = 0

    def next(self):
        j = self.i % len(self.t)
        self.i += 1
        return self.t[j], self.d[j]


def phase1(K, cfg, io, scr):
    nc = K.nc
    T = cfg["T"]
    NT = T // 512
    with ExitStack() as es:
        ident = K.sb([128, 128], BF16, "ident", es)
        d_ident = Dep()
        K.sp.dma(lambda e: e.dma_start(out=ident[:], in_=io["ident_bf"][:, :]), writes=[d_ident])
        nw = K.sb([128, 8], F32, "nw", es)
        d_nw = Dep()
        K.sp.dma(lambda e: e.dma_start(out=nw[:], in_=io["norm_mix_w"].rearrange("o (c p) -> p (o c)", p=128)),
                 writes=[d_nw])
        wt = K.sb([128, 8, IN_COLS], BF16, "wt", es)
        d_wt = Dep()
        wst = Rot(K, 2, [128, 1344], F32, "wst", es=es)
        q = 0
        for kc in range(8):
            for cp in range(4):
                st, dst = wst.next()
                eng = K.sp if q % 2 == 0 else K.pool
                q += 1
                eng.dma(lambda e, st=st, kc=kc, cp=cp: e.dma_start(
                    out=st[:], in_=io["w_in"][0, kc * 128:(kc + 1) * 128, cp * 1344:(cp + 1) * 1344]), writes=[dst])
                K.act.op(lambda e, st=st, kc=kc, cp=cp: e.activation(
                    out=wt[:, kc, cp * 1344:(cp + 1) * 1344], in_=st[:], func=AF.Copy, scale=nw[:, kc:kc + 1]),
                    reads=[dst, d_nw], writes=[d_wt])

        xs = Rot(K, 2, [128, D], F32, "xs", es=es)
        junk = K.sb([128, D], BF16, "junk", es)
        d_junk = Dep()
        xn = Rot(K, 2, [128, D], BF16, "xn", es=es)
        st4 = Rot(K, 4, [128, 4], F32, "st4", es=es)
        hT = Rot(K, 2, [128, 8, 512], BF16, "hT", es=es)
        ptr = Rot(K, 2, [128, 8, 128], BF16, "ptr", psum=True, es=es)
        pmm = Rot(K, 4, [128, 512], F32, "pmm", psum=True, es=es)
        o32 = Rot(K, 3, [128, 512], F32, "o32", es=es)
        o16 = Rot(K, 3, [128, 512], BF16, "o16", es=es)
        ev = [0]

        def evac(pt, pd, ncols, kind, dst_ap):
            if kind == "f32":
                ot, od = o32.next()
            else:
                ot, od = o16.next()
            use_act = (kind == "sig") or (ev[0] % 2 == 0)
            ev[0] += 1
            if kind == "sig":
                K.act.op(lambda e: e.activation(out=ot[:, :ncols], in_=pt[:, :ncols], func=AF.Sigmoid),
                         reads=[pd], writes=[od])
            elif use_act:
                K.act.op(lambda e: e.activation(out=ot[:, :ncols], in_=pt[:, :ncols], func=AF.Copy),
                         reads=[pd], writes=[od])
            else:
                K.dve.op(lambda e: e.tensor_copy(out=ot[:, :ncols], in_=pt[:, :ncols]), reads=[pd], writes=[od])
            K.sp.dma(lambda e: e.dma_start(out=dst_ap, in_=ot[:, :ncols]), reads=[od], adds=[scr["d_p1"]])

        for ti in range(NT):
            h_t, h_d = hT.next()
            for sub in range(4):
                t0 = ti * 512 + sub * 128
                x_t, x_d = xs.next()
                K.pool.dma(lambda e, x_t=x_t, t0=t0: e.dma_start(out=x_t[:], in_=io["x"][t0:t0 + 128, :]),
                           writes=[x_d])
                s_t, s_d = st4.next()
                K.dve.op(lambda e, s_t=s_t: e.memset(s_t[:], 0.0), writes=[s_d])
                K.act.op(lambda e, x_t=x_t, s_t=s_t: e.activation(out=junk[:], in_=x_t[:], func=AF.Square,
                                                                    accum_out=s_t[:, 0:1]),
                         reads=[x_d], writes=[d_junk, s_d])
                K.dve.op(lambda e, s_t=s_t: e.tensor_scalar(out=s_t[:, 1:2], in0=s_t[:, 0:1], scalar1=1.0 / D,
                                                            scalar2=1e-6, op0=ALU.mult, op1=ALU.add),
                         reads=[s_d], writes=[s_d])
                K.act.op(lambda e, s_t=s_t: e.activation(out=s_t[:, 3:4], in_=s_t[:, 1:2], func=AF.Sqrt),
                         reads=[s_d], writes=[s_d])
                K.dve.op(lambda e, s_t=s_t: e.reciprocal(out=s_t[:, 2:3], in_=s_t[:, 3:4]),
                         reads=[s_d], writes=[s_d])
                n_t, n_d = xn.next()
                K.dve.op(lambda e, x_t=x_t, s_t=s_t, n_t=n_t: e.tensor_scalar(
                    out=n_t[:], in0=x_t[:], scalar1=s_t[:, 2:3], scalar2=None, op0=ALU.mult),
                    reads=[x_d, s_d], writes=[n_d])
                p_t, p_d = ptr.next()
                for kc in range(8):
                    K.pe.op(lambda e, p_t=p_t, n_t=n_t, kc=kc: e.transpose(
                        out=p_t[:, kc, :], in_=n_t[:, kc * 128:(kc + 1) * 128], identity=ident[:]),
                        reads=[n_d, d_ident], writes=[p_d])
                K.act.op(lambda e, p_t=p_t, h_t=h_t, sub=sub: e.activation(
                    out=h_t[:, :, sub * 128:(sub + 1) * 128], in_=p_t[:], func=AF.Copy),
                    reads=[p_d], writes=[h_d])
            tsl = slice(ti * 512, (ti + 1) * 512)
            for sub in range(4):
                r0 = ti * 512 + sub * 128
                for (c0, ncols, kind, name, dc0) in [(0, 512, "f32", "zs_tm", 0), (512, 512, "f32", "zs_tm", 512),
                                                    (1024, 512, "f32", "zs_tm", 1024),
                                                    (1536, 256, "f32", "zs_tm", 1536),
                                                    (2816, 512, "bf16", "av_tm", 0)]:
                    pt, pd = pmm.next()
                    for kc in range(8):
                        K.pe.op(lambda e, pt=pt, kc=kc, sub=sub, c0=c0, ncols=ncols, h_t=h_t: e.matmul(
                            pt[:, :ncols], h_t[:, kc, sub * 128:(sub + 1) * 128], wt[:, kc, c0:c0 + ncols],
                            start=(kc == 0), stop=(kc == 7)), reads=[h_d, d_wt], writes=[pd])
                    evac(pt, pd, ncols, kind, scr[name][r0:r0 + 128, dc0:dc0 + ncols])
            fm = []
            for j in range(8):
                fm.append((1792 + j * 128, "bf16", "qk_fm", j * 128))
            for j in range(16):
                fm.append((3328 + j * 128, "sig", "sg_fm", j * 128))
            for (c0, kind, name, r0) in fm:
                pt, pd = pmm.next()
                for kc in range(8):
                    K.pe.op(lambda e, pt=pt, kc=kc, c0=c0, h_t=h_t: e.matmul(
                        pt[:, :], wt[:, kc, c0:c0 + 128], h_t[:, kc, :], start=(kc == 0), stop=(kc == 7)),
                        reads=[h_d, d_wt], writes=[pd])
                evac(pt, pd, 512, kind, scr[name][r0:r0 + 128, tsl])


def dap(apobj, offset, dims):
    return bass.AP(tensor=apobj.tensor, offset=offset, ap=[list(d) for d in dims])


def bcast_load(K, eng, dst, src_row_ap, n, dep):
    eng.dma(lambda e: e.dma_start(out=dst, in_=src_row_ap.broadcast_to([128, n])), writes=[dep])


def phase2_prep(K, cfg, io, scr):
    T, S, NB = cfg["T"], cfg["S"], cfg["NB"]
    NTT = T // 128
    with ExitStack() as es:
        identb = K.sb([128, 128], BF16, "identb", es)
        identf = K.sb([128, 128], F32, "identf", es)
        d_c = Dep()
        K.sp.dma(lambda e: e.dma_start(out=identb[:], in_=io["ident_bf"][:, :]), adds=[d_c])
        K.sp.dma(lambda e: e.dma_start(out=identf[:], in_=io["ident_f"][:, :]), adds=[d_c])
        MU = K.sb([128, SHIFT_COLS], F32, "MU", es)
        PR = K.sb([128, 5, 512], F32, "PR", es)
        K.sp.dma(lambda e: e.dma_start(out=MU[:], in_=io["shift_mu"][0:1, :].broadcast_to([128, SHIFT_COLS])), adds=[d_c])
        for j, nm in enumerate(["w0", "a0", "k_k", "k_a"]):
            K.pool.dma(lambda e, j=j, nm=nm: e.dma_start(out=PR[:, j, :], in_=io[nm][0:1, :].broadcast_to([128, 512])),
                       adds=[d_c])
        K.pool.dma(lambda e: e.dma_start(out=PR[:, 4, :], in_=io["r_k"].rearrange("o h k -> o (h k)").broadcast_to([128, 512])),
                   adds=[d_c])
        cst = K.sb([128, 2], F32, "cst", es)
        K.dve.op(lambda e: e.memset(cst[:, 0:1], 1.0), adds=[d_c])
        K.dve.op(lambda e: e.memset(cst[:, 1:2], -0.5), adds=[d_c])
        wst = K.sb([128, 3, 512], F32, "lwst", es)
        d_wst = Dep()
        K.dve.op(lambda e: e.memset(wst[:], 0.0), writes=[d_wst])
        K.sp.dma(lambda e: e.dma_start(out=wst[0:64, 0, :], in_=io["w2"][0, :, :]), reads=[d_wst], adds=[d_wst])
        K.sp.dma(lambda e: e.dma_start(out=wst[64:128, 1, :], in_=io["a2"][0, :, :]), reads=[d_wst], adds=[d_wst])
        K.sp.dma(lambda e: e.dma_start(out=wst[:, 2, :], in_=io["g2"][0, :, :]), reads=[d_wst], adds=[d_wst])
        LW = K.sb([128, 3, 512], BF16, "LW", es)
        K.dve.op(lambda e: e.tensor_copy(out=LW[:], in_=wst[:]), reads=[d_wst], adds=[d_c])

        Zr = Rot(K, 2, [128, SHIFT_COLS], F32, "Z", es=es)
        Zpr = Rot(K, 2, [128, SHIFT_COLS], F32, "Zp", es=es)
        ZSr = Rot(K, 2, [128, SHIFT_COLS], F32, "ZS", es=es)
        OUTr = Rot(K, 2, [128, 5, 512], F32, "OUT", es=es)
        Er = Rot(K, 2, [128, 192], F32, "E", es=es)
        Lr = Rot(K, 2, [128, 256], BF16, "L", es=es)
        LTr = Rot(K, 2, [128, 2, 128], BF16, "LT", es=es)
        Ur = Rot(K, 2, [128, 512], F32, "U", es=es)
        UAr = Rot(K, 2, [128, 512], F32, "UA", es=es)
        KKr = Rot(K, 2, [128, 512], F32, "KKt", es=es)
        SQr = Rot(K, 2, [128, 512], F32, "SQ", es=es)
        T1r = Rot(K, 2, [128, 512], F32, "T1", es=es)
        T2r = Rot(K, 2, [128, 512], F32, "T2", es=es)
        S8r = Rot(K, 2, [128, 4, 8], F32, "S8", es=es)
        VTr = Rot(K, 2, [128, 4, 128], F32, "VT", es=es)
        GTr = Rot(K, 2, [128, 4, 128], F32, "GT", es=es)
        CTr = Rot(K, 2, [8, 128], F32, "CT", es=es)
        PT = Rot(K, 1, [128, 2, 128], BF16, "PT", psum=True, es=es)
        PW = Rot(K, 1, [128, 512], F32, "PW", psum=True, es=es)
        PA = Rot(K, 1, [128, 512], F32, "PA", psum=True, es=es)
        PG = Rot(K, 1, [128, 4, 128], F32, "PG", psum=True, es=es)
        PV = Rot(K, 1, [128, 4, 128], F32, "PV", psum=True, es=es)
        PC = Rot(K, 1, [8, 128], F32, "PC", psum=True, es=es)
        chunked = cfg.get("chunked", True)
        if chunked:
            PL = Rot(K, 1, [128, 512], F32, "PL", psum=True, es=es)
            TRI = K.sb([128, 128], F32, "TRI", es)
            K.sp.dma(lambda e: e.dma_start(out=TRI[:], in_=io["tri"][:, :]), adds=[d_c])
            LWr = Rot(K, 2, [128, 512], F32, "LWt", es=es)
            ELr = Rot(K, 2, [128, 3, 512], F32, "EL", es=es)
            ABr = Rot(K, 2, [128, 5, 512], BF16, "AB", es=es)
        dq = [0]

        def ldq():
            dq[0] += 1
            return K.sp if dq[0] % 2 == 0 else K.pool

        def tile_gen(ti):
            t0 = ti * 128
            first = (t0 % S == 0)
            Z, dZ = Zr.next()
            Zp, dZp = Zpr.next()
            ZS, dZS = ZSr.next()
            OUT, dO = OUTr.next()
            ldq().dma(lambda e, Z=Z, t0=t0: e.dma_start(out=Z[:], in_=scr["zs_tm"][t0:t0 + 128, :]),
                      reads=[scr["d_p1"]], writes=[dZ])
            if first:
                K.pool.op(lambda e, Zp=Zp: e.memset(Zp[0:32, :], 0.0), writes=[dZp])
                ldq().dma(lambda e, Zp=Zp, t0=t0: e.dma_start(out=Zp[1:128, :], in_=scr["zs_tm"][t0:t0 + 127, :]),
                          reads=[scr["d_p1"], dZp], adds=[dZp])
            else:
                ldq().dma(lambda e, Zp=Zp, t0=t0: e.dma_start(out=Zp[:], in_=scr["zs_tm"][t0 - 1:t0 + 127, :]),
                          reads=[scr["d_p1"]], writes=[dZp])
            CS = 1216
            dZSa, dZSb = Dep(), Dep()
            K.dve.op(lambda e, Z=Z, Zp=Zp, ZS=ZS: e.tensor_tensor(out=ZS[:, :CS], in0=Zp[:, :CS], in1=Z[:, :CS], op=ALU.subtract),
                     reads=[dZ, dZp], writes=[dZS])
            K.pool.op(lambda e, Z=Z, Zp=Zp, ZS=ZS: e.tensor_tensor(out=ZS[:, CS:], in0=Zp[:, CS:], in1=Z[:, CS:], op=ALU.subtract),
                      reads=[dZ, dZp, dZS], writes=[dZSb])
            K.dve.op(lambda e, ZS=ZS: e.tensor_tensor(out=ZS[:, :CS], in0=ZS[:, :CS], in1=MU[:, :CS], op=ALU.mult),
                     reads=[d_c, dZS], writes=[dZSa])
            K.pool.op(lambda e, ZS=ZS: e.tensor_tensor(out=ZS[:, CS:], in0=ZS[:, CS:], in1=MU[:, CS:], op=ALU.mult),
                      reads=[d_c], writes=[dZSb])
            K.dve.op(lambda e, Z=Z, ZS=ZS: e.tensor_tensor(out=ZS[:, :CS], in0=ZS[:, :CS], in1=Z[:, :CS], op=ALU.add),
                     reads=[dZ], writes=[dZSa])
            K.pool.op(lambda e, Z=Z, ZS=ZS: e.tensor_tensor(out=ZS[:, CS:], in0=ZS[:, CS:], in1=Z[:, CS:], op=ALU.add),
                      reads=[dZ], writes=[dZSb])
            K.dve.op(lambda e, ZS=ZS: e.tensor_copy(out=ZS[:, 0:1], in_=ZS[:, 0:1]), reads=[dZSa, dZSb], writes=[dZS])
            yield
            r_ap = ZS[:, 0:512]
            k_ap = ZS[:, 512:1024]
            K.act.op(lambda e, OUT=OUT, ZS=ZS: e.activation(out=OUT[:, 4, :], in_=ZS[:, 0:512], func=AF.Copy),
                     reads=[dZS], writes=[dO])
            yield
            E, dE = Er.next()
            L, dL = Lr.next()
            K.act.op(lambda e, E=E, ZS=ZS: e.activation(out=E[:, 0:64], in_=ZS[:, 1536:1600], func=AF.Exp, scale=-2.0),
                     reads=[dZS], writes=[dE])
            K.act.op(lambda e, E=E, ZS=ZS: e.activation(out=E[:, 64:192], in_=ZS[:, 1664:1792], func=AF.Exp, scale=-1.0),
                     reads=[dZS], adds=[dE])
            K.act.op(lambda e, E=E: e.activation(out=E[:], in_=E[:], func=AF.Ln, bias=cst[:, 0:1]), reads=[d_c], writes=[dE])
            K.act.op(lambda e, E=E: e.activation(out=E[:], in_=E[:], func=AF.Exp, scale=-1.0), writes=[dE])
            yield
            K.dve.op(lambda e, E=E, L=L: e.tensor_scalar(out=L[:, 0:64], in0=E[:, 0:64], scalar1=2.0, scalar2=-1.0,
                                                        op0=ALU.mult, op1=ALU.add), reads=[dE], writes=[dL])
            K.act.op(lambda e, L=L, ZS=ZS: e.activation(out=L[:, 64:128], in_=ZS[:, 1600:1664], func=AF.Copy),
                     reads=[dZS, dL], adds=[dL])
            K.act.op(lambda e, L=L, E=E: e.activation(out=L[:, 128:256], in_=E[:, 64:192], func=AF.Copy),
                     reads=[dE, dL], adds=[dL])
            yield
            pt, dpt = PT.next()
            K.pe.op(lambda e, pt=pt, L=L: e.transpose(out=pt[:, 0, :], in_=L[:, 0:128], identity=identb[:]),
                    reads=[dL, d_c], writes=[dpt])
            K.pe.op(lambda e, pt=pt, L=L: e.transpose(out=pt[:, 1, :], in_=L[:, 128:256], identity=identb[:]),
                    reads=[dL, d_c], adds=[dpt])
            LT, dLT = LTr.next()
            K.act.op(lambda e, pt=pt, LT=LT: e.activation(out=LT[:], in_=pt[:], func=AF.Copy), reads=[dpt], writes=[dLT])
            yield "pre_pw"
            pw, dpw = PW.next()
            pa, dpa = PA.next()
            pg, dpg = PG.next()
            K.pe.op(lambda e, pw=pw, LT=LT: e.matmul(pw[:], LT[:, 0, :], LW[:, 0, :], start=True, stop=True),
                    reads=[dLT, d_c], writes=[dpw])
            K.pe.op(lambda e, pa=pa, LT=LT: e.matmul(pa[:], LT[:, 0, :], LW[:, 1, :], start=True, stop=True),
                    reads=[dLT, d_c], writes=[dpa])
            for j in range(4):
                K.pe.op(lambda e, pg=pg, LT=LT, j=j: e.matmul(pg[:, j, :], LW[:, 2, j * 128:(j + 1) * 128], LT[:, 1, :],
                                                             start=True, stop=True),
                        reads=[dLT, d_c], writes=[dpg] if j == 0 else [], adds=[] if j == 0 else [dpg])
            GT, dGT = GTr.next()
            K.act.op(lambda e, pg=pg, GT=GT: e.activation(out=GT[:], in_=pg[:], func=AF.Copy), reads=[dpg], writes=[dGT])
            K.sp.dma(lambda e, GT=GT, t0=t0: e.dma_start(
                out=dap(scr["g_fm"], t0, [[T, 128], [128 * T, 4], [1, 128]]), in_=GT[:]),
                reads=[dGT], adds=[scr["d_p2a"]])
            yield
            U, dU = Ur.next()
            K.dve.op(lambda e, U=U, pw=pw: e.tensor_tensor(out=U[:], in0=pw[:], in1=PR[:, 0, :], op=ALU.add),
                     reads=[dpw, d_c], writes=[dU])
            yield
            K.act.op(lambda e, U=U: e.activation(out=U[:], in_=U[:], func=AF.Exp, scale=-1.0), writes=[dU])
            K.act.op(lambda e, U=U: e.activation(out=U[:], in_=U[:], func=AF.Ln, bias=cst[:, 0:1]), reads=[d_c], writes=[dU])
            yield
            K.act.op(lambda e, U=U: e.activation(out=U[:], in_=U[:], func=AF.Exp, scale=-1.0, bias=cst[:, 1:2]),
                     reads=[d_c], writes=[dU])
            K.act.op(lambda e, U=U, OUT=OUT: e.activation(out=OUT[:, 0, :], in_=U[:], func=AF.Exp, scale=-1.0),
                     reads=[dU], adds=[dO])
            yield
            UA, dUA = UAr.next()
            K.dve.op(lambda e, UA=UA, pa=pa: e.tensor_tensor(out=UA[:], in0=pa[:], in1=PR[:, 1, :], op=ALU.add),
                     reads=[dpa, d_c], writes=[dUA])
            yield
            K.act.op(lambda e, UA=UA: e.activation(out=UA[:], in_=UA[:], func=AF.Exp, scale=-1.0), writes=[dUA])
            K.act.op(lambda e, UA=UA: e.activation(out=UA[:], in_=UA[:], func=AF.Ln, bias=cst[:, 0:1]), reads=[d_c], writes=[dUA])
            K.act.op(lambda e, UA=UA: e.activation(out=UA[:], in_=UA[:], func=AF.Exp, scale=-1.0), writes=[dUA])
            yield "post_a"
            KKt, dKK = KKr.next()
            SQ, dSQ = SQr.next()
            S8, dS8 = S8r.next()
            K.dve.op(lambda e, KKt=KKt, ZS=ZS: e.tensor_tensor(out=KKt[:], in0=ZS[:, 512:1024], in1=PR[:, 2, :], op=ALU.mult),
                     reads=[dZS, d_c], writes=[dKK])
            K.pool.op(lambda e, KKt=KKt, SQ=SQ: e.tensor_tensor(out=SQ[:], in0=KKt[:], in1=KKt[:], op=ALU.mult),
                      reads=[dKK], writes=[dSQ])
            yield
            K.dve.op(lambda e, SQ=SQ, S8=S8: e.tensor_reduce(out=S8[:, 0, :], in_=SQ[:].rearrange("p (h k) -> p h k", k=64),
                                                            axis=AX.X, op=ALU.add), reads=[dSQ], writes=[dS8])
            K.dve.op(lambda e, S8=S8: e.tensor_scalar(out=S8[:, 0, :], in0=S8[:, 0, :], scalar1=1e-24, scalar2=None,
                                                      op0=ALU.max), writes=[dS8])
            K.act.op(lambda e, S8=S8: e.activation(out=S8[:, 1, :], in_=S8[:, 0, :], func=AF.Ln), writes=[dS8])
            K.act.op(lambda e, S8=S8: e.activation(out=S8[:, 2, :], in_=S8[:, 1, :], func=AF.Exp, scale=-0.5), writes=[dS8])
            yield
            K.dve.op(lambda e, KKt=KKt, S8=S8, OUT=OUT: e.tensor_tensor(
                out=OUT[:, 1, :].rearrange("p (h k) -> p h k", k=64), in0=KKt[:].rearrange("p (h k) -> p h k", k=64),
                in1=S8[:, 2, :].unsqueeze(2).broadcast_to([128, 8, 64]), op=ALU.mult),
                reads=[dKK, dS8, dO], adds=[dO])
            K.dve.op(lambda e, OUT=OUT, UA=UA: e.scalar_tensor_tensor(
                out=OUT[:, 2, :], in0=OUT[:, 1, :], scalar=-1.0, in1=UA[:], op0=ALU.mult, op1=ALU.mult),
                reads=[dUA, dO], adds=[dO])
            yield
            T1, dT1 = T1r.next()
            K.dve.op(lambda e, T1=T1, UA=UA: e.scalar_tensor_tensor(
                out=T1[:], in0=UA[:], scalar=-1.0, in1=PR[:, 3, :], op0=ALU.add, op1=ALU.mult),
                reads=[dUA, d_c], writes=[dT1])
            K.dve.op(lambda e, T1=T1, OUT=OUT, ZS=ZS: e.scalar_tensor_tensor(
                out=OUT[:, 3, :], in0=T1[:], scalar=1.0, in1=ZS[:, 512:1024], op0=ALU.add, op1=ALU.mult),
                reads=[dT1, dZS, dO], adds=[dO])
            T2, dT2 = T2r.next()
            K.pool.op(lambda e, T2=T2, OUT=OUT, ZS=ZS: e.tensor_tensor(out=T2[:], in0=OUT[:, 3, :], in1=ZS[:, 0:512], op=ALU.mult),
                      reads=[dO, dZS], writes=[dT2])
            K.pool.op(lambda e, T2=T2: e.tensor_tensor(out=T2[:], in0=T2[:], in1=PR[:, 4, :], op=ALU.mult),
                      reads=[d_c], writes=[dT2])
            yield
            K.dve.op(lambda e, T2=T2, S8=S8: e.tensor_reduce(out=S8[:, 3, :], in_=T2[:].rearrange("p (h k) -> p h k", k=64),
                                                            axis=AX.X, op=ALU.add), reads=[dT2], writes=[dS8])
            yield
            pv, dpv = PV.next()
            pc, dpc = PC.next()
            for j in range(4):
                K.pe.op(lambda e, pv=pv, ZS=ZS, j=j: e.transpose(out=pv[:, j, :], in_=ZS[:, 1024 + j * 128:1024 + (j + 1) * 128],
                                                                identity=identf[:]),
                        reads=[dZS, d_c], writes=[dpv] if j == 0 else [], adds=[] if j == 0 else [dpv])
            K.pe.op(lambda e, pc=pc, S8=S8: e.transpose(out=pc[:, :], in_=S8[:, 3, :], identity=identf[:]),
                    reads=[dS8, d_c], writes=[dpc])
            VT, dVT = VTr.next()
            CT, dCT = CTr.next()
            K.act.op(lambda e, pv=pv, VT=VT: e.activation(out=VT[:], in_=pv[:], func=AF.Copy), reads=[dpv], writes=[dVT])
            K.act.op(lambda e, pc=pc, CT=CT: e.activation(out=CT[:], in_=pc[:], func=AF.Copy), reads=[dpc], writes=[dCT])
            K.sp.dma(lambda e, VT=VT, t0=t0: e.dma_start(
                out=dap(scr["v_fm"], t0, [[T, 128], [128 * T, 4], [1, 128]]), in_=VT[:]),
                reads=[dVT], adds=[scr["d_p2a"]])
            K.sp.dma(lambda e, CT=CT, t0=t0: e.dma_start(out=scr["coef_fm"][:, t0:t0 + 128], in_=CT[:]),
                     reads=[dCT], adds=[scr["d_p2a"]])
            yield
            if not chunked:
                K.pool.dma(lambda e, OUT=OUT, t0=t0: e.dma_start(out=scr["rw_tm"][t0:t0 + 128, :, :], in_=OUT[:]),
                           reads=[dO], adds=[scr["d_p2a"]])
            else:
                LWt, dLW = LWr.next()
                K.dve.op(lambda e, LWt=LWt, U=U: e.tensor_scalar(out=LWt[:], in0=U[:], scalar1=-1.0, scalar2=None, op0=ALU.mult),
                         reads=[dU], writes=[dLW])
                pl, dpl = PL.next()
                K.pe.op(lambda e, pl=pl, LWt=LWt: e.matmul(pl[:], TRI[:], LWt[:], start=True, stop=True),
                        reads=[dLW, d_c], writes=[dpl])
                EL, dEL = ELr.next()
                K.act.op(lambda e, EL=EL, pl=pl: e.activation(out=EL[:, 0, :], in_=pl[:], func=AF.Exp), reads=[dpl], writes=[dEL])
                K.act.op(lambda e, EL=EL, pl=pl: e.activation(out=EL[:, 1, :], in_=pl[:], func=AF.Exp, scale=-1.0),
                         reads=[dpl], adds=[dEL])
                K.dve.op(lambda e, EL=EL, pl=pl, U=U: e.tensor_tensor(out=EL[:, 2, :], in0=pl[:], in1=U[:], op=ALU.add),
                         reads=[dpl, dU, dEL], adds=[dEL])
                K.act.op(lambda e, EL=EL: e.activation(out=EL[:, 2, :], in_=EL[:, 2, :], func=AF.Exp), reads=[dEL], adds=[dEL])
                AB, dAB = ABr.next()
                K.dve.op(lambda e, AB=AB, OUT=OUT, EL=EL: e.tensor_tensor(out=AB[:, 0, :], in0=OUT[:, 1, :], in1=EL[:, 2, :], op=ALU.mult),
                         reads=[dO, dEL], writes=[dAB])
                K.dve.op(lambda e, AB=AB, OUT=OUT, EL=EL: e.scalar_tensor_tensor(
                    out=AB[:, 1, :], in0=OUT[:, 2, :], scalar=-1.0, in1=EL[:, 1, :], op0=ALU.mult, op1=ALU.mult),
                    reads=[dO, dEL, dAB], adds=[dAB])
                K.pool.op(lambda e, AB=AB, OUT=OUT, EL=EL: e.tensor_tensor(out=AB[:, 2, :], in0=OUT[:, 3, :], in1=EL[:, 1, :], op=ALU.mult),
                          reads=[dO, dEL, dAB], adds=[dAB])
                K.pool.op(lambda e, AB=AB, OUT=OUT, EL=EL: e.tensor_tensor(out=AB[:, 3, :], in0=OUT[:, 4, :], in1=EL[:, 0, :], op=ALU.mult),
                          reads=[dO, dEL, dAB], adds=[dAB])
                K.act.op(lambda e, AB=AB, ZS=ZS: e.activation(out=AB[:, 4, :], in_=ZS[:, 1024:1536], func=AF.Copy),
                         reads=[dZS, dAB], adds=[dAB])
                K.pool.dma(lambda e, AB=AB, t0=t0: e.dma_start(out=scr["ab_tm"][t0:t0 + 128, :, :], in_=AB[:]),
                           reads=[dAB], adds=[scr["d_p2a"]])
                K.sp.dma(lambda e, LWt=LWt, t0=t0: e.dma_start(out=scr["lw_tm"][t0:t0 + 128, :], in_=LWt[:]),
                         reads=[dLW], adds=[scr["d_p2a"]])

        active = []
        nxt = 0
        while nxt < NTT or active:
            if len(active) < 2 and nxt < NTT and (not active or active[0][2] or active[0][3] >= 2):
                active.append([tile_gen(nxt), None, False, 0])
                nxt += 1
            for idx, a_ in enumerate(list(active)):
                if a_[1] == "pre_pw" and idx > 0 and not active[0][2]:
                    continue
                try:
                    tok = next(a_[0])
                    a_[1] = tok
                    a_[3] += 1
                    if tok == "post_a":
                        a_[2] = True
                except StopIteration:
                    active.remove(a_)


def phase2_scan(K, cfg, io, scr):
    T, S, NB = cfg["T"], cfg["S"], cfg["NB"]
    NBH = 2 if NB >= 2 else 1
    NBL = NB // NBH
    NP = 64 * NBH
    TS = 2
    TC = 128
    RW = 2560
    with ExitStack() as es:
        St = K.sb([128, NBL, 8, 64], F32, "St", es)
        dS = Dep()
        TMP = K.sb([128, NBL, 8, 64], F32, "TMP", es)
        dT = Dep()
        SA = K.sb([128, NBL, 8], F32, "SA", es)
        dSA = Dep()
        T2r = Rot(K, 2, [128, NBL, 8, 64], F32, "TMP2", es=es)
        T3r = Rot(K, 2, [128, NBL, 8, 64], F32, "TMP3", es=es)
        BCr = Rot(K, 3, [128, TS, NBL, 5, 8, 64], F32, "BC", es=es)
        Vr = Rot(K, 2, [128, NBL, 8, TC], F32, "Vf", es=es)
        Yr = Rot(K, 2, [128, NBL, 8, TC], F32, "Yf", es=es)
        K.dve.op(lambda e: e.memset(St[:], 0.0), writes=[dS])
        qi = [0]

        def q():
            qi[0] += 1
            return K.sp if qi[0] % 2 == 0 else K.act

        def load_bc(ci):
            t = ci * TS
            BC, dBC = BCr.next()
            first = True
            for bhi in range(NBH):
                for blo in range(NBL):
                    src = dap(scr["rw_tm"], ((bhi * NBL + blo) * S + t) * RW, [[0, 64], [RW, TS], [1, RW]])
                    dst = BC[bhi * 64:(bhi + 1) * 64, :, blo].rearrange("p t j h k -> p t (j h k)")
                    q().dma(lambda e, src=src, dst=dst: e.dma_start(out=dst, in_=src), reads=[scr["d_p2a"]],
                            writes=[dBC] if first else [], adds=[] if first else [dBC])
                    first = False
            return BC, dBC

        def vy_ap(name, bhi, blo, t):
            return dap(scr[name], (bhi * NBL + blo) * S + t, [[T, 64], [64 * T, 8], [1, TC]])

        def load_v(ni):
            Vf, dV = Vr.next()
            first = True
            for bhi in range(NBH):
                for blo in range(NBL):
                    src = vy_ap("v_fm", bhi, blo, ni * TC)
                    dst = Vf[bhi * 64:(bhi + 1) * 64, blo]
                    q().dma(lambda e, src=src, dst=dst: e.dma_start(out=dst, in_=src), reads=[scr["d_p2a"]],
                            writes=[dV] if first else [], adds=[] if first else [dV])
                    first = False
            return Vf, dV

        nch = S // TS
        bcs = {}
        bcs[0] = load_bc(0)
        if nch > 1:
            bcs[1] = load_bc(1)
        vs = {0: load_v(0)}
        P = slice(0, NP)
        for t in range(S):
            ci, ts = divmod(t, TS)
            ni, tt = divmod(t, TC)
            if ts == 0 and ci + 2 < nch:
                bcs[ci + 2] = load_bc(ci + 2)
            if tt == 0:
                if (ni + 1) * TC < S:
                    vs[ni + 1] = load_v(ni + 1)
                Yf, dY = Yr.next()
            BC, dBC = bcs[ci]
            Vf, dV = vs[ni]
            W_ = BC[P, ts, :, 0]
            KN = BC[P, ts, :, 1]
            KA = BC[P, ts, :, 2]
            KP = BC[P, ts, :, 3]
            R_ = BC[P, ts, :, 4]
            shp = [NP, NBL, 8, 64]
            K.dve.op(lambda e, KN=KN: e.tensor_tensor(out=TMP[P], in0=St[P], in1=KN, op=ALU.mult),
                     reads=[dS, dBC], writes=[dT])
            K.dve.op(lambda e: e.tensor_reduce(out=SA[P], in_=TMP[P], axis=AX.X, op=ALU.add), reads=[dT], writes=[dSA])
            K.dve.op(lambda e, W_=W_: e.tensor_tensor(out=St[P], in0=St[P], in1=W_, op=ALU.mult),
                     reads=[dBC], writes=[dS])
            K.dve.op(lambda e, KA=KA: e.tensor_tensor(out=TMP[P], in0=KA, in1=SA[P].unsqueeze(3).broadcast_to(shp),
                                                     op=ALU.mult), reads=[dBC, dSA], writes=[dT])
            K.dve.op(lambda e: e.tensor_tensor(out=St[P], in0=St[P], in1=TMP[P], op=ALU.add), reads=[dT], writes=[dS])
            T2, dT2 = T2r.next()
            K.pool.op(lambda e, KP=KP, T2=T2, Vf=Vf, tt=tt: e.tensor_tensor(
                out=T2[P], in0=KP, in1=Vf[P, :, :, tt:tt + 1].broadcast_to(shp), op=ALU.mult),
                reads=[dBC, dV], writes=[dT2])
            K.dve.op(lambda e, T2=T2: e.tensor_tensor(out=St[P], in0=St[P], in1=T2[P], op=ALU.add),
                     reads=[dT2], writes=[dS])
            T3, dT3 = T3r.next()
            K.pool.op(lambda e, T3=T3, R_=R_: e.tensor_tensor(out=T3[P], in0=St[P], in1=R_, op=ALU.mult),
                      reads=[dS, dBC], writes=[dT3])
            K.dve.op(lambda e, T3=T3, Yf=Yf, tt=tt: e.tensor_reduce(out=Yf[P, :, :, tt], in_=T3[P], axis=AX.X, op=ALU.add),
                      reads=[dT3], writes=[dY] if tt == 0 else [], adds=[] if tt == 0 else [dY])
            if tt == TC - 1:
                for bhi in range(NBH):
                    for blo in range(NBL):
                        dst = vy_ap("y_fm", bhi, blo, ni * TC)
                        srcp = Yf[bhi * 64:(bhi + 1) * 64, blo]
                        K.sp.dma(lambda e, dst=dst, srcp=srcp: e.dma_start(out=dst, in_=srcp), reads=[dY],
                                 adds=[scr["d_p2b"]])


def phase2_chunk(K, cfg, io, scr):
    T, S, NB = cfg["T"], cfg["S"], cfg["NB"]
    C = 64
    NCH = S // C
    with ExitStack() as es:
        d_c = Dep()
        id64 = K.sb([64, 64], BF16, "c_id64", es)
        K.sp.dma(lambda e: e.dma_start(out=id64[:], in_=io["ident64"][:, :]), adds=[d_c])
        MK = K.sb([64, 3, 64], F32, "c_MK", es)
        K.sp.dma(lambda e: e.dma_start(out=MK[:], in_=io["masks"][:, :, :]), adds=[d_c])
        ONES = K.sb([64, 1], F32, "c_ones", es)
        K.sp.dma(lambda e: e.dma_start(out=ONES[:], in_=io["ones64"][:, :]), adds=[d_c])
        IDF = K.sb([64, 8, 64], F32, "c_IDF", es)
        K.sp.dma(lambda e: e.dma_start(out=IDF[:], in_=io["ident_f"][0:64, 0:64].unsqueeze(1).broadcast_to([64, 8, 64])), adds=[d_c])
        ST = [K.sb([64, 8, 64], F32, "c_S%d" % b, es) for b in range(NB)]
        STb = [K.sb([64, 8, 64], BF16, "c_Sb%d" % b, es) for b in range(NB)]
        dST = [Dep() for _ in range(NB)]
        dSTb = [Dep() for _ in range(NB)]
        for b in range(NB):
            K.dve.op(lambda e, b=b: e.memset(ST[b][:], 0.0), writes=[dST[b]])
            K.pool.op(lambda e, b=b: e.memset(STb[b][:], 0.0), writes=[dSTb[b]])
        TMr = Rot(K, 3, [64, 5, 512], BF16, "c_TM", es=es)
        LWr = Rot(K, 3, [64, 512], F32, "c_LW", es=es)
        FMr = Rot(K, 2, [64, 4, 8, 64], BF16, "c_FM", es=es)
        PCr = Rot(K, 2, [64, 8], F32, "c_PC", es=es)
        Nr = Rot(K, 3, [64, 8, 64], BF16, "c_N", es=es)
        NTr = Rot(K, 3, [64, 8, 64], BF16, "c_NT", es=es)
        MTr = Rot(K, 3, [64, 8, 64], BF16, "c_MT", es=es)
        MTfr = Rot(K, 2, [64, 8, 64], F32, "c_MTf", es=es)
        NAKr = Rot(K, 2, [64, 8, 64], BF16, "c_NAK", es=es)
        MRBr = Rot(K, 2, [64, 8, 64], BF16, "c_MRB", es=es)
        MRKr = Rot(K, 2, [64, 8, 64], BF16, "c_MRK", es=es)
        Xr = Rot(K, 2, [64, 8, 64], BF16, "c_X", es=es)
        NUr = Rot(K, 2, [64, 8, 64], BF16, "c_NU", es=es)
        Yr = Rot(K, 2, [64, 8, 64], F32, "c_Y", es=es)
        TSr = Rot(K, 2, [64, 8, 64], F32, "c_TS", es=es)
        PTf = Rot(K, 1, [64, 4, 8, 64], BF16, "c_PTf", psum=True, es=es)
        PA = Rot(K, 5, [64, 8, 64], F32, "c_PA", psum=True, es=es)
        PPC = Rot(K, 1, [64, 8], F32, "c_PPC", psum=True, es=es)
        ce = [0]

        def evac_copy(dst_ap, src_ap, reads, writes=(), adds=(), scale=None):
            ce[0] += 1
            if scale is not None or ce[0] % 2 == 0:
                if scale is None:
                    K.act.op(lambda e: e.activation(out=dst_ap, in_=src_ap, func=AF.Copy), reads=reads, writes=writes, adds=adds)
                else:
                    K.act.op(lambda e: e.activation(out=dst_ap, in_=src_ap, func=AF.Copy, scale=scale), reads=reads, writes=writes, adds=adds)
            else:
                K.dve.op(lambda e: e.tensor_copy(out=dst_ap, in_=src_ap), reads=reads, writes=writes, adds=adds)

        def mm8(pt, dpt, lhs_fn, rhs_fn, reads, first=True, last=True, wr=True):
            mmN(pt, dpt, [(lhs_fn, rhs_fn)], reads)

        def mmN(pt, dpt, terms, reads):
            n = len(terms)
            for h in range(8):
                for i, (lf, rf) in enumerate(terms):
                    K.pe.op(lambda e, h=h, lf=lf, rf=rf, i=i: e.matmul(pt[:, h, :], lf(h), rf(h), start=(i == 0), stop=(i == n - 1)),
                            reads=reads, writes=[dpt] if (h == 0 and i == 0) else [], adds=[] if (h == 0 and i == 0) else [dpt])

        q = [0]

        def dq():
            q[0] += 1
            return K.sp if q[0] % 2 == 0 else K.pool

        for ci in range(NCH):
            for b in range(NB):
                t0 = b * S + ci * C
                TM, dTM = TMr.next()
                LW, dLW = LWr.next()
                dq().dma(lambda e, TM=TM, t0=t0: e.dma_start(out=TM[:], in_=scr["ab_tm"][t0:t0 + C, :, :]),
                         reads=[scr["d_p2a"]], writes=[dTM])
                dq().dma(lambda e, LW=LW, t0=t0: e.dma_start(out=LW[:], in_=scr["lw_tm"][t0:t0 + C, :]),
                         reads=[scr["d_p2a"]], writes=[dLW])
                ptf, dptf = PTf.next()
                first = True
                for j in range(4):
                    for h in range(8):
                        K.pe.op(lambda e, ptf=ptf, TM=TM, j=j, h=h: e.transpose(
                            out=ptf[:, j, h, :], in_=TM[:, j, h * 64:(h + 1) * 64], identity=id64[:]),
                            reads=[dTM, d_c], writes=[dptf] if first else [], adds=[] if first else [dptf])
                        first = False
                FM, dFM = FMr.next()
                K.act.op(lambda e, FM=FM, ptf=ptf: e.activation(out=FM[:, 0:2], in_=ptf[:, 0:2], func=AF.Copy), reads=[dptf], writes=[dFM])
                K.dve.op(lambda e, FM=FM, ptf=ptf: e.tensor_copy(out=FM[:, 2:4], in_=ptf[:, 2:4]), reads=[dptf, dFM], adds=[dFM])
                Af = lambda h, FM=FM: FM[:, 0, h, :]
                Bf = lambda h, FM=FM: FM[:, 1, h, :]
                Kf = lambda h, FM=FM: FM[:, 2, h, :]
                Rf = lambda h, FM=FM: FM[:, 3, h, :]
                Vt = lambda h, TM=TM: TM[:, 4, h * 64:(h + 1) * 64]
                Bt = lambda h, TM=TM: TM[:, 1, h * 64:(h + 1) * 64]
                Kt = lambda h, TM=TM: TM[:, 2, h * 64:(h + 1) * 64]
                ppc, dppc = PPC.next()
                for h in range(8):
                    K.pe.op(lambda e, ppc=ppc, LW=LW, h=h: e.matmul(ppc[:, h:h + 1], LW[:, h * 64:(h + 1) * 64], ONES[:], start=True, stop=True),
                            reads=[dLW, d_c], writes=[dppc] if h == 0 else [], adds=[] if h == 0 else [dppc])
                PCt, dPC = PCr.next()
                K.act.op(lambda e, PCt=PCt, ppc=ppc: e.activation(out=PCt[:], in_=ppc[:], func=AF.Exp), reads=[dppc], writes=[dPC])
                mbc = lambda i: MK[:, i, :].unsqueeze(1).broadcast_to([64, 8, 64])
                pa, dpa = PA.next()
                mm8(pa, dpa, Af, Bf, [dFM])
                N0, dN0 = Nr.next()
                K.dve.op(lambda e, N0=N0, pa=pa: e.tensor_tensor(out=N0[:], in0=pa[:], in1=mbc(0), op=ALU.mult), reads=[dpa, d_c], writes=[dN0])
                pa, dpa = PA.next()
                mm8(pa, dpa, Bf, Af, [dFM])
                NT0, dNT0 = NTr.next()
                MTf, dMTf = MTfr.next()
                K.dve.op(lambda e, NT0=NT0, pa=pa: e.tensor_tensor(out=NT0[:], in0=pa[:], in1=mbc(1), op=ALU.mult), reads=[dpa, d_c], writes=[dNT0])
                K.pool.op(lambda e, MTf=MTf, NT0=NT0: e.tensor_tensor(out=MTf[:], in0=IDF[:], in1=NT0[:], op=ALU.subtract),
                          reads=[dNT0, d_c], writes=[dMTf])
                MT, dMT = MTr.next()
                K.act.op(lambda e, MT=MT, MTf=MTf: e.activation(out=MT[:], in_=MTf[:], func=AF.Copy), reads=[dMTf], writes=[dMT])
                pa, dpa = PA.next()
                mm8(pa, dpa, Kf, Af, [dFM])
                NAK, dNAK = NAKr.next()
                K.dve.op(lambda e, NAK=NAK, pa=pa: e.tensor_tensor(out=NAK[:], in0=pa[:], in1=mbc(1), op=ALU.mult), reads=[dpa, d_c], writes=[dNAK])
                pa, dpa = PA.next()
                mm8(pa, dpa, Bf, Rf, [dFM])
                MRB, dMRB = MRBr.next()
                K.dve.op(lambda e, MRB=MRB, pa=pa: e.tensor_tensor(out=MRB[:], in0=pa[:], in1=mbc(2), op=ALU.mult), reads=[dpa, d_c], writes=[dMRB])
                pa, dpa = PA.next()
                mm8(pa, dpa, Kf, Rf, [dFM])
                MRK, dMRK = MRKr.next()
                K.dve.op(lambda e, MRK=MRK, pa=pa: e.tensor_tensor(out=MRK[:], in0=pa[:], in1=mbc(2), op=ALU.mult), reads=[dpa, d_c], writes=[dMRK])
                Np, dNp, NTp, dNTp = N0, dN0, NT0, dNT0
                for lvl in range(1, 6):
                    pa, dpa = PA.next()
                    mm8(pa, dpa, lambda h, NTp=NTp: NTp[:, h, :], lambda h, Np=Np: Np[:, h, :], [dNp, dNTp])
                    Nn, dNn = Nr.next()
                    evac_copy(Nn[:], pa[:], [dpa], writes=[dNn])
                    if lvl < 5:
                        pa2, dpa2 = PA.next()
                        mm8(pa2, dpa2, lambda h, Np=Np: Np[:, h, :], lambda h, NTp=NTp: NTp[:, h, :], [dNp, dNTp])
                        NTn, dNTn = NTr.next()
                        evac_copy(NTn[:], pa2[:], [dpa2], writes=[dNTn])
                    pa3, dpa3 = PA.next()
                    mm8(pa3, dpa3, lambda h, Nn=Nn: Nn[:, h, :], lambda h, MT=MT: MT[:, h, :], [dNn, dMT])
                    K.dve.op(lambda e, MTf=MTf, pa3=pa3: e.tensor_tensor(out=MTf[:], in0=MTf[:], in1=pa3[:], op=ALU.add),
                             reads=[dpa3], writes=[dMTf])
                    MT, dMT = MTr.next()
                    K.act.op(lambda e, MT=MT, MTf=MTf: e.activation(out=MT[:], in_=MTf[:], func=AF.Copy), reads=[dMTf], writes=[dMT])
                    Np, dNp = Nn, dNn
                    if lvl < 5:
                        NTp, dNTp = NTn, dNTn
                Sb = STb[b]
                pa, dpa = PA.next()
                mmN(pa, dpa, [(Af, lambda h, Sb=Sb: Sb[:, h, :]), (lambda h, NAK=NAK: NAK[:, h, :], Vt)], [dFM, dSTb[b], dNAK, dTM])
                X, dX = Xr.next()
                K.act.op(lambda e, X=X, pa=pa: e.activation(out=X[:], in_=pa[:], func=AF.Copy), reads=[dpa], writes=[dX])
                pa, dpa = PA.next()
                mm8(pa, dpa, lambda h, MT=MT: MT[:, h, :], lambda h, X=X: X[:, h, :], [dMT, dX])
                NU, dNU = NUr.next()
                K.act.op(lambda e, NU=NU, pa=pa: e.activation(out=NU[:], in_=pa[:], func=AF.Copy, scale=-1.0), reads=[dpa], writes=[dNU])
                pa, dpa = PA.next()
                mmN(pa, dpa, [(lambda h, Sb=Sb: Sb[:, h, :], Rf), (lambda h, NU=NU: NU[:, h, :], lambda h, MRB=MRB: MRB[:, h, :]),
                              (Vt, lambda h, MRK=MRK: MRK[:, h, :])], [dFM, dSTb[b], dNU, dMRB, dTM, dMRK])
                Y, dY = Yr.next()
                K.dve.op(lambda e, Y=Y, pa=pa: e.tensor_copy(out=Y[:], in_=pa[:]), reads=[dpa], writes=[dY])
                K.sp.dma(lambda e, Y=Y, t0=t0: e.dma_start(out=dap(scr["y_fm"], t0, [[T, 64], [64 * T, 8], [1, 64]]), in_=Y[:]),
                         reads=[dY], adds=[scr["d_p2b"]])
                pa, dpa = PA.next()
                mmN(pa, dpa, [(Bt, lambda h, NU=NU: NU[:, h, :]), (Kt, Vt)], [dTM, dNU])
                TS_, dTS = TSr.next()
                K.dve.op(lambda e, TS_=TS_, pa=pa, b=b: e.tensor_tensor(out=TS_[:], in0=pa[:], in1=ST[b][:], op=ALU.add),
                         reads=[dpa, dST[b]], writes=[dTS])
                K.dve.op(lambda e, TS_=TS_, PCt=PCt, b=b: e.tensor_tensor(
                    out=ST[b][:], in0=TS_[:], in1=PCt[:].unsqueeze(2).broadcast_to([64, 8, 64]), op=ALU.mult),
                    reads=[dTS, dPC], writes=[dST[b]])
                K.act.op(lambda e, b=b: e.activation(out=STb[b][:], in_=ST[b][:], func=AF.Copy), reads=[dST[b]], writes=[dSTb[b]])


def phase2_post(K, cfg, io, scr):
    T, S, NB = cfg["T"], cfg["S"], cfg["NB"]
    NT = T // 512
    with ExitStack() as es:
        BO = K.sb([128, 128], F32, "BO", es)
        d_c = Dep()
        K.sp.dma(lambda e: e.dma_start(out=BO[:], in_=io["blockones"][:, :]), adds=[d_c])
        LN = K.sb([128, 2, 4], F32, "LN", es)
        K.sp.dma(lambda e: e.dma_start(out=LN[:, 0, :], in_=io["lnx_w"].rearrange("o (j p) -> p (o j)", p=128)), adds=[d_c])
        K.sp.dma(lambda e: e.dma_start(out=LN[:, 1, :], in_=io["lnx_b"].rearrange("o (j p) -> p (o j)", p=128)), adds=[d_c])
        Yr = Rot(K, 2, [128, 512], F32, "pY", es=es)
        Vr = Rot(K, 2, [128, 512], F32, "pV", es=es)
        Gr = Rot(K, 2, [128, 512], F32, "pG", es=es)
        Cr = Rot(K, 2, [128, 512], F32, "pC", es=es)
        YCr = Rot(K, 2, [128, 512], F32, "pYC", es=es)
        SQr = Rot(K, 2, [128, 512], F32, "pSQ", es=es)
        Rr = Rot(K, 2, [128, 512], F32, "pR", es=es)
        Or = Rot(K, 2, [128, 512], BF16, "pO", es=es)
        PM = Rot(K, 2, [128, 512], F32, "pPM", psum=True, es=es)
        PVr = Rot(K, 2, [128, 512], F32, "pPV", psum=True, es=es)
        def make_gen(ti, j):
            cs = slice(ti * 512, (ti + 1) * 512)
            if True:
                rs = slice(j * 128, (j + 1) * 128)
                Y, dY = Yr.next()
                V, dV = Vr.next()
                G, dG = Gr.next()
                C, dC = Cr.next()
                K.sp.dma(lambda e, Y=Y, rs=rs, cs=cs: e.dma_start(out=Y[:], in_=scr["y_fm"][rs, cs]),
                         reads=[scr["d_p2b"]], writes=[dY])
                K.pool.dma(lambda e, V=V, rs=rs, cs=cs: e.dma_start(out=V[:], in_=scr["v_fm"][rs, cs]),
                           reads=[scr["d_p2a"]], writes=[dV])
                K.sp.dma(lambda e, G=G, rs=rs, cs=cs: e.dma_start(out=G[:], in_=scr["g_fm"][rs, cs]),
                         reads=[scr["d_p2a"]], writes=[dG])
                K.pool.dma(lambda e, C=C, j=j, cs=cs: e.dma_start(
                    out=C[0:64, :], in_=scr["coef_fm"][2 * j:2 * j + 1, cs].broadcast_to([64, 512])),
                    reads=[scr["d_p2a"]], writes=[dC])
                K.pool.dma(lambda e, C=C, j=j, cs=cs: e.dma_start(
                    out=C[64:128, :], in_=scr["coef_fm"][2 * j + 1:2 * j + 2, cs].broadcast_to([64, 512])),
                    reads=[scr["d_p2a"]], adds=[dC])
                pm, dpm = PM.next()
                K.pe.op(lambda e, pm=pm, Y=Y: e.matmul(pm[:], BO[:], Y[:], start=True, stop=True),
                        reads=[dY, d_c], writes=[dpm])
                YC, dYC = YCr.next()
                K.dve.op(lambda e, YC=YC, Y=Y, pm=pm: e.tensor_tensor(out=YC[:], in0=Y[:], in1=pm[:], op=ALU.subtract),
                         reads=[dY, dpm], writes=[dYC])
                SQ, dSQ = SQr.next()
                K.act.op(lambda e, SQ=SQ, YC=YC: e.activation(out=SQ[:], in_=YC[:], func=AF.Square),
                         reads=[dYC], writes=[dSQ])
                yield
                pv, dpv = PVr.next()
                K.pe.op(lambda e, pv=pv, SQ=SQ: e.matmul(pv[:], BO[:], SQ[:], start=True, stop=True),
                        reads=[dSQ, d_c], writes=[dpv])
                yield
                R, dR = Rr.next()
                K.dve.op(lambda e, R=R, pv=pv: e.tensor_scalar(out=R[:], in0=pv[:], scalar1=64e-5, scalar2=None, op0=ALU.add),
                         reads=[dpv], writes=[dR])
                K.act.op(lambda e, R=R: e.activation(out=R[:], in_=R[:], func=AF.Ln), writes=[dR])
                K.act.op(lambda e, R=R: e.activation(out=R[:], in_=R[:], func=AF.Exp, scale=-0.5), writes=[dR])
                K.dve.op(lambda e, YC=YC, R=R: e.tensor_tensor(out=YC[:], in0=YC[:], in1=R[:], op=ALU.mult),
                         reads=[dR], writes=[dYC])
                K.dve.op(lambda e, YC=YC, j=j: e.tensor_scalar(out=YC[:], in0=YC[:], scalar1=LN[:, 0, j:j + 1],
                                                              scalar2=LN[:, 1, j:j + 1], op0=ALU.mult, op1=ALU.add),
                         reads=[d_c], writes=[dYC])
                yield
                K.pool.op(lambda e, C=C, V=V: e.tensor_tensor(out=C[:], in0=C[:], in1=V[:], op=ALU.mult),
                          reads=[dV], writes=[dC])
                K.dve.op(lambda e, YC=YC, C=C: e.tensor_tensor(out=YC[:], in0=YC[:], in1=C[:], op=ALU.add),
                         reads=[dC], writes=[dYC])
                O, dO = Or.next()
                K.dve.op(lambda e, O=O, YC=YC, G=G: e.tensor_tensor(out=O[:], in0=YC[:], in1=G[:], op=ALU.mult),
                         reads=[dYC, dG], writes=[dO])
                K.sp.dma(lambda e, O=O, rs=rs, cs=cs: e.dma_start(out=scr["ya_fm"][rs, cs], in_=O[:]),
                         reads=[dO], adds=[scr["d_p2c"]])

        work = [(ti, j) for ti in range(NT) for j in range(4)]

        active = []
        nxt = 0
        while nxt < len(work) or active:
            if len(active) < 2 and nxt < len(work):
                active.append(make_gen(*work[nxt]))
                nxt += 1
            for a in list(active):
                try:
                    next(a)
                except StopIteration:
                    active.remove(a)


def phase3(K, cfg, io, scr):
    T, S, NB = cfg["T"], cfg["S"], cfg["NB"]
    NQ = S // 128
    lam_init = 0.2
    with ExitStack() as es:
        d_c = Dep()
        identb = K.sb([128, 128], BF16, "a_identb", es)
        K.sp.dma(lambda e: e.dma_start(out=identb[:], in_=io["ident_bf"][:, :]), adds=[d_c])
        TB = K.sb([128, 4, S], F32, "TB", es)
        for h in range(4):
            (K.sp if h % 2 == 0 else K.pool).dma(lambda e, h=h: e.dma_start(out=TB[:, h, :], in_=io["alibi"][h, :, :]), adds=[d_c])
        SW = K.sb([128, 128], F32, "SW", es)
        K.sp.dma(lambda e: e.dma_start(out=SW[:], in_=io["subln_w"][0:1, :].broadcast_to([128, 128])), adds=[d_c])
        LQ = K.sb([128, 4, 64], F32, "LQ", es)
        for j, nm in enumerate(["lam_q1", "lam_k1", "lam_q2", "lam_k2"]):
            K.pool.dma(lambda e, j=j, nm=nm: e.dma_start(out=LQ[:, j, :], in_=io[nm][0:1, :].broadcast_to([128, 64])), adds=[d_c])
        LM = K.sb([128, 8], F32, "LM", es)
        d_lm = Dep()
        LT_ = K.sb([128, 2, 64], F32, "LTt", es)
        K.dve.op(lambda e: e.tensor_tensor(out=LT_[:, 0, :], in0=LQ[:, 0, :], in1=LQ[:, 1, :], op=ALU.mult), reads=[d_c], writes=[d_lm])
        K.dve.op(lambda e: e.tensor_tensor(out=LT_[:, 1, :], in0=LQ[:, 2, :], in1=LQ[:, 3, :], op=ALU.mult), reads=[d_c], writes=[d_lm])
        K.dve.op(lambda e: e.tensor_reduce(out=LM[:, 0:2], in_=LT_[:], axis=AX.X, op=ALU.add), writes=[d_lm])
        K.act.op(lambda e: e.activation(out=LM[:, 2:4], in_=LM[:, 0:2], func=AF.Exp), writes=[d_lm])
        K.dve.op(lambda e: e.tensor_tensor(out=LM[:, 4:5], in0=LM[:, 3:4], in1=LM[:, 2:3], op=ALU.subtract), writes=[d_lm])
        K.dve.op(lambda e: e.tensor_scalar(out=LM[:, 4:5], in0=LM[:, 4:5], scalar1=-lam_init, scalar2=None, op0=ALU.add), writes=[d_lm])
        K.dve.op(lambda e: e.tensor_scalar(out=SW[:], in0=SW[:], scalar1=1.0 - lam_init, scalar2=None, op0=ALU.mult),
                 reads=[d_c], writes=[d_c])

        Vr = Rot(K, 2, [128, NQ, 512], BF16, "aV", es=es)
        QKr = Rot(K, 2, [64, 4, S], BF16, "aQK", es=es)
        SSr = Rot(K, 3, [128, 512], F32, "aSS", es=es)
        Pr = Rot(K, 3, [128, 512], BF16, "aP", es=es)
        PTsr = Rot(K, 4, [128, 4, 128], BF16, "aPTs", es=es)
        YB = K.sb([128, NQ, 512], BF16, "aYB", es)
        dYB = Dep()
        STr = Rot(K, 4, [128, 24], F32, "aST", es=es)
        O1r = Rot(K, 2, [128, 128], F32, "aO1", es=es)
        Or_ = Rot(K, 2, [128, 128], F32, "aO", es=es)
        junk = K.sb([128, 128], F32, "ajunk", es)
        d_junk = Dep()
        YTr = Rot(K, 2, [128, 4, 128], BF16, "aYT", es=es)
        PS = Rot(K, 3, [128, 512], F32, "aPS", psum=True, es=es)
        PTp = Rot(K, 2, [128, 4, 128], BF16, "aPTp", psum=True, es=es)
        PO = Rot(K, 2, [128, 2, 128], F32, "aPO", psum=True, es=es)
        cp = [0]

        def copy_eng():
            cp[0] += 1
            return cp[0] % 2

        SSQ = K.sb([128, NQ * 4], F32, "aSSQ", es)
        dSSQ = Dep()
        SWb = K.sb([128, 128], BF16, "aSWb", es)
        K.act.op(lambda e: e.activation(out=SWb[:], in_=SW[:], func=AF.Copy), reads=[d_c], adds=[d_c])
        pipe = []
        pidx = [0]

        def step_pipe():
            j = len(pipe) - 1
            pipe[j][0]()
            if j - 1 >= pidx[0]:
                pipe[j - 1][2]()
            if j - 2 >= pidx[0]:
                pipe[j - 2][3]()
                if pipe[j - 2][4] is not None:
                    pipe[j - 2][4]()
            pipe[j][1]()

        def flush_pipe():
            j = len(pipe) - 1
            if j - 0 >= pidx[0] and j >= 0:
                pipe[j][2]()
            for k in (j - 1, j):
                if k >= pidx[0] and k >= 0:
                    pipe[k][3]()
                    if pipe[k][4] is not None:
                        pipe[k][4]()
            pidx[0] = len(pipe)
        for b in range(NB):
            V, dV = Vr.next()
            K.sp.dma(lambda e, V=V, b=b: e.dma_start(
                out=V[:], in_=scr["av_tm"][b * S:(b + 1) * S, :].rearrange("(n p) c -> p n c", p=128)),
                reads=[scr["d_p1"]], writes=[dV])
            first_yb = True
            for h in range(4):
                QK, dQK = QKr.next()
                for j in range(4):
                    r0 = (0 if j < 2 else 512) + h * 128 + (j % 2) * 64
                    (K.sp if j % 2 == 0 else K.pool).dma(lambda e, QK=QK, j=j, r0=r0, b=b: e.dma_start(
                        out=QK[:, j, :], in_=scr["qk_fm"][r0:r0 + 64, b * S:(b + 1) * S]),
                        reads=[scr["d_p1"]], writes=[dQK] if j == 0 else [], adds=[] if j == 0 else [dQK])
                for qi in range(NQ):
                    nk = (qi + 1) * 128
                    off = (S - 128) - qi * 128
                    ST, dST = STr.next()
                    K.pool.op(lambda e, ST=ST: e.memset(ST[:], 0.0), writes=[dST])
                    po, dpo = PO.next()
                    items = []
                    for c in range(2):
                        nch = (nk + 511) // 512
                        for ch in range(nch):
                            items.append((c, ch))
                    for ii, (c, ch) in enumerate(items):
                        kb0 = ch * 512
                        n = min(512, nk - kb0)
                        nb = n // 128
                        ps, dps = PS.next()
                        SS, dSS = SSr.next()
                        Pt, dP = Pr.next()
                        hold = {}

                        def stA_pe(ps=ps, dps=dps, c=c, qi=qi, kb0=kb0, n=n, QK=QK, dQK=dQK):
                            K.pe.op(lambda e: e.matmul(
                                ps[:, :n], QK[:, c, qi * 128:(qi + 1) * 128], QK[:, 2 + c, kb0:kb0 + n],
                                start=True, stop=True), reads=[dQK], writes=[dps])

                        def stA_rest(ps=ps, dps=dps, SS=SS, dSS=dSS, Pt=Pt, dP=dP, ST=ST, dST=dST, n=n, off=off, h=h, kb0=kb0, c=c, ch=ch):
                            K.dve.op(lambda e: e.scalar_tensor_tensor(
                                out=SS[:, :n], in0=ps[:, :n], scalar=0.125, in1=TB[:, h, off + kb0:off + kb0 + n],
                                op0=ALU.mult, op1=ALU.add), reads=[dps, d_c], writes=[dSS])
                            K.act.op(lambda e: e.activation(
                                out=Pt[:, :n], in_=SS[:, :n], func=AF.Exp,
                                accum_out=ST[:, 4 * c + ch:4 * c + ch + 1]), reads=[dSS, dST], writes=[dP], adds=[dST])

                        def stB(Pt=Pt, dP=dP, nb=nb, hold=hold):
                            ptp, dptp = PTp.next()
                            for kk_ in range(nb):
                                K.pe.op(lambda e, kk_=kk_: e.transpose(
                                    out=ptp[:, kk_, :], in_=Pt[:, kk_ * 128:(kk_ + 1) * 128], identity=identb[:]),
                                    reads=[dP, d_c], writes=[dptp] if kk_ == 0 else [], adds=[] if kk_ == 0 else [dptp])
                            PTs, dPTs = PTsr.next()
                            hold["PTs"] = (PTs, dPTs)
                            if copy_eng():
                                K.act.op(lambda e: e.activation(
                                    out=PTs[:, :nb, :], in_=ptp[:, :nb, :], func=AF.Copy), reads=[dptp], writes=[dPTs])
                            else:
                                K.dve.op(lambda e: e.tensor_copy(
                                    out=PTs[:, :nb, :], in_=ptp[:, :nb, :]), reads=[dptp], writes=[dPTs])

                        def stC(nb=nb, kb0=kb0, c=c, h=h, qi=qi, po=po, dpo=dpo, V=V, dV=dV, hold=hold):
                            PTs, dPTs = hold["PTs"]
                            for kk_ in range(nb):
                                kb = kb0 // 128 + kk_
                                K.pe.op(lambda e, kb=kb, kk_=kk_: e.matmul(
                                    po[:, c, :], PTs[:, kk_, :], V[:, kb, h * 128:(h + 1) * 128],
                                    start=(kb == 0), stop=(kb == qi)), reads=[dPTs, dV],
                                    writes=[dpo] if (kb == 0 and c == 0) else [], adds=[] if (kb == 0 and c == 0) else [dpo])

                        def combine(ST=ST, dST=dST, po=po, dpo=dpo, qi=qi, h=h, fy=first_yb):
                            K.dve.op(lambda e: e.tensor_reduce(out=ST[:, 8:10], in_=ST[:, 0:8].rearrange("p (c k) -> p c k", k=4),
                                                               axis=AX.X, op=ALU.add), reads=[dST], writes=[dST])
                            K.dve.op(lambda e: e.reciprocal(out=ST[:, 10:12], in_=ST[:, 8:10]), writes=[dST])
                            K.dve.op(lambda e: e.tensor_tensor(out=ST[:, 12:13], in0=ST[:, 11:12], in1=LM[:, 4:5], op=ALU.mult),
                                     reads=[d_lm], writes=[dST])
                            O1, dO1 = O1r.next()
                            K.dve.op(lambda e: e.tensor_scalar(out=O1[:], in0=po[:, 1, :], scalar1=ST[:, 12:13],
                                                               scalar2=None, op0=ALU.mult), reads=[dpo, dST], writes=[dO1])
                            K.dve.op(lambda e: e.scalar_tensor_tensor(
                                out=YB[:, qi, h * 128:(h + 1) * 128], in0=po[:, 0, :], scalar=ST[:, 10:11], in1=O1[:], op0=ALU.mult, op1=ALU.add),
                                reads=[dpo, dST, dO1], writes=[dYB] if fy else [], adds=[] if fy else [dYB])
                            K.act.op(lambda e: e.activation(out=junk[:], in_=YB[:, qi, h * 128:(h + 1) * 128], func=AF.Square,
                                                            accum_out=SSQ[:, qi * 4 + h:qi * 4 + h + 1]),
                                     reads=[dYB], writes=[d_junk], adds=[dSSQ])

                        last = (ii == len(items) - 1)
                        pipe.append([stA_pe, stA_rest, stB, stC, combine if last else None])
                        step_pipe()
                    first_yb = False
            flush_pipe()
            K.dve.op(lambda e: e.tensor_scalar(out=SSQ[:], in0=SSQ[:], scalar1=1.0 / 128, scalar2=1e-5, op0=ALU.mult, op1=ALU.add),
                     reads=[dSSQ], writes=[dSSQ])
            K.act.op(lambda e: e.activation(out=SSQ[:], in_=SSQ[:], func=AF.Ln), writes=[dSSQ])
            K.act.op(lambda e: e.activation(out=SSQ[:], in_=SSQ[:], func=AF.Exp, scale=-0.5), writes=[dSSQ])
            YBv = YB[:].rearrange("p q (h e) -> p (q h) e", e=128)
            K.dve.op(lambda e: e.tensor_tensor(out=YBv, in0=YBv, in1=SSQ[:].unsqueeze(2).broadcast_to([128, NQ * 4, 128]), op=ALU.mult),
                     reads=[dSSQ], writes=[dYB])
            K.pool.op(lambda e: e.tensor_tensor(out=YBv, in0=YBv, in1=SWb[:].unsqueeze(1).broadcast_to([128, NQ * 4, 128]), op=ALU.mult),
                      reads=[d_c], writes=[dYB])
            for qi in range(NQ):
                ptp, dptp = PTp.next()
                for h in range(4):
                    K.pe.op(lambda e, ptp=ptp, qi=qi, h=h: e.transpose(out=ptp[:, h, :], in_=YB[:, qi, h * 128:(h + 1) * 128],
                                                                      identity=identb[:]),
                            reads=[dYB, d_c], writes=[dptp] if h == 0 else [], adds=[] if h == 0 else [dptp])
                YT, dYT = YTr.next()
                K.act.op(lambda e, ptp=ptp, YT=YT: e.activation(out=YT[:], in_=ptp[:, 0:4, :], func=AF.Copy),
                         reads=[dptp], writes=[dYT])
                t0 = b * S + qi * 128
                K.sp.dma(lambda e, YT=YT, t0=t0: e.dma_start(
                    out=dap(scr["yb_fm"], t0, [[T, 128], [128 * T, 4], [1, 128]]), in_=YT[:]),
                    reads=[dYT], adds=[scr["d_p3"]])


def load_w_bf16(K, es, src2d, rows, cols, name, dep, stage_rot, q, W=None):
    nk = rows // 128
    if W is None:
        W = K.sb([128, nk, cols], BF16, name, es)
    for kc in range(nk):
        for c0 in range(0, cols, 1024):
            n = min(1024, cols - c0)
            st, dst = stage_rot.next()
            q[0] += 1
            (K.sp if q[0] % 2 == 0 else K.pool).dma(lambda e, st=st, kc=kc, c0=c0, n=n: e.dma_start(
                out=st[:, :n], in_=src2d[kc * 128:(kc + 1) * 128, c0:c0 + n]), writes=[dst])
            if q[0] % 2 == 0:
                K.act.op(lambda e, st=st, kc=kc, c0=c0, n=n: e.activation(out=W[:, kc, c0:c0 + n], in_=st[:, :n], func=AF.Copy),
                         reads=[dst], adds=[dep])
            else:
                K.dve.op(lambda e, st=st, kc=kc, c0=c0, n=n: e.tensor_copy(out=W[:, kc, c0:c0 + n], in_=st[:, :n]),
                         reads=[dst], adds=[dep])
    return W


def phase4(K, cfg, io, scr):
    T, S, NB = cfg["T"], cfg["S"], cfg["NB"]
    NT = T // 512
    with ExitStack() as es:
        d_c = Dep()
        identb = K.sb([128, 128], BF16, "m_identb", es)
        K.sp.dma(lambda e: e.dma_start(out=identb[:], in_=io["ident_bf"][:, :]), adds=[d_c])
        NF = K.sb([128, D], F32, "NF", es)
        K.sp.dma(lambda e: e.dma_start(out=NF[:], in_=io["norm_ffn_w"][0:1, :].broadcast_to([128, D])), adds=[d_c])
        stg = Rot(K, 2, [128, 1024], F32, "m_stg", es=es)
        q = [0]
        PAw = load_w_bf16(K, es, io["proj_a"][0], 512, D, "PAw", d_c, stg, q)
        PBw = load_w_bf16(K, es, io["proj_b"][0], 512, D, "PBw", d_c, stg, q)
        WO = load_w_bf16(K, es, io["w_out"][0], D, D, "WO", d_c, stg, q)
        YAr = Rot(K, 2, [128, 4, 512], BF16, "mYA", es=es)
        YBr = Rot(K, 2, [128, 4, 512], BF16, "mYB", es=es)
        SGr = Rot(K, 2, [128, 16, 512], BF16, "mSG", es=es)
        MGr = Rot(K, 2, [128, 8, 512], BF16, "mMG", es=es)
        t1r = Rot(K, 2, [128, 512], F32, "mt1", es=es)
        t2r = Rot(K, 2, [128, 512], F32, "mt2", es=es)
        Xr = Rot(K, 2, [128, D], F32, "mX", es=es)
        X1r = Rot(K, 2, [128, D], F32, "mX1", es=es)
        XHr = Rot(K, 2, [128, D], F32, "mXH", es=es)
        XBr = Rot(K, 2, [128, D], BF16, "mXB", es=es)
        XTr = Rot(K, 2, [128, 8, 128], BF16, "mXT", es=es)
        junk = K.sb([128, D], BF16, "mjunk", es)
        d_junk = Dep()
        STr = Rot(K, 4, [128, 4], F32, "mST", es=es)
        PP = Rot(K, 2, [128, 2, 512], F32, "mPP", psum=True, es=es)
        PO2 = Rot(K, 1, [128, 2, 512], F32, "mPO", psum=True, es=es)
        PTp = Rot(K, 1, [128, 8, 128], BF16, "mPTp", psum=True, es=es)
        def make_gen(ti):
            cs = slice(ti * 512, (ti + 1) * 512)
            YA, dYA = YAr.next()
            YB, dYB = YBr.next()
            SG, dSG = SGr.next()
            K.sp.dma(lambda e, YA=YA, cs=cs: e.dma_start(out=YA[:], in_=scr["ya_fm"][:, cs].rearrange("(c p) t -> p c t", p=128)),
                     reads=[scr["d_p2c"]], writes=[dYA])
            K.pool.dma(lambda e, YB=YB, cs=cs: e.dma_start(out=YB[:], in_=scr["yb_fm"][:, cs].rearrange("(c p) t -> p c t", p=128)),
                       reads=[scr["d_p3"]], writes=[dYB])
            K.sp.dma(lambda e, SG=SG, cs=cs: e.dma_start(out=SG[:], in_=scr["sg_fm"][:, cs].rearrange("(c p) t -> p c t", p=128)),
                     reads=[scr["d_p1"]], writes=[dSG])
            MG, dMG = MGr.next()
            for m in range(8):
                pp, dpp = PP.next()
                for c in range(4):
                    K.pe.op(lambda e, pp=pp, YA=YA, c=c, m=m: e.matmul(pp[:, 0, :], PAw[:, c, m * 128:(m + 1) * 128], YA[:, c, :],
                                                                      start=(c == 0), stop=(c == 3)),
                            reads=[dYA, d_c], writes=[dpp] if c == 0 else [], adds=[] if c == 0 else [dpp])
                for c in range(4):
                    K.pe.op(lambda e, pp=pp, YB=YB, c=c, m=m: e.matmul(pp[:, 1, :], PBw[:, c, m * 128:(m + 1) * 128], YB[:, c, :],
                                                                      start=(c == 0), stop=(c == 3)),
                            reads=[dYB, d_c], adds=[dpp])
                t1, dt1 = t1r.next()
                t2, dt2 = t2r.next()
                K.dve.op(lambda e, t1=t1, pp=pp, SG=SG, m=m: e.tensor_tensor(out=t1[:], in0=pp[:, 0, :], in1=SG[:, m, :], op=ALU.mult),
                         reads=[dpp, dSG], writes=[dt1])
                K.dve.op(lambda e, t2=t2, pp=pp, SG=SG, m=m: e.tensor_tensor(out=t2[:], in0=pp[:, 1, :], in1=SG[:, 8 + m, :], op=ALU.mult),
                         reads=[dpp, dSG], writes=[dt2])
                K.pool.op(lambda e, t1=t1, t2=t2, MG=MG, m=m: e.tensor_tensor(out=MG[:, m, :], in0=t1[:], in1=t2[:], op=ALU.add),
                          reads=[dt1, dt2], writes=[dMG] if m == 0 else [], adds=[] if m == 0 else [dMG])
                if m % 2 == 1:
                    yield
            for sub in range(4):
                t0 = ti * 512 + sub * 128
                X, dX = Xr.next()
                K.pool.dma(lambda e, X=X, t0=t0: e.dma_start(out=X[:], in_=io["x"][t0:t0 + 128, :]), writes=[dX])
                po, dpo = PO2.next()
                for n in range(2):
                    for m in range(8):
                        K.pe.op(lambda e, po=po, MG=MG, m=m, n=n, sub=sub: e.matmul(
                            po[:, n, :], MG[:, m, sub * 128:(sub + 1) * 128], WO[:, m, n * 512:(n + 1) * 512],
                            start=(m == 0), stop=(m == 7)), reads=[dMG, d_c],
                            writes=[dpo] if (m == 0 and n == 0) else [], adds=[] if (m == 0 and n == 0) else [dpo])
                X1, dX1 = X1r.next()
                K.dve.op(lambda e, X1=X1, X=X, po=po: e.tensor_tensor(out=X1[:], in0=X[:], in1=po[:].rearrange("p a b -> p (a b)"),
                                                                     op=ALU.add), reads=[dX, dpo], writes=[dX1])
                K.sp.dma(lambda e, X1=X1, t0=t0: e.dma_start(out=scr["x1_tm"][t0:t0 + 128, :], in_=X1[:]),
                         reads=[dX1], adds=[scr["d_p4"]])
                ST, dST = STr.next()
                K.act.op(lambda e, X1=X1, ST=ST: e.activation(out=junk[:], in_=X1[:], func=AF.Square, accum_out=ST[:, 0:1]),
                         reads=[dX1], writes=[d_junk, dST])
                K.dve.op(lambda e, ST=ST: e.tensor_scalar(out=ST[:, 1:2], in0=ST[:, 0:1], scalar1=1.0 / D, scalar2=1e-6,
                                                          op0=ALU.mult, op1=ALU.add), writes=[dST])
                K.act.op(lambda e, ST=ST: e.activation(out=ST[:, 2:3], in_=ST[:, 1:2], func=AF.Ln), writes=[dST])
                K.act.op(lambda e, ST=ST: e.activation(out=ST[:, 3:4], in_=ST[:, 2:3], func=AF.Exp, scale=-0.5), writes=[dST])
                XH, dXH = XHr.next()
                K.dve.op(lambda e, XH=XH, X1=X1, ST=ST: e.scalar_tensor_tensor(
                    out=XH[:], in0=X1[:], scalar=ST[:, 3:4], in1=NF[:], op0=ALU.mult, op1=ALU.mult),
                    reads=[dX1, dST, d_c], writes=[dXH])
                K.sp.dma(lambda e, XH=XH, t0=t0: e.dma_start(out=scr["xh_tm"][t0:t0 + 128, :], in_=XH[:]),
                         reads=[dXH], adds=[scr["d_p4"]])
                XB, dXB = XBr.next()
                K.act.op(lambda e, XB=XB, XH=XH: e.activation(out=XB[:], in_=XH[:], func=AF.Copy), reads=[dXH], writes=[dXB])
                yield
                ptp, dptp = PTp.next()
                for kc in range(8):
                    K.pe.op(lambda e, ptp=ptp, XB=XB, kc=kc: e.transpose(out=ptp[:, kc, :], in_=XB[:, kc * 128:(kc + 1) * 128],
                                                                        identity=identb[:]),
                            reads=[dXB, d_c], writes=[dptp] if kc == 0 else [], adds=[] if kc == 0 else [dptp])
                XT, dXT = XTr.next()
                K.act.op(lambda e, ptp=ptp, XT=XT: e.activation(out=XT[:], in_=ptp[:], func=AF.Copy), reads=[dptp], writes=[dXT])
                K.sp.dma(lambda e, XT=XT, t0=t0: e.dma_start(
                    out=dap(scr["xhT_fm"], t0, [[T, 128], [128 * T, 8], [1, 128]]), in_=XT[:]),
                    reads=[dXT], adds=[scr["d_p4"]])
                yield

        work = [(ti,) for ti in range(NT)]

        active = []
        nxt = 0
        while nxt < len(work) or active:
            if len(active) < 2 and nxt < len(work):
                active.append(make_gen(*work[nxt]))
                nxt += 1
            for a in list(active):
                try:
                    next(a)
                except StopIteration:
                    active.remove(a)


def phase5(K, cfg, io, scr):
    T, S, NB = cfg["T"], cfg["S"], cfg["NB"]
    NTT = T // 128
    with ExitStack() as es:
        d_c = Dep()
        identb = K.sb([128, 128], BF16, "f_identb", es)
        K.sp.dma(lambda e: e.dma_start(out=identb[:], in_=io["ident_bf"][:, :]), adds=[d_c])
        IOTA = K.sb([128, 16], F32, "IOTA", es)
        K.sp.dma(lambda e: e.dma_start(out=IOTA[:], in_=io["iota16"][:, :]), adds=[d_c])
        FNW = K.sb([128, D], F32, "FNW", es)
        K.sp.dma(lambda e: e.dma_start(out=FNW[:], in_=io["final_norm_w"][0:1, :].broadcast_to([128, D])), adds=[d_c])
        WQ = K.sb([128, 8, 2048], BF16, "WQ", es)
        KT = K.sb([128, 16, 128], BF16, "KT", es)
        PQ = Rot(K, 1, [128, 8, 128], F32, "fPQ", psum=True, es=es)
        PSc = Rot(K, 1, [128, 8, 128], F32, "fPSc", psum=True, es=es)
        PKT = Rot(K, 1, [128, 8, 128], BF16, "fPKT", psum=True, es=es)
        es_setup = ExitStack()
        stg = Rot(K, 2, [128, 1024], F32, "f_stg", es=es_setup)
        q = [0]
        load_w_bf16(K, es, io["peer_wq"][0], D, 2048, "WQ", d_c, stg, q, W=WQ)
        KF = K.sb([128, 16, 128], F32, "KF", es_setup)
        dKF = Dep()
        K.sp.dma(lambda e: e.dma_start(out=KF[:], in_=io["peer_keys"][0].rearrange("h c n d -> n (h c) d")), writes=[dKF])
        KB = K.sb([128, 16, 128], BF16, "KB", es_setup)
        K.dve.op(lambda e: e.tensor_copy(out=KB[:], in_=KF[:]), reads=[dKF], writes=[dKF])
        for half in range(2):
            pk, dpk = PKT.next()
            for i in range(8):
                K.pe.op(lambda e, pk=pk, i=i, half=half: e.transpose(out=pk[:, i, :], in_=KB[:, half * 8 + i, :], identity=identb[:]),
                        reads=[dKF, d_c], writes=[dpk] if i == 0 else [], adds=[] if i == 0 else [dpk])
            K.act.op(lambda e, pk=pk, half=half: e.activation(out=KT[:, half * 8:(half + 1) * 8, :], in_=pk[:], func=AF.Copy),
                     reads=[dpk], adds=[d_c])

        K.barrier()
        es_setup.close()
        XTr = Rot(K, 2, [128, 8, 128], BF16, "fXT", es=es)
        XHr = Rot(K, 2, [128, D], F32, "fXH", es=es)
        X1r = Rot(K, 2, [128, D], F32, "fX1", es=es)
        QTr = Rot(K, 1, [128, 16, 128], BF16, "fQT", es=es)
        SCr = Rot(K, 1, [128, 16, 128], F32, "fSC", es=es)
        SC2 = K.sb([128, 256], F32, "fSC2", es)
        dSC2 = Dep()
        M16r = Rot(K, 1, [128, 16, 16], F32, "fM16", es=es)
        I16r = Rot(K, 1, [128, 16, 16], U32, "fI16", es=es)
        I16fr = Rot(K, 1, [128, 16, 16], F32, "fI16f", es=es)
        CANDr = Rot(K, 1, [128, 8, 256], F32, "fCAND", es=es)
        VALr = Rot(K, 1, [128, 8, 16], F32, "fVAL", es=es)
        CIr = Rot(K, 1, [128, 3, 128], U32, "fCI", es=es)
        ABr = Rot(K, 1, [128, 2, 128], F32, "fAB", es=es)
        OHr = CANDr
        E12r = Rot(K, 1, [128, 3, 128], F32, "fE12", es=es)
        IDSr = Rot(K, 2, [128, 128], I32, "fIDS", es=es)
        GTr = Rot(K, 2, [128, 4, 128], F32, "fGT", es=es)
        S8r = Rot(K, 2, [128, 16], F32, "fS8", es=es)
        GRP = cfg.get("grp", 4)
        ACTDOT = tuple(cfg.get("actdot", (0, 2)))
        if isinstance(cfg.get("actdot_mask"), int):
            ACTDOT = tuple(i for i in range(GRP) if (cfg["actdot_mask"] >> i) & 1)
        junk3r = Rot(K, 2, [128, D], BF16, "fjunk3", es=es)
        junkr = Rot(K, 3, [128, D], BF16, "fjunkr", es=es)
        PRDr = Rot(K, 3, [128, D], BF16, "fPRD", es=es)
        GBr = Rot(K, cfg.get("ngbuf", 22), [128, 2 * D], BF16, "fGB", es=es)
        junk2 = K.sb([128, D], BF16, "fjunk2", es)
        d_junk2 = Dep()
        XHbr = Rot(K, 2, [128, D], BF16, "fXHb", es=es)
        DGr = Rot(K, 4, [128, 128], BF16, "fDG", es=es)
        PY = Rot(K, 1, [128, 2, 512], F32, "fPY", psum=True, es=es)

        RES = {}

        def routing(ti):
            t0 = ti * 128
            XT, dXT = XTr.next()
            XH, dXH = XHr.next()
            X1, dX1 = X1r.next()
            K.sp.dma(lambda e, XT=XT, t0=t0: e.dma_start(out=XT[:], in_=dap(scr["xhT_fm"], t0, [[T, 128], [128 * T, 8], [1, 128]])),
                     reads=[scr["d_p4"]], writes=[dXT])
            K.sp.dma(lambda e, XH=XH, t0=t0: e.dma_start(out=XH[:], in_=scr["xh_tm"][t0:t0 + 128, :]), reads=[scr["d_p4"]], writes=[dXH])
            K.sp.dma(lambda e, X1=X1, t0=t0: e.dma_start(out=X1[:], in_=scr["x1_tm"][t0:t0 + 128, :]), reads=[scr["d_p4"]], writes=[dX1])
            QT, dQT = QTr.next()
            for half in range(2):
                pq, dpq = PQ.next()
                for i in range(8):
                    hc = half * 8 + i
                    for kc in range(8):
                        K.pe.op(lambda e, pq=pq, i=i, hc=hc, kc=kc, XT=XT: e.matmul(
                            pq[:, i, :], WQ[:, kc, hc * 128:(hc + 1) * 128], XT[:, kc, :], start=(kc == 0), stop=(kc == 7)),
                            reads=[dXT, d_c], writes=[dpq] if (i == 0 and kc == 0) else [], adds=[] if (i == 0 and kc == 0) else [dpq])
                K.act.op(lambda e, pq=pq, QT=QT, half=half: e.activation(out=QT[:, half * 8:(half + 1) * 8, :], in_=pq[:], func=AF.Copy),
                         reads=[dpq], writes=[dQT] if half == 0 else [], adds=[] if half == 0 else [dQT])
            SC, dSC = SCr.next()
            for half in range(2):
                psc, dpsc = PSc.next()
                for i in range(8):
                    hc = half * 8 + i
                    K.pe.op(lambda e, psc=psc, i=i, hc=hc, QT=QT: e.matmul(psc[:, i, :], QT[:, hc, :], KT[:, hc, :], start=True, stop=True),
                            reads=[dQT, d_c], writes=[dpsc] if i == 0 else [], adds=[] if i == 0 else [dpsc])
                K.act.op(lambda e, psc=psc, SC=SC, half=half: e.activation(out=SC[:, half * 8:(half + 1) * 8, :], in_=psc[:], func=AF.Copy),
                         reads=[dpsc], writes=[dSC] if half == 0 else [], adds=[] if half == 0 else [dSC])
            yield
            M16, dM = M16r.next()
            I16, dI = I16r.next()
            for hc in range(16):
                if hc % 4 == 0 and hc > 0:
                    yield
                K.dve.op(lambda e, M16=M16, SC=SC, hc=hc: e.max(out=M16[:, hc, 0:8], in_=SC[:, hc, :]), reads=[dSC],
                         writes=[dM] if hc == 0 else [], adds=[] if hc == 0 else [dM])
                K.dve.op(lambda e, M16=M16, SC=SC, hc=hc: e.match_replace(out=SC2[:, 0:128], in_to_replace=M16[:, hc, 0:8],
                                                                         in_values=SC[:, hc, :], imm_value=-1e30),
                         reads=[dSC, dM], writes=[dSC2])
                K.dve.op(lambda e, M16=M16, hc=hc: e.max(out=M16[:, hc, 8:16], in_=SC2[:, 0:128]), reads=[dSC2], adds=[dM])
                K.dve.op(lambda e, M16=M16, I16=I16, SC=SC, hc=hc: e.max_index(out=I16[:, hc, 0:8], in_max=M16[:, hc, 0:8],
                                                                              in_values=SC[:, hc, :]),
                         reads=[dSC, dM], writes=[dI] if hc == 0 else [], adds=[] if hc == 0 else [dI])
                K.dve.op(lambda e, M16=M16, I16=I16, SC=SC, hc=hc: e.max_index(out=I16[:, hc, 8:16], in_max=M16[:, hc, 8:16],
                                                                              in_values=SC[:, hc, :]),
                         reads=[dSC, dM], adds=[dI])
            yield
            I16f, dIf = I16fr.next()
            K.dve.op(lambda e, I16f=I16f, I16=I16: e.tensor_copy(out=I16f[:], in_=I16[:]), reads=[dI], writes=[dIf])
            I16fv = I16f[:].rearrange("p (h c) k -> p h c k", c=2)
            K.dve.op(lambda e, I16fv=I16fv: e.tensor_scalar(out=I16fv[:, :, 0, :], in0=I16fv[:, :, 0, :], scalar1=128.0, scalar2=None,
                                                            op0=ALU.mult), writes=[dIf])
            CAND, dCA = CANDr.next()
            M16v = M16[:].rearrange("p (h c) k -> p h c k", c=2)
            K.dve.op(lambda e, CAND=CAND, M16v=M16v: e.tensor_tensor(
                out=CAND[:].rearrange("p h (a b) -> p h a b", b=16),
                in0=M16v[:, :, 0, :].unsqueeze(3).broadcast_to([128, 8, 16, 16]),
                in1=M16v[:, :, 1, :].unsqueeze(2).broadcast_to([128, 8, 16, 16]), op=ALU.add),
                reads=[dM], writes=[dCA])
            VAL, dVAL = VALr.next()
            CI, dCI = CIr.next()
            CIv = CI[:, 0, :].rearrange("p (h k) -> p h k", k=16)
            for h in range(8):
                if h % 4 == 0:
                    yield
                K.dve.op(lambda e, VAL=VAL, CAND=CAND, h=h: e.max(out=VAL[:, h, 0:8], in_=CAND[:, h, :]), reads=[dCA],
                         writes=[dVAL] if h == 0 else [], adds=[] if h == 0 else [dVAL])
                K.dve.op(lambda e, VAL=VAL, CAND=CAND, h=h: e.match_replace(out=SC2[:, :], in_to_replace=VAL[:, h, 0:8],
                                                                           in_values=CAND[:, h, :], imm_value=-1e30),
                         reads=[dCA, dVAL], writes=[dSC2])
                K.dve.op(lambda e, VAL=VAL, h=h: e.max(out=VAL[:, h, 8:16], in_=SC2[:, :]), reads=[dSC2], adds=[dVAL])
                K.dve.op(lambda e, VAL=VAL, CIv=CIv, CAND=CAND, h=h: e.max_index(out=CIv[:, h, 0:8], in_max=VAL[:, h, 0:8],
                                                                                in_values=CAND[:, h, :]),
                         reads=[dCA, dVAL], writes=[dCI] if h == 0 else [], adds=[] if h == 0 else [dCI])
                K.dve.op(lambda e, VAL=VAL, CIv=CIv, CAND=CAND, h=h: e.max_index(out=CIv[:, h, 8:16], in_max=VAL[:, h, 8:16],
                                                                                in_values=CAND[:, h, :]),
                         reads=[dCA, dVAL], adds=[dCI])
            yield
            GT, dGT = GTr.next()
            S8, dS8 = S8r.next()
            Ev = GT[:, 0, :].rearrange("p (h k) -> p h k", k=16)
            Gv = GT[:, 1, :].rearrange("p (h k) -> p h k", k=16)
            K.dve.op(lambda e, Ev=Ev, VAL=VAL: e.tensor_tensor(out=Ev, in0=VAL[:], in1=VAL[:, :, 0:1].broadcast_to([128, 8, 16]),
                                                              op=ALU.subtract), reads=[dVAL], writes=[dGT])
            K.act.op(lambda e, GT=GT: e.activation(out=GT[:, 0, :], in_=GT[:, 0, :], func=AF.Exp), writes=[dGT])
            K.dve.op(lambda e, Ev=Ev, S8=S8: e.tensor_reduce(out=S8[:, 0:8], in_=Ev, axis=AX.X, op=ALU.add), reads=[dGT], writes=[dS8])
            K.dve.op(lambda e, S8=S8: e.reciprocal(out=S8[:, 8:16], in_=S8[:, 0:8]), writes=[dS8])
            K.dve.op(lambda e, Ev=Ev, Gv=Gv, S8=S8: e.tensor_tensor(out=Gv, in0=Ev, in1=S8[:, 8:16].unsqueeze(2).broadcast_to([128, 8, 16]),
                                                                   op=ALU.mult), reads=[dS8], writes=[dGT])
            yield
            K.dve.op(lambda e, CI=CI: e.tensor_single_scalar(out=CI[:, 1, :], in_=CI[:, 0, :], scalar=4, op=ALU.logical_shift_right),
                     writes=[dCI])
            K.dve.op(lambda e, CI=CI: e.tensor_single_scalar(out=CI[:, 2, :], in_=CI[:, 0, :], scalar=15, op=ALU.bitwise_and),
                     writes=[dCI])
            AB, dAB = ABr.next()
            K.dve.op(lambda e, AB=AB, CI=CI: e.tensor_copy(out=AB[:], in_=CI[:, 1:3, :]), reads=[dCI], writes=[dAB])
            OH, dOH = OHr.next()
            E12, dE12 = E12r.next()
            OHv = OH[:].rearrange("p h (j a) -> p h j a", a=16)
            for c in range(2):
                ABv = AB[:, c, :].rearrange("p (h j) -> p h j", j=16)
                K.dve.op(lambda e, OHv=OHv, ABv=ABv: e.tensor_tensor(
                    out=OHv, in0=ABv.unsqueeze(3).broadcast_to([128, 8, 16, 16]),
                    in1=IOTA[:].unsqueeze(1).unsqueeze(1).broadcast_to([128, 8, 16, 16]), op=ALU.is_equal),
                    reads=[dAB, d_c], writes=[dOH])
                K.dve.op(lambda e, OHv=OHv, I16fv=I16fv, c=c: e.tensor_tensor(
                    out=OHv, in0=OHv, in1=I16fv[:, :, c, :].unsqueeze(2).broadcast_to([128, 8, 16, 16]), op=ALU.mult),
                    reads=[dIf], writes=[dOH])
                K.dve.op(lambda e, OHv=OHv, E12=E12, c=c: e.tensor_reduce(
                    out=E12[:, c, :].rearrange("p (h j) -> p h j", j=16), in_=OHv, axis=AX.X, op=ALU.add),
                    reads=[dOH], writes=[dE12] if c == 0 else [], adds=[] if c == 0 else [dE12])
            K.dve.op(lambda e, E12=E12: e.tensor_tensor(out=E12[:, 2, :], in0=E12[:, 0, :], in1=E12[:, 1, :], op=ALU.add), writes=[dE12])
            IDS, dIDS = IDSr.next()
            K.dve.op(lambda e, IDS=IDS, E12=E12: e.tensor_copy(out=IDS[:], in_=E12[:, 2, :]), reads=[dE12], writes=[dIDS])
            if "ids_dbg" in scr:
                K.sp.dma(lambda e, IDS=IDS, t0=t0: e.dma_start(out=scr["ids_dbg"][t0:t0 + 128, :], in_=IDS[:]), reads=[dIDS])
                K.sp.dma(lambda e, GT=GT, t0=t0: e.dma_start(out=scr["gate_dbg"][t0:t0 + 128, :], in_=GT[:, 1, :]), reads=[dGT])
            RES[ti] = dict(t0=t0, XH=XH, dXH=dXH, X1=X1, dX1=dX1, IDS=IDS, dIDS=dIDS, GT=GT, dGT=dGT, S8=S8, dS8=dS8)

        GDEPS = {}

        def expert(R):
            t0, XH, dXH, X1, dX1, IDS, dIDS, GT, dGT, S8, dS8 = (R[k] for k in
                ("t0", "XH", "dXH", "X1", "dX1", "IDS", "dIDS", "GT", "dGT", "S8", "dS8"))
            XHb, dXHb = XHbr.next()
            K.act.op(lambda e: e.activation(out=XHb[:], in_=XH[:], func=AF.Copy), reads=[dXH], writes=[dXHb])
            py, dpy = PY.next()
            NGRP = 128 // GRP
            bufs = {}
            gd = GDEPS.setdefault(id(GT), [(Dep(), Dep()) for _ in range(NGRP)])

            def stage_a(g):
                for jj in range(GRP):
                    j = g * GRP + jj
                    GB, dGB = GBr.next()
                    bufs[j] = (GB, dGB)
                    K.pool.dma(lambda e, GB=GB, j=j: e.indirect_dma_start(
                        out=GB[:], out_offset=None, in_=scr["uv_tab"][:, :],
                        in_offset=bass.IndirectOffsetOnAxis(ap=IDS[:, j:j + 1], axis=0)), reads=[dIDS, scr["d_uv"]], writes=[dGB])
                    if jj in ACTDOT:
                        PRD, dPRD = PRDr.next()
                        K.dve.op(lambda e, GB=GB, PRD=PRD: e.tensor_tensor(out=PRD[:], in0=GB[:, 0:D], in1=XHb[:], op=ALU.mult),
                                 reads=[dGB, dXHb], writes=[dPRD])
                        j3, dj3 = junk3r.next()
                        K.act.op(lambda e, PRD=PRD, j=j, j3=j3: e.activation(out=j3[:], in_=PRD[:], func=AF.Copy, accum_out=GT[:, 2, j:j + 1]),
                                 reads=[dPRD], writes=[dj3] + ([gd[g][0]] if jj == 0 else []), adds=[] if jj == 0 else [gd[g][0]])
                    else:
                        j1, dj1 = junkr.next()
                        K.dve.op(lambda e, GB=GB, j=j, j1=j1: e.scalar_tensor_tensor(
                            out=j1[:], in0=GB[:, 0:D], scalar=1.0, in1=XHb[:], op0=ALU.mult, op1=ALU.mult, accum_out=GT[:, 2, j:j + 1]),
                            reads=[dGB, dXHb], writes=[dj1] + ([gd[g][0]] if jj == 0 else []), adds=[] if jj == 0 else [gd[g][0]])
                gs = slice(g * GRP, (g + 1) * GRP)
                K.act.op(lambda e: e.activation(out=GT[:, 3, gs], in_=GT[:, 2, gs], func=AF.Gelu), reads=[gd[g][0]], writes=[gd[g][1]])

            def stage_b(g):
                gs = slice(g * GRP, (g + 1) * GRP)
                K.dve.op(lambda e: e.tensor_tensor(out=GT[:, 3, gs], in0=GT[:, 3, gs], in1=GT[:, 1, gs], op=ALU.mult), reads=[dGT], writes=[gd[g][1]])
                for jj in range(GRP):
                    j = g * GRP + jj
                    GB, dGB = bufs.pop(j)
                    DG, dDG = DGr.next()
                    K.act.op(lambda e, DG=DG, j=j: e.activation(out=DG[:], in_=identb[:], func=AF.Copy, scale=GT[:, 3, j:j + 1]),
                             reads=[gd[g][1], d_c], writes=[dDG])
                    for n in range(2):
                        K.pe.op(lambda e, DG=DG, GB=GB, n=n, j=j: e.matmul(
                            py[:, n, :], DG[:], GB[:, D + n * 512:D + (n + 1) * 512], start=(j == 0), stop=(j == 127)),
                            reads=[dDG, dGB], writes=[dpy] if (j == 0 and n == 0) else [], adds=[] if (j == 0 and n == 0) else [dpy])

            gen = routing(R["next"]) if R.get("next") is not None else None
            for g in range(NGRP):
                stage_a(g)
                if g >= 1:
                    stage_b(g - 1)
                if gen is not None and g >= 2 and g % 2 == 0:
                    try:
                        next(gen)
                    except StopIteration:
                        gen = None
            stage_b(NGRP - 1)
            if gen is not None:
                for _ in gen:
                    pass
            K.dve.op(lambda e: e.tensor_tensor(out=X1[:], in0=X1[:], in1=py[:].rearrange("p a b -> p (a b)"), op=ALU.add),
                     reads=[dpy], writes=[dX1])
            K.act.op(lambda e: e.activation(out=junk2[:], in_=X1[:], func=AF.Square, accum_out=S8[:, 0:1]),
                     reads=[dX1], writes=[d_junk2, dS8])
            K.dve.op(lambda e: e.tensor_scalar(out=S8[:, 1:2], in0=S8[:, 0:1], scalar1=1.0 / D, scalar2=1e-6,
                                               op0=ALU.mult, op1=ALU.add), writes=[dS8])
            K.act.op(lambda e: e.activation(out=S8[:, 2:3], in_=S8[:, 1:2], func=AF.Sqrt), writes=[dS8])
            K.dve.op(lambda e: e.reciprocal(out=S8[:, 3:4], in_=S8[:, 2:3]), writes=[dS8])
            K.dve.op(lambda e: e.scalar_tensor_tensor(
                out=XH[:], in0=X1[:], scalar=S8[:, 3:4], in1=FNW[:], op0=ALU.mult, op1=ALU.mult),
                reads=[dX1, dS8, d_c], writes=[dXH])
            K.sp.dma(lambda e: e.dma_start(out=io["out"][t0:t0 + 128, :], in_=XH[:]), reads=[dXH])

        for _ in routing(0):
            pass
        for ti in range(NTT):
            R = RES.pop(ti)
            R["next"] = ti + 1 if ti + 1 < NTT else None
            expert(R)


def make_consts():
    c = {}
    c["ident_bf"] = np.eye(128, dtype=np.float32).astype(ml_dtypes.bfloat16)
    c["ident_f"] = np.eye(128, dtype=np.float32)
    bo = np.zeros((128, 128), np.float32)
    bo[:64, :64] = 1.0 / 64
    bo[64:, 64:] = 1.0 / 64
    c["blockones"] = bo
    pp = np.arange(128)[:, None]
    ff = np.arange(128)[None, :]
    c["tri"] = ((pp <= ff) & (pp // 64 == ff // 64)).astype(np.float32)
    p6 = np.arange(64)[:, None]
    f6 = np.arange(64)[None, :]
    mk = np.zeros((64, 3, 64), np.float32)
    mk[:, 0, :] = (f6 < p6)
    mk[:, 1, :] = (f6 > p6)
    mk[:, 2, :] = (f6 >= p6)
    c["masks"] = mk
    c["ident64"] = np.eye(64, dtype=np.float32).astype(ml_dtypes.bfloat16)
    c["ones64"] = np.ones((64, 1), np.float32)
    c["iota16"] = np.tile(np.arange(16, dtype=np.float32)[None, :], (128, 1))
    return c


def make_alibi(S):
    al = np.zeros((4, 128, S), np.float32)
    ql = np.arange(128)[:, None]
    m = np.arange(S)[None, :]
    for h in range(4):
        slope = 2.0 ** (-8.0 * (h + 1) / 4)
        v = -slope * (ql - m + (S - 128)).astype(np.float32)
        al[h] = np.where(m <= ql + (S - 128), v, -30000.0)
    return al


def _unused():
    c = {}
    return c


def build(cfg):
    NB, S = cfg["NB"], cfg["S"]
    T = NB * S
    cfg["T"] = T
    dbg = set(cfg.get("debug", ()))
    phases = cfg.get("phases", (1,))
    nc = bass.Bass("TRN2", target_bir_lowering=False)
    io = {}

    def inp(name, shape, dt=F32):
        io[name] = nc.dram_tensor(name, list(shape), dt, kind="ExternalInput").ap()

    inp("x", [T, D])
    inp("norm_mix_w", [1, D])
    inp("w_in", [1, D, IN_COLS])
    inp("ident_bf", [128, 128], BF16)
    inp("ident_f", [128, 128])
    inp("blockones", [128, 128])
    inp("tri", [128, 128])
    inp("masks", [64, 3, 64])
    inp("ident64", [64, 64], BF16)
    inp("ones64", [64, 1])
    inp("alibi", [4, 128, S])
    for nm, shp in [("lam_q1", [1, 64]), ("lam_k1", [1, 64]), ("lam_q2", [1, 64]), ("lam_k2", [1, 64]),
                    ("subln_w", [1, 128])]:
        inp(nm, shp)
    scr = {}

    def scratch(name, shape, dt):
        kind = "ExternalOutput" if name in dbg else "Internal"
        scr[name] = nc.dram_tensor(name, list(shape), dt, kind=kind).ap()

    scratch("zs_tm", [T, SHIFT_COLS], F32)
    scratch("zv_fm", [512, T], F32)
    scratch("qk_fm", [1024, T], BF16)
    scratch("av_tm", [T, 512], BF16)
    scratch("sg_fm", [2048, T], BF16)
    if not cfg.get("chunked", True):
        scratch("rw_tm", [T, 5, 512], F32)
    scratch("ab_tm", [T, 5, 512], BF16)
    scratch("lw_tm", [T, 512], F32)
    scratch("v_fm", [512, T], F32)
    scratch("g_fm", [512, T], F32)
    scratch("coef_fm", [8, T], F32)
    scratch("y_fm", [512, T], F32)
    scratch("ya_fm", [512, T], BF16)
    scr["d_p1"] = Dep()
    scr["d_p2a"] = Dep()
    scr["d_p2b"] = Dep()
    scr["d_p2c"] = Dep()
    scr["d_p3"] = Dep()
    scr["d_p4"] = Dep()
    scr["d_uv"] = Dep()
    scratch("uv_tab", [16384, 2 * D], BF16)
    if "ids_dbg" in dbg:
        scratch("ids_dbg", [T, 128], I32)
        scratch("gate_dbg", [T, 128], F32)
    inp("peer_wq", [1, D, 2048])
    inp("peer_keys", [1, 8, 2, 128, 128])
    inp("peer_u", [1, 16384, D])
    inp("peer_v", [1, 16384, D])
    inp("final_norm_w", [1, D])
    inp("iota16", [128, 16])
    io["out"] = nc.dram_tensor("out", [T, D], F32, kind="ExternalOutput").ap()
    scratch("x1_tm", [T, D], F32)
    scratch("xh_tm", [T, D], F32)
    scratch("xhT_fm", [D, T], BF16)
    for nm, shp in [("proj_a", [1, 512, D]), ("proj_b", [1, 512, D]), ("w_out", [1, D, D]), ("norm_ffn_w", [1, D])]:
        inp(nm, shp)
    scratch("yb_fm", [512, T], BF16)
    for nm, shp in [("shift_mu", [1, SHIFT_COLS]), ("w0", [1, 512]), ("w2", [1, 64, 512]), ("a0", [1, 512]),
                    ("a2", [1, 64, 512]), ("g2", [1, 128, 512]), ("k_k", [1, 512]), ("k_a", [1, 512]),
                    ("r_k", [1, 8, 64]), ("lnx_w", [1, 512]), ("lnx_b", [1, 512])]:
        inp(nm, shp)
    with ExitStack() as es:
        K = Kern(nc, es, pool_slots=cfg.get("pool_slots", 8))
        K.scopes = bool(cfg.get("scopes", False))
        if 5 in phases:
            K.phase = "p0_uvtab"
            RB = 2048
            for r0 in range(0, 16384, RB):
                K.pool.dma(lambda e, r0=r0: e.dma_start(out=scr["uv_tab"][r0:r0 + RB, 0:D], in_=io["peer_u"][0, r0:r0 + RB, :]),
                           adds=[scr["d_uv"]])
                K.pool.dma(lambda e, r0=r0: e.dma_start(out=scr["uv_tab"][r0:r0 + RB, D:2 * D], in_=io["peer_v"][0, r0:r0 + RB, :]),
                           adds=[scr["d_uv"]])
        if 1 in phases:
            K.phase = "p1_inproj"
            phase1(K, cfg, io, scr)
            K.barrier()
        if 2 in phases:
            K.phase = "p2a_prep"
            phase2_prep(K, cfg, io, scr)
            K.barrier()
            K.phase = "p2b_scan"
            if cfg.get("chunked", True):
                phase2_chunk(K, cfg, io, scr)
            else:
                phase2_scan(K, cfg, io, scr)
            K.barrier()
            K.phase = "p2c_post"
            phase2_post(K, cfg, io, scr)
            K.barrier()
        if 3 in phases:
            K.phase = "p3_attn"
            phase3(K, cfg, io, scr)
            K.barrier()
        if 4 in phases:
            K.phase = "p4_merge"
            phase4(K, cfg, io, scr)
            K.barrier()
        if 5 in phases:
            K.phase = "p5_peer"
            phase5(K, cfg, io, scr)
            K.barrier()
        K.finish()
    return nc, io, scr


def kernel(**inputs):
    NB, S = 4, 2048
    cfg = dict(NB=NB, S=S, phases=(1, 2, 3, 4, 5))
    nc, io, scr = build(cfg)
    consts = make_consts()
    consts["alibi"] = make_alibi(S)
    x = np.ascontiguousarray(np.asarray(inputs["x"], dtype=np.float32))
    shared = {}
    for name in io:
        if name in ("x", "out"):
            continue
        if name in consts:
            shared[name] = consts[name]
        elif name == "final_norm_w":
            shared[name] = np.ascontiguousarray(np.asarray(inputs[name], dtype=np.float32).reshape(1, D))
        else:
            shared[name] = np.ascontiguousarray(np.asarray(inputs[name], dtype=np.float32))
    in_maps = []
    for c in range(NCORES):
        m = dict(shared)
        m["x"] = x[c * NB:(c + 1) * NB].reshape(NB * S, D)
        in_maps.append(m)
    res = run_bass_kernel_spmd(nc, in_maps, core_ids=list(range(NCORES)))
    out = np.concatenate([np.asarray(r["out"]).reshape(NB, S, D) for r in res.results], axis=0)
    return out.astype(np.float32)
```
